# Optimizing a Trainium2 kernel written in Bass

```python
import math
import jax, jax.numpy as jnp
from jax import lax
import numpy as np

D_MODEL = 1024
BATCH = 8
SEQ = 8192
DEPTH = 2

HEAD_DIM = 64
NSA_HEADS = 8
NSA_KV_HEADS = 2
NSA_GROUP = NSA_HEADS // NSA_KV_HEADS
RET_HEADS = 8
CMP_BLOCK = 32
CMP_STRIDE = 16
CMP_HIDDEN = 256
SEL_BLOCK = 64
N_SELECT = 16
N_LOCAL_SEL = 2
WINDOW = 512
Q_BLOCK = 128
RET_CHUNK = 128
D_FF = 3584
N_EXPERTS = 8
TOP_K = 2
PLE_DIM = 256
EPS = 1e-6
NEG_INF = -1e30
BIG = 1e9
N_DENSE = (DEPTH + 1) // 2
N_MOE = DEPTH // 2

NSA_Q_COLS = NSA_HEADS * HEAD_DIM
NSA_KV_COLS = NSA_KV_HEADS * HEAD_DIM
NSA_GATE_COLS = 3 * NSA_HEADS
RET_COLS = RET_HEADS * HEAD_DIM
SPLIT_SIZES = (NSA_Q_COLS,) + (NSA_KV_COLS,) * 6 + (NSA_GATE_COLS,) + (RET_COLS,) * 4
IN_COLS = NSA_Q_COLS + 6 * NSA_KV_COLS + NSA_GATE_COLS + 4 * RET_COLS
MIX_WIDTH = NSA_Q_COLS + RET_COLS

kernel_name = "hybrid_nsa_retention_moe_ple"


def rmsnorm(x, g):
    xf = x.astype(jnp.float32)
    y = xf * lax.rsqrt(jnp.mean(xf * xf, axis=-1, keepdims=True) + EPS)
    return (y * g.astype(jnp.float32)).astype(x.dtype)


def alibi_slopes(n):
    return jnp.exp2(-8.0 * jnp.arange(1, n + 1, dtype=jnp.float32) / n)


def masked_softmax(logits, valid):
    l = jnp.where(valid, logits, NEG_INF)
    m = jnp.max(l, axis=-1, keepdims=True)
    e = jnp.where(valid, jnp.exp(l - m), 0.0)
    return e / jnp.maximum(jnp.sum(e, axis=-1, keepdims=True), 1e-30)


def compress(k, pos, w1, w2):
    b, g, s, d = k.shape
    r = CMP_BLOCK // CMP_STRIDE
    nch = s // CMP_STRIDE
    nc = nch - r + 1
    chunks = k.reshape(b, g, nch, CMP_STRIDE, d)
    w1r = w1.reshape(r, CMP_STRIDE, d, CMP_HIDDEN)
    u = jnp.einsum('bgncd,rcdh->bgnrh', chunks, w1r)
    hid = jnp.einsum('ld,ldh->h', pos, w1)
    for a in range(r):
        hid = hid + u[:, :, a:a + nc, a, :]
    return jnp.einsum('bgnh,hd->bgnd', jax.nn.gelu(hid), w2)


def nsa_mixer(q, kc, vc, ks, vs, kw, vw, gate_logits, cmp_pos, cmp_w1, cmp_w2):
    f32 = jnp.float32
    b, s, _, d = q.shape
    G, R = NSA_KV_HEADS, NSA_GROUP
    q = q.astype(f32) * (d ** -0.5)
    to_bgsd = lambda t: jnp.transpose(t.astype(f32), (0, 2, 1, 3))
    kc, vc, ks, vs, kw, vw = [to_bgsd(t) for t in (kc, vc, ks, vs, kw, vw)]
    k_cmp = compress(kc, cmp_pos[0].astype(f32), cmp_w1[0].astype(f32), cmp_w2[0].astype(f32))
    v_cmp = compress(vc, cmp_pos[1].astype(f32), cmp_w1[1].astype(f32), cmp_w2[1].astype(f32))
    nc = k_cmp.shape[2]
    nsel = s // SEL_BLOCK
    n_pick = min(N_SELECT, nsel)
    cmp_start = jnp.arange(nc) * CMP_STRIDE
    cmp_end = cmp_start + CMP_BLOCK - 1
    sel_start = jnp.arange(nsel) * SEL_BLOCK
    overlap = (jnp.minimum(cmp_start[:, None] + CMP_BLOCK, sel_start[None, :] + SEL_BLOCK)
               > jnp.maximum(cmp_start[:, None], sel_start[None, :])).astype(f32)
    ks_blk = ks.reshape(b, G, nsel, SEL_BLOCK, d)
    vs_blk = vs.reshape(b, G, nsel, SEL_BLOCK, d)
    kw_pad = jnp.pad(kw, ((0, 0), (0, 0), (WINDOW, 0), (0, 0)))
    vw_pad = jnp.pad(vw, ((0, 0), (0, 0), (WINDOW, 0), (0, 0)))
    slopes = alibi_slopes(NSA_HEADS).reshape(G, R)
    gates = jax.nn.sigmoid(gate_logits.astype(f32)).reshape(b, s, 3, G, R)
    gather = jax.vmap(jax.vmap(lambda blocks, ix: blocks[ix]))
    blk = jnp.arange(nsel)

    def block(i):
        t0 = i * Q_BLOCK
        t = t0 + jnp.arange(Q_BLOCK)
        qb = lax.dynamic_slice_in_dim(q, t0, Q_BLOCK, axis=1).reshape(b, Q_BLOCK, G, R, d)
        dist_c = t[:, None] - cmp_end[None, :]
        s_c = jnp.einsum('bqgrd,bgnd->bgrqn', qb, k_cmp)
        p_c = masked_softmax(s_c - slopes[:, :, None, None] * dist_c.astype(f32), dist_c >= 0)
        o_c = jnp.einsum('bgrqn,bgnd->bgrqd', p_c, v_cmp)
        imp = jnp.einsum('bgrqn,nm->bgqm', p_c, overlap)
        back = (t // SEL_BLOCK)[:, None] - blk[None, :]
        forced = (blk[None, :] == 0) | ((back >= 0) & (back < N_LOCAL_SEL))
        score = jnp.where(forced, BIG, jnp.where(back >= 0, imp, -BIG))
        _, idx = lax.top_k(score, n_pick)
        k_pick = gather(ks_blk, idx)
        v_pick = gather(vs_blk, idx)
        pos = idx[..., None] * SEL_BLOCK + jnp.arange(SEL_BLOCK)
        dist_s = t[None, None, :, None, None] - pos
        s_s = jnp.einsum('bqgrd,bgqkld->bgrqkl', qb, k_pick)
        logit_s = (s_s - slopes[None, :, :, None, None, None] * dist_s[:, :, None].astype(f32))
        logit_s = logit_s.reshape(b, G, R, Q_BLOCK, n_pick * SEL_BLOCK)
        valid_s = (dist_s >= 0)[:, :, None].reshape(b, G, 1, Q_BLOCK, n_pick * SEL_BLOCK)
        p_s = masked_softmax(logit_s, valid_s).reshape(b, G, R, Q_BLOCK, n_pick, SEL_BLOCK)
        o_s = jnp.einsum('bgrqkl,bgqkld->bgrqd', p_s, v_pick)
        kwb = lax.dynamic_slice_in_dim(kw_pad, t0, WINDOW + Q_BLOCK, axis=2)
        vwb = lax.dynamic_slice_in_dim(vw_pad, t0, WINDOW + Q_BLOCK, axis=2)
        kpos = t0 - WINDOW + jnp.arange(WINDOW + Q_BLOCK)
        dist_w = t[:, None] - kpos[None, :]
        valid_w = (dist_w >= 0) & (dist_w < WINDOW) & (kpos[None, :] >= 0)
        s_w = jnp.einsum('bqgrd,bgkd->bgrqk', qb, kwb)
        p_w = masked_softmax(s_w - slopes[:, :, None, None] * dist_w.astype(f32), valid_w)
        o_w = jnp.einsum('bgrqk,bgkd->bgrqd', p_w, vwb)
        gb = lax.dynamic_slice_in_dim(gates, t0, Q_BLOCK, axis=1)
        gb = jnp.transpose(gb, (2, 0, 3, 4, 1))[..., None]
        o = gb[0] * o_c + gb[1] * o_s + gb[2] * o_w
        return jnp.transpose(o, (0, 3, 1, 2, 4)).reshape(b, Q_BLOCK, NSA_HEADS * d)

    out = lax.map(block, jnp.arange(s // Q_BLOCK))
    return jnp.transpose(out, (1, 0, 2, 3)).reshape(b, s, NSA_HEADS * d)


def retention(q, k, v, g, gn_gain):
    f32 = jnp.float32
    b, s, h, d = q.shape
    C = RET_CHUNK
    n = s // C
    log_gamma = jnp.log1p(-jnp.exp2(-5.0 - jnp.arange(h, dtype=f32)))
    pos = jnp.arange(C, dtype=f32)
    diff = pos[:, None] - pos[None, :]
    decay_in = jnp.where(diff >= 0, jnp.exp(jnp.maximum(diff, 0.0)[None] * log_gamma[:, None, None]), 0.0)
    xi = jnp.exp((pos + 1.0)[None] * log_gamma[:, None])[..., None]
    zeta = jnp.exp((C - 1.0 - pos)[None] * log_gamma[:, None])[..., None]
    g_chunk = jnp.exp(C * log_gamma)[:, None, None]

    def chunks(t):
        return jnp.transpose(t.astype(f32).reshape(b, n, C, h, d), (1, 0, 3, 2, 4))

    qc, kc, vc = chunks(q), chunks(k) * (d ** -0.5), chunks(v)

    def step(state, inp):
        qi, ki, vi = inp
        inner = jnp.einsum('bhnd,bhmd->bhnm', qi, ki) * decay_in
        o = (jnp.einsum('bhnm,bhme->bhne', inner, vi)
             + jnp.einsum('bhnd,bhde->bhne', qi, state) * xi)
        state = g_chunk * state + jnp.einsum('bhmd,bhme->bhde', ki * zeta, vi)
        return state, o

    state0 = jnp.zeros((b, h, d, d), f32)
    _, o = lax.scan(step, state0, (qc, kc, vc))
    o = jnp.transpose(o, (1, 0, 3, 2, 4)).reshape(b, s, h, d)
    mu = jnp.mean(o, axis=-1, keepdims=True)
    var = jnp.mean(jnp.square(o - mu), axis=-1, keepdims=True)
    o = ((o - mu) * lax.rsqrt(var + EPS)).reshape(b, s, h * d) * gn_gain.astype(f32)
    return (jax.nn.silu(g.astype(f32)).reshape(b, s, h * d) * o).astype(q.dtype)


def swiglu(x, wg, wu, wd):
    return (jax.nn.silu(x @ wg) * (x @ wu)) @ wd


def moe(x, router, wg, wu, wd):
    b, s, dm = x.shape
    xt = x.reshape(-1, dm)
    logits = (xt @ router).astype(jnp.float32)
    top_v, top_i = lax.top_k(logits, TOP_K)
    w = jax.nn.softmax(top_v, axis=-1)
    gate = jnp.sum(jax.nn.one_hot(top_i, N_EXPERTS, dtype=jnp.float32) * w[..., None], axis=1)
    out = jnp.zeros(xt.shape, jnp.float32)
    for e in range(N_EXPERTS):
        out = out + gate[:, e:e + 1] * swiglu(xt, wg[e], wu[e], wd[e]).astype(jnp.float32)
    return out.astype(x.dtype).reshape(b, s, dm)


def setup_inputs(seed: int = 0) -> dict:
    key = jax.random.key(seed)
    k = jax.random.split(key, 21)
    nrm = lambda kk, shape, scale: jax.random.normal(kk, shape, jnp.float32) * scale
    gain = lambda kk, shape: 1.0 + 0.01 * jax.random.normal(kk, shape, jnp.float32)
    return {
        "x": nrm(k[0], (BATCH, SEQ, D_MODEL), 1.0),
        "p": nrm(k[1], (DEPTH, BATCH, SEQ, PLE_DIM), 1.0),
        "w_in": nrm(k[2], (DEPTH, D_MODEL, IN_COLS), D_MODEL ** -0.5),
        "w_out": nrm(k[3], (DEPTH, MIX_WIDTH, D_MODEL), MIX_WIDTH ** -0.5),
        "g_mix": gain(k[4], (DEPTH, D_MODEL)),
        "g_ffn": gain(k[5], (DEPTH, D_MODEL)),
        "g_ple": gain(k[6], (DEPTH, D_MODEL)),
        "g_final": gain(k[7], (D_MODEL,)),
        "cmp_pos": nrm(k[8], (DEPTH, 2, CMP_BLOCK, HEAD_DIM), 0.1),
        "cmp_w1": nrm(k[9], (DEPTH, 2, CMP_BLOCK, HEAD_DIM, CMP_HIDDEN), (CMP_BLOCK * HEAD_DIM) ** -0.5),
        "cmp_w2": nrm(k[10], (DEPTH, 2, CMP_HIDDEN, HEAD_DIM), CMP_HIDDEN ** -0.5),
        "ret_gn": gain(k[11], (DEPTH, RET_COLS)),
        "ffn_gate": nrm(k[12], (N_DENSE, D_MODEL, D_FF), D_MODEL ** -0.5),
        "ffn_up": nrm(k[13], (N_DENSE, D_MODEL, D_FF), D_MODEL ** -0.5),
        "ffn_down": nrm(k[14], (N_DENSE, D_FF, D_MODEL), D_FF ** -0.5),
        "moe_router": nrm(k[15], (N_MOE, D_MODEL, N_EXPERTS), D_MODEL ** -0.5),
        "moe_gate": nrm(k[16], (N_MOE, N_EXPERTS, D_MODEL, D_FF), D_MODEL ** -0.5),
        "moe_up": nrm(k[17], (N_MOE, N_EXPERTS, D_MODEL, D_FF), D_MODEL ** -0.5),
        "moe_down": nrm(k[18], (N_MOE, N_EXPERTS, D_FF, D_MODEL), D_FF ** -0.5),
        "ple_proj": nrm(k[19], (DEPTH, PLE_DIM, D_MODEL), PLE_DIM ** -0.5),
        "ple_gate": nrm(k[20], (DEPTH, D_MODEL, D_MODEL), D_MODEL ** -0.5),
    }


def reference(x, p, w_in, w_out, g_mix, g_ffn, g_ple, g_final, cmp_pos, cmp_w1, cmp_w2, ret_gn,
              ffn_gate, ffn_up, ffn_down, moe_router, moe_gate, moe_up, moe_down, ple_proj, ple_gate):
    b, s, _ = x.shape
    split_points = np.cumsum(SPLIT_SIZES)[:-1].tolist()
    h = x
    for i in range(DEPTH):
        hn = rmsnorm(h, g_mix[i])
        z = hn @ w_in[i]
        (q_n, kc, vc, ks, vs, kw, vw, gl, q_r, k_r, v_r, g_r) = jnp.split(z, split_points, axis=-1)
        kvr = lambda t: t.reshape(b, s, NSA_KV_HEADS, HEAD_DIM)
        rr = lambda t: t.reshape(b, s, RET_HEADS, HEAD_DIM)
        a = nsa_mixer(q_n.reshape(b, s, NSA_HEADS, HEAD_DIM), kvr(kc), kvr(vc), kvr(ks), kvr(vs),
                      kvr(kw), kvr(vw), gl, cmp_pos[i], cmp_w1[i], cmp_w2[i]).astype(h.dtype)
        r = retention(rr(q_r), rr(k_r), rr(v_r), rr(g_r), ret_gn[i])
        h = h + jnp.concatenate([a, r], axis=-1) @ w_out[i]
        hn = rmsnorm(h, g_ffn[i])
        if i % 2 == 0:
            f = swiglu(hn, ffn_gate[i // 2], ffn_up[i // 2], ffn_down[i // 2])
        else:
            f = moe(hn, moe_router[i // 2], moe_gate[i // 2], moe_up[i // 2], moe_down[i // 2])
        h = h + f
        h = h + (p[i] @ ple_proj[i]) * jax.nn.sigmoid(rmsnorm(h, g_ple[i]) @ ple_gate[i])
    return rmsnorm(h, g_final)
```

```python
import math
import os
from contextlib import ExitStack

import numpy as np
import concourse.bass as bass
import concourse.mybir as mybir
from concourse.bass_utils import run_bass_kernel_spmd

F32 = mybir.dt.float32
BF16 = mybir.dt.bfloat16
AF = mybir.ActivationFunctionType
ALU = mybir.AluOpType
AX = mybir.AxisListType

D = 1024
SEQ = 8192
NT = SEQ // 128
DEPTH = 2
HD = 64
IN_COLS = 3352
D_FF = 3584
NE = 8
EPS = 1e-6
NEG = -30000.0

C_QN, C_KC, C_VC, C_KS, C_VS, C_KW, C_VW, C_GL, C_QR, C_KR, C_VR, C_GR = (
    0, 512, 640, 768, 896, 1024, 1152, 1280, 1304, 1816, 2328, 2840)


class S:
    def __init__(self, nc, es, n_dma_sems=24):
        self.nc = nc
        self.eng = {"pe": nc.tensor, "dve": nc.vector, "act": nc.scalar,
                    "pool": nc.gpsimd, "sp": nc.sync}
        self.sem = {k: es.enter_context(nc.semaphore("c_" + k)) for k in self.eng}
        self.cnt = {k: 0 for k in self.eng}
        self.seen = {k: {} for k in self.eng}
        self.drained = {k: 0 for k in self.eng}
        self.dsem = [es.enter_context(nc.semaphore("d%d" % i)) for i in range(n_dma_sems)]
        self.dval = [0] * n_dma_sems
        self.dnext = 0
        self.lastw = {}
        self.readers = {}
        self.nops = 0

    def _wait(self, eng, tok):
        if tok is None:
            return
        if tok[0] == "c":
            _, e2, c = tok
            if e2 == eng:
                if eng != "pe" and c > self.drained[eng]:
                    self.eng[eng].drain()
                    self.drained[eng] = self.cnt[eng]
                return
            key = e2
            sem = self.sem[e2]
        else:
            _, si, c = tok
            key = ("d", si)
            sem = self.dsem[si]
        if self.seen[eng].get(key, 0) >= c:
            return
        self.eng[eng].wait_ge(sem, c)
        self.seen[eng][key] = c

    def _deps(self, eng, reads, writes):
        for r in reads:
            self._wait(eng, self.lastw.get(r))
        for w in writes:
            self._wait(eng, self.lastw.get(w))
            for t in self.readers.get(w, {}).values():
                self._wait(eng, t)

    def _commit(self, tok, reads, writes):
        for r in reads:
            self.readers.setdefault(r, {})[tok[1] if tok[0] == "c" else ("d", tok[1])] = tok
        for w in writes:
            self.lastw[w] = tok
            self.readers[w] = {}

    def op(self, eng, fn, reads=(), writes=()):
        self._deps(eng, reads, writes)
        ins = fn(self.eng[eng])
        self.cnt[eng] += 1
        ins.then_inc(self.sem[eng], 1)
        self._commit(("c", eng, self.cnt[eng]), reads, writes)
        self.nops += 1

    def dma(self, q, out, in_, reads=(), writes=(), **kw):
        self._deps(q, reads, writes)
        si = self.dnext
        self.dnext = (self.dnext + 1) % len(self.dsem)
        self._wait(q, ("d", si, self.dval[si]))
        ins = self.eng[q].dma_start(out=out, in_=in_, **kw)
        self.dval[si] += 16
        ins.then_inc(self.dsem[si], 16)
        self._commit(("d", si, self.dval[si]), reads, writes)
        self.nops += 1

    def barrier(self):
        for e in self.eng:
            for e2 in self.eng:
                if e2 != e and self.cnt[e2] > 0:
                    self._wait(e, ("c", e2, self.cnt[e2]))
            for si in range(len(self.dsem)):
                if self.dval[si] > 0:
                    self._wait(e, ("d", si, self.dval[si]))
        self.lastw = {}
        self.readers = {}

    def drain(self, eng="sp"):
        for r, t in list(self.lastw.items()):
            self._wait(eng, t)
        for r, d in list(self.readers.items()):
            for t in d.values():
                self._wait(eng, t)


def bcast_mid(ap2, n):
    return ap2.unsqueeze(2).to_broadcast([ap2.shape[0], ap2.shape[1], n])


def bcast_rep(ap2, n):
    return ap2.unsqueeze(1).to_broadcast([ap2.shape[0], n, ap2.shape[1]])


def make_consts():
    c = {}
    c["c_ident"] = np.eye(128, dtype=np.float32)
    t = np.arange(SEQ)
    slopes = 2.0 ** (-(np.arange(8) + 1.0))
    qa = np.zeros((8, 3, SEQ), np.float32)
    for h in range(8):
        qa[h, 0] = 128.0 * slopes[h]
        qa[h, 1] = slopes[h]
        qa[h, 2] = -slopes[h] * 128.0 * (t // 128 + 1)
    c["c_qaug"] = qa
    ka = np.zeros((3, SEQ), np.float32)
    ka[0] = t // 128
    ka[1] = t % 128
    ka[2] = 1.0
    c["c_kaug"] = ka
    n = np.arange(512)
    pos = 16 * n + 31
    kc = np.zeros((3, 512), np.float32)
    kc[0] = pos // 128
    kc[1] = pos % 128
    kc[2] = 1.0
    c["c_kcaug"] = kc
    hh = np.arange(8)
    lg = np.log1p(-(2.0 ** (-5.0 - hh)))
    nn = np.arange(128)
    xiT = np.zeros((4, 128, 128), np.float32)
    kdT = np.zeros((4, 128, 128), np.float32)
    gC = np.zeros((128, 4), np.float32)
    for hp in range(4):
        for half in range(2):
            h = 2 * hp + half
            xiT[hp, half * 64:(half + 1) * 64, :] = np.exp((nn + 1.0) * lg[h])[None, :]
            kdT[hp, half * 64:(half + 1) * 64, :] = 0.125 * np.exp(-(nn + 1.0) * lg[h])[None, :]
            gC[half * 64:(half + 1) * 64, hp] = np.exp(128.0 * lg[h])
    c["c_xiT"] = xiT
    c["c_kdT"] = kdT
    c["c_gC"] = gC
    c["c_gC8"] = np.tile(np.exp(128.0 * lg)[None, :], (64, 1)).astype(np.float32)
    c["c_ktab"] = (0.125 * np.exp(-(nn[:, None] + 1.0) * lg[None, :])).astype(np.float32)
    dm = (nn[None, :] >= nn[:, None]).astype(np.float32)
    c["c_rmask"] = np.tile(dm, (1, 4))
    jj = nn[:, None]
    ii = nn[None, :]
    c["c_mcausal"] = np.tile(np.where(jj > ii, NEG, 0.0).astype(np.float32), (1, 4))
    c["c_mwin4"] = np.tile(np.where(jj > ii, 0.0, NEG).astype(np.float32), (1, 4))
    pats = []
    pidx = {}
    cmp_plan = []
    for qi in range(NT):
        row = []
        for cc in range(4):
            tq = 128 * qi + ii
            nk = 128 * cc + jj
            valid = (tq - (16 * nk + 31) >= 0) & (nk <= 510)
            if valid.all():
                row.append(-1)
            elif not valid.any():
                row.append(-2)
            else:
                key = valid.tobytes()
                if key not in pidx:
                    pidx[key] = len(pats)
                    pats.append(np.tile(np.where(valid, 0.0, NEG).astype(np.float32), (1, 4)))
                row.append(pidx[key])
        cmp_plan.append(row)
    c["c_mcmp"] = np.stack(pats, 0)
    E = np.zeros((128, NT, 128), np.float32)
    for kt in range(NT):
        E[2 * kt, kt, 0:64] = 1.0
        E[2 * kt + 1, kt, 64:128] = 1.0
    c["c_E"] = E
    ncmp = np.arange(512)[:, None]
    msel = np.arange(128)[None, :]
    ov = ((np.minimum(16 * ncmp + 32, 64 * msel + 64) > np.maximum(16 * ncmp, 64 * msel))
          & (ncmp <= 510)).astype(np.float32)
    c["c_ov"] = ov
    AB = np.zeros((NT, 128, 256), np.float32)
    for qi in range(NT):
        tq = 128 * qi + nn[:, None]
        back = tq // 64 - msel
        forced = (msel == 0) | ((back >= 0) & (back < 2))
        A = ((back >= 0) & (~forced)).astype(np.float32)
        B = np.where(forced, 1.0e9 + 1024.0 * msel, np.where(back >= 0, 0.0, -1.0)).astype(np.float32)
        AB[qi, :, 0:128] = A
        AB[qi, :, 128:256] = B
    c["c_AB"] = AB
    return c, cmp_plan


_CONSTS = None


def get_consts():
    global _CONSTS
    if _CONSTS is None:
        _CONSTS = make_consts()
    return _CONSTS


class Ctx:
    def __getattr__(self, k):
        if k in WEIGHT_SPECS:
            ap = self.nc.dram_tensor(k, WEIGHT_SPECS[k], F32, kind="ExternalInput").ap()
            self.__dict__[k] = ap
            self.used.append(k)
            return ap
        raise AttributeError(k)


def ln_a(s, X, src, src_res, k):
    junk, ss, xs = X.junk, X.ss[k], X.xs[k]
    s.op("act", lambda e: e.activation(out=junk[:], in_=src, func=AF.Square, accum_out=ss[:, 0:1]),
         reads=[src_res], writes=["junk", "ss%d" % k])
    s.op("dve", lambda e: e.tensor_scalar(out=ss[:, 1:2], in0=ss[:, 0:1], scalar1=1.0 / D, scalar2=EPS,
                                          op0=ALU.mult, op1=ALU.add), reads=["ss%d" % k], writes=["ss%d" % k])
    s.op("act", lambda e: e.activation(out=ss[:, 1:2], in_=ss[:, 1:2], func=AF.Sqrt),
         reads=["ss%d" % k], writes=["ss%d" % k])
    s.op("dve", lambda e: e.reciprocal(out=ss[:, 2:3], in_=ss[:, 1:2]), reads=["ss%d" % k], writes=["ss%d" % k])
    s.op("act", lambda e: e.activation(out=xs[:], in_=src, func=AF.Copy, scale=ss[:, 2:3]),
         reads=[src_res, "ss%d" % k], writes=["xs%d" % k])


def ln_b(s, X, k, gT, gT_res, dst, dst_res):
    xs = X.xs[k]
    psb = X.ps[7][:].bitcast(BF16)
    for kc in range(8):
        s.op("pe", lambda e: e.transpose(out=psb[:, kc * 128:(kc + 1) * 128], in_=xs[:, kc * 128:(kc + 1) * 128],
                                         identity=X.ident[:]), reads=["xs%d" % k, "ident"], writes=["ps7"])
    s.op("dve", lambda e: e.tensor_tensor(out=dst, in0=psb.rearrange("p (k t) -> p k t", k=8),
                                          in1=bcast_mid(gT, 128), op=ALU.mult),
         reads=["ps7", gT_res], writes=[dst_res])


def phase_proj(s, X, L, h_in):
    nc = X.nc
    with ExitStack() as es:
        sb = lambda name, shape, dt: es.enter_context(nc.sbuf_tensor("%s_L%d" % (name, L), shape, dt))
        wsb = sb("p1_w", [128, 8, IN_COLS], BF16)
        gT = sb("p1_gT", [128, 8], F32)
        ht = [sb("p1_ht%d" % i, [128, D], F32) for i in range(4)]
        hnT = [sb("p1_hnT%d" % i, [128, 8, 512], BF16) for i in range(2)]
        fm = [sb("p1_fm%d" % i, [128, 16, 512], BF16) for i in range(2)]
        vsx = [sb("p1_vsx%d" % i, [128, 4, 2, 65], BF16) for i in range(2)]
        vwx = [sb("p1_vwx%d" % i, [128, 4, 2, 65], BF16) for i in range(2)]
        gls = [sb("p1_gls%d" % i, [128, 4, 24], F32) for i in range(2)]
        krm = [sb("p1_krm%d" % i, [128, 4, 512], BF16) for i in range(2)]
        vrm = [sb("p1_vrm%d" % i, [128, 4, 512], BF16) for i in range(2)]
        grm = [sb("p1_grm%d" % i, [128, 4, 512], BF16) for i in range(2)]
        sg = sb("p1_sg", [128, 512], F32)
        xiT = sb("p1_xiT", [128, 4, 128], F32)
        kdT = sb("p1_kdT", [128, 4, 128], F32)
        ktab = sb("p1_ktab", [128, 8], F32)
        s.dma("sp", xiT[:], X.c["c_xiT"].rearrange("j p t -> p j t"), writes=["xiT"])
        s.dma("sp", kdT[:], X.c["c_kdT"].rearrange("j p t -> p j t"), writes=["kdT"])
        s.dma("sp", ktab[:], X.c["c_ktab"], writes=["ktab"])
        s.dma("sp", gT[:], X.g_mix[L].rearrange("(k p) -> p k", p=128), writes=["p1gT"],
              allow_slow_non_contiguous=True)
        for kc in range(8):
            s.dma("pool", wsb[:, kc, :], X.w_in[L, kc * 128:(kc + 1) * 128, :], writes=["p1w"])
        for i in range(2):
            s.op("pool", lambda e: e.memset(vsx[i][:, :, :, 64:65], 1.0), writes=["vsx%d" % i])
            s.op("pool", lambda e: e.memset(vwx[i][:, :, :, 64:65], 1.0), writes=["vwx%d" % i])
        fm_cols = [C_QN, C_QN + 128, C_QN + 256, C_QN + 384, C_KC, C_VC, C_KS, C_KW,
                   C_QR, C_QR + 128, C_QR + 256, C_QR + 384, C_KR, C_KR + 128, C_KR + 256, C_KR + 384]
        def emit_a(tg):
            for tt in range(4):
                ti = 4 * tg + tt
                s.dma("sp", ht[tt][:], h_in[ti * 128:(ti + 1) * 128, :], writes=["p1ht%d" % tt])
                ln_a(s, X, ht[tt][:], "p1ht%d" % tt, tt)

        def emit_b(tg):
            for tt in range(4):
                ln_b(s, X, tt, gT[:], "p1gT", hnT[tg % 2][:, :, tt * 128:(tt + 1) * 128], "hnT%d" % (tg % 2))

        emit_a(0)
        emit_b(0)
        for tg in range(NT // 4):
            b = tg % 2
            toks = slice(tg * 512, (tg + 1) * 512)
            if tg + 1 < NT // 4:
                emit_a(tg + 1)
            for ci, c0 in enumerate(fm_cols):
                pb = ci % 2
                ps = X.ps[pb]
                for kc in range(8):
                    s.op("pe", lambda e: e.matmul(ps[:], lhsT=wsb[:, kc, c0:c0 + 128], rhs=hnT[b][:, kc, :],
                                                  start=(kc == 0), stop=(kc == 7)),
                         reads=["p1w", "hnT%d" % b], writes=["ps%d" % pb])
                dst = fm[b][:, ci, :]
                if ci < 4:
                    s.op("act", lambda e: e.activation(out=dst, in_=ps[:], func=AF.Copy, scale=0.125),
                         reads=["ps%d" % pb], writes=["fm%d" % b])
                elif ci < 8:
                    eng = "act" if ci % 2 == 0 else "dve"
                    if eng == "act":
                        s.op("act", lambda e: e.activation(out=dst, in_=ps[:], func=AF.Copy),
                             reads=["ps%d" % pb], writes=["fm%d" % b])
                    else:
                        s.op("dve", lambda e: e.tensor_copy(out=dst, in_=ps[:]),
                             reads=["ps%d" % pb], writes=["fm%d" % b])
                else:
                    tab = xiT if ci < 12 else kdT
                    j = (ci - 8) % 4
                    s.op("dve", lambda e: e.tensor_tensor(
                        out=dst.rearrange("p (a t) -> p a t", a=4), in0=ps[:].rearrange("p (a t) -> p a t", a=4),
                        in1=bcast_rep(tab[:, j, :], 4), op=ALU.mult),
                        reads=["ps%d" % pb, "xiT", "kdT"], writes=["fm%d" % b])
            for tt in range(4):
                lhs = lambda kc: hnT[b][:, kc, tt * 128:(tt + 1) * 128]
                groups = [(2, C_VS, 408), (3, C_KR, 512), (4, C_VR, 512), (5, C_GR, 512)]
                for (pi, c0, n) in groups:
                    for kc in range(8):
                        s.op("pe", lambda e: e.matmul(X.ps[pi][:, 0:n], lhsT=lhs(kc), rhs=wsb[:, kc, c0:c0 + n],
                                                      start=(kc == 0), stop=(kc == 7)),
                             reads=["p1w", "hnT%d" % b], writes=["ps%d" % pi])
                pA = X.ps[2]
                s.op("dve", lambda e: e.tensor_copy(out=vsx[b][:, tt, :, 0:64],
                                                    in_=pA[:, 0:128].rearrange("p (g d) -> p g d", g=2)),
                     reads=["ps2"], writes=["vsx%d" % b])
                s.op("dve", lambda e: e.tensor_copy(out=vwx[b][:, tt, :, 0:64],
                                                    in_=pA[:, 256:384].rearrange("p (g d) -> p g d", g=2)),
                     reads=["ps2"], writes=["vwx%d" % b])
                s.op("act", lambda e: e.activation(out=gls[b][:, tt, :], in_=pA[:, 384:408], func=AF.Sigmoid),
                     reads=["ps2"], writes=["gls%d" % b])
                s.op("dve", lambda e: e.tensor_tensor(
                    out=krm[b][:, tt, :].rearrange("p (h d) -> p h d", h=8),
                    in0=X.ps[3][:].rearrange("p (h d) -> p h d", h=8),
                    in1=bcast_mid(ktab[:], 64), op=ALU.mult),
                    reads=["ps3", "ktab"], writes=["krm%d" % b])
                s.op("act", lambda e: e.activation(out=vrm[b][:, tt, :], in_=X.ps[4][:], func=AF.Copy),
                     reads=["ps4"], writes=["vrm%d" % b])
                s.op("act", lambda e: e.activation(out=sg[:], in_=X.ps[5][:], func=AF.Sigmoid),
                     reads=["ps5"], writes=["p1sg"])
                s.op("dve", lambda e: e.tensor_tensor(out=grm[b][:, tt, :], in0=X.ps[5][:], in1=sg[:], op=ALU.mult),
                     reads=["ps5", "p1sg"], writes=["grm%d" % b])
            if tg + 1 < NT // 4:
                emit_b(tg + 1)
            s.dma("pool", X.QTP[:, :, toks].rearrange("j p t -> p j t"), fm[b][:, 0:4, :], reads=["fm%d" % b], writes=["QTP"])
            s.dma("pool", X.KCP[:, toks], fm[b][:, 4, :], reads=["fm%d" % b], writes=["KCP"])
            s.dma("pool", X.VCP[:, toks], fm[b][:, 5, :], reads=["fm%d" % b], writes=["VCP"])
            s.dma("pool", X.KSP[:, toks], fm[b][:, 6, :], reads=["fm%d" % b], writes=["KSP"])
            s.dma("pool", X.KWP[:, toks], fm[b][:, 7, :], reads=["fm%d" % b], writes=["KWP"])
            s.dma("pool", X.QRT[:, :, toks].rearrange("j p t -> p j t"), fm[b][:, 8:12, :], reads=["fm%d" % b], writes=["QRT"])
            s.dma("pool", X.KRT[:, :, toks].rearrange("j p t -> p j t"), fm[b][:, 12:16, :], reads=["fm%d" % b], writes=["KRT"])
            s.dma("pool", X.VSX[toks].rearrange("(tt p) g c -> p tt g c", p=128), vsx[b][:], reads=["vsx%d" % b], writes=["VSX"])
            s.dma("pool", X.VWX[toks].rearrange("(tt p) g c -> p tt g c", p=128), vwx[b][:], reads=["vwx%d" % b], writes=["VWX"])
            s.dma("pool", X.GLS[toks].rearrange("(tt p) c -> p tt c", p=128), gls[b][:], reads=["gls%d" % b], writes=["GLS"])
            s.dma("pool", X.KRM[toks].rearrange("(tt p) c -> p tt c", p=128), krm[b][:], reads=["krm%d" % b], writes=["KRM"])
            s.dma("pool", X.VRM[toks].rearrange("(tt p) c -> p tt c", p=128), vrm[b][:], reads=["vrm%d" % b], writes=["VRM"])
            s.dma("pool", X.GRM[toks].rearrange("(tt p) c -> p tt c", p=128), grm[b][:], reads=["grm%d" % b], writes=["GRM"])
        s.barrier()


WEIGHT_SPECS = {
    "w_in": [DEPTH, D, IN_COLS], "w_out": [DEPTH, D, D], "g_mix": [DEPTH, D], "g_ffn": [DEPTH, D],
    "g_ple": [DEPTH, D], "g_final": [D], "cmp_pos": [DEPTH, 2, 32, 64], "cmp_w1": [DEPTH, 2, 32, 64, 256],
    "cmp_w2": [DEPTH, 2, 256, 64], "ret_gn": [DEPTH, 512], "ffn_gate": [1, D, D_FF], "ffn_up": [1, D, D_FF],
    "ffn_down": [1, D_FF, D], "moe_router": [1, D, NE], "moe_gate": [1, NE, D, D_FF], "moe_up": [1, NE, D, D_FF],
    "moe_down": [1, NE, D_FF, D], "ple_proj": [DEPTH, 256, D], "ple_gate": [DEPTH, D, D],
}

SCRATCH_SPECS = {
    "QTP": ([4, 128, SEQ], BF16), "QAUG": ([8, 3, SEQ], BF16), "KAUG": ([3, SEQ], BF16), "KCAUG": ([3, 512], BF16),
    "KSP": ([128, SEQ], BF16), "KWP": ([128, SEQ], BF16), "KCP": ([128, SEQ], BF16), "VCP": ([128, SEQ], BF16),
    "VSX": ([SEQ, 2, 65], BF16), "VWX": ([SEQ, 2, 65], BF16), "GLS": ([SEQ, 24], F32),
    "QRT": ([4, 128, SEQ], BF16), "KRT": ([4, 128, SEQ], BF16),
    "KRM": ([SEQ, 512], BF16), "VRM": ([SEQ, 512], BF16), "GRM": ([SEQ, 512], BF16),
    "AT": ([D, SEQ], BF16), "HB": ([SEQ, D], F32),
    "FG16": ([1, D, D_FF], BF16), "FU16": ([1, D, D_FF], BF16), "FD16": ([1, D_FF, D], BF16),
    "MG16": ([NE, D, D_FF], BF16), "MU16": ([NE, D, D_FF], BF16), "MD16": ([NE, D_FF, D], BF16),
    "DBG_KCMP": ([67, 2, 512], BF16), "DBG_VCX": ([128, 2, 4, 193], BF16),
}


def build(stop_after=None, debug=(), nsa_tiles=NT):
    nc = bass.Bass("TRN2", target_bir_lowering=False)
    X = Ctx()
    X.nc = nc
    consts, cmp_plan = get_consts()
    X.cmp_plan = cmp_plan
    X.x = nc.dram_tensor("x", [SEQ, D], F32, kind="ExternalInput").ap()
    X.p = nc.dram_tensor("p", [DEPTH, SEQ, 256], F32, kind="ExternalInput").ap()
    X.used = []
    X.dbg_cmp = "DBG_KCMP" in debug
    X.nsa_tiles = nsa_tiles
    X.conv_layers = [0] if (stop_after is not None and stop_after[0] == 0) else [0, 1]
    X.ret_chunks = int(os.environ.get('RET_CHUNKS', NT))
    X.c = {k: nc.dram_tensor(k, list(v.shape), F32, kind="ExternalInput").ap() for k, v in consts.items()}
    X.out = nc.dram_tensor("out", [SEQ, D], F32, kind="ExternalOutput").ap()
    for k, (shp, dt) in SCRATCH_SPECS.items():
        kind = "ExternalOutput" if k in debug else "Internal"
        setattr(X, k, nc.dram_tensor(k, shp, dt, kind=kind).ap())
    with ExitStack() as es:
        s = S(nc, es)
        X.ps = [es.enter_context(nc.psum_tensor("ps%d" % i, [128, 512], F32)) for i in range(8)]
        X.ident = es.enter_context(nc.sbuf_tensor("ident", [128, 128], BF16))
        X.junk = es.enter_context(nc.sbuf_tensor("junk", [128, D], F32))
        X.ss = [es.enter_context(nc.sbuf_tensor("ss%d" % i, [128, 4], F32)) for i in range(4)]
        X.xs = [es.enter_context(nc.sbuf_tensor("xs%d" % i, [128, D], BF16)) for i in range(4)]
        s.dma("pool", X.ident[:], X.c["c_ident"], writes=["ident"])
        s.dma("pool", X.QAUG, X.c["c_qaug"], writes=["QAUG"])
        s.dma("pool", X.KAUG, X.c["c_kaug"], writes=["KAUG"])
        s.dma("pool", X.KCAUG, X.c["c_kcaug"], writes=["KCAUG"])
        convert_weights(s, X, X.conv_layers)
        done = False
        for L in range(DEPTH):
            h_in = X.x if L == 0 else X.HB
            for name, fn in PHASES:
                fn(s, X, L, h_in)
                if stop_after == (L, name):
                    done = True
                    break
            if done:
                break
        s.barrier()
        s.drain("sp")
    X.nops = s.nops
    return nc, X


PHASES = [("proj", phase_proj)]

_BUILT = {}


def run(inputs, stop_after=None, debug=(), cores=8, trace=False, nsa_tiles=NT):
    key = (stop_after, tuple(debug), nsa_tiles)
    if key not in _BUILT:
        _BUILT[key] = build(stop_after, debug, nsa_tiles)
    nc, X = _BUILT[key]
    consts, _ = get_consts()
    in_maps = []
    for b in range(cores):
        m = {"x": np.ascontiguousarray(inputs["x"][b]), "p": np.ascontiguousarray(inputs["p"][:, b])}
        for k in X.used:
            m[k] = np.ascontiguousarray(inputs[k])
        m.update(consts)
        in_maps.append(m)
    return run_bass_kernel_spmd(nc, in_maps, core_ids=list(range(cores)), trace=trace)


def kernel(**inputs):
    inputs = {k: np.asarray(v) for k, v in inputs.items()}
    res = run(inputs)
    return np.stack([r["out"] for r in res.results], 0).astype(np.float32)


def phase_nsa(s, X, L, h_in):
    nc = X.nc
    with ExitStack() as es:
        sb = lambda name, shape, dt: es.enter_context(nc.sbuf_tensor("%s_L%d" % (name, L), shape, dt))
        KCMP = sb("n_kcmp", [67, 2, 512], BF16)
        VCX = sb("n_vcx", [128, 2, 4, 193], BF16)
        s.op("pool", lambda e: e.memset(KCMP[:], 0.0), writes=["KCMP"])
        s.op("pool", lambda e: e.memset(VCX[:], 0.0), writes=["VCX"])
        s.op("pool", lambda e: e.memset(VCX[:, :, :, 64:65], 1.0), writes=["VCX"])
        for g in range(2):
            s.dma("pool", VCX[:, g, :, 65:193], X.c["c_ov"].rearrange("(ct p) m -> p ct m", p=128), writes=["VCX"])
            s.dma("sp", KCMP[64:67, g, :], X.KCAUG, reads=["KCAUG"], writes=["KCMP"])
        with ExitStack() as es2:
            sb2 = lambda name, shape, dt: es2.enter_context(nc.sbuf_tensor("%s_L%d" % (name, L), shape, dt))
            kcT = [sb2("c_kcT%d" % i, [64, SEQ], BF16) for i in range(2)]
            w1 = sb2("c_w1", [64, 32, 256], BF16)
            posT = sb2("c_posT", [64, 32], BF16)
            w2 = sb2("c_w2", [128, 2, 64], BF16)
            hb = sb2("c_hb", [128, 2], F32)
            u = sb2("c_u", [128, 2, 512], F32)
            t1 = sb2("c_t1", [128, 2, 512], F32)
            sg = sb2("c_sg", [128, 2, 512], F32)
            gel = sb2("c_gel", [128, 2, 512], BF16)
            it = 0
            for kv in range(2):
                s.dma("pool", w1[:], X.cmp_w1[L, kv].rearrange("l d h -> d l h"), writes=["c_w1"])
                s.dma("pool", posT[:], X.cmp_pos[L, kv].rearrange("l d -> d l"), writes=["c_posT"],
                      allow_slow_non_contiguous=True)
                s.dma("pool", w2[:], X.cmp_w2[L, kv].rearrange("(c p) d -> p c d", p=128), writes=["c_w2"])
                src = X.KCP if kv == 0 else X.VCP
                for g in range(2):
                    kt_ = kcT[it % 2]
                    kres = "c_kcT%d" % (it % 2)
                    it += 1
                    s.dma("sp", kt_[:], src[g * 64:(g + 1) * 64, :], reads=["KCP", "VCP"], writes=[kres])
                    kview = kt_[:].rearrange("d (n c) -> d n c", c=16)
                    for hc in range(2):
                        ps = X.ps[hc]
                        for l in range(32):
                            a, c_ = l // 16, l % 16
                            s.op("pe", lambda e: e.matmul(ps[:, 0:511], lhsT=w1[:, l, hc * 128:(hc + 1) * 128],
                                                          rhs=kview[:, a:a + 511, c_], start=(l == 0), stop=(l == 31)),
                                 reads=["c_w1", kres], writes=["ps%d" % hc])
                        for l in range(32):
                            s.op("pe", lambda e: e.matmul(X.ps[2][:, hc:hc + 1], lhsT=w1[:, l, hc * 128:(hc + 1) * 128],
                                                          rhs=posT[:, l:l + 1], start=(l == 0), stop=(l == 31)),
                                 reads=["c_w1", "c_posT"], writes=["ps2"])
                        s.op("dve", lambda e: e.tensor_copy(out=hb[:, hc:hc + 1], in_=X.ps[2][:, hc:hc + 1]),
                             reads=["ps2"], writes=["c_hb"])
                        s.op("act", lambda e: e.activation(out=u[:, hc, 0:511], in_=ps[:, 0:511], func=AF.Identity,
                                                           bias=hb[:, hc:hc + 1]),
                             reads=["ps%d" % hc, "c_hb"], writes=["c_u"])
                    uu = u[:, :, 0:511]
                    s.op("pool", lambda e: e.tensor_tensor(out=t1[:, :, 0:511], in0=uu, in1=uu, op=ALU.mult),
                         reads=["c_u"], writes=["c_t1"])
                    s.op("dve", lambda e: e.tensor_scalar(out=t1[:, :, 0:511], in0=t1[:, :, 0:511], scalar1=0.044715,
                                                          scalar2=1.0, op0=ALU.mult, op1=ALU.add),
                         reads=["c_t1"], writes=["c_t1"])
                    s.op("dve", lambda e: e.tensor_tensor(out=t1[:, :, 0:511], in0=t1[:, :, 0:511], in1=uu, op=ALU.mult),
                         reads=["c_t1", "c_u"], writes=["c_t1"])
                    s.op("act", lambda e: e.activation(out=sg[:, :, 0:511], in_=t1[:, :, 0:511], func=AF.Sigmoid,
                                                       scale=1.5957691216057308),
                         reads=["c_t1"], writes=["c_sg"])
                    s.op("dve", lambda e: e.tensor_tensor(out=gel[:, :, 0:511], in0=sg[:, :, 0:511], in1=uu, op=ALU.mult),
                         reads=["c_sg", "c_u"], writes=["c_gel"])
                    if kv == 0:
                        for hc in range(2):
                            s.op("pe", lambda e: e.matmul(X.ps[3][0:64, 0:511], lhsT=w2[:, hc, :], rhs=gel[:, hc, 0:511],
                                                          start=(hc == 0), stop=(hc == 1)),
                                 reads=["c_w2", "c_gel"], writes=["ps3"])
                        s.op("act", lambda e: e.activation(out=KCMP[0:64, g, 0:511], in_=X.ps[3][0:64, 0:511], func=AF.Copy),
                             reads=["ps3"], writes=["KCMP"])
                    else:
                        for ct in range(4):
                            nn = 128 if ct < 3 else 127
                            for hc in range(2):
                                s.op("pe", lambda e: e.matmul(X.ps[3][0:nn, ct * 64:(ct + 1) * 64],
                                                              lhsT=gel[:, hc, ct * 128:ct * 128 + nn], rhs=w2[:, hc, :],
                                                              start=(hc == 0), stop=(hc == 1)),
                                     reads=["c_w2", "c_gel"], writes=["ps3"])
                        for ct in range(4):
                            nn = 128 if ct < 3 else 127
                            s.op("act", lambda e: e.activation(out=VCX[0:nn, g, ct, 0:64],
                                                               in_=X.ps[3][0:nn, ct * 64:(ct + 1) * 64], func=AF.Copy),
                                 reads=["ps3"], writes=["VCX"])
            s.barrier()
        if X.dbg_cmp:
            s.dma("sp", X.DBG_KCMP, KCMP[:], reads=["KCMP"])
            s.dma("sp", X.DBG_VCX, VCX[:], reads=["VCX"])
        KS = sb("n_ks", [67, 2, SEQ], BF16)
        KW = sb("n_kw", [67, 2, SEQ], BF16)
        VS = sb("n_vs", [128, NT, 2, 65], BF16)
        VW = sb("n_vw", [128, NT, 2, 65], BF16)
        E = sb("n_E", [128, NT, 128], BF16)
        mca = sb("n_mca", [128, 512], BF16)
        mw4 = sb("n_mw4", [128, 512], BF16)
        npat = X.c["c_mcmp"].shape[0]
        mcmp = sb("n_mcmp", [128, npat, 512], BF16)
        qt = [sb("n_qt%d" % i, [67, 1024], BF16) for i in range(2)]
        AB = [sb("n_AB%d" % i, [128, 256], F32) for i in range(2)]
        glt = [sb("n_gl%d" % i, [128, 24], F32) for i in range(2)]
        PT = [sb("n_PT%d" % i, [128, 512], BF16) for i in range(4)]
        negT = [sb("n_negT%d" % i, [128, 512], BF16) for i in range(2)]
        acc = [sb("n_acc%d" % i, [128, 512], F32) for i in range(2)]
        accb = [sb("n_accb%d" % i, [128, 512], BF16) for i in range(2)]
        aT = [sb("n_aT%d" % i, [128, 4, 128], BF16) for i in range(2)]
        sm = [sb("n_sm%d" % i, [128, 64], F32) for i in range(2)]
        sc = [sb("n_sc%d" % i, [128, 128], F32) for i in range(2)]
        sc2 = [sb("n_sc2%d" % i, [128, 128], F32) for i in range(2)]
        seln = [sb("n_seln%d" % i, [128, 128], BF16) for i in range(2)]
        s.dma("sp", KS[0:64, :, :], X.KSP.rearrange("(g d) t -> d g t", g=2), reads=["KSP"], writes=["KS"])
        s.dma("sp", KW[0:64, :, :], X.KWP.rearrange("(g d) t -> d g t", g=2), reads=["KWP"], writes=["KW"])
        for g in range(2):
            s.dma("sp", KS[64:67, g, :], X.KAUG, reads=["KAUG"], writes=["KS"])
            s.dma("sp", KW[64:67, g, :], X.KAUG, reads=["KAUG"], writes=["KW"])
        for q4 in range(4):
            tsl = slice(q4 * 2048, (q4 + 1) * 2048)
            ksl = slice(q4 * 16, (q4 + 1) * 16)
            s.dma("sp", VS[:, ksl], X.VSX[tsl].rearrange("(kt p) g c -> p kt g c", p=128), reads=["VSX"], writes=["VS"])
            s.dma("sp", VW[:, ksl], X.VWX[tsl].rearrange("(kt p) g c -> p kt g c", p=128), reads=["VWX"], writes=["VW"])
            s.dma("pool", E[:, ksl, :], X.c["c_E"][:, ksl, :], writes=["E"])
        s.dma("pool", mca[:], X.c["c_mcausal"], writes=["mca"])
        s.dma("pool", mw4[:], X.c["c_mwin4"], writes=["mw4"])
        s.dma("pool", mcmp[:], X.c["c_mcmp"].rearrange("n p c -> p n c"), writes=["mcmp"])
        st = {"sb": 0, "pt": 0}

        def qk_tile(g, qb, lhsT, lres, extra, mask):
            pi = st["sb"] % 3
            st["sb"] += 1
            ps = X.ps[pi]
            pres = "ps%d" % pi
            nmm = 1 + (extra is not None) + (mask is not None)
            s.op("pe", lambda e: e.matmul(ps[:], lhsT=lhsT, rhs=qt[qb][:, g * 512:(g + 1) * 512], start=True,
                                          stop=(nmm == 1)), reads=[lres, "qt%d" % qb], writes=[pres])
            k = 1
            if extra is not None:
                el, er, eres = extra
                s.op("pe", lambda e: e.matmul(ps[:], lhsT=el, rhs=er, start=False, stop=(k + 1 == nmm)),
                     reads=eres, writes=[pres])
                k += 1
            if mask is not None:
                ml, mres = mask
                s.op("pe", lambda e: e.matmul(ps[:], lhsT=X.ident[:], rhs=ml, start=False, stop=True),
                     reads=["ident", mres], writes=[pres])
            pk = st["pt"] % 4
            st["pt"] += 1
            s.op("act", lambda e: e.activation(out=PT[pk][:], in_=ps[:], func=AF.Exp),
                 reads=[pres], writes=["PT%d" % pk])
            return PT[pk], "PT%d" % pk

        def coef_and_acc(g, qb, branch, heads_ap, heads_res, first):
            smt = sm[g]
            sres = "sm%d" % g
            for r in range(4):
                o_ap = heads_ap(r)
                s.op("dve", lambda e: e.tensor_scalar(out=smt[:, r:r + 1], in0=o_ap[:, 64:65], scalar1=1e-36,
                                                      scalar2=None, op0=ALU.max),
                     reads=heads_res, writes=[sres])
            s.op("dve", lambda e: e.reciprocal(out=smt[:, 4:8], in_=smt[:, 0:4]), reads=[sres], writes=[sres])
            c0 = branch * 8 + g * 4
            s.op("dve", lambda e: e.tensor_tensor(out=smt[:, 8:12], in0=smt[:, 4:8], in1=glt[qb][:, c0:c0 + 4],
                                                  op=ALU.mult), reads=[sres, "gl%d" % qb], writes=[sres])
            for r in range(4):
                o_ap = heads_ap(r)
                dst = acc[qb][:, (g * 4 + r) * 64:(g * 4 + r + 1) * 64]
                if first:
                    s.op("dve", lambda e: e.tensor_scalar(out=dst, in0=o_ap[:, 0:64], scalar1=smt[:, 8 + r:9 + r],
                                                          scalar2=None, op0=ALU.mult),
                         reads=heads_res + [sres], writes=["acc%d" % qb])
                else:
                    s.op("dve", lambda e: e.scalar_tensor_tensor(out=dst, in0=o_ap[:, 0:64], scalar=smt[:, 8 + r:9 + r],
                                                                 in1=dst, op0=ALU.mult, op1=ALU.add),
                         reads=heads_res + [sres], writes=["acc%d" % qb])

        def emit_loads(qi):
            qb = qi % 2
            t0 = qi * 128
            for j in range(4):
                s.dma("sp", qt[qb][0:64, :].rearrange("d (j two t) -> d j two t", j=4, two=2)[:, j],
                      X.QTP[j, :, t0:t0 + 128].rearrange("(two d) t -> d two t", two=2),
                      reads=["QTP"], writes=["qt%d" % qb])
            s.dma("sp", qt[qb][64:67, :].rearrange("r (h t) -> r h t", h=8),
                  X.QAUG[:, :, t0:t0 + 128].rearrange("h r t -> r h t"), reads=["QAUG"], writes=["qt%d" % qb])
            s.dma("sp", AB[qb][:], X.c["c_AB"][qi], writes=["AB%d" % qb])
            s.dma("sp", glt[qb][:], X.GLS[t0:t0 + 128, :], reads=["GLS"], writes=["gl%d" % qb])

        def cmp_post_a(qi, g):
            qb = qi % 2
            ocb = lambda r: X.ps[3 + r // 2][:, (r % 2) * 193:(r % 2) * 193 + 193]
            coef_and_acc(g, qb, 0, ocb, ["ps3", "ps4"], True)
            smt = sm[g]
            sres = "sm%d" % g
            for r in range(4):
                if r == 0:
                    s.op("dve", lambda e: e.tensor_scalar(out=sc[g][:], in0=ocb(r)[:, 65:193], scalar1=smt[:, 4:5],
                                                          scalar2=None, op0=ALU.mult),
                         reads=["ps3", "ps4", sres], writes=["sc%d" % g])
                else:
                    s.op("dve", lambda e: e.scalar_tensor_tensor(out=sc[g][:], in0=ocb(r)[:, 65:193],
                                                                 scalar=smt[:, 4 + r:5 + r], in1=sc[g][:],
                                                                 op0=ALU.mult, op1=ALU.add),
                         reads=["ps3", "ps4", sres], writes=["sc%d" % g])
            s.op("dve", lambda e: e.tensor_tensor(out=sc[g][:], in0=sc[g][:], in1=AB[qb][:, 0:128], op=ALU.mult),
                 reads=["AB%d" % qb], writes=["sc%d" % g])
            s.op("dve", lambda e: e.tensor_tensor(out=sc[g][:], in0=sc[g][:], in1=AB[qb][:, 128:256], op=ALU.add),
                 reads=["AB%d" % qb], writes=["sc%d" % g])
            s.op("dve", lambda e: e.max(out=smt[:, 16:24], in_=sc[g][:]), reads=["sc%d" % g], writes=[sres])
            s.op("dve", lambda e: e.match_replace(out=sc2[g][:], in_to_replace=smt[:, 16:24], in_values=sc[g][:],
                                                  imm_value=-2.0), reads=["sc%d" % g, sres], writes=["sc2%d" % g])
            s.op("dve", lambda e: e.max(out=smt[:, 24:32], in_=sc2[g][:]), reads=["sc2%d" % g], writes=[sres])
            s.op("dve", lambda e: e.tensor_scalar(out=seln[g][:], in0=sc[g][:], scalar1=smt[:, 31:32], scalar2=NEG,
                                                  op0=ALU.is_lt, op1=ALU.mult),
                 reads=["sc%d" % g, sres], writes=["seln%d" % g])

        def cmp_post_b(qi, g):
            psb = X.ps[7][:].bitcast(BF16)
            s.op("pe", lambda e: e.transpose(out=psb[:, g * 128:(g + 1) * 128], in_=seln[g][:], identity=X.ident[:]),
                 reads=["seln%d" % g, "ident"], writes=["ps7"])
            s.op("dve", lambda e: e.tensor_copy(out=negT[g][:].rearrange("p (a t) -> p a t", a=4),
                                                in_=bcast_rep(psb[:, g * 128:(g + 1) * 128], 4)),
                 reads=["ps7"], writes=["negT%d" % g])

        def write_out(qi):
            qb = qi % 2
            t0 = qi * 128
            s.op("act", lambda e: e.activation(out=accb[qb][:], in_=acc[qb][:], func=AF.Copy),
                 reads=["acc%d" % qb], writes=["accb%d" % qb])
            psb = X.ps[7][:].bitcast(BF16)
            for j in range(4):
                s.op("pe", lambda e: e.transpose(out=psb[:, 256 + j * 128:256 + (j + 1) * 128],
                                                 in_=accb[qb][:, j * 128:(j + 1) * 128], identity=X.ident[:]),
                     reads=["accb%d" % qb, "ident"], writes=["ps7"])
            s.op("act", lambda e: e.activation(out=aT[qb][:].rearrange("p j t -> p (j t)"), in_=psb[:, 256:768], func=AF.Copy),
                 reads=["ps7"], writes=["aT%d" % qb])
            s.dma("pool", X.AT[0:512, t0:t0 + 128].rearrange("(j p) t -> p j t", p=128), aT[qb][:],
                  reads=["aT%d" % qb], writes=["AT"])

        def pv_fn(out_ap, out_res, rhs, rhs_res, first, lastt, pair_start):
            def f(pt, ptres):
                for r in range(4):
                    st_ = first and ((r % 2 == 0) if pair_start else (r == 0))
                    s.op("pe", lambda e: e.matmul(out_ap(r), lhsT=pt[:, r * 128:(r + 1) * 128], rhs=rhs,
                                                  start=st_, stop=lastt), reads=[ptres, rhs_res], writes=out_res(r))
            return f

        jobs = []
        for qi in range(X.nsa_tiles):
            qb = qi % 2
            first_job = len(jobs)
            plan = X.cmp_plan[qi]
            cs = [c for c in range(4) if plan[c] != -2]
            ocb = lambda r: X.ps[3 + r // 2][:, (r % 2) * 193:(r % 2) * 193 + 193]
            ocres = lambda r: ["ps%d" % (3 + r // 2)]
            for g in range(2):
                for ci, c in enumerate(cs):
                    mask = None if plan[c] == -1 else (mcmp[:, plan[c], :], "mcmp")
                    jobs.append(dict(
                        qk=(lambda g=g, qb=qb, c=c, mask=mask: qk_tile(g, qb, KCMP[:, g, c * 128:(c + 1) * 128], "KCMP", None, mask)),
                        pv=pv_fn(ocb, ocres, VCX[:, g, c, :], "VCX", ci == 0, ci == len(cs) - 1, True),
                        post=((lambda qi=qi, g=g: (cmp_post_a(qi, g), (cmp_post_b(qi, g) if qi < 2 else None)))
                              if ci == len(cs) - 1 else None)))
            for g in range(2):
                obr = (lambda g: (lambda r: X.ps[5 + g][:, r * 65:(r + 1) * 65]))(g)
                obres = (lambda g: (lambda r: ["ps%d" % (5 + g)]))(g)
                dl = list(range(min(4, qi), -1, -1))
                for di, dd in enumerate(dl):
                    kt = qi - dd
                    mask = (mca[:], "mca") if dd == 0 else ((mw4[:], "mw4") if dd == 4 else None)
                    posts = []
                    if di == 0 and qi >= 2:
                        posts.append(lambda qi=qi, g=g: cmp_post_b(qi, g))
                    if di == len(dl) - 1:
                        posts.append(lambda g=g, qb=qb, obr=obr: coef_and_acc(g, qb, 2, obr, ["ps%d" % (5 + g)], False))
                    jobs.append(dict(
                        qk=(lambda g=g, qb=qb, kt=kt, mask=mask: qk_tile(g, qb, KW[:, g, kt * 128:(kt + 1) * 128], "KW", None, mask)),
                        pv=pv_fn(obr, obres, VW[:, kt, g, :], "VW", di == 0, di == len(dl) - 1, False),
                        post=(lambda posts=posts: [p_() for p_ in posts])))
            for g in range(2):
                obr = (lambda g: (lambda r: X.ps[5 + g][:, r * 65:(r + 1) * 65]))(g)
                obres = (lambda g: (lambda r: ["ps%d" % (5 + g)]))(g)
                for kt in range(qi + 1):
                    mask = (mca[:], "mca") if kt == qi else None
                    posts = []
                    if kt == qi:
                        posts.append(lambda g=g, qb=qb, obr=obr: coef_and_acc(g, qb, 1, obr, ["ps%d" % (5 + g)], False))
                        if g == 1:
                            posts.append(lambda qi=qi: write_out(qi))
                    jobs.append(dict(
                        qk=(lambda g=g, qb=qb, kt=kt, mask=mask: qk_tile(
                            g, qb, KS[:, g, kt * 128:(kt + 1) * 128], "KS", (E[:, kt, :], negT[g][:], ["E", "negT%d" % g]), mask)),
                        pv=pv_fn(obr, obres, VS[:, kt, g, :], "VS", kt == 0, kt == qi, False),
                        post=(lambda posts=posts: [p_() for p_ in posts])))
            if qi + 1 < X.nsa_tiles:
                old = jobs[first_job]["post"]
                jobs[first_job]["post"] = (lambda old=old, qi=qi: ((old() if old else None), emit_loads(qi + 1)))

        LA = 2
        emit_loads(0)
        pend = {}

        def start(j):
            pend[j] = jobs[j]["qk"]()

        for j in range(min(LA, len(jobs))):
            start(j)
        for j in range(len(jobs)):
            if j + LA < len(jobs):
                start(j + LA)
            pt, ptres = pend.pop(j)
            jobs[j]["pv"](pt, ptres)
            if jobs[j]["post"]:
                jobs[j]["post"]()
        s.barrier()


PHASES.append(("nsa", phase_nsa))


def phase_ret(s, X, L, h_in):
    nc = X.nc
    with ExitStack() as es:
        sb = lambda name, shape, dt: es.enter_context(nc.sbuf_tensor("%s_L%d" % (name, L), shape, dt))
        qT = [sb("r_qT%d" % i, [64, 8, 128], BF16) for i in range(2)]
        kT = [sb("r_kT%d" % i, [64, 8, 128], BF16) for i in range(2)]
        ones1 = sb("r_ones1", [1, 128], F32)
        gn1 = sb("r_gn1", [1, 512], F32)
        km = [sb("r_km%d" % i, [128, 512], BF16) for i in range(2)]
        vm = [sb("r_vm%d" % i, [128, 512], BF16) for i in range(2)]
        gm = [sb("r_gm%d" % i, [128, 512], BF16) for i in range(2)]
        state = sb("r_state", [64, 8, 64], F32)
        stateb = sb("r_stateb", [64, 8, 64], BF16)
        rmask = sb("r_mask", [128, 512], F32)
        gC = sb("r_gC", [64, 8], F32)
        gn = sb("r_gn", [128, 512], F32)
        Pm = sb("r_Pm", [128, 8, 128], BF16)
        sq = sb("r_sq", [128, 512], F32)
        y = sb("r_y", [128, 512], F32)
        yb = [sb("r_yb%d" % i, [128, 512], BF16) for i in range(2)]
        rT = [sb("r_rT%d" % i, [128, 4, 128], BF16) for i in range(2)]
        stt = sb("r_st", [128, 64], F32)
        s.dma("sp", rmask[:], X.c["c_rmask"], writes=["rmask"])
        s.dma("sp", gC[:], X.c["c_gC8"], writes=["gC"])
        s.dma("sp", gn1[:], X.ret_gn[L:L + 1, :], writes=["gn1"])
        s.op("dve", lambda e: e.memset(ones1[:], 1.0), writes=["ones1"])
        s.op("pe", lambda e: e.matmul(X.ps[6][:], lhsT=ones1[:], rhs=gn1[:], start=True, stop=True),
             reads=["ones1", "gn1"], writes=["ps6"])
        s.op("dve", lambda e: e.tensor_copy(out=gn[:], in_=X.ps[6][:]), reads=["ps6"], writes=["gn"])
        s.op("dve", lambda e: e.memset(state[:], 0.0), writes=["state"])
        for c in range(X.ret_chunks):
            b = c % 2
            tok = slice(c * 128, (c + 1) * 128)
            for j in range(4):
                s.dma("sp", qT[b][:, 2 * j:2 * j + 2, :], X.QRT[j, :, tok].rearrange("(two d) t -> d two t", two=2),
                      reads=["QRT"], writes=["qT%d" % b])
                s.dma("sp", kT[b][:, 2 * j:2 * j + 2, :], X.KRT[j, :, tok].rearrange("(two d) t -> d two t", two=2),
                      reads=["KRT"], writes=["kT%d" % b])
            s.dma("sp", km[b][:], X.KRM[tok, :], reads=["KRM"], writes=["km%d" % b])
            s.dma("sp", vm[b][:], X.VRM[tok, :], reads=["VRM"], writes=["vm%d" % b])
            s.dma("sp", gm[b][:], X.GRM[tok, :], reads=["GRM"], writes=["gm%d" % b])
            for h in range(8):
                hp, base = h // 2, 64 * (h % 2)
                pb = h // 4
                s.op("pe", lambda e: e.matmul(X.ps[pb][:, (h % 4) * 128:(h % 4 + 1) * 128],
                                              lhsT=kT[b][:, h, :], rhs=qT[b][:, h, :],
                                              start=(h % 4 == 0), stop=True),
                     reads=["kT%d" % b, "qT%d" % b], writes=["ps%d" % pb])
            for pb in range(2):
                s.op("dve", lambda e: e.tensor_tensor(out=Pm[:, 4 * pb:4 * pb + 4, :].rearrange("p a t -> p (a t)"),
                                                      in0=X.ps[pb][:], in1=rmask[:], op=ALU.mult),
                     reads=["ps%d" % pb, "rmask"], writes=["Pm"])
            for h in range(8):
                hp, base = h // 2, 64 * (h % 2)
                s.op("pe", lambda e: e.matmul(X.ps[2][:, h * 64:(h + 1) * 64], lhsT=Pm[:, h, :],
                                              rhs=vm[b][:, h * 64:(h + 1) * 64], start=(h == 0), stop=(c == 0)),
                     reads=["Pm", "vm%d" % b], writes=["ps2"])
                if c > 0:
                    s.op("pe", lambda e: e.matmul(X.ps[2][:, h * 64:(h + 1) * 64], lhsT=qT[b][:, h, :],
                                                  rhs=stateb[:, h, :], start=False, stop=True),
                         reads=["qT%d" % b, "stateb"], writes=["ps2"])
            if True:
                for h in range(8):
                    s.op("pe", lambda e: e.matmul(X.ps[3][0:64, h * 64:(h + 1) * 64], lhsT=km[b][:, h * 64:(h + 1) * 64],
                                                  rhs=vm[b][:, h * 64:(h + 1) * 64], start=(h == 0), stop=True),
                         reads=["km%d" % b, "vm%d" % b], writes=["ps3"])
                s.op("dve", lambda e: e.tensor_tensor(out=state[:], in0=state[:],
                                                      in1=X.ps[3][0:64, :].rearrange("p (h x) -> p h x", h=8), op=ALU.add),
                     reads=["ps3"], writes=["state"])
                s.op("dve", lambda e: e.tensor_tensor(out=state[:], in0=state[:], in1=bcast_mid(gC[:], 64), op=ALU.mult),
                     reads=["gC"], writes=["state"])
                s.op("dve", lambda e: e.tensor_copy(out=stateb[:], in_=state[:]), reads=["state"], writes=["stateb"])
            o3 = X.ps[2][:].rearrange("p (h d) -> p h d", h=8)
            s.op("dve", lambda e: e.tensor_reduce(out=stt[:, 0:8], in_=o3, axis=AX.X, op=ALU.add),
                 reads=["ps2"], writes=["r_st"])
            s.op("act", lambda e: e.activation(out=sq[:], in_=X.ps[2][:], func=AF.Square), reads=["ps2"], writes=["r_sq"])
            s.op("dve", lambda e: e.tensor_reduce(out=stt[:, 8:16], in_=sq[:].rearrange("p (h d) -> p h d", h=8),
                                                  axis=AX.X, op=ALU.add), reads=["r_sq"], writes=["r_st"])
            s.op("dve", lambda e: e.tensor_scalar(out=stt[:, 16:24], in0=stt[:, 0:8], scalar1=1.0 / 64, scalar2=None,
                                                  op0=ALU.mult), reads=["r_st"], writes=["r_st"])
            s.op("dve", lambda e: e.tensor_tensor(out=stt[:, 24:32], in0=stt[:, 16:24], in1=stt[:, 16:24], op=ALU.mult),
                 reads=["r_st"], writes=["r_st"])
            s.op("dve", lambda e: e.scalar_tensor_tensor(out=stt[:, 32:40], in0=stt[:, 8:16], scalar=1.0 / 64,
                                                         in1=stt[:, 24:32], op0=ALU.mult, op1=ALU.subtract),
                 reads=["r_st"], writes=["r_st"])
            s.op("dve", lambda e: e.tensor_scalar(out=stt[:, 32:40], in0=stt[:, 32:40], scalar1=EPS, scalar2=None,
                                                  op0=ALU.add), reads=["r_st"], writes=["r_st"])
            s.op("act", lambda e: e.activation(out=stt[:, 40:48], in_=stt[:, 32:40], func=AF.Sqrt),
                 reads=["r_st"], writes=["r_st"])
            s.op("dve", lambda e: e.reciprocal(out=stt[:, 48:56], in_=stt[:, 40:48]), reads=["r_st"], writes=["r_st"])
            y3 = y[:].rearrange("p (h d) -> p h d", h=8)
            s.op("dve", lambda e: e.tensor_tensor(out=y3, in0=o3, in1=bcast_mid(stt[:, 16:24], 64), op=ALU.subtract),
                 reads=["ps2", "r_st"], writes=["r_y"])
            s.op("dve", lambda e: e.tensor_tensor(out=y3, in0=y3, in1=bcast_mid(stt[:, 48:56], 64), op=ALU.mult),
                 reads=["r_st"], writes=["r_y"])
            s.op("pool", lambda e: e.tensor_tensor(out=y[:], in0=y[:], in1=gn[:], op=ALU.mult),
                 reads=["r_y", "gn"], writes=["r_y"])
            s.op("pool", lambda e: e.tensor_tensor(out=yb[b][:], in0=y[:], in1=gm[b][:], op=ALU.mult),
                 reads=["r_y", "gm%d" % b], writes=["r_yb%d" % b])
            psb = X.ps[7][:].bitcast(BF16)
            for j in range(4):
                s.op("pe", lambda e: e.transpose(out=psb[:, j * 128:(j + 1) * 128], in_=yb[b][:, j * 128:(j + 1) * 128],
                                                 identity=X.ident[:]), reads=["r_yb%d" % b, "ident"], writes=["ps7"])
            s.op("act", lambda e: e.activation(out=rT[b][:].rearrange("p j t -> p (j t)"), in_=psb[:, 0:512], func=AF.Copy),
                 reads=["ps7"], writes=["r_rT%d" % b])
            s.dma("pool", X.AT[512:1024, tok].rearrange("(j p) t -> p j t", p=128), rT[b][:],
                  reads=["r_rT%d" % b], writes=["AT"])
        s.barrier()


PHASES.append(("ret", phase_ret))


def convert_weights(s, X, layers):
    def conv(dst, src, rows):
        for r0 in range(0, rows, 512):
            s.dma("pool", dst[r0:r0 + 512, :], src[r0:r0 + 512, :], writes=["W16"])
    if 0 in layers:
        conv(X.FG16[0], X.ffn_gate[0], D)
        conv(X.FU16[0], X.ffn_up[0], D)
        conv(X.FD16[0], X.ffn_down[0], D_FF)
    if 1 in layers:
        for e in range(NE):
            conv(X.MG16[e], X.moe_gate[0, e], D)
            conv(X.MU16[e], X.moe_up[0, e], D)
            conv(X.MD16[e], X.moe_down[0, e], D_FF)


def phase_ffn(s, X, L, h_in):
    nc = X.nc
    last = (L == DEPTH - 1)
    moe = (L % 2 == 1)
    h_out = X.out if last else X.HB
    G16, U16, D16 = (X.MG16, X.MU16, X.MD16) if moe else (X.FG16, X.FU16, X.FD16)
    experts = list(range(NE)) if moe else [0]
    with ExitStack() as es:
        sb = lambda name, shape, dt: es.enter_context(nc.sbuf_tensor("%s_L%d" % (name, L), shape, dt))
        wout = sb("f_wout", [128, 8, D], BF16)
        pgate = sb("f_pgate", [128, 8, D], BF16)
        pproj = sb("f_pproj", [128, 2, D], BF16)
        g2T = sb("f_g2T", [128, 8], F32)
        g3T = sb("f_g3T", [128, 8], F32)
        rw = sb("f_rw", [128, 8, NE], BF16)
        cT = sb("f_cT", [128, 8, 512], BF16)
        hin = [sb("f_hin%d" % i, [128, D], F32) for i in range(2)]
        h1 = sb("f_h1", [128, 4, D], F32)
        hnT = sb("f_hnT", [128, 8, 512], BF16)
        acc = sb("f_acc", [128, 4, D], F32)
        wg = [sb("f_wg%d" % i, [128, 8, 512], BF16) for i in range(2)]
        wu = [sb("f_wu%d" % i, [128, 8, 512], BF16) for i in range(2)]
        wd = [sb("f_wd%d" % i, [128, 4, D], BF16) for i in range(2)]
        silu_t = [sb("f_silu%d" % i, [128, 512], BF16) for i in range(2)]
        actb = [sb("f_actb%d" % i, [128, 4, 512], BF16) for i in range(2)]
        sig = [sb("f_sig%d" % i, [128, 512], F32) for i in range(2)]
        ptile = [sb("f_pt%d" % i, [128, 256], F32) for i in range(2)]
        pb = sb("f_pb", [128, 256], BF16)
        pT = sb("f_pT", [128, 2, 512], BF16)
        gates = sb("f_gates", [128, 4, NE], F32)
        lg = sb("f_lg", [128, 16], F32)
        sm = sb("f_sm", [128, 32], F32)
        for kc in range(8):
            s.dma("pool", wout[:, kc, :], X.w_out[L, kc * 128:(kc + 1) * 128, :], writes=["wout"])
            s.dma("pool", pgate[:, kc, :], X.ple_gate[L, kc * 128:(kc + 1) * 128, :], writes=["pgate"])
        for kc in range(2):
            s.dma("pool", pproj[:, kc, :], X.ple_proj[L, kc * 128:(kc + 1) * 128, :], writes=["pproj"])
        s.dma("sp", g2T[:], X.g_ffn[L].rearrange("(k p) -> p k", p=128), writes=["g2T"], allow_slow_non_contiguous=True)
        s.dma("sp", g3T[:], X.g_ple[L].rearrange("(k p) -> p k", p=128), writes=["g3T"], allow_slow_non_contiguous=True)
        if moe:
            s.dma("pool", rw[:], X.moe_router[0].rearrange("(k p) e -> p k e", p=128), writes=["rw"])
        if last:
            gfin = sb("f_gfin", [128, D], F32)
            ones1 = sb("f_ones1", [1, 128], F32)
            gf1 = sb("f_gf1", [1, D], F32)
            s.dma("sp", gf1[:], X.g_final.rearrange("(o n) -> o n", o=1), writes=["gf1"])
            s.op("dve", lambda e: e.memset(ones1[:], 1.0), writes=["ones1"])
            for half in range(2):
                s.op("pe", lambda e: e.matmul(X.ps[6][:], lhsT=ones1[:], rhs=gf1[:, half * 512:(half + 1) * 512],
                                              start=True, stop=True), reads=["ones1", "gf1"], writes=["ps6"])
                s.op("dve", lambda e: e.tensor_copy(out=gfin[:, half * 512:(half + 1) * 512], in_=X.ps[6][:]),
                     reads=["ps6"], writes=["gfin"])
        wcnt = 0
        for tg in range(NT // 4):
            toks = slice(tg * 512, (tg + 1) * 512)
            s.dma("sp", cT[:], X.AT[:, toks].rearrange("(k p) t -> p k t", p=128), reads=["AT"], writes=["cT"])
            for tt in range(4):
                ti = 4 * tg + tt
                hb_ = hin[ti % 2]
                s.dma("sp", hb_[:], h_in[ti * 128:(ti + 1) * 128, :], reads=["HB"], writes=["hin%d" % (ti % 2)])
                for half in range(2):
                    hs = slice(half * 512, (half + 1) * 512)
                    for k in range(8):
                        s.op("pe", lambda e: e.matmul(X.ps[6][:], lhsT=cT[:, k, tt * 128:(tt + 1) * 128], rhs=wout[:, k, hs],
                                                      start=(k == 0), stop=(k == 7)), reads=["cT", "wout"], writes=["ps6"])
                    s.op("dve", lambda e: e.tensor_tensor(out=h1[:, tt, hs], in0=X.ps[6][:], in1=hb_[:, hs], op=ALU.add),
                         reads=["ps6", "hin%d" % (ti % 2)], writes=["h1_%d" % tt])
                ln_a(s, X, h1[:, tt, :], "h1_%d" % tt, tt)
            for tt in range(4):
                ln_b(s, X, tt, g2T[:], "g2T", hnT[:, :, tt * 128:(tt + 1) * 128], "f_hnT")
            if moe:
                for tt in range(4):
                    for k in range(8):
                        s.op("pe", lambda e: e.matmul(X.ps[6][:, 0:NE], lhsT=hnT[:, k, tt * 128:(tt + 1) * 128], rhs=rw[:, k, :],
                                                      start=(k == 0), stop=(k == 7)), reads=["f_hnT", "rw"], writes=["ps6"])
                    s.op("dve", lambda e: e.tensor_copy(out=lg[:, 0:8], in_=X.ps[6][:, 0:NE]), reads=["ps6"], writes=["lg"])
                    s.op("dve", lambda e: e.max(out=sm[:, 0:8], in_=lg[:, 0:8]), reads=["lg"], writes=["fsm"])
                    s.op("dve", lambda e: e.tensor_tensor(out=sm[:, 8:9], in0=sm[:, 1:2], in1=sm[:, 0:1], op=ALU.subtract),
                         reads=["fsm"], writes=["fsm"])
                    s.op("act", lambda e: e.activation(out=sm[:, 9:10], in_=sm[:, 8:9], func=AF.Exp), reads=["fsm"], writes=["fsm"])
                    s.op("dve", lambda e: e.tensor_scalar(out=sm[:, 10:11], in0=sm[:, 9:10], scalar1=1.0, scalar2=None,
                                                          op0=ALU.add), reads=["fsm"], writes=["fsm"])
                    s.op("dve", lambda e: e.reciprocal(out=sm[:, 11:12], in_=sm[:, 10:11]), reads=["fsm"], writes=["fsm"])
                    s.op("dve", lambda e: e.tensor_tensor(out=sm[:, 12:13], in0=sm[:, 9:10], in1=sm[:, 11:12], op=ALU.mult),
                         reads=["fsm"], writes=["fsm"])
                    s.op("dve", lambda e: e.tensor_scalar(out=lg[:, 8:16], in0=lg[:, 0:8], scalar1=sm[:, 0:1],
                                                          scalar2=sm[:, 11:12], op0=ALU.is_equal, op1=ALU.mult),
                         reads=["lg", "fsm"], writes=["lg"])
                    s.op("dve", lambda e: e.tensor_scalar(out=gates[:, tt, :], in0=lg[:, 0:8], scalar1=sm[:, 1:2],
                                                          scalar2=sm[:, 12:13], op0=ALU.is_equal, op1=ALU.mult),
                         reads=["lg", "fsm"], writes=["gates"])
                    s.op("dve", lambda e: e.tensor_tensor(out=gates[:, tt, :], in0=gates[:, tt, :], in1=lg[:, 8:16], op=ALU.add),
                         reads=["lg"], writes=["gates"])
            first = True
            for ex in experts:
                for blk in range(D_FF // 512):
                    wb = wcnt % 2
                    wcnt += 1
                    fs = slice(blk * 512, (blk + 1) * 512)
                    s.dma("sp", wg[wb][:], G16[ex][:, fs].rearrange("(k p) f -> p k f", p=128), reads=["W16"], writes=["wg%d" % wb])
                    s.dma("sp", wu[wb][:], U16[ex][:, fs].rearrange("(k p) f -> p k f", p=128), reads=["W16"], writes=["wu%d" % wb])
                    s.dma("sp", wd[wb][:], D16[ex][fs, :].rearrange("(c p) n -> p c n", p=128), reads=["W16"], writes=["wd%d" % wb])
                    for fc in range(4):
                        pg, pu = 2 * (fc % 2), 2 * (fc % 2) + 1
                        for k in range(8):
                            s.op("pe", lambda e: e.matmul(X.ps[pg][:], lhsT=wg[wb][:, k, fc * 128:(fc + 1) * 128], rhs=hnT[:, k, :],
                                                          start=(k == 0), stop=(k == 7)),
                                 reads=["wg%d" % wb, "f_hnT"], writes=["ps%d" % pg])
                        for k in range(8):
                            s.op("pe", lambda e: e.matmul(X.ps[pu][:], lhsT=wu[wb][:, k, fc * 128:(fc + 1) * 128], rhs=hnT[:, k, :],
                                                          start=(k == 0), stop=(k == 7)),
                                 reads=["wu%d" % wb, "f_hnT"], writes=["ps%d" % pu])
                        s.op("act", lambda e: e.activation(out=silu_t[fc % 2][:], in_=X.ps[pg][:], func=AF.Silu),
                             reads=["ps%d" % pg], writes=["silu%d" % (fc % 2)])
                        s.op("dve", lambda e: e.tensor_tensor(out=actb[wb][:, fc, :], in0=X.ps[pu][:], in1=silu_t[fc % 2][:],
                                                              op=ALU.mult),
                             reads=["ps%d" % pu, "silu%d" % (fc % 2)], writes=["actb%d" % wb])
                    for tt in range(4):
                        for half in range(2):
                            hs = slice(half * 512, (half + 1) * 512)
                            pd = 4 + half
                            for fc in range(4):
                                s.op("pe", lambda e: e.matmul(X.ps[pd][:], lhsT=actb[wb][:, fc, tt * 128:(tt + 1) * 128],
                                                              rhs=wd[wb][:, fc, hs], start=(fc == 0), stop=(fc == 3)),
                                     reads=["actb%d" % wb, "wd%d" % wb], writes=["ps%d" % pd])
                            dst = acc[:, tt, hs]
                            if moe:
                                gsc = gates[:, tt, ex:ex + 1]
                                if first:
                                    s.op("dve", lambda e: e.tensor_scalar(out=dst, in0=X.ps[pd][:], scalar1=gsc, scalar2=None,
                                                                          op0=ALU.mult),
                                         reads=["ps%d" % pd, "gates"], writes=["acc%d" % tt])
                                else:
                                    s.op("dve", lambda e: e.scalar_tensor_tensor(out=dst, in0=X.ps[pd][:], scalar=gsc, in1=dst,
                                                                                 op0=ALU.mult, op1=ALU.add),
                                         reads=["ps%d" % pd, "gates"], writes=["acc%d" % tt])
                            else:
                                if first:
                                    s.op("dve", lambda e: e.tensor_copy(out=dst, in_=X.ps[pd][:]),
                                         reads=["ps%d" % pd], writes=["acc%d" % tt])
                                else:
                                    s.op("dve", lambda e: e.tensor_tensor(out=dst, in0=X.ps[pd][:], in1=dst, op=ALU.add),
                                         reads=["ps%d" % pd], writes=["acc%d" % tt])
                    first = False
            for tt in range(4):
                s.op("pool", lambda e: e.tensor_tensor(out=h1[:, tt, :], in0=h1[:, tt, :], in1=acc[:, tt, :], op=ALU.add),
                     reads=["acc%d" % tt], writes=["h1_%d" % tt])
                ln_a(s, X, h1[:, tt, :], "h1_%d" % tt, tt)
            for tt in range(4):
                ln_b(s, X, tt, g3T[:], "g3T", hnT[:, :, tt * 128:(tt + 1) * 128], "f_hnT")
            psb = X.ps[7][:].bitcast(BF16)
            for tt in range(4):
                ti = 4 * tg + tt
                pt_ = ptile[ti % 2]
                s.dma("sp", pt_[:], X.p[L, ti * 128:(ti + 1) * 128, :], writes=["pt%d" % (ti % 2)])
                s.op("act", lambda e: e.activation(out=pb[:], in_=pt_[:], func=AF.Copy), reads=["pt%d" % (ti % 2)], writes=["pb"])
                for k in range(2):
                    s.op("pe", lambda e: e.transpose(out=psb[:, k * 128:(k + 1) * 128], in_=pb[:, k * 128:(k + 1) * 128],
                                                     identity=X.ident[:]), reads=["pb", "ident"], writes=["ps7"])
                s.op("act", lambda e: e.activation(out=pT[:, :, tt * 128:(tt + 1) * 128],
                                                   in_=psb[:, 0:256].rearrange("p (k t) -> p k t", k=2), func=AF.Copy),
                     reads=["ps7"], writes=["pT"])
            for tt in range(4):
                ti = 4 * tg + tt
                for half in range(2):
                    hs = slice(half * 512, (half + 1) * 512)
                    for k in range(2):
                        s.op("pe", lambda e: e.matmul(X.ps[6][:], lhsT=pT[:, k, tt * 128:(tt + 1) * 128], rhs=pproj[:, k, hs],
                                                      start=(k == 0), stop=(k == 1)), reads=["pT", "pproj"], writes=["ps6"])
                    pgb = 4 + half
                    for k in range(8):
                        s.op("pe", lambda e: e.matmul(X.ps[pgb][:], lhsT=hnT[:, k, tt * 128:(tt + 1) * 128], rhs=pgate[:, k, hs],
                                                      start=(k == 0), stop=(k == 7)), reads=["f_hnT", "pgate"], writes=["ps%d" % pgb])
                    s.op("act", lambda e: e.activation(out=sig[half][:], in_=X.ps[pgb][:], func=AF.Sigmoid),
                         reads=["ps%d" % pgb], writes=["sig%d" % half])
                    s.op("dve", lambda e: e.tensor_tensor(out=acc[:, tt, hs], in0=X.ps[6][:], in1=sig[half][:], op=ALU.mult),
                         reads=["ps6", "sig%d" % half], writes=["acc%d" % tt])
                s.op("pool", lambda e: e.tensor_tensor(out=h1[:, tt, :], in0=h1[:, tt, :], in1=acc[:, tt, :], op=ALU.add),
                     reads=["acc%d" % tt], writes=["h1_%d" % tt])
                if last:
                    ln_a(s, X, h1[:, tt, :], "h1_%d" % tt, tt)
                    s.op("act", lambda e: e.activation(out=acc[:, tt, :], in_=h1[:, tt, :], func=AF.Copy, scale=X.ss[tt][:, 2:3]),
                         reads=["h1_%d" % tt, "ss%d" % tt], writes=["acc%d" % tt])
                    s.op("pool", lambda e: e.tensor_tensor(out=acc[:, tt, :], in0=acc[:, tt, :], in1=gfin[:], op=ALU.mult),
                         reads=["gfin"], writes=["acc%d" % tt])
                    s.dma("pool", h_out[ti * 128:(ti + 1) * 128, :], acc[:, tt, :], reads=["acc%d" % tt], writes=["HOUT"])
                else:
                    s.dma("pool", h_out[ti * 128:(ti + 1) * 128, :], h1[:, tt, :], reads=["h1_%d" % tt], writes=["HB"])
        s.barrier()


PHASES.append(("ffn", phase_ffn))
```

```python
import math
import os
from contextlib import ExitStack

import numpy as np
import concourse.bass as bass
import concourse.mybir as mybir
from concourse.bass_utils import run_bass_kernel_spmd

F32 = mybir.dt.float32
BF16 = mybir.dt.bfloat16
AF = mybir.ActivationFunctionType
ALU = mybir.AluOpType
AX = mybir.AxisListType

D = 1024
SEQ = 8192
NT = SEQ // 128
DEPTH = 2
HD = 64
IN_COLS = 3352
D_FF = 3584
NE = 8
EPS = 1e-6
NEG = -30000.0
NGRP = 40
NSLOT = NGRP * 512
I32 = mybir.dt.int32

C_QN, C_KC, C_VC, C_KS, C_VS, C_KW, C_VW, C_GL, C_QR, C_KR, C_VR, C_GR = (
    0, 512, 640, 768, 896, 1024, 1152, 1280, 1304, 1816, 2328, 2840)


class S:
    def __init__(self, nc, es, n_dma_sems=24):
        self.nc = nc
        self.eng = {"pe": nc.tensor, "dve": nc.vector, "act": nc.scalar,
                    "pool": nc.gpsimd, "sp": nc.sync}
        self.sem = {k: es.enter_context(nc.semaphore("c_" + k)) for k in self.eng}
        self.cnt = {k: 0 for k in self.eng}
        self.seen = {k: {} for k in self.eng}
        self.drained = {k: 0 for k in self.eng}
        self.dsem = [es.enter_context(nc.semaphore("d%d" % i)) for i in range(n_dma_sems)]
        self.dval = [0] * n_dma_sems
        self.dnext = 0
        self.lastw = {}
        self.readers = {}
        self.nops = 0

    def _wait(self, eng, tok):
        if tok is None:
            return
        if tok[0] == "c":
            _, e2, c = tok
            if e2 == eng:
                if eng != "pe" and c > self.drained[eng]:
                    self.eng[eng].drain()
                    self.drained[eng] = self.cnt[eng]
                return
            key = e2
            sem = self.sem[e2]
        else:
            _, si, c = tok
            key = ("d", si)
            sem = self.dsem[si]
        if self.seen[eng].get(key, 0) >= c:
            return
        self.eng[eng].wait_ge(sem, c)
        self.seen[eng][key] = c

    def _deps(self, eng, reads, writes):
        for r in reads:
            self._wait(eng, self.lastw.get(r))
        for w in writes:
            self._wait(eng, self.lastw.get(w))
            for t in self.readers.get(w, {}).values():
                self._wait(eng, t)

    def _commit(self, tok, reads, writes):
        for r in reads:
            self.readers.setdefault(r, {})[tok[1] if tok[0] == "c" else ("d", tok[1])] = tok
        for w in writes:
            self.lastw[w] = tok
            self.readers[w] = {}

    def op(self, eng, fn, reads=(), writes=()):
        self._deps(eng, reads, writes)
        ins = fn(self.eng[eng])
        self.cnt[eng] += 1
        ins.then_inc(self.sem[eng], 1)
        self._commit(("c", eng, self.cnt[eng]), reads, writes)
        self.nops += 1

    def dma(self, q, out, in_, reads=(), writes=(), **kw):
        self._deps(q, reads, writes)
        si = self.dnext
        self.dnext = (self.dnext + 1) % len(self.dsem)
        self._wait(q, ("d", si, self.dval[si]))
        ins = self.eng[q].dma_start(out=out, in_=in_, **kw)
        self.dval[si] += 16
        ins.then_inc(self.dsem[si], 16)
        self._commit(("d", si, self.dval[si]), reads, writes)
        self.nops += 1

    def idma(self, out, out_off, in_, in_off, bound, reads=(), writes=()):
        q = "pool"
        self._deps(q, reads, writes)
        si = self.dnext
        self.dnext = (self.dnext + 1) % len(self.dsem)
        self._wait(q, ("d", si, self.dval[si]))
        oo = None if out_off is None else bass.IndirectOffsetOnAxis(ap=out_off, axis=0)
        io = None if in_off is None else bass.IndirectOffsetOnAxis(ap=in_off, axis=0)
        ins = self.nc.gpsimd.indirect_dma_start(out=out, out_offset=oo, in_=in_, in_offset=io)
        self.dval[si] += 16
        ins.then_inc(self.dsem[si], 16)
        self._commit(("d", si, self.dval[si]), reads, writes)
        self.nops += 1

    def barrier(self):
        for e in self.eng:
            for e2 in self.eng:
                if e2 != e and self.cnt[e2] > 0:
                    self._wait(e, ("c", e2, self.cnt[e2]))
            for si in range(len(self.dsem)):
                if self.dval[si] > 0:
                    self._wait(e, ("d", si, self.dval[si]))
        self.lastw = {}
        self.readers = {}

    def drain(self, eng="sp"):
        for r, t in list(self.lastw.items()):
            self._wait(eng, t)
        for r, d in list(self.readers.items()):
            for t in d.values():
                self._wait(eng, t)


def bcast_mid(ap2, n):
    return ap2.unsqueeze(2).to_broadcast([ap2.shape[0], ap2.shape[1], n])


def bcast_rep(ap2, n):
    return ap2.unsqueeze(1).to_broadcast([ap2.shape[0], n, ap2.shape[1]])


def make_consts():
    c = {}
    c["c_ident"] = np.eye(128, dtype=np.float32)
    t = np.arange(SEQ)
    slopes = 2.0 ** (-(np.arange(8) + 1.0))
    qa = np.zeros((8, 3, SEQ), np.float32)
    for h in range(8):
        qa[h, 0] = 128.0 * slopes[h]
        qa[h, 1] = slopes[h]
        qa[h, 2] = -slopes[h] * 128.0 * (t // 128 + 1)
    c["c_qaug"] = qa
    ka = np.zeros((3, SEQ), np.float32)
    ka[0] = t // 128
    ka[1] = t % 128
    ka[2] = 1.0
    c["c_kaug"] = ka
    n = np.arange(512)
    pos = 16 * n + 31
    kc = np.zeros((3, 512), np.float32)
    kc[0] = pos // 128
    kc[1] = pos % 128
    kc[2] = 1.0
    c["c_kcaug"] = kc
    hh = np.arange(8)
    lg = np.log1p(-(2.0 ** (-5.0 - hh)))
    nn = np.arange(128)
    xiT = np.zeros((4, 128, 128), np.float32)
    kdT = np.zeros((4, 128, 128), np.float32)
    gC = np.zeros((128, 4), np.float32)
    for hp in range(4):
        for half in range(2):
            h = 2 * hp + half
            xiT[hp, half * 64:(half + 1) * 64, :] = np.exp((nn + 1.0) * lg[h])[None, :]
            kdT[hp, half * 64:(half + 1) * 64, :] = 0.125 * np.exp(-(nn + 1.0) * lg[h])[None, :]
            gC[half * 64:(half + 1) * 64, hp] = np.exp(128.0 * lg[h])
    c["c_xiT"] = xiT
    c["c_kdT"] = kdT
    c["c_gC"] = gC
    c["c_gC8"] = np.tile(np.exp(128.0 * lg)[None, :], (64, 1)).astype(np.float32)
    c["c_ktab"] = (0.125 * np.exp(-(nn[:, None] + 1.0) * lg[None, :])).astype(np.float32)
    dm = (nn[None, :] >= nn[:, None]).astype(np.float32)
    c["c_rmask"] = np.tile(dm, (1, 4))
    jj = nn[:, None]
    ii = nn[None, :]
    c["c_mcausal"] = np.tile(np.where(jj > ii, NEG, 0.0).astype(np.float32), (1, 4))
    c["c_mwin4"] = np.tile(np.where(jj > ii, 0.0, NEG).astype(np.float32), (1, 4))
    pats = []
    pidx = {}
    cmp_plan = []
    for qi in range(NT):
        row = []
        for cc in range(4):
            tq = 128 * qi + ii
            nk = 128 * cc + jj
            valid = (tq - (16 * nk + 31) >= 0) & (nk <= 510)
            if valid.all():
                row.append(-1)
            elif not valid.any():
                row.append(-2)
            else:
                key = valid.tobytes()
                if key not in pidx:
                    pidx[key] = len(pats)
                    pats.append(np.tile(np.where(valid, 0.0, NEG).astype(np.float32), (1, 4)))
                row.append(pidx[key])
        cmp_plan.append(row)
    c["c_mcmp"] = np.stack(pats, 0)
    E = np.zeros((128, NT, 128), np.float32)
    for kt in range(NT):
        E[2 * kt, kt, 0:64] = 1.0
        E[2 * kt + 1, kt, 64:128] = 1.0
    c["c_E"] = E
    ncmp = np.arange(512)[:, None]
    msel = np.arange(128)[None, :]
    ov = ((np.minimum(16 * ncmp + 32, 64 * msel + 64) > np.maximum(16 * ncmp, 64 * msel))
          & (ncmp <= 510)).astype(np.float32)
    c["c_ov"] = ov
    AB = np.zeros((NT, 128, 256), np.float32)
    for qi in range(NT):
        tq = 128 * qi + nn[:, None]
        back = tq // 64 - msel
        forced = (msel == 0) | ((back >= 0) & (back < 2))
        A = ((back >= 0) & (~forced)).astype(np.float32)
        B = np.where(forced, 1.0e9 + 1024.0 * msel, np.where(back >= 0, 0.0, -1.0)).astype(np.float32)
        AB[qi, :, 0:128] = A
        AB[qi, :, 128:256] = B
    c["c_AB"] = AB
    c["c_gi512"] = np.tile((512.0 * np.arange(NGRP))[None, :], (128, 1)).astype(np.float32)
    c["c_tokid"] = (np.arange(NT)[None, :] * 128 + np.arange(128)[:, None]).astype(np.float32)
    c["c_U"] = (nn[:, None] < nn[None, :]).astype(np.float32)
    c["c_blkp"] = (np.arange(7)[None, :] * 128 + np.arange(128)[:, None]).astype(np.float32)
    return c, cmp_plan


_CONSTS = None


def get_consts():
    global _CONSTS
    if _CONSTS is None:
        _CONSTS = make_consts()
    return _CONSTS


class Ctx:
    def __getattr__(self, k):
        if k in WEIGHT_SPECS:
            ap = self.nc.dram_tensor(k, WEIGHT_SPECS[k], F32, kind="ExternalInput").ap()
            self.__dict__[k] = ap
            self.used.append(k)
            return ap
        raise AttributeError(k)


def ln_a(s, X, src, src_res, k):
    junk, ss, xs = X.junk, X.ss[k], X.xs[k]
    s.op("act", lambda e: e.activation(out=junk[:], in_=src, func=AF.Square, accum_out=ss[:, 0:1]),
         reads=[src_res], writes=["junk", "ss%d" % k])
    s.op("dve", lambda e: e.tensor_scalar(out=ss[:, 1:2], in0=ss[:, 0:1], scalar1=1.0 / D, scalar2=EPS,
                                          op0=ALU.mult, op1=ALU.add), reads=["ss%d" % k], writes=["ss%d" % k])
    s.op("act", lambda e: e.activation(out=ss[:, 1:2], in_=ss[:, 1:2], func=AF.Sqrt),
         reads=["ss%d" % k], writes=["ss%d" % k])
    s.op("dve", lambda e: e.reciprocal(out=ss[:, 2:3], in_=ss[:, 1:2]), reads=["ss%d" % k], writes=["ss%d" % k])
    s.op("act", lambda e: e.activation(out=xs[:], in_=src, func=AF.Copy, scale=ss[:, 2:3]),
         reads=[src_res, "ss%d" % k], writes=["xs%d" % k])


def ln_b(s, X, k, gT, gT_res, dst, dst_res, src=None, src_res=None):
    xs = X.xs[k] if src is None else src
    xres = ("xs%d" % k) if src is None else src_res
    psb = X.ps[7][:].bitcast(BF16)
    for kc in range(8):
        s.op("pe", lambda e: e.transpose(out=psb[:, kc * 128:(kc + 1) * 128], in_=xs[:, kc * 128:(kc + 1) * 128],
                                         identity=X.ident[:]), reads=[xres, "ident"], writes=["ps7"])
    s.op("dve", lambda e: e.tensor_tensor(out=dst, in0=psb.rearrange("p (k t) -> p k t", k=8),
                                          in1=bcast_mid(gT, 128), op=ALU.mult),
         reads=["ps7", gT_res], writes=[dst_res])


def phase_proj(s, X, L, h_in):
    nc = X.nc
    with ExitStack() as es:
        sb = lambda name, shape, dt: es.enter_context(nc.sbuf_tensor("%s_L%d" % (name, L), shape, dt))
        wsb = sb("p1_w", [128, 8, IN_COLS], BF16)
        gT = sb("p1_gT", [128, 8], F32)
        ht = [sb("p1_ht%d" % i, [128, D], F32) for i in range(4)]
        hnT = [sb("p1_hnT%d" % i, [128, 8, 512], BF16) for i in range(2)]
        fm = [sb("p1_fm%d" % i, [128, 16, 512], BF16) for i in range(2)]
        vsx = [sb("p1_vsx%d" % i, [128, 4, 2, 65], BF16) for i in range(2)]
        vwx = [sb("p1_vwx%d" % i, [128, 4, 2, 65], BF16) for i in range(2)]
        gls = [sb("p1_gls%d" % i, [128, 4, 24], F32) for i in range(2)]
        krm = [sb("p1_krm%d" % i, [128, 4, 512], BF16) for i in range(2)]
        vrm = [sb("p1_vrm%d" % i, [128, 4, 512], BF16) for i in range(2)]
        grm = [sb("p1_grm%d" % i, [128, 4, 512], BF16) for i in range(2)]
        sg = sb("p1_sg", [128, 512], F32)
        xiT = sb("p1_xiT", [128, 4, 128], F32)
        kdT = sb("p1_kdT", [128, 4, 128], F32)
        ktab = sb("p1_ktab", [128, 8], F32)
        s.dma("sp", xiT[:], X.c["c_xiT"].rearrange("j p t -> p j t"), writes=["xiT"])
        s.dma("sp", kdT[:], X.c["c_kdT"].rearrange("j p t -> p j t"), writes=["kdT"])
        s.dma("sp", ktab[:], X.c["c_ktab"], writes=["ktab"])
        s.dma("sp", gT[:], X.g_mix[L].rearrange("(k p) -> p k", p=128), writes=["p1gT"],
              allow_slow_non_contiguous=True)
        for kc in range(8):
            s.dma("pool", wsb[:, kc, :], X.w_in[L, kc * 128:(kc + 1) * 128, :], writes=["p1w"])
        for i in range(2):
            s.op("pool", lambda e: e.memset(vsx[i][:, :, :, 64:65], 1.0), writes=["vsx%d" % i])
            s.op("pool", lambda e: e.memset(vwx[i][:, :, :, 64:65], 1.0), writes=["vwx%d" % i])
        fm_cols = [C_QN, C_QN + 128, C_QN + 256, C_QN + 384, C_KC, C_VC, C_KS, C_KW,
                   C_QR, C_QR + 128, C_QR + 256, C_QR + 384, C_KR, C_KR + 128, C_KR + 256, C_KR + 384]
        def emit_a(tg):
            for tt in range(4):
                ti = 4 * tg + tt
                s.dma("sp", ht[tt][:], h_in[ti * 128:(ti + 1) * 128, :], writes=["p1ht%d" % tt])
                ln_a(s, X, ht[tt][:], "p1ht%d" % tt, tt)

        def emit_b(tg):
            for tt in range(4):
                ln_b(s, X, tt, gT[:], "p1gT", hnT[tg % 2][:, :, tt * 128:(tt + 1) * 128], "hnT%d" % (tg % 2))

        emit_a(0)
        emit_b(0)
        for tg in range(NT // 4):
            b = tg % 2
            toks = slice(tg * 512, (tg + 1) * 512)
            if tg + 1 < NT // 4:
                emit_a(tg + 1)
            for ci, c0 in enumerate(fm_cols):
                pb = ci % 2
                ps = X.ps[pb]
                for kc in range(8):
                    s.op("pe", lambda e: e.matmul(ps[:], lhsT=wsb[:, kc, c0:c0 + 128], rhs=hnT[b][:, kc, :],
                                                  start=(kc == 0), stop=(kc == 7)),
                         reads=["p1w", "hnT%d" % b], writes=["ps%d" % pb])
                dst = fm[b][:, ci, :]
                if ci < 4:
                    s.op("act", lambda e: e.activation(out=dst, in_=ps[:], func=AF.Copy, scale=0.125),
                         reads=["ps%d" % pb], writes=["fm%d" % b])
                elif ci < 8:
                    eng = "act" if ci % 2 == 0 else "dve"
                    if eng == "act":
                        s.op("act", lambda e: e.activation(out=dst, in_=ps[:], func=AF.Copy),
                             reads=["ps%d" % pb], writes=["fm%d" % b])
                    else:
                        s.op("dve", lambda e: e.tensor_copy(out=dst, in_=ps[:]),
                             reads=["ps%d" % pb], writes=["fm%d" % b])
                else:
                    tab = xiT if ci < 12 else kdT
                    j = (ci - 8) % 4
                    s.op("dve", lambda e: e.tensor_tensor(
                        out=dst.rearrange("p (a t) -> p a t", a=4), in0=ps[:].rearrange("p (a t) -> p a t", a=4),
                        in1=bcast_rep(tab[:, j, :], 4), op=ALU.mult),
                        reads=["ps%d" % pb, "xiT", "kdT"], writes=["fm%d" % b])
            for tt in range(4):
                lhs = lambda kc: hnT[b][:, kc, tt * 128:(tt + 1) * 128]
                groups = [(2, C_VS, 408), (3, C_KR, 512), (4, C_VR, 512), (5, C_GR, 512)]
                for (pi, c0, n) in groups:
                    for kc in range(8):
                        s.op("pe", lambda e: e.matmul(X.ps[pi][:, 0:n], lhsT=lhs(kc), rhs=wsb[:, kc, c0:c0 + n],
                                                      start=(kc == 0), stop=(kc == 7)),
                             reads=["p1w", "hnT%d" % b], writes=["ps%d" % pi])
                pA = X.ps[2]
                s.op("dve", lambda e: e.tensor_copy(out=vsx[b][:, tt, :, 0:64],
                                                    in_=pA[:, 0:128].rearrange("p (g d) -> p g d", g=2)),
                     reads=["ps2"], writes=["vsx%d" % b])
                s.op("dve", lambda e: e.tensor_copy(out=vwx[b][:, tt, :, 0:64],
                                                    in_=pA[:, 256:384].rearrange("p (g d) -> p g d", g=2)),
                     reads=["ps2"], writes=["vwx%d" % b])
                s.op("act", lambda e: e.activation(out=gls[b][:, tt, :], in_=pA[:, 384:408], func=AF.Sigmoid),
                     reads=["ps2"], writes=["gls%d" % b])
                s.op("dve", lambda e: e.tensor_tensor(
                    out=krm[b][:, tt, :].rearrange("p (h d) -> p h d", h=8),
                    in0=X.ps[3][:].rearrange("p (h d) -> p h d", h=8),
                    in1=bcast_mid(ktab[:], 64), op=ALU.mult),
                    reads=["ps3", "ktab"], writes=["krm%d" % b])
                s.op("act", lambda e: e.activation(out=vrm[b][:, tt, :], in_=X.ps[4][:], func=AF.Copy),
                     reads=["ps4"], writes=["vrm%d" % b])
                s.op("act", lambda e: e.activation(out=sg[:], in_=X.ps[5][:], func=AF.Sigmoid),
                     reads=["ps5"], writes=["p1sg"])
                s.op("dve", lambda e: e.tensor_tensor(out=grm[b][:, tt, :], in0=X.ps[5][:], in1=sg[:], op=ALU.mult),
                     reads=["ps5", "p1sg"], writes=["grm%d" % b])
            if tg + 1 < NT // 4:
                emit_b(tg + 1)
            s.dma("pool", X.QTP[:, :, toks].rearrange("j p t -> p j t"), fm[b][:, 0:4, :], reads=["fm%d" % b], writes=["QTP"])
            s.dma("pool", X.KCP[:, toks], fm[b][:, 4, :], reads=["fm%d" % b], writes=["KCP"])
            s.dma("pool", X.VCP[:, toks], fm[b][:, 5, :], reads=["fm%d" % b], writes=["VCP"])
            s.dma("pool", X.KSP[:, toks], fm[b][:, 6, :], reads=["fm%d" % b], writes=["KSP"])
            s.dma("pool", X.KWP[:, toks], fm[b][:, 7, :], reads=["fm%d" % b], writes=["KWP"])
            s.dma("pool", X.QRT[:, :, toks].rearrange("j p t -> p j t"), fm[b][:, 8:12, :], reads=["fm%d" % b], writes=["QRT"])
            s.dma("pool", X.KRT[:, :, toks].rearrange("j p t -> p j t"), fm[b][:, 12:16, :], reads=["fm%d" % b], writes=["KRT"])
            s.dma("pool", X.VSX[toks].rearrange("(tt p) g c -> p tt g c", p=128), vsx[b][:], reads=["vsx%d" % b], writes=["VSX"])
            s.dma("pool", X.VWX[toks].rearrange("(tt p) g c -> p tt g c", p=128), vwx[b][:], reads=["vwx%d" % b], writes=["VWX"])
            s.dma("pool", X.GLS[toks].rearrange("(tt p) c -> p tt c", p=128), gls[b][:], reads=["gls%d" % b], writes=["GLS"])
            s.dma("pool", X.KRM[toks].rearrange("(tt p) c -> p tt c", p=128), krm[b][:], reads=["krm%d" % b], writes=["KRM"])
            s.dma("pool", X.VRM[toks].rearrange("(tt p) c -> p tt c", p=128), vrm[b][:], reads=["vrm%d" % b], writes=["VRM"])
            s.dma("pool", X.GRM[toks].rearrange("(tt p) c -> p tt c", p=128), grm[b][:], reads=["grm%d" % b], writes=["GRM"])
        s.barrier()


WEIGHT_SPECS = {
    "w_in": [DEPTH, D, IN_COLS], "w_out": [DEPTH, D, D], "g_mix": [DEPTH, D], "g_ffn": [DEPTH, D],
    "g_ple": [DEPTH, D], "g_final": [D], "cmp_pos": [DEPTH, 2, 32, 64], "cmp_w1": [DEPTH, 2, 32, 64, 256],
    "cmp_w2": [DEPTH, 2, 256, 64], "ret_gn": [DEPTH, 512], "ffn_gate": [1, D, D_FF], "ffn_up": [1, D, D_FF],
    "ffn_down": [1, D_FF, D], "moe_router": [1, D, NE], "moe_gate": [1, NE, D, D_FF], "moe_up": [1, NE, D, D_FF],
    "moe_down": [1, NE, D_FF, D], "ple_proj": [DEPTH, 256, D], "ple_gate": [DEPTH, D, D],
}

SCRATCH_SPECS = {
    "QTP": ([4, 128, SEQ], BF16), "QAUG": ([8, 3, SEQ], BF16), "KAUG": ([3, SEQ], BF16), "KCAUG": ([3, 512], BF16),
    "KSP": ([128, SEQ], BF16), "KWP": ([128, SEQ], BF16), "KCP": ([128, SEQ], BF16), "VCP": ([128, SEQ], BF16),
    "VSX": ([SEQ, 2, 65], BF16), "VWX": ([SEQ, 2, 65], BF16), "GLS": ([SEQ, 24], F32),
    "QRT": ([4, 128, SEQ], BF16), "KRT": ([4, 128, SEQ], BF16),
    "KRM": ([SEQ, 512], BF16), "VRM": ([SEQ, 512], BF16), "GRM": ([SEQ, 512], BF16),
    "AT": ([D, SEQ], BF16), "HB": ([SEQ, D], F32),
    "FG16": ([1, D, D_FF], BF16), "FU16": ([1, D, D_FF], BF16), "FD16": ([1, D_FF, D], BF16),
    "WGB": ([NE * 7 * 128, 4096], BF16), "WUB": ([NE * 7 * 128, 4096], BF16), "WDB": ([NE * 7 * 128, 4096], BF16),
    "H1": ([SEQ, D], F32), "XS2": ([SEQ, D], BF16), "SLOT": ([NSLOT, 2], I32), "YS": ([NSLOT, D], F32),
    "DBG_KCMP": ([67, 2, 512], BF16), "DBG_VCX": ([128, 2, 4, 193], BF16),
}


def build(stop_after=None, debug=(), nsa_tiles=NT):
    nc = bass.Bass("TRN2", target_bir_lowering=False)
    X = Ctx()
    X.nc = nc
    consts, cmp_plan = get_consts()
    X.cmp_plan = cmp_plan
    X.x = nc.dram_tensor("x", [SEQ, D], F32, kind="ExternalInput").ap()
    X.p = nc.dram_tensor("p", [DEPTH, SEQ, 256], F32, kind="ExternalInput").ap()
    X.used = []
    X.dbg_cmp = "DBG_KCMP" in debug
    X.nsa_tiles = nsa_tiles
    X.routed = True
    X.conv_layers = [0] if (stop_after is not None and stop_after[0] == 0) else [0, 1]
    X.ret_chunks = int(os.environ.get('RET_CHUNKS', NT))
    X.c = {k: nc.dram_tensor(k, list(v.shape), F32, kind="ExternalInput").ap() for k, v in consts.items()}
    X.out = nc.dram_tensor("out", [SEQ, D], F32, kind="ExternalOutput").ap()
    for k, (shp, dt) in SCRATCH_SPECS.items():
        kind = "ExternalOutput" if k in debug else "Internal"
        setattr(X, k, nc.dram_tensor(k, shp, dt, kind=kind).ap())
    with ExitStack() as es:
        s = S(nc, es)
        X.ps = [es.enter_context(nc.psum_tensor("ps%d" % i, [128, 512], F32)) for i in range(8)]
        X.ident = es.enter_context(nc.sbuf_tensor("ident", [128, 128], BF16))
        X.junk = es.enter_context(nc.sbuf_tensor("junk", [128, D], F32))
        X.ss = [es.enter_context(nc.sbuf_tensor("ss%d" % i, [128, 4], F32)) for i in range(4)]
        X.xs = [es.enter_context(nc.sbuf_tensor("xs%d" % i, [128, D], BF16)) for i in range(4)]
        s.dma("pool", X.ident[:], X.c["c_ident"], writes=["ident"])
        s.dma("pool", X.QAUG, X.c["c_qaug"], writes=["QAUG"])
        s.dma("pool", X.KAUG, X.c["c_kaug"], writes=["KAUG"])
        s.dma("pool", X.KCAUG, X.c["c_kcaug"], writes=["KCAUG"])
        convert_weights(s, X, X.conv_layers)
        done = False
        for L in range(DEPTH):
            h_in = X.x if L == 0 else X.HB
            for name, fn in PHASES:
                fn(s, X, L, h_in)
                if stop_after == (L, name):
                    done = True
                    break
            if done:
                break
        s.barrier()
        s.drain("sp")
    X.nops = s.nops
    return nc, X


PHASES = [("proj", phase_proj)]

_BUILT = {}


def run(inputs, stop_after=None, debug=(), cores=8, trace=False, nsa_tiles=NT):
    key = (stop_after, tuple(debug), nsa_tiles)
    if key not in _BUILT:
        _BUILT[key] = build(stop_after, debug, nsa_tiles)
    nc, X = _BUILT[key]
    consts, _ = get_consts()
    in_maps = []
    for b in range(cores):
        m = {"x": np.ascontiguousarray(inputs["x"][b]), "p": np.ascontiguousarray(inputs["p"][:, b])}
        for k in X.used:
            m[k] = np.ascontiguousarray(inputs[k])
        m.update(consts)
        in_maps.append(m)
    return run_bass_kernel_spmd(nc, in_maps, core_ids=list(range(cores)), trace=trace)


def kernel(**inputs):
    inputs = {k: np.asarray(v) for k, v in inputs.items()}
    res = run(inputs)
    return np.stack([r["out"] for r in res.results], 0).astype(np.float32)


def phase_nsa(s, X, L, h_in):
    nc = X.nc
    with ExitStack() as es:
        sb = lambda name, shape, dt: es.enter_context(nc.sbuf_tensor("%s_L%d" % (name, L), shape, dt))
        KCMP = sb("n_kcmp", [67, 2, 512], BF16)
        VCX = sb("n_vcx", [128, 2, 4, 193], BF16)
        s.op("pool", lambda e: e.memset(KCMP[:], 0.0), writes=["KCMP"])
        s.op("pool", lambda e: e.memset(VCX[:], 0.0), writes=["VCX"])
        s.op("pool", lambda e: e.memset(VCX[:, :, :, 64:65], 1.0), writes=["VCX"])
        for g in range(2):
            s.dma("pool", VCX[:, g, :, 65:193], X.c["c_ov"].rearrange("(ct p) m -> p ct m", p=128), writes=["VCX"])
            s.dma("sp", KCMP[64:67, g, :], X.KCAUG, reads=["KCAUG"], writes=["KCMP"])
        with ExitStack() as es2:
            sb2 = lambda name, shape, dt: es2.enter_context(nc.sbuf_tensor("%s_L%d" % (name, L), shape, dt))
            kcT = [sb2("c_kcT%d" % i, [64, SEQ], BF16) for i in range(2)]
            w1 = sb2("c_w1", [64, 32, 256], BF16)
            posT = sb2("c_posT", [64, 32], BF16)
            w2 = sb2("c_w2", [128, 2, 64], BF16)
            hb = sb2("c_hb", [128, 2], F32)
            u = sb2("c_u", [128, 2, 512], F32)
            t1 = sb2("c_t1", [128, 2, 512], F32)
            sg = sb2("c_sg", [128, 2, 512], F32)
            gel = sb2("c_gel", [128, 2, 512], BF16)
            it = 0
            for kv in range(2):
                s.dma("pool", w1[:], X.cmp_w1[L, kv].rearrange("l d h -> d l h"), writes=["c_w1"])
                s.dma("pool", posT[:], X.cmp_pos[L, kv].rearrange("l d -> d l"), writes=["c_posT"],
                      allow_slow_non_contiguous=True)
                s.dma("pool", w2[:], X.cmp_w2[L, kv].rearrange("(c p) d -> p c d", p=128), writes=["c_w2"])
                src = X.KCP if kv == 0 else X.VCP
                for g in range(2):
                    kt_ = kcT[it % 2]
                    kres = "c_kcT%d" % (it % 2)
                    it += 1
                    s.dma("sp", kt_[:], src[g * 64:(g + 1) * 64, :], reads=["KCP", "VCP"], writes=[kres])
                    kview = kt_[:].rearrange("d (n c) -> d n c", c=16)
                    for hc in range(2):
                        ps = X.ps[hc]
                        for l in range(32):
                            a, c_ = l // 16, l % 16
                            s.op("pe", lambda e: e.matmul(ps[:, 0:511], lhsT=w1[:, l, hc * 128:(hc + 1) * 128],
                                                          rhs=kview[:, a:a + 511, c_], start=(l == 0), stop=(l == 31)),
                                 reads=["c_w1", kres], writes=["ps%d" % hc])
                        for l in range(32):
                            s.op("pe", lambda e: e.matmul(X.ps[2][:, hc:hc + 1], lhsT=w1[:, l, hc * 128:(hc + 1) * 128],
                                                          rhs=posT[:, l:l + 1], start=(l == 0), stop=(l == 31)),
                                 reads=["c_w1", "c_posT"], writes=["ps2"])
                        s.op("dve", lambda e: e.tensor_copy(out=hb[:, hc:hc + 1], in_=X.ps[2][:, hc:hc + 1]),
                             reads=["ps2"], writes=["c_hb"])
                        s.op("act", lambda e: e.activation(out=u[:, hc, 0:511], in_=ps[:, 0:511], func=AF.Identity,
                                                           bias=hb[:, hc:hc + 1]),
                             reads=["ps%d" % hc, "c_hb"], writes=["c_u"])
                    uu = u[:, :, 0:511]
                    s.op("pool", lambda e: e.tensor_tensor(out=t1[:, :, 0:511], in0=uu, in1=uu, op=ALU.mult),
                         reads=["c_u"], writes=["c_t1"])
                    s.op("dve", lambda e: e.tensor_scalar(out=t1[:, :, 0:511], in0=t1[:, :, 0:511], scalar1=0.044715,
                                                          scalar2=1.0, op0=ALU.mult, op1=ALU.add),
                         reads=["c_t1"], writes=["c_t1"])
                    s.op("dve", lambda e: e.tensor_tensor(out=t1[:, :, 0:511], in0=t1[:, :, 0:511], in1=uu, op=ALU.mult),
                         reads=["c_t1", "c_u"], writes=["c_t1"])
                    s.op("act", lambda e: e.activation(out=sg[:, :, 0:511], in_=t1[:, :, 0:511], func=AF.Sigmoid,
                                                       scale=1.5957691216057308),
                         reads=["c_t1"], writes=["c_sg"])
                    s.op("dve", lambda e: e.tensor_tensor(out=gel[:, :, 0:511], in0=sg[:, :, 0:511], in1=uu, op=ALU.mult),
                         reads=["c_sg", "c_u"], writes=["c_gel"])
                    if kv == 0:
                        for hc in range(2):
                            s.op("pe", lambda e: e.matmul(X.ps[3][0:64, 0:511], lhsT=w2[:, hc, :], rhs=gel[:, hc, 0:511],
                                                          start=(hc == 0), stop=(hc == 1)),
                                 reads=["c_w2", "c_gel"], writes=["ps3"])
                        s.op("act", lambda e: e.activation(out=KCMP[0:64, g, 0:511], in_=X.ps[3][0:64, 0:511], func=AF.Copy),
                             reads=["ps3"], writes=["KCMP"])
                    else:
                        for ct in range(4):
                            nn = 128 if ct < 3 else 127
                            for hc in range(2):
                                s.op("pe", lambda e: e.matmul(X.ps[3][0:nn, ct * 64:(ct + 1) * 64],
                                                              lhsT=gel[:, hc, ct * 128:ct * 128 + nn], rhs=w2[:, hc, :],
                                                              start=(hc == 0), stop=(hc == 1)),
                                     reads=["c_w2", "c_gel"], writes=["ps3"])
                        for ct in range(4):
                            nn = 128 if ct < 3 else 127
                            s.op("act", lambda e: e.activation(out=VCX[0:nn, g, ct, 0:64],
                                                               in_=X.ps[3][0:nn, ct * 64:(ct + 1) * 64], func=AF.Copy),
                                 reads=["ps3"], writes=["VCX"])
            s.barrier()
        if X.dbg_cmp:
            s.dma("sp", X.DBG_KCMP, KCMP[:], reads=["KCMP"])
            s.dma("sp", X.DBG_VCX, VCX[:], reads=["VCX"])
        KS = sb("n_ks", [67, 2, SEQ], BF16)
        KW = sb("n_kw", [67, 2, SEQ], BF16)
        VS = sb("n_vs", [128, NT, 2, 65], BF16)
        VW = sb("n_vw", [128, NT, 2, 65], BF16)
        E = sb("n_E", [128, NT, 128], BF16)
        mca = sb("n_mca", [128, 512], BF16)
        mw4 = sb("n_mw4", [128, 512], BF16)
        npat = X.c["c_mcmp"].shape[0]
        mcmp = sb("n_mcmp", [128, npat, 512], BF16)
        qt = [sb("n_qt%d" % i, [67, 1024], BF16) for i in range(2)]
        AB = [sb("n_AB%d" % i, [128, 256], F32) for i in range(2)]
        glt = [sb("n_gl%d" % i, [128, 24], F32) for i in range(2)]
        PT = [sb("n_PT%d" % i, [128, 512], BF16) for i in range(4)]
        negT = [sb("n_negT%d" % i, [128, 512], BF16) for i in range(2)]
        acc = [sb("n_acc%d" % i, [128, 512], F32) for i in range(2)]
        accb = [sb("n_accb%d" % i, [128, 512], BF16) for i in range(2)]
        aT = [sb("n_aT%d" % i, [128, 4, 128], BF16) for i in range(2)]
        sm = [sb("n_sm%d" % i, [128, 64], F32) for i in range(2)]
        sc = [sb("n_sc%d" % i, [128, 128], F32) for i in range(2)]
        sc2 = [sb("n_sc2%d" % i, [128, 128], F32) for i in range(2)]
        seln = [sb("n_seln%d" % i, [128, 128], BF16) for i in range(2)]
        s.dma("sp", KS[0:64, :, :], X.KSP.rearrange("(g d) t -> d g t", g=2), reads=["KSP"], writes=["KS"])
        s.dma("sp", KW[0:64, :, :], X.KWP.rearrange("(g d) t -> d g t", g=2), reads=["KWP"], writes=["KW"])
        for g in range(2):
            s.dma("sp", KS[64:67, g, :], X.KAUG, reads=["KAUG"], writes=["KS"])
            s.dma("sp", KW[64:67, g, :], X.KAUG, reads=["KAUG"], writes=["KW"])
        for q4 in range(4):
            tsl = slice(q4 * 2048, (q4 + 1) * 2048)
            ksl = slice(q4 * 16, (q4 + 1) * 16)
            s.dma("sp", VS[:, ksl], X.VSX[tsl].rearrange("(kt p) g c -> p kt g c", p=128), reads=["VSX"], writes=["VS"])
            s.dma("sp", VW[:, ksl], X.VWX[tsl].rearrange("(kt p) g c -> p kt g c", p=128), reads=["VWX"], writes=["VW"])
            s.dma("pool", E[:, ksl, :], X.c["c_E"][:, ksl, :], writes=["E"])
        s.dma("pool", mca[:], X.c["c_mcausal"], writes=["mca"])
        s.dma("pool", mw4[:], X.c["c_mwin4"], writes=["mw4"])
        s.dma("pool", mcmp[:], X.c["c_mcmp"].rearrange("n p c -> p n c"), writes=["mcmp"])
        st = {"sb": 0, "pt": 0}

        def qk_tile(g, qb, lhsT, lres, extra, mask):
            pi = st["sb"] % 3
            st["sb"] += 1
            ps = X.ps[pi]
            pres = "ps%d" % pi
            nmm = 1 + (extra is not None) + (mask is not None)
            s.op("pe", lambda e: e.matmul(ps[:], lhsT=lhsT, rhs=qt[qb][:, g * 512:(g + 1) * 512], start=True,
                                          stop=(nmm == 1)), reads=[lres, "qt%d" % qb], writes=[pres])
            k = 1
            if extra is not None:
                el, er, eres = extra
                s.op("pe", lambda e: e.matmul(ps[:], lhsT=el, rhs=er, start=False, stop=(k + 1 == nmm)),
                     reads=eres, writes=[pres])
                k += 1
            if mask is not None:
                ml, mres = mask
                s.op("pe", lambda e: e.matmul(ps[:], lhsT=X.ident[:], rhs=ml, start=False, stop=True),
                     reads=["ident", mres], writes=[pres])
            pk = st["pt"] % 4
            st["pt"] += 1
            s.op("act", lambda e: e.activation(out=PT[pk][:], in_=ps[:], func=AF.Exp),
                 reads=[pres], writes=["PT%d" % pk])
            return PT[pk], "PT%d" % pk

        def coef_and_acc(g, qb, branch, heads_ap, heads_res, first):
            smt = sm[g]
            sres = "sm%d" % g
            for r in range(4):
                o_ap = heads_ap(r)
                s.op("dve", lambda e: e.tensor_scalar(out=smt[:, r:r + 1], in0=o_ap[:, 64:65], scalar1=1e-36,
                                                      scalar2=None, op0=ALU.max),
                     reads=heads_res, writes=[sres])
            s.op("dve", lambda e: e.reciprocal(out=smt[:, 4:8], in_=smt[:, 0:4]), reads=[sres], writes=[sres])
            c0 = branch * 8 + g * 4
            s.op("dve", lambda e: e.tensor_tensor(out=smt[:, 8:12], in0=smt[:, 4:8], in1=glt[qb][:, c0:c0 + 4],
                                                  op=ALU.mult), reads=[sres, "gl%d" % qb], writes=[sres])
            for r in range(4):
                o_ap = heads_ap(r)
                dst = acc[qb][:, (g * 4 + r) * 64:(g * 4 + r + 1) * 64]
                if first:
                    s.op("dve", lambda e: e.tensor_scalar(out=dst, in0=o_ap[:, 0:64], scalar1=smt[:, 8 + r:9 + r],
                                                          scalar2=None, op0=ALU.mult),
                         reads=heads_res + [sres], writes=["acc%d" % qb])
                else:
                    s.op("dve", lambda e: e.scalar_tensor_tensor(out=dst, in0=o_ap[:, 0:64], scalar=smt[:, 8 + r:9 + r],
                                                                 in1=dst, op0=ALU.mult, op1=ALU.add),
                         reads=heads_res + [sres], writes=["acc%d" % qb])

        def emit_loads(qi):
            qb = qi % 2
            t0 = qi * 128
            for j in range(4):
                s.dma("sp", qt[qb][0:64, :].rearrange("d (j two t) -> d j two t", j=4, two=2)[:, j],
                      X.QTP[j, :, t0:t0 + 128].rearrange("(two d) t -> d two t", two=2),
                      reads=["QTP"], writes=["qt%d" % qb])
            s.dma("sp", qt[qb][64:67, :].rearrange("r (h t) -> r h t", h=8),
                  X.QAUG[:, :, t0:t0 + 128].rearrange("h r t -> r h t"), reads=["QAUG"], writes=["qt%d" % qb])
            s.dma("sp", AB[qb][:], X.c["c_AB"][qi], writes=["AB%d" % qb])
            s.dma("sp", glt[qb][:], X.GLS[t0:t0 + 128, :], reads=["GLS"], writes=["gl%d" % qb])

        def cmp_post_a(qi, g):
            qb = qi % 2
            ocb = lambda r: X.ps[3 + r // 2][:, (r % 2) * 193:(r % 2) * 193 + 193]
            coef_and_acc(g, qb, 0, ocb, ["ps3", "ps4"], True)
            smt = sm[g]
            sres = "sm%d" % g
            for r in range(4):
                if r == 0:
                    s.op("dve", lambda e: e.tensor_scalar(out=sc[g][:], in0=ocb(r)[:, 65:193], scalar1=smt[:, 4:5],
                                                          scalar2=None, op0=ALU.mult),
                         reads=["ps3", "ps4", sres], writes=["sc%d" % g])
                else:
                    s.op("dve", lambda e: e.scalar_tensor_tensor(out=sc[g][:], in0=ocb(r)[:, 65:193],
                                                                 scalar=smt[:, 4 + r:5 + r], in1=sc[g][:],
                                                                 op0=ALU.mult, op1=ALU.add),
                         reads=["ps3", "ps4", sres], writes=["sc%d" % g])
            s.op("dve", lambda e: e.tensor_tensor(out=sc[g][:], in0=sc[g][:], in1=AB[qb][:, 0:128], op=ALU.mult),
                 reads=["AB%d" % qb], writes=["sc%d" % g])
            s.op("dve", lambda e: e.tensor_tensor(out=sc[g][:], in0=sc[g][:], in1=AB[qb][:, 128:256], op=ALU.add),
                 reads=["AB%d" % qb], writes=["sc%d" % g])
            s.op("dve", lambda e: e.max(out=smt[:, 16:24], in_=sc[g][:]), reads=["sc%d" % g], writes=[sres])
            s.op("dve", lambda e: e.match_replace(out=sc2[g][:], in_to_replace=smt[:, 16:24], in_values=sc[g][:],
                                                  imm_value=-2.0), reads=["sc%d" % g, sres], writes=["sc2%d" % g])
            s.op("dve", lambda e: e.max(out=smt[:, 24:32], in_=sc2[g][:]), reads=["sc2%d" % g], writes=[sres])
            s.op("dve", lambda e: e.tensor_scalar(out=seln[g][:], in0=sc[g][:], scalar1=smt[:, 31:32], scalar2=NEG,
                                                  op0=ALU.is_lt, op1=ALU.mult),
                 reads=["sc%d" % g, sres], writes=["seln%d" % g])

        def cmp_post_b(qi, g):
            psb = X.ps[7][:].bitcast(BF16)
            s.op("pe", lambda e: e.transpose(out=psb[:, g * 128:(g + 1) * 128], in_=seln[g][:], identity=X.ident[:]),
                 reads=["seln%d" % g, "ident"], writes=["ps7"])
            s.op("dve", lambda e: e.tensor_copy(out=negT[g][:].rearrange("p (a t) -> p a t", a=4),
                                                in_=bcast_rep(psb[:, g * 128:(g + 1) * 128], 4)),
                 reads=["ps7"], writes=["negT%d" % g])

        def write_out(qi):
            qb = qi % 2
            t0 = qi * 128
            s.op("act", lambda e: e.activation(out=accb[qb][:], in_=acc[qb][:], func=AF.Copy),
                 reads=["acc%d" % qb], writes=["accb%d" % qb])
            psb = X.ps[7][:].bitcast(BF16)
            for j in range(4):
                s.op("pe", lambda e: e.transpose(out=psb[:, 256 + j * 128:256 + (j + 1) * 128],
                                                 in_=accb[qb][:, j * 128:(j + 1) * 128], identity=X.ident[:]),
                     reads=["accb%d" % qb, "ident"], writes=["ps7"])
            s.op("act", lambda e: e.activation(out=aT[qb][:].rearrange("p j t -> p (j t)"), in_=psb[:, 256:768], func=AF.Copy),
                 reads=["ps7"], writes=["aT%d" % qb])
            s.dma("pool", X.AT[0:512, t0:t0 + 128].rearrange("(j p) t -> p j t", p=128), aT[qb][:],
                  reads=["aT%d" % qb], writes=["AT"])

        def pv_fn(out_ap, out_res, rhs, rhs_res, first, lastt, pair_start):
            def f(pt, ptres):
                for r in range(4):
                    st_ = first and ((r % 2 == 0) if pair_start else (r == 0))
                    s.op("pe", lambda e: e.matmul(out_ap(r), lhsT=pt[:, r * 128:(r + 1) * 128], rhs=rhs,
                                                  start=st_, stop=lastt), reads=[ptres, rhs_res], writes=out_res(r))
            return f

        jobs = []
        for qi in range(X.nsa_tiles):
            qb = qi % 2
            first_job = len(jobs)
            plan = X.cmp_plan[qi]
            cs = [c for c in range(4) if plan[c] != -2]
            ocb = lambda r: X.ps[3 + r // 2][:, (r % 2) * 193:(r % 2) * 193 + 193]
            ocres = lambda r: ["ps%d" % (3 + r // 2)]
            for g in range(2):
                for ci, c in enumerate(cs):
                    mask = None if plan[c] == -1 else (mcmp[:, plan[c], :], "mcmp")
                    jobs.append(dict(
                        qk=(lambda g=g, qb=qb, c=c, mask=mask: qk_tile(g, qb, KCMP[:, g, c * 128:(c + 1) * 128], "KCMP", None, mask)),
                        pv=pv_fn(ocb, ocres, VCX[:, g, c, :], "VCX", ci == 0, ci == len(cs) - 1, True),
                        post=((lambda qi=qi, g=g: (cmp_post_a(qi, g), (cmp_post_b(qi, g) if qi < 2 else None)))
                              if ci == len(cs) - 1 else None)))
            for g in range(2):
                obr = (lambda g: (lambda r: X.ps[5 + g][:, r * 65:(r + 1) * 65]))(g)
                obres = (lambda g: (lambda r: ["ps%d" % (5 + g)]))(g)
                dl = list(range(min(4, qi), -1, -1))
                for di, dd in enumerate(dl):
                    kt = qi - dd
                    mask = (mca[:], "mca") if dd == 0 else ((mw4[:], "mw4") if dd == 4 else None)
                    posts = []
                    if di == 0 and qi >= 2:
                        posts.append(lambda qi=qi, g=g: cmp_post_b(qi, g))
                    if di == len(dl) - 1:
                        posts.append(lambda g=g, qb=qb, obr=obr: coef_and_acc(g, qb, 2, obr, ["ps%d" % (5 + g)], False))
                    jobs.append(dict(
                        qk=(lambda g=g, qb=qb, kt=kt, mask=mask: qk_tile(g, qb, KW[:, g, kt * 128:(kt + 1) * 128], "KW", None, mask)),
                        pv=pv_fn(obr, obres, VW[:, kt, g, :], "VW", di == 0, di == len(dl) - 1, False),
                        post=(lambda posts=posts: [p_() for p_ in posts])))
            for g in range(2):
                obr = (lambda g: (lambda r: X.ps[5 + g][:, r * 65:(r + 1) * 65]))(g)
                obres = (lambda g: (lambda r: ["ps%d" % (5 + g)]))(g)
                for kt in range(qi + 1):
                    mask = (mca[:], "mca") if kt == qi else None
                    posts = []
                    if kt == qi:
                        posts.append(lambda g=g, qb=qb, obr=obr: coef_and_acc(g, qb, 1, obr, ["ps%d" % (5 + g)], False))
                        if g == 1:
                            posts.append(lambda qi=qi: write_out(qi))
                    jobs.append(dict(
                        qk=(lambda g=g, qb=qb, kt=kt, mask=mask: qk_tile(
                            g, qb, KS[:, g, kt * 128:(kt + 1) * 128], "KS", (E[:, kt, :], negT[g][:], ["E", "negT%d" % g]), mask)),
                        pv=pv_fn(obr, obres, VS[:, kt, g, :], "VS", kt == 0, kt == qi, False),
                        post=(lambda posts=posts: [p_() for p_ in posts])))
            if qi + 1 < X.nsa_tiles:
                old = jobs[first_job]["post"]
                jobs[first_job]["post"] = (lambda old=old, qi=qi: ((old() if old else None), emit_loads(qi + 1)))

        LA = 2
        emit_loads(0)
        pend = {}

        def start(j):
            pend[j] = jobs[j]["qk"]()

        for j in range(min(LA, len(jobs))):
            start(j)
        for j in range(len(jobs)):
            if j + LA < len(jobs):
                start(j + LA)
            pt, ptres = pend.pop(j)
            jobs[j]["pv"](pt, ptres)
            if jobs[j]["post"]:
                jobs[j]["post"]()
        s.barrier()


PHASES.append(("nsa", phase_nsa))


def phase_ret(s, X, L, h_in):
    nc = X.nc
    with ExitStack() as es:
        sb = lambda name, shape, dt: es.enter_context(nc.sbuf_tensor("%s_L%d" % (name, L), shape, dt))
        qT = [sb("r_qT%d" % i, [64, 8, 128], BF16) for i in range(2)]
        kT = [sb("r_kT%d" % i, [64, 8, 128], BF16) for i in range(2)]
        ones1 = sb("r_ones1", [1, 128], F32)
        gn1 = sb("r_gn1", [1, 512], F32)
        km = [sb("r_km%d" % i, [128, 512], BF16) for i in range(2)]
        vm = [sb("r_vm%d" % i, [128, 512], BF16) for i in range(2)]
        gm = [sb("r_gm%d" % i, [128, 512], BF16) for i in range(2)]
        state = sb("r_state", [64, 8, 64], F32)
        stateb = sb("r_stateb", [64, 8, 64], BF16)
        rmask = sb("r_mask", [128, 512], F32)
        gC = sb("r_gC", [64, 8], F32)
        gn = sb("r_gn", [128, 512], F32)
        Pm = sb("r_Pm", [128, 8, 128], BF16)
        sq = sb("r_sq", [128, 512], F32)
        y = sb("r_y", [128, 512], F32)
        yb = [sb("r_yb%d" % i, [128, 512], BF16) for i in range(2)]
        rT = [sb("r_rT%d" % i, [128, 4, 128], BF16) for i in range(2)]
        stt = sb("r_st", [128, 64], F32)
        s.dma("sp", rmask[:], X.c["c_rmask"], writes=["rmask"])
        s.dma("sp", gC[:], X.c["c_gC8"], writes=["gC"])
        s.dma("sp", gn1[:], X.ret_gn[L:L + 1, :], writes=["gn1"])
        s.op("dve", lambda e: e.memset(ones1[:], 1.0), writes=["ones1"])
        s.op("pe", lambda e: e.matmul(X.ps[6][:], lhsT=ones1[:], rhs=gn1[:], start=True, stop=True),
             reads=["ones1", "gn1"], writes=["ps6"])
        s.op("dve", lambda e: e.tensor_copy(out=gn[:], in_=X.ps[6][:]), reads=["ps6"], writes=["gn"])
        s.op("dve", lambda e: e.memset(state[:], 0.0), writes=["state"])
        for c in range(X.ret_chunks):
            b = c % 2
            tok = slice(c * 128, (c + 1) * 128)
            for j in range(4):
                s.dma("sp", qT[b][:, 2 * j:2 * j + 2, :], X.QRT[j, :, tok].rearrange("(two d) t -> d two t", two=2),
                      reads=["QRT"], writes=["qT%d" % b])
                s.dma("sp", kT[b][:, 2 * j:2 * j + 2, :], X.KRT[j, :, tok].rearrange("(two d) t -> d two t", two=2),
                      reads=["KRT"], writes=["kT%d" % b])
            s.dma("sp", km[b][:], X.KRM[tok, :], reads=["KRM"], writes=["km%d" % b])
            s.dma("sp", vm[b][:], X.VRM[tok, :], reads=["VRM"], writes=["vm%d" % b])
            s.dma("sp", gm[b][:], X.GRM[tok, :], reads=["GRM"], writes=["gm%d" % b])
            for h in range(8):
                hp, base = h // 2, 64 * (h % 2)
                pb = h // 4
                s.op("pe", lambda e: e.matmul(X.ps[pb][:, (h % 4) * 128:(h % 4 + 1) * 128],
                                              lhsT=kT[b][:, h, :], rhs=qT[b][:, h, :],
                                              start=(h % 4 == 0), stop=True),
                     reads=["kT%d" % b, "qT%d" % b], writes=["ps%d" % pb])
            for pb in range(2):
                s.op("dve", lambda e: e.tensor_tensor(out=Pm[:, 4 * pb:4 * pb + 4, :].rearrange("p a t -> p (a t)"),
                                                      in0=X.ps[pb][:], in1=rmask[:], op=ALU.mult),
                     reads=["ps%d" % pb, "rmask"], writes=["Pm"])
            for h in range(8):
                hp, base = h // 2, 64 * (h % 2)
                s.op("pe", lambda e: e.matmul(X.ps[2][:, h * 64:(h + 1) * 64], lhsT=Pm[:, h, :],
                                              rhs=vm[b][:, h * 64:(h + 1) * 64], start=(h == 0), stop=(c == 0)),
                     reads=["Pm", "vm%d" % b], writes=["ps2"])
                if c > 0:
                    s.op("pe", lambda e: e.matmul(X.ps[2][:, h * 64:(h + 1) * 64], lhsT=qT[b][:, h, :],
                                                  rhs=stateb[:, h, :], start=False, stop=True),
                         reads=["qT%d" % b, "stateb"], writes=["ps2"])
            if True:
                for h in range(8):
                    s.op("pe", lambda e: e.matmul(X.ps[3][0:64, h * 64:(h + 1) * 64], lhsT=km[b][:, h * 64:(h + 1) * 64],
                                                  rhs=vm[b][:, h * 64:(h + 1) * 64], start=(h == 0), stop=True),
                         reads=["km%d" % b, "vm%d" % b], writes=["ps3"])
                s.op("dve", lambda e: e.tensor_tensor(out=state[:], in0=state[:],
                                                      in1=X.ps[3][0:64, :].rearrange("p (h x) -> p h x", h=8), op=ALU.add),
                     reads=["ps3"], writes=["state"])
                s.op("dve", lambda e: e.tensor_tensor(out=state[:], in0=state[:], in1=bcast_mid(gC[:], 64), op=ALU.mult),
                     reads=["gC"], writes=["state"])
                s.op("dve", lambda e: e.tensor_copy(out=stateb[:], in_=state[:]), reads=["state"], writes=["stateb"])
            o3 = X.ps[2][:].rearrange("p (h d) -> p h d", h=8)
            s.op("dve", lambda e: e.tensor_reduce(out=stt[:, 0:8], in_=o3, axis=AX.X, op=ALU.add),
                 reads=["ps2"], writes=["r_st"])
            s.op("act", lambda e: e.activation(out=sq[:], in_=X.ps[2][:], func=AF.Square), reads=["ps2"], writes=["r_sq"])
            s.op("dve", lambda e: e.tensor_reduce(out=stt[:, 8:16], in_=sq[:].rearrange("p (h d) -> p h d", h=8),
                                                  axis=AX.X, op=ALU.add), reads=["r_sq"], writes=["r_st"])
            s.op("dve", lambda e: e.tensor_scalar(out=stt[:, 16:24], in0=stt[:, 0:8], scalar1=1.0 / 64, scalar2=None,
                                                  op0=ALU.mult), reads=["r_st"], writes=["r_st"])
            s.op("dve", lambda e: e.tensor_tensor(out=stt[:, 24:32], in0=stt[:, 16:24], in1=stt[:, 16:24], op=ALU.mult),
                 reads=["r_st"], writes=["r_st"])
            s.op("dve", lambda e: e.scalar_tensor_tensor(out=stt[:, 32:40], in0=stt[:, 8:16], scalar=1.0 / 64,
                                                         in1=stt[:, 24:32], op0=ALU.mult, op1=ALU.subtract),
                 reads=["r_st"], writes=["r_st"])
            s.op("dve", lambda e: e.tensor_scalar(out=stt[:, 32:40], in0=stt[:, 32:40], scalar1=EPS, scalar2=None,
                                                  op0=ALU.add), reads=["r_st"], writes=["r_st"])
            s.op("act", lambda e: e.activation(out=stt[:, 40:48], in_=stt[:, 32:40], func=AF.Sqrt),
                 reads=["r_st"], writes=["r_st"])
            s.op("dve", lambda e: e.reciprocal(out=stt[:, 48:56], in_=stt[:, 40:48]), reads=["r_st"], writes=["r_st"])
            y3 = y[:].rearrange("p (h d) -> p h d", h=8)
            s.op("dve", lambda e: e.tensor_tensor(out=y3, in0=o3, in1=bcast_mid(stt[:, 16:24], 64), op=ALU.subtract),
                 reads=["ps2", "r_st"], writes=["r_y"])
            s.op("dve", lambda e: e.tensor_tensor(out=y3, in0=y3, in1=bcast_mid(stt[:, 48:56], 64), op=ALU.mult),
                 reads=["r_st"], writes=["r_y"])
            s.op("pool", lambda e: e.tensor_tensor(out=y[:], in0=y[:], in1=gn[:], op=ALU.mult),
                 reads=["r_y", "gn"], writes=["r_y"])
            s.op("pool", lambda e: e.tensor_tensor(out=yb[b][:], in0=y[:], in1=gm[b][:], op=ALU.mult),
                 reads=["r_y", "gm%d" % b], writes=["r_yb%d" % b])
            psb = X.ps[7][:].bitcast(BF16)
            for j in range(4):
                s.op("pe", lambda e: e.transpose(out=psb[:, j * 128:(j + 1) * 128], in_=yb[b][:, j * 128:(j + 1) * 128],
                                                 identity=X.ident[:]), reads=["r_yb%d" % b, "ident"], writes=["ps7"])
            s.op("act", lambda e: e.activation(out=rT[b][:].rearrange("p j t -> p (j t)"), in_=psb[:, 0:512], func=AF.Copy),
                 reads=["ps7"], writes=["r_rT%d" % b])
            s.dma("pool", X.AT[512:1024, tok].rearrange("(j p) t -> p j t", p=128), rT[b][:],
                  reads=["r_rT%d" % b], writes=["AT"])
        s.barrier()


PHASES.append(("ret", phase_ret))


def convert_weights(s, X, layers):
    def conv(dst, src, rows):
        for r0 in range(0, rows, 512):
            s.dma("pool", dst[r0:r0 + 512, :], src[r0:r0 + 512, :], writes=["W16"])
    if 0 in layers:
        conv(X.FG16[0], X.ffn_gate[0], D)
        conv(X.FU16[0], X.ffn_up[0], D)
        conv(X.FD16[0], X.ffn_down[0], D_FF)
    if 1 in layers:
        for e in range(NE):
            for blk in range(7):
                r0 = (e * 7 + blk) * 128
                fs = slice(blk * 512, (blk + 1) * 512)
                s.dma("pool", X.WGB[r0:r0 + 128, :].rearrange("p (k f) -> p k f", k=8),
                      X.moe_gate[0, e][:, fs].rearrange("(k p) f -> p k f", p=128), writes=["W16"])
                s.dma("pool", X.WUB[r0:r0 + 128, :].rearrange("p (k f) -> p k f", k=8),
                      X.moe_up[0, e][:, fs].rearrange("(k p) f -> p k f", p=128), writes=["W16"])
                s.dma("pool", X.WDB[r0:r0 + 128, :].rearrange("p (c n) -> p c n", c=4),
                      X.moe_down[0, e][fs, :].rearrange("(c p) n -> p c n", p=128), writes=["W16"])


def phase_ffn(s, X, L, h_in):
    nc = X.nc
    last = (L == DEPTH - 1)
    moe = (L % 2 == 1)
    if moe and X.routed:
        return phase_moe(s, X, L, h_in)
    h_out = X.out if last else X.HB
    G16, U16, D16 = (X.MG16, X.MU16, X.MD16) if moe else (X.FG16, X.FU16, X.FD16)
    experts = list(range(NE)) if moe else [0]
    with ExitStack() as es:
        sb = lambda name, shape, dt: es.enter_context(nc.sbuf_tensor("%s_L%d" % (name, L), shape, dt))
        wout = sb("f_wout", [128, 8, D], BF16)
        pgate = sb("f_pgate", [128, 8, D], BF16)
        pproj = sb("f_pproj", [128, 2, D], BF16)
        g2T = sb("f_g2T", [128, 8], F32)
        g3T = sb("f_g3T", [128, 8], F32)
        rw = sb("f_rw", [128, 8, NE], BF16)
        cT = sb("f_cT", [128, 8, 512], BF16)
        hin = [sb("f_hin%d" % i, [128, D], F32) for i in range(2)]
        h1 = sb("f_h1", [128, 4, D], F32)
        hnT = sb("f_hnT", [128, 8, 512], BF16)
        acc = sb("f_acc", [128, 4, D], F32)
        wg = [sb("f_wg%d" % i, [128, 8, 512], BF16) for i in range(2)]
        wu = [sb("f_wu%d" % i, [128, 8, 512], BF16) for i in range(2)]
        wd = [sb("f_wd%d" % i, [128, 4, D], BF16) for i in range(2)]
        silu_t = [sb("f_silu%d" % i, [128, 512], BF16) for i in range(2)]
        actb = [sb("f_actb%d" % i, [128, 4, 512], BF16) for i in range(2)]
        sig = [sb("f_sig%d" % i, [128, 512], F32) for i in range(2)]
        ptile = [sb("f_pt%d" % i, [128, 256], F32) for i in range(2)]
        pb = sb("f_pb", [128, 256], BF16)
        pT = sb("f_pT", [128, 2, 512], BF16)
        gates = sb("f_gates", [128, 4, NE], F32)
        lg = sb("f_lg", [128, 16], F32)
        sm = sb("f_sm", [128, 32], F32)
        for kc in range(8):
            s.dma("pool", wout[:, kc, :], X.w_out[L, kc * 128:(kc + 1) * 128, :], writes=["wout"])
            s.dma("pool", pgate[:, kc, :], X.ple_gate[L, kc * 128:(kc + 1) * 128, :], writes=["pgate"])
        for kc in range(2):
            s.dma("pool", pproj[:, kc, :], X.ple_proj[L, kc * 128:(kc + 1) * 128, :], writes=["pproj"])
        s.dma("sp", g2T[:], X.g_ffn[L].rearrange("(k p) -> p k", p=128), writes=["g2T"], allow_slow_non_contiguous=True)
        s.dma("sp", g3T[:], X.g_ple[L].rearrange("(k p) -> p k", p=128), writes=["g3T"], allow_slow_non_contiguous=True)
        if moe:
            s.dma("pool", rw[:], X.moe_router[0].rearrange("(k p) e -> p k e", p=128), writes=["rw"])
        if last:
            gfin = sb("f_gfin", [128, D], F32)
            ones1 = sb("f_ones1", [1, 128], F32)
            gf1 = sb("f_gf1", [1, D], F32)
            s.dma("sp", gf1[:], X.g_final.rearrange("(o n) -> o n", o=1), writes=["gf1"])
            s.op("dve", lambda e: e.memset(ones1[:], 1.0), writes=["ones1"])
            for half in range(2):
                s.op("pe", lambda e: e.matmul(X.ps[6][:], lhsT=ones1[:], rhs=gf1[:, half * 512:(half + 1) * 512],
                                              start=True, stop=True), reads=["ones1", "gf1"], writes=["ps6"])
                s.op("dve", lambda e: e.tensor_copy(out=gfin[:, half * 512:(half + 1) * 512], in_=X.ps[6][:]),
                     reads=["ps6"], writes=["gfin"])
        wcnt = 0
        for tg in range(NT // 4):
            toks = slice(tg * 512, (tg + 1) * 512)
            s.dma("sp", cT[:], X.AT[:, toks].rearrange("(k p) t -> p k t", p=128), reads=["AT"], writes=["cT"])
            for tt in range(4):
                ti = 4 * tg + tt
                hb_ = hin[ti % 2]
                s.dma("sp", hb_[:], h_in[ti * 128:(ti + 1) * 128, :], reads=["HB"], writes=["hin%d" % (ti % 2)])
                for half in range(2):
                    hs = slice(half * 512, (half + 1) * 512)
                    for k in range(8):
                        s.op("pe", lambda e: e.matmul(X.ps[6][:], lhsT=cT[:, k, tt * 128:(tt + 1) * 128], rhs=wout[:, k, hs],
                                                      start=(k == 0), stop=(k == 7)), reads=["cT", "wout"], writes=["ps6"])
                    s.op("dve", lambda e: e.tensor_tensor(out=h1[:, tt, hs], in0=X.ps[6][:], in1=hb_[:, hs], op=ALU.add),
                         reads=["ps6", "hin%d" % (ti % 2)], writes=["h1_%d" % tt])
                ln_a(s, X, h1[:, tt, :], "h1_%d" % tt, tt)
            for tt in range(4):
                ln_b(s, X, tt, g2T[:], "g2T", hnT[:, :, tt * 128:(tt + 1) * 128], "f_hnT")
            if moe:
                for tt in range(4):
                    for k in range(8):
                        s.op("pe", lambda e: e.matmul(X.ps[6][:, 0:NE], lhsT=hnT[:, k, tt * 128:(tt + 1) * 128], rhs=rw[:, k, :],
                                                      start=(k == 0), stop=(k == 7)), reads=["f_hnT", "rw"], writes=["ps6"])
                    s.op("dve", lambda e: e.tensor_copy(out=lg[:, 0:8], in_=X.ps[6][:, 0:NE]), reads=["ps6"], writes=["lg"])
                    s.op("dve", lambda e: e.max(out=sm[:, 0:8], in_=lg[:, 0:8]), reads=["lg"], writes=["fsm"])
                    s.op("dve", lambda e: e.tensor_tensor(out=sm[:, 8:9], in0=sm[:, 1:2], in1=sm[:, 0:1], op=ALU.subtract),
                         reads=["fsm"], writes=["fsm"])
                    s.op("act", lambda e: e.activation(out=sm[:, 9:10], in_=sm[:, 8:9], func=AF.Exp), reads=["fsm"], writes=["fsm"])
                    s.op("dve", lambda e: e.tensor_scalar(out=sm[:, 10:11], in0=sm[:, 9:10], scalar1=1.0, scalar2=None,
                                                          op0=ALU.add), reads=["fsm"], writes=["fsm"])
                    s.op("dve", lambda e: e.reciprocal(out=sm[:, 11:12], in_=sm[:, 10:11]), reads=["fsm"], writes=["fsm"])
                    s.op("dve", lambda e: e.tensor_tensor(out=sm[:, 12:13], in0=sm[:, 9:10], in1=sm[:, 11:12], op=ALU.mult),
                         reads=["fsm"], writes=["fsm"])
                    s.op("dve", lambda e: e.tensor_scalar(out=lg[:, 8:16], in0=lg[:, 0:8], scalar1=sm[:, 0:1],
                                                          scalar2=sm[:, 11:12], op0=ALU.is_equal, op1=ALU.mult),
                         reads=["lg", "fsm"], writes=["lg"])
                    s.op("dve", lambda e: e.tensor_scalar(out=gates[:, tt, :], in0=lg[:, 0:8], scalar1=sm[:, 1:2],
                                                          scalar2=sm[:, 12:13], op0=ALU.is_equal, op1=ALU.mult),
                         reads=["lg", "fsm"], writes=["gates"])
                    s.op("dve", lambda e: e.tensor_tensor(out=gates[:, tt, :], in0=gates[:, tt, :], in1=lg[:, 8:16], op=ALU.add),
                         reads=["lg"], writes=["gates"])
            first = True
            for ex in experts:
                for blk in range(D_FF // 512):
                    wb = wcnt % 2
                    wcnt += 1
                    fs = slice(blk * 512, (blk + 1) * 512)
                    s.dma("sp", wg[wb][:], G16[ex][:, fs].rearrange("(k p) f -> p k f", p=128), reads=["W16"], writes=["wg%d" % wb])
                    s.dma("sp", wu[wb][:], U16[ex][:, fs].rearrange("(k p) f -> p k f", p=128), reads=["W16"], writes=["wu%d" % wb])
                    s.dma("sp", wd[wb][:], D16[ex][fs, :].rearrange("(c p) n -> p c n", p=128), reads=["W16"], writes=["wd%d" % wb])
                    for fc in range(4):
                        pg, pu = 2 * (fc % 2), 2 * (fc % 2) + 1
                        for k in range(8):
                            s.op("pe", lambda e: e.matmul(X.ps[pg][:], lhsT=wg[wb][:, k, fc * 128:(fc + 1) * 128], rhs=hnT[:, k, :],
                                                          start=(k == 0), stop=(k == 7)),
                                 reads=["wg%d" % wb, "f_hnT"], writes=["ps%d" % pg])
                        for k in range(8):
                            s.op("pe", lambda e: e.matmul(X.ps[pu][:], lhsT=wu[wb][:, k, fc * 128:(fc + 1) * 128], rhs=hnT[:, k, :],
                                                          start=(k == 0), stop=(k == 7)),
                                 reads=["wu%d" % wb, "f_hnT"], writes=["ps%d" % pu])
                        s.op("act", lambda e: e.activation(out=silu_t[fc % 2][:], in_=X.ps[pg][:], func=AF.Silu),
                             reads=["ps%d" % pg], writes=["silu%d" % (fc % 2)])
                        s.op("dve", lambda e: e.tensor_tensor(out=actb[wb][:, fc, :], in0=X.ps[pu][:], in1=silu_t[fc % 2][:],
                                                              op=ALU.mult),
                             reads=["ps%d" % pu, "silu%d" % (fc % 2)], writes=["actb%d" % wb])
                    for tt in range(4):
                        for half in range(2):
                            hs = slice(half * 512, (half + 1) * 512)
                            pd = 4 + half
                            for fc in range(4):
                                s.op("pe", lambda e: e.matmul(X.ps[pd][:], lhsT=actb[wb][:, fc, tt * 128:(tt + 1) * 128],
                                                              rhs=wd[wb][:, fc, hs], start=(fc == 0), stop=(fc == 3)),
                                     reads=["actb%d" % wb, "wd%d" % wb], writes=["ps%d" % pd])
                            dst = acc[:, tt, hs]
                            if moe:
                                gsc = gates[:, tt, ex:ex + 1]
                                if first:
                                    s.op("dve", lambda e: e.tensor_scalar(out=dst, in0=X.ps[pd][:], scalar1=gsc, scalar2=None,
                                                                          op0=ALU.mult),
                                         reads=["ps%d" % pd, "gates"], writes=["acc%d" % tt])
                                else:
                                    s.op("dve", lambda e: e.scalar_tensor_tensor(out=dst, in0=X.ps[pd][:], scalar=gsc, in1=dst,
                                                                                 op0=ALU.mult, op1=ALU.add),
                                         reads=["ps%d" % pd, "gates"], writes=["acc%d" % tt])
                            else:
                                if first:
                                    s.op("dve", lambda e: e.tensor_copy(out=dst, in_=X.ps[pd][:]),
                                         reads=["ps%d" % pd], writes=["acc%d" % tt])
                                else:
                                    s.op("dve", lambda e: e.tensor_tensor(out=dst, in0=X.ps[pd][:], in1=dst, op=ALU.add),
                                         reads=["ps%d" % pd], writes=["acc%d" % tt])
                    first = False
            for tt in range(4):
                s.op("pool", lambda e: e.tensor_tensor(out=h1[:, tt, :], in0=h1[:, tt, :], in1=acc[:, tt, :], op=ALU.add),
                     reads=["acc%d" % tt], writes=["h1_%d" % tt])
                ln_a(s, X, h1[:, tt, :], "h1_%d" % tt, tt)
            for tt in range(4):
                ln_b(s, X, tt, g3T[:], "g3T", hnT[:, :, tt * 128:(tt + 1) * 128], "f_hnT")
            psb = X.ps[7][:].bitcast(BF16)
            for tt in range(4):
                ti = 4 * tg + tt
                pt_ = ptile[ti % 2]
                s.dma("sp", pt_[:], X.p[L, ti * 128:(ti + 1) * 128, :], writes=["pt%d" % (ti % 2)])
                s.op("act", lambda e: e.activation(out=pb[:], in_=pt_[:], func=AF.Copy), reads=["pt%d" % (ti % 2)], writes=["pb"])
                for k in range(2):
                    s.op("pe", lambda e: e.transpose(out=psb[:, k * 128:(k + 1) * 128], in_=pb[:, k * 128:(k + 1) * 128],
                                                     identity=X.ident[:]), reads=["pb", "ident"], writes=["ps7"])
                s.op("act", lambda e: e.activation(out=pT[:, :, tt * 128:(tt + 1) * 128],
                                                   in_=psb[:, 0:256].rearrange("p (k t) -> p k t", k=2), func=AF.Copy),
                     reads=["ps7"], writes=["pT"])
            for tt in range(4):
                ti = 4 * tg + tt
                for half in range(2):
                    hs = slice(half * 512, (half + 1) * 512)
                    for k in range(2):
                        s.op("pe", lambda e: e.matmul(X.ps[6][:], lhsT=pT[:, k, tt * 128:(tt + 1) * 128], rhs=pproj[:, k, hs],
                                                      start=(k == 0), stop=(k == 1)), reads=["pT", "pproj"], writes=["ps6"])
                    pgb = 4 + half
                    for k in range(8):
                        s.op("pe", lambda e: e.matmul(X.ps[pgb][:], lhsT=hnT[:, k, tt * 128:(tt + 1) * 128], rhs=pgate[:, k, hs],
                                                      start=(k == 0), stop=(k == 7)), reads=["f_hnT", "pgate"], writes=["ps%d" % pgb])
                    s.op("act", lambda e: e.activation(out=sig[half][:], in_=X.ps[pgb][:], func=AF.Sigmoid),
                         reads=["ps%d" % pgb], writes=["sig%d" % half])
                    s.op("dve", lambda e: e.tensor_tensor(out=acc[:, tt, hs], in0=X.ps[6][:], in1=sig[half][:], op=ALU.mult),
                         reads=["ps6", "sig%d" % half], writes=["acc%d" % tt])
                s.op("pool", lambda e: e.tensor_tensor(out=h1[:, tt, :], in0=h1[:, tt, :], in1=acc[:, tt, :], op=ALU.add),
                     reads=["acc%d" % tt], writes=["h1_%d" % tt])
                if last:
                    ln_a(s, X, h1[:, tt, :], "h1_%d" % tt, tt)
                    s.op("act", lambda e: e.activation(out=acc[:, tt, :], in_=h1[:, tt, :], func=AF.Copy, scale=X.ss[tt][:, 2:3]),
                         reads=["h1_%d" % tt, "ss%d" % tt], writes=["acc%d" % tt])
                    s.op("pool", lambda e: e.tensor_tensor(out=acc[:, tt, :], in0=acc[:, tt, :], in1=gfin[:], op=ALU.mult),
                         reads=["gfin"], writes=["acc%d" % tt])
                    s.dma("pool", h_out[ti * 128:(ti + 1) * 128, :], acc[:, tt, :], reads=["acc%d" % tt], writes=["HOUT"])
                else:
                    s.dma("pool", h_out[ti * 128:(ti + 1) * 128, :], h1[:, tt, :], reads=["h1_%d" % tt], writes=["HB"])
        s.barrier()


PHASES.append(("ffn", phase_ffn))


def phase_moe(s, X, L, h_in):
    nc = X.nc
    last = (L == DEPTH - 1)
    h_out = X.out if last else X.HB
    with ExitStack() as es:
        sb = lambda name, shape, dt: es.enter_context(nc.sbuf_tensor("%s_L%d" % (name, L), shape, dt))
        OH = [sb("m_oh%d" % k, [128, NT, NE], F32) for k in range(2)]
        WK = [sb("m_wk%d" % k, [128, NT], F32) for k in range(2)]
        POSI2 = sb("m_posi", [128, 2 * NT], I32)
        POSI = POSI2[:].rearrange("p (k i) -> p k i", k=2)
        WIDX2 = sb("m_widx", [128, NGRP * 7], I32)
        WIDX = WIDX2[:].rearrange("p (g b) -> p g b", b=7)
        g2T = sb("m_g2T", [128, 8], F32)
        g3T = sb("m_g3T", [128, 8], F32)
        hnT = sb("m_hnT", [128, 8, 512], BF16)
        s.dma("sp", g2T[:], X.g_ffn[L].rearrange("(k p) -> p k", p=128), writes=["g2T"], allow_slow_non_contiguous=True)
        s.dma("sp", g3T[:], X.g_ple[L].rearrange("(k p) -> p k", p=128), writes=["g3T"], allow_slow_non_contiguous=True)
        with ExitStack() as es2:
            sb2 = lambda name, shape, dt: es2.enter_context(nc.sbuf_tensor("%s_L%d" % (name, L), shape, dt))
            wout = sb2("ma_wout", [128, 8, D], BF16)
            rw = sb2("ma_rw", [128, 8, NE], BF16)
            cT = sb2("ma_cT", [128, 8, 512], BF16)
            hin = [sb2("ma_hin%d" % i, [128, D], F32) for i in range(2)]
            h1 = [sb2("ma_h1%d" % i, [128, D], F32) for i in range(4)]
            lg = sb2("ma_lg", [128, 16], F32)
            sm = sb2("ma_sm", [128, 32], F32)
            for kc in range(8):
                s.dma("pool", wout[:, kc, :], X.w_out[L, kc * 128:(kc + 1) * 128, :], writes=["wout"])
            s.dma("pool", rw[:], X.moe_router[0].rearrange("(k p) e -> p k e", p=128), writes=["rw"])
            for tg in range(NT // 4):
                toks = slice(tg * 512, (tg + 1) * 512)
                s.dma("sp", cT[:], X.AT[:, toks].rearrange("(k p) t -> p k t", p=128), reads=["AT"], writes=["cT"])
                for tt in range(4):
                    ti = 4 * tg + tt
                    hb_ = hin[ti % 2]
                    s.dma("sp", hb_[:], h_in[ti * 128:(ti + 1) * 128, :], reads=["HB"], writes=["hin%d" % (ti % 2)])
                    for half in range(2):
                        hs = slice(half * 512, (half + 1) * 512)
                        pbk = 4 + half
                        for k in range(8):
                            s.op("pe", lambda e: e.matmul(X.ps[pbk][:], lhsT=cT[:, k, tt * 128:(tt + 1) * 128], rhs=wout[:, k, hs],
                                                          start=(k == 0), stop=(k == 7)), reads=["cT", "wout"], writes=["ps%d" % pbk])
                        s.op("dve", lambda e: e.tensor_tensor(out=h1[tt][:, hs], in0=X.ps[pbk][:], in1=hb_[:, hs], op=ALU.add),
                             reads=["ps%d" % pbk, "hin%d" % (ti % 2)], writes=["h1_%d" % tt])
                    s.dma("pool", X.H1[ti * 128:(ti + 1) * 128, :], h1[tt][:], reads=["h1_%d" % tt], writes=["H1"])
                    ln_a(s, X, h1[tt][:], "h1_%d" % tt, tt)
                    s.dma("pool", X.XS2[ti * 128:(ti + 1) * 128, :], X.xs[tt][:], reads=["xs%d" % tt], writes=["XS2"])
                for tt in range(4):
                    ln_b(s, X, tt, g2T[:], "g2T", hnT[:, :, tt * 128:(tt + 1) * 128], "m_hnT")
                for tt in range(4):
                    ti = 4 * tg + tt
                    for k in range(8):
                        s.op("pe", lambda e: e.matmul(X.ps[6][:, 0:NE], lhsT=hnT[:, k, tt * 128:(tt + 1) * 128], rhs=rw[:, k, :],
                                                      start=(k == 0), stop=(k == 7)), reads=["m_hnT", "rw"], writes=["ps6"])
                    s.op("dve", lambda e: e.tensor_copy(out=lg[:, 0:8], in_=X.ps[6][:, 0:NE]), reads=["ps6"], writes=["lg"])
                    s.op("dve", lambda e: e.max(out=sm[:, 0:8], in_=lg[:, 0:8]), reads=["lg"], writes=["fsm"])
                    s.op("dve", lambda e: e.tensor_tensor(out=sm[:, 8:9], in0=sm[:, 1:2], in1=sm[:, 0:1], op=ALU.subtract),
                         reads=["fsm"], writes=["fsm"])
                    s.op("act", lambda e: e.activation(out=sm[:, 9:10], in_=sm[:, 8:9], func=AF.Exp), reads=["fsm"], writes=["fsm"])
                    s.op("dve", lambda e: e.tensor_scalar(out=sm[:, 10:11], in0=sm[:, 9:10], scalar1=1.0, scalar2=None,
                                                          op0=ALU.add), reads=["fsm"], writes=["fsm"])
                    s.op("dve", lambda e: e.reciprocal(out=WK[0][:, ti:ti + 1], in_=sm[:, 10:11]), reads=["fsm"], writes=["WK"])
                    s.op("dve", lambda e: e.tensor_tensor(out=WK[1][:, ti:ti + 1], in0=sm[:, 9:10], in1=WK[0][:, ti:ti + 1],
                                                          op=ALU.mult), reads=["fsm", "WK"], writes=["WK"])
                    s.op("dve", lambda e: e.tensor_scalar(out=OH[0][:, ti, :], in0=lg[:, 0:8], scalar1=sm[:, 0:1], scalar2=None,
                                                          op0=ALU.is_equal), reads=["lg", "fsm"], writes=["OH"])
                    s.op("dve", lambda e: e.tensor_scalar(out=OH[1][:, ti, :], in0=lg[:, 0:8], scalar1=sm[:, 1:2], scalar2=None,
                                                          op0=ALU.is_equal), reads=["lg", "fsm"], writes=["OH"])
            s.barrier()
        with ExitStack() as es2:
            sb2 = lambda name, shape, dt: es2.enter_context(nc.sbuf_tensor("%s_L%d" % (name, L), shape, dt))
            Mf = sb2("mb_Mf", [128, NT, NE], F32)
            Mb = sb2("mb_Mb", [128, NT * NE], BF16)
            U = sb2("mb_U", [128, 128], BF16)
            ones = sb2("mb_ones", [128, 128], BF16)
            A_ = sb2("mb_A", [128, NT, NE], F32)
            B_ = sb2("mb_B", [128, NT, NE], F32)
            CN = sb2("mb_CN", [128, NT, NE], F32)
            POS = sb2("mb_POS", [128, NT, NE], F32)
            nE = sb2("mb_nE", [128, 64], F32)
            cmp16 = sb2("mb_cmp16", [128, NE, 16], F32)
            gi512 = sb2("mb_gi512", [128, NGRP], F32)
            tokid = sb2("mb_tokid", [128, NT], F32)
            blkp = sb2("mb_blkp", [128, 7], F32)
            cmpg = sb2("mb_cmpg", [128, NGRP, NE], F32)
            EG = sb2("mb_EG", [128, NGRP], F32)
            WF = sb2("mb_WF", [128, NGRP, 7], F32)
            PK = sb2("mb_PK", [128, 2, NT], F32)
            ROWS2 = sb2("mb_rows", [128, 2 * NT * 2], I32)
            ROWS = ROWS2[:].rearrange("p (k i c) -> p k i c", k=2, c=2)
            Z = sb2("mb_Z", [128, 2 * NSLOT // 128], I32)
            s.dma("pool", U[:], X.c["c_U"], writes=["U"])
            s.dma("sp", gi512[:], X.c["c_gi512"], writes=["gi512"])
            s.dma("sp", tokid[:], X.c["c_tokid"], writes=["tokid"])
            s.dma("sp", blkp[:], X.c["c_blkp"], writes=["blkp"])
            s.op("pool", lambda e: e.memset(ones[:], 1.0), writes=["ones"])
            s.op("pool", lambda e: e.memset(Z[:], 0), writes=["Z"])
            s.dma("sp", X.SLOT.rearrange("(p a) c -> p (a c)", p=128), Z[:], reads=["Z"], writes=["SLOT"])
            s.op("dve", lambda e: e.tensor_tensor(out=Mf[:], in0=OH[0][:], in1=OH[1][:], op=ALU.add), reads=["OH"], writes=["Mf"])
            s.op("dve", lambda e: e.tensor_copy(out=Mb[:], in_=Mf[:].rearrange("p i e -> p (i e)")), reads=["Mf"], writes=["Mb"])
            s.op("pe", lambda e: e.matmul(X.ps[0][:], lhsT=U[:], rhs=Mb[:], start=True, stop=True), reads=["U", "Mb"], writes=["ps0"])
            s.op("pe", lambda e: e.matmul(X.ps[1][:], lhsT=ones[:], rhs=Mb[:], start=True, stop=True), reads=["ones", "Mb"], writes=["ps1"])
            s.op("dve", lambda e: e.tensor_copy(out=CN[:].rearrange("p i e -> p (i e)"), in_=X.ps[1][:]), reads=["ps1"], writes=["CN"])
            s.op("dve", lambda e: e.tensor_copy(out=A_[:], in_=CN[:]), reads=["CN"], writes=["A"])
            src, dst, sres, dres = A_, B_, "A", "B"
            for sh in (1, 2, 4, 8, 16, 32):
                s.op("dve", lambda e: e.tensor_copy(out=dst[:, 0:sh, :], in_=src[:, 0:sh, :]), reads=[sres], writes=[dres])
                s.op("dve", lambda e: e.tensor_tensor(out=dst[:, sh:NT, :], in0=src[:, sh:NT, :], in1=src[:, 0:NT - sh, :],
                                                      op=ALU.add), reads=[sres], writes=[dres])
                src, dst, sres, dres = dst, src, dres, sres
            incl, ires = src, sres
            s.op("dve", lambda e: e.tensor_copy(out=nE[:, 0:8], in_=incl[:, NT - 1, :]), reads=[ires], writes=["nE"])
            s.op("dve", lambda e: e.tensor_tensor(out=cmp16[:], in0=bcast_mid(nE[:, 0:8], 16), in1=bcast_rep(gi512[:, 0:16], NE),
                                                  op=ALU.is_gt), reads=["nE", "gi512"], writes=["cmp16"])
            s.op("dve", lambda e: e.tensor_reduce(out=nE[:, 8:16], in_=cmp16[:], axis=AX.X, op=ALU.add), reads=["cmp16"], writes=["nE"])
            s.op("dve", lambda e: e.tensor_scalar(out=nE[:, 8:16], in0=nE[:, 8:16], scalar1=512.0, scalar2=None, op0=ALU.mult),
                 reads=["nE"], writes=["nE"])
            s.op("dve", lambda e: e.tensor_copy(out=nE[:, 16:17], in_=nE[:, 8:9]), reads=["nE"], writes=["nE"])
            for e_ in range(1, NE):
                s.op("dve", lambda e: e.tensor_tensor(out=nE[:, 16 + e_:17 + e_], in0=nE[:, 15 + e_:16 + e_], in1=nE[:, 8 + e_:9 + e_],
                                                      op=ALU.add), reads=["nE"], writes=["nE"])
            s.op("dve", lambda e: e.tensor_tensor(out=nE[:, 24:32], in0=nE[:, 16:24], in1=nE[:, 8:16], op=ALU.subtract),
                 reads=["nE"], writes=["nE"])
            s.op("dve", lambda e: e.tensor_tensor(out=POS[:], in0=incl[:], in1=CN[:], op=ALU.subtract), reads=[ires, "CN"], writes=["POS"])
            s.op("dve", lambda e: e.tensor_tensor(out=POS[:].rearrange("p i e -> p (i e)"), in0=POS[:].rearrange("p i e -> p (i e)"),
                                                  in1=X.ps[0][:], op=ALU.add), reads=["ps0"], writes=["POS"])
            s.op("dve", lambda e: e.tensor_tensor(out=POS[:], in0=POS[:], in1=bcast_rep(nE[:, 24:32], NT), op=ALU.add),
                 reads=["nE"], writes=["POS"])
            for k in range(2):
                s.op("dve", lambda e: e.tensor_tensor(out=Mf[:], in0=POS[:], in1=OH[k][:], op=ALU.mult), reads=["POS", "OH"], writes=["Mf"])
                s.op("dve", lambda e: e.tensor_reduce(out=PK[:, k, :], in_=Mf[:], axis=AX.X, op=ALU.add), reads=["Mf"], writes=["PK"])
                s.op("dve", lambda e: e.tensor_copy(out=ROWS[:, k, :, 0], in_=tokid[:]), reads=["tokid"], writes=["ROWS"])
                s.op("dve", lambda e: e.tensor_copy(out=ROWS[:, k, :, 1], in_=WK[k][:].bitcast(I32)), reads=["WK"], writes=["ROWS"])
            s.op("dve", lambda e: e.tensor_copy(out=POSI, in_=PK[:]), reads=["PK"], writes=["POSI"])
            s.op("dve", lambda e: e.tensor_tensor(out=cmpg[:], in0=bcast_rep(nE[:, 16:24], NGRP), in1=bcast_mid(gi512[:], NE),
                                                  op=ALU.is_le), reads=["nE", "gi512"], writes=["cmpg"])
            s.op("dve", lambda e: e.tensor_reduce(out=EG[:], in_=cmpg[:], axis=AX.X, op=ALU.add), reads=["cmpg"], writes=["EG"])
            s.op("dve", lambda e: e.tensor_scalar(out=EG[:], in0=EG[:], scalar1=float(NE - 1), scalar2=896.0, op0=ALU.min, op1=ALU.mult),
                 reads=["EG"], writes=["EG"])
            s.op("dve", lambda e: e.tensor_tensor(out=WF[:], in0=bcast_mid(EG[:], 7), in1=bcast_rep(blkp[:], NGRP), op=ALU.add),
                 reads=["EG", "blkp"], writes=["WF"])
            s.op("dve", lambda e: e.tensor_copy(out=WIDX, in_=WF[:]), reads=["WF"], writes=["WIDX"])
            for ti in range(NT):
                for k in range(2):
                    s.idma(X.SLOT[:, :], POSI2[:, k * NT + ti:k * NT + ti + 1],
                           ROWS2[:, (k * NT + ti) * 2:(k * NT + ti) * 2 + 2], None, NSLOT - 1,
                           reads=["ROWS", "POSI"], writes=["SLOT"])
            s.barrier()
        with ExitStack() as es2:
            sb2 = lambda name, shape, dt: es2.enter_context(nc.sbuf_tensor("%s_L%d" % (name, L), shape, dt))
            stt = [sb2("mc_st%d" % i, [128, 8], I32) for i in range(2)]
            xg = [sb2("mc_xg%d" % i, [128, D], BF16) for i in range(4)]
            acc = [sb2("mc_acc%d" % i, [128, 4, D], F32) for i in range(2)]
            wg = [sb2("mc_wg%d" % i, [128, 8, 512], BF16) for i in range(2)]
            wu = [sb2("mc_wu%d" % i, [128, 8, 512], BF16) for i in range(2)]
            wd = [sb2("mc_wd%d" % i, [128, 4, D], BF16) for i in range(2)]
            silu_t = [sb2("mc_silu%d" % i, [128, 512], BF16) for i in range(2)]
            actb = [sb2("mc_actb%d" % i, [128, 4, 512], BF16) for i in range(2)]
            hn = [sb2("mc_hn%d" % i, [128, 8, 512], BF16) for i in range(2)]
            wcnt = 0
            for gi in range(NGRP):
                gb = gi % 2
                s.dma("sp", stt[gb][:].rearrange("p (tt c) -> p tt c", c=2), X.SLOT[gi * 512:(gi + 1) * 512, :].rearrange("(tt p) c -> p tt c", p=128),
                      reads=["SLOT"], writes=["st%d" % gb])
                for tt in range(4):
                    s.idma(xg[tt][:, :], None, X.XS2[:, :], stt[gb][:, 2 * tt:2 * tt + 1], SEQ - 1,
                           reads=["XS2", "st%d" % gb], writes=["xg%d" % tt])
                for tt in range(4):
                    ln_b(s, X, tt, g2T[:], "g2T", hn[gb][:, :, tt * 128:(tt + 1) * 128], "hn%d" % gb, src=xg[tt], src_res="xg%d" % tt)
                for blk in range(7):
                    wb = wcnt % 2
                    wcnt += 1
                    s.idma(wg[wb][:].rearrange("p k f -> p (k f)"), None, X.WGB[:, :], WIDX2[:, gi * 7 + blk:gi * 7 + blk + 1], NE * 7 * 128 - 1,
                           reads=["W16", "WIDX"], writes=["wg%d" % wb])
                    s.idma(wu[wb][:].rearrange("p k f -> p (k f)"), None, X.WUB[:, :], WIDX2[:, gi * 7 + blk:gi * 7 + blk + 1], NE * 7 * 128 - 1,
                           reads=["W16", "WIDX"], writes=["wu%d" % wb])
                    s.idma(wd[wb][:].rearrange("p c n -> p (c n)"), None, X.WDB[:, :], WIDX2[:, gi * 7 + blk:gi * 7 + blk + 1], NE * 7 * 128 - 1,
                           reads=["W16", "WIDX"], writes=["wd%d" % wb])
                    for fc in range(4):
                        pg, pu = 2 * (fc % 2), 2 * (fc % 2) + 1
                        for k in range(8):
                            s.op("pe", lambda e: e.matmul(X.ps[pg][:], lhsT=wg[wb][:, k, fc * 128:(fc + 1) * 128], rhs=hn[gb][:, k, :],
                                                          start=(k == 0), stop=(k == 7)),
                                 reads=["wg%d" % wb, "hn%d" % gb], writes=["ps%d" % pg])
                        for k in range(8):
                            s.op("pe", lambda e: e.matmul(X.ps[pu][:], lhsT=wu[wb][:, k, fc * 128:(fc + 1) * 128], rhs=hn[gb][:, k, :],
                                                          start=(k == 0), stop=(k == 7)),
                                 reads=["wu%d" % wb, "hn%d" % gb], writes=["ps%d" % pu])
                        s.op("act", lambda e: e.activation(out=silu_t[fc % 2][:], in_=X.ps[pg][:], func=AF.Silu),
                             reads=["ps%d" % pg], writes=["silu%d" % (fc % 2)])
                        s.op("dve", lambda e: e.tensor_tensor(out=actb[wb][:, fc, :], in0=X.ps[pu][:], in1=silu_t[fc % 2][:],
                                                              op=ALU.mult),
                             reads=["ps%d" % pu, "silu%d" % (fc % 2)], writes=["actb%d" % wb])
                    for tt in range(4):
                        gsc = stt[gb][:, 2 * tt + 1:2 * tt + 2].bitcast(F32)
                        for half in range(2):
                            hs = slice(half * 512, (half + 1) * 512)
                            pd = 4 + half
                            for fc in range(4):
                                s.op("pe", lambda e: e.matmul(X.ps[pd][:], lhsT=actb[wb][:, fc, tt * 128:(tt + 1) * 128],
                                                              rhs=wd[wb][:, fc, hs], start=(fc == 0), stop=(fc == 3)),
                                     reads=["actb%d" % wb, "wd%d" % wb], writes=["ps%d" % pd])
                            dst = acc[gb][:, tt, hs]
                            if blk == 0:
                                s.op("dve", lambda e: e.tensor_scalar(out=dst, in0=X.ps[pd][:], scalar1=gsc, scalar2=None, op0=ALU.mult),
                                     reads=["ps%d" % pd, "st%d" % gb], writes=["acc%d_%d" % (gb, tt)])
                            else:
                                s.op("dve", lambda e: e.scalar_tensor_tensor(out=dst, in0=X.ps[pd][:], scalar=gsc, in1=dst,
                                                                             op0=ALU.mult, op1=ALU.add),
                                     reads=["ps%d" % pd, "st%d" % gb], writes=["acc%d_%d" % (gb, tt)])
                s.dma("sp", X.YS[gi * 512:(gi + 1) * 512, :].rearrange("(tt p) n -> p tt n", p=128), acc[gb][:],
                      reads=["acc%d_%d" % (gb, tt) for tt in range(4)], writes=["YS"])
            s.barrier()
        with ExitStack() as es2:
            sb2 = lambda name, shape, dt: es2.enter_context(nc.sbuf_tensor("%s_L%d" % (name, L), shape, dt))
            pgate = sb2("md_pgate", [128, 8, D], BF16)
            pproj = sb2("md_pproj", [128, 2, D], BF16)
            h1 = sb2("md_h1", [128, 4, D], F32)
            acc = sb2("md_acc", [128, 4, D], F32)
            yA = [sb2("md_yA%d" % i, [128, D], F32) for i in range(2)]
            yB = [sb2("md_yB%d" % i, [128, D], F32) for i in range(2)]
            sig = [sb2("md_sig%d" % i, [128, 512], F32) for i in range(2)]
            ptile = [sb2("md_pt%d" % i, [128, 256], F32) for i in range(2)]
            pb = sb2("md_pb", [128, 256], BF16)
            pT = sb2("md_pT", [128, 2, 512], BF16)
            for kc in range(8):
                s.dma("pool", pgate[:, kc, :], X.ple_gate[L, kc * 128:(kc + 1) * 128, :], writes=["pgate"])
            for kc in range(2):
                s.dma("pool", pproj[:, kc, :], X.ple_proj[L, kc * 128:(kc + 1) * 128, :], writes=["pproj"])
            if last:
                gfin = sb2("md_gfin", [128, D], F32)
                ones1 = sb2("md_ones1", [1, 128], F32)
                gf1 = sb2("md_gf1", [1, D], F32)
                s.dma("sp", gf1[:], X.g_final.rearrange("(o n) -> o n", o=1), writes=["gf1"])
                s.op("dve", lambda e: e.memset(ones1[:], 1.0), writes=["ones1"])
                for half in range(2):
                    s.op("pe", lambda e: e.matmul(X.ps[6][:], lhsT=ones1[:], rhs=gf1[:, half * 512:(half + 1) * 512],
                                                  start=True, stop=True), reads=["ones1", "gf1"], writes=["ps6"])
                    s.op("dve", lambda e: e.tensor_copy(out=gfin[:, half * 512:(half + 1) * 512], in_=X.ps[6][:]),
                         reads=["ps6"], writes=["gfin"])
            for tg in range(NT // 4):
                for tt in range(4):
                    ti = 4 * tg + tt
                    yb_ = ti % 2
                    s.dma("sp", h1[:, tt, :], X.H1[ti * 128:(ti + 1) * 128, :], reads=["H1"], writes=["h1_%d" % tt])
                    s.idma(yA[yb_][:, :], None, X.YS[:, :], POSI2[:, ti:ti + 1], NSLOT - 1, reads=["YS", "POSI"], writes=["yA%d" % yb_])
                    s.idma(yB[yb_][:, :], None, X.YS[:, :], POSI2[:, NT + ti:NT + ti + 1], NSLOT - 1, reads=["YS", "POSI"], writes=["yB%d" % yb_])
                    s.op("dve", lambda e: e.tensor_tensor(out=h1[:, tt, :], in0=h1[:, tt, :], in1=yA[yb_][:], op=ALU.add),
                         reads=["yA%d" % yb_], writes=["h1_%d" % tt])
                    s.op("dve", lambda e: e.tensor_tensor(out=h1[:, tt, :], in0=h1[:, tt, :], in1=yB[yb_][:], op=ALU.add),
                         reads=["yB%d" % yb_], writes=["h1_%d" % tt])
                    ln_a(s, X, h1[:, tt, :], "h1_%d" % tt, tt)
                for tt in range(4):
                    ln_b(s, X, tt, g3T[:], "g3T", hnT[:, :, tt * 128:(tt + 1) * 128], "m_hnT")
                psb = X.ps[7][:].bitcast(BF16)
                for tt in range(4):
                    ti = 4 * tg + tt
                    pt_ = ptile[ti % 2]
                    s.dma("sp", pt_[:], X.p[L, ti * 128:(ti + 1) * 128, :], writes=["pt%d" % (ti % 2)])
                    s.op("act", lambda e: e.activation(out=pb[:], in_=pt_[:], func=AF.Copy), reads=["pt%d" % (ti % 2)], writes=["pb"])
                    for k in range(2):
                        s.op("pe", lambda e: e.transpose(out=psb[:, k * 128:(k + 1) * 128], in_=pb[:, k * 128:(k + 1) * 128],
                                                         identity=X.ident[:]), reads=["pb", "ident"], writes=["ps7"])
                    s.op("act", lambda e: e.activation(out=pT[:, :, tt * 128:(tt + 1) * 128],
                                                       in_=psb[:, 0:256].rearrange("p (k t) -> p k t", k=2), func=AF.Copy),
                         reads=["ps7"], writes=["pT"])
                for tt in range(4):
                    ti = 4 * tg + tt
                    for half in range(2):
                        hs = slice(half * 512, (half + 1) * 512)
                        for k in range(2):
                            s.op("pe", lambda e: e.matmul(X.ps[6][:], lhsT=pT[:, k, tt * 128:(tt + 1) * 128], rhs=pproj[:, k, hs],
                                                          start=(k == 0), stop=(k == 1)), reads=["pT", "pproj"], writes=["ps6"])
                        pgb = 4 + half
                        for k in range(8):
                            s.op("pe", lambda e: e.matmul(X.ps[pgb][:], lhsT=hnT[:, k, tt * 128:(tt + 1) * 128], rhs=pgate[:, k, hs],
                                                          start=(k == 0), stop=(k == 7)), reads=["m_hnT", "pgate"], writes=["ps%d" % pgb])
                        s.op("act", lambda e: e.activation(out=sig[half][:], in_=X.ps[pgb][:], func=AF.Sigmoid),
                             reads=["ps%d" % pgb], writes=["sig%d" % half])
                        s.op("dve", lambda e: e.tensor_tensor(out=acc[:, tt, hs], in0=X.ps[6][:], in1=sig[half][:], op=ALU.mult),
                             reads=["ps6", "sig%d" % half], writes=["dacc%d" % tt])
                    s.op("pool", lambda e: e.tensor_tensor(out=h1[:, tt, :], in0=h1[:, tt, :], in1=acc[:, tt, :], op=ALU.add),
                         reads=["dacc%d" % tt], writes=["h1_%d" % tt])
                    if last:
                        ln_a(s, X, h1[:, tt, :], "h1_%d" % tt, tt)
                        s.op("act", lambda e: e.activation(out=acc[:, tt, :], in_=h1[:, tt, :], func=AF.Copy, scale=X.ss[tt][:, 2:3]),
                             reads=["h1_%d" % tt, "ss%d" % tt], writes=["dacc%d" % tt])
                        s.op("pool", lambda e: e.tensor_tensor(out=acc[:, tt, :], in0=acc[:, tt, :], in1=gfin[:], op=ALU.mult),
                             reads=["gfin"], writes=["dacc%d" % tt])
                        s.dma("pool", h_out[ti * 128:(ti + 1) * 128, :], acc[:, tt, :], reads=["dacc%d" % tt], writes=["HOUT"])
                    else:
                        s.dma("pool", h_out[ti * 128:(ti + 1) * 128, :], h1[:, tt, :], reads=["h1_%d" % tt], writes=["HB"])
            s.barrier()
        s.barrier()
```

```python
import math
import os
from contextlib import ExitStack

import numpy as np
import concourse.bass as bass
import concourse.mybir as mybir
from concourse.bass_utils import run_bass_kernel_spmd

F32 = mybir.dt.float32
BF16 = mybir.dt.bfloat16
AF = mybir.ActivationFunctionType
ALU = mybir.AluOpType
AX = mybir.AxisListType

D = 1024
SEQ = 8192
NT = SEQ // 128
DEPTH = 2
HD = 64
IN_COLS = 3352
D_FF = 3584
NE = 8
EPS = 1e-6
NEG = -30000.0
NGRP = 40
NSLOT = NGRP * 512
I32 = mybir.dt.int32

C_QN, C_KC, C_VC, C_KS, C_VS, C_KW, C_VW, C_GL, C_QR, C_KR, C_VR, C_GR = (
    0, 512, 640, 768, 896, 1024, 1152, 1280, 1304, 1816, 2328, 2840)


class S:
    def __init__(self, nc, es, n_dma_sems=24):
        self.nc = nc
        self.eng = {"pe": nc.tensor, "dve": nc.vector, "act": nc.scalar,
                    "pool": nc.gpsimd, "sp": nc.sync}
        self.sem = {k: es.enter_context(nc.semaphore("c_" + k)) for k in self.eng}
        self.cnt = {k: 0 for k in self.eng}
        self.seen = {k: {} for k in self.eng}
        self.drained = {k: 0 for k in self.eng}
        self.dsem = [es.enter_context(nc.semaphore("d%d" % i)) for i in range(n_dma_sems)]
        self.dval = [0] * n_dma_sems
        self.dnext = 0
        self.lastw = {}
        self.readers = {}
        self.nops = 0

    def _wait(self, eng, tok):
        if tok is None:
            return
        if tok[0] == "c":
            _, e2, c = tok
            if e2 == eng:
                if eng != "pe" and c > self.drained[eng]:
                    self.eng[eng].drain()
                    self.drained[eng] = self.cnt[eng]
                return
            key = e2
            sem = self.sem[e2]
        else:
            _, si, c = tok
            key = ("d", si)
            sem = self.dsem[si]
        if self.seen[eng].get(key, 0) >= c:
            return
        self.eng[eng].wait_ge(sem, c)
        self.seen[eng][key] = c

    def _deps(self, eng, reads, writes):
        for r in reads:
            self._wait(eng, self.lastw.get(r))
        for w in writes:
            self._wait(eng, self.lastw.get(w))
            for t in self.readers.get(w, {}).values():
                self._wait(eng, t)

    def _commit(self, tok, reads, writes):
        for r in reads:
            self.readers.setdefault(r, {})[tok[1] if tok[0] == "c" else ("d", tok[1])] = tok
        for w in writes:
            self.lastw[w] = tok
            self.readers[w] = {}

    def op(self, eng, fn, reads=(), writes=()):
        self._deps(eng, reads, writes)
        ins = fn(self.eng[eng])
        self.cnt[eng] += 1
        ins.then_inc(self.sem[eng], 1)
        self._commit(("c", eng, self.cnt[eng]), reads, writes)
        self.nops += 1

    def dma(self, q, out, in_, reads=(), writes=(), **kw):
        self._deps(q, reads, writes)
        si = self.dnext
        self.dnext = (self.dnext + 1) % len(self.dsem)
        self._wait(q, ("d", si, self.dval[si]))
        ins = self.eng[q].dma_start(out=out, in_=in_, **kw)
        self.dval[si] += 16
        ins.then_inc(self.dsem[si], 16)
        self._commit(("d", si, self.dval[si]), reads, writes)
        self.nops += 1

    def idma(self, out, out_off, in_, in_off, bound, reads=(), writes=()):
        q = "pool"
        self._deps(q, reads, writes)
        si = self.dnext
        self.dnext = (self.dnext + 1) % len(self.dsem)
        self._wait(q, ("d", si, self.dval[si]))
        oo = None if out_off is None else bass.IndirectOffsetOnAxis(ap=out_off, axis=0)
        io = None if in_off is None else bass.IndirectOffsetOnAxis(ap=in_off, axis=0)
        ins = self.nc.gpsimd.indirect_dma_start(out=out, out_offset=oo, in_=in_, in_offset=io)
        self.dval[si] += 16
        ins.then_inc(self.dsem[si], 16)
        self._commit(("d", si, self.dval[si]), reads, writes)
        self.nops += 1

    def barrier(self):
        for e in self.eng:
            for e2 in self.eng:
                if e2 != e and self.cnt[e2] > 0:
                    self._wait(e, ("c", e2, self.cnt[e2]))
            for si in range(len(self.dsem)):
                if self.dval[si] > 0:
                    self._wait(e, ("d", si, self.dval[si]))
        self.lastw = {}
        self.readers = {}

    def drain(self, eng="sp"):
        for r, t in list(self.lastw.items()):
            self._wait(eng, t)
        for r, d in list(self.readers.items()):
            for t in d.values():
                self._wait(eng, t)


def bcast_mid(ap2, n):
    return ap2.unsqueeze(2).to_broadcast([ap2.shape[0], ap2.shape[1], n])


def bcast_rep(ap2, n):
    return ap2.unsqueeze(1).to_broadcast([ap2.shape[0], n, ap2.shape[1]])


def make_consts():
    c = {}
    c["c_ident"] = np.eye(128, dtype=np.float32)
    t = np.arange(SEQ)
    slopes = 2.0 ** (-(np.arange(8) + 1.0))
    qa = np.zeros((8, 3, SEQ), np.float32)
    for h in range(8):
        qa[h, 0] = 128.0 * slopes[h]
        qa[h, 1] = slopes[h]
        qa[h, 2] = -slopes[h] * 128.0 * (t // 128 + 1)
    c["c_qaug"] = qa
    ka = np.zeros((3, SEQ), np.float32)
    ka[0] = t // 128
    ka[1] = t % 128
    ka[2] = 1.0
    c["c_kaug"] = ka
    n = np.arange(512)
    pos = 16 * n + 31
    kc = np.zeros((3, 512), np.float32)
    kc[0] = pos // 128
    kc[1] = pos % 128
    kc[2] = 1.0
    c["c_kcaug"] = kc
    hh = np.arange(8)
    lg = np.log1p(-(2.0 ** (-5.0 - hh)))
    nn = np.arange(128)
    xiT = np.zeros((4, 128, 128), np.float32)
    kdT = np.zeros((4, 128, 128), np.float32)
    gC = np.zeros((128, 4), np.float32)
    for hp in range(4):
        for half in range(2):
            h = 2 * hp + half
            xiT[hp, half * 64:(half + 1) * 64, :] = np.exp((nn + 1.0) * lg[h])[None, :]
            kdT[hp, half * 64:(half + 1) * 64, :] = 0.125 * np.exp(-(nn + 1.0) * lg[h])[None, :]
            gC[half * 64:(half + 1) * 64, hp] = np.exp(128.0 * lg[h])
    c["c_xiT"] = xiT
    c["c_kdT"] = kdT
    c["c_gC"] = gC
    c["c_gC8"] = np.tile(np.exp(128.0 * lg)[None, :], (64, 1)).astype(np.float32)
    c["c_ktab"] = (0.125 * np.exp(-(nn[:, None] + 1.0) * lg[None, :])).astype(np.float32)
    dm = (nn[None, :] >= nn[:, None]).astype(np.float32)
    c["c_rmask"] = np.tile(dm, (1, 4))
    jj = nn[:, None]
    ii = nn[None, :]
    c["c_mcausal"] = np.tile(np.where(jj > ii, NEG, 0.0).astype(np.float32), (1, 4))
    c["c_mwin4"] = np.tile(np.where(jj > ii, 0.0, NEG).astype(np.float32), (1, 4))
    pats = []
    pidx = {}
    cmp_plan = []
    for qi in range(NT):
        row = []
        for cc in range(4):
            tq = 128 * qi + ii
            nk = 128 * cc + jj
            valid = (tq - (16 * nk + 31) >= 0) & (nk <= 510)
            if valid.all():
                row.append(-1)
            elif not valid.any():
                row.append(-2)
            else:
                key = valid.tobytes()
                if key not in pidx:
                    pidx[key] = len(pats)
                    pats.append(np.tile(np.where(valid, 0.0, NEG).astype(np.float32), (1, 4)))
                row.append(pidx[key])
        cmp_plan.append(row)
    c["c_mcmp"] = np.stack(pats, 0)
    E = np.zeros((128, NT, 128), np.float32)
    for kt in range(NT):
        E[2 * kt, kt, 0:64] = 1.0
        E[2 * kt + 1, kt, 64:128] = 1.0
    c["c_E"] = E
    ncmp = np.arange(512)[:, None]
    msel = np.arange(128)[None, :]
    ov = ((np.minimum(16 * ncmp + 32, 64 * msel + 64) > np.maximum(16 * ncmp, 64 * msel))
          & (ncmp <= 510)).astype(np.float32)
    c["c_ov"] = ov
    AB = np.zeros((NT, 128, 256), np.float32)
    for qi in range(NT):
        tq = 128 * qi + nn[:, None]
        back = tq // 64 - msel
        forced = (msel == 0) | ((back >= 0) & (back < 2))
        A = ((back >= 0) & (~forced)).astype(np.float32)
        B = np.where(forced, 1.0e9 + 1024.0 * msel, np.where(back >= 0, 0.0, -1.0)).astype(np.float32)
        AB[qi, :, 0:128] = A
        AB[qi, :, 128:256] = B
    c["c_AB"] = AB
    c["c_gi512"] = np.tile((512.0 * np.arange(NGRP))[None, :], (128, 1)).astype(np.float32)
    c["c_tokid"] = (np.arange(NT)[None, :] * 128 + np.arange(128)[:, None]).astype(np.float32)
    c["c_U"] = (nn[:, None] < nn[None, :]).astype(np.float32)
    c["c_blkp"] = (np.arange(7)[None, :] * 128 + np.arange(128)[:, None]).astype(np.float32)
    return c, cmp_plan


_CONSTS = None


def get_consts():
    global _CONSTS
    if _CONSTS is None:
        _CONSTS = make_consts()
    return _CONSTS


class Ctx:
    def __getattr__(self, k):
        if k in WEIGHT_SPECS:
            ap = self.nc.dram_tensor(k, WEIGHT_SPECS[k], F32, kind="ExternalInput").ap()
            self.__dict__[k] = ap
            self.used.append(k)
            return ap
        raise AttributeError(k)


def ln_a(s, X, src, src_res, k):
    junk, ss, xs = X.junk, X.ss[k], X.xs[k]
    s.op("act", lambda e: e.activation(out=junk[:], in_=src, func=AF.Square, accum_out=ss[:, 0:1]),
         reads=[src_res], writes=["junk", "ss%d" % k])
    s.op("dve", lambda e: e.tensor_scalar(out=ss[:, 1:2], in0=ss[:, 0:1], scalar1=1.0 / D, scalar2=EPS,
                                          op0=ALU.mult, op1=ALU.add), reads=["ss%d" % k], writes=["ss%d" % k])
    s.op("act", lambda e: e.activation(out=ss[:, 1:2], in_=ss[:, 1:2], func=AF.Sqrt),
         reads=["ss%d" % k], writes=["ss%d" % k])
    s.op("dve", lambda e: e.reciprocal(out=ss[:, 2:3], in_=ss[:, 1:2]), reads=["ss%d" % k], writes=["ss%d" % k])
    s.op("act", lambda e: e.activation(out=xs[:], in_=src, func=AF.Copy, scale=ss[:, 2:3]),
         reads=[src_res, "ss%d" % k], writes=["xs%d" % k])


def ln_b(s, X, k, gT, gT_res, dst, dst_res, src=None, src_res=None):
    xs = X.xs[k] if src is None else src
    xres = ("xs%d" % k) if src is None else src_res
    psb = X.ps[7][:].bitcast(BF16)
    for kc in range(8):
        s.op("pe", lambda e: e.transpose(out=psb[:, kc * 128:(kc + 1) * 128], in_=xs[:, kc * 128:(kc + 1) * 128],
                                         identity=X.ident[:]), reads=[xres, "ident"], writes=["ps7"])
    s.op("dve", lambda e: e.tensor_tensor(out=dst, in0=psb.rearrange("p (k t) -> p k t", k=8),
                                          in1=bcast_mid(gT, 128), op=ALU.mult),
         reads=["ps7", gT_res], writes=[dst_res])


def phase_proj(s, X, L, h_in):
    nc = X.nc
    with ExitStack() as es:
        sb = lambda name, shape, dt: es.enter_context(nc.sbuf_tensor("%s_L%d" % (name, L), shape, dt))
        wsb = sb("p1_w", [128, 8, IN_COLS], BF16)
        gT = sb("p1_gT", [128, 8], F32)
        ht = [sb("p1_ht%d" % i, [128, D], F32) for i in range(4)]
        hnT = [sb("p1_hnT%d" % i, [128, 8, 512], BF16) for i in range(2)]
        fm = [sb("p1_fm%d" % i, [128, 16, 512], BF16) for i in range(2)]
        vsx = [sb("p1_vsx%d" % i, [128, 4, 2, 65], BF16) for i in range(2)]
        vwx = [sb("p1_vwx%d" % i, [128, 4, 2, 65], BF16) for i in range(2)]
        gls = [sb("p1_gls%d" % i, [128, 4, 24], F32) for i in range(2)]
        krm = [sb("p1_krm%d" % i, [128, 4, 512], BF16) for i in range(2)]
        vrm = [sb("p1_vrm%d" % i, [128, 4, 512], BF16) for i in range(2)]
        grm = [sb("p1_grm%d" % i, [128, 4, 512], BF16) for i in range(2)]
        sg = sb("p1_sg", [128, 512], F32)
        xiT = sb("p1_xiT", [128, 4, 128], F32)
        kdT = sb("p1_kdT", [128, 4, 128], F32)
        ktab = sb("p1_ktab", [128, 8], F32)
        s.dma("sp", xiT[:], X.c["c_xiT"].rearrange("j p t -> p j t"), writes=["xiT"])
        s.dma("sp", kdT[:], X.c["c_kdT"].rearrange("j p t -> p j t"), writes=["kdT"])
        s.dma("sp", ktab[:], X.c["c_ktab"], writes=["ktab"])
        s.dma("sp", gT[:], X.g_mix[L].rearrange("(k p) -> p k", p=128), writes=["p1gT"],
              allow_slow_non_contiguous=True)
        for kc in range(8):
            s.dma("pool", wsb[:, kc, :], X.w_in[L, kc * 128:(kc + 1) * 128, :], writes=["p1w"])
        for i in range(2):
            s.op("pool", lambda e: e.memset(vsx[i][:, :, :, 64:65], 1.0), writes=["vsx%d" % i])
            s.op("pool", lambda e: e.memset(vwx[i][:, :, :, 64:65], 1.0), writes=["vwx%d" % i])
        fm_cols = [C_QN, C_QN + 128, C_QN + 256, C_QN + 384, C_KC, C_VC, C_KS, C_KW,
                   C_QR, C_QR + 128, C_QR + 256, C_QR + 384, C_KR, C_KR + 128, C_KR + 256, C_KR + 384]
        def emit_a(tg):
            for tt in range(4):
                ti = 4 * tg + tt
                s.dma("sp", ht[tt][:], h_in[ti * 128:(ti + 1) * 128, :], writes=["p1ht%d" % tt])
                ln_a(s, X, ht[tt][:], "p1ht%d" % tt, tt)

        def emit_b(tg):
            for tt in range(4):
                ln_b(s, X, tt, gT[:], "p1gT", hnT[tg % 2][:, :, tt * 128:(tt + 1) * 128], "hnT%d" % (tg % 2))

        emit_a(0)
        emit_b(0)
        for tg in range(NT // 4):
            b = tg % 2
            toks = slice(tg * 512, (tg + 1) * 512)
            if tg + 1 < NT // 4:
                emit_a(tg + 1)
            for ci, c0 in enumerate(fm_cols):
                pb = ci % 2
                ps = X.ps[pb]
                for kc in range(8):
                    s.op("pe", lambda e: e.matmul(ps[:], lhsT=wsb[:, kc, c0:c0 + 128], rhs=hnT[b][:, kc, :],
                                                  start=(kc == 0), stop=(kc == 7)),
                         reads=["p1w", "hnT%d" % b], writes=["ps%d" % pb])
                dst = fm[b][:, ci, :]
                if ci < 4:
                    s.op("act", lambda e: e.activation(out=dst, in_=ps[:], func=AF.Copy, scale=0.125),
                         reads=["ps%d" % pb], writes=["fm%d" % b])
                elif ci < 8:
                    eng = "act" if ci % 2 == 0 else "dve"
                    if eng == "act":
                        s.op("act", lambda e: e.activation(out=dst, in_=ps[:], func=AF.Copy),
                             reads=["ps%d" % pb], writes=["fm%d" % b])
                    else:
                        s.op("dve", lambda e: e.tensor_copy(out=dst, in_=ps[:]),
                             reads=["ps%d" % pb], writes=["fm%d" % b])
                else:
                    tab = xiT if ci < 12 else kdT
                    j = (ci - 8) % 4
                    s.op("dve", lambda e: e.tensor_tensor(
                        out=dst.rearrange("p (a t) -> p a t", a=4), in0=ps[:].rearrange("p (a t) -> p a t", a=4),
                        in1=bcast_rep(tab[:, j, :], 4), op=ALU.mult),
                        reads=["ps%d" % pb, "xiT", "kdT"], writes=["fm%d" % b])
            for tt in range(4):
                lhs = lambda kc: hnT[b][:, kc, tt * 128:(tt + 1) * 128]
                groups = [(2, C_VS, 408), (3, C_KR, 512), (4, C_VR, 512), (5, C_GR, 512)]
                for (pi, c0, n) in groups:
                    for kc in range(8):
                        s.op("pe", lambda e: e.matmul(X.ps[pi][:, 0:n], lhsT=lhs(kc), rhs=wsb[:, kc, c0:c0 + n],
                                                      start=(kc == 0), stop=(kc == 7)),
                             reads=["p1w", "hnT%d" % b], writes=["ps%d" % pi])
                pA = X.ps[2]
                s.op("dve", lambda e: e.tensor_copy(out=vsx[b][:, tt, :, 0:64],
                                                    in_=pA[:, 0:128].rearrange("p (g d) -> p g d", g=2)),
                     reads=["ps2"], writes=["vsx%d" % b])
                s.op("dve", lambda e: e.tensor_copy(out=vwx[b][:, tt, :, 0:64],
                                                    in_=pA[:, 256:384].rearrange("p (g d) -> p g d", g=2)),
                     reads=["ps2"], writes=["vwx%d" % b])
                s.op("act", lambda e: e.activation(out=gls[b][:, tt, :], in_=pA[:, 384:408], func=AF.Sigmoid),
                     reads=["ps2"], writes=["gls%d" % b])
                s.op("dve", lambda e: e.tensor_tensor(
                    out=krm[b][:, tt, :].rearrange("p (h d) -> p h d", h=8),
                    in0=X.ps[3][:].rearrange("p (h d) -> p h d", h=8),
                    in1=bcast_mid(ktab[:], 64), op=ALU.mult),
                    reads=["ps3", "ktab"], writes=["krm%d" % b])
                s.op("act", lambda e: e.activation(out=vrm[b][:, tt, :], in_=X.ps[4][:], func=AF.Copy),
                     reads=["ps4"], writes=["vrm%d" % b])
                s.op("act", lambda e: e.activation(out=sg[:], in_=X.ps[5][:], func=AF.Sigmoid),
                     reads=["ps5"], writes=["p1sg"])
                s.op("dve", lambda e: e.tensor_tensor(out=grm[b][:, tt, :], in0=X.ps[5][:], in1=sg[:], op=ALU.mult),
                     reads=["ps5", "p1sg"], writes=["grm%d" % b])
            if tg + 1 < NT // 4:
                emit_b(tg + 1)
            s.dma("pool", X.QTP[:, :, toks].rearrange("j p t -> p j t"), fm[b][:, 0:4, :], reads=["fm%d" % b], writes=["QTP"])
            s.dma("pool", X.KCP[:, toks], fm[b][:, 4, :], reads=["fm%d" % b], writes=["KCP"])
            s.dma("pool", X.VCP[:, toks], fm[b][:, 5, :], reads=["fm%d" % b], writes=["VCP"])
            s.dma("pool", X.KSP[:, toks], fm[b][:, 6, :], reads=["fm%d" % b], writes=["KSP"])
            s.dma("pool", X.KWP[:, toks], fm[b][:, 7, :], reads=["fm%d" % b], writes=["KWP"])
            s.dma("pool", X.QRT[:, :, toks].rearrange("j p t -> p j t"), fm[b][:, 8:12, :], reads=["fm%d" % b], writes=["QRT"])
            s.dma("pool", X.KRT[:, :, toks].rearrange("j p t -> p j t"), fm[b][:, 12:16, :], reads=["fm%d" % b], writes=["KRT"])
            s.dma("pool", X.VSX[toks].rearrange("(tt p) g c -> p tt g c", p=128), vsx[b][:], reads=["vsx%d" % b], writes=["VSX"])
            s.dma("pool", X.VWX[toks].rearrange("(tt p) g c -> p tt g c", p=128), vwx[b][:], reads=["vwx%d" % b], writes=["VWX"])
            s.dma("pool", X.GLS[toks].rearrange("(tt p) c -> p tt c", p=128), gls[b][:], reads=["gls%d" % b], writes=["GLS"])
            s.dma("pool", X.KRM[toks].rearrange("(tt p) c -> p tt c", p=128), krm[b][:], reads=["krm%d" % b], writes=["KRM"])
            s.dma("pool", X.VRM[toks].rearrange("(tt p) c -> p tt c", p=128), vrm[b][:], reads=["vrm%d" % b], writes=["VRM"])
            s.dma("pool", X.GRM[toks].rearrange("(tt p) c -> p tt c", p=128), grm[b][:], reads=["grm%d" % b], writes=["GRM"])
        s.barrier()


WEIGHT_SPECS = {
    "w_in": [DEPTH, D, IN_COLS], "w_out": [DEPTH, D, D], "g_mix": [DEPTH, D], "g_ffn": [DEPTH, D],
    "g_ple": [DEPTH, D], "g_final": [D], "cmp_pos": [DEPTH, 2, 32, 64], "cmp_w1": [DEPTH, 2, 32, 64, 256],
    "cmp_w2": [DEPTH, 2, 256, 64], "ret_gn": [DEPTH, 512], "ffn_gate": [1, D, D_FF], "ffn_up": [1, D, D_FF],
    "ffn_down": [1, D_FF, D], "moe_router": [1, D, NE], "moe_gate": [1, NE, D, D_FF], "moe_up": [1, NE, D, D_FF],
    "moe_down": [1, NE, D_FF, D], "ple_proj": [DEPTH, 256, D], "ple_gate": [DEPTH, D, D],
}

SCRATCH_SPECS = {
    "QTP": ([4, 128, SEQ], BF16), "QAUG": ([8, 3, SEQ], BF16), "KAUG": ([3, SEQ], BF16), "KCAUG": ([3, 512], BF16),
    "KSP": ([128, SEQ], BF16), "KWP": ([128, SEQ], BF16), "KCP": ([128, SEQ], BF16), "VCP": ([128, SEQ], BF16),
    "VSX": ([SEQ, 2, 65], BF16), "VWX": ([SEQ, 2, 65], BF16), "GLS": ([SEQ, 24], F32),
    "QRT": ([4, 128, SEQ], BF16), "KRT": ([4, 128, SEQ], BF16),
    "KRM": ([SEQ, 512], BF16), "VRM": ([SEQ, 512], BF16), "GRM": ([SEQ, 512], BF16),
    "AT": ([D, SEQ], BF16), "HB": ([SEQ, D], F32),
    "FG16": ([1, D, D_FF], BF16), "FU16": ([1, D, D_FF], BF16), "FD16": ([1, D_FF, D], BF16),
    "WGB": ([NE * 7 * 128, 4096], BF16), "WUB": ([NE * 7 * 128, 4096], BF16), "WDB": ([NE * 7 * 128, 4096], BF16),
    "H1": ([SEQ, D], F32), "XS2": ([SEQ, D], BF16), "SLOT": ([NSLOT, 2], I32), "YS": ([NSLOT, D], F32),
    "DBG_KCMP": ([67, 2, 512], BF16), "DBG_VCX": ([128, 2, 4, 193], BF16),
}


def build(stop_after=None, debug=(), nsa_tiles=NT):
    nc = bass.Bass("TRN2", target_bir_lowering=False)
    X = Ctx()
    X.nc = nc
    consts, cmp_plan = get_consts()
    X.cmp_plan = cmp_plan
    X.x = nc.dram_tensor("x", [SEQ, D], F32, kind="ExternalInput").ap()
    X.p = nc.dram_tensor("p", [DEPTH, SEQ, 256], F32, kind="ExternalInput").ap()
    X.used = []
    X.dbg_cmp = "DBG_KCMP" in debug
    X.nsa_tiles = nsa_tiles
    X.routed = True
    X.conv_layers = [0] if (stop_after is not None and stop_after[0] == 0) else [0, 1]
    X.ret_chunks = int(os.environ.get('RET_CHUNKS', NT))
    X.c = {k: nc.dram_tensor(k, list(v.shape), F32, kind="ExternalInput").ap() for k, v in consts.items()}
    X.out = nc.dram_tensor("out", [SEQ, D], F32, kind="ExternalOutput").ap()
    for k, (shp, dt) in SCRATCH_SPECS.items():
        kind = "ExternalOutput" if k in debug else "Internal"
        setattr(X, k, nc.dram_tensor(k, shp, dt, kind=kind).ap())
    with ExitStack() as es:
        s = S(nc, es)
        X.ps = [es.enter_context(nc.psum_tensor("ps%d" % i, [128, 512], F32)) for i in range(8)]
        X.ident = es.enter_context(nc.sbuf_tensor("ident", [128, 128], BF16))
        X.junk = es.enter_context(nc.sbuf_tensor("junk", [128, D], F32))
        X.ss = [es.enter_context(nc.sbuf_tensor("ss%d" % i, [128, 4], F32)) for i in range(4)]
        X.xs = [es.enter_context(nc.sbuf_tensor("xs%d" % i, [128, D], BF16)) for i in range(4)]
        s.dma("pool", X.ident[:], X.c["c_ident"], writes=["ident"])
        s.dma("pool", X.QAUG, X.c["c_qaug"], writes=["QAUG"])
        s.dma("pool", X.KAUG, X.c["c_kaug"], writes=["KAUG"])
        s.dma("pool", X.KCAUG, X.c["c_kcaug"], writes=["KCAUG"])
        done = False
        for L in range(DEPTH):
            h_in = X.x if L == 0 else X.HB
            for name, fn in PHASES:
                fn(s, X, L, h_in)

                if stop_after == (L, name):
                    done = True
                    break
            if done:
                break
        s.barrier()
        s.drain("sp")
    X.nops = s.nops
    return nc, X


PHASES = [("proj", phase_proj)]

_BUILT = {}


def run(inputs, stop_after=None, debug=(), cores=8, trace=False, nsa_tiles=NT):
    key = (stop_after, tuple(debug), nsa_tiles)
    if key not in _BUILT:
        _BUILT[key] = build(stop_after, debug, nsa_tiles)
    nc, X = _BUILT[key]
    consts, _ = get_consts()
    in_maps = []
    for b in range(cores):
        m = {"x": np.ascontiguousarray(inputs["x"][b]), "p": np.ascontiguousarray(inputs["p"][:, b])}
        for k in X.used:
            m[k] = np.ascontiguousarray(inputs[k])
        m.update(consts)
        in_maps.append(m)
    return run_bass_kernel_spmd(nc, in_maps, core_ids=list(range(cores)), trace=trace)


def kernel(**inputs):
    inputs = {k: np.asarray(v) for k, v in inputs.items()}
    res = run(inputs)
    return np.stack([r["out"] for r in res.results], 0).astype(np.float32)


def phase_nsa(s, X, L, h_in):
    nc = X.nc
    with ExitStack() as es:
        sb = lambda name, shape, dt: es.enter_context(nc.sbuf_tensor("%s_L%d" % (name, L), shape, dt))
        KCMP = sb("n_kcmp", [67, 2, 512], BF16)
        VCX = sb("n_vcx", [128, 2, 4, 193], BF16)
        s.op("pool", lambda e: e.memset(KCMP[:], 0.0), writes=["KCMP"])
        s.op("pool", lambda e: e.memset(VCX[:], 0.0), writes=["VCX"])
        s.op("pool", lambda e: e.memset(VCX[:, :, :, 64:65], 1.0), writes=["VCX"])
        for g in range(2):
            s.dma("pool", VCX[:, g, :, 65:193], X.c["c_ov"].rearrange("(ct p) m -> p ct m", p=128), writes=["VCX"])
            s.dma("sp", KCMP[64:67, g, :], X.KCAUG, reads=["KCAUG"], writes=["KCMP"])
        with ExitStack() as es2:
            sb2 = lambda name, shape, dt: es2.enter_context(nc.sbuf_tensor("%s_L%d" % (name, L), shape, dt))
            kcT = [sb2("c_kcT%d" % i, [64, SEQ], BF16) for i in range(2)]
            w1 = sb2("c_w1", [64, 32, 256], BF16)
            posT = sb2("c_posT", [64, 32], BF16)
            w2 = sb2("c_w2", [128, 2, 64], BF16)
            hb = sb2("c_hb", [128, 2], F32)
            u = sb2("c_u", [128, 2, 512], F32)
            t1 = sb2("c_t1", [128, 2, 512], F32)
            sg = sb2("c_sg", [128, 2, 512], F32)
            gel = sb2("c_gel", [128, 2, 512], BF16)
            it = 0
            for kv in range(2):
                s.dma("pool", w1[:], X.cmp_w1[L, kv].rearrange("l d h -> d l h"), writes=["c_w1"])
                s.dma("pool", posT[:], X.cmp_pos[L, kv].rearrange("l d -> d l"), writes=["c_posT"],
                      allow_slow_non_contiguous=True)
                s.dma("pool", w2[:], X.cmp_w2[L, kv].rearrange("(c p) d -> p c d", p=128), writes=["c_w2"])
                src = X.KCP if kv == 0 else X.VCP
                for g in range(2):
                    kt_ = kcT[it % 2]
                    kres = "c_kcT%d" % (it % 2)
                    it += 1
                    s.dma("sp", kt_[:], src[g * 64:(g + 1) * 64, :], reads=["KCP", "VCP"], writes=[kres])
                    kview = kt_[:].rearrange("d (n c) -> d n c", c=16)
                    for hc in range(2):
                        ps = X.ps[hc]
                        for l in range(32):
                            a, c_ = l // 16, l % 16
                            s.op("pe", lambda e: e.matmul(ps[:, 0:511], lhsT=w1[:, l, hc * 128:(hc + 1) * 128],
                                                          rhs=kview[:, a:a + 511, c_], start=(l == 0), stop=(l == 31)),
                                 reads=["c_w1", kres], writes=["ps%d" % hc])
                        for l in range(32):
                            s.op("pe", lambda e: e.matmul(X.ps[2][:, hc:hc + 1], lhsT=w1[:, l, hc * 128:(hc + 1) * 128],
                                                          rhs=posT[:, l:l + 1], start=(l == 0), stop=(l == 31)),
                                 reads=["c_w1", "c_posT"], writes=["ps2"])
                        s.op("dve", lambda e: e.tensor_copy(out=hb[:, hc:hc + 1], in_=X.ps[2][:, hc:hc + 1]),
                             reads=["ps2"], writes=["c_hb"])
                        s.op("act", lambda e: e.activation(out=u[:, hc, 0:511], in_=ps[:, 0:511], func=AF.Identity,
                                                           bias=hb[:, hc:hc + 1]),
                             reads=["ps%d" % hc, "c_hb"], writes=["c_u"])
                    uu = u[:, :, 0:511]
                    s.op("pool", lambda e: e.tensor_tensor(out=t1[:, :, 0:511], in0=uu, in1=uu, op=ALU.mult),
                         reads=["c_u"], writes=["c_t1"])
                    s.op("dve", lambda e: e.tensor_scalar(out=t1[:, :, 0:511], in0=t1[:, :, 0:511], scalar1=0.044715,
                                                          scalar2=1.0, op0=ALU.mult, op1=ALU.add),
                         reads=["c_t1"], writes=["c_t1"])
                    s.op("dve", lambda e: e.tensor_tensor(out=t1[:, :, 0:511], in0=t1[:, :, 0:511], in1=uu, op=ALU.mult),
                         reads=["c_t1", "c_u"], writes=["c_t1"])
                    s.op("act", lambda e: e.activation(out=sg[:, :, 0:511], in_=t1[:, :, 0:511], func=AF.Sigmoid,
                                                       scale=1.5957691216057308),
                         reads=["c_t1"], writes=["c_sg"])
                    s.op("dve", lambda e: e.tensor_tensor(out=gel[:, :, 0:511], in0=sg[:, :, 0:511], in1=uu, op=ALU.mult),
                         reads=["c_sg", "c_u"], writes=["c_gel"])
                    if kv == 0:
                        for hc in range(2):
                            s.op("pe", lambda e: e.matmul(X.ps[3][0:64, 0:511], lhsT=w2[:, hc, :], rhs=gel[:, hc, 0:511],
                                                          start=(hc == 0), stop=(hc == 1)),
                                 reads=["c_w2", "c_gel"], writes=["ps3"])
                        s.op("act", lambda e: e.activation(out=KCMP[0:64, g, 0:511], in_=X.ps[3][0:64, 0:511], func=AF.Copy),
                             reads=["ps3"], writes=["KCMP"])
                    else:
                        for ct in range(4):
                            nn = 128 if ct < 3 else 127
                            for hc in range(2):
                                s.op("pe", lambda e: e.matmul(X.ps[3][0:nn, ct * 64:(ct + 1) * 64],
                                                              lhsT=gel[:, hc, ct * 128:ct * 128 + nn], rhs=w2[:, hc, :],
                                                              start=(hc == 0), stop=(hc == 1)),
                                     reads=["c_w2", "c_gel"], writes=["ps3"])
                        for ct in range(4):
                            nn = 128 if ct < 3 else 127
                            s.op("act", lambda e: e.activation(out=VCX[0:nn, g, ct, 0:64],
                                                               in_=X.ps[3][0:nn, ct * 64:(ct + 1) * 64], func=AF.Copy),
                                 reads=["ps3"], writes=["VCX"])
            s.barrier()
        if X.dbg_cmp:
            s.dma("sp", X.DBG_KCMP, KCMP[:], reads=["KCMP"])
            s.dma("sp", X.DBG_VCX, VCX[:], reads=["VCX"])
        KS = sb("n_ks", [67, 2, SEQ], BF16)
        KW = sb("n_kw", [67, 2, SEQ], BF16)
        VS = sb("n_vs", [128, NT, 2, 65], BF16)
        VW = sb("n_vw", [128, NT, 2, 65], BF16)
        E = sb("n_E", [128, NT, 128], BF16)
        mca = sb("n_mca", [128, 512], BF16)
        mw4 = sb("n_mw4", [128, 512], BF16)
        npat = X.c["c_mcmp"].shape[0]
        mcmp = sb("n_mcmp", [128, npat, 512], BF16)
        qt = [sb("n_qt%d" % i, [67, 1024], BF16) for i in range(2)]
        AB = [sb("n_AB%d" % i, [128, 256], F32) for i in range(2)]
        glt = [sb("n_gl%d" % i, [128, 24], F32) for i in range(2)]
        PT = [sb("n_PT%d" % i, [128, 512], BF16) for i in range(4)]
        negT = [sb("n_negT%d" % i, [128, 512], BF16) for i in range(2)]
        acc = [sb("n_acc%d" % i, [128, 512], F32) for i in range(2)]
        accb = [sb("n_accb%d" % i, [128, 512], BF16) for i in range(2)]
        aT = [sb("n_aT%d" % i, [128, 4, 128], BF16) for i in range(2)]
        sm = [sb("n_sm%d" % i, [128, 64], F32) for i in range(2)]
        sc = [sb("n_sc%d" % i, [128, 128], F32) for i in range(2)]
        sc2 = [sb("n_sc2%d" % i, [128, 128], F32) for i in range(2)]
        seln = [sb("n_seln%d" % i, [128, 128], BF16) for i in range(2)]
        s.dma("sp", KS[0:64, :, :], X.KSP.rearrange("(g d) t -> d g t", g=2), reads=["KSP"], writes=["KS"])
        s.dma("sp", KW[0:64, :, :], X.KWP.rearrange("(g d) t -> d g t", g=2), reads=["KWP"], writes=["KW"])
        for g in range(2):
            s.dma("sp", KS[64:67, g, :], X.KAUG, reads=["KAUG"], writes=["KS"])
            s.dma("sp", KW[64:67, g, :], X.KAUG, reads=["KAUG"], writes=["KW"])
        for q4 in range(4):
            tsl = slice(q4 * 2048, (q4 + 1) * 2048)
            ksl = slice(q4 * 16, (q4 + 1) * 16)
            s.dma("sp", VS[:, ksl], X.VSX[tsl].rearrange("(kt p) g c -> p kt g c", p=128), reads=["VSX"], writes=["VS"])
            s.dma("sp", VW[:, ksl], X.VWX[tsl].rearrange("(kt p) g c -> p kt g c", p=128), reads=["VWX"], writes=["VW"])
            s.dma("pool", E[:, ksl, :], X.c["c_E"][:, ksl, :], writes=["E"])
        s.dma("pool", mca[:], X.c["c_mcausal"], writes=["mca"])
        s.dma("pool", mw4[:], X.c["c_mwin4"], writes=["mw4"])
        s.dma("pool", mcmp[:], X.c["c_mcmp"].rearrange("n p c -> p n c"), writes=["mcmp"])
        if L == 0:
            convert_weights(s, X, X.conv_layers)
        st = {"sb": 0, "pt": 0}

        def qk_tile(g, qb, lhsT, lres, extra, mask):
            pi = st["sb"] % 3
            st["sb"] += 1
            ps = X.ps[pi]
            pres = "ps%d" % pi
            nmm = 1 + (extra is not None) + (mask is not None)
            s.op("pe", lambda e: e.matmul(ps[:], lhsT=lhsT, rhs=qt[qb][:, g * 512:(g + 1) * 512], start=True,
                                          stop=(nmm == 1)), reads=[lres, "qt%d" % qb], writes=[pres])
            k = 1
            if extra is not None:
                el, er, eres = extra
                s.op("pe", lambda e: e.matmul(ps[:], lhsT=el, rhs=er, start=False, stop=(k + 1 == nmm)),
                     reads=eres, writes=[pres])
                k += 1
            if mask is not None:
                ml, mres = mask
                s.op("pe", lambda e: e.matmul(ps[:], lhsT=X.ident[:], rhs=ml, start=False, stop=True),
                     reads=["ident", mres], writes=[pres])
            pk = st["pt"] % 4
            st["pt"] += 1
            s.op("act", lambda e: e.activation(out=PT[pk][:], in_=ps[:], func=AF.Exp),
                 reads=[pres], writes=["PT%d" % pk])
            return PT[pk], "PT%d" % pk

        def coef_and_acc(g, qb, branch, heads_ap, heads_res, first):
            smt = sm[g]
            sres = "sm%d" % g
            for r in range(4):
                o_ap = heads_ap(r)
                s.op("dve", lambda e: e.tensor_scalar(out=smt[:, r:r + 1], in0=o_ap[:, 64:65], scalar1=1e-36,
                                                      scalar2=None, op0=ALU.max),
                     reads=heads_res, writes=[sres])
            s.op("dve", lambda e: e.reciprocal(out=smt[:, 4:8], in_=smt[:, 0:4]), reads=[sres], writes=[sres])
            c0 = branch * 8 + g * 4
            s.op("dve", lambda e: e.tensor_tensor(out=smt[:, 8:12], in0=smt[:, 4:8], in1=glt[qb][:, c0:c0 + 4],
                                                  op=ALU.mult), reads=[sres, "gl%d" % qb], writes=[sres])
            for r in range(4):
                o_ap = heads_ap(r)
                dst = acc[qb][:, (g * 4 + r) * 64:(g * 4 + r + 1) * 64]
                if first:
                    s.op("dve", lambda e: e.tensor_scalar(out=dst, in0=o_ap[:, 0:64], scalar1=smt[:, 8 + r:9 + r],
                                                          scalar2=None, op0=ALU.mult),
                         reads=heads_res + [sres], writes=["acc%d" % qb])
                else:
                    s.op("dve", lambda e: e.scalar_tensor_tensor(out=dst, in0=o_ap[:, 0:64], scalar=smt[:, 8 + r:9 + r],
                                                                 in1=dst, op0=ALU.mult, op1=ALU.add),
                         reads=heads_res + [sres], writes=["acc%d" % qb])

        def emit_loads(qi):
            qb = qi % 2
            t0 = qi * 128
            for j in range(4):
                s.dma("sp", qt[qb][0:64, :].rearrange("d (j two t) -> d j two t", j=4, two=2)[:, j],
                      X.QTP[j, :, t0:t0 + 128].rearrange("(two d) t -> d two t", two=2),
                      reads=["QTP"], writes=["qt%d" % qb])
            s.dma("sp", qt[qb][64:67, :].rearrange("r (h t) -> r h t", h=8),
                  X.QAUG[:, :, t0:t0 + 128].rearrange("h r t -> r h t"), reads=["QAUG"], writes=["qt%d" % qb])
            s.dma("sp", AB[qb][:], X.c["c_AB"][qi], writes=["AB%d" % qb])
            s.dma("sp", glt[qb][:], X.GLS[t0:t0 + 128, :], reads=["GLS"], writes=["gl%d" % qb])

        def cmp_post_a(qi, g):
            qb = qi % 2
            ocb = lambda r: X.ps[3 + r // 2][:, (r % 2) * 193:(r % 2) * 193 + 193]
            coef_and_acc(g, qb, 0, ocb, ["ps3", "ps4"], True)
            smt = sm[g]
            sres = "sm%d" % g
            for r in range(4):
                if r == 0:
                    s.op("dve", lambda e: e.tensor_scalar(out=sc[g][:], in0=ocb(r)[:, 65:193], scalar1=smt[:, 4:5],
                                                          scalar2=None, op0=ALU.mult),
                         reads=["ps3", "ps4", sres], writes=["sc%d" % g])
                else:
                    s.op("dve", lambda e: e.scalar_tensor_tensor(out=sc[g][:], in0=ocb(r)[:, 65:193],
                                                                 scalar=smt[:, 4 + r:5 + r], in1=sc[g][:],
                                                                 op0=ALU.mult, op1=ALU.add),
                         reads=["ps3", "ps4", sres], writes=["sc%d" % g])
            s.op("dve", lambda e: e.tensor_tensor(out=sc[g][:], in0=sc[g][:], in1=AB[qb][:, 0:128], op=ALU.mult),
                 reads=["AB%d" % qb], writes=["sc%d" % g])
            s.op("dve", lambda e: e.tensor_tensor(out=sc[g][:], in0=sc[g][:], in1=AB[qb][:, 128:256], op=ALU.add),
                 reads=["AB%d" % qb], writes=["sc%d" % g])
            s.op("dve", lambda e: e.max(out=smt[:, 16:24], in_=sc[g][:]), reads=["sc%d" % g], writes=[sres])
            s.op("dve", lambda e: e.match_replace(out=sc2[g][:], in_to_replace=smt[:, 16:24], in_values=sc[g][:],
                                                  imm_value=-2.0), reads=["sc%d" % g, sres], writes=["sc2%d" % g])
            s.op("dve", lambda e: e.max(out=smt[:, 24:32], in_=sc2[g][:]), reads=["sc2%d" % g], writes=[sres])
            s.op("dve", lambda e: e.tensor_scalar(out=seln[g][:], in0=sc[g][:], scalar1=smt[:, 31:32], scalar2=NEG,
                                                  op0=ALU.is_lt, op1=ALU.mult),
                 reads=["sc%d" % g, sres], writes=["seln%d" % g])

        def cmp_post_b(qi, g):
            psb = X.ps[7][:].bitcast(BF16)
            s.op("pe", lambda e: e.transpose(out=psb[:, g * 128:(g + 1) * 128], in_=seln[g][:], identity=X.ident[:]),
                 reads=["seln%d" % g, "ident"], writes=["ps7"])
            s.op("dve", lambda e: e.tensor_copy(out=negT[g][:].rearrange("p (a t) -> p a t", a=4),
                                                in_=bcast_rep(psb[:, g * 128:(g + 1) * 128], 4)),
                 reads=["ps7"], writes=["negT%d" % g])

        def write_out(qi):
            qb = qi % 2
            t0 = qi * 128
            s.op("act", lambda e: e.activation(out=accb[qb][:], in_=acc[qb][:], func=AF.Copy),
                 reads=["acc%d" % qb], writes=["accb%d" % qb])
            psb = X.ps[7][:].bitcast(BF16)
            for j in range(4):
                s.op("pe", lambda e: e.transpose(out=psb[:, 256 + j * 128:256 + (j + 1) * 128],
                                                 in_=accb[qb][:, j * 128:(j + 1) * 128], identity=X.ident[:]),
                     reads=["accb%d" % qb, "ident"], writes=["ps7"])
            s.op("act", lambda e: e.activation(out=aT[qb][:].rearrange("p j t -> p (j t)"), in_=psb[:, 256:768], func=AF.Copy),
                 reads=["ps7"], writes=["aT%d" % qb])
            s.dma("sp", X.AT[0:512, t0:t0 + 128].rearrange("(j p) t -> p j t", p=128), aT[qb][:],
                  reads=["aT%d" % qb], writes=["AT"])

        def pv_fn(out_ap, out_res, rhs, rhs_res, first, lastt, pair_start):
            def f(pt, ptres):
                for r in range(4):
                    st_ = first and ((r % 2 == 0) if pair_start else (r == 0))
                    s.op("pe", lambda e: e.matmul(out_ap(r), lhsT=pt[:, r * 128:(r + 1) * 128], rhs=rhs,
                                                  start=st_, stop=lastt), reads=[ptres, rhs_res], writes=out_res(r))
            return f

        jobs = []
        for qi in range(X.nsa_tiles):
            qb = qi % 2
            first_job = len(jobs)
            plan = X.cmp_plan[qi]
            cs = [c for c in range(4) if plan[c] != -2]
            ocb = lambda r: X.ps[3 + r // 2][:, (r % 2) * 193:(r % 2) * 193 + 193]
            ocres = lambda r: ["ps%d" % (3 + r // 2)]
            for g in range(2):
                for ci, c in enumerate(cs):
                    mask = None if plan[c] == -1 else (mcmp[:, plan[c], :], "mcmp")
                    jobs.append(dict(
                        qk=(lambda g=g, qb=qb, c=c, mask=mask: qk_tile(g, qb, KCMP[:, g, c * 128:(c + 1) * 128], "KCMP", None, mask)),
                        pv=pv_fn(ocb, ocres, VCX[:, g, c, :], "VCX", ci == 0, ci == len(cs) - 1, True),
                        post=((lambda qi=qi, g=g: (cmp_post_a(qi, g), (cmp_post_b(qi, g) if qi < 2 else None)))
                              if ci == len(cs) - 1 else None)))
            for g in range(2):
                obr = (lambda g: (lambda r: X.ps[5 + g][:, r * 65:(r + 1) * 65]))(g)
                obres = (lambda g: (lambda r: ["ps%d" % (5 + g)]))(g)
                dl = list(range(min(4, qi), -1, -1))
                for di, dd in enumerate(dl):
                    kt = qi - dd
                    mask = (mca[:], "mca") if dd == 0 else ((mw4[:], "mw4") if dd == 4 else None)
                    posts = []
                    if di == 0 and qi >= 2:
                        posts.append(lambda qi=qi, g=g: cmp_post_b(qi, g))
                    if di == len(dl) - 1:
                        posts.append(lambda g=g, qb=qb, obr=obr: coef_and_acc(g, qb, 2, obr, ["ps%d" % (5 + g)], False))
                    jobs.append(dict(
                        qk=(lambda g=g, qb=qb, kt=kt, mask=mask: qk_tile(g, qb, KW[:, g, kt * 128:(kt + 1) * 128], "KW", None, mask)),
                        pv=pv_fn(obr, obres, VW[:, kt, g, :], "VW", di == 0, di == len(dl) - 1, False),
                        post=(lambda posts=posts: [p_() for p_ in posts])))
            for g in range(2):
                obr = (lambda g: (lambda r: X.ps[5 + g][:, r * 65:(r + 1) * 65]))(g)
                obres = (lambda g: (lambda r: ["ps%d" % (5 + g)]))(g)
                for kt in range(qi + 1):
                    mask = (mca[:], "mca") if kt == qi else None
                    posts = []
                    if kt == qi:
                        posts.append(lambda g=g, qb=qb, obr=obr: coef_and_acc(g, qb, 1, obr, ["ps%d" % (5 + g)], False))
                        if g == 1:
                            posts.append(lambda qi=qi: write_out(qi))
                    jobs.append(dict(
                        qk=(lambda g=g, qb=qb, kt=kt, mask=mask: qk_tile(
                            g, qb, KS[:, g, kt * 128:(kt + 1) * 128], "KS", (E[:, kt, :], negT[g][:], ["E", "negT%d" % g]), mask)),
                        pv=pv_fn(obr, obres, VS[:, kt, g, :], "VS", kt == 0, kt == qi, False),
                        post=(lambda posts=posts: [p_() for p_ in posts])))
            if qi + 1 < X.nsa_tiles:
                old = jobs[first_job]["post"]
                jobs[first_job]["post"] = (lambda old=old, qi=qi: ((old() if old else None), emit_loads(qi + 1)))

        LA = 2
        emit_loads(0)
        pend = {}

        def start(j):
            pend[j] = jobs[j]["qk"]()

        for j in range(min(LA, len(jobs))):
            start(j)
        for j in range(len(jobs)):
            if j + LA < len(jobs):
                start(j + LA)
            pt, ptres = pend.pop(j)
            jobs[j]["pv"](pt, ptres)
            if jobs[j]["post"]:
                jobs[j]["post"]()
        s.barrier()


PHASES.append(("nsa", phase_nsa))


def phase_ret(s, X, L, h_in):
    nc = X.nc
    with ExitStack() as es:
        sb = lambda name, shape, dt: es.enter_context(nc.sbuf_tensor("%s_L%d" % (name, L), shape, dt))
        qT = [sb("r_qT%d" % i, [64, 8, 128], BF16) for i in range(2)]
        kT = [sb("r_kT%d" % i, [64, 8, 128], BF16) for i in range(2)]
        ones1 = sb("r_ones1", [1, 128], F32)
        gn1 = sb("r_gn1", [1, 512], F32)
        km = [sb("r_km%d" % i, [128, 512], BF16) for i in range(2)]
        vm = [sb("r_vm%d" % i, [128, 512], BF16) for i in range(2)]
        gm = [sb("r_gm%d" % i, [128, 512], BF16) for i in range(2)]
        state = sb("r_state", [64, 8, 64], F32)
        stateb = sb("r_stateb", [64, 8, 64], BF16)
        rmask = sb("r_mask", [128, 512], F32)
        gC = sb("r_gC", [64, 8], F32)
        gn = sb("r_gn", [128, 512], F32)
        Pm = sb("r_Pm", [128, 8, 128], BF16)
        sq = sb("r_sq", [128, 512], F32)
        y = sb("r_y", [128, 512], F32)
        yb = [sb("r_yb%d" % i, [128, 512], BF16) for i in range(2)]
        rT = [sb("r_rT%d" % i, [128, 4, 128], BF16) for i in range(2)]
        stt = sb("r_st", [128, 64], F32)
        s.dma("sp", rmask[:], X.c["c_rmask"], writes=["rmask"])
        s.dma("sp", gC[:], X.c["c_gC8"], writes=["gC"])
        s.dma("sp", gn1[:], X.ret_gn[L:L + 1, :], writes=["gn1"])
        s.op("dve", lambda e: e.memset(ones1[:], 1.0), writes=["ones1"])
        s.op("pe", lambda e: e.matmul(X.ps[6][:], lhsT=ones1[:], rhs=gn1[:], start=True, stop=True),
             reads=["ones1", "gn1"], writes=["ps6"])
        s.op("dve", lambda e: e.tensor_copy(out=gn[:], in_=X.ps[6][:]), reads=["ps6"], writes=["gn"])
        s.op("dve", lambda e: e.memset(state[:], 0.0), writes=["state"])
        for c in range(X.ret_chunks):
            b = c % 2
            tok = slice(c * 128, (c + 1) * 128)
            for j in range(4):
                s.dma("sp", qT[b][:, 2 * j:2 * j + 2, :], X.QRT[j, :, tok].rearrange("(two d) t -> d two t", two=2),
                      reads=["QRT"], writes=["qT%d" % b])
                s.dma("sp", kT[b][:, 2 * j:2 * j + 2, :], X.KRT[j, :, tok].rearrange("(two d) t -> d two t", two=2),
                      reads=["KRT"], writes=["kT%d" % b])
            s.dma("sp", km[b][:], X.KRM[tok, :], reads=["KRM"], writes=["km%d" % b])
            s.dma("sp", vm[b][:], X.VRM[tok, :], reads=["VRM"], writes=["vm%d" % b])
            s.dma("sp", gm[b][:], X.GRM[tok, :], reads=["GRM"], writes=["gm%d" % b])
            for h in range(8):
                hp, base = h // 2, 64 * (h % 2)
                pb = h // 4
                s.op("pe", lambda e: e.matmul(X.ps[pb][:, (h % 4) * 128:(h % 4 + 1) * 128],
                                              lhsT=kT[b][:, h, :], rhs=qT[b][:, h, :],
                                              start=(h % 4 == 0), stop=True),
                     reads=["kT%d" % b, "qT%d" % b], writes=["ps%d" % pb])
            for pb in range(2):
                s.op("dve", lambda e: e.tensor_tensor(out=Pm[:, 4 * pb:4 * pb + 4, :].rearrange("p a t -> p (a t)"),
                                                      in0=X.ps[pb][:], in1=rmask[:], op=ALU.mult),
                     reads=["ps%d" % pb, "rmask"], writes=["Pm"])
            for h in range(8):
                hp, base = h // 2, 64 * (h % 2)
                s.op("pe", lambda e: e.matmul(X.ps[2][:, h * 64:(h + 1) * 64], lhsT=Pm[:, h, :],
                                              rhs=vm[b][:, h * 64:(h + 1) * 64], start=(h == 0), stop=(c == 0)),
                     reads=["Pm", "vm%d" % b], writes=["ps2"])
                if c > 0:
                    s.op("pe", lambda e: e.matmul(X.ps[2][:, h * 64:(h + 1) * 64], lhsT=qT[b][:, h, :],
                                                  rhs=stateb[:, h, :], start=False, stop=True),
                         reads=["qT%d" % b, "stateb"], writes=["ps2"])
            if True:
                for h in range(8):
                    s.op("pe", lambda e: e.matmul(X.ps[3][0:64, h * 64:(h + 1) * 64], lhsT=km[b][:, h * 64:(h + 1) * 64],
                                                  rhs=vm[b][:, h * 64:(h + 1) * 64], start=(h == 0), stop=True),
                         reads=["km%d" % b, "vm%d" % b], writes=["ps3"])
                s.op("dve", lambda e: e.tensor_tensor(out=state[:], in0=state[:],
                                                      in1=X.ps[3][0:64, :].rearrange("p (h x) -> p h x", h=8), op=ALU.add),
                     reads=["ps3"], writes=["state"])
                s.op("dve", lambda e: e.tensor_tensor(out=state[:], in0=state[:], in1=bcast_mid(gC[:], 64), op=ALU.mult),
                     reads=["gC"], writes=["state"])
                s.op("dve", lambda e: e.tensor_copy(out=stateb[:], in_=state[:]), reads=["state"], writes=["stateb"])
            o3 = X.ps[2][:].rearrange("p (h d) -> p h d", h=8)
            s.op("dve", lambda e: e.tensor_reduce(out=stt[:, 0:8], in_=o3, axis=AX.X, op=ALU.add),
                 reads=["ps2"], writes=["r_st"])
            s.op("act", lambda e: e.activation(out=sq[:], in_=X.ps[2][:], func=AF.Square), reads=["ps2"], writes=["r_sq"])
            s.op("dve", lambda e: e.tensor_reduce(out=stt[:, 8:16], in_=sq[:].rearrange("p (h d) -> p h d", h=8),
                                                  axis=AX.X, op=ALU.add), reads=["r_sq"], writes=["r_st"])
            s.op("dve", lambda e: e.tensor_scalar(out=stt[:, 16:24], in0=stt[:, 0:8], scalar1=1.0 / 64, scalar2=None,
                                                  op0=ALU.mult), reads=["r_st"], writes=["r_st"])
            s.op("dve", lambda e: e.tensor_tensor(out=stt[:, 24:32], in0=stt[:, 16:24], in1=stt[:, 16:24], op=ALU.mult),
                 reads=["r_st"], writes=["r_st"])
            s.op("dve", lambda e: e.scalar_tensor_tensor(out=stt[:, 32:40], in0=stt[:, 8:16], scalar=1.0 / 64,
                                                         in1=stt[:, 24:32], op0=ALU.mult, op1=ALU.subtract),
                 reads=["r_st"], writes=["r_st"])
            s.op("dve", lambda e: e.tensor_scalar(out=stt[:, 32:40], in0=stt[:, 32:40], scalar1=EPS, scalar2=None,
                                                  op0=ALU.add), reads=["r_st"], writes=["r_st"])
            s.op("act", lambda e: e.activation(out=stt[:, 40:48], in_=stt[:, 32:40], func=AF.Sqrt),
                 reads=["r_st"], writes=["r_st"])
            s.op("dve", lambda e: e.reciprocal(out=stt[:, 48:56], in_=stt[:, 40:48]), reads=["r_st"], writes=["r_st"])
            y3 = y[:].rearrange("p (h d) -> p h d", h=8)
            s.op("dve", lambda e: e.tensor_tensor(out=y3, in0=o3, in1=bcast_mid(stt[:, 16:24], 64), op=ALU.subtract),
                 reads=["ps2", "r_st"], writes=["r_y"])
            s.op("dve", lambda e: e.tensor_tensor(out=y3, in0=y3, in1=bcast_mid(stt[:, 48:56], 64), op=ALU.mult),
                 reads=["r_st"], writes=["r_y"])
            s.op("pool", lambda e: e.tensor_tensor(out=y[:], in0=y[:], in1=gn[:], op=ALU.mult),
                 reads=["r_y", "gn"], writes=["r_y"])
            s.op("pool", lambda e: e.tensor_tensor(out=yb[b][:], in0=y[:], in1=gm[b][:], op=ALU.mult),
                 reads=["r_y", "gm%d" % b], writes=["r_yb%d" % b])
            psb = X.ps[7][:].bitcast(BF16)
            for j in range(4):
                s.op("pe", lambda e: e.transpose(out=psb[:, j * 128:(j + 1) * 128], in_=yb[b][:, j * 128:(j + 1) * 128],
                                                 identity=X.ident[:]), reads=["r_yb%d" % b, "ident"], writes=["ps7"])
            s.op("act", lambda e: e.activation(out=rT[b][:].rearrange("p j t -> p (j t)"), in_=psb[:, 0:512], func=AF.Copy),
                 reads=["ps7"], writes=["r_rT%d" % b])
            s.dma("pool", X.AT[512:1024, tok].rearrange("(j p) t -> p j t", p=128), rT[b][:],
                  reads=["r_rT%d" % b], writes=["AT"])
        s.barrier()


PHASES.append(("ret", phase_ret))


def convert_weights(s, X, layers):
    def conv(dst, src, rows):
        for r0 in range(0, rows, 512):
            s.dma("pool", dst[r0:r0 + 512, :], src[r0:r0 + 512, :], writes=["W16"])
    if 0 in layers:
        conv(X.FG16[0], X.ffn_gate[0], D)
        conv(X.FU16[0], X.ffn_up[0], D)
        conv(X.FD16[0], X.ffn_down[0], D_FF)
    if 1 in layers:
        for e in range(NE):
            for blk in range(7):
                r0 = (e * 7 + blk) * 128
                fs = slice(blk * 512, (blk + 1) * 512)
                s.dma("pool", X.WGB[r0:r0 + 128, :].rearrange("p (k f) -> p k f", k=8),
                      X.moe_gate[0, e][:, fs].rearrange("(k p) f -> p k f", p=128), writes=["W16"])
                s.dma("pool", X.WUB[r0:r0 + 128, :].rearrange("p (k f) -> p k f", k=8),
                      X.moe_up[0, e][:, fs].rearrange("(k p) f -> p k f", p=128), writes=["W16"])
                s.dma("pool", X.WDB[r0:r0 + 128, :].rearrange("p (c n) -> p c n", c=4),
                      X.moe_down[0, e][fs, :].rearrange("(c p) n -> p c n", p=128), writes=["W16"])


def phase_ffn(s, X, L, h_in):
    nc = X.nc
    last = (L == DEPTH - 1)
    moe = (L % 2 == 1)
    if moe and X.routed:
        return phase_moe(s, X, L, h_in)
    h_out = X.out if last else X.HB
    G16, U16, D16 = X.FG16, X.FU16, X.FD16
    with ExitStack() as es:
        sb = lambda name, shape, dt: es.enter_context(nc.sbuf_tensor("%s_L%d" % (name, L), shape, dt))
        wout = sb("f_wout", [128, 8, D], BF16)
        pgate = sb("f_pgate", [128, 8, D], BF16)
        pproj = sb("f_pproj", [128, 2, D], BF16)
        g2T = sb("f_g2T", [128, 8], F32)
        g3T = sb("f_g3T", [128, 8], F32)
        rw = sb("f_rw", [128, 8, NE], BF16)
        cT = sb("f_cT", [128, 8, 512], BF16)
        hin = [sb("f_hin%d" % i, [128, D], F32) for i in range(2)]
        h1 = sb("f_h1", [128, 4, D], F32)
        hnT = sb("f_hnT", [128, 8, 512], BF16)
        acc = sb("f_acc", [128, 4, D], F32)
        wg = [sb("f_wg%d" % i, [128, 8, 512], BF16) for i in range(3)]
        wu = [sb("f_wu%d" % i, [128, 8, 512], BF16) for i in range(3)]
        wd = [sb("f_wd%d" % i, [128, 4, D], BF16) for i in range(3)]
        silu_t = [sb("f_silu%d" % i, [128, 512], BF16) for i in range(2)]
        actb = [sb("f_actb%d" % i, [128, 4, 512], BF16) for i in range(2)]
        sig = [sb("f_sig%d" % i, [128, 512], F32) for i in range(2)]
        ptile = [sb("f_pt%d" % i, [128, 256], F32) for i in range(2)]
        pb = sb("f_pb", [128, 256], BF16)
        pT = sb("f_pT", [128, 2, 512], BF16)
        gates = sb("f_gates", [128, 4, NE], F32)
        lg = sb("f_lg", [128, 16], F32)
        sm = sb("f_sm", [128, 32], F32)
        for kc in range(8):
            s.dma("pool", wout[:, kc, :], X.w_out[L, kc * 128:(kc + 1) * 128, :], writes=["wout"])
            s.dma("pool", pgate[:, kc, :], X.ple_gate[L, kc * 128:(kc + 1) * 128, :], writes=["pgate"])
        for kc in range(2):
            s.dma("pool", pproj[:, kc, :], X.ple_proj[L, kc * 128:(kc + 1) * 128, :], writes=["pproj"])
        s.dma("sp", g2T[:], X.g_ffn[L].rearrange("(k p) -> p k", p=128), writes=["g2T"], allow_slow_non_contiguous=True)
        s.dma("sp", g3T[:], X.g_ple[L].rearrange("(k p) -> p k", p=128), writes=["g3T"], allow_slow_non_contiguous=True)
        if moe:
            s.dma("pool", rw[:], X.moe_router[0].rearrange("(k p) e -> p k e", p=128), writes=["rw"])
        if last:
            gfin = sb("f_gfin", [128, D], F32)
            ones1 = sb("f_ones1", [1, 128], F32)
            gf1 = sb("f_gf1", [1, D], F32)
            s.dma("sp", gf1[:], X.g_final.rearrange("(o n) -> o n", o=1), writes=["gf1"])
            s.op("dve", lambda e: e.memset(ones1[:], 1.0), writes=["ones1"])
            for half in range(2):
                s.op("pe", lambda e: e.matmul(X.ps[6][:], lhsT=ones1[:], rhs=gf1[:, half * 512:(half + 1) * 512],
                                              start=True, stop=True), reads=["ones1", "gf1"], writes=["ps6"])
                s.op("dve", lambda e: e.tensor_copy(out=gfin[:, half * 512:(half + 1) * 512], in_=X.ps[6][:]),
                     reads=["ps6"], writes=["gfin"])
        wcnt = 0
        for tg in range(NT // 4):
            toks = slice(tg * 512, (tg + 1) * 512)
            s.dma("sp", cT[:], X.AT[:, toks].rearrange("(k p) t -> p k t", p=128), reads=["AT"], writes=["cT"])
            for tt in range(4):
                ti = 4 * tg + tt
                hb_ = hin[ti % 2]
                s.dma("sp", hb_[:], h_in[ti * 128:(ti + 1) * 128, :], reads=["HB"], writes=["hin%d" % (ti % 2)])
                for half in range(2):
                    hs = slice(half * 512, (half + 1) * 512)
                    for k in range(8):
                        s.op("pe", lambda e: e.matmul(X.ps[6][:], lhsT=cT[:, k, tt * 128:(tt + 1) * 128], rhs=wout[:, k, hs],
                                                      start=(k == 0), stop=(k == 7)), reads=["cT", "wout"], writes=["ps6"])
                    s.op("dve", lambda e: e.tensor_tensor(out=h1[:, tt, hs], in0=X.ps[6][:], in1=hb_[:, hs], op=ALU.add),
                         reads=["ps6", "hin%d" % (ti % 2)], writes=["h1_%d" % tt])
                ln_a(s, X, h1[:, tt, :], "h1_%d" % tt, tt)
            for tt in range(4):
                ln_b(s, X, tt, g2T[:], "g2T", hnT[:, :, tt * 128:(tt + 1) * 128], "f_hnT")
            if moe:
                for tt in range(4):
                    for k in range(8):
                        s.op("pe", lambda e: e.matmul(X.ps[6][:, 0:NE], lhsT=hnT[:, k, tt * 128:(tt + 1) * 128], rhs=rw[:, k, :],
                                                      start=(k == 0), stop=(k == 7)), reads=["f_hnT", "rw"], writes=["ps6"])
                    s.op("dve", lambda e: e.tensor_copy(out=lg[:, 0:8], in_=X.ps[6][:, 0:NE]), reads=["ps6"], writes=["lg"])
                    s.op("dve", lambda e: e.max(out=sm[:, 0:8], in_=lg[:, 0:8]), reads=["lg"], writes=["fsm"])
                    s.op("dve", lambda e: e.tensor_tensor(out=sm[:, 8:9], in0=sm[:, 1:2], in1=sm[:, 0:1], op=ALU.subtract),
                         reads=["fsm"], writes=["fsm"])
                    s.op("act", lambda e: e.activation(out=sm[:, 9:10], in_=sm[:, 8:9], func=AF.Exp), reads=["fsm"], writes=["fsm"])
                    s.op("dve", lambda e: e.tensor_scalar(out=sm[:, 10:11], in0=sm[:, 9:10], scalar1=1.0, scalar2=None,
                                                          op0=ALU.add), reads=["fsm"], writes=["fsm"])
                    s.op("dve", lambda e: e.reciprocal(out=sm[:, 11:12], in_=sm[:, 10:11]), reads=["fsm"], writes=["fsm"])
                    s.op("dve", lambda e: e.tensor_tensor(out=sm[:, 12:13], in0=sm[:, 9:10], in1=sm[:, 11:12], op=ALU.mult),
                         reads=["fsm"], writes=["fsm"])
                    s.op("dve", lambda e: e.tensor_scalar(out=lg[:, 8:16], in0=lg[:, 0:8], scalar1=sm[:, 0:1],
                                                          scalar2=sm[:, 11:12], op0=ALU.is_equal, op1=ALU.mult),
                         reads=["lg", "fsm"], writes=["lg"])
                    s.op("dve", lambda e: e.tensor_scalar(out=gates[:, tt, :], in0=lg[:, 0:8], scalar1=sm[:, 1:2],
                                                          scalar2=sm[:, 12:13], op0=ALU.is_equal, op1=ALU.mult),
                         reads=["lg", "fsm"], writes=["gates"])
                    s.op("dve", lambda e: e.tensor_tensor(out=gates[:, tt, :], in0=gates[:, tt, :], in1=lg[:, 8:16], op=ALU.add),
                         reads=["lg"], writes=["gates"])
            def stage_a(J):
                wb, ab = J % 3, J % 2
                for fc in range(4):
                    pg, pu = 2 * (fc % 2), 2 * (fc % 2) + 1
                    for k in range(8):
                        s.op("pe", lambda e: e.matmul(X.ps[pg][:], lhsT=wg[wb][:, k, fc * 128:(fc + 1) * 128], rhs=hnT[:, k, :],
                                                      start=(k == 0), stop=(k == 7)),
                             reads=["wg%d" % wb, "f_hnT"], writes=["ps%d" % pg])
                    for k in range(8):
                        s.op("pe", lambda e: e.matmul(X.ps[pu][:], lhsT=wu[wb][:, k, fc * 128:(fc + 1) * 128], rhs=hnT[:, k, :],
                                                      start=(k == 0), stop=(k == 7)),
                             reads=["wu%d" % wb, "f_hnT"], writes=["ps%d" % pu])
                    s.op("act", lambda e: e.activation(out=silu_t[fc % 2][:], in_=X.ps[pg][:], func=AF.Silu),
                         reads=["ps%d" % pg], writes=["silu%d" % (fc % 2)])
                    s.op("dve", lambda e: e.tensor_tensor(out=actb[ab][:, fc, :], in0=X.ps[pu][:], in1=silu_t[fc % 2][:],
                                                          op=ALU.mult),
                         reads=["ps%d" % pu, "silu%d" % (fc % 2)], writes=["actb%d" % ab])

            def stage_b(J):
                wb, ab, blk = J % 3, J % 2, J % 7
                for tt in range(4):
                    for half in range(2):
                        hs = slice(half * 512, (half + 1) * 512)
                        pd = 4 + half
                        for fc in range(4):
                            s.op("pe", lambda e: e.matmul(X.ps[pd][:], lhsT=actb[ab][:, fc, tt * 128:(tt + 1) * 128],
                                                          rhs=wd[wb][:, fc, hs], start=(fc == 0), stop=(fc == 3)),
                                 reads=["actb%d" % ab, "wd%d" % wb], writes=["ps%d" % pd])
                        dst = acc[:, tt, hs]
                        if blk == 0:
                            s.op("dve", lambda e: e.tensor_copy(out=dst, in_=X.ps[pd][:]),
                                 reads=["ps%d" % pd], writes=["acc%d" % tt])
                        else:
                            s.op("dve", lambda e: e.tensor_tensor(out=dst, in0=X.ps[pd][:], in1=dst, op=ALU.add),
                                 reads=["ps%d" % pd], writes=["acc%d" % tt])

            def wload(J):
                blk, wb = J % 7, J % 3
                fs = slice(blk * 512, (blk + 1) * 512)
                s.dma("sp", wg[wb][:], G16[0][:, fs].rearrange("(k p) f -> p k f", p=128), reads=["W16"], writes=["wg%d" % wb])
                s.dma("sp", wu[wb][:], U16[0][:, fs].rearrange("(k p) f -> p k f", p=128), reads=["W16"], writes=["wu%d" % wb])
                s.dma("sp", wd[wb][:], D16[0][fs, :].rearrange("(c p) n -> p c n", p=128), reads=["W16"], writes=["wd%d" % wb])

            NJ = (NT // 4) * 7
            if tg == 0:
                wload(0)
                wload(1)
            for blk in range(7):
                J = tg * 7 + blk
                stage_a(J)
                if blk >= 1:
                    stage_b(J - 1)
                if J + 2 < NJ:
                    wload(J + 2)
            stage_b(tg * 7 + 6)
            for tt in range(4):
                s.op("pool", lambda e: e.tensor_tensor(out=h1[:, tt, :], in0=h1[:, tt, :], in1=acc[:, tt, :], op=ALU.add),
                     reads=["acc%d" % tt], writes=["h1_%d" % tt])
                ln_a(s, X, h1[:, tt, :], "h1_%d" % tt, tt)
            for tt in range(4):
                ln_b(s, X, tt, g3T[:], "g3T", hnT[:, :, tt * 128:(tt + 1) * 128], "f_hnT")
            psb = X.ps[7][:].bitcast(BF16)
            for tt in range(4):
                ti = 4 * tg + tt
                pt_ = ptile[ti % 2]
                s.dma("sp", pt_[:], X.p[L, ti * 128:(ti + 1) * 128, :], writes=["pt%d" % (ti % 2)])
                s.op("act", lambda e: e.activation(out=pb[:], in_=pt_[:], func=AF.Copy), reads=["pt%d" % (ti % 2)], writes=["pb"])
                for k in range(2):
                    s.op("pe", lambda e: e.transpose(out=psb[:, k * 128:(k + 1) * 128], in_=pb[:, k * 128:(k + 1) * 128],
                                                     identity=X.ident[:]), reads=["pb", "ident"], writes=["ps7"])
                s.op("act", lambda e: e.activation(out=pT[:, :, tt * 128:(tt + 1) * 128],
                                                   in_=psb[:, 0:256].rearrange("p (k t) -> p k t", k=2), func=AF.Copy),
                     reads=["ps7"], writes=["pT"])
            for tt in range(4):
                ti = 4 * tg + tt
                for half in range(2):
                    hs = slice(half * 512, (half + 1) * 512)
                    for k in range(2):
                        s.op("pe", lambda e: e.matmul(X.ps[6][:], lhsT=pT[:, k, tt * 128:(tt + 1) * 128], rhs=pproj[:, k, hs],
                                                      start=(k == 0), stop=(k == 1)), reads=["pT", "pproj"], writes=["ps6"])
                    pgb = 4 + half
                    for k in range(8):
                        s.op("pe", lambda e: e.matmul(X.ps[pgb][:], lhsT=hnT[:, k, tt * 128:(tt + 1) * 128], rhs=pgate[:, k, hs],
                                                      start=(k == 0), stop=(k == 7)), reads=["f_hnT", "pgate"], writes=["ps%d" % pgb])
                    s.op("act", lambda e: e.activation(out=sig[half][:], in_=X.ps[pgb][:], func=AF.Sigmoid),
                         reads=["ps%d" % pgb], writes=["sig%d" % half])
                    s.op("dve", lambda e: e.tensor_tensor(out=acc[:, tt, hs], in0=X.ps[6][:], in1=sig[half][:], op=ALU.mult),
                         reads=["ps6", "sig%d" % half], writes=["acc%d" % tt])
                s.op("pool", lambda e: e.tensor_tensor(out=h1[:, tt, :], in0=h1[:, tt, :], in1=acc[:, tt, :], op=ALU.add),
                     reads=["acc%d" % tt], writes=["h1_%d" % tt])
                if last:
                    ln_a(s, X, h1[:, tt, :], "h1_%d" % tt, tt)
                    s.op("act", lambda e: e.activation(out=acc[:, tt, :], in_=h1[:, tt, :], func=AF.Copy, scale=X.ss[tt][:, 2:3]),
                         reads=["h1_%d" % tt, "ss%d" % tt], writes=["acc%d" % tt])
                    s.op("pool", lambda e: e.tensor_tensor(out=acc[:, tt, :], in0=acc[:, tt, :], in1=gfin[:], op=ALU.mult),
                         reads=["gfin"], writes=["acc%d" % tt])
                    s.dma("pool", h_out[ti * 128:(ti + 1) * 128, :], acc[:, tt, :], reads=["acc%d" % tt], writes=["HOUT"])
                else:
                    s.dma("pool", h_out[ti * 128:(ti + 1) * 128, :], h1[:, tt, :], reads=["h1_%d" % tt], writes=["HB"])
        s.barrier()


PHASES.append(("ffn", phase_ffn))


def phase_moe(s, X, L, h_in):
    nc = X.nc
    last = (L == DEPTH - 1)
    h_out = X.out if last else X.HB
    with ExitStack() as es:
        sb = lambda name, shape, dt: es.enter_context(nc.sbuf_tensor("%s_L%d" % (name, L), shape, dt))
        OH = [sb("m_oh%d" % k, [128, NT, NE], F32) for k in range(2)]
        WK = [sb("m_wk%d" % k, [128, NT], F32) for k in range(2)]
        POSI2 = sb("m_posi", [128, 2 * NT], I32)
        POSI = POSI2[:].rearrange("p (k i) -> p k i", k=2)
        WIDX2 = sb("m_widx", [128, NGRP * 7], I32)
        WIDX = WIDX2[:].rearrange("p (g b) -> p g b", b=7)
        g2T = sb("m_g2T", [128, 8], F32)
        g3T = sb("m_g3T", [128, 8], F32)
        hnT = sb("m_hnT", [128, 8, 512], BF16)
        s.dma("sp", g2T[:], X.g_ffn[L].rearrange("(k p) -> p k", p=128), writes=["g2T"], allow_slow_non_contiguous=True)
        s.dma("sp", g3T[:], X.g_ple[L].rearrange("(k p) -> p k", p=128), writes=["g3T"], allow_slow_non_contiguous=True)
        with ExitStack() as es2:
            sb2 = lambda name, shape, dt: es2.enter_context(nc.sbuf_tensor("%s_L%d" % (name, L), shape, dt))
            wout = sb2("ma_wout", [128, 8, D], BF16)
            rw = sb2("ma_rw", [128, 8, NE], BF16)
            cT = sb2("ma_cT", [128, 8, 512], BF16)
            hin = [sb2("ma_hin%d" % i, [128, D], F32) for i in range(2)]
            h1 = [sb2("ma_h1%d" % i, [128, D], F32) for i in range(4)]
            lg = sb2("ma_lg", [128, 16], F32)
            sm = sb2("ma_sm", [128, 32], F32)
            for kc in range(8):
                s.dma("pool", wout[:, kc, :], X.w_out[L, kc * 128:(kc + 1) * 128, :], writes=["wout"])
            s.dma("pool", rw[:], X.moe_router[0].rearrange("(k p) e -> p k e", p=128), writes=["rw"])
            for tg in range(NT // 4):
                toks = slice(tg * 512, (tg + 1) * 512)
                s.dma("sp", cT[:], X.AT[:, toks].rearrange("(k p) t -> p k t", p=128), reads=["AT"], writes=["cT"])
                for tt in range(4):
                    ti = 4 * tg + tt
                    hb_ = hin[ti % 2]
                    s.dma("sp", hb_[:], h_in[ti * 128:(ti + 1) * 128, :], reads=["HB"], writes=["hin%d" % (ti % 2)])
                    for half in range(2):
                        hs = slice(half * 512, (half + 1) * 512)
                        pbk = 4 + half
                        for k in range(8):
                            s.op("pe", lambda e: e.matmul(X.ps[pbk][:], lhsT=cT[:, k, tt * 128:(tt + 1) * 128], rhs=wout[:, k, hs],
                                                          start=(k == 0), stop=(k == 7)), reads=["cT", "wout"], writes=["ps%d" % pbk])
                        s.op("dve", lambda e: e.tensor_tensor(out=h1[tt][:, hs], in0=X.ps[pbk][:], in1=hb_[:, hs], op=ALU.add),
                             reads=["ps%d" % pbk, "hin%d" % (ti % 2)], writes=["h1_%d" % tt])
                    s.dma("pool", X.H1[ti * 128:(ti + 1) * 128, :], h1[tt][:], reads=["h1_%d" % tt], writes=["H1"])
                    ln_a(s, X, h1[tt][:], "h1_%d" % tt, tt)
                    s.dma("pool", X.XS2[ti * 128:(ti + 1) * 128, :], X.xs[tt][:], reads=["xs%d" % tt], writes=["XS2"])
                for tt in range(4):
                    ln_b(s, X, tt, g2T[:], "g2T", hnT[:, :, tt * 128:(tt + 1) * 128], "m_hnT")
                for tt in range(4):
                    ti = 4 * tg + tt
                    for k in range(8):
                        s.op("pe", lambda e: e.matmul(X.ps[6][:, 0:NE], lhsT=hnT[:, k, tt * 128:(tt + 1) * 128], rhs=rw[:, k, :],
                                                      start=(k == 0), stop=(k == 7)), reads=["m_hnT", "rw"], writes=["ps6"])
                    s.op("dve", lambda e: e.tensor_copy(out=lg[:, 0:8], in_=X.ps[6][:, 0:NE]), reads=["ps6"], writes=["lg"])
                    s.op("dve", lambda e: e.max(out=sm[:, 0:8], in_=lg[:, 0:8]), reads=["lg"], writes=["fsm"])
                    s.op("dve", lambda e: e.tensor_tensor(out=sm[:, 8:9], in0=sm[:, 1:2], in1=sm[:, 0:1], op=ALU.subtract),
                         reads=["fsm"], writes=["fsm"])
                    s.op("act", lambda e: e.activation(out=sm[:, 9:10], in_=sm[:, 8:9], func=AF.Exp), reads=["fsm"], writes=["fsm"])
                    s.op("dve", lambda e: e.tensor_scalar(out=sm[:, 10:11], in0=sm[:, 9:10], scalar1=1.0, scalar2=None,
                                                          op0=ALU.add), reads=["fsm"], writes=["fsm"])
                    s.op("dve", lambda e: e.reciprocal(out=WK[0][:, ti:ti + 1], in_=sm[:, 10:11]), reads=["fsm"], writes=["WK"])
                    s.op("dve", lambda e: e.tensor_tensor(out=WK[1][:, ti:ti + 1], in0=sm[:, 9:10], in1=WK[0][:, ti:ti + 1],
                                                          op=ALU.mult), reads=["fsm", "WK"], writes=["WK"])
                    s.op("dve", lambda e: e.tensor_scalar(out=OH[0][:, ti, :], in0=lg[:, 0:8], scalar1=sm[:, 0:1], scalar2=None,
                                                          op0=ALU.is_equal), reads=["lg", "fsm"], writes=["OH"])
                    s.op("dve", lambda e: e.tensor_scalar(out=OH[1][:, ti, :], in0=lg[:, 0:8], scalar1=sm[:, 1:2], scalar2=None,
                                                          op0=ALU.is_equal), reads=["lg", "fsm"], writes=["OH"])
            s.barrier()
        with ExitStack() as es2:
            sb2 = lambda name, shape, dt: es2.enter_context(nc.sbuf_tensor("%s_L%d" % (name, L), shape, dt))
            Mf = sb2("mb_Mf", [128, NT, NE], F32)
            Mb = sb2("mb_Mb", [128, NT * NE], BF16)
            U = sb2("mb_U", [128, 128], BF16)
            ones = sb2("mb_ones", [128, 128], BF16)
            A_ = sb2("mb_A", [128, NT, NE], F32)
            B_ = sb2("mb_B", [128, NT, NE], F32)
            CN = sb2("mb_CN", [128, NT, NE], F32)
            POS = sb2("mb_POS", [128, NT, NE], F32)
            nE = sb2("mb_nE", [128, 64], F32)
            cmp16 = sb2("mb_cmp16", [128, NE, 16], F32)
            gi512 = sb2("mb_gi512", [128, NGRP], F32)
            tokid = sb2("mb_tokid", [128, NT], F32)
            blkp = sb2("mb_blkp", [128, 7], F32)
            cmpg = sb2("mb_cmpg", [128, NGRP, NE], F32)
            EG = sb2("mb_EG", [128, NGRP], F32)
            WF = sb2("mb_WF", [128, NGRP, 7], F32)
            PK = sb2("mb_PK", [128, 2, NT], F32)
            ROWS2 = sb2("mb_rows", [128, 2 * NT * 2], I32)
            ROWS = ROWS2[:].rearrange("p (k i c) -> p k i c", k=2, c=2)
            Z = sb2("mb_Z", [128, 2 * NSLOT // 128], I32)
            s.dma("pool", U[:], X.c["c_U"], writes=["U"])
            s.dma("sp", gi512[:], X.c["c_gi512"], writes=["gi512"])
            s.dma("sp", tokid[:], X.c["c_tokid"], writes=["tokid"])
            s.dma("sp", blkp[:], X.c["c_blkp"], writes=["blkp"])
            s.op("pool", lambda e: e.memset(ones[:], 1.0), writes=["ones"])
            s.op("pool", lambda e: e.memset(Z[:], 0), writes=["Z"])
            s.dma("sp", X.SLOT.rearrange("(p a) c -> p (a c)", p=128), Z[:], reads=["Z"], writes=["SLOT"])
            s.op("dve", lambda e: e.tensor_tensor(out=Mf[:], in0=OH[0][:], in1=OH[1][:], op=ALU.add), reads=["OH"], writes=["Mf"])
            s.op("dve", lambda e: e.tensor_copy(out=Mb[:], in_=Mf[:].rearrange("p i e -> p (i e)")), reads=["Mf"], writes=["Mb"])
            s.op("pe", lambda e: e.matmul(X.ps[0][:], lhsT=U[:], rhs=Mb[:], start=True, stop=True), reads=["U", "Mb"], writes=["ps0"])
            s.op("pe", lambda e: e.matmul(X.ps[1][:], lhsT=ones[:], rhs=Mb[:], start=True, stop=True), reads=["ones", "Mb"], writes=["ps1"])
            s.op("dve", lambda e: e.tensor_copy(out=CN[:].rearrange("p i e -> p (i e)"), in_=X.ps[1][:]), reads=["ps1"], writes=["CN"])
            s.op("dve", lambda e: e.tensor_copy(out=A_[:], in_=CN[:]), reads=["CN"], writes=["A"])
            src, dst, sres, dres = A_, B_, "A", "B"
            for sh in (1, 2, 4, 8, 16, 32):
                s.op("dve", lambda e: e.tensor_copy(out=dst[:, 0:sh, :], in_=src[:, 0:sh, :]), reads=[sres], writes=[dres])
                s.op("dve", lambda e: e.tensor_tensor(out=dst[:, sh:NT, :], in0=src[:, sh:NT, :], in1=src[:, 0:NT - sh, :],
                                                      op=ALU.add), reads=[sres], writes=[dres])
                src, dst, sres, dres = dst, src, dres, sres
            incl, ires = src, sres
            s.op("dve", lambda e: e.tensor_copy(out=nE[:, 0:8], in_=incl[:, NT - 1, :]), reads=[ires], writes=["nE"])
            s.op("dve", lambda e: e.tensor_tensor(out=cmp16[:], in0=bcast_mid(nE[:, 0:8], 16), in1=bcast_rep(gi512[:, 0:16], NE),
                                                  op=ALU.is_gt), reads=["nE", "gi512"], writes=["cmp16"])
            s.op("dve", lambda e: e.tensor_reduce(out=nE[:, 8:16], in_=cmp16[:], axis=AX.X, op=ALU.add), reads=["cmp16"], writes=["nE"])
            s.op("dve", lambda e: e.tensor_scalar(out=nE[:, 8:16], in0=nE[:, 8:16], scalar1=512.0, scalar2=None, op0=ALU.mult),
                 reads=["nE"], writes=["nE"])
            s.op("dve", lambda e: e.tensor_copy(out=nE[:, 16:17], in_=nE[:, 8:9]), reads=["nE"], writes=["nE"])
            for e_ in range(1, NE):
                s.op("dve", lambda e: e.tensor_tensor(out=nE[:, 16 + e_:17 + e_], in0=nE[:, 15 + e_:16 + e_], in1=nE[:, 8 + e_:9 + e_],
                                                      op=ALU.add), reads=["nE"], writes=["nE"])
            s.op("dve", lambda e: e.tensor_tensor(out=nE[:, 24:32], in0=nE[:, 16:24], in1=nE[:, 8:16], op=ALU.subtract),
                 reads=["nE"], writes=["nE"])
            s.op("dve", lambda e: e.tensor_tensor(out=POS[:], in0=incl[:], in1=CN[:], op=ALU.subtract), reads=[ires, "CN"], writes=["POS"])
            s.op("dve", lambda e: e.tensor_tensor(out=POS[:].rearrange("p i e -> p (i e)"), in0=POS[:].rearrange("p i e -> p (i e)"),
                                                  in1=X.ps[0][:], op=ALU.add), reads=["ps0"], writes=["POS"])
            s.op("dve", lambda e: e.tensor_tensor(out=POS[:], in0=POS[:], in1=bcast_rep(nE[:, 24:32], NT), op=ALU.add),
                 reads=["nE"], writes=["POS"])
            for k in range(2):
                s.op("dve", lambda e: e.tensor_tensor(out=Mf[:], in0=POS[:], in1=OH[k][:], op=ALU.mult), reads=["POS", "OH"], writes=["Mf"])
                s.op("dve", lambda e: e.tensor_reduce(out=PK[:, k, :], in_=Mf[:], axis=AX.X, op=ALU.add), reads=["Mf"], writes=["PK"])
                s.op("dve", lambda e: e.tensor_copy(out=ROWS[:, k, :, 0], in_=tokid[:]), reads=["tokid"], writes=["ROWS"])
                s.op("dve", lambda e: e.tensor_copy(out=ROWS[:, k, :, 1], in_=WK[k][:].bitcast(I32)), reads=["WK"], writes=["ROWS"])
            s.op("dve", lambda e: e.tensor_copy(out=POSI, in_=PK[:]), reads=["PK"], writes=["POSI"])
            s.op("dve", lambda e: e.tensor_tensor(out=cmpg[:], in0=bcast_rep(nE[:, 16:24], NGRP), in1=bcast_mid(gi512[:], NE),
                                                  op=ALU.is_le), reads=["nE", "gi512"], writes=["cmpg"])
            s.op("dve", lambda e: e.tensor_reduce(out=EG[:], in_=cmpg[:], axis=AX.X, op=ALU.add), reads=["cmpg"], writes=["EG"])
            s.op("dve", lambda e: e.tensor_scalar(out=EG[:], in0=EG[:], scalar1=float(NE - 1), scalar2=896.0, op0=ALU.min, op1=ALU.mult),
                 reads=["EG"], writes=["EG"])
            s.op("dve", lambda e: e.tensor_tensor(out=WF[:], in0=bcast_mid(EG[:], 7), in1=bcast_rep(blkp[:], NGRP), op=ALU.add),
                 reads=["EG", "blkp"], writes=["WF"])
            s.op("dve", lambda e: e.tensor_copy(out=WIDX, in_=WF[:]), reads=["WF"], writes=["WIDX"])
            for ti in range(NT):
                for k in range(2):
                    s.idma(X.SLOT[:, :], POSI2[:, k * NT + ti:k * NT + ti + 1],
                           ROWS2[:, (k * NT + ti) * 2:(k * NT + ti) * 2 + 2], None, NSLOT - 1,
                           reads=["ROWS", "POSI"], writes=["SLOT"])
            s.barrier()
        with ExitStack() as es2:
            sb2 = lambda name, shape, dt: es2.enter_context(nc.sbuf_tensor("%s_L%d" % (name, L), shape, dt))
            stt = [sb2("mc_st%d" % i, [128, 8], I32) for i in range(2)]
            xg = [sb2("mc_xg%d" % i, [128, D], BF16) for i in range(8)]
            acc = [sb2("mc_acc%d" % i, [128, 4, D], F32) for i in range(2)]
            wg = [sb2("mc_wg%d" % i, [128, 8, 512], BF16) for i in range(3)]
            wu = [sb2("mc_wu%d" % i, [128, 8, 512], BF16) for i in range(3)]
            wd = [sb2("mc_wd%d" % i, [128, 4, D], BF16) for i in range(3)]
            silu_t = [sb2("mc_silu%d" % i, [128, 512], BF16) for i in range(2)]
            actb = [sb2("mc_actb%d" % i, [128, 4, 512], BF16) for i in range(2)]
            hn = [sb2("mc_hn%d" % i, [128, 8, 512], BF16) for i in range(2)]
            NWB = len(wg)

            def prologue_loads(gi):
                gb = gi % 2
                s.dma("sp", stt[gb][:].rearrange("p (tt c) -> p tt c", c=2),
                      X.SLOT[gi * 512:(gi + 1) * 512, :].rearrange("(tt p) c -> p tt c", p=128),
                      reads=["SLOT"], writes=["st%d" % gb])
                for tt in range(4):
                    s.idma(xg[gb * 4 + tt][:, :], None, X.XS2[:, :], stt[gb][:, 2 * tt:2 * tt + 1], SEQ - 1,
                           reads=["XS2", "st%d" % gb], writes=["xg%d" % (gb * 4 + tt)])

            def prologue_tr(gi):
                gb = gi % 2
                for tt in range(4):
                    ln_b(s, X, tt, g2T[:], "g2T", hn[gb][:, :, tt * 128:(tt + 1) * 128], "hn%d" % gb,
                         src=xg[gb * 4 + tt], src_res="xg%d" % (gb * 4 + tt))

            def wload(j):
                gi, blk = divmod(j, 7)
                wb = j % NWB
                off = WIDX2[:, gi * 7 + blk:gi * 7 + blk + 1]
                s.idma(wg[wb][:].rearrange("p k f -> p (k f)"), None, X.WGB[:, :], off, 0, reads=["W16", "WIDX"], writes=["wg%d" % wb])
                s.idma(wu[wb][:].rearrange("p k f -> p (k f)"), None, X.WUB[:, :], off, 0, reads=["W16", "WIDX"], writes=["wu%d" % wb])
                s.idma(wd[wb][:].rearrange("p c n -> p (c n)"), None, X.WDB[:, :], off, 0, reads=["W16", "WIDX"], writes=["wd%d" % wb])

            def stage_a(j):
                gi, blk = divmod(j, 7)
                gb, wb, ab = gi % 2, j % NWB, j % 2
                for fc in range(4):
                    pg, pu = 2 * (fc % 2), 2 * (fc % 2) + 1
                    for k in range(8):
                        s.op("pe", lambda e: e.matmul(X.ps[pg][:], lhsT=wg[wb][:, k, fc * 128:(fc + 1) * 128], rhs=hn[gb][:, k, :],
                                                      start=(k == 0), stop=(k == 7)),
                             reads=["wg%d" % wb, "hn%d" % gb], writes=["ps%d" % pg])
                    for k in range(8):
                        s.op("pe", lambda e: e.matmul(X.ps[pu][:], lhsT=wu[wb][:, k, fc * 128:(fc + 1) * 128], rhs=hn[gb][:, k, :],
                                                      start=(k == 0), stop=(k == 7)),
                             reads=["wu%d" % wb, "hn%d" % gb], writes=["ps%d" % pu])
                    s.op("act", lambda e: e.activation(out=silu_t[fc % 2][:], in_=X.ps[pg][:], func=AF.Silu),
                         reads=["ps%d" % pg], writes=["silu%d" % (fc % 2)])
                    s.op("dve", lambda e: e.tensor_tensor(out=actb[ab][:, fc, :], in0=X.ps[pu][:], in1=silu_t[fc % 2][:],
                                                          op=ALU.mult),
                         reads=["ps%d" % pu, "silu%d" % (fc % 2)], writes=["actb%d" % ab])

            def stage_b(j):
                gi, blk = divmod(j, 7)
                gb, wb, ab = gi % 2, j % NWB, j % 2
                for tt in range(4):
                    gsc = stt[gb][:, 2 * tt + 1:2 * tt + 2].bitcast(F32)
                    for half in range(2):
                        hs = slice(half * 512, (half + 1) * 512)
                        pd = 4 + half
                        for fc in range(4):
                            s.op("pe", lambda e: e.matmul(X.ps[pd][:], lhsT=actb[ab][:, fc, tt * 128:(tt + 1) * 128],
                                                          rhs=wd[wb][:, fc, hs], start=(fc == 0), stop=(fc == 3)),
                                 reads=["actb%d" % ab, "wd%d" % wb], writes=["ps%d" % pd])
                        dst = acc[gb][:, tt, hs]
                        if blk == 0:
                            s.op("dve", lambda e: e.tensor_scalar(out=dst, in0=X.ps[pd][:], scalar1=gsc, scalar2=None, op0=ALU.mult),
                                 reads=["ps%d" % pd, "st%d" % gb], writes=["acc%d_%d" % (gb, tt)])
                        else:
                            s.op("dve", lambda e: e.scalar_tensor_tensor(out=dst, in0=X.ps[pd][:], scalar=gsc, in1=dst,
                                                                         op0=ALU.mult, op1=ALU.add),
                                 reads=["ps%d" % pd, "st%d" % gb], writes=["acc%d_%d" % (gb, tt)])
                if blk == 6:
                    s.dma("sp", X.YS[gi * 512:(gi + 1) * 512, :].rearrange("(tt p) n -> p tt n", p=128), acc[gb][:],
                          reads=["acc%d_%d" % (gb, tt) for tt in range(4)], writes=["YS"])

            NJ = NGRP * 7
            prologue_loads(0)
            prologue_tr(0)
            for j in range(min(NWB - 1, NJ)):
                wload(j)
            for j in range(NJ + 1):
                if j < NJ:
                    gi, blk = divmod(j, 7)
                    stage_a(j)
                    if blk == 6 and gi + 1 < NGRP:
                        prologue_tr(gi + 1)
                if j >= 1:
                    stage_b(j - 1)
                if j < NJ and j % 7 == 0 and j // 7 + 1 < NGRP:
                    prologue_loads(j // 7 + 1)
                if j + NWB - 1 < NJ:
                    wload(j + NWB - 1)
            s.barrier()
        with ExitStack() as es2:
            sb2 = lambda name, shape, dt: es2.enter_context(nc.sbuf_tensor("%s_L%d" % (name, L), shape, dt))
            pgate = sb2("md_pgate", [128, 8, D], BF16)
            pproj = sb2("md_pproj", [128, 2, D], BF16)
            h1 = sb2("md_h1", [128, 4, D], F32)
            acc = sb2("md_acc", [128, 4, D], F32)
            yA = [sb2("md_yA%d" % i, [128, D], F32) for i in range(2)]
            yB = [sb2("md_yB%d" % i, [128, D], F32) for i in range(2)]
            sig = [sb2("md_sig%d" % i, [128, 512], F32) for i in range(2)]
            ptile = [sb2("md_pt%d" % i, [128, 256], F32) for i in range(2)]
            pb = sb2("md_pb", [128, 256], BF16)
            pT = sb2("md_pT", [128, 2, 512], BF16)
            for kc in range(8):
                s.dma("pool", pgate[:, kc, :], X.ple_gate[L, kc * 128:(kc + 1) * 128, :], writes=["pgate"])
            for kc in range(2):
                s.dma("pool", pproj[:, kc, :], X.ple_proj[L, kc * 128:(kc + 1) * 128, :], writes=["pproj"])
            if last:
                gfin = sb2("md_gfin", [128, D], F32)
                ones1 = sb2("md_ones1", [1, 128], F32)
                gf1 = sb2("md_gf1", [1, D], F32)
                s.dma("sp", gf1[:], X.g_final.rearrange("(o n) -> o n", o=1), writes=["gf1"])
                s.op("dve", lambda e: e.memset(ones1[:], 1.0), writes=["ones1"])
                for half in range(2):
                    s.op("pe", lambda e: e.matmul(X.ps[6][:], lhsT=ones1[:], rhs=gf1[:, half * 512:(half + 1) * 512],
                                                  start=True, stop=True), reads=["ones1", "gf1"], writes=["ps6"])
                    s.op("dve", lambda e: e.tensor_copy(out=gfin[:, half * 512:(half + 1) * 512], in_=X.ps[6][:]),
                         reads=["ps6"], writes=["gfin"])
            for tg in range(NT // 4):
                for tt in range(4):
                    ti = 4 * tg + tt
                    yb_ = ti % 2
                    s.dma("sp", h1[:, tt, :], X.H1[ti * 128:(ti + 1) * 128, :], reads=["H1"], writes=["h1_%d" % tt])
                    s.idma(yA[yb_][:, :], None, X.YS[:, :], POSI2[:, ti:ti + 1], NSLOT - 1, reads=["YS", "POSI"], writes=["yA%d" % yb_])
                    s.idma(yB[yb_][:, :], None, X.YS[:, :], POSI2[:, NT + ti:NT + ti + 1], NSLOT - 1, reads=["YS", "POSI"], writes=["yB%d" % yb_])
                    s.op("dve", lambda e: e.tensor_tensor(out=h1[:, tt, :], in0=h1[:, tt, :], in1=yA[yb_][:], op=ALU.add),
                         reads=["yA%d" % yb_], writes=["h1_%d" % tt])
                    s.op("dve", lambda e: e.tensor_tensor(out=h1[:, tt, :], in0=h1[:, tt, :], in1=yB[yb_][:], op=ALU.add),
                         reads=["yB%d" % yb_], writes=["h1_%d" % tt])
                    ln_a(s, X, h1[:, tt, :], "h1_%d" % tt, tt)
                for tt in range(4):
                    ln_b(s, X, tt, g3T[:], "g3T", hnT[:, :, tt * 128:(tt + 1) * 128], "m_hnT")
                psb = X.ps[7][:].bitcast(BF16)
                for tt in range(4):
                    ti = 4 * tg + tt
                    pt_ = ptile[ti % 2]
                    s.dma("sp", pt_[:], X.p[L, ti * 128:(ti + 1) * 128, :], writes=["pt%d" % (ti % 2)])
                    s.op("act", lambda e: e.activation(out=pb[:], in_=pt_[:], func=AF.Copy), reads=["pt%d" % (ti % 2)], writes=["pb"])
                    for k in range(2):
                        s.op("pe", lambda e: e.transpose(out=psb[:, k * 128:(k + 1) * 128], in_=pb[:, k * 128:(k + 1) * 128],
                                                         identity=X.ident[:]), reads=["pb", "ident"], writes=["ps7"])
                    s.op("act", lambda e: e.activation(out=pT[:, :, tt * 128:(tt + 1) * 128],
                                                       in_=psb[:, 0:256].rearrange("p (k t) -> p k t", k=2), func=AF.Copy),
                         reads=["ps7"], writes=["pT"])
                for tt in range(4):
                    ti = 4 * tg + tt
                    for half in range(2):
                        hs = slice(half * 512, (half + 1) * 512)
                        for k in range(2):
                            s.op("pe", lambda e: e.matmul(X.ps[6][:], lhsT=pT[:, k, tt * 128:(tt + 1) * 128], rhs=pproj[:, k, hs],
                                                          start=(k == 0), stop=(k == 1)), reads=["pT", "pproj"], writes=["ps6"])
                        pgb = 4 + half
                        for k in range(8):
                            s.op("pe", lambda e: e.matmul(X.ps[pgb][:], lhsT=hnT[:, k, tt * 128:(tt + 1) * 128], rhs=pgate[:, k, hs],
                                                          start=(k == 0), stop=(k == 7)), reads=["m_hnT", "pgate"], writes=["ps%d" % pgb])
                        s.op("act", lambda e: e.activation(out=sig[half][:], in_=X.ps[pgb][:], func=AF.Sigmoid),
                             reads=["ps%d" % pgb], writes=["sig%d" % half])
                        s.op("dve", lambda e: e.tensor_tensor(out=acc[:, tt, hs], in0=X.ps[6][:], in1=sig[half][:], op=ALU.mult),
                             reads=["ps6", "sig%d" % half], writes=["dacc%d" % tt])
                    s.op("pool", lambda e: e.tensor_tensor(out=h1[:, tt, :], in0=h1[:, tt, :], in1=acc[:, tt, :], op=ALU.add),
                         reads=["dacc%d" % tt], writes=["h1_%d" % tt])
                    if last:
                        ln_a(s, X, h1[:, tt, :], "h1_%d" % tt, tt)
                        s.op("act", lambda e: e.activation(out=acc[:, tt, :], in_=h1[:, tt, :], func=AF.Copy, scale=X.ss[tt][:, 2:3]),
                             reads=["h1_%d" % tt, "ss%d" % tt], writes=["dacc%d" % tt])
                        s.op("pool", lambda e: e.tensor_tensor(out=acc[:, tt, :], in0=acc[:, tt, :], in1=gfin[:], op=ALU.mult),
                             reads=["gfin"], writes=["dacc%d" % tt])
                        s.dma("pool", h_out[ti * 128:(ti + 1) * 128, :], acc[:, tt, :], reads=["dacc%d" % tt], writes=["HOUT"])
                    else:
                        s.dma("pool", h_out[ti * 128:(ti + 1) * 128, :], h1[:, tt, :], reads=["h1_%d" % tt], writes=["HB"])
            s.barrier()
        s.barrier()
```

```python
import math
import os
from contextlib import ExitStack

import numpy as np
import concourse.bass as bass
import concourse.mybir as mybir
from concourse.bass_utils import run_bass_kernel_spmd

F32 = mybir.dt.float32
BF16 = mybir.dt.bfloat16
AF = mybir.ActivationFunctionType
ALU = mybir.AluOpType
AX = mybir.AxisListType

D = 1024
SEQ = 8192
NT = SEQ // 128
DEPTH = 2
HD = 64
IN_COLS = 3352
D_FF = 3584
NE = 8
EPS = 1e-6
NEG = -30000.0
NGRP = 40
NSLOT = NGRP * 512
I32 = mybir.dt.int32

C_QN, C_KC, C_VC, C_KS, C_VS, C_KW, C_VW, C_GL, C_QR, C_KR, C_VR, C_GR = (
    0, 512, 640, 768, 896, 1024, 1152, 1280, 1304, 1816, 2328, 2840)


class S:
    def __init__(self, nc, es, n_dma_sems=24):
        self.nc = nc
        self.eng = {"pe": nc.tensor, "dve": nc.vector, "act": nc.scalar,
                    "pool": nc.gpsimd, "sp": nc.sync}
        self.sem = {k: es.enter_context(nc.semaphore("c_" + k)) for k in self.eng}
        self.cnt = {k: 0 for k in self.eng}
        self.seen = {k: {} for k in self.eng}
        self.drained = {k: 0 for k in self.eng}
        self.dsem = [es.enter_context(nc.semaphore("d%d" % i)) for i in range(n_dma_sems)]
        self.dval = [0] * n_dma_sems
        self.dnext = 0
        self.lastw = {}
        self.readers = {}
        self.nops = 0

    def _wait(self, eng, tok):
        if tok is None:
            return
        if tok[0] == "c":
            _, e2, c = tok
            if e2 == eng:
                if eng != "pe" and c > self.drained[eng]:
                    self.eng[eng].drain()
                    self.drained[eng] = self.cnt[eng]
                return
            key = e2
            sem = self.sem[e2]
        else:
            _, si, c = tok
            key = ("d", si)
            sem = self.dsem[si]
        if self.seen[eng].get(key, 0) >= c:
            return
        self.eng[eng].wait_ge(sem, c)
        self.seen[eng][key] = c

    def _deps(self, eng, reads, writes):
        for r in reads:
            self._wait(eng, self.lastw.get(r))
        for w in writes:
            self._wait(eng, self.lastw.get(w))
            for t in self.readers.get(w, {}).values():
                self._wait(eng, t)

    def _commit(self, tok, reads, writes):
        for r in reads:
            self.readers.setdefault(r, {})[tok[1] if tok[0] == "c" else ("d", tok[1])] = tok
        for w in writes:
            self.lastw[w] = tok
            self.readers[w] = {}

    def op(self, eng, fn, reads=(), writes=()):
        self._deps(eng, reads, writes)
        ins = fn(self.eng[eng])
        self.cnt[eng] += 1
        ins.then_inc(self.sem[eng], 1)
        self._commit(("c", eng, self.cnt[eng]), reads, writes)
        self.nops += 1

    def dma(self, q, out, in_, reads=(), writes=(), **kw):
        self._deps(q, reads, writes)
        si = self.dnext
        self.dnext = (self.dnext + 1) % len(self.dsem)
        self._wait(q, ("d", si, self.dval[si]))
        ins = self.eng[q].dma_start(out=out, in_=in_, **kw)
        self.dval[si] += 16
        ins.then_inc(self.dsem[si], 16)
        self._commit(("d", si, self.dval[si]), reads, writes)
        self.nops += 1

    def idma(self, out, out_off, in_, in_off, bound, reads=(), writes=()):
        q = "pool"
        self._deps(q, reads, writes)
        si = self.dnext
        self.dnext = (self.dnext + 1) % len(self.dsem)
        self._wait(q, ("d", si, self.dval[si]))
        oo = None if out_off is None else bass.IndirectOffsetOnAxis(ap=out_off, axis=0)
        io = None if in_off is None else bass.IndirectOffsetOnAxis(ap=in_off, axis=0)
        ins = self.nc.gpsimd.indirect_dma_start(out=out, out_offset=oo, in_=in_, in_offset=io)
        self.dval[si] += 16
        ins.then_inc(self.dsem[si], 16)
        self._commit(("d", si, self.dval[si]), reads, writes)
        self.nops += 1

    def barrier(self):
        for e in self.eng:
            for e2 in self.eng:
                if e2 != e and self.cnt[e2] > 0:
                    self._wait(e, ("c", e2, self.cnt[e2]))
            for si in range(len(self.dsem)):
                if self.dval[si] > 0:
                    self._wait(e, ("d", si, self.dval[si]))
        self.lastw = {}
        self.readers = {}

    def drain(self, eng="sp"):
        for r, t in list(self.lastw.items()):
            self._wait(eng, t)
        for r, d in list(self.readers.items()):
            for t in d.values():
                self._wait(eng, t)


def bcast_mid(ap2, n):
    return ap2.unsqueeze(2).to_broadcast([ap2.shape[0], ap2.shape[1], n])


def bcast_rep(ap2, n):
    return ap2.unsqueeze(1).to_broadcast([ap2.shape[0], n, ap2.shape[1]])


def make_consts():
    c = {}
    c["c_ident"] = np.eye(128, dtype=np.float32)
    t = np.arange(SEQ)
    slopes = 2.0 ** (-(np.arange(8) + 1.0))
    qa = np.zeros((8, 3, SEQ), np.float32)
    for h in range(8):
        qa[h, 0] = 128.0 * slopes[h]
        qa[h, 1] = slopes[h]
        qa[h, 2] = -slopes[h] * 128.0 * (t // 128 + 1)
    c["c_qaug"] = qa
    ka = np.zeros((3, SEQ), np.float32)
    ka[0] = t // 128
    ka[1] = t % 128
    ka[2] = 1.0
    c["c_kaug"] = ka
    n = np.arange(512)
    pos = 16 * n + 31
    kc = np.zeros((3, 512), np.float32)
    kc[0] = pos // 128
    kc[1] = pos % 128
    kc[2] = 1.0
    c["c_kcaug"] = kc
    hh = np.arange(8)
    lg = np.log1p(-(2.0 ** (-5.0 - hh)))
    nn = np.arange(128)
    xiT = np.zeros((4, 128, 128), np.float32)
    kdT = np.zeros((4, 128, 128), np.float32)
    gC = np.zeros((128, 4), np.float32)
    for hp in range(4):
        for half in range(2):
            h = 2 * hp + half
            xiT[hp, half * 64:(half + 1) * 64, :] = np.exp((nn + 1.0) * lg[h])[None, :]
            kdT[hp, half * 64:(half + 1) * 64, :] = 0.125 * np.exp(-(nn + 1.0) * lg[h])[None, :]
            gC[half * 64:(half + 1) * 64, hp] = np.exp(128.0 * lg[h])
    c["c_xiT"] = xiT
    c["c_kdT"] = kdT
    c["c_gC"] = gC
    c["c_gC8"] = np.tile(np.exp(128.0 * lg)[None, :], (64, 1)).astype(np.float32)
    c["c_ktab"] = (0.125 * np.exp(-(nn[:, None] + 1.0) * lg[None, :])).astype(np.float32)
    dm = (nn[None, :] >= nn[:, None]).astype(np.float32)
    c["c_rmask"] = np.tile(dm, (1, 4))
    jj = nn[:, None]
    ii = nn[None, :]
    c["c_mcausal"] = np.tile(np.where(jj > ii, NEG, 0.0).astype(np.float32), (1, 4))
    c["c_mwin4"] = np.tile(np.where(jj > ii, 0.0, NEG).astype(np.float32), (1, 4))
    pats = []
    pidx = {}
    cmp_plan = []
    for qi in range(NT):
        row = []
        for cc in range(4):
            tq = 128 * qi + ii
            nk = 128 * cc + jj
            valid = (tq - (16 * nk + 31) >= 0) & (nk <= 510)
            if valid.all():
                row.append(-1)
            elif not valid.any():
                row.append(-2)
            else:
                key = valid.tobytes()
                if key not in pidx:
                    pidx[key] = len(pats)
                    pats.append(np.tile(np.where(valid, 0.0, NEG).astype(np.float32), (1, 4)))
                row.append(pidx[key])
        cmp_plan.append(row)
    c["c_mcmp"] = np.stack(pats, 0)
    E = np.zeros((128, NT, 128), np.float32)
    for kt in range(NT):
        E[2 * kt, kt, 0:64] = 1.0
        E[2 * kt + 1, kt, 64:128] = 1.0
    c["c_E"] = E
    ncmp = np.arange(512)[:, None]
    msel = np.arange(128)[None, :]
    ov = ((np.minimum(16 * ncmp + 32, 64 * msel + 64) > np.maximum(16 * ncmp, 64 * msel))
          & (ncmp <= 510)).astype(np.float32)
    c["c_ov"] = ov
    AB = np.zeros((NT, 128, 256), np.float32)
    for qi in range(NT):
        tq = 128 * qi + nn[:, None]
        back = tq // 64 - msel
        forced = (msel == 0) | ((back >= 0) & (back < 2))
        A = ((back >= 0) & (~forced)).astype(np.float32)
        B = np.where(forced, 1.0e9 + 1024.0 * msel, np.where(back >= 0, 0.0, -1.0)).astype(np.float32)
        AB[qi, :, 0:128] = A
        AB[qi, :, 128:256] = B
    c["c_AB"] = AB
    c["c_gi512"] = np.tile((512.0 * np.arange(NGRP))[None, :], (128, 1)).astype(np.float32)
    c["c_tokid"] = (np.arange(NT)[None, :] * 128 + np.arange(128)[:, None]).astype(np.float32)
    c["c_U"] = (nn[:, None] < nn[None, :]).astype(np.float32)
    c["c_blkp"] = (np.arange(7)[None, :] * 128 + np.arange(128)[:, None]).astype(np.float32)
    return c, cmp_plan


_CONSTS = None


def get_consts():
    global _CONSTS
    if _CONSTS is None:
        _CONSTS = make_consts()
    return _CONSTS


class Ctx:
    def __getattr__(self, k):
        if k in WEIGHT_SPECS:
            ap = self.nc.dram_tensor(k, WEIGHT_SPECS[k], F32, kind="ExternalInput").ap()
            self.__dict__[k] = ap
            self.used.append(k)
            return ap
        raise AttributeError(k)


def ln_a(s, X, src, src_res, k):
    junk, ss, xs = X.junk, X.ss[k], X.xs[k]
    s.op("act", lambda e: e.activation(out=junk[:], in_=src, func=AF.Square, accum_out=ss[:, 0:1]),
         reads=[src_res], writes=["junk", "ss%d" % k])
    s.op("dve", lambda e: e.tensor_scalar(out=ss[:, 1:2], in0=ss[:, 0:1], scalar1=1.0 / D, scalar2=EPS,
                                          op0=ALU.mult, op1=ALU.add), reads=["ss%d" % k], writes=["ss%d" % k])
    s.op("act", lambda e: e.activation(out=ss[:, 1:2], in_=ss[:, 1:2], func=AF.Sqrt),
         reads=["ss%d" % k], writes=["ss%d" % k])
    s.op("dve", lambda e: e.reciprocal(out=ss[:, 2:3], in_=ss[:, 1:2]), reads=["ss%d" % k], writes=["ss%d" % k])
    s.op("act", lambda e: e.activation(out=xs[:], in_=src, func=AF.Copy, scale=ss[:, 2:3]),
         reads=[src_res, "ss%d" % k], writes=["xs%d" % k])


def ln_b(s, X, k, gT, gT_res, dst, dst_res, src=None, src_res=None):
    xs = X.xs[k] if src is None else src
    xres = ("xs%d" % k) if src is None else src_res
    psb = X.ps[7][:].bitcast(BF16)
    for kc in range(8):
        s.op("pe", lambda e: e.transpose(out=psb[:, kc * 128:(kc + 1) * 128], in_=xs[:, kc * 128:(kc + 1) * 128],
                                         identity=X.ident[:]), reads=[xres, "ident"], writes=["ps7"])
    s.op("dve", lambda e: e.tensor_tensor(out=dst, in0=psb.rearrange("p (k t) -> p k t", k=8),
                                          in1=bcast_mid(gT, 128), op=ALU.mult),
         reads=["ps7", gT_res], writes=[dst_res])


def phase_proj(s, X, L, h_in):
    nc = X.nc
    with ExitStack() as es:
        sb = lambda name, shape, dt: es.enter_context(nc.sbuf_tensor("%s_L%d" % (name, L), shape, dt))
        wsb = sb("p1_w", [128, 8, IN_COLS], BF16)
        gT = sb("p1_gT", [128, 8], F32)
        ht = [sb("p1_ht%d" % i, [128, D], F32) for i in range(4)]
        hnT = [sb("p1_hnT%d" % i, [128, 8, 512], BF16) for i in range(2)]
        fm = [sb("p1_fm%d" % i, [128, 16, 512], BF16) for i in range(2)]
        vsx = [sb("p1_vsx%d" % i, [128, 4, 2, 65], BF16) for i in range(2)]
        vwx = [sb("p1_vwx%d" % i, [128, 4, 2, 65], BF16) for i in range(2)]
        gls = [sb("p1_gls%d" % i, [128, 4, 24], F32) for i in range(2)]
        krm = [sb("p1_krm%d" % i, [128, 4, 512], BF16) for i in range(2)]
        vrm = [sb("p1_vrm%d" % i, [128, 4, 512], BF16) for i in range(2)]
        grm = [sb("p1_grm%d" % i, [128, 4, 512], BF16) for i in range(2)]
        sg = sb("p1_sg", [128, 512], F32)
        xiT = sb("p1_xiT", [128, 4, 128], F32)
        kdT = sb("p1_kdT", [128, 4, 128], F32)
        ktab = sb("p1_ktab", [128, 8], F32)
        s.dma("sp", xiT[:], X.c["c_xiT"].rearrange("j p t -> p j t"), writes=["xiT"])
        s.dma("sp", kdT[:], X.c["c_kdT"].rearrange("j p t -> p j t"), writes=["kdT"])
        s.dma("sp", ktab[:], X.c["c_ktab"], writes=["ktab"])
        s.dma("sp", gT[:], X.g_mix[L].rearrange("(k p) -> p k", p=128), writes=["p1gT"],
              allow_slow_non_contiguous=True)
        for kc in range(8):
            s.dma("pool", wsb[:, kc, :], X.w_in[L, kc * 128:(kc + 1) * 128, :], writes=["p1w"])
        for i in range(2):
            s.op("pool", lambda e: e.memset(vsx[i][:, :, :, 64:65], 1.0), writes=["vsx%d" % i])
            s.op("pool", lambda e: e.memset(vwx[i][:, :, :, 64:65], 1.0), writes=["vwx%d" % i])
        fm_cols = [C_QN, C_QN + 128, C_QN + 256, C_QN + 384, C_KC, C_VC, C_KS, C_KW,
                   C_QR, C_QR + 128, C_QR + 256, C_QR + 384, C_KR, C_KR + 128, C_KR + 256, C_KR + 384]
        def emit_a(tg):
            for tt in range(4):
                ti = 4 * tg + tt
                s.dma("sp", ht[tt][:], h_in[ti * 128:(ti + 1) * 128, :], writes=["p1ht%d" % tt])
                ln_a(s, X, ht[tt][:], "p1ht%d" % tt, tt)

        def emit_b(tg):
            for tt in range(4):
                ln_b(s, X, tt, gT[:], "p1gT", hnT[tg % 2][:, :, tt * 128:(tt + 1) * 128], "hnT%d" % (tg % 2))

        emit_a(0)
        emit_b(0)
        for tg in range(NT // 4):
            b = tg % 2
            toks = slice(tg * 512, (tg + 1) * 512)
            if tg + 1 < NT // 4:
                emit_a(tg + 1)
            for ci, c0 in enumerate(fm_cols):
                pb = ci % 2
                ps = X.ps[pb]
                for kc in range(8):
                    s.op("pe", lambda e: e.matmul(ps[:], lhsT=wsb[:, kc, c0:c0 + 128], rhs=hnT[b][:, kc, :],
                                                  start=(kc == 0), stop=(kc == 7)),
                         reads=["p1w", "hnT%d" % b], writes=["ps%d" % pb])
                dst = fm[b][:, ci, :]
                if ci < 4:
                    s.op("act", lambda e: e.activation(out=dst, in_=ps[:], func=AF.Copy, scale=0.125),
                         reads=["ps%d" % pb], writes=["fm%d" % b])
                elif ci < 8:
                    eng = "act" if ci % 2 == 0 else "dve"
                    if eng == "act":
                        s.op("act", lambda e: e.activation(out=dst, in_=ps[:], func=AF.Copy),
                             reads=["ps%d" % pb], writes=["fm%d" % b])
                    else:
                        s.op("dve", lambda e: e.tensor_copy(out=dst, in_=ps[:]),
                             reads=["ps%d" % pb], writes=["fm%d" % b])
                else:
                    tab = xiT if ci < 12 else kdT
                    j = (ci - 8) % 4
                    s.op("dve", lambda e: e.tensor_tensor(
                        out=dst.rearrange("p (a t) -> p a t", a=4), in0=ps[:].rearrange("p (a t) -> p a t", a=4),
                        in1=bcast_rep(tab[:, j, :], 4), op=ALU.mult),
                        reads=["ps%d" % pb, "xiT", "kdT"], writes=["fm%d" % b])
            for tt in range(4):
                lhs = lambda kc: hnT[b][:, kc, tt * 128:(tt + 1) * 128]
                groups = [(2, C_VS, 408), (3, C_KR, 512), (4, C_VR, 512), (5, C_GR, 512)]
                for (pi, c0, n) in groups:
                    for kc in range(8):
                        s.op("pe", lambda e: e.matmul(X.ps[pi][:, 0:n], lhsT=lhs(kc), rhs=wsb[:, kc, c0:c0 + n],
                                                      start=(kc == 0), stop=(kc == 7)),
                             reads=["p1w", "hnT%d" % b], writes=["ps%d" % pi])
                pA = X.ps[2]
                s.op("dve", lambda e: e.tensor_copy(out=vsx[b][:, tt, :, 0:64],
                                                    in_=pA[:, 0:128].rearrange("p (g d) -> p g d", g=2)),
                     reads=["ps2"], writes=["vsx%d" % b])
                s.op("dve", lambda e: e.tensor_copy(out=vwx[b][:, tt, :, 0:64],
                                                    in_=pA[:, 256:384].rearrange("p (g d) -> p g d", g=2)),
                     reads=["ps2"], writes=["vwx%d" % b])
                s.op("act", lambda e: e.activation(out=gls[b][:, tt, :], in_=pA[:, 384:408], func=AF.Sigmoid),
                     reads=["ps2"], writes=["gls%d" % b])
                s.op("dve", lambda e: e.tensor_tensor(
                    out=krm[b][:, tt, :].rearrange("p (h d) -> p h d", h=8),
                    in0=X.ps[3][:].rearrange("p (h d) -> p h d", h=8),
                    in1=bcast_mid(ktab[:], 64), op=ALU.mult),
                    reads=["ps3", "ktab"], writes=["krm%d" % b])
                s.op("act", lambda e: e.activation(out=vrm[b][:, tt, :], in_=X.ps[4][:], func=AF.Copy),
                     reads=["ps4"], writes=["vrm%d" % b])
                s.op("act", lambda e: e.activation(out=sg[:], in_=X.ps[5][:], func=AF.Sigmoid),
                     reads=["ps5"], writes=["p1sg"])
                s.op("dve", lambda e: e.tensor_tensor(out=grm[b][:, tt, :], in0=X.ps[5][:], in1=sg[:], op=ALU.mult),
                     reads=["ps5", "p1sg"], writes=["grm%d" % b])
            if tg + 1 < NT // 4:
                emit_b(tg + 1)
            s.dma("pool", X.QTP[:, :, toks].rearrange("j p t -> p j t"), fm[b][:, 0:4, :], reads=["fm%d" % b], writes=["QTP"])
            s.dma("pool", X.KCP[:, toks], fm[b][:, 4, :], reads=["fm%d" % b], writes=["KCP"])
            s.dma("pool", X.VCP[:, toks], fm[b][:, 5, :], reads=["fm%d" % b], writes=["VCP"])
            s.dma("pool", X.KSP[:, toks], fm[b][:, 6, :], reads=["fm%d" % b], writes=["KSP"])
            s.dma("pool", X.KWP[:, toks], fm[b][:, 7, :], reads=["fm%d" % b], writes=["KWP"])
            s.dma("pool", X.QRT[:, :, toks].rearrange("j p t -> p j t"), fm[b][:, 8:12, :], reads=["fm%d" % b], writes=["QRT"])
            s.dma("pool", X.KRT[:, :, toks].rearrange("j p t -> p j t"), fm[b][:, 12:16, :], reads=["fm%d" % b], writes=["KRT"])
            s.dma("pool", X.VSX[toks].rearrange("(tt p) g c -> p tt g c", p=128), vsx[b][:], reads=["vsx%d" % b], writes=["VSX"])
            s.dma("pool", X.VWX[toks].rearrange("(tt p) g c -> p tt g c", p=128), vwx[b][:], reads=["vwx%d" % b], writes=["VWX"])
            s.dma("pool", X.GLS[toks].rearrange("(tt p) c -> p tt c", p=128), gls[b][:], reads=["gls%d" % b], writes=["GLS"])
            s.dma("pool", X.KRM[toks].rearrange("(tt p) c -> p tt c", p=128), krm[b][:], reads=["krm%d" % b], writes=["KRM"])
            s.dma("pool", X.VRM[toks].rearrange("(tt p) c -> p tt c", p=128), vrm[b][:], reads=["vrm%d" % b], writes=["VRM"])
            s.dma("pool", X.GRM[toks].rearrange("(tt p) c -> p tt c", p=128), grm[b][:], reads=["grm%d" % b], writes=["GRM"])
        s.barrier()


WEIGHT_SPECS = {
    "w_in": [DEPTH, D, IN_COLS], "w_out": [DEPTH, D, D], "g_mix": [DEPTH, D], "g_ffn": [DEPTH, D],
    "g_ple": [DEPTH, D], "g_final": [D], "cmp_pos": [DEPTH, 2, 32, 64], "cmp_w1": [DEPTH, 2, 32, 64, 256],
    "cmp_w2": [DEPTH, 2, 256, 64], "ret_gn": [DEPTH, 512], "ffn_gate": [1, D, D_FF], "ffn_up": [1, D, D_FF],
    "ffn_down": [1, D_FF, D], "moe_router": [1, D, NE], "moe_gate": [1, NE, D, D_FF], "moe_up": [1, NE, D, D_FF],
    "moe_down": [1, NE, D_FF, D], "ple_proj": [DEPTH, 256, D], "ple_gate": [DEPTH, D, D],
}

SCRATCH_SPECS = {
    "QTP": ([4, 128, SEQ], BF16), "QAUG": ([8, 3, SEQ], BF16), "KAUG": ([3, SEQ], BF16), "KCAUG": ([3, 512], BF16),
    "KSP": ([128, SEQ], BF16), "KWP": ([128, SEQ], BF16), "KCP": ([128, SEQ], BF16), "VCP": ([128, SEQ], BF16),
    "VSX": ([SEQ, 2, 65], BF16), "VWX": ([SEQ, 2, 65], BF16), "GLS": ([SEQ, 24], F32),
    "QRT": ([4, 128, SEQ], BF16), "KRT": ([4, 128, SEQ], BF16),
    "KRM": ([SEQ, 512], BF16), "VRM": ([SEQ, 512], BF16), "GRM": ([SEQ, 512], BF16),
    "AT": ([D, SEQ], BF16), "HB": ([SEQ, D], F32),
    "FG16": ([1, D, D_FF], BF16), "FU16": ([1, D, D_FF], BF16), "FD16": ([1, D_FF, D], BF16),
    "WGB": ([NE * 7 * 128, 4096], BF16), "WUB": ([NE * 7 * 128, 4096], BF16), "WDB": ([NE * 7 * 128, 4096], BF16),
    "H1": ([SEQ, D], F32), "XS2": ([SEQ, D], BF16), "SLOT": ([NSLOT, 2], I32), "YS": ([NSLOT, D], F32),
    "DBG_KCMP": ([67, 2, 512], BF16), "DBG_VCX": ([128, 2, 4, 193], BF16),
}


def build(stop_after=None, debug=(), nsa_tiles=NT):
    nc = bass.Bass("TRN2", target_bir_lowering=False)
    X = Ctx()
    X.nc = nc
    consts, cmp_plan = get_consts()
    X.cmp_plan = cmp_plan
    X.x = nc.dram_tensor("x", [SEQ, D], F32, kind="ExternalInput").ap()
    X.p = nc.dram_tensor("p", [DEPTH, SEQ, 256], F32, kind="ExternalInput").ap()
    X.used = []
    X.dbg_cmp = "DBG_KCMP" in debug
    X.nsa_tiles = nsa_tiles
    X.routed = True
    X.conv_layers = [0] if (stop_after is not None and stop_after[0] == 0) else [0, 1]
    X.ret_chunks = int(os.environ.get('RET_CHUNKS', NT))
    X.c = {k: nc.dram_tensor(k, list(v.shape), F32, kind="ExternalInput").ap() for k, v in consts.items()}
    X.out = nc.dram_tensor("out", [SEQ, D], F32, kind="ExternalOutput").ap()
    for k, (shp, dt) in SCRATCH_SPECS.items():
        kind = "ExternalOutput" if k in debug else "Internal"
        setattr(X, k, nc.dram_tensor(k, shp, dt, kind=kind).ap())
    with ExitStack() as es:
        s = S(nc, es)
        X.ps = [es.enter_context(nc.psum_tensor("ps%d" % i, [128, 512], F32)) for i in range(8)]
        X.ident = es.enter_context(nc.sbuf_tensor("ident", [128, 128], BF16))
        X.junk = es.enter_context(nc.sbuf_tensor("junk", [128, D], F32))
        X.ss = [es.enter_context(nc.sbuf_tensor("ss%d" % i, [128, 4], F32)) for i in range(8)]
        X.xs = [es.enter_context(nc.sbuf_tensor("xs%d" % i, [128, D], BF16)) for i in range(8)]
        s.dma("pool", X.ident[:], X.c["c_ident"], writes=["ident"])
        s.dma("pool", X.QAUG, X.c["c_qaug"], writes=["QAUG"])
        s.dma("pool", X.KAUG, X.c["c_kaug"], writes=["KAUG"])
        s.dma("pool", X.KCAUG, X.c["c_kcaug"], writes=["KCAUG"])
        done = False
        for L in range(DEPTH):
            h_in = X.x if L == 0 else X.HB
            for name, fn in PHASES:
                fn(s, X, L, h_in)

                if stop_after == (L, name):
                    done = True
                    break
            if done:
                break
        s.barrier()
        s.drain("sp")
    X.nops = s.nops
    return nc, X


PHASES = [("proj", phase_proj)]

_BUILT = {}


def run(inputs, stop_after=None, debug=(), cores=8, trace=False, nsa_tiles=NT):
    key = (stop_after, tuple(debug), nsa_tiles)
    if key not in _BUILT:
        _BUILT[key] = build(stop_after, debug, nsa_tiles)
    nc, X = _BUILT[key]
    consts, _ = get_consts()
    in_maps = []
    for b in range(cores):
        m = {"x": np.ascontiguousarray(inputs["x"][b]), "p": np.ascontiguousarray(inputs["p"][:, b])}
        for k in X.used:
            m[k] = np.ascontiguousarray(inputs[k])
        m.update(consts)
        in_maps.append(m)
    return run_bass_kernel_spmd(nc, in_maps, core_ids=list(range(cores)), trace=trace)


def kernel(**inputs):
    inputs = {k: np.asarray(v) for k, v in inputs.items()}
    res = run(inputs)
    return np.stack([r["out"] for r in res.results], 0).astype(np.float32)


def phase_nsa(s, X, L, h_in):
    nc = X.nc
    with ExitStack() as es:
        sb = lambda name, shape, dt: es.enter_context(nc.sbuf_tensor("%s_L%d" % (name, L), shape, dt))
        KCMP = sb("n_kcmp", [67, 2, 512], BF16)
        VCX = sb("n_vcx", [128, 2, 4, 193], BF16)
        s.op("pool", lambda e: e.memset(KCMP[:], 0.0), writes=["KCMP"])
        s.op("pool", lambda e: e.memset(VCX[:], 0.0), writes=["VCX"])
        s.op("pool", lambda e: e.memset(VCX[:, :, :, 64:65], 1.0), writes=["VCX"])
        for g in range(2):
            s.dma("pool", VCX[:, g, :, 65:193], X.c["c_ov"].rearrange("(ct p) m -> p ct m", p=128), writes=["VCX"])
            s.dma("sp", KCMP[64:67, g, :], X.KCAUG, reads=["KCAUG"], writes=["KCMP"])
        with ExitStack() as es2:
            sb2 = lambda name, shape, dt: es2.enter_context(nc.sbuf_tensor("%s_L%d" % (name, L), shape, dt))
            kcT = [sb2("c_kcT%d" % i, [64, SEQ], BF16) for i in range(2)]
            w1 = sb2("c_w1", [64, 32, 256], BF16)
            posT = sb2("c_posT", [64, 32], BF16)
            w2 = sb2("c_w2", [128, 2, 64], BF16)
            hb = sb2("c_hb", [128, 2], F32)
            u = sb2("c_u", [128, 2, 512], F32)
            t1 = sb2("c_t1", [128, 2, 512], F32)
            sg = sb2("c_sg", [128, 2, 512], F32)
            gel = sb2("c_gel", [128, 2, 512], BF16)
            it = 0
            for kv in range(2):
                s.dma("pool", w1[:], X.cmp_w1[L, kv].rearrange("l d h -> d l h"), writes=["c_w1"])
                s.dma("pool", posT[:], X.cmp_pos[L, kv].rearrange("l d -> d l"), writes=["c_posT"],
                      allow_slow_non_contiguous=True)
                s.dma("pool", w2[:], X.cmp_w2[L, kv].rearrange("(c p) d -> p c d", p=128), writes=["c_w2"])
                src = X.KCP if kv == 0 else X.VCP
                for g in range(2):
                    kt_ = kcT[it % 2]
                    kres = "c_kcT%d" % (it % 2)
                    it += 1
                    s.dma("sp", kt_[:], src[g * 64:(g + 1) * 64, :], reads=["KCP", "VCP"], writes=[kres])
                    kview = kt_[:].rearrange("d (n c) -> d n c", c=16)
                    for hc in range(2):
                        ps = X.ps[hc]
                        for l in range(32):
                            a, c_ = l // 16, l % 16
                            s.op("pe", lambda e: e.matmul(ps[:, 0:511], lhsT=w1[:, l, hc * 128:(hc + 1) * 128],
                                                          rhs=kview[:, a:a + 511, c_], start=(l == 0), stop=(l == 31)),
                                 reads=["c_w1", kres], writes=["ps%d" % hc])
                        for l in range(32):
                            s.op("pe", lambda e: e.matmul(X.ps[2][:, hc:hc + 1], lhsT=w1[:, l, hc * 128:(hc + 1) * 128],
                                                          rhs=posT[:, l:l + 1], start=(l == 0), stop=(l == 31)),
                                 reads=["c_w1", "c_posT"], writes=["ps2"])
                        s.op("dve", lambda e: e.tensor_copy(out=hb[:, hc:hc + 1], in_=X.ps[2][:, hc:hc + 1]),
                             reads=["ps2"], writes=["c_hb"])
                        s.op("act", lambda e: e.activation(out=u[:, hc, 0:511], in_=ps[:, 0:511], func=AF.Identity,
                                                           bias=hb[:, hc:hc + 1]),
                             reads=["ps%d" % hc, "c_hb"], writes=["c_u"])
                    uu = u[:, :, 0:511]
                    s.op("pool", lambda e: e.tensor_tensor(out=t1[:, :, 0:511], in0=uu, in1=uu, op=ALU.mult),
                         reads=["c_u"], writes=["c_t1"])
                    s.op("dve", lambda e: e.tensor_scalar(out=t1[:, :, 0:511], in0=t1[:, :, 0:511], scalar1=0.044715,
                                                          scalar2=1.0, op0=ALU.mult, op1=ALU.add),
                         reads=["c_t1"], writes=["c_t1"])
                    s.op("dve", lambda e: e.tensor_tensor(out=t1[:, :, 0:511], in0=t1[:, :, 0:511], in1=uu, op=ALU.mult),
                         reads=["c_t1", "c_u"], writes=["c_t1"])
                    s.op("act", lambda e: e.activation(out=sg[:, :, 0:511], in_=t1[:, :, 0:511], func=AF.Sigmoid,
                                                       scale=1.5957691216057308),
                         reads=["c_t1"], writes=["c_sg"])
                    s.op("dve", lambda e: e.tensor_tensor(out=gel[:, :, 0:511], in0=sg[:, :, 0:511], in1=uu, op=ALU.mult),
                         reads=["c_sg", "c_u"], writes=["c_gel"])
                    if kv == 0:
                        for hc in range(2):
                            s.op("pe", lambda e: e.matmul(X.ps[3][0:64, 0:511], lhsT=w2[:, hc, :], rhs=gel[:, hc, 0:511],
                                                          start=(hc == 0), stop=(hc == 1)),
                                 reads=["c_w2", "c_gel"], writes=["ps3"])
                        s.op("act", lambda e: e.activation(out=KCMP[0:64, g, 0:511], in_=X.ps[3][0:64, 0:511], func=AF.Copy),
                             reads=["ps3"], writes=["KCMP"])
                    else:
                        for ct in range(4):
                            nn = 128 if ct < 3 else 127
                            for hc in range(2):
                                s.op("pe", lambda e: e.matmul(X.ps[3][0:nn, ct * 64:(ct + 1) * 64],
                                                              lhsT=gel[:, hc, ct * 128:ct * 128 + nn], rhs=w2[:, hc, :],
                                                              start=(hc == 0), stop=(hc == 1)),
                                     reads=["c_w2", "c_gel"], writes=["ps3"])
                        for ct in range(4):
                            nn = 128 if ct < 3 else 127
                            s.op("act", lambda e: e.activation(out=VCX[0:nn, g, ct, 0:64],
                                                               in_=X.ps[3][0:nn, ct * 64:(ct + 1) * 64], func=AF.Copy),
                                 reads=["ps3"], writes=["VCX"])
            s.barrier()
        if X.dbg_cmp:
            s.dma("sp", X.DBG_KCMP, KCMP[:], reads=["KCMP"])
            s.dma("sp", X.DBG_VCX, VCX[:], reads=["VCX"])
        KS = sb("n_ks", [67, 2, SEQ], BF16)
        KW = sb("n_kw", [67, 2, SEQ], BF16)
        VS = sb("n_vs", [128, NT, 2, 65], BF16)
        VW = sb("n_vw", [128, NT, 2, 65], BF16)
        E = sb("n_E", [128, NT, 128], BF16)
        mca = sb("n_mca", [128, 512], BF16)
        mw4 = sb("n_mw4", [128, 512], BF16)
        npat = X.c["c_mcmp"].shape[0]
        mcmp = sb("n_mcmp", [128, npat, 512], BF16)
        qt = [sb("n_qt%d" % i, [67, 1024], BF16) for i in range(2)]
        AB = [sb("n_AB%d" % i, [128, 256], F32) for i in range(2)]
        glt = [sb("n_gl%d" % i, [128, 24], F32) for i in range(2)]
        PT = [sb("n_PT%d" % i, [128, 512], BF16) for i in range(4)]
        negT = [sb("n_negT%d" % i, [128, 512], BF16) for i in range(2)]
        acc = [sb("n_acc%d" % i, [128, 512], F32) for i in range(2)]
        accb = [sb("n_accb%d" % i, [128, 512], BF16) for i in range(2)]
        aT = [sb("n_aT%d" % i, [128, 4, 128], BF16) for i in range(2)]
        sm = [sb("n_sm%d" % i, [128, 64], F32) for i in range(2)]
        sc = [sb("n_sc%d" % i, [128, 128], F32) for i in range(2)]
        sc2 = [sb("n_sc2%d" % i, [128, 128], F32) for i in range(2)]
        seln = [sb("n_seln%d" % i, [128, 128], BF16) for i in range(2)]
        s.dma("sp", KS[0:64, :, :], X.KSP.rearrange("(g d) t -> d g t", g=2), reads=["KSP"], writes=["KS"])
        s.dma("sp", KW[0:64, :, :], X.KWP.rearrange("(g d) t -> d g t", g=2), reads=["KWP"], writes=["KW"])
        for g in range(2):
            s.dma("sp", KS[64:67, g, :], X.KAUG, reads=["KAUG"], writes=["KS"])
            s.dma("sp", KW[64:67, g, :], X.KAUG, reads=["KAUG"], writes=["KW"])
        for q4 in range(4):
            tsl = slice(q4 * 2048, (q4 + 1) * 2048)
            ksl = slice(q4 * 16, (q4 + 1) * 16)
            s.dma("sp", VS[:, ksl], X.VSX[tsl].rearrange("(kt p) g c -> p kt g c", p=128), reads=["VSX"], writes=["VS"])
            s.dma("sp", VW[:, ksl], X.VWX[tsl].rearrange("(kt p) g c -> p kt g c", p=128), reads=["VWX"], writes=["VW"])
            s.dma("pool", E[:, ksl, :], X.c["c_E"][:, ksl, :], writes=["E"])
        s.dma("pool", mca[:], X.c["c_mcausal"], writes=["mca"])
        s.dma("pool", mw4[:], X.c["c_mwin4"], writes=["mw4"])
        s.dma("pool", mcmp[:], X.c["c_mcmp"].rearrange("n p c -> p n c"), writes=["mcmp"])
        st = {"sb": 0, "pt": 0}

        def qk_tile(g, qb, lhsT, lres, extra, mask):
            pi = st["sb"] % 3
            st["sb"] += 1
            ps = X.ps[pi]
            pres = "ps%d" % pi
            nmm = 1 + (extra is not None) + (mask is not None)
            s.op("pe", lambda e: e.matmul(ps[:], lhsT=lhsT, rhs=qt[qb][:, g * 512:(g + 1) * 512], start=True,
                                          stop=(nmm == 1)), reads=[lres, "qt%d" % qb], writes=[pres])
            k = 1
            if extra is not None:
                el, er, eres = extra
                s.op("pe", lambda e: e.matmul(ps[:], lhsT=el, rhs=er, start=False, stop=(k + 1 == nmm)),
                     reads=eres, writes=[pres])
                k += 1
            if mask is not None:
                ml, mres = mask
                s.op("pe", lambda e: e.matmul(ps[:], lhsT=X.ident[:], rhs=ml, start=False, stop=True),
                     reads=["ident", mres], writes=[pres])
            pk = st["pt"] % 4
            st["pt"] += 1
            s.op("act", lambda e: e.activation(out=PT[pk][:], in_=ps[:], func=AF.Exp),
                 reads=[pres], writes=["PT%d" % pk])
            return PT[pk], "PT%d" % pk

        def coef_and_acc(g, qb, branch, heads_ap, heads_res, first):
            smt = sm[g]
            sres = "sm%d" % g
            for r in range(4):
                o_ap = heads_ap(r)
                s.op("dve", lambda e: e.tensor_scalar(out=smt[:, r:r + 1], in0=o_ap[:, 64:65], scalar1=1e-36,
                                                      scalar2=None, op0=ALU.max),
                     reads=heads_res, writes=[sres])
            s.op("dve", lambda e: e.reciprocal(out=smt[:, 4:8], in_=smt[:, 0:4]), reads=[sres], writes=[sres])
            c0 = branch * 8 + g * 4
            s.op("dve", lambda e: e.tensor_tensor(out=smt[:, 8:12], in0=smt[:, 4:8], in1=glt[qb][:, c0:c0 + 4],
                                                  op=ALU.mult), reads=[sres, "gl%d" % qb], writes=[sres])
            for r in range(4):
                o_ap = heads_ap(r)
                dst = acc[qb][:, (g * 4 + r) * 64:(g * 4 + r + 1) * 64]
                if first:
                    s.op("dve", lambda e: e.tensor_scalar(out=dst, in0=o_ap[:, 0:64], scalar1=smt[:, 8 + r:9 + r],
                                                          scalar2=None, op0=ALU.mult),
                         reads=heads_res + [sres], writes=["acc%d" % qb])
                else:
                    s.op("dve", lambda e: e.scalar_tensor_tensor(out=dst, in0=o_ap[:, 0:64], scalar=smt[:, 8 + r:9 + r],
                                                                 in1=dst, op0=ALU.mult, op1=ALU.add),
                         reads=heads_res + [sres], writes=["acc%d" % qb])

        def emit_loads(qi):
            qb = qi % 2
            t0 = qi * 128
            for j in range(4):
                s.dma("sp", qt[qb][0:64, :].rearrange("d (j two t) -> d j two t", j=4, two=2)[:, j],
                      X.QTP[j, :, t0:t0 + 128].rearrange("(two d) t -> d two t", two=2),
                      reads=["QTP"], writes=["qt%d" % qb])
            s.dma("sp", qt[qb][64:67, :].rearrange("r (h t) -> r h t", h=8),
                  X.QAUG[:, :, t0:t0 + 128].rearrange("h r t -> r h t"), reads=["QAUG"], writes=["qt%d" % qb])
            s.dma("sp", AB[qb][:], X.c["c_AB"][qi], writes=["AB%d" % qb])
            s.dma("sp", glt[qb][:], X.GLS[t0:t0 + 128, :], reads=["GLS"], writes=["gl%d" % qb])

        def cmp_post_a(qi, g):
            qb = qi % 2
            ocb = lambda r: X.ps[3 + r // 2][:, (r % 2) * 193:(r % 2) * 193 + 193]
            coef_and_acc(g, qb, 0, ocb, ["ps3", "ps4"], True)
            smt = sm[g]
            sres = "sm%d" % g
            for r in range(4):
                if r == 0:
                    s.op("dve", lambda e: e.tensor_scalar(out=sc[g][:], in0=ocb(r)[:, 65:193], scalar1=smt[:, 4:5],
                                                          scalar2=None, op0=ALU.mult),
                         reads=["ps3", "ps4", sres], writes=["sc%d" % g])
                else:
                    s.op("dve", lambda e: e.scalar_tensor_tensor(out=sc[g][:], in0=ocb(r)[:, 65:193],
                                                                 scalar=smt[:, 4 + r:5 + r], in1=sc[g][:],
                                                                 op0=ALU.mult, op1=ALU.add),
                         reads=["ps3", "ps4", sres], writes=["sc%d" % g])
            s.op("dve", lambda e: e.tensor_tensor(out=sc[g][:], in0=sc[g][:], in1=AB[qb][:, 0:128], op=ALU.mult),
                 reads=["AB%d" % qb], writes=["sc%d" % g])
            s.op("dve", lambda e: e.tensor_tensor(out=sc[g][:], in0=sc[g][:], in1=AB[qb][:, 128:256], op=ALU.add),
                 reads=["AB%d" % qb], writes=["sc%d" % g])
            s.op("dve", lambda e: e.max(out=smt[:, 16:24], in_=sc[g][:]), reads=["sc%d" % g], writes=[sres])
            s.op("dve", lambda e: e.match_replace(out=sc2[g][:], in_to_replace=smt[:, 16:24], in_values=sc[g][:],
                                                  imm_value=-2.0), reads=["sc%d" % g, sres], writes=["sc2%d" % g])
            s.op("dve", lambda e: e.max(out=smt[:, 24:32], in_=sc2[g][:]), reads=["sc2%d" % g], writes=[sres])
            s.op("dve", lambda e: e.tensor_scalar(out=seln[g][:], in0=sc[g][:], scalar1=smt[:, 31:32], scalar2=NEG,
                                                  op0=ALU.is_lt, op1=ALU.mult),
                 reads=["sc%d" % g, sres], writes=["seln%d" % g])

        def cmp_post_b(qi, g):
            psb = X.ps[7][:].bitcast(BF16)
            s.op("pe", lambda e: e.transpose(out=psb[:, g * 128:(g + 1) * 128], in_=seln[g][:], identity=X.ident[:]),
                 reads=["seln%d" % g, "ident"], writes=["ps7"])
            s.op("dve", lambda e: e.tensor_copy(out=negT[g][:].rearrange("p (a t) -> p a t", a=4),
                                                in_=bcast_rep(psb[:, g * 128:(g + 1) * 128], 4)),
                 reads=["ps7"], writes=["negT%d" % g])

        def write_out(qi):
            qb = qi % 2
            t0 = qi * 128
            s.op("act", lambda e: e.activation(out=accb[qb][:], in_=acc[qb][:], func=AF.Copy),
                 reads=["acc%d" % qb], writes=["accb%d" % qb])
            psb = X.ps[7][:].bitcast(BF16)
            for j in range(4):
                s.op("pe", lambda e: e.transpose(out=psb[:, 256 + j * 128:256 + (j + 1) * 128],
                                                 in_=accb[qb][:, j * 128:(j + 1) * 128], identity=X.ident[:]),
                     reads=["accb%d" % qb, "ident"], writes=["ps7"])
            s.op("act", lambda e: e.activation(out=aT[qb][:].rearrange("p j t -> p (j t)"), in_=psb[:, 256:768], func=AF.Copy),
                 reads=["ps7"], writes=["aT%d" % qb])
            s.dma("sp", X.AT[0:512, t0:t0 + 128].rearrange("(j p) t -> p j t", p=128), aT[qb][:],
                  reads=["aT%d" % qb], writes=["AT"])

        def pv_fn(out_ap, out_res, rhs, rhs_res, first, lastt, pair_start):
            def f(pt, ptres):
                for r in range(4):
                    st_ = first and ((r % 2 == 0) if pair_start else (r == 0))
                    s.op("pe", lambda e: e.matmul(out_ap(r), lhsT=pt[:, r * 128:(r + 1) * 128], rhs=rhs,
                                                  start=st_, stop=lastt), reads=[ptres, rhs_res], writes=out_res(r))
            return f

        jobs = []
        for qi in range(X.nsa_tiles):
            qb = qi % 2
            first_job = len(jobs)
            plan = X.cmp_plan[qi]
            cs = [c for c in range(4) if plan[c] != -2]
            ocb = lambda r: X.ps[3 + r // 2][:, (r % 2) * 193:(r % 2) * 193 + 193]
            ocres = lambda r: ["ps%d" % (3 + r // 2)]
            for g in range(2):
                for ci, c in enumerate(cs):
                    mask = None if plan[c] == -1 else (mcmp[:, plan[c], :], "mcmp")
                    jobs.append(dict(
                        qk=(lambda g=g, qb=qb, c=c, mask=mask: qk_tile(g, qb, KCMP[:, g, c * 128:(c + 1) * 128], "KCMP", None, mask)),
                        pv=pv_fn(ocb, ocres, VCX[:, g, c, :], "VCX", ci == 0, ci == len(cs) - 1, True),
                        post=((lambda qi=qi, g=g: (cmp_post_a(qi, g), (cmp_post_b(qi, g) if qi < 2 else None)))
                              if ci == len(cs) - 1 else None)))
            for g in range(2):
                obr = (lambda g: (lambda r: X.ps[5 + g][:, r * 65:(r + 1) * 65]))(g)
                obres = (lambda g: (lambda r: ["ps%d" % (5 + g)]))(g)
                dl = list(range(min(4, qi), -1, -1))
                for di, dd in enumerate(dl):
                    kt = qi - dd
                    mask = (mca[:], "mca") if dd == 0 else ((mw4[:], "mw4") if dd == 4 else None)
                    posts = []
                    if di == 0 and qi >= 2:
                        posts.append(lambda qi=qi, g=g: cmp_post_b(qi, g))
                    if di == len(dl) - 1:
                        posts.append(lambda g=g, qb=qb, obr=obr: coef_and_acc(g, qb, 2, obr, ["ps%d" % (5 + g)], False))
                    jobs.append(dict(
                        qk=(lambda g=g, qb=qb, kt=kt, mask=mask: qk_tile(g, qb, KW[:, g, kt * 128:(kt + 1) * 128], "KW", None, mask)),
                        pv=pv_fn(obr, obres, VW[:, kt, g, :], "VW", di == 0, di == len(dl) - 1, False),
                        post=(lambda posts=posts: [p_() for p_ in posts])))
            for g in range(2):
                obr = (lambda g: (lambda r: X.ps[5 + g][:, r * 65:(r + 1) * 65]))(g)
                obres = (lambda g: (lambda r: ["ps%d" % (5 + g)]))(g)
                for kt in range(qi + 1):
                    mask = (mca[:], "mca") if kt == qi else None
                    posts = []
                    if kt == qi:
                        posts.append(lambda g=g, qb=qb, obr=obr: coef_and_acc(g, qb, 1, obr, ["ps%d" % (5 + g)], False))
                        if g == 1:
                            posts.append(lambda qi=qi: write_out(qi))
                    jobs.append(dict(
                        qk=(lambda g=g, qb=qb, kt=kt, mask=mask: qk_tile(
                            g, qb, KS[:, g, kt * 128:(kt + 1) * 128], "KS", (E[:, kt, :], negT[g][:], ["E", "negT%d" % g]), mask)),
                        pv=pv_fn(obr, obres, VS[:, kt, g, :], "VS", kt == 0, kt == qi, False),
                        post=(lambda posts=posts: [p_() for p_ in posts])))
            if qi + 1 < X.nsa_tiles:
                old = jobs[first_job]["post"]
                jobs[first_job]["post"] = (lambda old=old, qi=qi: ((old() if old else None), emit_loads(qi + 1)))

        LA = 2
        emit_loads(0)
        pend = {}

        def start(j):
            pend[j] = jobs[j]["qk"]()

        for j in range(min(LA, len(jobs))):
            start(j)
        conv_at = min(len(jobs) - 1, 200)
        for j in range(len(jobs)):
            if L == 0 and j == conv_at:
                convert_weights(s, X, X.conv_layers)
            if j + LA < len(jobs):
                start(j + LA)
            pt, ptres = pend.pop(j)
            jobs[j]["pv"](pt, ptres)
            if jobs[j]["post"]:
                jobs[j]["post"]()
        s.barrier()


PHASES.append(("nsa", phase_nsa))


def phase_ret(s, X, L, h_in):
    nc = X.nc
    with ExitStack() as es:
        sb = lambda name, shape, dt: es.enter_context(nc.sbuf_tensor("%s_L%d" % (name, L), shape, dt))
        qT = [sb("r_qT%d" % i, [64, 8, 128], BF16) for i in range(2)]
        kT = [sb("r_kT%d" % i, [64, 8, 128], BF16) for i in range(2)]
        ones1 = sb("r_ones1", [1, 128], F32)
        gn1 = sb("r_gn1", [1, 512], F32)
        km = [sb("r_km%d" % i, [128, 512], BF16) for i in range(2)]
        vm = [sb("r_vm%d" % i, [128, 512], BF16) for i in range(2)]
        gm = [sb("r_gm%d" % i, [128, 512], BF16) for i in range(2)]
        state = sb("r_state", [64, 8, 64], F32)
        stateb = sb("r_stateb", [64, 8, 64], BF16)
        rmask = sb("r_mask", [128, 512], F32)
        gC = sb("r_gC", [64, 8], F32)
        gn = sb("r_gn", [128, 512], F32)
        Pm = sb("r_Pm", [128, 8, 128], BF16)
        sq = sb("r_sq", [128, 512], F32)
        y = sb("r_y", [128, 512], F32)
        yb = [sb("r_yb%d" % i, [128, 512], BF16) for i in range(2)]
        rT = [sb("r_rT%d" % i, [128, 4, 128], BF16) for i in range(2)]
        stt = sb("r_st", [128, 64], F32)
        s.dma("sp", rmask[:], X.c["c_rmask"], writes=["rmask"])
        s.dma("sp", gC[:], X.c["c_gC8"], writes=["gC"])
        s.dma("sp", gn1[:], X.ret_gn[L:L + 1, :], writes=["gn1"])
        s.op("dve", lambda e: e.memset(ones1[:], 1.0), writes=["ones1"])
        s.op("pe", lambda e: e.matmul(X.ps[6][:], lhsT=ones1[:], rhs=gn1[:], start=True, stop=True),
             reads=["ones1", "gn1"], writes=["ps6"])
        s.op("dve", lambda e: e.tensor_copy(out=gn[:], in_=X.ps[6][:]), reads=["ps6"], writes=["gn"])
        s.op("dve", lambda e: e.memset(state[:], 0.0), writes=["state"])
        for c in range(X.ret_chunks):
            b = c % 2
            tok = slice(c * 128, (c + 1) * 128)
            for j in range(4):
                s.dma("sp", qT[b][:, 2 * j:2 * j + 2, :], X.QRT[j, :, tok].rearrange("(two d) t -> d two t", two=2),
                      reads=["QRT"], writes=["qT%d" % b])
                s.dma("sp", kT[b][:, 2 * j:2 * j + 2, :], X.KRT[j, :, tok].rearrange("(two d) t -> d two t", two=2),
                      reads=["KRT"], writes=["kT%d" % b])
            s.dma("sp", km[b][:], X.KRM[tok, :], reads=["KRM"], writes=["km%d" % b])
            s.dma("sp", vm[b][:], X.VRM[tok, :], reads=["VRM"], writes=["vm%d" % b])
            s.dma("sp", gm[b][:], X.GRM[tok, :], reads=["GRM"], writes=["gm%d" % b])
            for h in range(8):
                hp, base = h // 2, 64 * (h % 2)
                pb = h // 4
                s.op("pe", lambda e: e.matmul(X.ps[pb][:, (h % 4) * 128:(h % 4 + 1) * 128],
                                              lhsT=kT[b][:, h, :], rhs=qT[b][:, h, :],
                                              start=(h % 4 == 0), stop=True),
                     reads=["kT%d" % b, "qT%d" % b], writes=["ps%d" % pb])
            for pb in range(2):
                s.op("dve", lambda e: e.tensor_tensor(out=Pm[:, 4 * pb:4 * pb + 4, :].rearrange("p a t -> p (a t)"),
                                                      in0=X.ps[pb][:], in1=rmask[:], op=ALU.mult),
                     reads=["ps%d" % pb, "rmask"], writes=["Pm"])
            for h in range(8):
                hp, base = h // 2, 64 * (h % 2)
                s.op("pe", lambda e: e.matmul(X.ps[2][:, h * 64:(h + 1) * 64], lhsT=Pm[:, h, :],
                                              rhs=vm[b][:, h * 64:(h + 1) * 64], start=(h == 0), stop=(c == 0)),
                     reads=["Pm", "vm%d" % b], writes=["ps2"])
                if c > 0:
                    s.op("pe", lambda e: e.matmul(X.ps[2][:, h * 64:(h + 1) * 64], lhsT=qT[b][:, h, :],
                                                  rhs=stateb[:, h, :], start=False, stop=True),
                         reads=["qT%d" % b, "stateb"], writes=["ps2"])
            if True:
                for h in range(8):
                    s.op("pe", lambda e: e.matmul(X.ps[3][0:64, h * 64:(h + 1) * 64], lhsT=km[b][:, h * 64:(h + 1) * 64],
                                                  rhs=vm[b][:, h * 64:(h + 1) * 64], start=(h == 0), stop=True),
                         reads=["km%d" % b, "vm%d" % b], writes=["ps3"])
                s.op("dve", lambda e: e.tensor_tensor(out=state[:], in0=state[:],
                                                      in1=X.ps[3][0:64, :].rearrange("p (h x) -> p h x", h=8), op=ALU.add),
                     reads=["ps3"], writes=["state"])
                s.op("dve", lambda e: e.tensor_tensor(out=state[:], in0=state[:], in1=bcast_mid(gC[:], 64), op=ALU.mult),
                     reads=["gC"], writes=["state"])
                s.op("dve", lambda e: e.tensor_copy(out=stateb[:], in_=state[:]), reads=["state"], writes=["stateb"])
            o3 = X.ps[2][:].rearrange("p (h d) -> p h d", h=8)
            s.op("dve", lambda e: e.tensor_reduce(out=stt[:, 0:8], in_=o3, axis=AX.X, op=ALU.add),
                 reads=["ps2"], writes=["r_st"])
            s.op("act", lambda e: e.activation(out=sq[:], in_=X.ps[2][:], func=AF.Square), reads=["ps2"], writes=["r_sq"])
            s.op("dve", lambda e: e.tensor_reduce(out=stt[:, 8:16], in_=sq[:].rearrange("p (h d) -> p h d", h=8),
                                                  axis=AX.X, op=ALU.add), reads=["r_sq"], writes=["r_st"])
            s.op("dve", lambda e: e.tensor_scalar(out=stt[:, 16:24], in0=stt[:, 0:8], scalar1=1.0 / 64, scalar2=None,
                                                  op0=ALU.mult), reads=["r_st"], writes=["r_st"])
            s.op("dve", lambda e: e.tensor_tensor(out=stt[:, 24:32], in0=stt[:, 16:24], in1=stt[:, 16:24], op=ALU.mult),
                 reads=["r_st"], writes=["r_st"])
            s.op("dve", lambda e: e.scalar_tensor_tensor(out=stt[:, 32:40], in0=stt[:, 8:16], scalar=1.0 / 64,
                                                         in1=stt[:, 24:32], op0=ALU.mult, op1=ALU.subtract),
                 reads=["r_st"], writes=["r_st"])
            s.op("dve", lambda e: e.tensor_scalar(out=stt[:, 32:40], in0=stt[:, 32:40], scalar1=EPS, scalar2=None,
                                                  op0=ALU.add), reads=["r_st"], writes=["r_st"])
            s.op("act", lambda e: e.activation(out=stt[:, 40:48], in_=stt[:, 32:40], func=AF.Sqrt),
                 reads=["r_st"], writes=["r_st"])
            s.op("dve", lambda e: e.reciprocal(out=stt[:, 48:56], in_=stt[:, 40:48]), reads=["r_st"], writes=["r_st"])
            y3 = y[:].rearrange("p (h d) -> p h d", h=8)
            s.op("dve", lambda e: e.tensor_tensor(out=y3, in0=o3, in1=bcast_mid(stt[:, 16:24], 64), op=ALU.subtract),
                 reads=["ps2", "r_st"], writes=["r_y"])
            s.op("dve", lambda e: e.tensor_tensor(out=y3, in0=y3, in1=bcast_mid(stt[:, 48:56], 64), op=ALU.mult),
                 reads=["r_st"], writes=["r_y"])
            s.op("pool", lambda e: e.tensor_tensor(out=y[:], in0=y[:], in1=gn[:], op=ALU.mult),
                 reads=["r_y", "gn"], writes=["r_y"])
            s.op("pool", lambda e: e.tensor_tensor(out=yb[b][:], in0=y[:], in1=gm[b][:], op=ALU.mult),
                 reads=["r_y", "gm%d" % b], writes=["r_yb%d" % b])
            psb = X.ps[7][:].bitcast(BF16)
            for j in range(4):
                s.op("pe", lambda e: e.transpose(out=psb[:, j * 128:(j + 1) * 128], in_=yb[b][:, j * 128:(j + 1) * 128],
                                                 identity=X.ident[:]), reads=["r_yb%d" % b, "ident"], writes=["ps7"])
            s.op("act", lambda e: e.activation(out=rT[b][:].rearrange("p j t -> p (j t)"), in_=psb[:, 0:512], func=AF.Copy),
                 reads=["ps7"], writes=["r_rT%d" % b])
            s.dma("pool", X.AT[512:1024, tok].rearrange("(j p) t -> p j t", p=128), rT[b][:],
                  reads=["r_rT%d" % b], writes=["AT"])
        s.barrier()


PHASES.append(("ret", phase_ret))


def convert_weights(s, X, layers):
    def conv(dst, src, rows):
        for r0 in range(0, rows, 512):
            s.dma("pool", dst[r0:r0 + 512, :], src[r0:r0 + 512, :], writes=["W16"])
    if 0 in layers:
        conv(X.FG16[0], X.ffn_gate[0], D)
        conv(X.FU16[0], X.ffn_up[0], D)
        conv(X.FD16[0], X.ffn_down[0], D_FF)
    if 1 in layers:
        for e in range(NE):
            for blk in range(7):
                r0 = (e * 7 + blk) * 128
                fs = slice(blk * 512, (blk + 1) * 512)
                s.dma("pool", X.WGB[r0:r0 + 128, :].rearrange("p (k f) -> p k f", k=8),
                      X.moe_gate[0, e][:, fs].rearrange("(k p) f -> p k f", p=128), writes=["W16"])
                s.dma("pool", X.WUB[r0:r0 + 128, :].rearrange("p (k f) -> p k f", k=8),
                      X.moe_up[0, e][:, fs].rearrange("(k p) f -> p k f", p=128), writes=["W16"])
                s.dma("pool", X.WDB[r0:r0 + 128, :].rearrange("p (c n) -> p c n", c=4),
                      X.moe_down[0, e][fs, :].rearrange("(c p) n -> p c n", p=128), writes=["W16"])


def phase_ffn(s, X, L, h_in):
    nc = X.nc
    last = (L == DEPTH - 1)
    moe = (L % 2 == 1)
    if moe and X.routed:
        return phase_moe(s, X, L, h_in)
    h_out = X.out if last else X.HB
    G16, U16, D16 = X.FG16, X.FU16, X.FD16
    with ExitStack() as es:
        sb = lambda name, shape, dt: es.enter_context(nc.sbuf_tensor("%s_L%d" % (name, L), shape, dt))
        wout = sb("f_wout", [128, 8, D], BF16)
        pgate = sb("f_pgate", [128, 8, D], BF16)
        pproj = sb("f_pproj", [128, 2, D], BF16)
        g2T = sb("f_g2T", [128, 8], F32)
        g3T = sb("f_g3T", [128, 8], F32)
        rw = sb("f_rw", [128, 8, NE], BF16)
        cT = sb("f_cT", [128, 8, 512], BF16)
        hin = [sb("f_hin%d" % i, [128, D], F32) for i in range(2)]
        h1 = sb("f_h1", [128, 4, D], F32)
        hnT = sb("f_hnT", [128, 8, 512], BF16)
        acc = sb("f_acc", [128, 4, D], F32)
        wg = [sb("f_wg%d" % i, [128, 8, 512], BF16) for i in range(3)]
        wu = [sb("f_wu%d" % i, [128, 8, 512], BF16) for i in range(3)]
        wd = [sb("f_wd%d" % i, [128, 4, D], BF16) for i in range(3)]
        silu_t = [sb("f_silu%d" % i, [128, 512], BF16) for i in range(2)]
        actb = [sb("f_actb%d" % i, [128, 4, 512], BF16) for i in range(2)]
        sig = [sb("f_sig%d" % i, [128, 512], F32) for i in range(2)]
        ptile = [sb("f_pt%d" % i, [128, 256], F32) for i in range(2)]
        pb = sb("f_pb", [128, 256], BF16)
        pT = sb("f_pT", [128, 2, 512], BF16)
        gates = sb("f_gates", [128, 4, NE], F32)
        lg = sb("f_lg", [128, 16], F32)
        sm = sb("f_sm", [128, 32], F32)
        for kc in range(8):
            s.dma("pool", wout[:, kc, :], X.w_out[L, kc * 128:(kc + 1) * 128, :], writes=["wout"])
            s.dma("pool", pgate[:, kc, :], X.ple_gate[L, kc * 128:(kc + 1) * 128, :], writes=["pgate"])
        for kc in range(2):
            s.dma("pool", pproj[:, kc, :], X.ple_proj[L, kc * 128:(kc + 1) * 128, :], writes=["pproj"])
        s.dma("sp", g2T[:], X.g_ffn[L].rearrange("(k p) -> p k", p=128), writes=["g2T"], allow_slow_non_contiguous=True)
        s.dma("sp", g3T[:], X.g_ple[L].rearrange("(k p) -> p k", p=128), writes=["g3T"], allow_slow_non_contiguous=True)
        if moe:
            s.dma("pool", rw[:], X.moe_router[0].rearrange("(k p) e -> p k e", p=128), writes=["rw"])
        if last:
            gfin = sb("f_gfin", [128, D], F32)
            ones1 = sb("f_ones1", [1, 128], F32)
            gf1 = sb("f_gf1", [1, D], F32)
            s.dma("sp", gf1[:], X.g_final.rearrange("(o n) -> o n", o=1), writes=["gf1"])
            s.op("dve", lambda e: e.memset(ones1[:], 1.0), writes=["ones1"])
            for half in range(2):
                s.op("pe", lambda e: e.matmul(X.ps[6][:], lhsT=ones1[:], rhs=gf1[:, half * 512:(half + 1) * 512],
                                              start=True, stop=True), reads=["ones1", "gf1"], writes=["ps6"])
                s.op("dve", lambda e: e.tensor_copy(out=gfin[:, half * 512:(half + 1) * 512], in_=X.ps[6][:]),
                     reads=["ps6"], writes=["gfin"])
        wcnt = 0
        for tg in range(NT // 4):
            toks = slice(tg * 512, (tg + 1) * 512)
            s.dma("sp", cT[:], X.AT[:, toks].rearrange("(k p) t -> p k t", p=128), reads=["AT"], writes=["cT"])
            for tt in range(4):
                ti = 4 * tg + tt
                hb_ = hin[ti % 2]
                s.dma("sp", hb_[:], h_in[ti * 128:(ti + 1) * 128, :], reads=["HB"], writes=["hin%d" % (ti % 2)])
                for half in range(2):
                    hs = slice(half * 512, (half + 1) * 512)
                    for k in range(8):
                        s.op("pe", lambda e: e.matmul(X.ps[6][:], lhsT=cT[:, k, tt * 128:(tt + 1) * 128], rhs=wout[:, k, hs],
                                                      start=(k == 0), stop=(k == 7)), reads=["cT", "wout"], writes=["ps6"])
                    s.op("dve", lambda e: e.tensor_tensor(out=h1[:, tt, hs], in0=X.ps[6][:], in1=hb_[:, hs], op=ALU.add),
                         reads=["ps6", "hin%d" % (ti % 2)], writes=["h1_%d" % tt])
                ln_a(s, X, h1[:, tt, :], "h1_%d" % tt, tt)
            for tt in range(4):
                ln_b(s, X, tt, g2T[:], "g2T", hnT[:, :, tt * 128:(tt + 1) * 128], "f_hnT")
            if moe:
                for tt in range(4):
                    for k in range(8):
                        s.op("pe", lambda e: e.matmul(X.ps[6][:, 0:NE], lhsT=hnT[:, k, tt * 128:(tt + 1) * 128], rhs=rw[:, k, :],
                                                      start=(k == 0), stop=(k == 7)), reads=["f_hnT", "rw"], writes=["ps6"])
                    s.op("dve", lambda e: e.tensor_copy(out=lg[:, 0:8], in_=X.ps[6][:, 0:NE]), reads=["ps6"], writes=["lg"])
                    s.op("dve", lambda e: e.max(out=sm[:, 0:8], in_=lg[:, 0:8]), reads=["lg"], writes=["fsm"])
                    s.op("dve", lambda e: e.tensor_tensor(out=sm[:, 8:9], in0=sm[:, 1:2], in1=sm[:, 0:1], op=ALU.subtract),
                         reads=["fsm"], writes=["fsm"])
                    s.op("act", lambda e: e.activation(out=sm[:, 9:10], in_=sm[:, 8:9], func=AF.Exp), reads=["fsm"], writes=["fsm"])
                    s.op("dve", lambda e: e.tensor_scalar(out=sm[:, 10:11], in0=sm[:, 9:10], scalar1=1.0, scalar2=None,
                                                          op0=ALU.add), reads=["fsm"], writes=["fsm"])
                    s.op("dve", lambda e: e.reciprocal(out=sm[:, 11:12], in_=sm[:, 10:11]), reads=["fsm"], writes=["fsm"])
                    s.op("dve", lambda e: e.tensor_tensor(out=sm[:, 12:13], in0=sm[:, 9:10], in1=sm[:, 11:12], op=ALU.mult),
                         reads=["fsm"], writes=["fsm"])
                    s.op("dve", lambda e: e.tensor_scalar(out=lg[:, 8:16], in0=lg[:, 0:8], scalar1=sm[:, 0:1],
                                                          scalar2=sm[:, 11:12], op0=ALU.is_equal, op1=ALU.mult),
                         reads=["lg", "fsm"], writes=["lg"])
                    s.op("dve", lambda e: e.tensor_scalar(out=gates[:, tt, :], in0=lg[:, 0:8], scalar1=sm[:, 1:2],
                                                          scalar2=sm[:, 12:13], op0=ALU.is_equal, op1=ALU.mult),
                         reads=["lg", "fsm"], writes=["gates"])
                    s.op("dve", lambda e: e.tensor_tensor(out=gates[:, tt, :], in0=gates[:, tt, :], in1=lg[:, 8:16], op=ALU.add),
                         reads=["lg"], writes=["gates"])
            def stage_a(J):
                wb, ab = J % 3, J % 2
                for fc in range(4):
                    pg, pu = 2 * (fc % 2), 2 * (fc % 2) + 1
                    for k in range(8):
                        s.op("pe", lambda e: e.matmul(X.ps[pg][:], lhsT=wg[wb][:, k, fc * 128:(fc + 1) * 128], rhs=hnT[:, k, :],
                                                      start=(k == 0), stop=(k == 7)),
                             reads=["wg%d" % wb, "f_hnT"], writes=["ps%d" % pg])
                    for k in range(8):
                        s.op("pe", lambda e: e.matmul(X.ps[pu][:], lhsT=wu[wb][:, k, fc * 128:(fc + 1) * 128], rhs=hnT[:, k, :],
                                                      start=(k == 0), stop=(k == 7)),
                             reads=["wu%d" % wb, "f_hnT"], writes=["ps%d" % pu])
                    s.op("act", lambda e: e.activation(out=silu_t[fc % 2][:], in_=X.ps[pg][:], func=AF.Silu),
                         reads=["ps%d" % pg], writes=["silu%d" % (fc % 2)])
                    s.op("dve", lambda e: e.tensor_tensor(out=actb[ab][:, fc, :], in0=X.ps[pu][:], in1=silu_t[fc % 2][:],
                                                          op=ALU.mult),
                         reads=["ps%d" % pu, "silu%d" % (fc % 2)], writes=["actb%d" % ab])

            def stage_b(J):
                wb, ab, blk = J % 3, J % 2, J % 7
                for tt in range(4):
                    for half in range(2):
                        hs = slice(half * 512, (half + 1) * 512)
                        pd = 4 + half
                        for fc in range(4):
                            s.op("pe", lambda e: e.matmul(X.ps[pd][:], lhsT=actb[ab][:, fc, tt * 128:(tt + 1) * 128],
                                                          rhs=wd[wb][:, fc, hs], start=(fc == 0), stop=(fc == 3)),
                                 reads=["actb%d" % ab, "wd%d" % wb], writes=["ps%d" % pd])
                        dst = acc[:, tt, hs]
                        if blk == 0:
                            s.op("dve", lambda e: e.tensor_copy(out=dst, in_=X.ps[pd][:]),
                                 reads=["ps%d" % pd], writes=["acc%d" % tt])
                        else:
                            s.op("dve", lambda e: e.tensor_tensor(out=dst, in0=X.ps[pd][:], in1=dst, op=ALU.add),
                                 reads=["ps%d" % pd], writes=["acc%d" % tt])

            def wload(J):
                blk, wb = J % 7, J % 3
                fs = slice(blk * 512, (blk + 1) * 512)
                s.dma("sp", wg[wb][:], G16[0][:, fs].rearrange("(k p) f -> p k f", p=128), reads=["W16"], writes=["wg%d" % wb])
                s.dma("sp", wu[wb][:], U16[0][:, fs].rearrange("(k p) f -> p k f", p=128), reads=["W16"], writes=["wu%d" % wb])
                s.dma("sp", wd[wb][:], D16[0][fs, :].rearrange("(c p) n -> p c n", p=128), reads=["W16"], writes=["wd%d" % wb])

            NJ = (NT // 4) * 7
            if tg == 0:
                wload(0)
                wload(1)
            for blk in range(7):
                J = tg * 7 + blk
                stage_a(J)
                if blk >= 1:
                    stage_b(J - 1)
                if J + 2 < NJ:
                    wload(J + 2)
            stage_b(tg * 7 + 6)
            for tt in range(4):
                s.op("pool", lambda e: e.tensor_tensor(out=h1[:, tt, :], in0=h1[:, tt, :], in1=acc[:, tt, :], op=ALU.add),
                     reads=["acc%d" % tt], writes=["h1_%d" % tt])
                ln_a(s, X, h1[:, tt, :], "h1_%d" % tt, tt)
            for tt in range(4):
                ln_b(s, X, tt, g3T[:], "g3T", hnT[:, :, tt * 128:(tt + 1) * 128], "f_hnT")
            psb = X.ps[7][:].bitcast(BF16)
            for tt in range(4):
                ti = 4 * tg + tt
                pt_ = ptile[ti % 2]
                s.dma("sp", pt_[:], X.p[L, ti * 128:(ti + 1) * 128, :], writes=["pt%d" % (ti % 2)])
                s.op("act", lambda e: e.activation(out=pb[:], in_=pt_[:], func=AF.Copy), reads=["pt%d" % (ti % 2)], writes=["pb"])
                for k in range(2):
                    s.op("pe", lambda e: e.transpose(out=psb[:, k * 128:(k + 1) * 128], in_=pb[:, k * 128:(k + 1) * 128],
                                                     identity=X.ident[:]), reads=["pb", "ident"], writes=["ps7"])
                s.op("act", lambda e: e.activation(out=pT[:, :, tt * 128:(tt + 1) * 128],
                                                   in_=psb[:, 0:256].rearrange("p (k t) -> p k t", k=2), func=AF.Copy),
                     reads=["ps7"], writes=["pT"])
            for tt in range(4):
                ti = 4 * tg + tt
                for half in range(2):
                    hs = slice(half * 512, (half + 1) * 512)
                    for k in range(2):
                        s.op("pe", lambda e: e.matmul(X.ps[6][:], lhsT=pT[:, k, tt * 128:(tt + 1) * 128], rhs=pproj[:, k, hs],
                                                      start=(k == 0), stop=(k == 1)), reads=["pT", "pproj"], writes=["ps6"])
                    pgb = 4 + half
                    for k in range(8):
                        s.op("pe", lambda e: e.matmul(X.ps[pgb][:], lhsT=hnT[:, k, tt * 128:(tt + 1) * 128], rhs=pgate[:, k, hs],
                                                      start=(k == 0), stop=(k == 7)), reads=["f_hnT", "pgate"], writes=["ps%d" % pgb])
                    s.op("act", lambda e: e.activation(out=sig[half][:], in_=X.ps[pgb][:], func=AF.Sigmoid),
                         reads=["ps%d" % pgb], writes=["sig%d" % half])
                    s.op("dve", lambda e: e.tensor_tensor(out=acc[:, tt, hs], in0=X.ps[6][:], in1=sig[half][:], op=ALU.mult),
                         reads=["ps6", "sig%d" % half], writes=["acc%d" % tt])
                s.op("pool", lambda e: e.tensor_tensor(out=h1[:, tt, :], in0=h1[:, tt, :], in1=acc[:, tt, :], op=ALU.add),
                     reads=["acc%d" % tt], writes=["h1_%d" % tt])
                if last:
                    ln_a(s, X, h1[:, tt, :], "h1_%d" % tt, tt)
                    s.op("act", lambda e: e.activation(out=acc[:, tt, :], in_=h1[:, tt, :], func=AF.Copy, scale=X.ss[tt][:, 2:3]),
                         reads=["h1_%d" % tt, "ss%d" % tt], writes=["acc%d" % tt])
                    s.op("pool", lambda e: e.tensor_tensor(out=acc[:, tt, :], in0=acc[:, tt, :], in1=gfin[:], op=ALU.mult),
                         reads=["gfin"], writes=["acc%d" % tt])
                    s.dma("pool", h_out[ti * 128:(ti + 1) * 128, :], acc[:, tt, :], reads=["acc%d" % tt], writes=["HOUT"])
                else:
                    s.dma("pool", h_out[ti * 128:(ti + 1) * 128, :], h1[:, tt, :], reads=["h1_%d" % tt], writes=["HB"])
        s.barrier()


PHASES.append(("ffn", phase_ffn))


def phase_moe(s, X, L, h_in):
    nc = X.nc
    last = (L == DEPTH - 1)
    h_out = X.out if last else X.HB
    with ExitStack() as es:
        sb = lambda name, shape, dt: es.enter_context(nc.sbuf_tensor("%s_L%d" % (name, L), shape, dt))
        OH = [sb("m_oh%d" % k, [128, NT, NE], F32) for k in range(2)]
        WK = [sb("m_wk%d" % k, [128, NT], F32) for k in range(2)]
        POSI2 = sb("m_posi", [128, 2 * NT], I32)
        POSI = POSI2[:].rearrange("p (k i) -> p k i", k=2)
        WIDX2 = sb("m_widx", [128, NGRP * 7], I32)
        WIDX = WIDX2[:].rearrange("p (g b) -> p g b", b=7)
        g2T = sb("m_g2T", [128, 8], F32)
        g3T = sb("m_g3T", [128, 8], F32)
        hnT = sb("m_hnT", [128, 8, 512], BF16)
        s.dma("sp", g2T[:], X.g_ffn[L].rearrange("(k p) -> p k", p=128), writes=["g2T"], allow_slow_non_contiguous=True)
        s.dma("sp", g3T[:], X.g_ple[L].rearrange("(k p) -> p k", p=128), writes=["g3T"], allow_slow_non_contiguous=True)
        with ExitStack() as es2:
            sb2 = lambda name, shape, dt: es2.enter_context(nc.sbuf_tensor("%s_L%d" % (name, L), shape, dt))
            wout = sb2("ma_wout", [128, 8, D], BF16)
            rw = sb2("ma_rw", [128, 8, NE], BF16)
            cT = sb2("ma_cT", [128, 8, 512], BF16)
            hin = [sb2("ma_hin%d" % i, [128, D], F32) for i in range(2)]
            h1 = [sb2("ma_h1%d" % i, [128, D], F32) for i in range(4)]
            lg = sb2("ma_lg", [128, 16], F32)
            sm = sb2("ma_sm", [128, 32], F32)
            for kc in range(8):
                s.dma("pool", wout[:, kc, :], X.w_out[L, kc * 128:(kc + 1) * 128, :], writes=["wout"])
            s.dma("pool", rw[:], X.moe_router[0].rearrange("(k p) e -> p k e", p=128), writes=["rw"])
            for tg in range(NT // 4):
                toks = slice(tg * 512, (tg + 1) * 512)
                s.dma("sp", cT[:], X.AT[:, toks].rearrange("(k p) t -> p k t", p=128), reads=["AT"], writes=["cT"])
                for tt in range(4):
                    ti = 4 * tg + tt
                    hb_ = hin[ti % 2]
                    s.dma("sp", hb_[:], h_in[ti * 128:(ti + 1) * 128, :], reads=["HB"], writes=["hin%d" % (ti % 2)])
                    for half in range(2):
                        hs = slice(half * 512, (half + 1) * 512)
                        pbk = 4 + half
                        for k in range(8):
                            s.op("pe", lambda e: e.matmul(X.ps[pbk][:], lhsT=cT[:, k, tt * 128:(tt + 1) * 128], rhs=wout[:, k, hs],
                                                          start=(k == 0), stop=(k == 7)), reads=["cT", "wout"], writes=["ps%d" % pbk])
                        s.op("dve", lambda e: e.tensor_tensor(out=h1[tt][:, hs], in0=X.ps[pbk][:], in1=hb_[:, hs], op=ALU.add),
                             reads=["ps%d" % pbk, "hin%d" % (ti % 2)], writes=["h1_%d" % tt])
                    s.dma("pool", X.H1[ti * 128:(ti + 1) * 128, :], h1[tt][:], reads=["h1_%d" % tt], writes=["H1"])
                    ln_a(s, X, h1[tt][:], "h1_%d" % tt, tt)
                    s.dma("pool", X.XS2[ti * 128:(ti + 1) * 128, :], X.xs[tt][:], reads=["xs%d" % tt], writes=["XS2"])
                for tt in range(4):
                    ln_b(s, X, tt, g2T[:], "g2T", hnT[:, :, tt * 128:(tt + 1) * 128], "m_hnT")
                for tt in range(4):
                    ti = 4 * tg + tt
                    for k in range(8):
                        s.op("pe", lambda e: e.matmul(X.ps[6][:, 0:NE], lhsT=hnT[:, k, tt * 128:(tt + 1) * 128], rhs=rw[:, k, :],
                                                      start=(k == 0), stop=(k == 7)), reads=["m_hnT", "rw"], writes=["ps6"])
                    s.op("dve", lambda e: e.tensor_copy(out=lg[:, 0:8], in_=X.ps[6][:, 0:NE]), reads=["ps6"], writes=["lg"])
                    s.op("dve", lambda e: e.max(out=sm[:, 0:8], in_=lg[:, 0:8]), reads=["lg"], writes=["fsm"])
                    s.op("dve", lambda e: e.tensor_tensor(out=sm[:, 8:9], in0=sm[:, 1:2], in1=sm[:, 0:1], op=ALU.subtract),
                         reads=["fsm"], writes=["fsm"])
                    s.op("act", lambda e: e.activation(out=sm[:, 9:10], in_=sm[:, 8:9], func=AF.Exp), reads=["fsm"], writes=["fsm"])
                    s.op("dve", lambda e: e.tensor_scalar(out=sm[:, 10:11], in0=sm[:, 9:10], scalar1=1.0, scalar2=None,
                                                          op0=ALU.add), reads=["fsm"], writes=["fsm"])
                    s.op("dve", lambda e: e.reciprocal(out=WK[0][:, ti:ti + 1], in_=sm[:, 10:11]), reads=["fsm"], writes=["WK"])
                    s.op("dve", lambda e: e.tensor_tensor(out=WK[1][:, ti:ti + 1], in0=sm[:, 9:10], in1=WK[0][:, ti:ti + 1],
                                                          op=ALU.mult), reads=["fsm", "WK"], writes=["WK"])
                    s.op("dve", lambda e: e.tensor_scalar(out=OH[0][:, ti, :], in0=lg[:, 0:8], scalar1=sm[:, 0:1], scalar2=None,
                                                          op0=ALU.is_equal), reads=["lg", "fsm"], writes=["OH"])
                    s.op("dve", lambda e: e.tensor_scalar(out=OH[1][:, ti, :], in0=lg[:, 0:8], scalar1=sm[:, 1:2], scalar2=None,
                                                          op0=ALU.is_equal), reads=["lg", "fsm"], writes=["OH"])
            s.barrier()
        with ExitStack() as es2:
            sb2 = lambda name, shape, dt: es2.enter_context(nc.sbuf_tensor("%s_L%d" % (name, L), shape, dt))
            Mf = sb2("mb_Mf", [128, NT, NE], F32)
            Mb = sb2("mb_Mb", [128, NT * NE], BF16)
            U = sb2("mb_U", [128, 128], BF16)
            ones = sb2("mb_ones", [128, 128], BF16)
            A_ = sb2("mb_A", [128, NT, NE], F32)
            B_ = sb2("mb_B", [128, NT, NE], F32)
            CN = sb2("mb_CN", [128, NT, NE], F32)
            POS = sb2("mb_POS", [128, NT, NE], F32)
            nE = sb2("mb_nE", [128, 64], F32)
            cmp16 = sb2("mb_cmp16", [128, NE, 16], F32)
            gi512 = sb2("mb_gi512", [128, NGRP], F32)
            tokid = sb2("mb_tokid", [128, NT], F32)
            blkp = sb2("mb_blkp", [128, 7], F32)
            cmpg = sb2("mb_cmpg", [128, NGRP, NE], F32)
            EG = sb2("mb_EG", [128, NGRP], F32)
            WF = sb2("mb_WF", [128, NGRP, 7], F32)
            PK = sb2("mb_PK", [128, 2, NT], F32)
            ROWS2 = sb2("mb_rows", [128, 2 * NT * 2], I32)
            ROWS = ROWS2[:].rearrange("p (k i c) -> p k i c", k=2, c=2)
            Z = sb2("mb_Z", [128, 2 * NSLOT // 128], I32)
            s.dma("pool", U[:], X.c["c_U"], writes=["U"])
            s.dma("sp", gi512[:], X.c["c_gi512"], writes=["gi512"])
            s.dma("sp", tokid[:], X.c["c_tokid"], writes=["tokid"])
            s.dma("sp", blkp[:], X.c["c_blkp"], writes=["blkp"])
            s.op("pool", lambda e: e.memset(ones[:], 1.0), writes=["ones"])
            s.op("pool", lambda e: e.memset(Z[:], 0), writes=["Z"])
            s.dma("sp", X.SLOT.rearrange("(p a) c -> p (a c)", p=128), Z[:], reads=["Z"], writes=["SLOT"])
            s.op("dve", lambda e: e.tensor_tensor(out=Mf[:], in0=OH[0][:], in1=OH[1][:], op=ALU.add), reads=["OH"], writes=["Mf"])
            s.op("dve", lambda e: e.tensor_copy(out=Mb[:], in_=Mf[:].rearrange("p i e -> p (i e)")), reads=["Mf"], writes=["Mb"])
            s.op("pe", lambda e: e.matmul(X.ps[0][:], lhsT=U[:], rhs=Mb[:], start=True, stop=True), reads=["U", "Mb"], writes=["ps0"])
            s.op("pe", lambda e: e.matmul(X.ps[1][:], lhsT=ones[:], rhs=Mb[:], start=True, stop=True), reads=["ones", "Mb"], writes=["ps1"])
            s.op("dve", lambda e: e.tensor_copy(out=CN[:].rearrange("p i e -> p (i e)"), in_=X.ps[1][:]), reads=["ps1"], writes=["CN"])
            s.op("dve", lambda e: e.tensor_copy(out=A_[:], in_=CN[:]), reads=["CN"], writes=["A"])
            src, dst, sres, dres = A_, B_, "A", "B"
            for sh in (1, 2, 4, 8, 16, 32):
                s.op("dve", lambda e: e.tensor_copy(out=dst[:, 0:sh, :], in_=src[:, 0:sh, :]), reads=[sres], writes=[dres])
                s.op("dve", lambda e: e.tensor_tensor(out=dst[:, sh:NT, :], in0=src[:, sh:NT, :], in1=src[:, 0:NT - sh, :],
                                                      op=ALU.add), reads=[sres], writes=[dres])
                src, dst, sres, dres = dst, src, dres, sres
            incl, ires = src, sres
            s.op("dve", lambda e: e.tensor_copy(out=nE[:, 0:8], in_=incl[:, NT - 1, :]), reads=[ires], writes=["nE"])
            s.op("dve", lambda e: e.tensor_tensor(out=cmp16[:], in0=bcast_mid(nE[:, 0:8], 16), in1=bcast_rep(gi512[:, 0:16], NE),
                                                  op=ALU.is_gt), reads=["nE", "gi512"], writes=["cmp16"])
            s.op("dve", lambda e: e.tensor_reduce(out=nE[:, 8:16], in_=cmp16[:], axis=AX.X, op=ALU.add), reads=["cmp16"], writes=["nE"])
            s.op("dve", lambda e: e.tensor_scalar(out=nE[:, 8:16], in0=nE[:, 8:16], scalar1=512.0, scalar2=None, op0=ALU.mult),
                 reads=["nE"], writes=["nE"])
            s.op("dve", lambda e: e.tensor_copy(out=nE[:, 16:17], in_=nE[:, 8:9]), reads=["nE"], writes=["nE"])
            for e_ in range(1, NE):
                s.op("dve", lambda e: e.tensor_tensor(out=nE[:, 16 + e_:17 + e_], in0=nE[:, 15 + e_:16 + e_], in1=nE[:, 8 + e_:9 + e_],
                                                      op=ALU.add), reads=["nE"], writes=["nE"])
            s.op("dve", lambda e: e.tensor_tensor(out=nE[:, 24:32], in0=nE[:, 16:24], in1=nE[:, 8:16], op=ALU.subtract),
                 reads=["nE"], writes=["nE"])
            s.op("dve", lambda e: e.tensor_tensor(out=POS[:], in0=incl[:], in1=CN[:], op=ALU.subtract), reads=[ires, "CN"], writes=["POS"])
            s.op("dve", lambda e: e.tensor_tensor(out=POS[:].rearrange("p i e -> p (i e)"), in0=POS[:].rearrange("p i e -> p (i e)"),
                                                  in1=X.ps[0][:], op=ALU.add), reads=["ps0"], writes=["POS"])
            s.op("dve", lambda e: e.tensor_tensor(out=POS[:], in0=POS[:], in1=bcast_rep(nE[:, 24:32], NT), op=ALU.add),
                 reads=["nE"], writes=["POS"])
            for k in range(2):
                s.op("dve", lambda e: e.tensor_tensor(out=Mf[:], in0=POS[:], in1=OH[k][:], op=ALU.mult), reads=["POS", "OH"], writes=["Mf"])
                s.op("dve", lambda e: e.tensor_reduce(out=PK[:, k, :], in_=Mf[:], axis=AX.X, op=ALU.add), reads=["Mf"], writes=["PK"])
                s.op("dve", lambda e: e.tensor_copy(out=ROWS[:, k, :, 0], in_=tokid[:]), reads=["tokid"], writes=["ROWS"])
                s.op("dve", lambda e: e.tensor_copy(out=ROWS[:, k, :, 1], in_=WK[k][:].bitcast(I32)), reads=["WK"], writes=["ROWS"])
            s.op("dve", lambda e: e.tensor_copy(out=POSI, in_=PK[:]), reads=["PK"], writes=["POSI"])
            s.op("dve", lambda e: e.tensor_tensor(out=cmpg[:], in0=bcast_rep(nE[:, 16:24], NGRP), in1=bcast_mid(gi512[:], NE),
                                                  op=ALU.is_le), reads=["nE", "gi512"], writes=["cmpg"])
            s.op("dve", lambda e: e.tensor_reduce(out=EG[:], in_=cmpg[:], axis=AX.X, op=ALU.add), reads=["cmpg"], writes=["EG"])
            s.op("dve", lambda e: e.tensor_scalar(out=EG[:], in0=EG[:], scalar1=float(NE - 1), scalar2=896.0, op0=ALU.min, op1=ALU.mult),
                 reads=["EG"], writes=["EG"])
            s.op("dve", lambda e: e.tensor_tensor(out=WF[:], in0=bcast_mid(EG[:], 7), in1=bcast_rep(blkp[:], NGRP), op=ALU.add),
                 reads=["EG", "blkp"], writes=["WF"])
            s.op("dve", lambda e: e.tensor_copy(out=WIDX, in_=WF[:]), reads=["WF"], writes=["WIDX"])
            for ti in range(NT):
                for k in range(2):
                    s.idma(X.SLOT[:, :], POSI2[:, k * NT + ti:k * NT + ti + 1],
                           ROWS2[:, (k * NT + ti) * 2:(k * NT + ti) * 2 + 2], None, NSLOT - 1,
                           reads=["ROWS", "POSI"], writes=["SLOT"])
            s.barrier()
        with ExitStack() as es2:
            sb2 = lambda name, shape, dt: es2.enter_context(nc.sbuf_tensor("%s_L%d" % (name, L), shape, dt))
            stt = [sb2("mc_st%d" % i, [128, 8], I32) for i in range(2)]
            xg = [sb2("mc_xg%d" % i, [128, D], BF16) for i in range(8)]
            acc = [sb2("mc_acc%d" % i, [128, 4, D], F32) for i in range(2)]
            wg = [sb2("mc_wg%d" % i, [128, 8, 512], BF16) for i in range(3)]
            wu = [sb2("mc_wu%d" % i, [128, 8, 512], BF16) for i in range(3)]
            wd = [sb2("mc_wd%d" % i, [128, 4, D], BF16) for i in range(3)]
            silu_t = [sb2("mc_silu%d" % i, [128, 512], BF16) for i in range(2)]
            actb = [sb2("mc_actb%d" % i, [128, 4, 512], BF16) for i in range(2)]
            hn = [sb2("mc_hn%d" % i, [128, 8, 512], BF16) for i in range(2)]
            NWB = len(wg)

            def prologue_loads(gi):
                gb = gi % 2
                s.dma("sp", stt[gb][:].rearrange("p (tt c) -> p tt c", c=2),
                      X.SLOT[gi * 512:(gi + 1) * 512, :].rearrange("(tt p) c -> p tt c", p=128),
                      reads=["SLOT"], writes=["st%d" % gb])
                for tt in range(4):
                    s.idma(xg[gb * 4 + tt][:, :], None, X.XS2[:, :], stt[gb][:, 2 * tt:2 * tt + 1], SEQ - 1,
                           reads=["XS2", "st%d" % gb], writes=["xg%d" % (gb * 4 + tt)])

            def prologue_tr(gi):
                gb = gi % 2
                for tt in range(4):
                    ln_b(s, X, tt, g2T[:], "g2T", hn[gb][:, :, tt * 128:(tt + 1) * 128], "hn%d" % gb,
                         src=xg[gb * 4 + tt], src_res="xg%d" % (gb * 4 + tt))

            def wload(j):
                gi, blk = divmod(j, 7)
                wb = j % NWB
                off = WIDX2[:, gi * 7 + blk:gi * 7 + blk + 1]
                s.idma(wg[wb][:].rearrange("p k f -> p (k f)"), None, X.WGB[:, :], off, 0, reads=["W16", "WIDX"], writes=["wg%d" % wb])
                s.idma(wu[wb][:].rearrange("p k f -> p (k f)"), None, X.WUB[:, :], off, 0, reads=["W16", "WIDX"], writes=["wu%d" % wb])
                s.idma(wd[wb][:].rearrange("p c n -> p (c n)"), None, X.WDB[:, :], off, 0, reads=["W16", "WIDX"], writes=["wd%d" % wb])

            def stage_a(j):
                gi, blk = divmod(j, 7)
                gb, wb, ab = gi % 2, j % NWB, j % 2
                for fc in range(4):
                    pg, pu = 2 * (fc % 2), 2 * (fc % 2) + 1
                    for k in range(8):
                        s.op("pe", lambda e: e.matmul(X.ps[pg][:], lhsT=wg[wb][:, k, fc * 128:(fc + 1) * 128], rhs=hn[gb][:, k, :],
                                                      start=(k == 0), stop=(k == 7)),
                             reads=["wg%d" % wb, "hn%d" % gb], writes=["ps%d" % pg])
                    for k in range(8):
                        s.op("pe", lambda e: e.matmul(X.ps[pu][:], lhsT=wu[wb][:, k, fc * 128:(fc + 1) * 128], rhs=hn[gb][:, k, :],
                                                      start=(k == 0), stop=(k == 7)),
                             reads=["wu%d" % wb, "hn%d" % gb], writes=["ps%d" % pu])
                    s.op("act", lambda e: e.activation(out=silu_t[fc % 2][:], in_=X.ps[pg][:], func=AF.Silu),
                         reads=["ps%d" % pg], writes=["silu%d" % (fc % 2)])
                    s.op("dve", lambda e: e.tensor_tensor(out=actb[ab][:, fc, :], in0=X.ps[pu][:], in1=silu_t[fc % 2][:],
                                                          op=ALU.mult),
                         reads=["ps%d" % pu, "silu%d" % (fc % 2)], writes=["actb%d" % ab])

            def stage_b(j):
                gi, blk = divmod(j, 7)
                gb, wb, ab = gi % 2, j % NWB, j % 2
                for tt in range(4):
                    gsc = stt[gb][:, 2 * tt + 1:2 * tt + 2].bitcast(F32)
                    for half in range(2):
                        hs = slice(half * 512, (half + 1) * 512)
                        pd = 4 + half
                        for fc in range(4):
                            s.op("pe", lambda e: e.matmul(X.ps[pd][:], lhsT=actb[ab][:, fc, tt * 128:(tt + 1) * 128],
                                                          rhs=wd[wb][:, fc, hs], start=(fc == 0), stop=(fc == 3)),
                                 reads=["actb%d" % ab, "wd%d" % wb], writes=["ps%d" % pd])
                        dst = acc[gb][:, tt, hs]
                        if blk == 0:
                            s.op("dve", lambda e: e.tensor_scalar(out=dst, in0=X.ps[pd][:], scalar1=gsc, scalar2=None, op0=ALU.mult),
                                 reads=["ps%d" % pd, "st%d" % gb], writes=["acc%d_%d" % (gb, tt)])
                        else:
                            s.op("dve", lambda e: e.scalar_tensor_tensor(out=dst, in0=X.ps[pd][:], scalar=gsc, in1=dst,
                                                                         op0=ALU.mult, op1=ALU.add),
                                 reads=["ps%d" % pd, "st%d" % gb], writes=["acc%d_%d" % (gb, tt)])
                if blk == 6:
                    s.dma("sp", X.YS[gi * 512:(gi + 1) * 512, :].rearrange("(tt p) n -> p tt n", p=128), acc[gb][:],
                          reads=["acc%d_%d" % (gb, tt) for tt in range(4)], writes=["YS"])

            NJ = NGRP * 7
            prologue_loads(0)
            prologue_tr(0)
            for j in range(min(NWB - 1, NJ)):
                wload(j)
            for j in range(NJ + 1):
                if j < NJ:
                    gi, blk = divmod(j, 7)
                    stage_a(j)
                    if blk == 6 and gi + 1 < NGRP:
                        prologue_tr(gi + 1)
                if j >= 1:
                    stage_b(j - 1)
                if j < NJ and j % 7 == 0 and j // 7 + 1 < NGRP:
                    prologue_loads(j // 7 + 1)
                if j + NWB - 1 < NJ:
                    wload(j + NWB - 1)
            s.barrier()
        with ExitStack() as es2:
            sb2 = lambda name, shape, dt: es2.enter_context(nc.sbuf_tensor("%s_L%d" % (name, L), shape, dt))
            pgate = sb2("md_pgate", [128, 8, D], BF16)
            pproj = sb2("md_pproj", [128, 2, D], BF16)
            h1 = [sb2("md_h1%d" % i, [128, 4, D], F32) for i in range(2)]
            acc = [sb2("md_acc%d" % i, [128, 4, D], F32) for i in range(2)]
            hnd = [sb2("md_hnd%d" % i, [128, 8, 512], BF16) for i in range(2)]
            yA = [sb2("md_yA%d" % i, [128, D], F32) for i in range(2)]
            yB = [sb2("md_yB%d" % i, [128, D], F32) for i in range(2)]
            sig = [sb2("md_sig%d" % i, [128, 512], F32) for i in range(2)]
            ptile = [sb2("md_pt%d" % i, [128, 256], F32) for i in range(2)]
            pb = sb2("md_pb", [128, 256], BF16)
            pT = [sb2("md_pT%d" % i, [128, 2, 512], BF16) for i in range(2)]
            for kc in range(8):
                s.dma("pool", pgate[:, kc, :], X.ple_gate[L, kc * 128:(kc + 1) * 128, :], writes=["pgate"])
            for kc in range(2):
                s.dma("pool", pproj[:, kc, :], X.ple_proj[L, kc * 128:(kc + 1) * 128, :], writes=["pproj"])
            if last:
                gfin = sb2("md_gfin", [128, D], F32)
                ones1 = sb2("md_ones1", [1, 128], F32)
                gf1 = sb2("md_gf1", [1, D], F32)
                s.dma("sp", gf1[:], X.g_final.rearrange("(o n) -> o n", o=1), writes=["gf1"])
                s.op("dve", lambda e: e.memset(ones1[:], 1.0), writes=["ones1"])
                for half in range(2):
                    s.op("pe", lambda e: e.matmul(X.ps[6][:], lhsT=ones1[:], rhs=gf1[:, half * 512:(half + 1) * 512],
                                                  start=True, stop=True), reads=["ones1", "gf1"], writes=["ps6"])
                    s.op("dve", lambda e: e.tensor_copy(out=gfin[:, half * 512:(half + 1) * 512], in_=X.ps[6][:]),
                         reads=["ps6"], writes=["gfin"])
            def stage1(tg):
                hb = tg % 2
                for tt in range(4):
                    ti = 4 * tg + tt
                    yb_ = ti % 2
                    hres = "h1_%d_%d" % (hb, tt)
                    s.dma("sp", h1[hb][:, tt, :], X.H1[ti * 128:(ti + 1) * 128, :], reads=["H1"], writes=[hres])
                    s.idma(yA[yb_][:, :], None, X.YS[:, :], POSI2[:, ti:ti + 1], 0, reads=["YS", "POSI"], writes=["yA%d" % yb_])
                    s.idma(yB[yb_][:, :], None, X.YS[:, :], POSI2[:, NT + ti:NT + ti + 1], 0, reads=["YS", "POSI"], writes=["yB%d" % yb_])
                    s.op("dve", lambda e: e.tensor_tensor(out=h1[hb][:, tt, :], in0=h1[hb][:, tt, :], in1=yA[yb_][:], op=ALU.add),
                         reads=["yA%d" % yb_], writes=[hres])
                    s.op("dve", lambda e: e.tensor_tensor(out=h1[hb][:, tt, :], in0=h1[hb][:, tt, :], in1=yB[yb_][:], op=ALU.add),
                         reads=["yB%d" % yb_], writes=[hres])
                    ln_a(s, X, h1[hb][:, tt, :], hres, tt)
                for tt in range(4):
                    ln_b(s, X, tt, g3T[:], "g3T", hnd[hb][:, :, tt * 128:(tt + 1) * 128], "hnd%d" % hb)
                psb = X.ps[7][:].bitcast(BF16)
                for tt in range(4):
                    ti = 4 * tg + tt
                    pt_ = ptile[ti % 2]
                    s.dma("sp", pt_[:], X.p[L, ti * 128:(ti + 1) * 128, :], writes=["pt%d" % (ti % 2)])
                    s.op("act", lambda e: e.activation(out=pb[:], in_=pt_[:], func=AF.Copy), reads=["pt%d" % (ti % 2)], writes=["pb"])
                    for k in range(2):
                        s.op("pe", lambda e: e.transpose(out=psb[:, k * 128:(k + 1) * 128], in_=pb[:, k * 128:(k + 1) * 128],
                                                         identity=X.ident[:]), reads=["pb", "ident"], writes=["ps7"])
                    s.op("act", lambda e: e.activation(out=pT[hb][:, :, tt * 128:(tt + 1) * 128],
                                                       in_=psb[:, 0:256].rearrange("p (k t) -> p k t", k=2), func=AF.Copy),
                         reads=["ps7"], writes=["pT%d" % hb])

            def stage2(tg):
                hb = tg % 2
                for tt in range(4):
                    ti = 4 * tg + tt
                    hres = "h1_%d_%d" % (hb, tt)
                    ares = "dacc%d_%d" % (hb, tt)
                    for half in range(2):
                        hs = slice(half * 512, (half + 1) * 512)
                        for k in range(2):
                            s.op("pe", lambda e: e.matmul(X.ps[6][:], lhsT=pT[hb][:, k, tt * 128:(tt + 1) * 128], rhs=pproj[:, k, hs],
                                                          start=(k == 0), stop=(k == 1)), reads=["pT%d" % hb, "pproj"], writes=["ps6"])
                        pgb = 4 + half
                        for k in range(8):
                            s.op("pe", lambda e: e.matmul(X.ps[pgb][:], lhsT=hnd[hb][:, k, tt * 128:(tt + 1) * 128], rhs=pgate[:, k, hs],
                                                          start=(k == 0), stop=(k == 7)), reads=["hnd%d" % hb, "pgate"], writes=["ps%d" % pgb])
                        s.op("act", lambda e: e.activation(out=sig[half][:], in_=X.ps[pgb][:], func=AF.Sigmoid),
                             reads=["ps%d" % pgb], writes=["sig%d" % half])
                        s.op("dve", lambda e: e.tensor_tensor(out=acc[hb][:, tt, hs], in0=X.ps[6][:], in1=sig[half][:], op=ALU.mult),
                             reads=["ps6", "sig%d" % half], writes=[ares])
                    s.op("pool", lambda e: e.tensor_tensor(out=h1[hb][:, tt, :], in0=h1[hb][:, tt, :], in1=acc[hb][:, tt, :], op=ALU.add),
                         reads=[ares], writes=[hres])
                    if last:
                        k4 = 4 + tt % 4 if len(X.ss) > 4 else tt
                        ln_a(s, X, h1[hb][:, tt, :], hres, k4)
                        s.op("act", lambda e: e.activation(out=acc[hb][:, tt, :], in_=h1[hb][:, tt, :], func=AF.Copy, scale=X.ss[k4][:, 2:3]),
                             reads=[hres, "ss%d" % k4], writes=[ares])
                        s.op("pool", lambda e: e.tensor_tensor(out=acc[hb][:, tt, :], in0=acc[hb][:, tt, :], in1=gfin[:], op=ALU.mult),
                             reads=["gfin"], writes=[ares])
                        s.dma("pool", h_out[ti * 128:(ti + 1) * 128, :], acc[hb][:, tt, :], reads=[ares], writes=["HOUT"])
                    else:
                        s.dma("pool", h_out[ti * 128:(ti + 1) * 128, :], h1[hb][:, tt, :], reads=[hres], writes=["HB"])

            stage1(0)
            for tg in range(NT // 4):
                if tg + 1 < NT // 4:
                    stage1(tg + 1)
                stage2(tg)
            s.barrier()
        s.barrier()
```

```python
import math
import os
from contextlib import ExitStack

import numpy as np
import concourse.bass as bass
import concourse.mybir as mybir
from concourse.bass_utils import run_bass_kernel_spmd

F32 = mybir.dt.float32
BF16 = mybir.dt.bfloat16
AF = mybir.ActivationFunctionType
ALU = mybir.AluOpType
AX = mybir.AxisListType

D = 1024
SEQ = 8192
NT = SEQ // 128
DEPTH = 2
HD = 64
IN_COLS = 3352
D_FF = 3584
NE = 8
EPS = 1e-6
NEG = -30000.0
NGRP = 40
NSLOT = NGRP * 512
I32 = mybir.dt.int32

C_QN, C_KC, C_VC, C_KS, C_VS, C_KW, C_VW, C_GL, C_QR, C_KR, C_VR, C_GR = (
    0, 512, 640, 768, 896, 1024, 1152, 1280, 1304, 1816, 2328, 2840)


class S:
    def __init__(self, nc, es, n_dma_sems=24):
        self.nc = nc
        self.eng = {"pe": nc.tensor, "dve": nc.vector, "act": nc.scalar,
                    "pool": nc.gpsimd, "sp": nc.sync}
        self.sem = {k: es.enter_context(nc.semaphore("c_" + k)) for k in self.eng}
        self.cnt = {k: 0 for k in self.eng}
        self.seen = {k: {} for k in self.eng}
        self.drained = {k: 0 for k in self.eng}
        self.dsem = [es.enter_context(nc.semaphore("d%d" % i)) for i in range(n_dma_sems)]
        self.dval = [0] * n_dma_sems
        self.dnext = 0
        self.lastw = {}
        self.readers = {}
        self.nops = 0

    def _wait(self, eng, tok):
        if tok is None:
            return
        if tok[0] == "c":
            _, e2, c = tok
            if e2 == eng:
                if eng != "pe" and c > self.drained[eng]:
                    self.eng[eng].drain()
                    self.drained[eng] = self.cnt[eng]
                return
            key = e2
            sem = self.sem[e2]
        else:
            _, si, c = tok
            key = ("d", si)
            sem = self.dsem[si]
        if self.seen[eng].get(key, 0) >= c:
            return
        self.eng[eng].wait_ge(sem, c)
        self.seen[eng][key] = c

    def _deps(self, eng, reads, writes):
        for r in reads:
            self._wait(eng, self.lastw.get(r))
        for w in writes:
            self._wait(eng, self.lastw.get(w))
            for t in self.readers.get(w, {}).values():
                self._wait(eng, t)

    def _commit(self, tok, reads, writes):
        for r in reads:
            self.readers.setdefault(r, {})[tok[1] if tok[0] == "c" else ("d", tok[1])] = tok
        for w in writes:
            self.lastw[w] = tok
            self.readers[w] = {}

    def op(self, eng, fn, reads=(), writes=()):
        self._deps(eng, reads, writes)
        ins = fn(self.eng[eng])
        self.cnt[eng] += 1
        ins.then_inc(self.sem[eng], 1)
        self._commit(("c", eng, self.cnt[eng]), reads, writes)
        self.nops += 1

    def dma(self, q, out, in_, reads=(), writes=(), **kw):
        self._deps(q, reads, writes)
        si = self.dnext
        self.dnext = (self.dnext + 1) % len(self.dsem)
        self._wait(q, ("d", si, self.dval[si]))
        ins = self.eng[q].dma_start(out=out, in_=in_, **kw)
        self.dval[si] += 16
        ins.then_inc(self.dsem[si], 16)
        self._commit(("d", si, self.dval[si]), reads, writes)
        self.nops += 1

    def idma(self, out, out_off, in_, in_off, bound, reads=(), writes=()):
        q = "pool"
        self._deps(q, reads, writes)
        si = self.dnext
        self.dnext = (self.dnext + 1) % len(self.dsem)
        self._wait(q, ("d", si, self.dval[si]))
        oo = None if out_off is None else bass.IndirectOffsetOnAxis(ap=out_off, axis=0)
        io = None if in_off is None else bass.IndirectOffsetOnAxis(ap=in_off, axis=0)
        ins = self.nc.gpsimd.indirect_dma_start(out=out, out_offset=oo, in_=in_, in_offset=io)
        self.dval[si] += 16
        ins.then_inc(self.dsem[si], 16)
        self._commit(("d", si, self.dval[si]), reads, writes)
        self.nops += 1

    def barrier(self):
        for e in self.eng:
            for e2 in self.eng:
                if e2 != e and self.cnt[e2] > 0:
                    self._wait(e, ("c", e2, self.cnt[e2]))
            for si in range(len(self.dsem)):
                if self.dval[si] > 0:
                    self._wait(e, ("d", si, self.dval[si]))
        self.lastw = {}
        self.readers = {}

    def drain(self, eng="sp"):
        for r, t in list(self.lastw.items()):
            self._wait(eng, t)
        for r, d in list(self.readers.items()):
            for t in d.values():
                self._wait(eng, t)


def bcast_mid(ap2, n):
    return ap2.unsqueeze(2).to_broadcast([ap2.shape[0], ap2.shape[1], n])


def bcast_rep(ap2, n):
    return ap2.unsqueeze(1).to_broadcast([ap2.shape[0], n, ap2.shape[1]])


def make_consts():
    c = {}
    c["c_ident"] = np.eye(128, dtype=np.float32)
    t = np.arange(SEQ)
    slopes = 2.0 ** (-(np.arange(8) + 1.0))
    qa = np.zeros((8, 3, SEQ), np.float32)
    for h in range(8):
        qa[h, 0] = 128.0 * slopes[h]
        qa[h, 1] = slopes[h]
        qa[h, 2] = -slopes[h] * 128.0 * (t // 128 + 1)
    c["c_qaug"] = qa
    ka = np.zeros((3, SEQ), np.float32)
    ka[0] = t // 128
    ka[1] = t % 128
    ka[2] = 1.0
    c["c_kaug"] = ka
    n = np.arange(512)
    pos = 16 * n + 31
    kc = np.zeros((3, 512), np.float32)
    kc[0] = pos // 128
    kc[1] = pos % 128
    kc[2] = 1.0
    c["c_kcaug"] = kc
    hh = np.arange(8)
    lg = np.log1p(-(2.0 ** (-5.0 - hh)))
    nn = np.arange(128)
    xiT = np.zeros((4, 128, 128), np.float32)
    kdT = np.zeros((4, 128, 128), np.float32)
    gC = np.zeros((128, 4), np.float32)
    for hp in range(4):
        for half in range(2):
            h = 2 * hp + half
            xiT[hp, half * 64:(half + 1) * 64, :] = np.exp((nn + 1.0) * lg[h])[None, :]
            kdT[hp, half * 64:(half + 1) * 64, :] = 0.125 * np.exp(-(nn + 1.0) * lg[h])[None, :]
            gC[half * 64:(half + 1) * 64, hp] = np.exp(128.0 * lg[h])
    c["c_xiT"] = xiT
    c["c_kdT"] = kdT
    c["c_gC"] = gC
    c["c_gC8"] = np.tile(np.exp(128.0 * lg)[None, :], (64, 1)).astype(np.float32)
    c["c_ktab"] = (0.125 * np.exp(-(nn[:, None] + 1.0) * lg[None, :])).astype(np.float32)
    dm = (nn[None, :] >= nn[:, None]).astype(np.float32)
    c["c_rmask"] = np.tile(dm, (1, 4))
    jj = nn[:, None]
    ii = nn[None, :]
    c["c_mcausal"] = np.tile(np.where(jj > ii, NEG, 0.0).astype(np.float32), (1, 4))
    c["c_mwin4"] = np.tile(np.where(jj > ii, 0.0, NEG).astype(np.float32), (1, 4))
    pats = []
    pidx = {}
    cmp_plan = []
    for qi in range(NT):
        row = []
        for cc in range(4):
            tq = 128 * qi + ii
            nk = 128 * cc + jj
            valid = (tq - (16 * nk + 31) >= 0) & (nk <= 510)
            if valid.all():
                row.append(-1)
            elif not valid.any():
                row.append(-2)
            else:
                key = valid.tobytes()
                if key not in pidx:
                    pidx[key] = len(pats)
                    pats.append(np.tile(np.where(valid, 0.0, NEG).astype(np.float32), (1, 4)))
                row.append(pidx[key])
        cmp_plan.append(row)
    c["c_mcmp"] = np.stack(pats, 0)
    E = np.zeros((128, NT, 128), np.float32)
    for kt in range(NT):
        E[2 * kt, kt, 0:64] = 1.0
        E[2 * kt + 1, kt, 64:128] = 1.0
    c["c_E"] = E
    ncmp = np.arange(512)[:, None]
    msel = np.arange(128)[None, :]
    ov = ((np.minimum(16 * ncmp + 32, 64 * msel + 64) > np.maximum(16 * ncmp, 64 * msel))
          & (ncmp <= 510)).astype(np.float32)
    c["c_ov"] = ov
    AB = np.zeros((NT, 128, 256), np.float32)
    for qi in range(NT):
        tq = 128 * qi + nn[:, None]
        back = tq // 64 - msel
        forced = (msel == 0) | ((back >= 0) & (back < 2))
        A = ((back >= 0) & (~forced)).astype(np.float32)
        B = np.where(forced, 1.0e9 + 1024.0 * msel, np.where(back >= 0, 0.0, -1.0)).astype(np.float32)
        AB[qi, :, 0:128] = A
        AB[qi, :, 128:256] = B
    c["c_AB"] = AB
    c["c_gi512"] = np.tile((512.0 * np.arange(NGRP))[None, :], (128, 1)).astype(np.float32)
    c["c_tokid"] = (np.arange(NT)[None, :] * 128 + np.arange(128)[:, None]).astype(np.float32)
    c["c_U"] = (nn[:, None] < nn[None, :]).astype(np.float32)
    c["c_blkp"] = (np.arange(7)[None, :] * 128 + np.arange(128)[:, None]).astype(np.float32)
    return c, cmp_plan


_CONSTS = None


def get_consts():
    global _CONSTS
    if _CONSTS is None:
        _CONSTS = make_consts()
    return _CONSTS


class Ctx:
    def __getattr__(self, k):
        if k in WEIGHT_SPECS:
            ap = self.nc.dram_tensor(k, WEIGHT_SPECS[k], F32, kind="ExternalInput").ap()
            self.__dict__[k] = ap
            self.used.append(k)
            return ap
        raise AttributeError(k)


def ln_a(s, X, src, src_res, k):
    junk, ss, xs = X.junk, X.ss[k], X.xs[k]
    s.op("act", lambda e: e.activation(out=junk[:], in_=src, func=AF.Square, accum_out=ss[:, 0:1]),
         reads=[src_res], writes=["junk", "ss%d" % k])
    s.op("dve", lambda e: e.tensor_scalar(out=ss[:, 1:2], in0=ss[:, 0:1], scalar1=1.0 / D, scalar2=EPS,
                                          op0=ALU.mult, op1=ALU.add), reads=["ss%d" % k], writes=["ss%d" % k])
    s.op("act", lambda e: e.activation(out=ss[:, 1:2], in_=ss[:, 1:2], func=AF.Sqrt),
         reads=["ss%d" % k], writes=["ss%d" % k])
    s.op("dve", lambda e: e.reciprocal(out=ss[:, 2:3], in_=ss[:, 1:2]), reads=["ss%d" % k], writes=["ss%d" % k])
    s.op("act", lambda e: e.activation(out=xs[:], in_=src, func=AF.Copy, scale=ss[:, 2:3]),
         reads=[src_res, "ss%d" % k], writes=["xs%d" % k])


def ln_b(s, X, k, gT, gT_res, dst, dst_res, src=None, src_res=None):
    xs = X.xs[k] if src is None else src
    xres = ("xs%d" % k) if src is None else src_res
    psb = X.ps[7][:].bitcast(BF16)
    for kc in range(8):
        s.op("pe", lambda e: e.transpose(out=psb[:, kc * 128:(kc + 1) * 128], in_=xs[:, kc * 128:(kc + 1) * 128],
                                         identity=X.ident[:]), reads=[xres, "ident"], writes=["ps7"])
    s.op("dve", lambda e: e.tensor_tensor(out=dst, in0=psb.rearrange("p (k t) -> p k t", k=8),
                                          in1=bcast_mid(gT, 128), op=ALU.mult),
         reads=["ps7", gT_res], writes=[dst_res])


def phase_proj(s, X, L, h_in):
    nc = X.nc
    with ExitStack() as es:
        sb = lambda name, shape, dt: es.enter_context(nc.sbuf_tensor("%s_L%d" % (name, L), shape, dt))
        wsb = sb("p1_w", [128, 8, IN_COLS], BF16)
        gT = sb("p1_gT", [128, 8], F32)
        ht = [sb("p1_ht%d" % i, [128, D], F32) for i in range(4)]
        hnT = [sb("p1_hnT%d" % i, [128, 8, 512], BF16) for i in range(2)]
        fm = [sb("p1_fm%d" % i, [128, 16, 512], BF16) for i in range(2)]
        vsx = [sb("p1_vsx%d" % i, [128, 4, 2, 65], BF16) for i in range(2)]
        vwx = [sb("p1_vwx%d" % i, [128, 4, 2, 65], BF16) for i in range(2)]
        gls = [sb("p1_gls%d" % i, [128, 4, 24], F32) for i in range(2)]
        krm = [sb("p1_krm%d" % i, [128, 4, 512], BF16) for i in range(2)]
        vrm = [sb("p1_vrm%d" % i, [128, 4, 512], BF16) for i in range(2)]
        grm = [sb("p1_grm%d" % i, [128, 4, 512], BF16) for i in range(2)]
        sg = sb("p1_sg", [128, 512], F32)
        xiT = sb("p1_xiT", [128, 4, 128], F32)
        kdT = sb("p1_kdT", [128, 4, 128], F32)
        ktab = sb("p1_ktab", [128, 8], F32)
        s.dma("sp", xiT[:], X.c["c_xiT"].rearrange("j p t -> p j t"), writes=["xiT"])
        s.dma("sp", kdT[:], X.c["c_kdT"].rearrange("j p t -> p j t"), writes=["kdT"])
        s.dma("sp", ktab[:], X.c["c_ktab"], writes=["ktab"])
        s.dma("sp", gT[:], X.g_mix[L].rearrange("(k p) -> p k", p=128), writes=["p1gT"],
              allow_slow_non_contiguous=True)
        for kc in range(8):
            s.dma("pool", wsb[:, kc, :], X.w_in[L, kc * 128:(kc + 1) * 128, :], writes=["p1w"])
        for i in range(2):
            s.op("pool", lambda e: e.memset(vsx[i][:, :, :, 64:65], 1.0), writes=["vsx%d" % i])
            s.op("pool", lambda e: e.memset(vwx[i][:, :, :, 64:65], 1.0), writes=["vwx%d" % i])
        fm_cols = [C_QN, C_QN + 128, C_QN + 256, C_QN + 384, C_KC, C_VC, C_KS, C_KW,
                   C_QR, C_QR + 128, C_QR + 256, C_QR + 384, C_KR, C_KR + 128, C_KR + 256, C_KR + 384]
        def emit_a(tg):
            for tt in range(4):
                ti = 4 * tg + tt
                s.dma("sp", ht[tt][:], h_in[ti * 128:(ti + 1) * 128, :], writes=["p1ht%d" % tt])
                ln_a(s, X, ht[tt][:], "p1ht%d" % tt, tt)

        def emit_b(tg):
            for tt in range(4):
                ln_b(s, X, tt, gT[:], "p1gT", hnT[tg % 2][:, :, tt * 128:(tt + 1) * 128], "hnT%d" % (tg % 2))

        emit_a(0)
        emit_b(0)
        for tg in range(NT // 4):
            b = tg % 2
            toks = slice(tg * 512, (tg + 1) * 512)
            if tg + 1 < NT // 4:
                emit_a(tg + 1)
            for ci, c0 in enumerate(fm_cols):
                pb = ci % 2
                ps = X.ps[pb]
                for kc in range(8):
                    s.op("pe", lambda e: e.matmul(ps[:], lhsT=wsb[:, kc, c0:c0 + 128], rhs=hnT[b][:, kc, :],
                                                  start=(kc == 0), stop=(kc == 7)),
                         reads=["p1w", "hnT%d" % b], writes=["ps%d" % pb])
                dst = fm[b][:, ci, :]
                if ci < 4:
                    s.op("act", lambda e: e.activation(out=dst, in_=ps[:], func=AF.Copy, scale=0.125),
                         reads=["ps%d" % pb], writes=["fm%d" % b])
                elif ci < 8:
                    eng = "act" if ci % 2 == 0 else "dve"
                    if eng == "act":
                        s.op("act", lambda e: e.activation(out=dst, in_=ps[:], func=AF.Copy),
                             reads=["ps%d" % pb], writes=["fm%d" % b])
                    else:
                        s.op("dve", lambda e: e.tensor_copy(out=dst, in_=ps[:]),
                             reads=["ps%d" % pb], writes=["fm%d" % b])
                else:
                    tab = xiT if ci < 12 else kdT
                    j = (ci - 8) % 4
                    s.op("dve", lambda e: e.tensor_tensor(
                        out=dst.rearrange("p (a t) -> p a t", a=4), in0=ps[:].rearrange("p (a t) -> p a t", a=4),
                        in1=bcast_rep(tab[:, j, :], 4), op=ALU.mult),
                        reads=["ps%d" % pb, "xiT", "kdT"], writes=["fm%d" % b])
            for tt in range(4):
                lhs = lambda kc: hnT[b][:, kc, tt * 128:(tt + 1) * 128]
                groups = [(2, C_VS, 408), (3, C_KR, 512), (4, C_VR, 512), (5, C_GR, 512)]
                for (pi, c0, n) in groups:
                    for kc in range(8):
                        s.op("pe", lambda e: e.matmul(X.ps[pi][:, 0:n], lhsT=lhs(kc), rhs=wsb[:, kc, c0:c0 + n],
                                                      start=(kc == 0), stop=(kc == 7)),
                             reads=["p1w", "hnT%d" % b], writes=["ps%d" % pi])
                pA = X.ps[2]
                s.op("dve", lambda e: e.tensor_copy(out=vsx[b][:, tt, :, 0:64],
                                                    in_=pA[:, 0:128].rearrange("p (g d) -> p g d", g=2)),
                     reads=["ps2"], writes=["vsx%d" % b])
                s.op("dve", lambda e: e.tensor_copy(out=vwx[b][:, tt, :, 0:64],
                                                    in_=pA[:, 256:384].rearrange("p (g d) -> p g d", g=2)),
                     reads=["ps2"], writes=["vwx%d" % b])
                s.op("act", lambda e: e.activation(out=gls[b][:, tt, :], in_=pA[:, 384:408], func=AF.Sigmoid),
                     reads=["ps2"], writes=["gls%d" % b])
                s.op("dve", lambda e: e.tensor_tensor(
                    out=krm[b][:, tt, :].rearrange("p (h d) -> p h d", h=8),
                    in0=X.ps[3][:].rearrange("p (h d) -> p h d", h=8),
                    in1=bcast_mid(ktab[:], 64), op=ALU.mult),
                    reads=["ps3", "ktab"], writes=["krm%d" % b])
                s.op("act", lambda e: e.activation(out=vrm[b][:, tt, :], in_=X.ps[4][:], func=AF.Copy),
                     reads=["ps4"], writes=["vrm%d" % b])
                s.op("act", lambda e: e.activation(out=sg[:], in_=X.ps[5][:], func=AF.Sigmoid),
                     reads=["ps5"], writes=["p1sg"])
                s.op("dve", lambda e: e.tensor_tensor(out=grm[b][:, tt, :], in0=X.ps[5][:], in1=sg[:], op=ALU.mult),
                     reads=["ps5", "p1sg"], writes=["grm%d" % b])
            if tg + 1 < NT // 4:
                emit_b(tg + 1)
            s.dma("pool", X.QTP[:, :, toks].rearrange("j p t -> p j t"), fm[b][:, 0:4, :], reads=["fm%d" % b], writes=["QTP"])
            s.dma("pool", X.KCP[:, toks], fm[b][:, 4, :], reads=["fm%d" % b], writes=["KCP"])
            s.dma("pool", X.VCP[:, toks], fm[b][:, 5, :], reads=["fm%d" % b], writes=["VCP"])
            s.dma("pool", X.KSP[:, toks], fm[b][:, 6, :], reads=["fm%d" % b], writes=["KSP"])
            s.dma("pool", X.KWP[:, toks], fm[b][:, 7, :], reads=["fm%d" % b], writes=["KWP"])
            s.dma("pool", X.QRT[:, :, toks].rearrange("j p t -> p j t"), fm[b][:, 8:12, :], reads=["fm%d" % b], writes=["QRT"])
            s.dma("pool", X.KRT[:, :, toks].rearrange("j p t -> p j t"), fm[b][:, 12:16, :], reads=["fm%d" % b], writes=["KRT"])
            s.dma("pool", X.VSX[toks].rearrange("(tt p) g c -> p tt g c", p=128), vsx[b][:], reads=["vsx%d" % b], writes=["VSX"])
            s.dma("pool", X.VWX[toks].rearrange("(tt p) g c -> p tt g c", p=128), vwx[b][:], reads=["vwx%d" % b], writes=["VWX"])
            s.dma("pool", X.GLS[toks].rearrange("(tt p) c -> p tt c", p=128), gls[b][:], reads=["gls%d" % b], writes=["GLS"])
            s.dma("pool", X.KRM[toks].rearrange("(tt p) c -> p tt c", p=128), krm[b][:], reads=["krm%d" % b], writes=["KRM"])
            s.dma("pool", X.VRM[toks].rearrange("(tt p) c -> p tt c", p=128), vrm[b][:], reads=["vrm%d" % b], writes=["VRM"])
            s.dma("pool", X.GRM[toks].rearrange("(tt p) c -> p tt c", p=128), grm[b][:], reads=["grm%d" % b], writes=["GRM"])
        s.barrier()


WEIGHT_SPECS = {
    "w_in": [DEPTH, D, IN_COLS], "w_out": [DEPTH, D, D], "g_mix": [DEPTH, D], "g_ffn": [DEPTH, D],
    "g_ple": [DEPTH, D], "g_final": [D], "cmp_pos": [DEPTH, 2, 32, 64], "cmp_w1": [DEPTH, 2, 32, 64, 256],
    "cmp_w2": [DEPTH, 2, 256, 64], "ret_gn": [DEPTH, 512], "ffn_gate": [1, D, D_FF], "ffn_up": [1, D, D_FF],
    "ffn_down": [1, D_FF, D], "moe_router": [1, D, NE], "moe_gate": [1, NE, D, D_FF], "moe_up": [1, NE, D, D_FF],
    "moe_down": [1, NE, D_FF, D], "ple_proj": [DEPTH, 256, D], "ple_gate": [DEPTH, D, D],
}

SCRATCH_SPECS = {
    "QTP": ([4, 128, SEQ], BF16), "QAUG": ([8, 3, SEQ], BF16), "KAUG": ([3, SEQ], BF16), "KCAUG": ([3, 512], BF16),
    "KSP": ([128, SEQ], BF16), "KWP": ([128, SEQ], BF16), "KCP": ([128, SEQ], BF16), "VCP": ([128, SEQ], BF16),
    "VSX": ([SEQ, 2, 65], BF16), "VWX": ([SEQ, 2, 65], BF16), "GLS": ([SEQ, 24], F32),
    "QRT": ([4, 128, SEQ], BF16), "KRT": ([4, 128, SEQ], BF16),
    "KRM": ([SEQ, 512], BF16), "VRM": ([SEQ, 512], BF16), "GRM": ([SEQ, 512], BF16),
    "AT": ([D, SEQ], BF16), "HB": ([SEQ, D], F32),
    "FG16": ([1, D, D_FF], BF16), "FU16": ([1, D, D_FF], BF16), "FD16": ([1, D_FF, D], BF16),
    "WGB": ([NE * 7 * 128, 4096], BF16), "WUB": ([NE * 7 * 128, 4096], BF16), "WDB": ([NE * 7 * 128, 4096], BF16),
    "H1": ([SEQ, D], F32), "XS2": ([SEQ, D], BF16), "SLOT": ([NSLOT, 2], I32), "YS": ([NSLOT, D], F32),
    "DBG_KCMP": ([67, 2, 512], BF16), "DBG_VCX": ([128, 2, 4, 193], BF16),
}


def build(stop_after=None, debug=(), nsa_tiles=NT):
    nc = bass.Bass("TRN2", target_bir_lowering=False)
    X = Ctx()
    X.nc = nc
    consts, cmp_plan = get_consts()
    X.cmp_plan = cmp_plan
    X.x = nc.dram_tensor("x", [SEQ, D], F32, kind="ExternalInput").ap()
    X.p = nc.dram_tensor("p", [DEPTH, SEQ, 256], F32, kind="ExternalInput").ap()
    X.used = []
    X.dbg_cmp = "DBG_KCMP" in debug
    X.nsa_tiles = nsa_tiles
    X.routed = True
    X.conv_layers = [0] if (stop_after is not None and stop_after[0] == 0) else [0, 1]
    X.ret_chunks = int(os.environ.get('RET_CHUNKS', NT))
    X.c = {k: nc.dram_tensor(k, list(v.shape), F32, kind="ExternalInput").ap() for k, v in consts.items()}
    X.out = nc.dram_tensor("out", [SEQ, D], F32, kind="ExternalOutput").ap()
    for k, (shp, dt) in SCRATCH_SPECS.items():
        kind = "ExternalOutput" if k in debug else "Internal"
        setattr(X, k, nc.dram_tensor(k, shp, dt, kind=kind).ap())
    with ExitStack() as es:
        s = S(nc, es)
        X.ps = [es.enter_context(nc.psum_tensor("ps%d" % i, [128, 512], F32)) for i in range(8)]
        X.ident = es.enter_context(nc.sbuf_tensor("ident", [128, 128], BF16))
        X.junk = es.enter_context(nc.sbuf_tensor("junk", [128, D], F32))
        X.ss = [es.enter_context(nc.sbuf_tensor("ss%d" % i, [128, 4], F32)) for i in range(8)]
        X.xs = [es.enter_context(nc.sbuf_tensor("xs%d" % i, [128, D], BF16)) for i in range(8)]
        s.dma("pool", X.ident[:], X.c["c_ident"], writes=["ident"])
        s.dma("pool", X.QAUG, X.c["c_qaug"], writes=["QAUG"])
        s.dma("pool", X.KAUG, X.c["c_kaug"], writes=["KAUG"])
        s.dma("pool", X.KCAUG, X.c["c_kcaug"], writes=["KCAUG"])
        done = False
        for L in range(DEPTH):
            h_in = X.x if L == 0 else X.HB
            for name, fn in PHASES:
                fn(s, X, L, h_in)

                if stop_after == (L, name):
                    done = True
                    break
            if done:
                break
        s.barrier()
        s.drain("sp")
    X.nops = s.nops
    return nc, X


PHASES = [("proj", phase_proj)]

_BUILT = {}


def run(inputs, stop_after=None, debug=(), cores=8, trace=False, nsa_tiles=NT):
    key = (stop_after, tuple(debug), nsa_tiles)
    if key not in _BUILT:
        _BUILT[key] = build(stop_after, debug, nsa_tiles)
    nc, X = _BUILT[key]
    consts, _ = get_consts()
    in_maps = []
    for b in range(cores):
        m = {"x": np.ascontiguousarray(inputs["x"][b]), "p": np.ascontiguousarray(inputs["p"][:, b])}
        for k in X.used:
            m[k] = np.ascontiguousarray(inputs[k])
        m.update(consts)
        in_maps.append(m)
    return run_bass_kernel_spmd(nc, in_maps, core_ids=list(range(cores)), trace=trace)


def kernel(**inputs):
    inputs = {k: np.asarray(v) for k, v in inputs.items()}
    res = run(inputs)
    return np.stack([r["out"] for r in res.results], 0).astype(np.float32)


def phase_nsa(s, X, L, h_in):
    nc = X.nc
    with ExitStack() as es:
        sb = lambda name, shape, dt: es.enter_context(nc.sbuf_tensor("%s_L%d" % (name, L), shape, dt))
        KCMP = sb("n_kcmp", [67, 2, 512], BF16)
        VCX = sb("n_vcx", [128, 2, 4, 193], BF16)
        s.op("pool", lambda e: e.memset(KCMP[:], 0.0), writes=["KCMP"])
        s.op("pool", lambda e: e.memset(VCX[:], 0.0), writes=["VCX"])
        s.op("pool", lambda e: e.memset(VCX[:, :, :, 64:65], 1.0), writes=["VCX"])
        for g in range(2):
            s.dma("pool", VCX[:, g, :, 65:193], X.c["c_ov"].rearrange("(ct p) m -> p ct m", p=128), writes=["VCX"])
            s.dma("sp", KCMP[64:67, g, :], X.KCAUG, reads=["KCAUG"], writes=["KCMP"])
        with ExitStack() as es2:
            sb2 = lambda name, shape, dt: es2.enter_context(nc.sbuf_tensor("%s_L%d" % (name, L), shape, dt))
            kcT = [sb2("c_kcT%d" % i, [64, SEQ], BF16) for i in range(2)]
            w1 = sb2("c_w1", [64, 32, 256], BF16)
            posT = sb2("c_posT", [64, 32], BF16)
            w2 = sb2("c_w2", [128, 2, 64], BF16)
            hb = sb2("c_hb", [128, 2], F32)
            u = sb2("c_u", [128, 2, 512], F32)
            t1 = sb2("c_t1", [128, 2, 512], F32)
            sg = sb2("c_sg", [128, 2, 512], F32)
            gel = sb2("c_gel", [128, 2, 512], BF16)
            it = 0
            for kv in range(2):
                s.dma("pool", w1[:], X.cmp_w1[L, kv].rearrange("l d h -> d l h"), writes=["c_w1"])
                s.dma("pool", posT[:], X.cmp_pos[L, kv].rearrange("l d -> d l"), writes=["c_posT"],
                      allow_slow_non_contiguous=True)
                s.dma("pool", w2[:], X.cmp_w2[L, kv].rearrange("(c p) d -> p c d", p=128), writes=["c_w2"])
                src = X.KCP if kv == 0 else X.VCP
                for g in range(2):
                    kt_ = kcT[it % 2]
                    kres = "c_kcT%d" % (it % 2)
                    it += 1
                    s.dma("sp", kt_[:], src[g * 64:(g + 1) * 64, :], reads=["KCP", "VCP"], writes=[kres])
                    kview = kt_[:].rearrange("d (n c) -> d n c", c=16)
                    for hc in range(2):
                        ps = X.ps[hc]
                        for l in range(32):
                            a, c_ = l // 16, l % 16
                            s.op("pe", lambda e: e.matmul(ps[:, 0:511], lhsT=w1[:, l, hc * 128:(hc + 1) * 128],
                                                          rhs=kview[:, a:a + 511, c_], start=(l == 0), stop=(l == 31)),
                                 reads=["c_w1", kres], writes=["ps%d" % hc])
                        for l in range(32):
                            s.op("pe", lambda e: e.matmul(X.ps[2][:, hc:hc + 1], lhsT=w1[:, l, hc * 128:(hc + 1) * 128],
                                                          rhs=posT[:, l:l + 1], start=(l == 0), stop=(l == 31)),
                                 reads=["c_w1", "c_posT"], writes=["ps2"])
                        s.op("dve", lambda e: e.tensor_copy(out=hb[:, hc:hc + 1], in_=X.ps[2][:, hc:hc + 1]),
                             reads=["ps2"], writes=["c_hb"])
                        s.op("act", lambda e: e.activation(out=u[:, hc, 0:511], in_=ps[:, 0:511], func=AF.Identity,
                                                           bias=hb[:, hc:hc + 1]),
                             reads=["ps%d" % hc, "c_hb"], writes=["c_u"])
                    uu = u[:, :, 0:511]
                    s.op("pool", lambda e: e.tensor_tensor(out=t1[:, :, 0:511], in0=uu, in1=uu, op=ALU.mult),
                         reads=["c_u"], writes=["c_t1"])
                    s.op("dve", lambda e: e.tensor_scalar(out=t1[:, :, 0:511], in0=t1[:, :, 0:511], scalar1=0.044715,
                                                          scalar2=1.0, op0=ALU.mult, op1=ALU.add),
                         reads=["c_t1"], writes=["c_t1"])
                    s.op("dve", lambda e: e.tensor_tensor(out=t1[:, :, 0:511], in0=t1[:, :, 0:511], in1=uu, op=ALU.mult),
                         reads=["c_t1", "c_u"], writes=["c_t1"])
                    s.op("act", lambda e: e.activation(out=sg[:, :, 0:511], in_=t1[:, :, 0:511], func=AF.Sigmoid,
                                                       scale=1.5957691216057308),
                         reads=["c_t1"], writes=["c_sg"])
                    s.op("dve", lambda e: e.tensor_tensor(out=gel[:, :, 0:511], in0=sg[:, :, 0:511], in1=uu, op=ALU.mult),
                         reads=["c_sg", "c_u"], writes=["c_gel"])
                    if kv == 0:
                        for hc in range(2):
                            s.op("pe", lambda e: e.matmul(X.ps[3][0:64, 0:511], lhsT=w2[:, hc, :], rhs=gel[:, hc, 0:511],
                                                          start=(hc == 0), stop=(hc == 1)),
                                 reads=["c_w2", "c_gel"], writes=["ps3"])
                        s.op("act", lambda e: e.activation(out=KCMP[0:64, g, 0:511], in_=X.ps[3][0:64, 0:511], func=AF.Copy),
                             reads=["ps3"], writes=["KCMP"])
                    else:
                        for ct in range(4):
                            nn = 128 if ct < 3 else 127
                            for hc in range(2):
                                s.op("pe", lambda e: e.matmul(X.ps[3][0:nn, ct * 64:(ct + 1) * 64],
                                                              lhsT=gel[:, hc, ct * 128:ct * 128 + nn], rhs=w2[:, hc, :],
                                                              start=(hc == 0), stop=(hc == 1)),
                                     reads=["c_w2", "c_gel"], writes=["ps3"])
                        for ct in range(4):
                            nn = 128 if ct < 3 else 127
                            s.op("act", lambda e: e.activation(out=VCX[0:nn, g, ct, 0:64],
                                                               in_=X.ps[3][0:nn, ct * 64:(ct + 1) * 64], func=AF.Copy),
                                 reads=["ps3"], writes=["VCX"])
            s.barrier()
        if X.dbg_cmp:
            s.dma("sp", X.DBG_KCMP, KCMP[:], reads=["KCMP"])
            s.dma("sp", X.DBG_VCX, VCX[:], reads=["VCX"])
        KS = sb("n_ks", [67, 2, SEQ], BF16)
        KW = sb("n_kw", [67, 2, SEQ], BF16)
        VS = sb("n_vs", [128, NT, 2, 65], BF16)
        VW = sb("n_vw", [128, NT, 2, 65], BF16)
        E = sb("n_E", [128, NT, 128], BF16)
        mca = sb("n_mca", [128, 512], BF16)
        mw4 = sb("n_mw4", [128, 512], BF16)
        npat = X.c["c_mcmp"].shape[0]
        mcmp = sb("n_mcmp", [128, npat, 512], BF16)
        qt = [sb("n_qt%d" % i, [67, 1024], BF16) for i in range(2)]
        AB = [sb("n_AB%d" % i, [128, 256], F32) for i in range(2)]
        glt = [sb("n_gl%d" % i, [128, 24], F32) for i in range(2)]
        PT = [sb("n_PT%d" % i, [128, 512], BF16) for i in range(4)]
        negT = [sb("n_negT%d" % i, [128, 512], BF16) for i in range(2)]
        acc = [sb("n_acc%d" % i, [128, 512], F32) for i in range(2)]
        accb = [sb("n_accb%d" % i, [128, 512], BF16) for i in range(2)]
        aT = [sb("n_aT%d" % i, [128, 4, 128], BF16) for i in range(2)]
        sm = [sb("n_sm%d" % i, [128, 64], F32) for i in range(2)]
        sc = [sb("n_sc%d" % i, [128, 128], F32) for i in range(2)]
        sc2 = [sb("n_sc2%d" % i, [128, 128], F32) for i in range(2)]
        seln = [sb("n_seln%d" % i, [128, 128], BF16) for i in range(2)]
        s.dma("sp", KS[0:64, :, :], X.KSP.rearrange("(g d) t -> d g t", g=2), reads=["KSP"], writes=["KS"])
        s.dma("sp", KW[0:64, :, :], X.KWP.rearrange("(g d) t -> d g t", g=2), reads=["KWP"], writes=["KW"])
        for g in range(2):
            s.dma("sp", KS[64:67, g, :], X.KAUG, reads=["KAUG"], writes=["KS"])
            s.dma("sp", KW[64:67, g, :], X.KAUG, reads=["KAUG"], writes=["KW"])
        for q4 in range(4):
            tsl = slice(q4 * 2048, (q4 + 1) * 2048)
            ksl = slice(q4 * 16, (q4 + 1) * 16)
            s.dma("sp", VS[:, ksl], X.VSX[tsl].rearrange("(kt p) g c -> p kt g c", p=128), reads=["VSX"], writes=["VS"])
            s.dma("sp", VW[:, ksl], X.VWX[tsl].rearrange("(kt p) g c -> p kt g c", p=128), reads=["VWX"], writes=["VW"])
            s.dma("pool", E[:, ksl, :], X.c["c_E"][:, ksl, :], writes=["E"])
        s.dma("pool", mca[:], X.c["c_mcausal"], writes=["mca"])
        s.dma("pool", mw4[:], X.c["c_mwin4"], writes=["mw4"])
        s.dma("pool", mcmp[:], X.c["c_mcmp"].rearrange("n p c -> p n c"), writes=["mcmp"])
        st = {"sb": 0, "pt": 0}

        def qk_tile(g, qb, lhsT, lres, extra, mask):
            pi = st["sb"] % 3
            st["sb"] += 1
            ps = X.ps[pi]
            pres = "ps%d" % pi
            nmm = 1 + (extra is not None) + (mask is not None)
            s.op("pe", lambda e: e.matmul(ps[:], lhsT=lhsT, rhs=qt[qb][:, g * 512:(g + 1) * 512], start=True,
                                          stop=(nmm == 1)), reads=[lres, "qt%d" % qb], writes=[pres])
            k = 1
            if extra is not None:
                el, er, eres = extra
                s.op("pe", lambda e: e.matmul(ps[:], lhsT=el, rhs=er, start=False, stop=(k + 1 == nmm)),
                     reads=eres, writes=[pres])
                k += 1
            if mask is not None:
                ml, mres = mask
                s.op("pe", lambda e: e.matmul(ps[:], lhsT=X.ident[:], rhs=ml, start=False, stop=True),
                     reads=["ident", mres], writes=[pres])
            pk = st["pt"] % 4
            st["pt"] += 1
            s.op("act", lambda e: e.activation(out=PT[pk][:], in_=ps[:], func=AF.Exp),
                 reads=[pres], writes=["PT%d" % pk])
            return PT[pk], "PT%d" % pk

        def coef_and_acc(g, qb, branch, heads_ap, heads_res, first):
            smt = sm[g]
            sres = "sm%d" % g
            for r in range(4):
                o_ap = heads_ap(r)
                s.op("dve", lambda e: e.tensor_scalar(out=smt[:, r:r + 1], in0=o_ap[:, 64:65], scalar1=1e-36,
                                                      scalar2=None, op0=ALU.max),
                     reads=heads_res, writes=[sres])
            s.op("dve", lambda e: e.reciprocal(out=smt[:, 4:8], in_=smt[:, 0:4]), reads=[sres], writes=[sres])
            c0 = branch * 8 + g * 4
            s.op("dve", lambda e: e.tensor_tensor(out=smt[:, 8:12], in0=smt[:, 4:8], in1=glt[qb][:, c0:c0 + 4],
                                                  op=ALU.mult), reads=[sres, "gl%d" % qb], writes=[sres])
            for r in range(4):
                o_ap = heads_ap(r)
                dst = acc[qb][:, (g * 4 + r) * 64:(g * 4 + r + 1) * 64]
                if first:
                    s.op("dve", lambda e: e.tensor_scalar(out=dst, in0=o_ap[:, 0:64], scalar1=smt[:, 8 + r:9 + r],
                                                          scalar2=None, op0=ALU.mult),
                         reads=heads_res + [sres], writes=["acc%d" % qb])
                else:
                    s.op("dve", lambda e: e.scalar_tensor_tensor(out=dst, in0=o_ap[:, 0:64], scalar=smt[:, 8 + r:9 + r],
                                                                 in1=dst, op0=ALU.mult, op1=ALU.add),
                         reads=heads_res + [sres], writes=["acc%d" % qb])

        def emit_loads(qi):
            qb = qi % 2
            t0 = qi * 128
            for j in range(4):
                s.dma("sp", qt[qb][0:64, :].rearrange("d (j two t) -> d j two t", j=4, two=2)[:, j],
                      X.QTP[j, :, t0:t0 + 128].rearrange("(two d) t -> d two t", two=2),
                      reads=["QTP"], writes=["qt%d" % qb])
            s.dma("sp", qt[qb][64:67, :].rearrange("r (h t) -> r h t", h=8),
                  X.QAUG[:, :, t0:t0 + 128].rearrange("h r t -> r h t"), reads=["QAUG"], writes=["qt%d" % qb])
            s.dma("sp", AB[qb][:], X.c["c_AB"][qi], writes=["AB%d" % qb])
            s.dma("sp", glt[qb][:], X.GLS[t0:t0 + 128, :], reads=["GLS"], writes=["gl%d" % qb])

        def cmp_post_a(qi, g):
            qb = qi % 2
            ocb = lambda r: X.ps[3 + r // 2][:, (r % 2) * 193:(r % 2) * 193 + 193]
            coef_and_acc(g, qb, 0, ocb, ["ps3", "ps4"], True)
            smt = sm[g]
            sres = "sm%d" % g
            for r in range(4):
                if r == 0:
                    s.op("dve", lambda e: e.tensor_scalar(out=sc[g][:], in0=ocb(r)[:, 65:193], scalar1=smt[:, 4:5],
                                                          scalar2=None, op0=ALU.mult),
                         reads=["ps3", "ps4", sres], writes=["sc%d" % g])
                else:
                    s.op("dve", lambda e: e.scalar_tensor_tensor(out=sc[g][:], in0=ocb(r)[:, 65:193],
                                                                 scalar=smt[:, 4 + r:5 + r], in1=sc[g][:],
                                                                 op0=ALU.mult, op1=ALU.add),
                         reads=["ps3", "ps4", sres], writes=["sc%d" % g])
            s.op("dve", lambda e: e.tensor_tensor(out=sc[g][:], in0=sc[g][:], in1=AB[qb][:, 0:128], op=ALU.mult),
                 reads=["AB%d" % qb], writes=["sc%d" % g])
            s.op("dve", lambda e: e.tensor_tensor(out=sc[g][:], in0=sc[g][:], in1=AB[qb][:, 128:256], op=ALU.add),
                 reads=["AB%d" % qb], writes=["sc%d" % g])
            s.op("dve", lambda e: e.max(out=smt[:, 16:24], in_=sc[g][:]), reads=["sc%d" % g], writes=[sres])
            s.op("dve", lambda e: e.match_replace(out=sc2[g][:], in_to_replace=smt[:, 16:24], in_values=sc[g][:],
                                                  imm_value=-2.0), reads=["sc%d" % g, sres], writes=["sc2%d" % g])
            s.op("dve", lambda e: e.max(out=smt[:, 24:32], in_=sc2[g][:]), reads=["sc2%d" % g], writes=[sres])
            s.op("dve", lambda e: e.tensor_scalar(out=seln[g][:], in0=sc[g][:], scalar1=smt[:, 31:32], scalar2=NEG,
                                                  op0=ALU.is_lt, op1=ALU.mult),
                 reads=["sc%d" % g, sres], writes=["seln%d" % g])

        def cmp_post_b(qi, g):
            psb = X.ps[7][:].bitcast(BF16)
            s.op("pe", lambda e: e.transpose(out=psb[:, g * 128:(g + 1) * 128], in_=seln[g][:], identity=X.ident[:]),
                 reads=["seln%d" % g, "ident"], writes=["ps7"])
            s.op("dve", lambda e: e.tensor_copy(out=negT[g][:].rearrange("p (a t) -> p a t", a=4),
                                                in_=bcast_rep(psb[:, g * 128:(g + 1) * 128], 4)),
                 reads=["ps7"], writes=["negT%d" % g])

        def write_out(qi):
            qb = qi % 2
            t0 = qi * 128
            s.op("act", lambda e: e.activation(out=accb[qb][:], in_=acc[qb][:], func=AF.Copy),
                 reads=["acc%d" % qb], writes=["accb%d" % qb])
            psb = X.ps[7][:].bitcast(BF16)
            for j in range(4):
                s.op("pe", lambda e: e.transpose(out=psb[:, 256 + j * 128:256 + (j + 1) * 128],
                                                 in_=accb[qb][:, j * 128:(j + 1) * 128], identity=X.ident[:]),
                     reads=["accb%d" % qb, "ident"], writes=["ps7"])
            s.op("act", lambda e: e.activation(out=aT[qb][:].rearrange("p j t -> p (j t)"), in_=psb[:, 256:768], func=AF.Copy),
                 reads=["ps7"], writes=["aT%d" % qb])
            s.dma("sp", X.AT[0:512, t0:t0 + 128].rearrange("(j p) t -> p j t", p=128), aT[qb][:],
                  reads=["aT%d" % qb], writes=["AT"])

        def pv_fn(out_ap, out_res, rhs, rhs_res, first, lastt, pair_start):
            def f(pt, ptres):
                for r in range(4):
                    st_ = first and ((r % 2 == 0) if pair_start else (r == 0))
                    s.op("pe", lambda e: e.matmul(out_ap(r), lhsT=pt[:, r * 128:(r + 1) * 128], rhs=rhs,
                                                  start=st_, stop=lastt), reads=[ptres, rhs_res], writes=out_res(r))
            return f

        jobs = []
        for qi in range(X.nsa_tiles):
            qb = qi % 2
            first_job = len(jobs)
            plan = X.cmp_plan[qi]
            cs = [c for c in range(4) if plan[c] != -2]
            ocb = lambda r: X.ps[3 + r // 2][:, (r % 2) * 193:(r % 2) * 193 + 193]
            ocres = lambda r: ["ps%d" % (3 + r // 2)]
            for g in range(2):
                for ci, c in enumerate(cs):
                    mask = None if plan[c] == -1 else (mcmp[:, plan[c], :], "mcmp")
                    jobs.append(dict(
                        qk=(lambda g=g, qb=qb, c=c, mask=mask: qk_tile(g, qb, KCMP[:, g, c * 128:(c + 1) * 128], "KCMP", None, mask)),
                        pv=pv_fn(ocb, ocres, VCX[:, g, c, :], "VCX", ci == 0, ci == len(cs) - 1, True),
                        post=((lambda qi=qi, g=g: (cmp_post_a(qi, g), (cmp_post_b(qi, g) if qi < 2 else None)))
                              if ci == len(cs) - 1 else None)))
            for g in range(2):
                obr = (lambda g: (lambda r: X.ps[5 + g][:, r * 65:(r + 1) * 65]))(g)
                obres = (lambda g: (lambda r: ["ps%d" % (5 + g)]))(g)
                dl = list(range(min(4, qi), -1, -1))
                for di, dd in enumerate(dl):
                    kt = qi - dd
                    mask = (mca[:], "mca") if dd == 0 else ((mw4[:], "mw4") if dd == 4 else None)
                    posts = []
                    if di == 0 and qi >= 2:
                        posts.append(lambda qi=qi, g=g: cmp_post_b(qi, g))
                    if di == len(dl) - 1:
                        posts.append(lambda g=g, qb=qb, obr=obr: coef_and_acc(g, qb, 2, obr, ["ps%d" % (5 + g)], False))
                    jobs.append(dict(
                        qk=(lambda g=g, qb=qb, kt=kt, mask=mask: qk_tile(g, qb, KW[:, g, kt * 128:(kt + 1) * 128], "KW", None, mask)),
                        pv=pv_fn(obr, obres, VW[:, kt, g, :], "VW", di == 0, di == len(dl) - 1, False),
                        post=(lambda posts=posts: [p_() for p_ in posts])))
            for g in range(2):
                obr = (lambda g: (lambda r: X.ps[5 + g][:, r * 65:(r + 1) * 65]))(g)
                obres = (lambda g: (lambda r: ["ps%d" % (5 + g)]))(g)
                for kt in range(qi + 1):
                    mask = (mca[:], "mca") if kt == qi else None
                    posts = []
                    if kt == qi:
                        posts.append(lambda g=g, qb=qb, obr=obr: coef_and_acc(g, qb, 1, obr, ["ps%d" % (5 + g)], False))
                        if g == 1:
                            posts.append(lambda qi=qi: write_out(qi))
                    jobs.append(dict(
                        qk=(lambda g=g, qb=qb, kt=kt, mask=mask: qk_tile(
                            g, qb, KS[:, g, kt * 128:(kt + 1) * 128], "KS", (E[:, kt, :], negT[g][:], ["E", "negT%d" % g]), mask)),
                        pv=pv_fn(obr, obres, VS[:, kt, g, :], "VS", kt == 0, kt == qi, False),
                        post=(lambda posts=posts: [p_() for p_ in posts])))
            if qi + 1 < X.nsa_tiles:
                old = jobs[first_job]["post"]
                jobs[first_job]["post"] = (lambda old=old, qi=qi: ((old() if old else None), emit_loads(qi + 1)))

        LA = 2
        emit_loads(0)
        pend = {}

        def start(j):
            pend[j] = jobs[j]["qk"]()

        for j in range(min(LA, len(jobs))):
            start(j)
        if L == 0:
            X.conv_todo = conv_thunks(s, X, X.conv_layers)
        n_here = len(X.conv_todo) if L == DEPTH - 1 else min(len(X.conv_todo), 11 + 80)
        every = max(1, (len(jobs) - 100) // max(1, n_here))
        for j in range(len(jobs)):
            if n_here > 0 and j >= 50 and (j - 50) % every == 0 and X.conv_todo:
                X.conv_todo.pop(0)()
                n_here -= 1
            if j + LA < len(jobs):
                start(j + LA)
            pt, ptres = pend.pop(j)
            jobs[j]["pv"](pt, ptres)
            if jobs[j]["post"]:
                jobs[j]["post"]()
        while L == DEPTH - 1 and X.conv_todo:
            X.conv_todo.pop(0)()
        s.barrier()


PHASES.append(("nsa", phase_nsa))


def phase_ret(s, X, L, h_in):
    nc = X.nc
    with ExitStack() as es:
        sb = lambda name, shape, dt: es.enter_context(nc.sbuf_tensor("%s_L%d" % (name, L), shape, dt))
        qT = [sb("r_qT%d" % i, [64, 8, 128], BF16) for i in range(2)]
        kT = [sb("r_kT%d" % i, [64, 8, 128], BF16) for i in range(2)]
        ones1 = sb("r_ones1", [1, 128], F32)
        gn1 = sb("r_gn1", [1, 512], F32)
        km = [sb("r_km%d" % i, [128, 512], BF16) for i in range(2)]
        vm = [sb("r_vm%d" % i, [128, 512], BF16) for i in range(2)]
        gm = [sb("r_gm%d" % i, [128, 512], BF16) for i in range(2)]
        state = sb("r_state", [64, 8, 64], F32)
        stateb = sb("r_stateb", [64, 8, 64], BF16)
        rmask = sb("r_mask", [128, 512], F32)
        gC = sb("r_gC", [64, 8], F32)
        gn = sb("r_gn", [128, 512], F32)
        Pm = sb("r_Pm", [128, 8, 128], BF16)
        sq = sb("r_sq", [128, 512], F32)
        y = sb("r_y", [128, 512], F32)
        yb = [sb("r_yb%d" % i, [128, 512], BF16) for i in range(2)]
        rT = [sb("r_rT%d" % i, [128, 4, 128], BF16) for i in range(2)]
        stt = sb("r_st", [128, 64], F32)
        s.dma("sp", rmask[:], X.c["c_rmask"], writes=["rmask"])
        s.dma("sp", gC[:], X.c["c_gC8"], writes=["gC"])
        s.dma("sp", gn1[:], X.ret_gn[L:L + 1, :], writes=["gn1"])
        s.op("dve", lambda e: e.memset(ones1[:], 1.0), writes=["ones1"])
        s.op("pe", lambda e: e.matmul(X.ps[6][:], lhsT=ones1[:], rhs=gn1[:], start=True, stop=True),
             reads=["ones1", "gn1"], writes=["ps6"])
        s.op("dve", lambda e: e.tensor_copy(out=gn[:], in_=X.ps[6][:]), reads=["ps6"], writes=["gn"])
        s.op("dve", lambda e: e.memset(state[:], 0.0), writes=["state"])
        for c in range(X.ret_chunks):
            b = c % 2
            tok = slice(c * 128, (c + 1) * 128)
            for j in range(4):
                s.dma("sp", qT[b][:, 2 * j:2 * j + 2, :], X.QRT[j, :, tok].rearrange("(two d) t -> d two t", two=2),
                      reads=["QRT"], writes=["qT%d" % b])
                s.dma("sp", kT[b][:, 2 * j:2 * j + 2, :], X.KRT[j, :, tok].rearrange("(two d) t -> d two t", two=2),
                      reads=["KRT"], writes=["kT%d" % b])
            s.dma("sp", km[b][:], X.KRM[tok, :], reads=["KRM"], writes=["km%d" % b])
            s.dma("sp", vm[b][:], X.VRM[tok, :], reads=["VRM"], writes=["vm%d" % b])
            s.dma("sp", gm[b][:], X.GRM[tok, :], reads=["GRM"], writes=["gm%d" % b])
            for h in range(8):
                hp, base = h // 2, 64 * (h % 2)
                pb = h // 4
                s.op("pe", lambda e: e.matmul(X.ps[pb][:, (h % 4) * 128:(h % 4 + 1) * 128],
                                              lhsT=kT[b][:, h, :], rhs=qT[b][:, h, :],
                                              start=(h % 4 == 0), stop=True),
                     reads=["kT%d" % b, "qT%d" % b], writes=["ps%d" % pb])
            for pb in range(2):
                s.op("dve", lambda e: e.tensor_tensor(out=Pm[:, 4 * pb:4 * pb + 4, :].rearrange("p a t -> p (a t)"),
                                                      in0=X.ps[pb][:], in1=rmask[:], op=ALU.mult),
                     reads=["ps%d" % pb, "rmask"], writes=["Pm"])
            for h in range(8):
                hp, base = h // 2, 64 * (h % 2)
                s.op("pe", lambda e: e.matmul(X.ps[2][:, h * 64:(h + 1) * 64], lhsT=Pm[:, h, :],
                                              rhs=vm[b][:, h * 64:(h + 1) * 64], start=(h == 0), stop=(c == 0)),
                     reads=["Pm", "vm%d" % b], writes=["ps2"])
                if c > 0:
                    s.op("pe", lambda e: e.matmul(X.ps[2][:, h * 64:(h + 1) * 64], lhsT=qT[b][:, h, :],
                                                  rhs=stateb[:, h, :], start=False, stop=True),
                         reads=["qT%d" % b, "stateb"], writes=["ps2"])
            if True:
                for h in range(8):
                    s.op("pe", lambda e: e.matmul(X.ps[3][0:64, h * 64:(h + 1) * 64], lhsT=km[b][:, h * 64:(h + 1) * 64],
                                                  rhs=vm[b][:, h * 64:(h + 1) * 64], start=(h == 0), stop=True),
                         reads=["km%d" % b, "vm%d" % b], writes=["ps3"])
                s.op("dve", lambda e: e.tensor_tensor(out=state[:], in0=state[:],
                                                      in1=X.ps[3][0:64, :].rearrange("p (h x) -> p h x", h=8), op=ALU.add),
                     reads=["ps3"], writes=["state"])
                s.op("dve", lambda e: e.tensor_tensor(out=state[:], in0=state[:], in1=bcast_mid(gC[:], 64), op=ALU.mult),
                     reads=["gC"], writes=["state"])
                s.op("dve", lambda e: e.tensor_copy(out=stateb[:], in_=state[:]), reads=["state"], writes=["stateb"])
            o3 = X.ps[2][:].rearrange("p (h d) -> p h d", h=8)
            s.op("dve", lambda e: e.tensor_reduce(out=stt[:, 0:8], in_=o3, axis=AX.X, op=ALU.add),
                 reads=["ps2"], writes=["r_st"])
            s.op("act", lambda e: e.activation(out=sq[:], in_=X.ps[2][:], func=AF.Square), reads=["ps2"], writes=["r_sq"])
            s.op("dve", lambda e: e.tensor_reduce(out=stt[:, 8:16], in_=sq[:].rearrange("p (h d) -> p h d", h=8),
                                                  axis=AX.X, op=ALU.add), reads=["r_sq"], writes=["r_st"])
            s.op("dve", lambda e: e.tensor_scalar(out=stt[:, 16:24], in0=stt[:, 0:8], scalar1=1.0 / 64, scalar2=None,
                                                  op0=ALU.mult), reads=["r_st"], writes=["r_st"])
            s.op("dve", lambda e: e.tensor_tensor(out=stt[:, 24:32], in0=stt[:, 16:24], in1=stt[:, 16:24], op=ALU.mult),
                 reads=["r_st"], writes=["r_st"])
            s.op("dve", lambda e: e.scalar_tensor_tensor(out=stt[:, 32:40], in0=stt[:, 8:16], scalar=1.0 / 64,
                                                         in1=stt[:, 24:32], op0=ALU.mult, op1=ALU.subtract),
                 reads=["r_st"], writes=["r_st"])
            s.op("dve", lambda e: e.tensor_scalar(out=stt[:, 32:40], in0=stt[:, 32:40], scalar1=EPS, scalar2=None,
                                                  op0=ALU.add), reads=["r_st"], writes=["r_st"])
            s.op("act", lambda e: e.activation(out=stt[:, 40:48], in_=stt[:, 32:40], func=AF.Sqrt),
                 reads=["r_st"], writes=["r_st"])
            s.op("dve", lambda e: e.reciprocal(out=stt[:, 48:56], in_=stt[:, 40:48]), reads=["r_st"], writes=["r_st"])
            y3 = y[:].rearrange("p (h d) -> p h d", h=8)
            s.op("dve", lambda e: e.tensor_tensor(out=y3, in0=o3, in1=bcast_mid(stt[:, 16:24], 64), op=ALU.subtract),
                 reads=["ps2", "r_st"], writes=["r_y"])
            s.op("dve", lambda e: e.tensor_tensor(out=y3, in0=y3, in1=bcast_mid(stt[:, 48:56], 64), op=ALU.mult),
                 reads=["r_st"], writes=["r_y"])
            s.op("pool", lambda e: e.tensor_tensor(out=y[:], in0=y[:], in1=gn[:], op=ALU.mult),
                 reads=["r_y", "gn"], writes=["r_y"])
            s.op("pool", lambda e: e.tensor_tensor(out=yb[b][:], in0=y[:], in1=gm[b][:], op=ALU.mult),
                 reads=["r_y", "gm%d" % b], writes=["r_yb%d" % b])
            psb = X.ps[7][:].bitcast(BF16)
            for j in range(4):
                s.op("pe", lambda e: e.transpose(out=psb[:, j * 128:(j + 1) * 128], in_=yb[b][:, j * 128:(j + 1) * 128],
                                                 identity=X.ident[:]), reads=["r_yb%d" % b, "ident"], writes=["ps7"])
            s.op("act", lambda e: e.activation(out=rT[b][:].rearrange("p j t -> p (j t)"), in_=psb[:, 0:512], func=AF.Copy),
                 reads=["ps7"], writes=["r_rT%d" % b])
            s.dma("pool", X.AT[512:1024, tok].rearrange("(j p) t -> p j t", p=128), rT[b][:],
                  reads=["r_rT%d" % b], writes=["AT"])
        s.barrier()


PHASES.append(("ret", phase_ret))


def conv_thunks(s, X, layers):
    th = []

    def conv(dst, src, rows):
        for r0 in range(0, rows, 512):
            th.append(lambda dst=dst, src=src, r0=r0: s.dma("pool", dst[r0:r0 + 512, :], src[r0:r0 + 512, :], writes=["W16"]))
    if 0 in layers:
        conv(X.FG16[0], X.ffn_gate[0], D)
        conv(X.FU16[0], X.ffn_up[0], D)
        conv(X.FD16[0], X.ffn_down[0], D_FF)
    if 1 in layers:
        for e in range(NE):
            for blk in range(7):
                r0 = (e * 7 + blk) * 128
                fs = slice(blk * 512, (blk + 1) * 512)
                th.append(lambda e=e, r0=r0, fs=fs: s.dma(
                    "pool", X.WGB[r0:r0 + 128, :].rearrange("p (k f) -> p k f", k=8),
                    X.moe_gate[0, e][:, fs].rearrange("(k p) f -> p k f", p=128), writes=["W16"]))
                th.append(lambda e=e, r0=r0, fs=fs: s.dma(
                    "pool", X.WUB[r0:r0 + 128, :].rearrange("p (k f) -> p k f", k=8),
                    X.moe_up[0, e][:, fs].rearrange("(k p) f -> p k f", p=128), writes=["W16"]))
                th.append(lambda e=e, r0=r0, fs=fs: s.dma(
                    "pool", X.WDB[r0:r0 + 128, :].rearrange("p (c n) -> p c n", c=4),
                    X.moe_down[0, e][fs, :].rearrange("(c p) n -> p c n", p=128), writes=["W16"]))
    return th


def phase_ffn(s, X, L, h_in):
    nc = X.nc
    last = (L == DEPTH - 1)
    moe = (L % 2 == 1)
    if moe and X.routed:
        return phase_moe(s, X, L, h_in)
    h_out = X.out if last else X.HB
    G16, U16, D16 = X.FG16, X.FU16, X.FD16
    with ExitStack() as es:
        sb = lambda name, shape, dt: es.enter_context(nc.sbuf_tensor("%s_L%d" % (name, L), shape, dt))
        wout = sb("f_wout", [128, 8, D], BF16)
        pgate = sb("f_pgate", [128, 8, D], BF16)
        pproj = sb("f_pproj", [128, 2, D], BF16)
        g2T = sb("f_g2T", [128, 8], F32)
        g3T = sb("f_g3T", [128, 8], F32)
        rw = sb("f_rw", [128, 8, NE], BF16)
        cT = sb("f_cT", [128, 8, 512], BF16)
        hin = [sb("f_hin%d" % i, [128, D], F32) for i in range(2)]
        h1 = sb("f_h1", [128, 4, D], F32)
        hnT = sb("f_hnT", [128, 8, 512], BF16)
        acc = sb("f_acc", [128, 4, D], F32)
        wg = [sb("f_wg%d" % i, [128, 8, 512], BF16) for i in range(3)]
        wu = [sb("f_wu%d" % i, [128, 8, 512], BF16) for i in range(3)]
        wd = [sb("f_wd%d" % i, [128, 4, D], BF16) for i in range(3)]
        silu_t = [sb("f_silu%d" % i, [128, 512], BF16) for i in range(2)]
        actb = [sb("f_actb%d" % i, [128, 4, 512], BF16) for i in range(2)]
        sig = [sb("f_sig%d" % i, [128, 512], F32) for i in range(2)]
        ptile = [sb("f_pt%d" % i, [128, 256], F32) for i in range(2)]
        pb = sb("f_pb", [128, 256], BF16)
        pT = sb("f_pT", [128, 2, 512], BF16)
        gates = sb("f_gates", [128, 4, NE], F32)
        lg = sb("f_lg", [128, 16], F32)
        sm = sb("f_sm", [128, 32], F32)
        for kc in range(8):
            s.dma("pool", wout[:, kc, :], X.w_out[L, kc * 128:(kc + 1) * 128, :], writes=["wout"])
            s.dma("pool", pgate[:, kc, :], X.ple_gate[L, kc * 128:(kc + 1) * 128, :], writes=["pgate"])
        for kc in range(2):
            s.dma("pool", pproj[:, kc, :], X.ple_proj[L, kc * 128:(kc + 1) * 128, :], writes=["pproj"])
        s.dma("sp", g2T[:], X.g_ffn[L].rearrange("(k p) -> p k", p=128), writes=["g2T"], allow_slow_non_contiguous=True)
        s.dma("sp", g3T[:], X.g_ple[L].rearrange("(k p) -> p k", p=128), writes=["g3T"], allow_slow_non_contiguous=True)
        if moe:
            s.dma("pool", rw[:], X.moe_router[0].rearrange("(k p) e -> p k e", p=128), writes=["rw"])
        if last:
            gfin = sb("f_gfin", [128, D], F32)
            ones1 = sb("f_ones1", [1, 128], F32)
            gf1 = sb("f_gf1", [1, D], F32)
            s.dma("sp", gf1[:], X.g_final.rearrange("(o n) -> o n", o=1), writes=["gf1"])
            s.op("dve", lambda e: e.memset(ones1[:], 1.0), writes=["ones1"])
            for half in range(2):
                s.op("pe", lambda e: e.matmul(X.ps[6][:], lhsT=ones1[:], rhs=gf1[:, half * 512:(half + 1) * 512],
                                              start=True, stop=True), reads=["ones1", "gf1"], writes=["ps6"])
                s.op("dve", lambda e: e.tensor_copy(out=gfin[:, half * 512:(half + 1) * 512], in_=X.ps[6][:]),
                     reads=["ps6"], writes=["gfin"])
        wcnt = 0
        for tg in range(NT // 4):
            toks = slice(tg * 512, (tg + 1) * 512)
            s.dma("sp", cT[:], X.AT[:, toks].rearrange("(k p) t -> p k t", p=128), reads=["AT"], writes=["cT"])
            for tt in range(4):
                ti = 4 * tg + tt
                hb_ = hin[ti % 2]
                s.dma("sp", hb_[:], h_in[ti * 128:(ti + 1) * 128, :], reads=["HB"], writes=["hin%d" % (ti % 2)])
                for half in range(2):
                    hs = slice(half * 512, (half + 1) * 512)
                    for k in range(8):
                        s.op("pe", lambda e: e.matmul(X.ps[6][:], lhsT=cT[:, k, tt * 128:(tt + 1) * 128], rhs=wout[:, k, hs],
                                                      start=(k == 0), stop=(k == 7)), reads=["cT", "wout"], writes=["ps6"])
                    s.op("dve", lambda e: e.tensor_tensor(out=h1[:, tt, hs], in0=X.ps[6][:], in1=hb_[:, hs], op=ALU.add),
                         reads=["ps6", "hin%d" % (ti % 2)], writes=["h1_%d" % tt])
                ln_a(s, X, h1[:, tt, :], "h1_%d" % tt, tt)
            for tt in range(4):
                ln_b(s, X, tt, g2T[:], "g2T", hnT[:, :, tt * 128:(tt + 1) * 128], "f_hnT")
            if moe:
                for tt in range(4):
                    for k in range(8):
                        s.op("pe", lambda e: e.matmul(X.ps[6][:, 0:NE], lhsT=hnT[:, k, tt * 128:(tt + 1) * 128], rhs=rw[:, k, :],
                                                      start=(k == 0), stop=(k == 7)), reads=["f_hnT", "rw"], writes=["ps6"])
                    s.op("dve", lambda e: e.tensor_copy(out=lg[:, 0:8], in_=X.ps[6][:, 0:NE]), reads=["ps6"], writes=["lg"])
                    s.op("dve", lambda e: e.max(out=sm[:, 0:8], in_=lg[:, 0:8]), reads=["lg"], writes=["fsm"])
                    s.op("dve", lambda e: e.tensor_tensor(out=sm[:, 8:9], in0=sm[:, 1:2], in1=sm[:, 0:1], op=ALU.subtract),
                         reads=["fsm"], writes=["fsm"])
                    s.op("act", lambda e: e.activation(out=sm[:, 9:10], in_=sm[:, 8:9], func=AF.Exp), reads=["fsm"], writes=["fsm"])
                    s.op("dve", lambda e: e.tensor_scalar(out=sm[:, 10:11], in0=sm[:, 9:10], scalar1=1.0, scalar2=None,
                                                          op0=ALU.add), reads=["fsm"], writes=["fsm"])
                    s.op("dve", lambda e: e.reciprocal(out=sm[:, 11:12], in_=sm[:, 10:11]), reads=["fsm"], writes=["fsm"])
                    s.op("dve", lambda e: e.tensor_tensor(out=sm[:, 12:13], in0=sm[:, 9:10], in1=sm[:, 11:12], op=ALU.mult),
                         reads=["fsm"], writes=["fsm"])
                    s.op("dve", lambda e: e.tensor_scalar(out=lg[:, 8:16], in0=lg[:, 0:8], scalar1=sm[:, 0:1],
                                                          scalar2=sm[:, 11:12], op0=ALU.is_equal, op1=ALU.mult),
                         reads=["lg", "fsm"], writes=["lg"])
                    s.op("dve", lambda e: e.tensor_scalar(out=gates[:, tt, :], in0=lg[:, 0:8], scalar1=sm[:, 1:2],
                                                          scalar2=sm[:, 12:13], op0=ALU.is_equal, op1=ALU.mult),
                         reads=["lg", "fsm"], writes=["gates"])
                    s.op("dve", lambda e: e.tensor_tensor(out=gates[:, tt, :], in0=gates[:, tt, :], in1=lg[:, 8:16], op=ALU.add),
                         reads=["lg"], writes=["gates"])
            def stage_a(J):
                wb, ab = J % 3, J % 2
                for fc in range(4):
                    pg, pu = 2 * (fc % 2), 2 * (fc % 2) + 1
                    for k in range(8):
                        s.op("pe", lambda e: e.matmul(X.ps[pg][:], lhsT=wg[wb][:, k, fc * 128:(fc + 1) * 128], rhs=hnT[:, k, :],
                                                      start=(k == 0), stop=(k == 7)),
                             reads=["wg%d" % wb, "f_hnT"], writes=["ps%d" % pg])
                    for k in range(8):
                        s.op("pe", lambda e: e.matmul(X.ps[pu][:], lhsT=wu[wb][:, k, fc * 128:(fc + 1) * 128], rhs=hnT[:, k, :],
                                                      start=(k == 0), stop=(k == 7)),
                             reads=["wu%d" % wb, "f_hnT"], writes=["ps%d" % pu])
                    s.op("act", lambda e: e.activation(out=silu_t[fc % 2][:], in_=X.ps[pg][:], func=AF.Silu),
                         reads=["ps%d" % pg], writes=["silu%d" % (fc % 2)])
                    s.op("dve", lambda e: e.tensor_tensor(out=actb[ab][:, fc, :], in0=X.ps[pu][:], in1=silu_t[fc % 2][:],
                                                          op=ALU.mult),
                         reads=["ps%d" % pu, "silu%d" % (fc % 2)], writes=["actb%d" % ab])

            def stage_b(J):
                wb, ab, blk = J % 3, J % 2, J % 7
                for tt in range(4):
                    for half in range(2):
                        hs = slice(half * 512, (half + 1) * 512)
                        pd = 4 + half
                        for fc in range(4):
                            s.op("pe", lambda e: e.matmul(X.ps[pd][:], lhsT=actb[ab][:, fc, tt * 128:(tt + 1) * 128],
                                                          rhs=wd[wb][:, fc, hs], start=(fc == 0), stop=(fc == 3)),
                                 reads=["actb%d" % ab, "wd%d" % wb], writes=["ps%d" % pd])
                        dst = acc[:, tt, hs]
                        if blk == 0:
                            s.op("dve", lambda e: e.tensor_copy(out=dst, in_=X.ps[pd][:]),
                                 reads=["ps%d" % pd], writes=["acc%d" % tt])
                        else:
                            s.op("dve", lambda e: e.tensor_tensor(out=dst, in0=X.ps[pd][:], in1=dst, op=ALU.add),
                                 reads=["ps%d" % pd], writes=["acc%d" % tt])

            def wload(J):
                blk, wb = J % 7, J % 3
                fs = slice(blk * 512, (blk + 1) * 512)
                s.dma("sp", wg[wb][:], G16[0][:, fs].rearrange("(k p) f -> p k f", p=128), reads=["W16"], writes=["wg%d" % wb])
                s.dma("sp", wu[wb][:], U16[0][:, fs].rearrange("(k p) f -> p k f", p=128), reads=["W16"], writes=["wu%d" % wb])
                s.dma("sp", wd[wb][:], D16[0][fs, :].rearrange("(c p) n -> p c n", p=128), reads=["W16"], writes=["wd%d" % wb])

            NJ = (NT // 4) * 7
            if tg == 0:
                wload(0)
                wload(1)
            for blk in range(7):
                J = tg * 7 + blk
                stage_a(J)
                if blk >= 1:
                    stage_b(J - 1)
                if J + 2 < NJ:
                    wload(J + 2)
            stage_b(tg * 7 + 6)
            for tt in range(4):
                s.op("pool", lambda e: e.tensor_tensor(out=h1[:, tt, :], in0=h1[:, tt, :], in1=acc[:, tt, :], op=ALU.add),
                     reads=["acc%d" % tt], writes=["h1_%d" % tt])
                ln_a(s, X, h1[:, tt, :], "h1_%d" % tt, tt)
            for tt in range(4):
                ln_b(s, X, tt, g3T[:], "g3T", hnT[:, :, tt * 128:(tt + 1) * 128], "f_hnT")
            psb = X.ps[7][:].bitcast(BF16)
            for tt in range(4):
                ti = 4 * tg + tt
                pt_ = ptile[ti % 2]
                s.dma("sp", pt_[:], X.p[L, ti * 128:(ti + 1) * 128, :], writes=["pt%d" % (ti % 2)])
                s.op("act", lambda e: e.activation(out=pb[:], in_=pt_[:], func=AF.Copy), reads=["pt%d" % (ti % 2)], writes=["pb"])
                for k in range(2):
                    s.op("pe", lambda e: e.transpose(out=psb[:, k * 128:(k + 1) * 128], in_=pb[:, k * 128:(k + 1) * 128],
                                                     identity=X.ident[:]), reads=["pb", "ident"], writes=["ps7"])
                s.op("act", lambda e: e.activation(out=pT[:, :, tt * 128:(tt + 1) * 128],
                                                   in_=psb[:, 0:256].rearrange("p (k t) -> p k t", k=2), func=AF.Copy),
                     reads=["ps7"], writes=["pT"])
            for tt in range(4):
                ti = 4 * tg + tt
                for half in range(2):
                    hs = slice(half * 512, (half + 1) * 512)
                    for k in range(2):
                        s.op("pe", lambda e: e.matmul(X.ps[6][:], lhsT=pT[:, k, tt * 128:(tt + 1) * 128], rhs=pproj[:, k, hs],
                                                      start=(k == 0), stop=(k == 1)), reads=["pT", "pproj"], writes=["ps6"])
                    pgb = 4 + half
                    for k in range(8):
                        s.op("pe", lambda e: e.matmul(X.ps[pgb][:], lhsT=hnT[:, k, tt * 128:(tt + 1) * 128], rhs=pgate[:, k, hs],
                                                      start=(k == 0), stop=(k == 7)), reads=["f_hnT", "pgate"], writes=["ps%d" % pgb])
                    s.op("act", lambda e: e.activation(out=sig[half][:], in_=X.ps[pgb][:], func=AF.Sigmoid),
                         reads=["ps%d" % pgb], writes=["sig%d" % half])
                    s.op("dve", lambda e: e.tensor_tensor(out=acc[:, tt, hs], in0=X.ps[6][:], in1=sig[half][:], op=ALU.mult),
                         reads=["ps6", "sig%d" % half], writes=["acc%d" % tt])
                s.op("pool", lambda e: e.tensor_tensor(out=h1[:, tt, :], in0=h1[:, tt, :], in1=acc[:, tt, :], op=ALU.add),
                     reads=["acc%d" % tt], writes=["h1_%d" % tt])
                if last:
                    ln_a(s, X, h1[:, tt, :], "h1_%d" % tt, tt)
                    s.op("act", lambda e: e.activation(out=acc[:, tt, :], in_=h1[:, tt, :], func=AF.Copy, scale=X.ss[tt][:, 2:3]),
                         reads=["h1_%d" % tt, "ss%d" % tt], writes=["acc%d" % tt])
                    s.op("pool", lambda e: e.tensor_tensor(out=acc[:, tt, :], in0=acc[:, tt, :], in1=gfin[:], op=ALU.mult),
                         reads=["gfin"], writes=["acc%d" % tt])
                    s.dma("pool", h_out[ti * 128:(ti + 1) * 128, :], acc[:, tt, :], reads=["acc%d" % tt], writes=["HOUT"])
                else:
                    s.dma("pool", h_out[ti * 128:(ti + 1) * 128, :], h1[:, tt, :], reads=["h1_%d" % tt], writes=["HB"])
        s.barrier()


PHASES.append(("ffn", phase_ffn))


def phase_moe(s, X, L, h_in):
    nc = X.nc
    last = (L == DEPTH - 1)
    h_out = X.out if last else X.HB
    with ExitStack() as es:
        sb = lambda name, shape, dt: es.enter_context(nc.sbuf_tensor("%s_L%d" % (name, L), shape, dt))
        OH = [sb("m_oh%d" % k, [128, NT, NE], F32) for k in range(2)]
        WK = [sb("m_wk%d" % k, [128, NT], F32) for k in range(2)]
        POSI2 = sb("m_posi", [128, 2 * NT], I32)
        POSI = POSI2[:].rearrange("p (k i) -> p k i", k=2)
        WIDX2 = sb("m_widx", [128, NGRP * 7], I32)
        WIDX = WIDX2[:].rearrange("p (g b) -> p g b", b=7)
        g2T = sb("m_g2T", [128, 8], F32)
        g3T = sb("m_g3T", [128, 8], F32)
        hnT = sb("m_hnT", [128, 8, 512], BF16)
        s.dma("sp", g2T[:], X.g_ffn[L].rearrange("(k p) -> p k", p=128), writes=["g2T"], allow_slow_non_contiguous=True)
        s.dma("sp", g3T[:], X.g_ple[L].rearrange("(k p) -> p k", p=128), writes=["g3T"], allow_slow_non_contiguous=True)
        with ExitStack() as es2:
            sb2 = lambda name, shape, dt: es2.enter_context(nc.sbuf_tensor("%s_L%d" % (name, L), shape, dt))
            wout = sb2("ma_wout", [128, 8, D], BF16)
            rw = sb2("ma_rw", [128, 8, NE], BF16)
            cT = sb2("ma_cT", [128, 8, 512], BF16)
            hin = [sb2("ma_hin%d" % i, [128, D], F32) for i in range(2)]
            h1 = [sb2("ma_h1%d" % i, [128, D], F32) for i in range(4)]
            lg = sb2("ma_lg", [128, 16], F32)
            sm = sb2("ma_sm", [128, 32], F32)
            for kc in range(8):
                s.dma("pool", wout[:, kc, :], X.w_out[L, kc * 128:(kc + 1) * 128, :], writes=["wout"])
            s.dma("pool", rw[:], X.moe_router[0].rearrange("(k p) e -> p k e", p=128), writes=["rw"])
            for tg in range(NT // 4):
                toks = slice(tg * 512, (tg + 1) * 512)
                s.dma("sp", cT[:], X.AT[:, toks].rearrange("(k p) t -> p k t", p=128), reads=["AT"], writes=["cT"])
                for tt in range(4):
                    ti = 4 * tg + tt
                    hb_ = hin[ti % 2]
                    s.dma("sp", hb_[:], h_in[ti * 128:(ti + 1) * 128, :], reads=["HB"], writes=["hin%d" % (ti % 2)])
                    for half in range(2):
                        hs = slice(half * 512, (half + 1) * 512)
                        pbk = 4 + half
                        for k in range(8):
                            s.op("pe", lambda e: e.matmul(X.ps[pbk][:], lhsT=cT[:, k, tt * 128:(tt + 1) * 128], rhs=wout[:, k, hs],
                                                          start=(k == 0), stop=(k == 7)), reads=["cT", "wout"], writes=["ps%d" % pbk])
                        s.op("dve", lambda e: e.tensor_tensor(out=h1[tt][:, hs], in0=X.ps[pbk][:], in1=hb_[:, hs], op=ALU.add),
                             reads=["ps%d" % pbk, "hin%d" % (ti % 2)], writes=["h1_%d" % tt])
                    s.dma("pool", X.H1[ti * 128:(ti + 1) * 128, :], h1[tt][:], reads=["h1_%d" % tt], writes=["H1"])
                    ln_a(s, X, h1[tt][:], "h1_%d" % tt, tt)
                    s.dma("pool", X.XS2[ti * 128:(ti + 1) * 128, :], X.xs[tt][:], reads=["xs%d" % tt], writes=["XS2"])
                for tt in range(4):
                    ln_b(s, X, tt, g2T[:], "g2T", hnT[:, :, tt * 128:(tt + 1) * 128], "m_hnT")
                for tt in range(4):
                    ti = 4 * tg + tt
                    for k in range(8):
                        s.op("pe", lambda e: e.matmul(X.ps[6][:, 0:NE], lhsT=hnT[:, k, tt * 128:(tt + 1) * 128], rhs=rw[:, k, :],
                                                      start=(k == 0), stop=(k == 7)), reads=["m_hnT", "rw"], writes=["ps6"])
                    s.op("dve", lambda e: e.tensor_copy(out=lg[:, 0:8], in_=X.ps[6][:, 0:NE]), reads=["ps6"], writes=["lg"])
                    s.op("dve", lambda e: e.max(out=sm[:, 0:8], in_=lg[:, 0:8]), reads=["lg"], writes=["fsm"])
                    s.op("dve", lambda e: e.tensor_tensor(out=sm[:, 8:9], in0=sm[:, 1:2], in1=sm[:, 0:1], op=ALU.subtract),
                         reads=["fsm"], writes=["fsm"])
                    s.op("act", lambda e: e.activation(out=sm[:, 9:10], in_=sm[:, 8:9], func=AF.Exp), reads=["fsm"], writes=["fsm"])
                    s.op("dve", lambda e: e.tensor_scalar(out=sm[:, 10:11], in0=sm[:, 9:10], scalar1=1.0, scalar2=None,
                                                          op0=ALU.add), reads=["fsm"], writes=["fsm"])
                    s.op("dve", lambda e: e.reciprocal(out=WK[0][:, ti:ti + 1], in_=sm[:, 10:11]), reads=["fsm"], writes=["WK"])
                    s.op("dve", lambda e: e.tensor_tensor(out=WK[1][:, ti:ti + 1], in0=sm[:, 9:10], in1=WK[0][:, ti:ti + 1],
                                                          op=ALU.mult), reads=["fsm", "WK"], writes=["WK"])
                    s.op("dve", lambda e: e.tensor_scalar(out=OH[0][:, ti, :], in0=lg[:, 0:8], scalar1=sm[:, 0:1], scalar2=None,
                                                          op0=ALU.is_equal), reads=["lg", "fsm"], writes=["OH"])
                    s.op("dve", lambda e: e.tensor_scalar(out=OH[1][:, ti, :], in0=lg[:, 0:8], scalar1=sm[:, 1:2], scalar2=None,
                                                          op0=ALU.is_equal), reads=["lg", "fsm"], writes=["OH"])
            s.barrier()
        with ExitStack() as es2:
            sb2 = lambda name, shape, dt: es2.enter_context(nc.sbuf_tensor("%s_L%d" % (name, L), shape, dt))
            Mf = sb2("mb_Mf", [128, NT, NE], F32)
            Mb = sb2("mb_Mb", [128, NT * NE], BF16)
            U = sb2("mb_U", [128, 128], BF16)
            ones = sb2("mb_ones", [128, 128], BF16)
            A_ = sb2("mb_A", [128, NT, NE], F32)
            B_ = sb2("mb_B", [128, NT, NE], F32)
            CN = sb2("mb_CN", [128, NT, NE], F32)
            POS = sb2("mb_POS", [128, NT, NE], F32)
            nE = sb2("mb_nE", [128, 64], F32)
            cmp16 = sb2("mb_cmp16", [128, NE, 16], F32)
            gi512 = sb2("mb_gi512", [128, NGRP], F32)
            tokid = sb2("mb_tokid", [128, NT], F32)
            blkp = sb2("mb_blkp", [128, 7], F32)
            cmpg = sb2("mb_cmpg", [128, NGRP, NE], F32)
            EG = sb2("mb_EG", [128, NGRP], F32)
            WF = sb2("mb_WF", [128, NGRP, 7], F32)
            PK = sb2("mb_PK", [128, 2, NT], F32)
            ROWS2 = sb2("mb_rows", [128, 2 * NT * 2], I32)
            ROWS = ROWS2[:].rearrange("p (k i c) -> p k i c", k=2, c=2)
            Z = sb2("mb_Z", [128, 2 * NSLOT // 128], I32)
            s.dma("pool", U[:], X.c["c_U"], writes=["U"])
            s.dma("sp", gi512[:], X.c["c_gi512"], writes=["gi512"])
            s.dma("sp", tokid[:], X.c["c_tokid"], writes=["tokid"])
            s.dma("sp", blkp[:], X.c["c_blkp"], writes=["blkp"])
            s.op("pool", lambda e: e.memset(ones[:], 1.0), writes=["ones"])
            s.op("pool", lambda e: e.memset(Z[:], 0), writes=["Z"])
            s.dma("sp", X.SLOT.rearrange("(p a) c -> p (a c)", p=128), Z[:], reads=["Z"], writes=["SLOT"])
            s.op("dve", lambda e: e.tensor_tensor(out=Mf[:], in0=OH[0][:], in1=OH[1][:], op=ALU.add), reads=["OH"], writes=["Mf"])
            s.op("dve", lambda e: e.tensor_copy(out=Mb[:], in_=Mf[:].rearrange("p i e -> p (i e)")), reads=["Mf"], writes=["Mb"])
            s.op("pe", lambda e: e.matmul(X.ps[0][:], lhsT=U[:], rhs=Mb[:], start=True, stop=True), reads=["U", "Mb"], writes=["ps0"])
            s.op("pe", lambda e: e.matmul(X.ps[1][:], lhsT=ones[:], rhs=Mb[:], start=True, stop=True), reads=["ones", "Mb"], writes=["ps1"])
            s.op("dve", lambda e: e.tensor_copy(out=CN[:].rearrange("p i e -> p (i e)"), in_=X.ps[1][:]), reads=["ps1"], writes=["CN"])
            s.op("dve", lambda e: e.tensor_copy(out=A_[:], in_=CN[:]), reads=["CN"], writes=["A"])
            src, dst, sres, dres = A_, B_, "A", "B"
            for sh in (1, 2, 4, 8, 16, 32):
                s.op("dve", lambda e: e.tensor_copy(out=dst[:, 0:sh, :], in_=src[:, 0:sh, :]), reads=[sres], writes=[dres])
                s.op("dve", lambda e: e.tensor_tensor(out=dst[:, sh:NT, :], in0=src[:, sh:NT, :], in1=src[:, 0:NT - sh, :],
                                                      op=ALU.add), reads=[sres], writes=[dres])
                src, dst, sres, dres = dst, src, dres, sres
            incl, ires = src, sres
            s.op("dve", lambda e: e.tensor_copy(out=nE[:, 0:8], in_=incl[:, NT - 1, :]), reads=[ires], writes=["nE"])
            s.op("dve", lambda e: e.tensor_tensor(out=cmp16[:], in0=bcast_mid(nE[:, 0:8], 16), in1=bcast_rep(gi512[:, 0:16], NE),
                                                  op=ALU.is_gt), reads=["nE", "gi512"], writes=["cmp16"])
            s.op("dve", lambda e: e.tensor_reduce(out=nE[:, 8:16], in_=cmp16[:], axis=AX.X, op=ALU.add), reads=["cmp16"], writes=["nE"])
            s.op("dve", lambda e: e.tensor_scalar(out=nE[:, 8:16], in0=nE[:, 8:16], scalar1=512.0, scalar2=None, op0=ALU.mult),
                 reads=["nE"], writes=["nE"])
            s.op("dve", lambda e: e.tensor_copy(out=nE[:, 16:17], in_=nE[:, 8:9]), reads=["nE"], writes=["nE"])
            for e_ in range(1, NE):
                s.op("dve", lambda e: e.tensor_tensor(out=nE[:, 16 + e_:17 + e_], in0=nE[:, 15 + e_:16 + e_], in1=nE[:, 8 + e_:9 + e_],
                                                      op=ALU.add), reads=["nE"], writes=["nE"])
            s.op("dve", lambda e: e.tensor_tensor(out=nE[:, 24:32], in0=nE[:, 16:24], in1=nE[:, 8:16], op=ALU.subtract),
                 reads=["nE"], writes=["nE"])
            s.op("dve", lambda e: e.tensor_tensor(out=POS[:], in0=incl[:], in1=CN[:], op=ALU.subtract), reads=[ires, "CN"], writes=["POS"])
            s.op("dve", lambda e: e.tensor_tensor(out=POS[:].rearrange("p i e -> p (i e)"), in0=POS[:].rearrange("p i e -> p (i e)"),
                                                  in1=X.ps[0][:], op=ALU.add), reads=["ps0"], writes=["POS"])
            s.op("dve", lambda e: e.tensor_tensor(out=POS[:], in0=POS[:], in1=bcast_rep(nE[:, 24:32], NT), op=ALU.add),
                 reads=["nE"], writes=["POS"])
            for k in range(2):
                s.op("dve", lambda e: e.tensor_tensor(out=Mf[:], in0=POS[:], in1=OH[k][:], op=ALU.mult), reads=["POS", "OH"], writes=["Mf"])
                s.op("dve", lambda e: e.tensor_reduce(out=PK[:, k, :], in_=Mf[:], axis=AX.X, op=ALU.add), reads=["Mf"], writes=["PK"])
                s.op("dve", lambda e: e.tensor_copy(out=ROWS[:, k, :, 0], in_=tokid[:]), reads=["tokid"], writes=["ROWS"])
                s.op("dve", lambda e: e.tensor_copy(out=ROWS[:, k, :, 1], in_=WK[k][:].bitcast(I32)), reads=["WK"], writes=["ROWS"])
            s.op("dve", lambda e: e.tensor_copy(out=POSI, in_=PK[:]), reads=["PK"], writes=["POSI"])
            s.op("dve", lambda e: e.tensor_tensor(out=cmpg[:], in0=bcast_rep(nE[:, 16:24], NGRP), in1=bcast_mid(gi512[:], NE),
                                                  op=ALU.is_le), reads=["nE", "gi512"], writes=["cmpg"])
            s.op("dve", lambda e: e.tensor_reduce(out=EG[:], in_=cmpg[:], axis=AX.X, op=ALU.add), reads=["cmpg"], writes=["EG"])
            s.op("dve", lambda e: e.tensor_scalar(out=EG[:], in0=EG[:], scalar1=float(NE - 1), scalar2=896.0, op0=ALU.min, op1=ALU.mult),
                 reads=["EG"], writes=["EG"])
            s.op("dve", lambda e: e.tensor_tensor(out=WF[:], in0=bcast_mid(EG[:], 7), in1=bcast_rep(blkp[:], NGRP), op=ALU.add),
                 reads=["EG", "blkp"], writes=["WF"])
            s.op("dve", lambda e: e.tensor_copy(out=WIDX, in_=WF[:]), reads=["WF"], writes=["WIDX"])
            for ti in range(NT):
                for k in range(2):
                    s.idma(X.SLOT[:, :], POSI2[:, k * NT + ti:k * NT + ti + 1],
                           ROWS2[:, (k * NT + ti) * 2:(k * NT + ti) * 2 + 2], None, NSLOT - 1,
                           reads=["ROWS", "POSI"], writes=["SLOT"])
            s.barrier()
        with ExitStack() as es2:
            sb2 = lambda name, shape, dt: es2.enter_context(nc.sbuf_tensor("%s_L%d" % (name, L), shape, dt))
            stt = [sb2("mc_st%d" % i, [128, 8], I32) for i in range(2)]
            xg = [sb2("mc_xg%d" % i, [128, D], BF16) for i in range(8)]
            acc = [sb2("mc_acc%d" % i, [128, 4, D], F32) for i in range(2)]
            wg = [sb2("mc_wg%d" % i, [128, 8, 512], BF16) for i in range(3)]
            wu = [sb2("mc_wu%d" % i, [128, 8, 512], BF16) for i in range(3)]
            wd = [sb2("mc_wd%d" % i, [128, 4, D], BF16) for i in range(3)]
            silu_t = [sb2("mc_silu%d" % i, [128, 512], BF16) for i in range(2)]
            actb = [sb2("mc_actb%d" % i, [128, 4, 512], BF16) for i in range(2)]
            hn = [sb2("mc_hn%d" % i, [128, 8, 512], BF16) for i in range(2)]
            NWB = len(wg)

            def prologue_loads(gi):
                gb = gi % 2
                s.dma("sp", stt[gb][:].rearrange("p (tt c) -> p tt c", c=2),
                      X.SLOT[gi * 512:(gi + 1) * 512, :].rearrange("(tt p) c -> p tt c", p=128),
                      reads=["SLOT"], writes=["st%d" % gb])
                for tt in range(4):
                    s.idma(xg[gb * 4 + tt][:, :], None, X.XS2[:, :], stt[gb][:, 2 * tt:2 * tt + 1], SEQ - 1,
                           reads=["XS2", "st%d" % gb], writes=["xg%d" % (gb * 4 + tt)])

            def prologue_tr(gi):
                gb = gi % 2
                for tt in range(4):
                    ln_b(s, X, tt, g2T[:], "g2T", hn[gb][:, :, tt * 128:(tt + 1) * 128], "hn%d" % gb,
                         src=xg[gb * 4 + tt], src_res="xg%d" % (gb * 4 + tt))

            def wload(j):
                gi, blk = divmod(j, 7)
                wb = j % NWB
                off = WIDX2[:, gi * 7 + blk:gi * 7 + blk + 1]
                s.idma(wg[wb][:].rearrange("p k f -> p (k f)"), None, X.WGB[:, :], off, 0, reads=["W16", "WIDX"], writes=["wg%d" % wb])
                s.idma(wu[wb][:].rearrange("p k f -> p (k f)"), None, X.WUB[:, :], off, 0, reads=["W16", "WIDX"], writes=["wu%d" % wb])
                s.idma(wd[wb][:].rearrange("p c n -> p (c n)"), None, X.WDB[:, :], off, 0, reads=["W16", "WIDX"], writes=["wd%d" % wb])

            def stage_a(j):
                gi, blk = divmod(j, 7)
                gb, wb, ab = gi % 2, j % NWB, j % 2
                for fc in range(4):
                    pg, pu = 2 * (fc % 2), 2 * (fc % 2) + 1
                    for k in range(8):
                        s.op("pe", lambda e: e.matmul(X.ps[pg][:], lhsT=wg[wb][:, k, fc * 128:(fc + 1) * 128], rhs=hn[gb][:, k, :],
                                                      start=(k == 0), stop=(k == 7)),
                             reads=["wg%d" % wb, "hn%d" % gb], writes=["ps%d" % pg])
                    for k in range(8):
                        s.op("pe", lambda e: e.matmul(X.ps[pu][:], lhsT=wu[wb][:, k, fc * 128:(fc + 1) * 128], rhs=hn[gb][:, k, :],
                                                      start=(k == 0), stop=(k == 7)),
                             reads=["wu%d" % wb, "hn%d" % gb], writes=["ps%d" % pu])
                    s.op("act", lambda e: e.activation(out=silu_t[fc % 2][:], in_=X.ps[pg][:], func=AF.Silu),
                         reads=["ps%d" % pg], writes=["silu%d" % (fc % 2)])
                    s.op("dve", lambda e: e.tensor_tensor(out=actb[ab][:, fc, :], in0=X.ps[pu][:], in1=silu_t[fc % 2][:],
                                                          op=ALU.mult),
                         reads=["ps%d" % pu, "silu%d" % (fc % 2)], writes=["actb%d" % ab])

            def stage_b(j):
                gi, blk = divmod(j, 7)
                gb, wb, ab = gi % 2, j % NWB, j % 2
                for tt in range(4):
                    gsc = stt[gb][:, 2 * tt + 1:2 * tt + 2].bitcast(F32)
                    for half in range(2):
                        hs = slice(half * 512, (half + 1) * 512)
                        pd = 4 + half
                        for fc in range(4):
                            s.op("pe", lambda e: e.matmul(X.ps[pd][:], lhsT=actb[ab][:, fc, tt * 128:(tt + 1) * 128],
                                                          rhs=wd[wb][:, fc, hs], start=(fc == 0), stop=(fc == 3)),
                                 reads=["actb%d" % ab, "wd%d" % wb], writes=["ps%d" % pd])
                        dst = acc[gb][:, tt, hs]
                        if blk == 0:
                            s.op("dve", lambda e: e.tensor_scalar(out=dst, in0=X.ps[pd][:], scalar1=gsc, scalar2=None, op0=ALU.mult),
                                 reads=["ps%d" % pd, "st%d" % gb], writes=["acc%d_%d" % (gb, tt)])
                        else:
                            s.op("dve", lambda e: e.scalar_tensor_tensor(out=dst, in0=X.ps[pd][:], scalar=gsc, in1=dst,
                                                                         op0=ALU.mult, op1=ALU.add),
                                 reads=["ps%d" % pd, "st%d" % gb], writes=["acc%d_%d" % (gb, tt)])
                if blk == 6:
                    s.dma("sp", X.YS[gi * 512:(gi + 1) * 512, :].rearrange("(tt p) n -> p tt n", p=128), acc[gb][:],
                          reads=["acc%d_%d" % (gb, tt) for tt in range(4)], writes=["YS"])

            NJ = NGRP * 7
            prologue_loads(0)
            prologue_tr(0)
            for j in range(min(NWB - 1, NJ)):
                wload(j)
            for j in range(NJ + 1):
                if j < NJ:
                    gi, blk = divmod(j, 7)
                    stage_a(j)
                    if blk == 6 and gi + 1 < NGRP:
                        prologue_tr(gi + 1)
                if j >= 1:
                    stage_b(j - 1)
                if j < NJ and j % 7 == 0 and j // 7 + 1 < NGRP:
                    prologue_loads(j // 7 + 1)
                if j + NWB - 1 < NJ:
                    wload(j + NWB - 1)
            s.barrier()
        with ExitStack() as es2:
            sb2 = lambda name, shape, dt: es2.enter_context(nc.sbuf_tensor("%s_L%d" % (name, L), shape, dt))
            pgate = sb2("md_pgate", [128, 8, D], BF16)
            pproj = sb2("md_pproj", [128, 2, D], BF16)
            h1 = [sb2("md_h1%d" % i, [128, 4, D], F32) for i in range(2)]
            acc = [sb2("md_acc%d" % i, [128, 4, D], F32) for i in range(2)]
            hnd = [sb2("md_hnd%d" % i, [128, 8, 512], BF16) for i in range(2)]
            yA = [sb2("md_yA%d" % i, [128, D], F32) for i in range(2)]
            yB = [sb2("md_yB%d" % i, [128, D], F32) for i in range(2)]
            sig = [sb2("md_sig%d" % i, [128, 512], F32) for i in range(2)]
            ptile = [sb2("md_pt%d" % i, [128, 256], F32) for i in range(2)]
            pb = sb2("md_pb", [128, 256], BF16)
            pT = [sb2("md_pT%d" % i, [128, 2, 512], BF16) for i in range(2)]
            for kc in range(8):
                s.dma("pool", pgate[:, kc, :], X.ple_gate[L, kc * 128:(kc + 1) * 128, :], writes=["pgate"])
            for kc in range(2):
                s.dma("pool", pproj[:, kc, :], X.ple_proj[L, kc * 128:(kc + 1) * 128, :], writes=["pproj"])
            if last:
                gfin = sb2("md_gfin", [128, D], F32)
                ones1 = sb2("md_ones1", [1, 128], F32)
                gf1 = sb2("md_gf1", [1, D], F32)
                s.dma("sp", gf1[:], X.g_final.rearrange("(o n) -> o n", o=1), writes=["gf1"])
                s.op("dve", lambda e: e.memset(ones1[:], 1.0), writes=["ones1"])
                for half in range(2):
                    s.op("pe", lambda e: e.matmul(X.ps[6][:], lhsT=ones1[:], rhs=gf1[:, half * 512:(half + 1) * 512],
                                                  start=True, stop=True), reads=["ones1", "gf1"], writes=["ps6"])
                    s.op("dve", lambda e: e.tensor_copy(out=gfin[:, half * 512:(half + 1) * 512], in_=X.ps[6][:]),
                         reads=["ps6"], writes=["gfin"])
            def stage1(tg):
                hb = tg % 2
                for tt in range(4):
                    ti = 4 * tg + tt
                    yb_ = ti % 2
                    hres = "h1_%d_%d" % (hb, tt)
                    s.dma("sp", h1[hb][:, tt, :], X.H1[ti * 128:(ti + 1) * 128, :], reads=["H1"], writes=[hres])
                    s.idma(yA[yb_][:, :], None, X.YS[:, :], POSI2[:, ti:ti + 1], 0, reads=["YS", "POSI"], writes=["yA%d" % yb_])
                    s.idma(yB[yb_][:, :], None, X.YS[:, :], POSI2[:, NT + ti:NT + ti + 1], 0, reads=["YS", "POSI"], writes=["yB%d" % yb_])
                    s.op("dve", lambda e: e.tensor_tensor(out=h1[hb][:, tt, :], in0=h1[hb][:, tt, :], in1=yA[yb_][:], op=ALU.add),
                         reads=["yA%d" % yb_], writes=[hres])
                    s.op("dve", lambda e: e.tensor_tensor(out=h1[hb][:, tt, :], in0=h1[hb][:, tt, :], in1=yB[yb_][:], op=ALU.add),
                         reads=["yB%d" % yb_], writes=[hres])
                    ln_a(s, X, h1[hb][:, tt, :], hres, tt)
                for tt in range(4):
                    ln_b(s, X, tt, g3T[:], "g3T", hnd[hb][:, :, tt * 128:(tt + 1) * 128], "hnd%d" % hb)
                psb = X.ps[7][:].bitcast(BF16)
                for tt in range(4):
                    ti = 4 * tg + tt
                    pt_ = ptile[ti % 2]
                    s.dma("sp", pt_[:], X.p[L, ti * 128:(ti + 1) * 128, :], writes=["pt%d" % (ti % 2)])
                    s.op("act", lambda e: e.activation(out=pb[:], in_=pt_[:], func=AF.Copy), reads=["pt%d" % (ti % 2)], writes=["pb"])
                    for k in range(2):
                        s.op("pe", lambda e: e.transpose(out=psb[:, k * 128:(k + 1) * 128], in_=pb[:, k * 128:(k + 1) * 128],
                                                         identity=X.ident[:]), reads=["pb", "ident"], writes=["ps7"])
                    s.op("act", lambda e: e.activation(out=pT[hb][:, :, tt * 128:(tt + 1) * 128],
                                                       in_=psb[:, 0:256].rearrange("p (k t) -> p k t", k=2), func=AF.Copy),
                         reads=["ps7"], writes=["pT%d" % hb])

            def stage2(tg):
                hb = tg % 2
                for tt in range(4):
                    ti = 4 * tg + tt
                    hres = "h1_%d_%d" % (hb, tt)
                    ares = "dacc%d_%d" % (hb, tt)
                    for half in range(2):
                        hs = slice(half * 512, (half + 1) * 512)
                        for k in range(2):
                            s.op("pe", lambda e: e.matmul(X.ps[6][:], lhsT=pT[hb][:, k, tt * 128:(tt + 1) * 128], rhs=pproj[:, k, hs],
                                                          start=(k == 0), stop=(k == 1)), reads=["pT%d" % hb, "pproj"], writes=["ps6"])
                        pgb = 4 + half
                        for k in range(8):
                            s.op("pe", lambda e: e.matmul(X.ps[pgb][:], lhsT=hnd[hb][:, k, tt * 128:(tt + 1) * 128], rhs=pgate[:, k, hs],
                                                          start=(k == 0), stop=(k == 7)), reads=["hnd%d" % hb, "pgate"], writes=["ps%d" % pgb])
                        s.op("act", lambda e: e.activation(out=sig[half][:], in_=X.ps[pgb][:], func=AF.Sigmoid),
                             reads=["ps%d" % pgb], writes=["sig%d" % half])
                        s.op("dve", lambda e: e.tensor_tensor(out=acc[hb][:, tt, hs], in0=X.ps[6][:], in1=sig[half][:], op=ALU.mult),
                             reads=["ps6", "sig%d" % half], writes=[ares])
                    s.op("pool", lambda e: e.tensor_tensor(out=h1[hb][:, tt, :], in0=h1[hb][:, tt, :], in1=acc[hb][:, tt, :], op=ALU.add),
                         reads=[ares], writes=[hres])
                    if last:
                        k4 = 4 + tt % 4 if len(X.ss) > 4 else tt
                        ln_a(s, X, h1[hb][:, tt, :], hres, k4)
                        s.op("act", lambda e: e.activation(out=acc[hb][:, tt, :], in_=h1[hb][:, tt, :], func=AF.Copy, scale=X.ss[k4][:, 2:3]),
                             reads=[hres, "ss%d" % k4], writes=[ares])
                        s.op("pool", lambda e: e.tensor_tensor(out=acc[hb][:, tt, :], in0=acc[hb][:, tt, :], in1=gfin[:], op=ALU.mult),
                             reads=["gfin"], writes=[ares])
                        s.dma("pool", h_out[ti * 128:(ti + 1) * 128, :], acc[hb][:, tt, :], reads=[ares], writes=["HOUT"])
                    else:
                        s.dma("pool", h_out[ti * 128:(ti + 1) * 128, :], h1[hb][:, tt, :], reads=[hres], writes=["HB"])

            stage1(0)
            for tg in range(NT // 4):
                if tg + 1 < NT // 4:
                    stage1(tg + 1)
                stage2(tg)
            s.barrier()
        s.barrier()
```

```python
import math
import os
from contextlib import ExitStack

import numpy as np
import concourse.bass as bass
import concourse.mybir as mybir
from concourse.bass_utils import run_bass_kernel_spmd

F32 = mybir.dt.float32
BF16 = mybir.dt.bfloat16
AF = mybir.ActivationFunctionType
ALU = mybir.AluOpType
AX = mybir.AxisListType

D = 1024
SEQ = 8192
NT = SEQ // 128
DEPTH = 2
HD = 64
IN_COLS = 3352
D_FF = 3584
NE = 8
EPS = 1e-6
NEG = -30000.0
NGRP = 40
NSLOT = NGRP * 512
I32 = mybir.dt.int32

C_QN, C_KC, C_VC, C_KS, C_VS, C_KW, C_VW, C_GL, C_QR, C_KR, C_VR, C_GR = (
    0, 512, 640, 768, 896, 1024, 1152, 1280, 1304, 1816, 2328, 2840)


class S:
    def __init__(self, nc, es, n_dma_sems=24):
        self.nc = nc
        self.eng = {"pe": nc.tensor, "dve": nc.vector, "act": nc.scalar,
                    "pool": nc.gpsimd, "sp": nc.sync}
        self.sem = {k: es.enter_context(nc.semaphore("c_" + k)) for k in self.eng}
        self.cnt = {k: 0 for k in self.eng}
        self.seen = {k: {} for k in self.eng}
        self.drained = {k: 0 for k in self.eng}
        self.dsem = [es.enter_context(nc.semaphore("d%d" % i)) for i in range(n_dma_sems)]
        self.dval = [0] * n_dma_sems
        self.dnext = 0
        self.lastw = {}
        self.readers = {}
        self.nops = 0

    def _wait(self, eng, tok):
        if tok is None:
            return
        if tok[0] == "c":
            _, e2, c = tok
            if e2 == eng:
                if eng != "pe" and c > self.drained[eng]:
                    self.eng[eng].drain()
                    self.drained[eng] = self.cnt[eng]
                return
            key = e2
            sem = self.sem[e2]
        else:
            _, si, c = tok
            key = ("d", si)
            sem = self.dsem[si]
        if self.seen[eng].get(key, 0) >= c:
            return
        self.eng[eng].wait_ge(sem, c)
        self.seen[eng][key] = c

    def _deps(self, eng, reads, writes):
        for r in reads:
            self._wait(eng, self.lastw.get(r))
        for w in writes:
            self._wait(eng, self.lastw.get(w))
            for t in self.readers.get(w, {}).values():
                self._wait(eng, t)

    def _commit(self, tok, reads, writes):
        for r in reads:
            self.readers.setdefault(r, {})[tok[1] if tok[0] == "c" else ("d", tok[1])] = tok
        for w in writes:
            self.lastw[w] = tok
            self.readers[w] = {}

    def op(self, eng, fn, reads=(), writes=()):
        self._deps(eng, reads, writes)
        ins = fn(self.eng[eng])
        self.cnt[eng] += 1
        ins.then_inc(self.sem[eng], 1)
        self._commit(("c", eng, self.cnt[eng]), reads, writes)
        self.nops += 1

    def dma(self, q, out, in_, reads=(), writes=(), **kw):
        self._deps(q, reads, writes)
        si = self.dnext
        self.dnext = (self.dnext + 1) % len(self.dsem)
        self._wait(q, ("d", si, self.dval[si]))
        ins = self.eng[q].dma_start(out=out, in_=in_, **kw)
        self.dval[si] += 16
        ins.then_inc(self.dsem[si], 16)
        self._commit(("d", si, self.dval[si]), reads, writes)
        self.nops += 1

    def idma(self, out, out_off, in_, in_off, bound, reads=(), writes=()):
        q = "pool"
        self._deps(q, reads, writes)
        si = self.dnext
        self.dnext = (self.dnext + 1) % len(self.dsem)
        self._wait(q, ("d", si, self.dval[si]))
        oo = None if out_off is None else bass.IndirectOffsetOnAxis(ap=out_off, axis=0)
        io = None if in_off is None else bass.IndirectOffsetOnAxis(ap=in_off, axis=0)
        ins = self.nc.gpsimd.indirect_dma_start(out=out, out_offset=oo, in_=in_, in_offset=io)
        self.dval[si] += 16
        ins.then_inc(self.dsem[si], 16)
        self._commit(("d", si, self.dval[si]), reads, writes)
        self.nops += 1

    def barrier(self):
        for e in self.eng:
            for e2 in self.eng:
                if e2 != e and self.cnt[e2] > 0:
                    self._wait(e, ("c", e2, self.cnt[e2]))
            for si in range(len(self.dsem)):
                if self.dval[si] > 0:
                    self._wait(e, ("d", si, self.dval[si]))
        self.lastw = {}
        self.readers = {}

    def drain(self, eng="sp"):
        for r, t in list(self.lastw.items()):
            self._wait(eng, t)
        for r, d in list(self.readers.items()):
            for t in d.values():
                self._wait(eng, t)


def bcast_mid(ap2, n):
    return ap2.unsqueeze(2).to_broadcast([ap2.shape[0], ap2.shape[1], n])


def bcast_rep(ap2, n):
    return ap2.unsqueeze(1).to_broadcast([ap2.shape[0], n, ap2.shape[1]])


def make_consts():
    c = {}
    c["c_ident"] = np.eye(128, dtype=np.float32)
    t = np.arange(SEQ)
    slopes = 2.0 ** (-(np.arange(8) + 1.0))
    qa = np.zeros((8, 3, SEQ), np.float32)
    for h in range(8):
        qa[h, 0] = 128.0 * slopes[h]
        qa[h, 1] = slopes[h]
        qa[h, 2] = -slopes[h] * 128.0 * (t // 128 + 1)
    c["c_qaug"] = qa
    ka = np.zeros((3, SEQ), np.float32)
    ka[0] = t // 128
    ka[1] = t % 128
    ka[2] = 1.0
    c["c_kaug"] = ka
    n = np.arange(512)
    pos = 16 * n + 31
    kc = np.zeros((3, 512), np.float32)
    kc[0] = pos // 128
    kc[1] = pos % 128
    kc[2] = 1.0
    c["c_kcaug"] = kc
    hh = np.arange(8)
    lg = np.log1p(-(2.0 ** (-5.0 - hh)))
    nn = np.arange(128)
    xiT = np.zeros((4, 128, 128), np.float32)
    kdT = np.zeros((4, 128, 128), np.float32)
    gC = np.zeros((128, 4), np.float32)
    for hp in range(4):
        for half in range(2):
            h = 2 * hp + half
            xiT[hp, half * 64:(half + 1) * 64, :] = np.exp((nn + 1.0) * lg[h])[None, :]
            kdT[hp, half * 64:(half + 1) * 64, :] = 0.125 * np.exp(-(nn + 1.0) * lg[h])[None, :]
            gC[half * 64:(half + 1) * 64, hp] = np.exp(128.0 * lg[h])
    c["c_xiT"] = xiT
    c["c_kdT"] = kdT
    c["c_gC"] = gC
    c["c_gC8"] = np.tile(np.exp(128.0 * lg)[None, :], (64, 1)).astype(np.float32)
    c["c_ktab"] = (0.125 * np.exp(-(nn[:, None] + 1.0) * lg[None, :])).astype(np.float32)
    dm = (nn[None, :] >= nn[:, None]).astype(np.float32)
    c["c_rmask"] = np.tile(dm, (1, 4))
    jj = nn[:, None]
    ii = nn[None, :]
    c["c_mcausal"] = np.tile(np.where(jj > ii, NEG, 0.0).astype(np.float32), (1, 4))
    c["c_mwin4"] = np.tile(np.where(jj > ii, 0.0, NEG).astype(np.float32), (1, 4))
    pats = []
    pidx = {}
    cmp_plan = []
    for qi in range(NT):
        row = []
        for cc in range(4):
            tq = 128 * qi + ii
            nk = 128 * cc + jj
            valid = (tq - (16 * nk + 31) >= 0) & (nk <= 510)
            if valid.all():
                row.append(-1)
            elif not valid.any():
                row.append(-2)
            else:
                key = valid.tobytes()
                if key not in pidx:
                    pidx[key] = len(pats)
                    pats.append(np.tile(np.where(valid, 0.0, NEG).astype(np.float32), (1, 4)))
                row.append(pidx[key])
        cmp_plan.append(row)
    c["c_mcmp"] = np.stack(pats, 0)
    E = np.zeros((128, NT, 128), np.float32)
    for kt in range(NT):
        E[2 * kt, kt, 0:64] = 1.0
        E[2 * kt + 1, kt, 64:128] = 1.0
    c["c_E"] = E
    ncmp = np.arange(512)[:, None]
    msel = np.arange(128)[None, :]
    ov = ((np.minimum(16 * ncmp + 32, 64 * msel + 64) > np.maximum(16 * ncmp, 64 * msel))
          & (ncmp <= 510)).astype(np.float32)
    c["c_ov"] = ov
    AB = np.zeros((NT, 128, 256), np.float32)
    for qi in range(NT):
        tq = 128 * qi + nn[:, None]
        back = tq // 64 - msel
        forced = (msel == 0) | ((back >= 0) & (back < 2))
        A = ((back >= 0) & (~forced)).astype(np.float32)
        B = np.where(forced, 1.0e9 + 1024.0 * msel, np.where(back >= 0, 0.0, -1.0)).astype(np.float32)
        AB[qi, :, 0:128] = A
        AB[qi, :, 128:256] = B
    c["c_AB"] = AB
    c["c_gi512"] = np.tile((512.0 * np.arange(NGRP))[None, :], (128, 1)).astype(np.float32)
    c["c_tokid"] = (np.arange(NT)[None, :] * 128 + np.arange(128)[:, None]).astype(np.float32)
    c["c_U"] = (nn[:, None] < nn[None, :]).astype(np.float32)
    c["c_blkp"] = (np.arange(7)[None, :] * 128 + np.arange(128)[:, None]).astype(np.float32)
    return c, cmp_plan


_CONSTS = None


def get_consts():
    global _CONSTS
    if _CONSTS is None:
        _CONSTS = make_consts()
    return _CONSTS


class Ctx:
    def __getattr__(self, k):
        if k in WEIGHT_SPECS:
            ap = self.nc.dram_tensor(k, WEIGHT_SPECS[k], F32, kind="ExternalInput").ap()
            self.__dict__[k] = ap
            self.used.append(k)
            return ap
        raise AttributeError(k)


def ln_a(s, X, src, src_res, k):
    junk, ss, xs = X.junk, X.ss[k], X.xs[k]
    s.op("act", lambda e: e.activation(out=junk[:], in_=src, func=AF.Square, accum_out=ss[:, 0:1]),
         reads=[src_res], writes=["junk", "ss%d" % k])
    s.op("dve", lambda e: e.tensor_scalar(out=ss[:, 1:2], in0=ss[:, 0:1], scalar1=1.0 / D, scalar2=EPS,
                                          op0=ALU.mult, op1=ALU.add), reads=["ss%d" % k], writes=["ss%d" % k])
    s.op("act", lambda e: e.activation(out=ss[:, 1:2], in_=ss[:, 1:2], func=AF.Sqrt),
         reads=["ss%d" % k], writes=["ss%d" % k])
    s.op("dve", lambda e: e.reciprocal(out=ss[:, 2:3], in_=ss[:, 1:2]), reads=["ss%d" % k], writes=["ss%d" % k])
    s.op("act", lambda e: e.activation(out=xs[:], in_=src, func=AF.Copy, scale=ss[:, 2:3]),
         reads=[src_res, "ss%d" % k], writes=["xs%d" % k])


def ln_b(s, X, k, gT, gT_res, dst, dst_res, src=None, src_res=None):
    xs = X.xs[k] if src is None else src
    xres = ("xs%d" % k) if src is None else src_res
    psb = X.ps[7][:].bitcast(BF16)
    for kc in range(8):
        s.op("pe", lambda e: e.transpose(out=psb[:, kc * 128:(kc + 1) * 128], in_=xs[:, kc * 128:(kc + 1) * 128],
                                         identity=X.ident[:]), reads=[xres, "ident"], writes=["ps7"])
    s.op("dve", lambda e: e.tensor_tensor(out=dst, in0=psb.rearrange("p (k t) -> p k t", k=8),
                                          in1=bcast_mid(gT, 128), op=ALU.mult),
         reads=["ps7", gT_res], writes=[dst_res])


def phase_proj(s, X, L, h_in):
    nc = X.nc
    with ExitStack() as es:
        sb = lambda name, shape, dt: es.enter_context(nc.sbuf_tensor("%s_L%d" % (name, L), shape, dt))
        wsb = sb("p1_w", [128, 8, IN_COLS], BF16)
        gT = sb("p1_gT", [128, 8], F32)
        ht = [sb("p1_ht%d" % i, [128, D], F32) for i in range(4)]
        hnT = [sb("p1_hnT%d" % i, [128, 8, 512], BF16) for i in range(2)]
        fm = [sb("p1_fm%d" % i, [128, 16, 512], BF16) for i in range(2)]
        vsx = [sb("p1_vsx%d" % i, [128, 4, 2, 65], BF16) for i in range(2)]
        vwx = [sb("p1_vwx%d" % i, [128, 4, 2, 65], BF16) for i in range(2)]
        gls = [sb("p1_gls%d" % i, [128, 4, 24], F32) for i in range(2)]
        krm = [sb("p1_krm%d" % i, [128, 4, 512], BF16) for i in range(2)]
        vrm = [sb("p1_vrm%d" % i, [128, 4, 512], BF16) for i in range(2)]
        grm = [sb("p1_grm%d" % i, [128, 4, 512], BF16) for i in range(2)]
        sg = sb("p1_sg", [128, 512], F32)
        xiT = sb("p1_xiT", [128, 4, 128], F32)
        kdT = sb("p1_kdT", [128, 4, 128], F32)
        ktab = sb("p1_ktab", [128, 8], F32)
        s.dma("sp", xiT[:], X.c["c_xiT"].rearrange("j p t -> p j t"), writes=["xiT"])
        s.dma("sp", kdT[:], X.c["c_kdT"].rearrange("j p t -> p j t"), writes=["kdT"])
        s.dma("sp", ktab[:], X.c["c_ktab"], writes=["ktab"])
        s.dma("sp", gT[:], X.g_mix[L].rearrange("(k p) -> p k", p=128), writes=["p1gT"],
              allow_slow_non_contiguous=True)
        for kc in range(8):
            s.dma("pool", wsb[:, kc, :], X.w_in[L, kc * 128:(kc + 1) * 128, :], writes=["p1w"])
        for i in range(2):
            s.op("pool", lambda e: e.memset(vsx[i][:, :, :, 64:65], 1.0), writes=["vsx%d" % i])
            s.op("pool", lambda e: e.memset(vwx[i][:, :, :, 64:65], 1.0), writes=["vwx%d" % i])
        fm_cols = [C_QN, C_QN + 128, C_QN + 256, C_QN + 384, C_KC, C_VC, C_KS, C_KW,
                   C_QR, C_QR + 128, C_QR + 256, C_QR + 384, C_KR, C_KR + 128, C_KR + 256, C_KR + 384]
        def emit_a(tg):
            for tt in range(4):
                ti = 4 * tg + tt
                s.dma("sp", ht[tt][:], h_in[ti * 128:(ti + 1) * 128, :], writes=["p1ht%d" % tt])
                ln_a(s, X, ht[tt][:], "p1ht%d" % tt, tt)

        def emit_b(tg):
            for tt in range(4):
                ln_b(s, X, tt, gT[:], "p1gT", hnT[tg % 2][:, :, tt * 128:(tt + 1) * 128], "hnT%d" % (tg % 2))

        emit_a(0)
        emit_b(0)
        for tg in range(NT // 4):
            b = tg % 2
            toks = slice(tg * 512, (tg + 1) * 512)
            if tg + 1 < NT // 4:
                emit_a(tg + 1)
            for ci, c0 in enumerate(fm_cols):
                pb = ci % 2
                ps = X.ps[pb]
                for kc in range(8):
                    s.op("pe", lambda e: e.matmul(ps[:], lhsT=wsb[:, kc, c0:c0 + 128], rhs=hnT[b][:, kc, :],
                                                  start=(kc == 0), stop=(kc == 7)),
                         reads=["p1w", "hnT%d" % b], writes=["ps%d" % pb])
                dst = fm[b][:, ci, :]
                if ci < 4:
                    s.op("act", lambda e: e.activation(out=dst, in_=ps[:], func=AF.Copy, scale=0.125),
                         reads=["ps%d" % pb], writes=["fm%d" % b])
                elif ci < 8:
                    eng = "act" if ci % 2 == 0 else "dve"
                    if eng == "act":
                        s.op("act", lambda e: e.activation(out=dst, in_=ps[:], func=AF.Copy),
                             reads=["ps%d" % pb], writes=["fm%d" % b])
                    else:
                        s.op("dve", lambda e: e.tensor_copy(out=dst, in_=ps[:]),
                             reads=["ps%d" % pb], writes=["fm%d" % b])
                else:
                    tab = xiT if ci < 12 else kdT
                    j = (ci - 8) % 4
                    s.op("dve", lambda e: e.tensor_tensor(
                        out=dst.rearrange("p (a t) -> p a t", a=4), in0=ps[:].rearrange("p (a t) -> p a t", a=4),
                        in1=bcast_rep(tab[:, j, :], 4), op=ALU.mult),
                        reads=["ps%d" % pb, "xiT", "kdT"], writes=["fm%d" % b])
            for tt in range(4):
                lhs = lambda kc: hnT[b][:, kc, tt * 128:(tt + 1) * 128]
                groups = [(2, C_VS, 408), (3, C_KR, 512), (4, C_VR, 512), (5, C_GR, 512)]
                for (pi, c0, n) in groups:
                    for kc in range(8):
                        s.op("pe", lambda e: e.matmul(X.ps[pi][:, 0:n], lhsT=lhs(kc), rhs=wsb[:, kc, c0:c0 + n],
                                                      start=(kc == 0), stop=(kc == 7)),
                             reads=["p1w", "hnT%d" % b], writes=["ps%d" % pi])
                pA = X.ps[2]
                s.op("dve", lambda e: e.tensor_copy(out=vsx[b][:, tt, :, 0:64],
                                                    in_=pA[:, 0:128].rearrange("p (g d) -> p g d", g=2)),
                     reads=["ps2"], writes=["vsx%d" % b])
                s.op("dve", lambda e: e.tensor_copy(out=vwx[b][:, tt, :, 0:64],
                                                    in_=pA[:, 256:384].rearrange("p (g d) -> p g d", g=2)),
                     reads=["ps2"], writes=["vwx%d" % b])
                s.op("act", lambda e: e.activation(out=gls[b][:, tt, :], in_=pA[:, 384:408], func=AF.Sigmoid),
                     reads=["ps2"], writes=["gls%d" % b])
                s.op("dve", lambda e: e.tensor_tensor(
                    out=krm[b][:, tt, :].rearrange("p (h d) -> p h d", h=8),
                    in0=X.ps[3][:].rearrange("p (h d) -> p h d", h=8),
                    in1=bcast_mid(ktab[:], 64), op=ALU.mult),
                    reads=["ps3", "ktab"], writes=["krm%d" % b])
                s.op("act", lambda e: e.activation(out=vrm[b][:, tt, :], in_=X.ps[4][:], func=AF.Copy),
                     reads=["ps4"], writes=["vrm%d" % b])
                s.op("act", lambda e: e.activation(out=sg[:], in_=X.ps[5][:], func=AF.Sigmoid),
                     reads=["ps5"], writes=["p1sg"])
                s.op("dve", lambda e: e.tensor_tensor(out=grm[b][:, tt, :], in0=X.ps[5][:], in1=sg[:], op=ALU.mult),
                     reads=["ps5", "p1sg"], writes=["grm%d" % b])
            if tg + 1 < NT // 4:
                emit_b(tg + 1)
            s.dma("pool", X.QTP[:, :, toks].rearrange("j p t -> p j t"), fm[b][:, 0:4, :], reads=["fm%d" % b], writes=["QTP"])
            s.dma("pool", X.KCP[:, toks], fm[b][:, 4, :], reads=["fm%d" % b], writes=["KCP"])
            s.dma("pool", X.VCP[:, toks], fm[b][:, 5, :], reads=["fm%d" % b], writes=["VCP"])
            s.dma("pool", X.KSP[:, toks], fm[b][:, 6, :], reads=["fm%d" % b], writes=["KSP"])
            s.dma("pool", X.KWP[:, toks], fm[b][:, 7, :], reads=["fm%d" % b], writes=["KWP"])
            s.dma("pool", X.QRT[:, :, toks].rearrange("j p t -> p j t"), fm[b][:, 8:12, :], reads=["fm%d" % b], writes=["QRT"])
            s.dma("pool", X.KRT[:, :, toks].rearrange("j p t -> p j t"), fm[b][:, 12:16, :], reads=["fm%d" % b], writes=["KRT"])
            s.dma("pool", X.VSX[toks].rearrange("(tt p) g c -> p tt g c", p=128), vsx[b][:], reads=["vsx%d" % b], writes=["VSX"])
            s.dma("pool", X.VWX[toks].rearrange("(tt p) g c -> p tt g c", p=128), vwx[b][:], reads=["vwx%d" % b], writes=["VWX"])
            s.dma("pool", X.GLS[toks].rearrange("(tt p) c -> p tt c", p=128), gls[b][:], reads=["gls%d" % b], writes=["GLS"])
            s.dma("pool", X.KRM[toks].rearrange("(tt p) c -> p tt c", p=128), krm[b][:], reads=["krm%d" % b], writes=["KRM"])
            s.dma("pool", X.VRM[toks].rearrange("(tt p) c -> p tt c", p=128), vrm[b][:], reads=["vrm%d" % b], writes=["VRM"])
            s.dma("pool", X.GRM[toks].rearrange("(tt p) c -> p tt c", p=128), grm[b][:], reads=["grm%d" % b], writes=["GRM"])
        s.barrier()


WEIGHT_SPECS = {
    "w_in": [DEPTH, D, IN_COLS], "w_out": [DEPTH, D, D], "g_mix": [DEPTH, D], "g_ffn": [DEPTH, D],
    "g_ple": [DEPTH, D], "g_final": [D], "cmp_pos": [DEPTH, 2, 32, 64], "cmp_w1": [DEPTH, 2, 32, 64, 256],
    "cmp_w2": [DEPTH, 2, 256, 64], "ret_gn": [DEPTH, 512], "ffn_gate": [1, D, D_FF], "ffn_up": [1, D, D_FF],
    "ffn_down": [1, D_FF, D], "moe_router": [1, D, NE], "moe_gate": [1, NE, D, D_FF], "moe_up": [1, NE, D, D_FF],
    "moe_down": [1, NE, D_FF, D], "ple_proj": [DEPTH, 256, D], "ple_gate": [DEPTH, D, D],
}

SCRATCH_SPECS = {
    "QTP": ([4, 128, SEQ], BF16), "QAUG": ([8, 3, SEQ], BF16), "KAUG": ([3, SEQ], BF16), "KCAUG": ([3, 512], BF16),
    "KSP": ([128, SEQ], BF16), "KWP": ([128, SEQ], BF16), "KCP": ([128, SEQ], BF16), "VCP": ([128, SEQ], BF16),
    "VSX": ([SEQ, 2, 65], BF16), "VWX": ([SEQ, 2, 65], BF16), "GLS": ([SEQ, 24], F32),
    "QRT": ([4, 128, SEQ], BF16), "KRT": ([4, 128, SEQ], BF16),
    "KRM": ([SEQ, 512], BF16), "VRM": ([SEQ, 512], BF16), "GRM": ([SEQ, 512], BF16),
    "AT": ([D, SEQ], BF16), "HB": ([SEQ, D], F32),
    "FG16": ([1, D, D_FF], BF16), "FU16": ([1, D, D_FF], BF16), "FD16": ([1, D_FF, D], BF16),
    "WGB": ([NE * 7 * 128, 4096], BF16), "WUB": ([NE * 7 * 128, 4096], BF16), "WDB": ([NE * 7 * 128, 4096], BF16),
    "H1": ([SEQ, D], F32), "XS2": ([SEQ, D], BF16), "SLOT": ([NSLOT, 2], I32), "YS": ([NSLOT, D], F32),
    "DBG_KCMP": ([67, 2, 512], BF16), "DBG_VCX": ([128, 2, 4, 193], BF16),
}


def build(stop_after=None, debug=(), nsa_tiles=NT):
    nc = bass.Bass("TRN2", target_bir_lowering=False)
    X = Ctx()
    X.nc = nc
    consts, cmp_plan = get_consts()
    X.cmp_plan = cmp_plan
    X.x = nc.dram_tensor("x", [SEQ, D], F32, kind="ExternalInput").ap()
    X.p = nc.dram_tensor("p", [DEPTH, SEQ, 256], F32, kind="ExternalInput").ap()
    X.used = []
    X.dbg_cmp = "DBG_KCMP" in debug
    X.nsa_tiles = nsa_tiles
    X.routed = True
    X.conv_layers = [0] if (stop_after is not None and stop_after[0] == 0) else [0, 1]
    X.ret_chunks = int(os.environ.get('RET_CHUNKS', NT))
    X.c = {k: nc.dram_tensor(k, list(v.shape), F32, kind="ExternalInput").ap() for k, v in consts.items()}
    X.out = nc.dram_tensor("out", [SEQ, D], F32, kind="ExternalOutput").ap()
    for k, (shp, dt) in SCRATCH_SPECS.items():
        kind = "ExternalOutput" if k in debug else "Internal"
        setattr(X, k, nc.dram_tensor(k, shp, dt, kind=kind).ap())
    with ExitStack() as es:
        s = S(nc, es)
        X.ps = [es.enter_context(nc.psum_tensor("ps%d" % i, [128, 512], F32)) for i in range(8)]
        X.ident = es.enter_context(nc.sbuf_tensor("ident", [128, 128], BF16))
        X.junk = es.enter_context(nc.sbuf_tensor("junk", [128, D], F32))
        X.ss = [es.enter_context(nc.sbuf_tensor("ss%d" % i, [128, 4], F32)) for i in range(8)]
        X.xs = [es.enter_context(nc.sbuf_tensor("xs%d" % i, [128, D], BF16)) for i in range(8)]
        s.dma("pool", X.ident[:], X.c["c_ident"], writes=["ident"])
        s.dma("pool", X.QAUG, X.c["c_qaug"], writes=["QAUG"])
        s.dma("pool", X.KAUG, X.c["c_kaug"], writes=["KAUG"])
        s.dma("pool", X.KCAUG, X.c["c_kcaug"], writes=["KCAUG"])
        done = False
        for L in range(DEPTH):
            h_in = X.x if L == 0 else X.HB
            for name, fn in PHASES:
                fn(s, X, L, h_in)

                if stop_after == (L, name):
                    done = True
                    break
            if done:
                break
        s.barrier()
        s.drain("sp")
    X.nops = s.nops
    return nc, X


PHASES = [("proj", phase_proj)]

_BUILT = {}


def run(inputs, stop_after=None, debug=(), cores=8, trace=False, nsa_tiles=NT):
    key = (stop_after, tuple(debug), nsa_tiles)
    if key not in _BUILT:
        _BUILT[key] = build(stop_after, debug, nsa_tiles)
    nc, X = _BUILT[key]
    consts, _ = get_consts()
    in_maps = []
    for b in range(cores):
        m = {"x": np.ascontiguousarray(inputs["x"][b]), "p": np.ascontiguousarray(inputs["p"][:, b])}
        for k in X.used:
            m[k] = np.ascontiguousarray(inputs[k])
        m.update(consts)
        in_maps.append(m)
    return run_bass_kernel_spmd(nc, in_maps, core_ids=list(range(cores)), trace=trace)


def kernel(**inputs):
    inputs = {k: np.asarray(v) for k, v in inputs.items()}
    res = run(inputs)
    return np.stack([r["out"] for r in res.results], 0).astype(np.float32)


def phase_nsa(s, X, L, h_in):
    nc = X.nc
    with ExitStack() as es:
        sb = lambda name, shape, dt: es.enter_context(nc.sbuf_tensor("%s_L%d" % (name, L), shape, dt))
        KCMP = sb("n_kcmp", [67, 2, 512], BF16)
        VCX = sb("n_vcx", [128, 2, 4, 193], BF16)
        s.op("pool", lambda e: e.memset(KCMP[:], 0.0), writes=["KCMP"])
        s.op("pool", lambda e: e.memset(VCX[:], 0.0), writes=["VCX"])
        s.op("pool", lambda e: e.memset(VCX[:, :, :, 64:65], 1.0), writes=["VCX"])
        for g in range(2):
            s.dma("pool", VCX[:, g, :, 65:193], X.c["c_ov"].rearrange("(ct p) m -> p ct m", p=128), writes=["VCX"])
            s.dma("sp", KCMP[64:67, g, :], X.KCAUG, reads=["KCAUG"], writes=["KCMP"])
        with ExitStack() as es2:
            sb2 = lambda name, shape, dt: es2.enter_context(nc.sbuf_tensor("%s_L%d" % (name, L), shape, dt))
            kcT = [sb2("c_kcT%d" % i, [64, SEQ], BF16) for i in range(2)]
            w1 = sb2("c_w1", [64, 32, 256], BF16)
            posT = sb2("c_posT", [64, 32], BF16)
            w2 = sb2("c_w2", [128, 2, 64], BF16)
            hb = sb2("c_hb", [128, 2], F32)
            u = sb2("c_u", [128, 2, 512], F32)
            t1 = sb2("c_t1", [128, 2, 512], F32)
            sg = sb2("c_sg", [128, 2, 512], F32)
            gel = sb2("c_gel", [128, 2, 512], BF16)
            it = 0
            for kv in range(2):
                s.dma("pool", w1[:], X.cmp_w1[L, kv].rearrange("l d h -> d l h"), writes=["c_w1"])
                s.dma("pool", posT[:], X.cmp_pos[L, kv].rearrange("l d -> d l"), writes=["c_posT"],
                      allow_slow_non_contiguous=True)
                s.dma("pool", w2[:], X.cmp_w2[L, kv].rearrange("(c p) d -> p c d", p=128), writes=["c_w2"])
                src = X.KCP if kv == 0 else X.VCP
                for g in range(2):
                    kt_ = kcT[it % 2]
                    kres = "c_kcT%d" % (it % 2)
                    it += 1
                    s.dma("sp", kt_[:], src[g * 64:(g + 1) * 64, :], reads=["KCP", "VCP"], writes=[kres])
                    kview = kt_[:].rearrange("d (n c) -> d n c", c=16)
                    for hc in range(2):
                        ps = X.ps[hc]
                        for l in range(32):
                            a, c_ = l // 16, l % 16
                            s.op("pe", lambda e: e.matmul(ps[:, 0:511], lhsT=w1[:, l, hc * 128:(hc + 1) * 128],
                                                          rhs=kview[:, a:a + 511, c_], start=(l == 0), stop=(l == 31)),
                                 reads=["c_w1", kres], writes=["ps%d" % hc])
                        for l in range(32):
                            s.op("pe", lambda e: e.matmul(X.ps[2][:, hc:hc + 1], lhsT=w1[:, l, hc * 128:(hc + 1) * 128],
                                                          rhs=posT[:, l:l + 1], start=(l == 0), stop=(l == 31)),
                                 reads=["c_w1", "c_posT"], writes=["ps2"])
                        s.op("dve", lambda e: e.tensor_copy(out=hb[:, hc:hc + 1], in_=X.ps[2][:, hc:hc + 1]),
                             reads=["ps2"], writes=["c_hb"])
                        s.op("act", lambda e: e.activation(out=u[:, hc, 0:511], in_=ps[:, 0:511], func=AF.Identity,
                                                           bias=hb[:, hc:hc + 1]),
                             reads=["ps%d" % hc, "c_hb"], writes=["c_u"])
                    uu = u[:, :, 0:511]
                    s.op("pool", lambda e: e.tensor_tensor(out=t1[:, :, 0:511], in0=uu, in1=uu, op=ALU.mult),
                         reads=["c_u"], writes=["c_t1"])
                    s.op("dve", lambda e: e.tensor_scalar(out=t1[:, :, 0:511], in0=t1[:, :, 0:511], scalar1=0.044715,
                                                          scalar2=1.0, op0=ALU.mult, op1=ALU.add),
                         reads=["c_t1"], writes=["c_t1"])
                    s.op("dve", lambda e: e.tensor_tensor(out=t1[:, :, 0:511], in0=t1[:, :, 0:511], in1=uu, op=ALU.mult),
                         reads=["c_t1", "c_u"], writes=["c_t1"])
                    s.op("act", lambda e: e.activation(out=sg[:, :, 0:511], in_=t1[:, :, 0:511], func=AF.Sigmoid,
                                                       scale=1.5957691216057308),
                         reads=["c_t1"], writes=["c_sg"])
                    s.op("dve", lambda e: e.tensor_tensor(out=gel[:, :, 0:511], in0=sg[:, :, 0:511], in1=uu, op=ALU.mult),
                         reads=["c_sg", "c_u"], writes=["c_gel"])
                    if kv == 0:
                        for hc in range(2):
                            s.op("pe", lambda e: e.matmul(X.ps[3][0:64, 0:511], lhsT=w2[:, hc, :], rhs=gel[:, hc, 0:511],
                                                          start=(hc == 0), stop=(hc == 1)),
                                 reads=["c_w2", "c_gel"], writes=["ps3"])
                        s.op("act", lambda e: e.activation(out=KCMP[0:64, g, 0:511], in_=X.ps[3][0:64, 0:511], func=AF.Copy),
                             reads=["ps3"], writes=["KCMP"])
                    else:
                        for ct in range(4):
                            nn = 128 if ct < 3 else 127
                            for hc in range(2):
                                s.op("pe", lambda e: e.matmul(X.ps[3][0:nn, ct * 64:(ct + 1) * 64],
                                                              lhsT=gel[:, hc, ct * 128:ct * 128 + nn], rhs=w2[:, hc, :],
                                                              start=(hc == 0), stop=(hc == 1)),
                                     reads=["c_w2", "c_gel"], writes=["ps3"])
                        for ct in range(4):
                            nn = 128 if ct < 3 else 127
                            s.op("act", lambda e: e.activation(out=VCX[0:nn, g, ct, 0:64],
                                                               in_=X.ps[3][0:nn, ct * 64:(ct + 1) * 64], func=AF.Copy),
                                 reads=["ps3"], writes=["VCX"])
            s.barrier()
        if X.dbg_cmp:
            s.dma("sp", X.DBG_KCMP, KCMP[:], reads=["KCMP"])
            s.dma("sp", X.DBG_VCX, VCX[:], reads=["VCX"])
        KS = sb("n_ks", [67, 2, SEQ], BF16)
        KW = sb("n_kw", [67, 2, SEQ], BF16)
        VS = sb("n_vs", [128, NT, 2, 65], BF16)
        VW = sb("n_vw", [128, NT, 2, 65], BF16)
        E = sb("n_E", [128, NT, 128], BF16)
        mca = sb("n_mca", [128, 512], BF16)
        mw4 = sb("n_mw4", [128, 512], BF16)
        npat = X.c["c_mcmp"].shape[0]
        mcmp = sb("n_mcmp", [128, npat, 512], BF16)
        qt = [sb("n_qt%d" % i, [67, 1024], BF16) for i in range(2)]
        AB = [sb("n_AB%d" % i, [128, 256], F32) for i in range(2)]
        glt = [sb("n_gl%d" % i, [128, 24], F32) for i in range(2)]
        PT = [sb("n_PT%d" % i, [128, 512], BF16) for i in range(4)]
        negT = [sb("n_negT%d" % i, [128, 512], BF16) for i in range(2)]
        acc = [sb("n_acc%d" % i, [128, 512], F32) for i in range(2)]
        accb = [sb("n_accb%d" % i, [128, 512], BF16) for i in range(2)]
        aT = [sb("n_aT%d" % i, [128, 4, 128], BF16) for i in range(2)]
        sm = [sb("n_sm%d" % i, [128, 64], F32) for i in range(2)]
        sc = [sb("n_sc%d" % i, [128, 128], F32) for i in range(2)]
        sc2 = [sb("n_sc2%d" % i, [128, 128], F32) for i in range(2)]
        seln = [sb("n_seln%d" % i, [128, 128], BF16) for i in range(2)]
        s.dma("sp", KS[0:64, :, :], X.KSP.rearrange("(g d) t -> d g t", g=2), reads=["KSP"], writes=["KS"])
        s.dma("sp", KW[0:64, :, :], X.KWP.rearrange("(g d) t -> d g t", g=2), reads=["KWP"], writes=["KW"])
        for g in range(2):
            s.dma("sp", KS[64:67, g, :], X.KAUG, reads=["KAUG"], writes=["KS"])
            s.dma("sp", KW[64:67, g, :], X.KAUG, reads=["KAUG"], writes=["KW"])
        for q4 in range(4):
            tsl = slice(q4 * 2048, (q4 + 1) * 2048)
            ksl = slice(q4 * 16, (q4 + 1) * 16)
            s.dma("sp", VS[:, ksl], X.VSX[tsl].rearrange("(kt p) g c -> p kt g c", p=128), reads=["VSX"], writes=["VS"])
            s.dma("sp", VW[:, ksl], X.VWX[tsl].rearrange("(kt p) g c -> p kt g c", p=128), reads=["VWX"], writes=["VW"])
            s.dma("pool", E[:, ksl, :], X.c["c_E"][:, ksl, :], writes=["E"])
        s.dma("pool", mca[:], X.c["c_mcausal"], writes=["mca"])
        s.dma("pool", mw4[:], X.c["c_mwin4"], writes=["mw4"])
        s.dma("pool", mcmp[:], X.c["c_mcmp"].rearrange("n p c -> p n c"), writes=["mcmp"])
        st = {"sb": 0, "pt": 0}

        def qk_tile(g, qb, lhsT, lres, extra, mask):
            pi = st["sb"] % 3
            st["sb"] += 1
            ps = X.ps[pi]
            pres = "ps%d" % pi
            nmm = 1 + (extra is not None) + (mask is not None)
            s.op("pe", lambda e: e.matmul(ps[:], lhsT=lhsT, rhs=qt[qb][:, g * 512:(g + 1) * 512], start=True,
                                          stop=(nmm == 1)), reads=[lres, "qt%d" % qb], writes=[pres])
            k = 1
            if extra is not None:
                el, er, eres = extra
                s.op("pe", lambda e: e.matmul(ps[:], lhsT=el, rhs=er, start=False, stop=(k + 1 == nmm)),
                     reads=eres, writes=[pres])
                k += 1
            if mask is not None:
                ml, mres = mask
                s.op("pe", lambda e: e.matmul(ps[:], lhsT=X.ident[:], rhs=ml, start=False, stop=True),
                     reads=["ident", mres], writes=[pres])
            pk = st["pt"] % 4
            st["pt"] += 1
            s.op("act", lambda e: e.activation(out=PT[pk][:], in_=ps[:], func=AF.Exp),
                 reads=[pres], writes=["PT%d" % pk])
            return PT[pk], "PT%d" % pk

        def coef_and_acc(g, qb, branch, heads_ap, heads_res, first):
            smt = sm[g]
            sres = "sm%d" % g
            for r in range(4):
                o_ap = heads_ap(r)
                s.op("dve", lambda e: e.tensor_scalar(out=smt[:, r:r + 1], in0=o_ap[:, 64:65], scalar1=1e-36,
                                                      scalar2=None, op0=ALU.max),
                     reads=heads_res, writes=[sres])
            s.op("dve", lambda e: e.reciprocal(out=smt[:, 4:8], in_=smt[:, 0:4]), reads=[sres], writes=[sres])
            c0 = branch * 8 + g * 4
            s.op("dve", lambda e: e.tensor_tensor(out=smt[:, 8:12], in0=smt[:, 4:8], in1=glt[qb][:, c0:c0 + 4],
                                                  op=ALU.mult), reads=[sres, "gl%d" % qb], writes=[sres])
            for r in range(4):
                o_ap = heads_ap(r)
                dst = acc[qb][:, (g * 4 + r) * 64:(g * 4 + r + 1) * 64]
                if first:
                    s.op("dve", lambda e: e.tensor_scalar(out=dst, in0=o_ap[:, 0:64], scalar1=smt[:, 8 + r:9 + r],
                                                          scalar2=None, op0=ALU.mult),
                         reads=heads_res + [sres], writes=["acc%d" % qb])
                else:
                    s.op("dve", lambda e: e.scalar_tensor_tensor(out=dst, in0=o_ap[:, 0:64], scalar=smt[:, 8 + r:9 + r],
                                                                 in1=dst, op0=ALU.mult, op1=ALU.add),
                         reads=heads_res + [sres], writes=["acc%d" % qb])

        def emit_loads(qi):
            qb = qi % 2
            t0 = qi * 128
            for j in range(4):
                s.dma("sp", qt[qb][0:64, :].rearrange("d (j two t) -> d j two t", j=4, two=2)[:, j],
                      X.QTP[j, :, t0:t0 + 128].rearrange("(two d) t -> d two t", two=2),
                      reads=["QTP"], writes=["qt%d" % qb])
            s.dma("sp", qt[qb][64:67, :].rearrange("r (h t) -> r h t", h=8),
                  X.QAUG[:, :, t0:t0 + 128].rearrange("h r t -> r h t"), reads=["QAUG"], writes=["qt%d" % qb])
            s.dma("sp", AB[qb][:], X.c["c_AB"][qi], writes=["AB%d" % qb])
            s.dma("sp", glt[qb][:], X.GLS[t0:t0 + 128, :], reads=["GLS"], writes=["gl%d" % qb])

        def cmp_post_a(qi, g):
            qb = qi % 2
            ocb = lambda r: X.ps[3 + r // 2][:, (r % 2) * 193:(r % 2) * 193 + 193]
            coef_and_acc(g, qb, 0, ocb, ["ps3", "ps4"], True)
            smt = sm[g]
            sres = "sm%d" % g
            for r in range(4):
                if r == 0:
                    s.op("dve", lambda e: e.tensor_scalar(out=sc[g][:], in0=ocb(r)[:, 65:193], scalar1=smt[:, 4:5],
                                                          scalar2=None, op0=ALU.mult),
                         reads=["ps3", "ps4", sres], writes=["sc%d" % g])
                else:
                    s.op("dve", lambda e: e.scalar_tensor_tensor(out=sc[g][:], in0=ocb(r)[:, 65:193],
                                                                 scalar=smt[:, 4 + r:5 + r], in1=sc[g][:],
                                                                 op0=ALU.mult, op1=ALU.add),
                         reads=["ps3", "ps4", sres], writes=["sc%d" % g])
            s.op("dve", lambda e: e.tensor_tensor(out=sc[g][:], in0=sc[g][:], in1=AB[qb][:, 0:128], op=ALU.mult),
                 reads=["AB%d" % qb], writes=["sc%d" % g])
            s.op("dve", lambda e: e.tensor_tensor(out=sc[g][:], in0=sc[g][:], in1=AB[qb][:, 128:256], op=ALU.add),
                 reads=["AB%d" % qb], writes=["sc%d" % g])
            s.op("dve", lambda e: e.max(out=smt[:, 16:24], in_=sc[g][:]), reads=["sc%d" % g], writes=[sres])
            s.op("dve", lambda e: e.match_replace(out=sc2[g][:], in_to_replace=smt[:, 16:24], in_values=sc[g][:],
                                                  imm_value=-2.0), reads=["sc%d" % g, sres], writes=["sc2%d" % g])
            s.op("dve", lambda e: e.max(out=smt[:, 24:32], in_=sc2[g][:]), reads=["sc2%d" % g], writes=[sres])
            s.op("dve", lambda e: e.tensor_scalar(out=seln[g][:], in0=sc[g][:], scalar1=smt[:, 31:32], scalar2=NEG,
                                                  op0=ALU.is_lt, op1=ALU.mult),
                 reads=["sc%d" % g, sres], writes=["seln%d" % g])

        def cmp_post_b(qi, g):
            psb = X.ps[7][:].bitcast(BF16)
            s.op("pe", lambda e: e.transpose(out=psb[:, g * 128:(g + 1) * 128], in_=seln[g][:], identity=X.ident[:]),
                 reads=["seln%d" % g, "ident"], writes=["ps7"])
            s.op("dve", lambda e: e.tensor_copy(out=negT[g][:].rearrange("p (a t) -> p a t", a=4),
                                                in_=bcast_rep(psb[:, g * 128:(g + 1) * 128], 4)),
                 reads=["ps7"], writes=["negT%d" % g])

        def write_out(qi):
            qb = qi % 2
            t0 = qi * 128
            s.op("act", lambda e: e.activation(out=accb[qb][:], in_=acc[qb][:], func=AF.Copy),
                 reads=["acc%d" % qb], writes=["accb%d" % qb])
            psb = X.ps[7][:].bitcast(BF16)
            for j in range(4):
                s.op("pe", lambda e: e.transpose(out=psb[:, 256 + j * 128:256 + (j + 1) * 128],
                                                 in_=accb[qb][:, j * 128:(j + 1) * 128], identity=X.ident[:]),
                     reads=["accb%d" % qb, "ident"], writes=["ps7"])
            s.op("act", lambda e: e.activation(out=aT[qb][:].rearrange("p j t -> p (j t)"), in_=psb[:, 256:768], func=AF.Copy),
                 reads=["ps7"], writes=["aT%d" % qb])
            s.dma("sp", X.AT[0:512, t0:t0 + 128].rearrange("(j p) t -> p j t", p=128), aT[qb][:],
                  reads=["aT%d" % qb], writes=["AT"])

        def pv_fn(out_ap, out_res, rhs, rhs_res, first, lastt, pair_start):
            def f(pt, ptres):
                for r in range(4):
                    st_ = first and ((r % 2 == 0) if pair_start else (r == 0))
                    s.op("pe", lambda e: e.matmul(out_ap(r), lhsT=pt[:, r * 128:(r + 1) * 128], rhs=rhs,
                                                  start=st_, stop=lastt), reads=[ptres, rhs_res], writes=out_res(r))
            return f

        jobs = []
        for qi in range(X.nsa_tiles):
            qb = qi % 2
            first_job = len(jobs)
            plan = X.cmp_plan[qi]
            cs = [c for c in range(4) if plan[c] != -2]
            ocb = lambda r: X.ps[3 + r // 2][:, (r % 2) * 193:(r % 2) * 193 + 193]
            ocres = lambda r: ["ps%d" % (3 + r // 2)]
            for g in range(2):
                for ci, c in enumerate(cs):
                    mask = None if plan[c] == -1 else (mcmp[:, plan[c], :], "mcmp")
                    jobs.append(dict(
                        qk=(lambda g=g, qb=qb, c=c, mask=mask: qk_tile(g, qb, KCMP[:, g, c * 128:(c + 1) * 128], "KCMP", None, mask)),
                        pv=pv_fn(ocb, ocres, VCX[:, g, c, :], "VCX", ci == 0, ci == len(cs) - 1, True),
                        post=((lambda qi=qi, g=g: (cmp_post_a(qi, g), (cmp_post_b(qi, g) if qi < 2 else None)))
                              if ci == len(cs) - 1 else None)))
            for g in range(2):
                obr = (lambda g: (lambda r: X.ps[5 + g][:, r * 65:(r + 1) * 65]))(g)
                obres = (lambda g: (lambda r: ["ps%d" % (5 + g)]))(g)
                dl = list(range(min(4, qi), -1, -1))
                for di, dd in enumerate(dl):
                    kt = qi - dd
                    mask = (mca[:], "mca") if dd == 0 else ((mw4[:], "mw4") if dd == 4 else None)
                    posts = []
                    if di == 0 and qi >= 2:
                        posts.append(lambda qi=qi, g=g: cmp_post_b(qi, g))
                    if di == len(dl) - 1:
                        posts.append(lambda g=g, qb=qb, obr=obr: coef_and_acc(g, qb, 2, obr, ["ps%d" % (5 + g)], False))
                    jobs.append(dict(
                        qk=(lambda g=g, qb=qb, kt=kt, mask=mask: qk_tile(g, qb, KW[:, g, kt * 128:(kt + 1) * 128], "KW", None, mask)),
                        pv=pv_fn(obr, obres, VW[:, kt, g, :], "VW", di == 0, di == len(dl) - 1, False),
                        post=(lambda posts=posts: [p_() for p_ in posts])))
            for g in range(2):
                obr = (lambda g: (lambda r: X.ps[5 + g][:, r * 65:(r + 1) * 65]))(g)
                obres = (lambda g: (lambda r: ["ps%d" % (5 + g)]))(g)
                for kt in range(qi + 1):
                    mask = (mca[:], "mca") if kt == qi else None
                    posts = []
                    if kt == qi:
                        posts.append(lambda g=g, qb=qb, obr=obr: coef_and_acc(g, qb, 1, obr, ["ps%d" % (5 + g)], False))
                        if g == 1:
                            posts.append(lambda qi=qi: write_out(qi))
                    jobs.append(dict(
                        qk=(lambda g=g, qb=qb, kt=kt, mask=mask: qk_tile(
                            g, qb, KS[:, g, kt * 128:(kt + 1) * 128], "KS", (E[:, kt, :], negT[g][:], ["E", "negT%d" % g]), mask)),
                        pv=pv_fn(obr, obres, VS[:, kt, g, :], "VS", kt == 0, kt == qi, False),
                        post=(lambda posts=posts: [p_() for p_ in posts])))
            if qi + 1 < X.nsa_tiles:
                old = jobs[first_job]["post"]
                jobs[first_job]["post"] = (lambda old=old, qi=qi: ((old() if old else None), emit_loads(qi + 1)))

        LA = 2
        emit_loads(0)
        pend = {}

        def start(j):
            pend[j] = jobs[j]["qk"]()

        for j in range(min(LA, len(jobs))):
            start(j)
        if L == 0:
            X.conv_todo = conv_thunks(s, X, X.conv_layers)
        n_here = len(X.conv_todo) if L == DEPTH - 1 else min(len(X.conv_todo), 11 + 80)
        every = max(1, (len(jobs) - 100) // max(1, n_here))
        for j in range(len(jobs)):
            if n_here > 0 and j >= 50 and (j - 50) % every == 0 and X.conv_todo:
                X.conv_todo.pop(0)()
                n_here -= 1
            if j + LA < len(jobs):
                start(j + LA)
            pt, ptres = pend.pop(j)
            jobs[j]["pv"](pt, ptres)
            if jobs[j]["post"]:
                jobs[j]["post"]()
        while L == DEPTH - 1 and X.conv_todo:
            X.conv_todo.pop(0)()
        s.barrier()


PHASES.append(("nsa", phase_nsa))


def phase_ret(s, X, L, h_in):
    nc = X.nc
    with ExitStack() as es:
        sb = lambda name, shape, dt: es.enter_context(nc.sbuf_tensor("%s_L%d" % (name, L), shape, dt))
        qT = [sb("r_qT%d" % i, [64, 8, 128], BF16) for i in range(2)]
        kT = [sb("r_kT%d" % i, [64, 8, 128], BF16) for i in range(2)]
        ones1 = sb("r_ones1", [1, 128], F32)
        gn1 = sb("r_gn1", [1, 512], F32)
        km = [sb("r_km%d" % i, [128, 512], BF16) for i in range(2)]
        vm = [sb("r_vm%d" % i, [128, 512], BF16) for i in range(2)]
        gm = [sb("r_gm%d" % i, [128, 512], BF16) for i in range(2)]
        state = sb("r_state", [64, 8, 64], F32)
        stateb = [sb("r_stateb%d" % i, [64, 8, 64], BF16) for i in range(2)]
        rmask = sb("r_mask", [128, 512], F32)
        gC = sb("r_gC", [64, 8], F32)
        gn = sb("r_gn", [128, 512], F32)
        Pm = [sb("r_Pm%d" % i, [128, 8, 128], BF16) for i in range(2)]
        sq = sb("r_sq", [128, 512], F32)
        y = sb("r_y", [128, 512], F32)
        yb = [sb("r_yb%d" % i, [128, 512], BF16) for i in range(2)]
        rT = [sb("r_rT%d" % i, [128, 4, 128], BF16) for i in range(2)]
        stt = sb("r_st", [128, 64], F32)
        s.dma("sp", rmask[:], X.c["c_rmask"], writes=["rmask"])
        s.dma("sp", gC[:], X.c["c_gC8"], writes=["gC"])
        s.dma("sp", gn1[:], X.ret_gn[L:L + 1, :], writes=["gn1"])
        s.op("dve", lambda e: e.memset(ones1[:], 1.0), writes=["ones1"])
        s.op("pe", lambda e: e.matmul(X.ps[6][:], lhsT=ones1[:], rhs=gn1[:], start=True, stop=True),
             reads=["ones1", "gn1"], writes=["ps6"])
        s.op("dve", lambda e: e.tensor_copy(out=gn[:], in_=X.ps[6][:]), reads=["ps6"], writes=["gn"])
        s.op("dve", lambda e: e.memset(state[:], 0.0), writes=["state"])
        def loads(c):
            b = c % 2
            tok = slice(c * 128, (c + 1) * 128)
            for j in range(4):
                s.dma("sp", qT[b][:, 2 * j:2 * j + 2, :], X.QRT[j, :, tok].rearrange("(two d) t -> d two t", two=2),
                      reads=["QRT"], writes=["qT%d" % b])
                s.dma("sp", kT[b][:, 2 * j:2 * j + 2, :], X.KRT[j, :, tok].rearrange("(two d) t -> d two t", two=2),
                      reads=["KRT"], writes=["kT%d" % b])
            s.dma("sp", km[b][:], X.KRM[tok, :], reads=["KRM"], writes=["km%d" % b])
            s.dma("sp", vm[b][:], X.VRM[tok, :], reads=["VRM"], writes=["vm%d" % b])
            s.dma("sp", gm[b][:], X.GRM[tok, :], reads=["GRM"], writes=["gm%d" % b])

        def t1m(c):
            b = c % 2
            for h in range(8):
                pb = h // 4
                s.op("pe", lambda e: e.matmul(X.ps[pb][:, (h % 4) * 128:(h % 4 + 1) * 128],
                                              lhsT=kT[b][:, h, :], rhs=qT[b][:, h, :],
                                              start=(h % 4 == 0), stop=True),
                     reads=["kT%d" % b, "qT%d" % b], writes=["ps%d" % pb])
            for pb in range(2):
                s.op("dve", lambda e: e.tensor_tensor(out=Pm[b][:, 4 * pb:4 * pb + 4, :].rearrange("p a t -> p (a t)"),
                                                      in0=X.ps[pb][:], in1=rmask[:], op=ALU.mult),
                     reads=["ps%d" % pb, "rmask"], writes=["Pm%d" % b])

        def upd(c):
            b = c % 2
            for h in range(8):
                s.op("pe", lambda e: e.matmul(X.ps[3][0:64, h * 64:(h + 1) * 64], lhsT=km[b][:, h * 64:(h + 1) * 64],
                                              rhs=vm[b][:, h * 64:(h + 1) * 64], start=(h == 0), stop=True),
                     reads=["km%d" % b, "vm%d" % b], writes=["ps3"])
            s.op("dve", lambda e: e.tensor_tensor(out=state[:], in0=state[:],
                                                  in1=X.ps[3][0:64, :].rearrange("p (h x) -> p h x", h=8), op=ALU.add),
                 reads=["ps3"], writes=["state"])
            s.op("dve", lambda e: e.tensor_tensor(out=state[:], in0=state[:], in1=bcast_mid(gC[:], 64), op=ALU.mult),
                 reads=["gC"], writes=["state"])
            s.op("dve", lambda e: e.tensor_copy(out=stateb[(c + 1) % 2][:], in_=state[:]), reads=["state"],
                 writes=["stateb%d" % ((c + 1) % 2)])

        def omm(c):
            b = c % 2
            po = 2 if b == 0 else 6
            for h in range(8):
                s.op("pe", lambda e: e.matmul(X.ps[po][:, h * 64:(h + 1) * 64], lhsT=Pm[b][:, h, :],
                                              rhs=vm[b][:, h * 64:(h + 1) * 64], start=(h == 0), stop=(c == 0)),
                     reads=["Pm%d" % b, "vm%d" % b], writes=["ps%d" % po])
                if c > 0:
                    s.op("pe", lambda e: e.matmul(X.ps[po][:, h * 64:(h + 1) * 64], lhsT=qT[b][:, h, :],
                                                  rhs=stateb[b][:, h, :], start=False, stop=True),
                         reads=["qT%d" % b, "stateb%d" % b], writes=["ps%d" % po])

        def gn_a(c):
            b = c % 2
            po = 2 if b == 0 else 6
            pres = "ps%d" % po
            o3 = X.ps[po][:].rearrange("p (h d) -> p h d", h=8)
            s.op("dve", lambda e: e.tensor_reduce(out=stt[:, 0:8], in_=o3, axis=AX.X, op=ALU.add),
                 reads=[pres], writes=["r_st"])
            s.op("act", lambda e: e.activation(out=sq[:], in_=X.ps[po][:], func=AF.Square), reads=[pres], writes=["r_sq"])
            s.op("dve", lambda e: e.tensor_reduce(out=stt[:, 8:16], in_=sq[:].rearrange("p (h d) -> p h d", h=8),
                                                  axis=AX.X, op=ALU.add), reads=["r_sq"], writes=["r_st"])
            s.op("dve", lambda e: e.tensor_scalar(out=stt[:, 16:24], in0=stt[:, 0:8], scalar1=1.0 / 64, scalar2=None,
                                                  op0=ALU.mult), reads=["r_st"], writes=["r_st"])
            s.op("dve", lambda e: e.tensor_tensor(out=stt[:, 24:32], in0=stt[:, 16:24], in1=stt[:, 16:24], op=ALU.mult),
                 reads=["r_st"], writes=["r_st"])
            s.op("dve", lambda e: e.scalar_tensor_tensor(out=stt[:, 32:40], in0=stt[:, 8:16], scalar=1.0 / 64,
                                                         in1=stt[:, 24:32], op0=ALU.mult, op1=ALU.subtract),
                 reads=["r_st"], writes=["r_st"])
            s.op("dve", lambda e: e.tensor_scalar(out=stt[:, 32:40], in0=stt[:, 32:40], scalar1=EPS, scalar2=None,
                                                  op0=ALU.add), reads=["r_st"], writes=["r_st"])
            s.op("act", lambda e: e.activation(out=stt[:, 40:48], in_=stt[:, 32:40], func=AF.Sqrt),
                 reads=["r_st"], writes=["r_st"])
            s.op("dve", lambda e: e.reciprocal(out=stt[:, 48:56], in_=stt[:, 40:48]), reads=["r_st"], writes=["r_st"])
            y3 = y[:].rearrange("p (h d) -> p h d", h=8)
            s.op("dve", lambda e: e.tensor_tensor(out=y3, in0=o3, in1=bcast_mid(stt[:, 16:24], 64), op=ALU.subtract),
                 reads=[pres, "r_st"], writes=["r_y"])
            s.op("dve", lambda e: e.tensor_tensor(out=y3, in0=y3, in1=bcast_mid(stt[:, 48:56], 64), op=ALU.mult),
                 reads=["r_st"], writes=["r_y"])
            s.op("pool", lambda e: e.tensor_tensor(out=y[:], in0=y[:], in1=gn[:], op=ALU.mult),
                 reads=["r_y", "gn"], writes=["r_y"])
            s.op("pool", lambda e: e.tensor_tensor(out=yb[b][:], in0=y[:], in1=gm[b][:], op=ALU.mult),
                 reads=["r_y", "gm%d" % b], writes=["r_yb%d" % b])

        def gn_b(c):
            b = c % 2
            tok = slice(c * 128, (c + 1) * 128)
            psb = X.ps[7][:].bitcast(BF16)
            for j in range(4):
                s.op("pe", lambda e: e.transpose(out=psb[:, j * 128:(j + 1) * 128], in_=yb[b][:, j * 128:(j + 1) * 128],
                                                 identity=X.ident[:]), reads=["r_yb%d" % b, "ident"], writes=["ps7"])
            s.op("act", lambda e: e.activation(out=rT[b][:].rearrange("p j t -> p (j t)"), in_=psb[:, 0:512], func=AF.Copy),
                 reads=["ps7"], writes=["r_rT%d" % b])
            s.dma("pool", X.AT[512:1024, tok].rearrange("(j p) t -> p j t", p=128), rT[b][:],
                  reads=["r_rT%d" % b], writes=["AT"])

        NCH = X.ret_chunks
        loads(0)
        for c in range(NCH):
            if c + 1 < NCH:
                loads(c + 1)
            t1m(c)
            upd(c)
            omm(c)
            if c >= 1:
                gn_b(c - 1)
            gn_a(c)
        gn_b(NCH - 1)
        s.barrier()


PHASES.append(("ret", phase_ret))


def conv_thunks(s, X, layers):
    th = []

    def conv(dst, src, rows):
        for r0 in range(0, rows, 512):
            th.append(lambda dst=dst, src=src, r0=r0: s.dma("pool", dst[r0:r0 + 512, :], src[r0:r0 + 512, :], writes=["W16"]))
    if 0 in layers:
        conv(X.FG16[0], X.ffn_gate[0], D)
        conv(X.FU16[0], X.ffn_up[0], D)
        conv(X.FD16[0], X.ffn_down[0], D_FF)
    if 1 in layers:
        for e in range(NE):
            for blk in range(7):
                r0 = (e * 7 + blk) * 128
                fs = slice(blk * 512, (blk + 1) * 512)
                th.append(lambda e=e, r0=r0, fs=fs: s.dma(
                    "pool", X.WGB[r0:r0 + 128, :].rearrange("p (k f) -> p k f", k=8),
                    X.moe_gate[0, e][:, fs].rearrange("(k p) f -> p k f", p=128), writes=["W16"]))
                th.append(lambda e=e, r0=r0, fs=fs: s.dma(
                    "pool", X.WUB[r0:r0 + 128, :].rearrange("p (k f) -> p k f", k=8),
                    X.moe_up[0, e][:, fs].rearrange("(k p) f -> p k f", p=128), writes=["W16"]))
                th.append(lambda e=e, r0=r0, fs=fs: s.dma(
                    "pool", X.WDB[r0:r0 + 128, :].rearrange("p (c n) -> p c n", c=4),
                    X.moe_down[0, e][fs, :].rearrange("(c p) n -> p c n", p=128), writes=["W16"]))
    return th


def phase_ffn(s, X, L, h_in):
    nc = X.nc
    last = (L == DEPTH - 1)
    moe = (L % 2 == 1)
    if moe and X.routed:
        return phase_moe(s, X, L, h_in)
    h_out = X.out if last else X.HB
    G16, U16, D16 = X.FG16, X.FU16, X.FD16
    with ExitStack() as es:
        sb = lambda name, shape, dt: es.enter_context(nc.sbuf_tensor("%s_L%d" % (name, L), shape, dt))
        wout = sb("f_wout", [128, 8, D], BF16)
        pgate = sb("f_pgate", [128, 8, D], BF16)
        pproj = sb("f_pproj", [128, 2, D], BF16)
        g2T = sb("f_g2T", [128, 8], F32)
        g3T = sb("f_g3T", [128, 8], F32)
        rw = sb("f_rw", [128, 8, NE], BF16)
        cT = sb("f_cT", [128, 8, 512], BF16)
        hin = [sb("f_hin%d" % i, [128, D], F32) for i in range(2)]
        h1 = sb("f_h1", [128, 4, D], F32)
        hnT = sb("f_hnT", [128, 8, 512], BF16)
        acc = sb("f_acc", [128, 4, D], F32)
        wg = [sb("f_wg%d" % i, [128, 8, 512], BF16) for i in range(3)]
        wu = [sb("f_wu%d" % i, [128, 8, 512], BF16) for i in range(3)]
        wd = [sb("f_wd%d" % i, [128, 4, D], BF16) for i in range(3)]
        silu_t = [sb("f_silu%d" % i, [128, 512], BF16) for i in range(2)]
        actb = [sb("f_actb%d" % i, [128, 4, 512], BF16) for i in range(2)]
        sig = [sb("f_sig%d" % i, [128, 512], F32) for i in range(2)]
        ptile = [sb("f_pt%d" % i, [128, 256], F32) for i in range(2)]
        pb = sb("f_pb", [128, 256], BF16)
        pT = sb("f_pT", [128, 2, 512], BF16)
        gates = sb("f_gates", [128, 4, NE], F32)
        lg = sb("f_lg", [128, 16], F32)
        sm = sb("f_sm", [128, 32], F32)
        for kc in range(8):
            s.dma("pool", wout[:, kc, :], X.w_out[L, kc * 128:(kc + 1) * 128, :], writes=["wout"])
            s.dma("pool", pgate[:, kc, :], X.ple_gate[L, kc * 128:(kc + 1) * 128, :], writes=["pgate"])
        for kc in range(2):
            s.dma("pool", pproj[:, kc, :], X.ple_proj[L, kc * 128:(kc + 1) * 128, :], writes=["pproj"])
        s.dma("sp", g2T[:], X.g_ffn[L].rearrange("(k p) -> p k", p=128), writes=["g2T"], allow_slow_non_contiguous=True)
        s.dma("sp", g3T[:], X.g_ple[L].rearrange("(k p) -> p k", p=128), writes=["g3T"], allow_slow_non_contiguous=True)
        if moe:
            s.dma("pool", rw[:], X.moe_router[0].rearrange("(k p) e -> p k e", p=128), writes=["rw"])
        if last:
            gfin = sb("f_gfin", [128, D], F32)
            ones1 = sb("f_ones1", [1, 128], F32)
            gf1 = sb("f_gf1", [1, D], F32)
            s.dma("sp", gf1[:], X.g_final.rearrange("(o n) -> o n", o=1), writes=["gf1"])
            s.op("dve", lambda e: e.memset(ones1[:], 1.0), writes=["ones1"])
            for half in range(2):
                s.op("pe", lambda e: e.matmul(X.ps[6][:], lhsT=ones1[:], rhs=gf1[:, half * 512:(half + 1) * 512],
                                              start=True, stop=True), reads=["ones1", "gf1"], writes=["ps6"])
                s.op("dve", lambda e: e.tensor_copy(out=gfin[:, half * 512:(half + 1) * 512], in_=X.ps[6][:]),
                     reads=["ps6"], writes=["gfin"])
        wcnt = 0
        for tg in range(NT // 4):
            toks = slice(tg * 512, (tg + 1) * 512)
            s.dma("sp", cT[:], X.AT[:, toks].rearrange("(k p) t -> p k t", p=128), reads=["AT"], writes=["cT"])
            for tt in range(4):
                ti = 4 * tg + tt
                hb_ = hin[ti % 2]
                s.dma("sp", hb_[:], h_in[ti * 128:(ti + 1) * 128, :], reads=["HB"], writes=["hin%d" % (ti % 2)])
                for half in range(2):
                    hs = slice(half * 512, (half + 1) * 512)
                    for k in range(8):
                        s.op("pe", lambda e: e.matmul(X.ps[6][:], lhsT=cT[:, k, tt * 128:(tt + 1) * 128], rhs=wout[:, k, hs],
                                                      start=(k == 0), stop=(k == 7)), reads=["cT", "wout"], writes=["ps6"])
                    s.op("dve", lambda e: e.tensor_tensor(out=h1[:, tt, hs], in0=X.ps[6][:], in1=hb_[:, hs], op=ALU.add),
                         reads=["ps6", "hin%d" % (ti % 2)], writes=["h1_%d" % tt])
                ln_a(s, X, h1[:, tt, :], "h1_%d" % tt, tt)
            for tt in range(4):
                ln_b(s, X, tt, g2T[:], "g2T", hnT[:, :, tt * 128:(tt + 1) * 128], "f_hnT")
            if moe:
                for tt in range(4):
                    for k in range(8):
                        s.op("pe", lambda e: e.matmul(X.ps[6][:, 0:NE], lhsT=hnT[:, k, tt * 128:(tt + 1) * 128], rhs=rw[:, k, :],
                                                      start=(k == 0), stop=(k == 7)), reads=["f_hnT", "rw"], writes=["ps6"])
                    s.op("dve", lambda e: e.tensor_copy(out=lg[:, 0:8], in_=X.ps[6][:, 0:NE]), reads=["ps6"], writes=["lg"])
                    s.op("dve", lambda e: e.max(out=sm[:, 0:8], in_=lg[:, 0:8]), reads=["lg"], writes=["fsm"])
                    s.op("dve", lambda e: e.tensor_tensor(out=sm[:, 8:9], in0=sm[:, 1:2], in1=sm[:, 0:1], op=ALU.subtract),
                         reads=["fsm"], writes=["fsm"])
                    s.op("act", lambda e: e.activation(out=sm[:, 9:10], in_=sm[:, 8:9], func=AF.Exp), reads=["fsm"], writes=["fsm"])
                    s.op("dve", lambda e: e.tensor_scalar(out=sm[:, 10:11], in0=sm[:, 9:10], scalar1=1.0, scalar2=None,
                                                          op0=ALU.add), reads=["fsm"], writes=["fsm"])
                    s.op("dve", lambda e: e.reciprocal(out=sm[:, 11:12], in_=sm[:, 10:11]), reads=["fsm"], writes=["fsm"])
                    s.op("dve", lambda e: e.tensor_tensor(out=sm[:, 12:13], in0=sm[:, 9:10], in1=sm[:, 11:12], op=ALU.mult),
                         reads=["fsm"], writes=["fsm"])
                    s.op("dve", lambda e: e.tensor_scalar(out=lg[:, 8:16], in0=lg[:, 0:8], scalar1=sm[:, 0:1],
                                                          scalar2=sm[:, 11:12], op0=ALU.is_equal, op1=ALU.mult),
                         reads=["lg", "fsm"], writes=["lg"])
                    s.op("dve", lambda e: e.tensor_scalar(out=gates[:, tt, :], in0=lg[:, 0:8], scalar1=sm[:, 1:2],
                                                          scalar2=sm[:, 12:13], op0=ALU.is_equal, op1=ALU.mult),
                         reads=["lg", "fsm"], writes=["gates"])
                    s.op("dve", lambda e: e.tensor_tensor(out=gates[:, tt, :], in0=gates[:, tt, :], in1=lg[:, 8:16], op=ALU.add),
                         reads=["lg"], writes=["gates"])
            def stage_a(J):
                wb, ab = J % 3, J % 2
                for fc in range(4):
                    pg, pu = 2 * (fc % 2), 2 * (fc % 2) + 1
                    for k in range(8):
                        s.op("pe", lambda e: e.matmul(X.ps[pg][:], lhsT=wg[wb][:, k, fc * 128:(fc + 1) * 128], rhs=hnT[:, k, :],
                                                      start=(k == 0), stop=(k == 7)),
                             reads=["wg%d" % wb, "f_hnT"], writes=["ps%d" % pg])
                    for k in range(8):
                        s.op("pe", lambda e: e.matmul(X.ps[pu][:], lhsT=wu[wb][:, k, fc * 128:(fc + 1) * 128], rhs=hnT[:, k, :],
                                                      start=(k == 0), stop=(k == 7)),
                             reads=["wu%d" % wb, "f_hnT"], writes=["ps%d" % pu])
                    s.op("act", lambda e: e.activation(out=silu_t[fc % 2][:], in_=X.ps[pg][:], func=AF.Silu),
                         reads=["ps%d" % pg], writes=["silu%d" % (fc % 2)])
                    s.op("dve", lambda e: e.tensor_tensor(out=actb[ab][:, fc, :], in0=X.ps[pu][:], in1=silu_t[fc % 2][:],
                                                          op=ALU.mult),
                         reads=["ps%d" % pu, "silu%d" % (fc % 2)], writes=["actb%d" % ab])

            def stage_b(J):
                wb, ab, blk = J % 3, J % 2, J % 7
                for tt in range(4):
                    for half in range(2):
                        hs = slice(half * 512, (half + 1) * 512)
                        pd = 4 + half
                        for fc in range(4):
                            s.op("pe", lambda e: e.matmul(X.ps[pd][:], lhsT=actb[ab][:, fc, tt * 128:(tt + 1) * 128],
                                                          rhs=wd[wb][:, fc, hs], start=(fc == 0), stop=(fc == 3)),
                                 reads=["actb%d" % ab, "wd%d" % wb], writes=["ps%d" % pd])
                        dst = acc[:, tt, hs]
                        if blk == 0:
                            s.op("dve", lambda e: e.tensor_copy(out=dst, in_=X.ps[pd][:]),
                                 reads=["ps%d" % pd], writes=["acc%d" % tt])
                        else:
                            s.op("dve", lambda e: e.tensor_tensor(out=dst, in0=X.ps[pd][:], in1=dst, op=ALU.add),
                                 reads=["ps%d" % pd], writes=["acc%d" % tt])

            def wload(J):
                blk, wb = J % 7, J % 3
                fs = slice(blk * 512, (blk + 1) * 512)
                s.dma("sp", wg[wb][:], G16[0][:, fs].rearrange("(k p) f -> p k f", p=128), reads=["W16"], writes=["wg%d" % wb])
                s.dma("sp", wu[wb][:], U16[0][:, fs].rearrange("(k p) f -> p k f", p=128), reads=["W16"], writes=["wu%d" % wb])
                s.dma("sp", wd[wb][:], D16[0][fs, :].rearrange("(c p) n -> p c n", p=128), reads=["W16"], writes=["wd%d" % wb])

            NJ = (NT // 4) * 7
            if tg == 0:
                wload(0)
                wload(1)
            for blk in range(7):
                J = tg * 7 + blk
                stage_a(J)
                if blk >= 1:
                    stage_b(J - 1)
                if J + 2 < NJ:
                    wload(J + 2)
            stage_b(tg * 7 + 6)
            for tt in range(4):
                s.op("pool", lambda e: e.tensor_tensor(out=h1[:, tt, :], in0=h1[:, tt, :], in1=acc[:, tt, :], op=ALU.add),
                     reads=["acc%d" % tt], writes=["h1_%d" % tt])
                ln_a(s, X, h1[:, tt, :], "h1_%d" % tt, tt)
            for tt in range(4):
                ln_b(s, X, tt, g3T[:], "g3T", hnT[:, :, tt * 128:(tt + 1) * 128], "f_hnT")
            psb = X.ps[7][:].bitcast(BF16)
            for tt in range(4):
                ti = 4 * tg + tt
                pt_ = ptile[ti % 2]
                s.dma("sp", pt_[:], X.p[L, ti * 128:(ti + 1) * 128, :], writes=["pt%d" % (ti % 2)])
                s.op("act", lambda e: e.activation(out=pb[:], in_=pt_[:], func=AF.Copy), reads=["pt%d" % (ti % 2)], writes=["pb"])
                for k in range(2):
                    s.op("pe", lambda e: e.transpose(out=psb[:, k * 128:(k + 1) * 128], in_=pb[:, k * 128:(k + 1) * 128],
                                                     identity=X.ident[:]), reads=["pb", "ident"], writes=["ps7"])
                s.op("act", lambda e: e.activation(out=pT[:, :, tt * 128:(tt + 1) * 128],
                                                   in_=psb[:, 0:256].rearrange("p (k t) -> p k t", k=2), func=AF.Copy),
                     reads=["ps7"], writes=["pT"])
            for tt in range(4):
                ti = 4 * tg + tt
                for half in range(2):
                    hs = slice(half * 512, (half + 1) * 512)
                    for k in range(2):
                        s.op("pe", lambda e: e.matmul(X.ps[6][:], lhsT=pT[:, k, tt * 128:(tt + 1) * 128], rhs=pproj[:, k, hs],
                                                      start=(k == 0), stop=(k == 1)), reads=["pT", "pproj"], writes=["ps6"])
                    pgb = 4 + half
                    for k in range(8):
                        s.op("pe", lambda e: e.matmul(X.ps[pgb][:], lhsT=hnT[:, k, tt * 128:(tt + 1) * 128], rhs=pgate[:, k, hs],
                                                      start=(k == 0), stop=(k == 7)), reads=["f_hnT", "pgate"], writes=["ps%d" % pgb])
                    s.op("act", lambda e: e.activation(out=sig[half][:], in_=X.ps[pgb][:], func=AF.Sigmoid),
                         reads=["ps%d" % pgb], writes=["sig%d" % half])
                    s.op("dve", lambda e: e.tensor_tensor(out=acc[:, tt, hs], in0=X.ps[6][:], in1=sig[half][:], op=ALU.mult),
                         reads=["ps6", "sig%d" % half], writes=["acc%d" % tt])
                s.op("pool", lambda e: e.tensor_tensor(out=h1[:, tt, :], in0=h1[:, tt, :], in1=acc[:, tt, :], op=ALU.add),
                     reads=["acc%d" % tt], writes=["h1_%d" % tt])
                if last:
                    ln_a(s, X, h1[:, tt, :], "h1_%d" % tt, tt)
                    s.op("act", lambda e: e.activation(out=acc[:, tt, :], in_=h1[:, tt, :], func=AF.Copy, scale=X.ss[tt][:, 2:3]),
                         reads=["h1_%d" % tt, "ss%d" % tt], writes=["acc%d" % tt])
                    s.op("pool", lambda e: e.tensor_tensor(out=acc[:, tt, :], in0=acc[:, tt, :], in1=gfin[:], op=ALU.mult),
                         reads=["gfin"], writes=["acc%d" % tt])
                    s.dma("pool", h_out[ti * 128:(ti + 1) * 128, :], acc[:, tt, :], reads=["acc%d" % tt], writes=["HOUT"])
                else:
                    s.dma("pool", h_out[ti * 128:(ti + 1) * 128, :], h1[:, tt, :], reads=["h1_%d" % tt], writes=["HB"])
        s.barrier()


PHASES.append(("ffn", phase_ffn))


def phase_moe(s, X, L, h_in):
    nc = X.nc
    last = (L == DEPTH - 1)
    h_out = X.out if last else X.HB
    with ExitStack() as es:
        sb = lambda name, shape, dt: es.enter_context(nc.sbuf_tensor("%s_L%d" % (name, L), shape, dt))
        OH = [sb("m_oh%d" % k, [128, NT, NE], F32) for k in range(2)]
        WK = [sb("m_wk%d" % k, [128, NT], F32) for k in range(2)]
        POSI2 = sb("m_posi", [128, 2 * NT], I32)
        POSI = POSI2[:].rearrange("p (k i) -> p k i", k=2)
        WIDX2 = sb("m_widx", [128, NGRP * 7], I32)
        WIDX = WIDX2[:].rearrange("p (g b) -> p g b", b=7)
        g2T = sb("m_g2T", [128, 8], F32)
        g3T = sb("m_g3T", [128, 8], F32)
        hnT = sb("m_hnT", [128, 8, 512], BF16)
        s.dma("sp", g2T[:], X.g_ffn[L].rearrange("(k p) -> p k", p=128), writes=["g2T"], allow_slow_non_contiguous=True)
        s.dma("sp", g3T[:], X.g_ple[L].rearrange("(k p) -> p k", p=128), writes=["g3T"], allow_slow_non_contiguous=True)
        with ExitStack() as es2:
            sb2 = lambda name, shape, dt: es2.enter_context(nc.sbuf_tensor("%s_L%d" % (name, L), shape, dt))
            wout = sb2("ma_wout", [128, 8, D], BF16)
            rw = sb2("ma_rw", [128, 8, NE], BF16)
            cT = sb2("ma_cT", [128, 8, 512], BF16)
            hin = [sb2("ma_hin%d" % i, [128, D], F32) for i in range(2)]
            h1 = [sb2("ma_h1%d" % i, [128, D], F32) for i in range(4)]
            lg = sb2("ma_lg", [128, 16], F32)
            sm = sb2("ma_sm", [128, 32], F32)
            for kc in range(8):
                s.dma("pool", wout[:, kc, :], X.w_out[L, kc * 128:(kc + 1) * 128, :], writes=["wout"])
            s.dma("pool", rw[:], X.moe_router[0].rearrange("(k p) e -> p k e", p=128), writes=["rw"])
            for tg in range(NT // 4):
                toks = slice(tg * 512, (tg + 1) * 512)
                s.dma("sp", cT[:], X.AT[:, toks].rearrange("(k p) t -> p k t", p=128), reads=["AT"], writes=["cT"])
                for tt in range(4):
                    ti = 4 * tg + tt
                    hb_ = hin[ti % 2]
                    s.dma("sp", hb_[:], h_in[ti * 128:(ti + 1) * 128, :], reads=["HB"], writes=["hin%d" % (ti % 2)])
                    for half in range(2):
                        hs = slice(half * 512, (half + 1) * 512)
                        pbk = 4 + half
                        for k in range(8):
                            s.op("pe", lambda e: e.matmul(X.ps[pbk][:], lhsT=cT[:, k, tt * 128:(tt + 1) * 128], rhs=wout[:, k, hs],
                                                          start=(k == 0), stop=(k == 7)), reads=["cT", "wout"], writes=["ps%d" % pbk])
                        s.op("dve", lambda e: e.tensor_tensor(out=h1[tt][:, hs], in0=X.ps[pbk][:], in1=hb_[:, hs], op=ALU.add),
                             reads=["ps%d" % pbk, "hin%d" % (ti % 2)], writes=["h1_%d" % tt])
                    s.dma("pool", X.H1[ti * 128:(ti + 1) * 128, :], h1[tt][:], reads=["h1_%d" % tt], writes=["H1"])
                    ln_a(s, X, h1[tt][:], "h1_%d" % tt, tt)
                    s.dma("pool", X.XS2[ti * 128:(ti + 1) * 128, :], X.xs[tt][:], reads=["xs%d" % tt], writes=["XS2"])
                for tt in range(4):
                    ln_b(s, X, tt, g2T[:], "g2T", hnT[:, :, tt * 128:(tt + 1) * 128], "m_hnT")
                for tt in range(4):
                    ti = 4 * tg + tt
                    for k in range(8):
                        s.op("pe", lambda e: e.matmul(X.ps[6][:, 0:NE], lhsT=hnT[:, k, tt * 128:(tt + 1) * 128], rhs=rw[:, k, :],
                                                      start=(k == 0), stop=(k == 7)), reads=["m_hnT", "rw"], writes=["ps6"])
                    s.op("dve", lambda e: e.tensor_copy(out=lg[:, 0:8], in_=X.ps[6][:, 0:NE]), reads=["ps6"], writes=["lg"])
                    s.op("dve", lambda e: e.max(out=sm[:, 0:8], in_=lg[:, 0:8]), reads=["lg"], writes=["fsm"])
                    s.op("dve", lambda e: e.tensor_tensor(out=sm[:, 8:9], in0=sm[:, 1:2], in1=sm[:, 0:1], op=ALU.subtract),
                         reads=["fsm"], writes=["fsm"])
                    s.op("act", lambda e: e.activation(out=sm[:, 9:10], in_=sm[:, 8:9], func=AF.Exp), reads=["fsm"], writes=["fsm"])
                    s.op("dve", lambda e: e.tensor_scalar(out=sm[:, 10:11], in0=sm[:, 9:10], scalar1=1.0, scalar2=None,
                                                          op0=ALU.add), reads=["fsm"], writes=["fsm"])
                    s.op("dve", lambda e: e.reciprocal(out=WK[0][:, ti:ti + 1], in_=sm[:, 10:11]), reads=["fsm"], writes=["WK"])
                    s.op("dve", lambda e: e.tensor_tensor(out=WK[1][:, ti:ti + 1], in0=sm[:, 9:10], in1=WK[0][:, ti:ti + 1],
                                                          op=ALU.mult), reads=["fsm", "WK"], writes=["WK"])
                    s.op("dve", lambda e: e.tensor_scalar(out=OH[0][:, ti, :], in0=lg[:, 0:8], scalar1=sm[:, 0:1], scalar2=None,
                                                          op0=ALU.is_equal), reads=["lg", "fsm"], writes=["OH"])
                    s.op("dve", lambda e: e.tensor_scalar(out=OH[1][:, ti, :], in0=lg[:, 0:8], scalar1=sm[:, 1:2], scalar2=None,
                                                          op0=ALU.is_equal), reads=["lg", "fsm"], writes=["OH"])
            s.barrier()
        with ExitStack() as es2:
            sb2 = lambda name, shape, dt: es2.enter_context(nc.sbuf_tensor("%s_L%d" % (name, L), shape, dt))
            Mf = sb2("mb_Mf", [128, NT, NE], F32)
            Mb = sb2("mb_Mb", [128, NT * NE], BF16)
            U = sb2("mb_U", [128, 128], BF16)
            ones = sb2("mb_ones", [128, 128], BF16)
            A_ = sb2("mb_A", [128, NT, NE], F32)
            B_ = sb2("mb_B", [128, NT, NE], F32)
            CN = sb2("mb_CN", [128, NT, NE], F32)
            POS = sb2("mb_POS", [128, NT, NE], F32)
            nE = sb2("mb_nE", [128, 64], F32)
            cmp16 = sb2("mb_cmp16", [128, NE, 16], F32)
            gi512 = sb2("mb_gi512", [128, NGRP], F32)
            tokid = sb2("mb_tokid", [128, NT], F32)
            blkp = sb2("mb_blkp", [128, 7], F32)
            cmpg = sb2("mb_cmpg", [128, NGRP, NE], F32)
            EG = sb2("mb_EG", [128, NGRP], F32)
            WF = sb2("mb_WF", [128, NGRP, 7], F32)
            PK = sb2("mb_PK", [128, 2, NT], F32)
            ROWS2 = sb2("mb_rows", [128, 2 * NT * 2], I32)
            ROWS = ROWS2[:].rearrange("p (k i c) -> p k i c", k=2, c=2)
            Z = sb2("mb_Z", [128, 2 * NSLOT // 128], I32)
            s.dma("pool", U[:], X.c["c_U"], writes=["U"])
            s.dma("sp", gi512[:], X.c["c_gi512"], writes=["gi512"])
            s.dma("sp", tokid[:], X.c["c_tokid"], writes=["tokid"])
            s.dma("sp", blkp[:], X.c["c_blkp"], writes=["blkp"])
            s.op("pool", lambda e: e.memset(ones[:], 1.0), writes=["ones"])
            s.op("pool", lambda e: e.memset(Z[:], 0), writes=["Z"])
            s.dma("sp", X.SLOT.rearrange("(p a) c -> p (a c)", p=128), Z[:], reads=["Z"], writes=["SLOT"])
            s.op("dve", lambda e: e.tensor_tensor(out=Mf[:], in0=OH[0][:], in1=OH[1][:], op=ALU.add), reads=["OH"], writes=["Mf"])
            s.op("dve", lambda e: e.tensor_copy(out=Mb[:], in_=Mf[:].rearrange("p i e -> p (i e)")), reads=["Mf"], writes=["Mb"])
            s.op("pe", lambda e: e.matmul(X.ps[0][:], lhsT=U[:], rhs=Mb[:], start=True, stop=True), reads=["U", "Mb"], writes=["ps0"])
            s.op("pe", lambda e: e.matmul(X.ps[1][:], lhsT=ones[:], rhs=Mb[:], start=True, stop=True), reads=["ones", "Mb"], writes=["ps1"])
            s.op("dve", lambda e: e.tensor_copy(out=CN[:].rearrange("p i e -> p (i e)"), in_=X.ps[1][:]), reads=["ps1"], writes=["CN"])
            s.op("dve", lambda e: e.tensor_copy(out=A_[:], in_=CN[:]), reads=["CN"], writes=["A"])
            src, dst, sres, dres = A_, B_, "A", "B"
            for sh in (1, 2, 4, 8, 16, 32):
                s.op("dve", lambda e: e.tensor_copy(out=dst[:, 0:sh, :], in_=src[:, 0:sh, :]), reads=[sres], writes=[dres])
                s.op("dve", lambda e: e.tensor_tensor(out=dst[:, sh:NT, :], in0=src[:, sh:NT, :], in1=src[:, 0:NT - sh, :],
                                                      op=ALU.add), reads=[sres], writes=[dres])
                src, dst, sres, dres = dst, src, dres, sres
            incl, ires = src, sres
            s.op("dve", lambda e: e.tensor_copy(out=nE[:, 0:8], in_=incl[:, NT - 1, :]), reads=[ires], writes=["nE"])
            s.op("dve", lambda e: e.tensor_tensor(out=cmp16[:], in0=bcast_mid(nE[:, 0:8], 16), in1=bcast_rep(gi512[:, 0:16], NE),
                                                  op=ALU.is_gt), reads=["nE", "gi512"], writes=["cmp16"])
            s.op("dve", lambda e: e.tensor_reduce(out=nE[:, 8:16], in_=cmp16[:], axis=AX.X, op=ALU.add), reads=["cmp16"], writes=["nE"])
            s.op("dve", lambda e: e.tensor_scalar(out=nE[:, 8:16], in0=nE[:, 8:16], scalar1=512.0, scalar2=None, op0=ALU.mult),
                 reads=["nE"], writes=["nE"])
            s.op("dve", lambda e: e.tensor_copy(out=nE[:, 16:17], in_=nE[:, 8:9]), reads=["nE"], writes=["nE"])
            for e_ in range(1, NE):
                s.op("dve", lambda e: e.tensor_tensor(out=nE[:, 16 + e_:17 + e_], in0=nE[:, 15 + e_:16 + e_], in1=nE[:, 8 + e_:9 + e_],
                                                      op=ALU.add), reads=["nE"], writes=["nE"])
            s.op("dve", lambda e: e.tensor_tensor(out=nE[:, 24:32], in0=nE[:, 16:24], in1=nE[:, 8:16], op=ALU.subtract),
                 reads=["nE"], writes=["nE"])
            s.op("dve", lambda e: e.tensor_tensor(out=POS[:], in0=incl[:], in1=CN[:], op=ALU.subtract), reads=[ires, "CN"], writes=["POS"])
            s.op("dve", lambda e: e.tensor_tensor(out=POS[:].rearrange("p i e -> p (i e)"), in0=POS[:].rearrange("p i e -> p (i e)"),
                                                  in1=X.ps[0][:], op=ALU.add), reads=["ps0"], writes=["POS"])
            s.op("dve", lambda e: e.tensor_tensor(out=POS[:], in0=POS[:], in1=bcast_rep(nE[:, 24:32], NT), op=ALU.add),
                 reads=["nE"], writes=["POS"])
            for k in range(2):
                s.op("dve", lambda e: e.tensor_tensor(out=Mf[:], in0=POS[:], in1=OH[k][:], op=ALU.mult), reads=["POS", "OH"], writes=["Mf"])
                s.op("dve", lambda e: e.tensor_reduce(out=PK[:, k, :], in_=Mf[:], axis=AX.X, op=ALU.add), reads=["Mf"], writes=["PK"])
                s.op("dve", lambda e: e.tensor_copy(out=ROWS[:, k, :, 0], in_=tokid[:]), reads=["tokid"], writes=["ROWS"])
                s.op("dve", lambda e: e.tensor_copy(out=ROWS[:, k, :, 1], in_=WK[k][:].bitcast(I32)), reads=["WK"], writes=["ROWS"])
            s.op("dve", lambda e: e.tensor_copy(out=POSI, in_=PK[:]), reads=["PK"], writes=["POSI"])
            s.op("dve", lambda e: e.tensor_tensor(out=cmpg[:], in0=bcast_rep(nE[:, 16:24], NGRP), in1=bcast_mid(gi512[:], NE),
                                                  op=ALU.is_le), reads=["nE", "gi512"], writes=["cmpg"])
            s.op("dve", lambda e: e.tensor_reduce(out=EG[:], in_=cmpg[:], axis=AX.X, op=ALU.add), reads=["cmpg"], writes=["EG"])
            s.op("dve", lambda e: e.tensor_scalar(out=EG[:], in0=EG[:], scalar1=float(NE - 1), scalar2=896.0, op0=ALU.min, op1=ALU.mult),
                 reads=["EG"], writes=["EG"])
            s.op("dve", lambda e: e.tensor_tensor(out=WF[:], in0=bcast_mid(EG[:], 7), in1=bcast_rep(blkp[:], NGRP), op=ALU.add),
                 reads=["EG", "blkp"], writes=["WF"])
            s.op("dve", lambda e: e.tensor_copy(out=WIDX, in_=WF[:]), reads=["WF"], writes=["WIDX"])
            for ti in range(NT):
                for k in range(2):
                    s.idma(X.SLOT[:, :], POSI2[:, k * NT + ti:k * NT + ti + 1],
                           ROWS2[:, (k * NT + ti) * 2:(k * NT + ti) * 2 + 2], None, NSLOT - 1,
                           reads=["ROWS", "POSI"], writes=["SLOT"])
            s.barrier()
        with ExitStack() as es2:
            sb2 = lambda name, shape, dt: es2.enter_context(nc.sbuf_tensor("%s_L%d" % (name, L), shape, dt))
            stt = [sb2("mc_st%d" % i, [128, 8], I32) for i in range(2)]
            xg = [sb2("mc_xg%d" % i, [128, D], BF16) for i in range(8)]
            acc = [sb2("mc_acc%d" % i, [128, 4, D], F32) for i in range(2)]
            wg = [sb2("mc_wg%d" % i, [128, 8, 512], BF16) for i in range(3)]
            wu = [sb2("mc_wu%d" % i, [128, 8, 512], BF16) for i in range(3)]
            wd = [sb2("mc_wd%d" % i, [128, 4, D], BF16) for i in range(3)]
            silu_t = [sb2("mc_silu%d" % i, [128, 512], BF16) for i in range(2)]
            actb = [sb2("mc_actb%d" % i, [128, 4, 512], BF16) for i in range(2)]
            hn = [sb2("mc_hn%d" % i, [128, 8, 512], BF16) for i in range(2)]
            NWB = len(wg)

            def prologue_loads(gi):
                gb = gi % 2
                s.dma("sp", stt[gb][:].rearrange("p (tt c) -> p tt c", c=2),
                      X.SLOT[gi * 512:(gi + 1) * 512, :].rearrange("(tt p) c -> p tt c", p=128),
                      reads=["SLOT"], writes=["st%d" % gb])
                for tt in range(4):
                    s.idma(xg[gb * 4 + tt][:, :], None, X.XS2[:, :], stt[gb][:, 2 * tt:2 * tt + 1], SEQ - 1,
                           reads=["XS2", "st%d" % gb], writes=["xg%d" % (gb * 4 + tt)])

            def prologue_tr(gi):
                gb = gi % 2
                for tt in range(4):
                    ln_b(s, X, tt, g2T[:], "g2T", hn[gb][:, :, tt * 128:(tt + 1) * 128], "hn%d" % gb,
                         src=xg[gb * 4 + tt], src_res="xg%d" % (gb * 4 + tt))

            def wload(j):
                gi, blk = divmod(j, 7)
                wb = j % NWB
                off = WIDX2[:, gi * 7 + blk:gi * 7 + blk + 1]
                s.idma(wg[wb][:].rearrange("p k f -> p (k f)"), None, X.WGB[:, :], off, 0, reads=["W16", "WIDX"], writes=["wg%d" % wb])
                s.idma(wu[wb][:].rearrange("p k f -> p (k f)"), None, X.WUB[:, :], off, 0, reads=["W16", "WIDX"], writes=["wu%d" % wb])
                s.idma(wd[wb][:].rearrange("p c n -> p (c n)"), None, X.WDB[:, :], off, 0, reads=["W16", "WIDX"], writes=["wd%d" % wb])

            def stage_a(j):
                gi, blk = divmod(j, 7)
                gb, wb, ab = gi % 2, j % NWB, j % 2
                for fc in range(4):
                    pg, pu = 2 * (fc % 2), 2 * (fc % 2) + 1
                    for k in range(8):
                        s.op("pe", lambda e: e.matmul(X.ps[pg][:], lhsT=wg[wb][:, k, fc * 128:(fc + 1) * 128], rhs=hn[gb][:, k, :],
                                                      start=(k == 0), stop=(k == 7)),
                             reads=["wg%d" % wb, "hn%d" % gb], writes=["ps%d" % pg])
                    for k in range(8):
                        s.op("pe", lambda e: e.matmul(X.ps[pu][:], lhsT=wu[wb][:, k, fc * 128:(fc + 1) * 128], rhs=hn[gb][:, k, :],
                                                      start=(k == 0), stop=(k == 7)),
                             reads=["wu%d" % wb, "hn%d" % gb], writes=["ps%d" % pu])
                    s.op("act", lambda e: e.activation(out=silu_t[fc % 2][:], in_=X.ps[pg][:], func=AF.Silu),
                         reads=["ps%d" % pg], writes=["silu%d" % (fc % 2)])
                    s.op("dve", lambda e: e.tensor_tensor(out=actb[ab][:, fc, :], in0=X.ps[pu][:], in1=silu_t[fc % 2][:],
                                                          op=ALU.mult),
                         reads=["ps%d" % pu, "silu%d" % (fc % 2)], writes=["actb%d" % ab])

            def stage_b(j):
                gi, blk = divmod(j, 7)
                gb, wb, ab = gi % 2, j % NWB, j % 2
                for tt in range(4):
                    gsc = stt[gb][:, 2 * tt + 1:2 * tt + 2].bitcast(F32)
                    for half in range(2):
                        hs = slice(half * 512, (half + 1) * 512)
                        pd = 4 + half
                        for fc in range(4):
                            s.op("pe", lambda e: e.matmul(X.ps[pd][:], lhsT=actb[ab][:, fc, tt * 128:(tt + 1) * 128],
                                                          rhs=wd[wb][:, fc, hs], start=(fc == 0), stop=(fc == 3)),
                                 reads=["actb%d" % ab, "wd%d" % wb], writes=["ps%d" % pd])
                        dst = acc[gb][:, tt, hs]
                        if blk == 0:
                            s.op("dve", lambda e: e.tensor_scalar(out=dst, in0=X.ps[pd][:], scalar1=gsc, scalar2=None, op0=ALU.mult),
                                 reads=["ps%d" % pd, "st%d" % gb], writes=["acc%d_%d" % (gb, tt)])
                        else:
                            s.op("dve", lambda e: e.scalar_tensor_tensor(out=dst, in0=X.ps[pd][:], scalar=gsc, in1=dst,
                                                                         op0=ALU.mult, op1=ALU.add),
                                 reads=["ps%d" % pd, "st%d" % gb], writes=["acc%d_%d" % (gb, tt)])
                if blk == 6:
                    s.dma("sp", X.YS[gi * 512:(gi + 1) * 512, :].rearrange("(tt p) n -> p tt n", p=128), acc[gb][:],
                          reads=["acc%d_%d" % (gb, tt) for tt in range(4)], writes=["YS"])

            NJ = NGRP * 7
            prologue_loads(0)
            prologue_tr(0)
            for j in range(min(NWB - 1, NJ)):
                wload(j)
            for j in range(NJ + 1):
                if j < NJ:
                    gi, blk = divmod(j, 7)
                    stage_a(j)
                    if blk == 6 and gi + 1 < NGRP:
                        prologue_tr(gi + 1)
                if j >= 1:
                    stage_b(j - 1)
                if j < NJ and j % 7 == 0 and j // 7 + 1 < NGRP:
                    prologue_loads(j // 7 + 1)
                if j + NWB - 1 < NJ:
                    wload(j + NWB - 1)
            s.barrier()
        with ExitStack() as es2:
            sb2 = lambda name, shape, dt: es2.enter_context(nc.sbuf_tensor("%s_L%d" % (name, L), shape, dt))
            pgate = sb2("md_pgate", [128, 8, D], BF16)
            pproj = sb2("md_pproj", [128, 2, D], BF16)
            h1 = [sb2("md_h1%d" % i, [128, 4, D], F32) for i in range(2)]
            acc = [sb2("md_acc%d" % i, [128, 4, D], F32) for i in range(2)]
            hnd = [sb2("md_hnd%d" % i, [128, 8, 512], BF16) for i in range(2)]
            yA = [sb2("md_yA%d" % i, [128, D], F32) for i in range(2)]
            yB = [sb2("md_yB%d" % i, [128, D], F32) for i in range(2)]
            sig = [sb2("md_sig%d" % i, [128, 512], F32) for i in range(2)]
            ptile = [sb2("md_pt%d" % i, [128, 256], F32) for i in range(2)]
            pb = sb2("md_pb", [128, 256], BF16)
            pT = [sb2("md_pT%d" % i, [128, 2, 512], BF16) for i in range(2)]
            for kc in range(8):
                s.dma("pool", pgate[:, kc, :], X.ple_gate[L, kc * 128:(kc + 1) * 128, :], writes=["pgate"])
            for kc in range(2):
                s.dma("pool", pproj[:, kc, :], X.ple_proj[L, kc * 128:(kc + 1) * 128, :], writes=["pproj"])
            if last:
                gfin = sb2("md_gfin", [128, D], F32)
                ones1 = sb2("md_ones1", [1, 128], F32)
                gf1 = sb2("md_gf1", [1, D], F32)
                s.dma("sp", gf1[:], X.g_final.rearrange("(o n) -> o n", o=1), writes=["gf1"])
                s.op("dve", lambda e: e.memset(ones1[:], 1.0), writes=["ones1"])
                for half in range(2):
                    s.op("pe", lambda e: e.matmul(X.ps[6][:], lhsT=ones1[:], rhs=gf1[:, half * 512:(half + 1) * 512],
                                                  start=True, stop=True), reads=["ones1", "gf1"], writes=["ps6"])
                    s.op("dve", lambda e: e.tensor_copy(out=gfin[:, half * 512:(half + 1) * 512], in_=X.ps[6][:]),
                         reads=["ps6"], writes=["gfin"])
            def stage1(tg):
                hb = tg % 2
                for tt in range(4):
                    ti = 4 * tg + tt
                    yb_ = ti % 2
                    hres = "h1_%d_%d" % (hb, tt)
                    s.dma("sp", h1[hb][:, tt, :], X.H1[ti * 128:(ti + 1) * 128, :], reads=["H1"], writes=[hres])
                    s.idma(yA[yb_][:, :], None, X.YS[:, :], POSI2[:, ti:ti + 1], 0, reads=["YS", "POSI"], writes=["yA%d" % yb_])
                    s.idma(yB[yb_][:, :], None, X.YS[:, :], POSI2[:, NT + ti:NT + ti + 1], 0, reads=["YS", "POSI"], writes=["yB%d" % yb_])
                    s.op("dve", lambda e: e.tensor_tensor(out=h1[hb][:, tt, :], in0=h1[hb][:, tt, :], in1=yA[yb_][:], op=ALU.add),
                         reads=["yA%d" % yb_], writes=[hres])
                    s.op("dve", lambda e: e.tensor_tensor(out=h1[hb][:, tt, :], in0=h1[hb][:, tt, :], in1=yB[yb_][:], op=ALU.add),
                         reads=["yB%d" % yb_], writes=[hres])
                    ln_a(s, X, h1[hb][:, tt, :], hres, tt)
                for tt in range(4):
                    ln_b(s, X, tt, g3T[:], "g3T", hnd[hb][:, :, tt * 128:(tt + 1) * 128], "hnd%d" % hb)
                psb = X.ps[7][:].bitcast(BF16)
                for tt in range(4):
                    ti = 4 * tg + tt
                    pt_ = ptile[ti % 2]
                    s.dma("sp", pt_[:], X.p[L, ti * 128:(ti + 1) * 128, :], writes=["pt%d" % (ti % 2)])
                    s.op("act", lambda e: e.activation(out=pb[:], in_=pt_[:], func=AF.Copy), reads=["pt%d" % (ti % 2)], writes=["pb"])
                    for k in range(2):
                        s.op("pe", lambda e: e.transpose(out=psb[:, k * 128:(k + 1) * 128], in_=pb[:, k * 128:(k + 1) * 128],
                                                         identity=X.ident[:]), reads=["pb", "ident"], writes=["ps7"])
                    s.op("act", lambda e: e.activation(out=pT[hb][:, :, tt * 128:(tt + 1) * 128],
                                                       in_=psb[:, 0:256].rearrange("p (k t) -> p k t", k=2), func=AF.Copy),
                         reads=["ps7"], writes=["pT%d" % hb])

            def stage2(tg):
                hb = tg % 2
                for tt in range(4):
                    ti = 4 * tg + tt
                    hres = "h1_%d_%d" % (hb, tt)
                    ares = "dacc%d_%d" % (hb, tt)
                    for half in range(2):
                        hs = slice(half * 512, (half + 1) * 512)
                        for k in range(2):
                            s.op("pe", lambda e: e.matmul(X.ps[6][:], lhsT=pT[hb][:, k, tt * 128:(tt + 1) * 128], rhs=pproj[:, k, hs],
                                                          start=(k == 0), stop=(k == 1)), reads=["pT%d" % hb, "pproj"], writes=["ps6"])
                        pgb = 4 + half
                        for k in range(8):
                            s.op("pe", lambda e: e.matmul(X.ps[pgb][:], lhsT=hnd[hb][:, k, tt * 128:(tt + 1) * 128], rhs=pgate[:, k, hs],
                                                          start=(k == 0), stop=(k == 7)), reads=["hnd%d" % hb, "pgate"], writes=["ps%d" % pgb])
                        s.op("act", lambda e: e.activation(out=sig[half][:], in_=X.ps[pgb][:], func=AF.Sigmoid),
                             reads=["ps%d" % pgb], writes=["sig%d" % half])
                        s.op("dve", lambda e: e.tensor_tensor(out=acc[hb][:, tt, hs], in0=X.ps[6][:], in1=sig[half][:], op=ALU.mult),
                             reads=["ps6", "sig%d" % half], writes=[ares])
                    s.op("pool", lambda e: e.tensor_tensor(out=h1[hb][:, tt, :], in0=h1[hb][:, tt, :], in1=acc[hb][:, tt, :], op=ALU.add),
                         reads=[ares], writes=[hres])
                    if last:
                        k4 = 4 + tt % 4 if len(X.ss) > 4 else tt
                        ln_a(s, X, h1[hb][:, tt, :], hres, k4)
                        s.op("act", lambda e: e.activation(out=acc[hb][:, tt, :], in_=h1[hb][:, tt, :], func=AF.Copy, scale=X.ss[k4][:, 2:3]),
                             reads=[hres, "ss%d" % k4], writes=[ares])
                        s.op("pool", lambda e: e.tensor_tensor(out=acc[hb][:, tt, :], in0=acc[hb][:, tt, :], in1=gfin[:], op=ALU.mult),
                             reads=["gfin"], writes=[ares])
                        s.dma("pool", h_out[ti * 128:(ti + 1) * 128, :], acc[hb][:, tt, :], reads=[ares], writes=["HOUT"])
                    else:
                        s.dma("pool", h_out[ti * 128:(ti + 1) * 128, :], h1[hb][:, tt, :], reads=[hres], writes=["HB"])

            stage1(0)
            for tg in range(NT // 4):
                if tg + 1 < NT // 4:
                    stage1(tg + 1)
                stage2(tg)
            s.barrier()
        s.barrier()
```

```python
import math
import os
from contextlib import ExitStack

import numpy as np
import concourse.bass as bass
import concourse.mybir as mybir
from concourse.bass_utils import run_bass_kernel_spmd

F32 = mybir.dt.float32
BF16 = mybir.dt.bfloat16
AF = mybir.ActivationFunctionType
ALU = mybir.AluOpType
AX = mybir.AxisListType

D = 1024
SEQ = 8192
NT = SEQ // 128
DEPTH = 2
HD = 64
IN_COLS = 3352
D_FF = 3584
NE = 8
EPS = 1e-6
NEG = -30000.0
NGRP = 40
NSLOT = NGRP * 512
I32 = mybir.dt.int32

C_QN, C_KC, C_VC, C_KS, C_VS, C_KW, C_VW, C_GL, C_QR, C_KR, C_VR, C_GR = (
    0, 512, 640, 768, 896, 1024, 1152, 1280, 1304, 1816, 2328, 2840)


class S:
    def __init__(self, nc, es, n_dma_sems=24):
        self.nc = nc
        self.eng = {"pe": nc.tensor, "dve": nc.vector, "act": nc.scalar,
                    "pool": nc.gpsimd, "sp": nc.sync}
        self.sem = {k: es.enter_context(nc.semaphore("c_" + k)) for k in self.eng}
        self.cnt = {k: 0 for k in self.eng}
        self.seen = {k: {} for k in self.eng}
        self.drained = {k: 0 for k in self.eng}
        self.dsem = [es.enter_context(nc.semaphore("d%d" % i)) for i in range(n_dma_sems)]
        self.dval = [0] * n_dma_sems
        self.dnext = 0
        self.lastw = {}
        self.readers = {}
        self.nops = 0

    def _wait(self, eng, tok):
        if tok is None:
            return
        if tok[0] == "c":
            _, e2, c = tok
            if e2 == eng:
                if eng != "pe" and c > self.drained[eng]:
                    self.eng[eng].drain()
                    self.drained[eng] = self.cnt[eng]
                return
            key = e2
            sem = self.sem[e2]
        else:
            _, si, c = tok
            key = ("d", si)
            sem = self.dsem[si]
        if self.seen[eng].get(key, 0) >= c:
            return
        self.eng[eng].wait_ge(sem, c)
        self.seen[eng][key] = c

    def _deps(self, eng, reads, writes):
        for r in reads:
            self._wait(eng, self.lastw.get(r))
        for w in writes:
            self._wait(eng, self.lastw.get(w))
            for t in self.readers.get(w, {}).values():
                self._wait(eng, t)

    def _commit(self, tok, reads, writes):
        for r in reads:
            self.readers.setdefault(r, {})[tok[1] if tok[0] == "c" else ("d", tok[1])] = tok
        for w in writes:
            self.lastw[w] = tok
            self.readers[w] = {}

    def op(self, eng, fn, reads=(), writes=()):
        self._deps(eng, reads, writes)
        ins = fn(self.eng[eng])
        self.cnt[eng] += 1
        ins.then_inc(self.sem[eng], 1)
        self._commit(("c", eng, self.cnt[eng]), reads, writes)
        self.nops += 1

    def dma(self, q, out, in_, reads=(), writes=(), **kw):
        self._deps(q, reads, writes)
        si = self.dnext
        self.dnext = (self.dnext + 1) % len(self.dsem)
        self._wait(q, ("d", si, self.dval[si]))
        ins = self.eng[q].dma_start(out=out, in_=in_, **kw)
        self.dval[si] += 16
        ins.then_inc(self.dsem[si], 16)
        self._commit(("d", si, self.dval[si]), reads, writes)
        self.nops += 1

    def idma(self, out, out_off, in_, in_off, bound, reads=(), writes=()):
        q = "pool"
        self._deps(q, reads, writes)
        si = self.dnext
        self.dnext = (self.dnext + 1) % len(self.dsem)
        self._wait(q, ("d", si, self.dval[si]))
        oo = None if out_off is None else bass.IndirectOffsetOnAxis(ap=out_off, axis=0)
        io = None if in_off is None else bass.IndirectOffsetOnAxis(ap=in_off, axis=0)
        ins = self.nc.gpsimd.indirect_dma_start(out=out, out_offset=oo, in_=in_, in_offset=io)
        self.dval[si] += 16
        ins.then_inc(self.dsem[si], 16)
        self._commit(("d", si, self.dval[si]), reads, writes)
        self.nops += 1

    def barrier(self):
        for e in self.eng:
            for e2 in self.eng:
                if e2 != e and self.cnt[e2] > 0:
                    self._wait(e, ("c", e2, self.cnt[e2]))
            for si in range(len(self.dsem)):
                if self.dval[si] > 0:
                    self._wait(e, ("d", si, self.dval[si]))
        self.lastw = {}
        self.readers = {}

    def drain(self, eng="sp"):
        for r, t in list(self.lastw.items()):
            self._wait(eng, t)
        for r, d in list(self.readers.items()):
            for t in d.values():
                self._wait(eng, t)


def bcast_mid(ap2, n):
    return ap2.unsqueeze(2).to_broadcast([ap2.shape[0], ap2.shape[1], n])


def bcast_rep(ap2, n):
    return ap2.unsqueeze(1).to_broadcast([ap2.shape[0], n, ap2.shape[1]])


def make_consts():
    c = {}
    c["c_ident"] = np.eye(128, dtype=np.float32)
    t = np.arange(SEQ)
    slopes = 2.0 ** (-(np.arange(8) + 1.0))
    qa = np.zeros((8, 3, SEQ), np.float32)
    for h in range(8):
        qa[h, 0] = 128.0 * slopes[h]
        qa[h, 1] = slopes[h]
        qa[h, 2] = -slopes[h] * 128.0 * (t // 128 + 1)
    c["c_qaug"] = qa
    ka = np.zeros((3, SEQ), np.float32)
    ka[0] = t // 128
    ka[1] = t % 128
    ka[2] = 1.0
    c["c_kaug"] = ka
    n = np.arange(512)
    pos = 16 * n + 31
    kc = np.zeros((3, 512), np.float32)
    kc[0] = pos // 128
    kc[1] = pos % 128
    kc[2] = 1.0
    c["c_kcaug"] = kc
    hh = np.arange(8)
    lg = np.log1p(-(2.0 ** (-5.0 - hh)))
    nn = np.arange(128)
    xiT = np.zeros((4, 128, 128), np.float32)
    kdT = np.zeros((4, 128, 128), np.float32)
    gC = np.zeros((128, 4), np.float32)
    for hp in range(4):
        for half in range(2):
            h = 2 * hp + half
            xiT[hp, half * 64:(half + 1) * 64, :] = np.exp((nn + 1.0) * lg[h])[None, :]
            kdT[hp, half * 64:(half + 1) * 64, :] = 0.125 * np.exp(-(nn + 1.0) * lg[h])[None, :]
            gC[half * 64:(half + 1) * 64, hp] = np.exp(128.0 * lg[h])
    c["c_xiT"] = xiT
    c["c_kdT"] = kdT
    c["c_gC"] = gC
    c["c_gC8"] = np.tile(np.exp(128.0 * lg)[None, :], (64, 1)).astype(np.float32)
    c["c_ktab"] = (0.125 * np.exp(-(nn[:, None] + 1.0) * lg[None, :])).astype(np.float32)
    dm = (nn[None, :] >= nn[:, None]).astype(np.float32)
    c["c_rmask"] = np.tile(dm, (1, 4))
    jj = nn[:, None]
    ii = nn[None, :]
    c["c_mcausal"] = np.tile(np.where(jj > ii, NEG, 0.0).astype(np.float32), (1, 4))
    c["c_mwin4"] = np.tile(np.where(jj > ii, 0.0, NEG).astype(np.float32), (1, 4))
    pats = []
    pidx = {}
    cmp_plan = []
    for qi in range(NT):
        row = []
        for cc in range(4):
            tq = 128 * qi + ii
            nk = 128 * cc + jj
            valid = (tq - (16 * nk + 31) >= 0) & (nk <= 510)
            if valid.all():
                row.append(-1)
            elif not valid.any():
                row.append(-2)
            else:
                key = valid.tobytes()
                if key not in pidx:
                    pidx[key] = len(pats)
                    pats.append(np.tile(np.where(valid, 0.0, NEG).astype(np.float32), (1, 4)))
                row.append(pidx[key])
        cmp_plan.append(row)
    c["c_mcmp"] = np.stack(pats, 0)
    E = np.zeros((128, NT, 128), np.float32)
    for kt in range(NT):
        E[2 * kt, kt, 0:64] = 1.0
        E[2 * kt + 1, kt, 64:128] = 1.0
    c["c_E"] = E
    ncmp = np.arange(512)[:, None]
    msel = np.arange(128)[None, :]
    ov = ((np.minimum(16 * ncmp + 32, 64 * msel + 64) > np.maximum(16 * ncmp, 64 * msel))
          & (ncmp <= 510)).astype(np.float32)
    c["c_ov"] = ov
    AB = np.zeros((NT, 128, 256), np.float32)
    for qi in range(NT):
        tq = 128 * qi + nn[:, None]
        back = tq // 64 - msel
        forced = (msel == 0) | ((back >= 0) & (back < 2))
        A = ((back >= 0) & (~forced)).astype(np.float32)
        B = np.where(forced, 1.0e9 + 1024.0 * msel, np.where(back >= 0, 0.0, -1.0)).astype(np.float32)
        AB[qi, :, 0:128] = A
        AB[qi, :, 128:256] = B
    c["c_AB"] = AB
    c["c_gi512"] = np.tile((512.0 * np.arange(NGRP))[None, :], (128, 1)).astype(np.float32)
    c["c_tokid"] = (np.arange(NT)[None, :] * 128 + np.arange(128)[:, None]).astype(np.float32)
    c["c_U"] = (nn[:, None] < nn[None, :]).astype(np.float32)
    c["c_blkp"] = (np.arange(7)[None, :] * 128 + np.arange(128)[:, None]).astype(np.float32)
    return c, cmp_plan


_CONSTS = None


def get_consts():
    global _CONSTS
    if _CONSTS is None:
        _CONSTS = make_consts()
    return _CONSTS


class Ctx:
    def __getattr__(self, k):
        if k in WEIGHT_SPECS:
            ap = self.nc.dram_tensor(k, WEIGHT_SPECS[k], F32, kind="ExternalInput").ap()
            self.__dict__[k] = ap
            self.used.append(k)
            return ap
        raise AttributeError(k)


def ln_a(s, X, src, src_res, k):
    junk, ss, xs = X.junk, X.ss[k], X.xs[k]
    s.op("act", lambda e: e.activation(out=junk[:], in_=src, func=AF.Square, accum_out=ss[:, 0:1]),
         reads=[src_res], writes=["junk", "ss%d" % k])
    s.op("dve", lambda e: e.tensor_scalar(out=ss[:, 1:2], in0=ss[:, 0:1], scalar1=1.0 / D, scalar2=EPS,
                                          op0=ALU.mult, op1=ALU.add), reads=["ss%d" % k], writes=["ss%d" % k])
    s.op("act", lambda e: e.activation(out=ss[:, 1:2], in_=ss[:, 1:2], func=AF.Sqrt),
         reads=["ss%d" % k], writes=["ss%d" % k])
    s.op("dve", lambda e: e.reciprocal(out=ss[:, 2:3], in_=ss[:, 1:2]), reads=["ss%d" % k], writes=["ss%d" % k])
    s.op("act", lambda e: e.activation(out=xs[:], in_=src, func=AF.Copy, scale=ss[:, 2:3]),
         reads=[src_res, "ss%d" % k], writes=["xs%d" % k])


def ln_b(s, X, k, gT, gT_res, dst, dst_res, src=None, src_res=None):
    xs = X.xs[k] if src is None else src
    xres = ("xs%d" % k) if src is None else src_res
    psb = X.ps[7][:].bitcast(BF16)
    for kc in range(8):
        s.op("pe", lambda e: e.transpose(out=psb[:, kc * 128:(kc + 1) * 128], in_=xs[:, kc * 128:(kc + 1) * 128],
                                         identity=X.ident[:]), reads=[xres, "ident"], writes=["ps7"])
    s.op("dve", lambda e: e.tensor_tensor(out=dst, in0=psb.rearrange("p (k t) -> p k t", k=8),
                                          in1=bcast_mid(gT, 128), op=ALU.mult),
         reads=["ps7", gT_res], writes=[dst_res])


def phase_proj(s, X, L, h_in):
    nc = X.nc
    with ExitStack() as es:
        sb = lambda name, shape, dt: es.enter_context(nc.sbuf_tensor("%s_L%d" % (name, L), shape, dt))
        wsb = sb("p1_w", [128, 8, IN_COLS], BF16)
        gT = sb("p1_gT", [128, 8], F32)
        ht = [sb("p1_ht%d" % i, [128, D], F32) for i in range(4)]
        hnT = [sb("p1_hnT%d" % i, [128, 8, 512], BF16) for i in range(2)]
        fm = [sb("p1_fm%d" % i, [128, 16, 512], BF16) for i in range(2)]
        vsx = [sb("p1_vsx%d" % i, [128, 4, 2, 65], BF16) for i in range(2)]
        vwx = [sb("p1_vwx%d" % i, [128, 4, 2, 65], BF16) for i in range(2)]
        gls = [sb("p1_gls%d" % i, [128, 4, 24], F32) for i in range(2)]
        krm = [sb("p1_krm%d" % i, [128, 4, 512], BF16) for i in range(2)]
        vrm = [sb("p1_vrm%d" % i, [128, 4, 512], BF16) for i in range(2)]
        grm = [sb("p1_grm%d" % i, [128, 4, 512], BF16) for i in range(2)]
        sg = sb("p1_sg", [128, 512], F32)
        xiT = sb("p1_xiT", [128, 4, 128], F32)
        kdT = sb("p1_kdT", [128, 4, 128], F32)
        ktab = sb("p1_ktab", [128, 8], F32)
        s.dma("sp", xiT[:], X.c["c_xiT"].rearrange("j p t -> p j t"), writes=["xiT"])
        s.dma("sp", kdT[:], X.c["c_kdT"].rearrange("j p t -> p j t"), writes=["kdT"])
        s.dma("sp", ktab[:], X.c["c_ktab"], writes=["ktab"])
        s.dma("sp", gT[:], X.g_mix[L].rearrange("(k p) -> p k", p=128), writes=["p1gT"],
              allow_slow_non_contiguous=True)
        for kc in range(8):
            s.dma("pool", wsb[:, kc, :], X.w_in[L, kc * 128:(kc + 1) * 128, :], writes=["p1w"])
        for i in range(2):
            s.op("pool", lambda e: e.memset(vsx[i][:, :, :, 64:65], 1.0), writes=["vsx%d" % i])
            s.op("pool", lambda e: e.memset(vwx[i][:, :, :, 64:65], 1.0), writes=["vwx%d" % i])
        fm_cols = [C_QN, C_QN + 128, C_QN + 256, C_QN + 384, C_KC, C_VC, C_KS, C_KW,
                   C_QR, C_QR + 128, C_QR + 256, C_QR + 384, C_KR, C_KR + 128, C_KR + 256, C_KR + 384]
        def emit_a(tg):
            for tt in range(4):
                ti = 4 * tg + tt
                s.dma("sp", ht[tt][:], h_in[ti * 128:(ti + 1) * 128, :], writes=["p1ht%d" % tt])
                ln_a(s, X, ht[tt][:], "p1ht%d" % tt, tt)

        def emit_b(tg):
            for tt in range(4):
                ln_b(s, X, tt, gT[:], "p1gT", hnT[tg % 2][:, :, tt * 128:(tt + 1) * 128], "hnT%d" % (tg % 2))

        emit_a(0)
        emit_b(0)
        for tg in range(NT // 4):
            b = tg % 2
            toks = slice(tg * 512, (tg + 1) * 512)
            if tg + 1 < NT // 4:
                emit_a(tg + 1)
            for ci, c0 in enumerate(fm_cols):
                pb = ci % 2
                ps = X.ps[pb]
                for kc in range(8):
                    s.op("pe", lambda e: e.matmul(ps[:], lhsT=wsb[:, kc, c0:c0 + 128], rhs=hnT[b][:, kc, :],
                                                  start=(kc == 0), stop=(kc == 7)),
                         reads=["p1w", "hnT%d" % b], writes=["ps%d" % pb])
                dst = fm[b][:, ci, :]
                if ci < 4:
                    s.op("act", lambda e: e.activation(out=dst, in_=ps[:], func=AF.Copy, scale=0.125),
                         reads=["ps%d" % pb], writes=["fm%d" % b])
                elif ci < 8:
                    eng = "act" if ci % 2 == 0 else "dve"
                    if eng == "act":
                        s.op("act", lambda e: e.activation(out=dst, in_=ps[:], func=AF.Copy),
                             reads=["ps%d" % pb], writes=["fm%d" % b])
                    else:
                        s.op("dve", lambda e: e.tensor_copy(out=dst, in_=ps[:]),
                             reads=["ps%d" % pb], writes=["fm%d" % b])
                else:
                    tab = xiT if ci < 12 else kdT
                    j = (ci - 8) % 4
                    s.op("dve", lambda e: e.tensor_tensor(
                        out=dst.rearrange("p (a t) -> p a t", a=4), in0=ps[:].rearrange("p (a t) -> p a t", a=4),
                        in1=bcast_rep(tab[:, j, :], 4), op=ALU.mult),
                        reads=["ps%d" % pb, "xiT", "kdT"], writes=["fm%d" % b])
            for tt in range(4):
                lhs = lambda kc: hnT[b][:, kc, tt * 128:(tt + 1) * 128]
                groups = [(2, C_VS, 408), (3, C_KR, 512), (4, C_VR, 512), (5, C_GR, 512)]
                for (pi, c0, n) in groups:
                    for kc in range(8):
                        s.op("pe", lambda e: e.matmul(X.ps[pi][:, 0:n], lhsT=lhs(kc), rhs=wsb[:, kc, c0:c0 + n],
                                                      start=(kc == 0), stop=(kc == 7)),
                             reads=["p1w", "hnT%d" % b], writes=["ps%d" % pi])
                pA = X.ps[2]
                s.op("dve", lambda e: e.tensor_copy(out=vsx[b][:, tt, :, 0:64],
                                                    in_=pA[:, 0:128].rearrange("p (g d) -> p g d", g=2)),
                     reads=["ps2"], writes=["vsx%d" % b])
                s.op("dve", lambda e: e.tensor_copy(out=vwx[b][:, tt, :, 0:64],
                                                    in_=pA[:, 256:384].rearrange("p (g d) -> p g d", g=2)),
                     reads=["ps2"], writes=["vwx%d" % b])
                s.op("act", lambda e: e.activation(out=gls[b][:, tt, :], in_=pA[:, 384:408], func=AF.Sigmoid),
                     reads=["ps2"], writes=["gls%d" % b])
                s.op("dve", lambda e: e.tensor_tensor(
                    out=krm[b][:, tt, :].rearrange("p (h d) -> p h d", h=8),
                    in0=X.ps[3][:].rearrange("p (h d) -> p h d", h=8),
                    in1=bcast_mid(ktab[:], 64), op=ALU.mult),
                    reads=["ps3", "ktab"], writes=["krm%d" % b])
                s.op("act", lambda e: e.activation(out=vrm[b][:, tt, :], in_=X.ps[4][:], func=AF.Copy),
                     reads=["ps4"], writes=["vrm%d" % b])
                s.op("act", lambda e: e.activation(out=sg[:], in_=X.ps[5][:], func=AF.Sigmoid),
                     reads=["ps5"], writes=["p1sg"])
                s.op("dve", lambda e: e.tensor_tensor(out=grm[b][:, tt, :], in0=X.ps[5][:], in1=sg[:], op=ALU.mult),
                     reads=["ps5", "p1sg"], writes=["grm%d" % b])
            if tg + 1 < NT // 4:
                emit_b(tg + 1)
            s.dma("pool", X.QTP[:, :, toks].rearrange("j p t -> p j t"), fm[b][:, 0:4, :], reads=["fm%d" % b], writes=["QTP"])
            s.dma("pool", X.KCP[:, toks], fm[b][:, 4, :], reads=["fm%d" % b], writes=["KCP"])
            s.dma("pool", X.VCP[:, toks], fm[b][:, 5, :], reads=["fm%d" % b], writes=["VCP"])
            s.dma("pool", X.KSP[:, toks], fm[b][:, 6, :], reads=["fm%d" % b], writes=["KSP"])
            s.dma("pool", X.KWP[:, toks], fm[b][:, 7, :], reads=["fm%d" % b], writes=["KWP"])
            s.dma("pool", X.QRT[:, :, toks].rearrange("j p t -> p j t"), fm[b][:, 8:12, :], reads=["fm%d" % b], writes=["QRT"])
            s.dma("pool", X.KRT[:, :, toks].rearrange("j p t -> p j t"), fm[b][:, 12:16, :], reads=["fm%d" % b], writes=["KRT"])
            s.dma("pool", X.VSX[toks].rearrange("(tt p) g c -> p tt g c", p=128), vsx[b][:], reads=["vsx%d" % b], writes=["VSX"])
            s.dma("pool", X.VWX[toks].rearrange("(tt p) g c -> p tt g c", p=128), vwx[b][:], reads=["vwx%d" % b], writes=["VWX"])
            s.dma("pool", X.GLS[toks].rearrange("(tt p) c -> p tt c", p=128), gls[b][:], reads=["gls%d" % b], writes=["GLS"])
            s.dma("pool", X.KRM[toks].rearrange("(tt p) c -> p tt c", p=128), krm[b][:], reads=["krm%d" % b], writes=["KRM"])
            s.dma("pool", X.VRM[toks].rearrange("(tt p) c -> p tt c", p=128), vrm[b][:], reads=["vrm%d" % b], writes=["VRM"])
            s.dma("pool", X.GRM[toks].rearrange("(tt p) c -> p tt c", p=128), grm[b][:], reads=["grm%d" % b], writes=["GRM"])
        s.barrier()


WEIGHT_SPECS = {
    "w_in": [DEPTH, D, IN_COLS], "w_out": [DEPTH, D, D], "g_mix": [DEPTH, D], "g_ffn": [DEPTH, D],
    "g_ple": [DEPTH, D], "g_final": [D], "cmp_pos": [DEPTH, 2, 32, 64], "cmp_w1": [DEPTH, 2, 32, 64, 256],
    "cmp_w2": [DEPTH, 2, 256, 64], "ret_gn": [DEPTH, 512], "ffn_gate": [1, D, D_FF], "ffn_up": [1, D, D_FF],
    "ffn_down": [1, D_FF, D], "moe_router": [1, D, NE], "moe_gate": [1, NE, D, D_FF], "moe_up": [1, NE, D, D_FF],
    "moe_down": [1, NE, D_FF, D], "ple_proj": [DEPTH, 256, D], "ple_gate": [DEPTH, D, D],
}

SCRATCH_SPECS = {
    "QTP": ([4, 128, SEQ], BF16), "QAUG": ([8, 3, SEQ], BF16), "KAUG": ([3, SEQ], BF16), "KCAUG": ([3, 512], BF16),
    "KSP": ([128, SEQ], BF16), "KWP": ([128, SEQ], BF16), "KCP": ([128, SEQ], BF16), "VCP": ([128, SEQ], BF16),
    "VSX": ([SEQ, 2, 65], BF16), "VWX": ([SEQ, 2, 65], BF16), "GLS": ([SEQ, 24], F32),
    "QRT": ([4, 128, SEQ], BF16), "KRT": ([4, 128, SEQ], BF16),
    "KRM": ([SEQ, 512], BF16), "VRM": ([SEQ, 512], BF16), "GRM": ([SEQ, 512], BF16),
    "AT": ([D, SEQ], BF16), "HB": ([SEQ, D], F32),
    "FG16": ([1, D, D_FF], BF16), "FU16": ([1, D, D_FF], BF16), "FD16": ([1, D_FF, D], BF16),
    "WGB": ([NE * 7 * 128, 4096], BF16), "WUB": ([NE * 7 * 128, 4096], BF16), "WDB": ([NE * 7 * 128, 4096], BF16),
    "H1": ([SEQ, D], F32), "XS2": ([SEQ, D], BF16), "SLOT": ([NSLOT, 2], I32), "YS": ([NSLOT, D], F32),
    "DBG_KCMP": ([67, 2, 512], BF16), "DBG_VCX": ([128, 2, 4, 193], BF16),
}


def build(stop_after=None, debug=(), nsa_tiles=NT):
    nc = bass.Bass("TRN2", target_bir_lowering=False)
    X = Ctx()
    X.nc = nc
    consts, cmp_plan = get_consts()
    X.cmp_plan = cmp_plan
    X.x = nc.dram_tensor("x", [SEQ, D], F32, kind="ExternalInput").ap()
    X.p = nc.dram_tensor("p", [DEPTH, SEQ, 256], F32, kind="ExternalInput").ap()
    X.used = []
    X.dbg_cmp = "DBG_KCMP" in debug
    X.nsa_tiles = nsa_tiles
    X.routed = True
    X.conv_layers = [0] if (stop_after is not None and stop_after[0] == 0) else [0, 1]
    X.ret_chunks = int(os.environ.get('RET_CHUNKS', NT))
    X.c = {k: nc.dram_tensor(k, list(v.shape), F32, kind="ExternalInput").ap() for k, v in consts.items()}
    X.out = nc.dram_tensor("out", [SEQ, D], F32, kind="ExternalOutput").ap()
    for k, (shp, dt) in SCRATCH_SPECS.items():
        kind = "ExternalOutput" if k in debug else "Internal"
        setattr(X, k, nc.dram_tensor(k, shp, dt, kind=kind).ap())
    with ExitStack() as es:
        s = S(nc, es)
        X.ps = [es.enter_context(nc.psum_tensor("ps%d" % i, [128, 512], F32)) for i in range(8)]
        X.ident = es.enter_context(nc.sbuf_tensor("ident", [128, 128], BF16))
        X.junk = es.enter_context(nc.sbuf_tensor("junk", [128, D], F32))
        X.ss = [es.enter_context(nc.sbuf_tensor("ss%d" % i, [128, 4], F32)) for i in range(8)]
        X.xs = [es.enter_context(nc.sbuf_tensor("xs%d" % i, [128, D], BF16)) for i in range(8)]
        s.dma("pool", X.ident[:], X.c["c_ident"], writes=["ident"])
        s.dma("pool", X.QAUG, X.c["c_qaug"], writes=["QAUG"])
        s.dma("pool", X.KAUG, X.c["c_kaug"], writes=["KAUG"])
        s.dma("pool", X.KCAUG, X.c["c_kcaug"], writes=["KCAUG"])
        done = False
        for L in range(DEPTH):
            h_in = X.x if L == 0 else X.HB
            for name, fn in PHASES:
                fn(s, X, L, h_in)

                if stop_after == (L, name):
                    done = True
                    break
            if done:
                break
        s.barrier()
        s.drain("sp")
    X.nops = s.nops
    return nc, X


PHASES = [("proj", phase_proj)]

_BUILT = {}


def run(inputs, stop_after=None, debug=(), cores=8, trace=False, nsa_tiles=NT):
    key = (stop_after, tuple(debug), nsa_tiles)
    if key not in _BUILT:
        _BUILT[key] = build(stop_after, debug, nsa_tiles)
    nc, X = _BUILT[key]
    consts, _ = get_consts()
    in_maps = []
    for b in range(cores):
        m = {"x": np.ascontiguousarray(inputs["x"][b]), "p": np.ascontiguousarray(inputs["p"][:, b])}
        for k in X.used:
            m[k] = np.ascontiguousarray(inputs[k])
        m.update(consts)
        in_maps.append(m)
    return run_bass_kernel_spmd(nc, in_maps, core_ids=list(range(cores)), trace=trace)


def kernel(**inputs):
    inputs = {k: np.asarray(v) for k, v in inputs.items()}
    res = run(inputs)
    return np.stack([r["out"] for r in res.results], 0).astype(np.float32)


def phase_nsa(s, X, L, h_in):
    nc = X.nc
    with ExitStack() as es:
        sb = lambda name, shape, dt: es.enter_context(nc.sbuf_tensor("%s_L%d" % (name, L), shape, dt))
        KCMP = sb("n_kcmp", [67, 2, 512], BF16)
        VCX = sb("n_vcx", [128, 2, 4, 193], BF16)
        s.op("pool", lambda e: e.memset(KCMP[:], 0.0), writes=["KCMP"])
        s.op("pool", lambda e: e.memset(VCX[:], 0.0), writes=["VCX"])
        s.op("pool", lambda e: e.memset(VCX[:, :, :, 64:65], 1.0), writes=["VCX"])
        for g in range(2):
            s.dma("pool", VCX[:, g, :, 65:193], X.c["c_ov"].rearrange("(ct p) m -> p ct m", p=128), writes=["VCX"])
            s.dma("sp", KCMP[64:67, g, :], X.KCAUG, reads=["KCAUG"], writes=["KCMP"])
        with ExitStack() as es2:
            sb2 = lambda name, shape, dt: es2.enter_context(nc.sbuf_tensor("%s_L%d" % (name, L), shape, dt))
            kcT = [sb2("c_kcT%d" % i, [64, SEQ], BF16) for i in range(2)]
            w1 = sb2("c_w1", [64, 32, 256], BF16)
            posT = sb2("c_posT", [64, 32], BF16)
            w2 = sb2("c_w2", [128, 2, 64], BF16)
            hb = sb2("c_hb", [128, 2], F32)
            u = sb2("c_u", [128, 2, 512], F32)
            t1 = sb2("c_t1", [128, 2, 512], F32)
            sg = sb2("c_sg", [128, 2, 512], F32)
            gel = sb2("c_gel", [128, 2, 512], BF16)
            it = 0
            for kv in range(2):
                s.dma("pool", w1[:], X.cmp_w1[L, kv].rearrange("l d h -> d l h"), writes=["c_w1"])
                s.dma("pool", posT[:], X.cmp_pos[L, kv].rearrange("l d -> d l"), writes=["c_posT"],
                      allow_slow_non_contiguous=True)
                s.dma("pool", w2[:], X.cmp_w2[L, kv].rearrange("(c p) d -> p c d", p=128), writes=["c_w2"])
                src = X.KCP if kv == 0 else X.VCP
                for g in range(2):
                    kt_ = kcT[it % 2]
                    kres = "c_kcT%d" % (it % 2)
                    it += 1
                    s.dma("sp", kt_[:], src[g * 64:(g + 1) * 64, :], reads=["KCP", "VCP"], writes=[kres])
                    kview = kt_[:].rearrange("d (n c) -> d n c", c=16)
                    for hc in range(2):
                        ps = X.ps[hc]
                        for l in range(32):
                            a, c_ = l // 16, l % 16
                            s.op("pe", lambda e: e.matmul(ps[:, 0:511], lhsT=w1[:, l, hc * 128:(hc + 1) * 128],
                                                          rhs=kview[:, a:a + 511, c_], start=(l == 0), stop=(l == 31)),
                                 reads=["c_w1", kres], writes=["ps%d" % hc])
                        for l in range(32):
                            s.op("pe", lambda e: e.matmul(X.ps[2][:, hc:hc + 1], lhsT=w1[:, l, hc * 128:(hc + 1) * 128],
                                                          rhs=posT[:, l:l + 1], start=(l == 0), stop=(l == 31)),
                                 reads=["c_w1", "c_posT"], writes=["ps2"])
                        s.op("dve", lambda e: e.tensor_copy(out=hb[:, hc:hc + 1], in_=X.ps[2][:, hc:hc + 1]),
                             reads=["ps2"], writes=["c_hb"])
                        s.op("act", lambda e: e.activation(out=u[:, hc, 0:511], in_=ps[:, 0:511], func=AF.Identity,
                                                           bias=hb[:, hc:hc + 1]),
                             reads=["ps%d" % hc, "c_hb"], writes=["c_u"])
                    uu = u[:, :, 0:511]
                    s.op("pool", lambda e: e.tensor_tensor(out=t1[:, :, 0:511], in0=uu, in1=uu, op=ALU.mult),
                         reads=["c_u"], writes=["c_t1"])
                    s.op("dve", lambda e: e.tensor_scalar(out=t1[:, :, 0:511], in0=t1[:, :, 0:511], scalar1=0.044715,
                                                          scalar2=1.0, op0=ALU.mult, op1=ALU.add),
                         reads=["c_t1"], writes=["c_t1"])
                    s.op("dve", lambda e: e.tensor_tensor(out=t1[:, :, 0:511], in0=t1[:, :, 0:511], in1=uu, op=ALU.mult),
                         reads=["c_t1", "c_u"], writes=["c_t1"])
                    s.op("act", lambda e: e.activation(out=sg[:, :, 0:511], in_=t1[:, :, 0:511], func=AF.Sigmoid,
                                                       scale=1.5957691216057308),
                         reads=["c_t1"], writes=["c_sg"])
                    s.op("dve", lambda e: e.tensor_tensor(out=gel[:, :, 0:511], in0=sg[:, :, 0:511], in1=uu, op=ALU.mult),
                         reads=["c_sg", "c_u"], writes=["c_gel"])
                    if kv == 0:
                        for hc in range(2):
                            s.op("pe", lambda e: e.matmul(X.ps[3][0:64, 0:511], lhsT=w2[:, hc, :], rhs=gel[:, hc, 0:511],
                                                          start=(hc == 0), stop=(hc == 1)),
                                 reads=["c_w2", "c_gel"], writes=["ps3"])
                        s.op("act", lambda e: e.activation(out=KCMP[0:64, g, 0:511], in_=X.ps[3][0:64, 0:511], func=AF.Copy),
                             reads=["ps3"], writes=["KCMP"])
                    else:
                        for ct in range(4):
                            nn = 128 if ct < 3 else 127
                            for hc in range(2):
                                s.op("pe", lambda e: e.matmul(X.ps[3][0:nn, ct * 64:(ct + 1) * 64],
                                                              lhsT=gel[:, hc, ct * 128:ct * 128 + nn], rhs=w2[:, hc, :],
                                                              start=(hc == 0), stop=(hc == 1)),
                                     reads=["c_w2", "c_gel"], writes=["ps3"])
                        for ct in range(4):
                            nn = 128 if ct < 3 else 127
                            s.op("act", lambda e: e.activation(out=VCX[0:nn, g, ct, 0:64],
                                                               in_=X.ps[3][0:nn, ct * 64:(ct + 1) * 64], func=AF.Copy),
                                 reads=["ps3"], writes=["VCX"])
            s.barrier()
        if X.dbg_cmp:
            s.dma("sp", X.DBG_KCMP, KCMP[:], reads=["KCMP"])
            s.dma("sp", X.DBG_VCX, VCX[:], reads=["VCX"])
        KS = sb("n_ks", [67, 2, SEQ], BF16)
        KW = sb("n_kw", [67, 2, SEQ], BF16)
        VS = sb("n_vs", [128, NT, 2, 65], BF16)
        VW = sb("n_vw", [128, NT, 2, 65], BF16)
        E = sb("n_E", [128, NT, 128], BF16)
        mca = sb("n_mca", [128, 512], BF16)
        mw4 = sb("n_mw4", [128, 512], BF16)
        npat = X.c["c_mcmp"].shape[0]
        mcmp = sb("n_mcmp", [128, npat, 512], BF16)
        qt = [sb("n_qt%d" % i, [67, 1024], BF16) for i in range(2)]
        AB = [sb("n_AB%d" % i, [128, 256], F32) for i in range(2)]
        glt = [sb("n_gl%d" % i, [128, 24], F32) for i in range(2)]
        PT = [sb("n_PT%d" % i, [128, 512], BF16) for i in range(4)]
        negT = [sb("n_negT%d" % i, [128, 512], BF16) for i in range(2)]
        acc = [sb("n_acc%d" % i, [128, 512], F32) for i in range(2)]
        accb = [sb("n_accb%d" % i, [128, 512], BF16) for i in range(2)]
        aT = [sb("n_aT%d" % i, [128, 4, 128], BF16) for i in range(2)]
        sm = [sb("n_sm%d" % i, [128, 64], F32) for i in range(2)]
        sc = [sb("n_sc%d" % i, [128, 128], F32) for i in range(2)]
        sc2 = [sb("n_sc2%d" % i, [128, 128], F32) for i in range(2)]
        seln = [sb("n_seln%d" % i, [128, 128], BF16) for i in range(2)]
        s.dma("sp", KS[0:64, :, :], X.KSP.rearrange("(g d) t -> d g t", g=2), reads=["KSP"], writes=["KS"])
        s.dma("sp", KW[0:64, :, :], X.KWP.rearrange("(g d) t -> d g t", g=2), reads=["KWP"], writes=["KW"])
        for g in range(2):
            s.dma("sp", KS[64:67, g, :], X.KAUG, reads=["KAUG"], writes=["KS"])
            s.dma("sp", KW[64:67, g, :], X.KAUG, reads=["KAUG"], writes=["KW"])
        for q4 in range(4):
            tsl = slice(q4 * 2048, (q4 + 1) * 2048)
            ksl = slice(q4 * 16, (q4 + 1) * 16)
            s.dma("sp", VS[:, ksl], X.VSX[tsl].rearrange("(kt p) g c -> p kt g c", p=128), reads=["VSX"], writes=["VS"])
            s.dma("sp", VW[:, ksl], X.VWX[tsl].rearrange("(kt p) g c -> p kt g c", p=128), reads=["VWX"], writes=["VW"])
            s.dma("pool", E[:, ksl, :], X.c["c_E"][:, ksl, :], writes=["E"])
        s.dma("pool", mca[:], X.c["c_mcausal"], writes=["mca"])
        s.dma("pool", mw4[:], X.c["c_mwin4"], writes=["mw4"])
        s.dma("pool", mcmp[:], X.c["c_mcmp"].rearrange("n p c -> p n c"), writes=["mcmp"])
        st = {"sb": 0, "pt": 0}

        def qk_tile(g, qb, lhsT, lres, extra, mask):
            pi = st["sb"] % 3
            st["sb"] += 1
            ps = X.ps[pi]
            pres = "ps%d" % pi
            nmm = 1 + (extra is not None) + (mask is not None)
            s.op("pe", lambda e: e.matmul(ps[:], lhsT=lhsT, rhs=qt[qb][:, g * 512:(g + 1) * 512], start=True,
                                          stop=(nmm == 1)), reads=[lres, "qt%d" % qb], writes=[pres])
            k = 1
            if extra is not None:
                el, er, eres = extra
                s.op("pe", lambda e: e.matmul(ps[:], lhsT=el, rhs=er, start=False, stop=(k + 1 == nmm)),
                     reads=eres, writes=[pres])
                k += 1
            if mask is not None:
                ml, mres = mask
                s.op("pe", lambda e: e.matmul(ps[:], lhsT=X.ident[:], rhs=ml, start=False, stop=True),
                     reads=["ident", mres], writes=[pres])
            pk = st["pt"] % 4
            st["pt"] += 1
            s.op("act", lambda e: e.activation(out=PT[pk][:], in_=ps[:], func=AF.Exp),
                 reads=[pres], writes=["PT%d" % pk])
            return PT[pk], "PT%d" % pk

        def coef_and_acc(g, qb, branch, heads_ap, heads_res, first):
            smt = sm[g]
            sres = "sm%d" % g
            for r in range(4):
                o_ap = heads_ap(r)
                s.op("dve", lambda e: e.tensor_scalar(out=smt[:, r:r + 1], in0=o_ap[:, 64:65], scalar1=1e-36,
                                                      scalar2=None, op0=ALU.max),
                     reads=heads_res, writes=[sres])
            s.op("dve", lambda e: e.reciprocal(out=smt[:, 4:8], in_=smt[:, 0:4]), reads=[sres], writes=[sres])
            c0 = branch * 8 + g * 4
            s.op("dve", lambda e: e.tensor_tensor(out=smt[:, 8:12], in0=smt[:, 4:8], in1=glt[qb][:, c0:c0 + 4],
                                                  op=ALU.mult), reads=[sres, "gl%d" % qb], writes=[sres])
            for r in range(4):
                o_ap = heads_ap(r)
                dst = acc[qb][:, (g * 4 + r) * 64:(g * 4 + r + 1) * 64]
                if first:
                    s.op("dve", lambda e: e.tensor_scalar(out=dst, in0=o_ap[:, 0:64], scalar1=smt[:, 8 + r:9 + r],
                                                          scalar2=None, op0=ALU.mult),
                         reads=heads_res + [sres], writes=["acc%d" % qb])
                else:
                    s.op("dve", lambda e: e.scalar_tensor_tensor(out=dst, in0=o_ap[:, 0:64], scalar=smt[:, 8 + r:9 + r],
                                                                 in1=dst, op0=ALU.mult, op1=ALU.add),
                         reads=heads_res + [sres], writes=["acc%d" % qb])

        def emit_loads(qi):
            qb = qi % 2
            t0 = qi * 128
            for j in range(4):
                s.dma("sp", qt[qb][0:64, :].rearrange("d (j two t) -> d j two t", j=4, two=2)[:, j],
                      X.QTP[j, :, t0:t0 + 128].rearrange("(two d) t -> d two t", two=2),
                      reads=["QTP"], writes=["qt%d" % qb])
            s.dma("sp", qt[qb][64:67, :].rearrange("r (h t) -> r h t", h=8),
                  X.QAUG[:, :, t0:t0 + 128].rearrange("h r t -> r h t"), reads=["QAUG"], writes=["qt%d" % qb])
            s.dma("sp", AB[qb][:], X.c["c_AB"][qi], writes=["AB%d" % qb])
            s.dma("sp", glt[qb][:], X.GLS[t0:t0 + 128, :], reads=["GLS"], writes=["gl%d" % qb])

        def cmp_post_a(qi, g):
            qb = qi % 2
            ocb = lambda r: X.ps[3 + r // 2][:, (r % 2) * 193:(r % 2) * 193 + 193]
            coef_and_acc(g, qb, 0, ocb, ["ps3", "ps4"], True)
            smt = sm[g]
            sres = "sm%d" % g
            for r in range(4):
                if r == 0:
                    s.op("dve", lambda e: e.tensor_scalar(out=sc[g][:], in0=ocb(r)[:, 65:193], scalar1=smt[:, 4:5],
                                                          scalar2=None, op0=ALU.mult),
                         reads=["ps3", "ps4", sres], writes=["sc%d" % g])
                else:
                    s.op("dve", lambda e: e.scalar_tensor_tensor(out=sc[g][:], in0=ocb(r)[:, 65:193],
                                                                 scalar=smt[:, 4 + r:5 + r], in1=sc[g][:],
                                                                 op0=ALU.mult, op1=ALU.add),
                         reads=["ps3", "ps4", sres], writes=["sc%d" % g])
            s.op("dve", lambda e: e.tensor_tensor(out=sc[g][:], in0=sc[g][:], in1=AB[qb][:, 0:128], op=ALU.mult),
                 reads=["AB%d" % qb], writes=["sc%d" % g])
            s.op("dve", lambda e: e.tensor_tensor(out=sc[g][:], in0=sc[g][:], in1=AB[qb][:, 128:256], op=ALU.add),
                 reads=["AB%d" % qb], writes=["sc%d" % g])
            s.op("dve", lambda e: e.max(out=smt[:, 16:24], in_=sc[g][:]), reads=["sc%d" % g], writes=[sres])
            s.op("dve", lambda e: e.match_replace(out=sc2[g][:], in_to_replace=smt[:, 16:24], in_values=sc[g][:],
                                                  imm_value=-2.0), reads=["sc%d" % g, sres], writes=["sc2%d" % g])
            s.op("dve", lambda e: e.max(out=smt[:, 24:32], in_=sc2[g][:]), reads=["sc2%d" % g], writes=[sres])
            s.op("dve", lambda e: e.tensor_scalar(out=seln[g][:], in0=sc[g][:], scalar1=smt[:, 31:32], scalar2=NEG,
                                                  op0=ALU.is_lt, op1=ALU.mult),
                 reads=["sc%d" % g, sres], writes=["seln%d" % g])

        def cmp_post_b(qi, g):
            psb = X.ps[7][:].bitcast(BF16)
            s.op("pe", lambda e: e.transpose(out=psb[:, g * 128:(g + 1) * 128], in_=seln[g][:], identity=X.ident[:]),
                 reads=["seln%d" % g, "ident"], writes=["ps7"])
            s.op("dve", lambda e: e.tensor_copy(out=negT[g][:].rearrange("p (a t) -> p a t", a=4),
                                                in_=bcast_rep(psb[:, g * 128:(g + 1) * 128], 4)),
                 reads=["ps7"], writes=["negT%d" % g])

        def write_out(qi):
            qb = qi % 2
            t0 = qi * 128
            s.op("act", lambda e: e.activation(out=accb[qb][:], in_=acc[qb][:], func=AF.Copy),
                 reads=["acc%d" % qb], writes=["accb%d" % qb])
            psb = X.ps[7][:].bitcast(BF16)
            for j in range(4):
                s.op("pe", lambda e: e.transpose(out=psb[:, 256 + j * 128:256 + (j + 1) * 128],
                                                 in_=accb[qb][:, j * 128:(j + 1) * 128], identity=X.ident[:]),
                     reads=["accb%d" % qb, "ident"], writes=["ps7"])
            s.op("act", lambda e: e.activation(out=aT[qb][:].rearrange("p j t -> p (j t)"), in_=psb[:, 256:768], func=AF.Copy),
                 reads=["ps7"], writes=["aT%d" % qb])
            s.dma("sp", X.AT[0:512, t0:t0 + 128].rearrange("(j p) t -> p j t", p=128), aT[qb][:],
                  reads=["aT%d" % qb], writes=["AT"])

        def pv_fn(out_ap, out_res, rhs, rhs_res, first, lastt, pair_start):
            def f(pt, ptres):
                for r in range(4):
                    st_ = first and ((r % 2 == 0) if pair_start else (r == 0))
                    s.op("pe", lambda e: e.matmul(out_ap(r), lhsT=pt[:, r * 128:(r + 1) * 128], rhs=rhs,
                                                  start=st_, stop=lastt), reads=[ptres, rhs_res], writes=out_res(r))
            return f

        jobs = []
        for qi in range(X.nsa_tiles):
            qb = qi % 2
            first_job = len(jobs)
            plan = X.cmp_plan[qi]
            cs = [c for c in range(4) if plan[c] != -2]
            ocb = lambda r: X.ps[3 + r // 2][:, (r % 2) * 193:(r % 2) * 193 + 193]
            ocres = lambda r: ["ps%d" % (3 + r // 2)]
            for g in range(2):
                for ci, c in enumerate(cs):
                    mask = None if plan[c] == -1 else (mcmp[:, plan[c], :], "mcmp")
                    jobs.append(dict(
                        qk=(lambda g=g, qb=qb, c=c, mask=mask: qk_tile(g, qb, KCMP[:, g, c * 128:(c + 1) * 128], "KCMP", None, mask)),
                        pv=pv_fn(ocb, ocres, VCX[:, g, c, :], "VCX", ci == 0, ci == len(cs) - 1, True),
                        post=((lambda qi=qi, g=g: (cmp_post_a(qi, g), (cmp_post_b(qi, g) if qi < 2 else None)))
                              if ci == len(cs) - 1 else None)))
            for g in range(2):
                obr = (lambda g: (lambda r: X.ps[5 + g][:, r * 65:(r + 1) * 65]))(g)
                obres = (lambda g: (lambda r: ["ps%d" % (5 + g)]))(g)
                dl = list(range(min(4, qi), -1, -1))
                for di, dd in enumerate(dl):
                    kt = qi - dd
                    mask = (mca[:], "mca") if dd == 0 else ((mw4[:], "mw4") if dd == 4 else None)
                    posts = []
                    if di == 0 and qi >= 2:
                        posts.append(lambda qi=qi, g=g: cmp_post_b(qi, g))
                    if di == len(dl) - 1:
                        posts.append(lambda g=g, qb=qb, obr=obr: coef_and_acc(g, qb, 2, obr, ["ps%d" % (5 + g)], False))
                    jobs.append(dict(
                        qk=(lambda g=g, qb=qb, kt=kt, mask=mask: qk_tile(g, qb, KW[:, g, kt * 128:(kt + 1) * 128], "KW", None, mask)),
                        pv=pv_fn(obr, obres, VW[:, kt, g, :], "VW", di == 0, di == len(dl) - 1, False),
                        post=(lambda posts=posts: [p_() for p_ in posts])))
            for g in range(2):
                obr = (lambda g: (lambda r: X.ps[5 + g][:, r * 65:(r + 1) * 65]))(g)
                obres = (lambda g: (lambda r: ["ps%d" % (5 + g)]))(g)
                for kt in range(qi + 1):
                    mask = (mca[:], "mca") if kt == qi else None
                    posts = []
                    if kt == qi:
                        posts.append(lambda g=g, qb=qb, obr=obr: coef_and_acc(g, qb, 1, obr, ["ps%d" % (5 + g)], False))
                        if g == 1:
                            posts.append(lambda qi=qi: write_out(qi))
                    jobs.append(dict(
                        qk=(lambda g=g, qb=qb, kt=kt, mask=mask: qk_tile(
                            g, qb, KS[:, g, kt * 128:(kt + 1) * 128], "KS", (E[:, kt, :], negT[g][:], ["E", "negT%d" % g]), mask)),
                        pv=pv_fn(obr, obres, VS[:, kt, g, :], "VS", kt == 0, kt == qi, False),
                        post=(lambda posts=posts: [p_() for p_ in posts])))
            if qi + 1 < X.nsa_tiles:
                old = jobs[first_job]["post"]
                jobs[first_job]["post"] = (lambda old=old, qi=qi: ((old() if old else None), emit_loads(qi + 1)))

        LA = 2
        emit_loads(0)
        pend = {}

        def start(j):
            pend[j] = jobs[j]["qk"]()

        for j in range(min(LA, len(jobs))):
            start(j)
        if L == 0:
            X.conv_todo = conv_thunks(s, X, X.conv_layers)
        n_here = len(X.conv_todo) if L == DEPTH - 1 else min(len(X.conv_todo), 11 + 80)
        every = max(1, (len(jobs) - 100) // max(1, n_here))
        for j in range(len(jobs)):
            if n_here > 0 and j >= 50 and (j - 50) % every == 0 and X.conv_todo:
                X.conv_todo.pop(0)()
                n_here -= 1
            if j + LA < len(jobs):
                start(j + LA)
            pt, ptres = pend.pop(j)
            jobs[j]["pv"](pt, ptres)
            if jobs[j]["post"]:
                jobs[j]["post"]()
        while L == DEPTH - 1 and X.conv_todo:
            X.conv_todo.pop(0)()
        s.barrier()


PHASES.append(("nsa", phase_nsa))


def phase_ret(s, X, L, h_in):
    nc = X.nc
    with ExitStack() as es:
        sb = lambda name, shape, dt: es.enter_context(nc.sbuf_tensor("%s_L%d" % (name, L), shape, dt))
        qT = [sb("r_qT%d" % i, [64, 8, 128], BF16) for i in range(2)]
        kT = [sb("r_kT%d" % i, [64, 8, 128], BF16) for i in range(2)]
        ones1 = sb("r_ones1", [1, 128], F32)
        gn1 = sb("r_gn1", [1, 512], F32)
        km = [sb("r_km%d" % i, [128, 512], BF16) for i in range(2)]
        vm = [sb("r_vm%d" % i, [128, 512], BF16) for i in range(2)]
        gm = [sb("r_gm%d" % i, [128, 512], BF16) for i in range(2)]
        state = sb("r_state", [64, 8, 64], F32)
        stateb = [sb("r_stateb%d" % i, [64, 8, 64], BF16) for i in range(2)]
        rmask = sb("r_mask", [128, 512], F32)
        gC = sb("r_gC", [64, 8], F32)
        gn = sb("r_gn", [128, 512], F32)
        Pm = [sb("r_Pm%d" % i, [128, 8, 128], BF16) for i in range(2)]
        sq = sb("r_sq", [128, 512], F32)
        y = sb("r_y", [128, 512], F32)
        yb = [sb("r_yb%d" % i, [128, 512], BF16) for i in range(2)]
        rT = [sb("r_rT%d" % i, [128, 4, 128], BF16) for i in range(2)]
        stt = sb("r_st", [128, 64], F32)
        s.dma("sp", rmask[:], X.c["c_rmask"], writes=["rmask"])
        s.dma("sp", gC[:], X.c["c_gC8"], writes=["gC"])
        s.dma("sp", gn1[:], X.ret_gn[L:L + 1, :], writes=["gn1"])
        s.op("dve", lambda e: e.memset(ones1[:], 1.0), writes=["ones1"])
        s.op("pe", lambda e: e.matmul(X.ps[6][:], lhsT=ones1[:], rhs=gn1[:], start=True, stop=True),
             reads=["ones1", "gn1"], writes=["ps6"])
        s.op("dve", lambda e: e.tensor_copy(out=gn[:], in_=X.ps[6][:]), reads=["ps6"], writes=["gn"])
        s.op("dve", lambda e: e.memset(state[:], 0.0), writes=["state"])
        def loads(c):
            b = c % 2
            tok = slice(c * 128, (c + 1) * 128)
            for j in range(4):
                s.dma("sp", qT[b][:, 2 * j:2 * j + 2, :], X.QRT[j, :, tok].rearrange("(two d) t -> d two t", two=2),
                      reads=["QRT"], writes=["qT%d" % b])
                s.dma("sp", kT[b][:, 2 * j:2 * j + 2, :], X.KRT[j, :, tok].rearrange("(two d) t -> d two t", two=2),
                      reads=["KRT"], writes=["kT%d" % b])
            s.dma("sp", km[b][:], X.KRM[tok, :], reads=["KRM"], writes=["km%d" % b])
            s.dma("sp", vm[b][:], X.VRM[tok, :], reads=["VRM"], writes=["vm%d" % b])
            s.dma("sp", gm[b][:], X.GRM[tok, :], reads=["GRM"], writes=["gm%d" % b])

        def t1m(c):
            b = c % 2
            for h in range(8):
                pb = h // 4
                s.op("pe", lambda e: e.matmul(X.ps[pb][:, (h % 4) * 128:(h % 4 + 1) * 128],
                                              lhsT=kT[b][:, h, :], rhs=qT[b][:, h, :],
                                              start=(h % 4 == 0), stop=True),
                     reads=["kT%d" % b, "qT%d" % b], writes=["ps%d" % pb])
            for pb in range(2):
                s.op("dve", lambda e: e.tensor_tensor(out=Pm[b][:, 4 * pb:4 * pb + 4, :].rearrange("p a t -> p (a t)"),
                                                      in0=X.ps[pb][:], in1=rmask[:], op=ALU.mult),
                     reads=["ps%d" % pb, "rmask"], writes=["Pm%d" % b])

        def upd(c):
            b = c % 2
            for h in range(8):
                s.op("pe", lambda e: e.matmul(X.ps[3][0:64, h * 64:(h + 1) * 64], lhsT=km[b][:, h * 64:(h + 1) * 64],
                                              rhs=vm[b][:, h * 64:(h + 1) * 64], start=(h == 0), stop=True),
                     reads=["km%d" % b, "vm%d" % b], writes=["ps3"])
            s.op("dve", lambda e: e.tensor_tensor(out=state[:], in0=state[:],
                                                  in1=X.ps[3][0:64, :].rearrange("p (h x) -> p h x", h=8), op=ALU.add),
                 reads=["ps3"], writes=["state"])
            s.op("dve", lambda e: e.tensor_tensor(out=state[:], in0=state[:], in1=bcast_mid(gC[:], 64), op=ALU.mult),
                 reads=["gC"], writes=["state"])
            s.op("dve", lambda e: e.tensor_copy(out=stateb[(c + 1) % 2][:], in_=state[:]), reads=["state"],
                 writes=["stateb%d" % ((c + 1) % 2)])

        def omm(c):
            b = c % 2
            po = 2 if b == 0 else 6
            for h in range(8):
                s.op("pe", lambda e: e.matmul(X.ps[po][:, h * 64:(h + 1) * 64], lhsT=Pm[b][:, h, :],
                                              rhs=vm[b][:, h * 64:(h + 1) * 64], start=(h == 0), stop=(c == 0)),
                     reads=["Pm%d" % b, "vm%d" % b], writes=["ps%d" % po])
                if c > 0:
                    s.op("pe", lambda e: e.matmul(X.ps[po][:, h * 64:(h + 1) * 64], lhsT=qT[b][:, h, :],
                                                  rhs=stateb[b][:, h, :], start=False, stop=True),
                         reads=["qT%d" % b, "stateb%d" % b], writes=["ps%d" % po])

        def gn_a(c):
            b = c % 2
            po = 2 if b == 0 else 6
            pres = "ps%d" % po
            o3 = X.ps[po][:].rearrange("p (h d) -> p h d", h=8)
            s.op("dve", lambda e: e.tensor_reduce(out=stt[:, 0:8], in_=o3, axis=AX.X, op=ALU.add),
                 reads=[pres], writes=["r_st"])
            s.op("act", lambda e: e.activation(out=sq[:], in_=X.ps[po][:], func=AF.Square), reads=[pres], writes=["r_sq"])
            s.op("dve", lambda e: e.tensor_reduce(out=stt[:, 8:16], in_=sq[:].rearrange("p (h d) -> p h d", h=8),
                                                  axis=AX.X, op=ALU.add), reads=["r_sq"], writes=["r_st"])
            s.op("dve", lambda e: e.tensor_scalar(out=stt[:, 16:24], in0=stt[:, 0:8], scalar1=1.0 / 64, scalar2=None,
                                                  op0=ALU.mult), reads=["r_st"], writes=["r_st"])
            s.op("dve", lambda e: e.tensor_tensor(out=stt[:, 24:32], in0=stt[:, 16:24], in1=stt[:, 16:24], op=ALU.mult),
                 reads=["r_st"], writes=["r_st"])
            s.op("dve", lambda e: e.scalar_tensor_tensor(out=stt[:, 32:40], in0=stt[:, 8:16], scalar=1.0 / 64,
                                                         in1=stt[:, 24:32], op0=ALU.mult, op1=ALU.subtract),
                 reads=["r_st"], writes=["r_st"])
            s.op("dve", lambda e: e.tensor_scalar(out=stt[:, 32:40], in0=stt[:, 32:40], scalar1=EPS, scalar2=None,
                                                  op0=ALU.add), reads=["r_st"], writes=["r_st"])
            s.op("act", lambda e: e.activation(out=stt[:, 40:48], in_=stt[:, 32:40], func=AF.Sqrt),
                 reads=["r_st"], writes=["r_st"])
            s.op("dve", lambda e: e.reciprocal(out=stt[:, 48:56], in_=stt[:, 40:48]), reads=["r_st"], writes=["r_st"])
            y3 = y[:].rearrange("p (h d) -> p h d", h=8)
            s.op("dve", lambda e: e.tensor_tensor(out=y3, in0=o3, in1=bcast_mid(stt[:, 16:24], 64), op=ALU.subtract),
                 reads=[pres, "r_st"], writes=["r_y"])
            s.op("dve", lambda e: e.tensor_tensor(out=y3, in0=y3, in1=bcast_mid(stt[:, 48:56], 64), op=ALU.mult),
                 reads=["r_st"], writes=["r_y"])
            s.op("pool", lambda e: e.tensor_tensor(out=y[:], in0=y[:], in1=gn[:], op=ALU.mult),
                 reads=["r_y", "gn"], writes=["r_y"])
            s.op("pool", lambda e: e.tensor_tensor(out=yb[b][:], in0=y[:], in1=gm[b][:], op=ALU.mult),
                 reads=["r_y", "gm%d" % b], writes=["r_yb%d" % b])

        def gn_b(c):
            b = c % 2
            tok = slice(c * 128, (c + 1) * 128)
            psb = X.ps[7][:].bitcast(BF16)
            for j in range(4):
                s.op("pe", lambda e: e.transpose(out=psb[:, j * 128:(j + 1) * 128], in_=yb[b][:, j * 128:(j + 1) * 128],
                                                 identity=X.ident[:]), reads=["r_yb%d" % b, "ident"], writes=["ps7"])
            s.op("act", lambda e: e.activation(out=rT[b][:].rearrange("p j t -> p (j t)"), in_=psb[:, 0:512], func=AF.Copy),
                 reads=["ps7"], writes=["r_rT%d" % b])
            s.dma("pool", X.AT[512:1024, tok].rearrange("(j p) t -> p j t", p=128), rT[b][:],
                  reads=["r_rT%d" % b], writes=["AT"])

        NCH = X.ret_chunks
        loads(0)
        for c in range(NCH):
            if c + 1 < NCH:
                loads(c + 1)
            t1m(c)
            upd(c)
            omm(c)
            if c >= 1:
                gn_b(c - 1)
            gn_a(c)
        gn_b(NCH - 1)
        s.barrier()


PHASES.append(("ret", phase_ret))


def conv_thunks(s, X, layers):
    th = []

    def conv(dst, src, rows):
        for r0 in range(0, rows, 512):
            th.append(lambda dst=dst, src=src, r0=r0: s.dma("pool", dst[r0:r0 + 512, :], src[r0:r0 + 512, :], writes=["W16"]))
    if 0 in layers:
        conv(X.FG16[0], X.ffn_gate[0], D)
        conv(X.FU16[0], X.ffn_up[0], D)
        conv(X.FD16[0], X.ffn_down[0], D_FF)
    if 1 in layers:
        for e in range(NE):
            for blk in range(7):
                r0 = (e * 7 + blk) * 128
                fs = slice(blk * 512, (blk + 1) * 512)
                th.append(lambda e=e, r0=r0, fs=fs: s.dma(
                    "pool", X.WGB[r0:r0 + 128, :].rearrange("p (k f) -> p k f", k=8),
                    X.moe_gate[0, e][:, fs].rearrange("(k p) f -> p k f", p=128), writes=["W16"]))
                th.append(lambda e=e, r0=r0, fs=fs: s.dma(
                    "pool", X.WUB[r0:r0 + 128, :].rearrange("p (k f) -> p k f", k=8),
                    X.moe_up[0, e][:, fs].rearrange("(k p) f -> p k f", p=128), writes=["W16"]))
                th.append(lambda e=e, r0=r0, fs=fs: s.dma(
                    "pool", X.WDB[r0:r0 + 128, :].rearrange("p (c n) -> p c n", c=4),
                    X.moe_down[0, e][fs, :].rearrange("(c p) n -> p c n", p=128), writes=["W16"]))
    return th


def phase_ffn(s, X, L, h_in):
    nc = X.nc
    last = (L == DEPTH - 1)
    moe = (L % 2 == 1)
    if moe and X.routed:
        return phase_moe(s, X, L, h_in)
    h_out = X.out if last else X.HB
    G16, U16, D16 = X.FG16, X.FU16, X.FD16
    with ExitStack() as es:
        sb = lambda name, shape, dt: es.enter_context(nc.sbuf_tensor("%s_L%d" % (name, L), shape, dt))
        wout = sb("f_wout", [128, 8, D], BF16)
        pgate = sb("f_pgate", [128, 8, D], BF16)
        pproj = sb("f_pproj", [128, 2, D], BF16)
        g2T = sb("f_g2T", [128, 8], F32)
        g3T = sb("f_g3T", [128, 8], F32)
        rw = sb("f_rw", [128, 8, NE], BF16)
        cT = sb("f_cT", [128, 8, 512], BF16)
        hin = [sb("f_hin%d" % i, [128, D], F32) for i in range(2)]
        h1 = sb("f_h1", [128, 4, D], F32)
        hnT = sb("f_hnT", [128, 8, 512], BF16)
        acc = sb("f_acc", [128, 4, D], F32)
        wg = [sb("f_wg%d" % i, [128, 8, 512], BF16) for i in range(3)]
        wu = [sb("f_wu%d" % i, [128, 8, 512], BF16) for i in range(3)]
        wd = [sb("f_wd%d" % i, [128, 4, D], BF16) for i in range(3)]
        silu_t = [sb("f_silu%d" % i, [128, 512], BF16) for i in range(2)]
        actb = [sb("f_actb%d" % i, [128, 4, 512], BF16) for i in range(2)]
        sig = [sb("f_sig%d" % i, [128, 512], F32) for i in range(2)]
        ptile = [sb("f_pt%d" % i, [128, 256], F32) for i in range(2)]
        pb = sb("f_pb", [128, 256], BF16)
        pT = sb("f_pT", [128, 2, 512], BF16)
        gates = sb("f_gates", [128, 4, NE], F32)
        lg = sb("f_lg", [128, 16], F32)
        sm = sb("f_sm", [128, 32], F32)
        for kc in range(8):
            s.dma("pool", wout[:, kc, :], X.w_out[L, kc * 128:(kc + 1) * 128, :], writes=["wout"])
            s.dma("pool", pgate[:, kc, :], X.ple_gate[L, kc * 128:(kc + 1) * 128, :], writes=["pgate"])
        for kc in range(2):
            s.dma("pool", pproj[:, kc, :], X.ple_proj[L, kc * 128:(kc + 1) * 128, :], writes=["pproj"])
        s.dma("sp", g2T[:], X.g_ffn[L].rearrange("(k p) -> p k", p=128), writes=["g2T"], allow_slow_non_contiguous=True)
        s.dma("sp", g3T[:], X.g_ple[L].rearrange("(k p) -> p k", p=128), writes=["g3T"], allow_slow_non_contiguous=True)
        if moe:
            s.dma("pool", rw[:], X.moe_router[0].rearrange("(k p) e -> p k e", p=128), writes=["rw"])
        if last:
            gfin = sb("f_gfin", [128, D], F32)
            ones1 = sb("f_ones1", [1, 128], F32)
            gf1 = sb("f_gf1", [1, D], F32)
            s.dma("sp", gf1[:], X.g_final.rearrange("(o n) -> o n", o=1), writes=["gf1"])
            s.op("dve", lambda e: e.memset(ones1[:], 1.0), writes=["ones1"])
            for half in range(2):
                s.op("pe", lambda e: e.matmul(X.ps[6][:], lhsT=ones1[:], rhs=gf1[:, half * 512:(half + 1) * 512],
                                              start=True, stop=True), reads=["ones1", "gf1"], writes=["ps6"])
                s.op("dve", lambda e: e.tensor_copy(out=gfin[:, half * 512:(half + 1) * 512], in_=X.ps[6][:]),
                     reads=["ps6"], writes=["gfin"])
        wcnt = 0
        for tg in range(NT // 4):
            toks = slice(tg * 512, (tg + 1) * 512)
            s.dma("sp", cT[:], X.AT[:, toks].rearrange("(k p) t -> p k t", p=128), reads=["AT"], writes=["cT"])
            for tt in range(4):
                ti = 4 * tg + tt
                hb_ = hin[ti % 2]
                s.dma("sp", hb_[:], h_in[ti * 128:(ti + 1) * 128, :], reads=["HB"], writes=["hin%d" % (ti % 2)])
                for half in range(2):
                    hs = slice(half * 512, (half + 1) * 512)
                    for k in range(8):
                        s.op("pe", lambda e: e.matmul(X.ps[6][:], lhsT=cT[:, k, tt * 128:(tt + 1) * 128], rhs=wout[:, k, hs],
                                                      start=(k == 0), stop=(k == 7)), reads=["cT", "wout"], writes=["ps6"])
                    s.op("dve", lambda e: e.tensor_tensor(out=h1[:, tt, hs], in0=X.ps[6][:], in1=hb_[:, hs], op=ALU.add),
                         reads=["ps6", "hin%d" % (ti % 2)], writes=["h1_%d" % tt])
                ln_a(s, X, h1[:, tt, :], "h1_%d" % tt, tt)
            for tt in range(4):
                ln_b(s, X, tt, g2T[:], "g2T", hnT[:, :, tt * 128:(tt + 1) * 128], "f_hnT")
            if moe:
                for tt in range(4):
                    for k in range(8):
                        s.op("pe", lambda e: e.matmul(X.ps[6][:, 0:NE], lhsT=hnT[:, k, tt * 128:(tt + 1) * 128], rhs=rw[:, k, :],
                                                      start=(k == 0), stop=(k == 7)), reads=["f_hnT", "rw"], writes=["ps6"])
                    s.op("dve", lambda e: e.tensor_copy(out=lg[:, 0:8], in_=X.ps[6][:, 0:NE]), reads=["ps6"], writes=["lg"])
                    s.op("dve", lambda e: e.max(out=sm[:, 0:8], in_=lg[:, 0:8]), reads=["lg"], writes=["fsm"])
                    s.op("dve", lambda e: e.tensor_tensor(out=sm[:, 8:9], in0=sm[:, 1:2], in1=sm[:, 0:1], op=ALU.subtract),
                         reads=["fsm"], writes=["fsm"])
                    s.op("act", lambda e: e.activation(out=sm[:, 9:10], in_=sm[:, 8:9], func=AF.Exp), reads=["fsm"], writes=["fsm"])
                    s.op("dve", lambda e: e.tensor_scalar(out=sm[:, 10:11], in0=sm[:, 9:10], scalar1=1.0, scalar2=None,
                                                          op0=ALU.add), reads=["fsm"], writes=["fsm"])
                    s.op("dve", lambda e: e.reciprocal(out=sm[:, 11:12], in_=sm[:, 10:11]), reads=["fsm"], writes=["fsm"])
                    s.op("dve", lambda e: e.tensor_tensor(out=sm[:, 12:13], in0=sm[:, 9:10], in1=sm[:, 11:12], op=ALU.mult),
                         reads=["fsm"], writes=["fsm"])
                    s.op("dve", lambda e: e.tensor_scalar(out=lg[:, 8:16], in0=lg[:, 0:8], scalar1=sm[:, 0:1],
                                                          scalar2=sm[:, 11:12], op0=ALU.is_equal, op1=ALU.mult),
                         reads=["lg", "fsm"], writes=["lg"])
                    s.op("dve", lambda e: e.tensor_scalar(out=gates[:, tt, :], in0=lg[:, 0:8], scalar1=sm[:, 1:2],
                                                          scalar2=sm[:, 12:13], op0=ALU.is_equal, op1=ALU.mult),
                         reads=["lg", "fsm"], writes=["gates"])
                    s.op("dve", lambda e: e.tensor_tensor(out=gates[:, tt, :], in0=gates[:, tt, :], in1=lg[:, 8:16], op=ALU.add),
                         reads=["lg"], writes=["gates"])
            def stage_a(J):
                wb, ab = J % 3, J % 2
                for fc in range(4):
                    pg, pu = 2 * (fc % 2), 2 * (fc % 2) + 1
                    for k in range(8):
                        s.op("pe", lambda e: e.matmul(X.ps[pg][:], lhsT=wg[wb][:, k, fc * 128:(fc + 1) * 128], rhs=hnT[:, k, :],
                                                      start=(k == 0), stop=(k == 7)),
                             reads=["wg%d" % wb, "f_hnT"], writes=["ps%d" % pg])
                    for k in range(8):
                        s.op("pe", lambda e: e.matmul(X.ps[pu][:], lhsT=wu[wb][:, k, fc * 128:(fc + 1) * 128], rhs=hnT[:, k, :],
                                                      start=(k == 0), stop=(k == 7)),
                             reads=["wu%d" % wb, "f_hnT"], writes=["ps%d" % pu])
                    s.op("act", lambda e: e.activation(out=silu_t[fc % 2][:], in_=X.ps[pg][:], func=AF.Silu),
                         reads=["ps%d" % pg], writes=["silu%d" % (fc % 2)])
                    s.op("dve", lambda e: e.tensor_tensor(out=actb[ab][:, fc, :], in0=X.ps[pu][:], in1=silu_t[fc % 2][:],
                                                          op=ALU.mult),
                         reads=["ps%d" % pu, "silu%d" % (fc % 2)], writes=["actb%d" % ab])

            def stage_b(J):
                wb, ab, blk = J % 3, J % 2, J % 7
                for tt in range(4):
                    for half in range(2):
                        hs = slice(half * 512, (half + 1) * 512)
                        pd = 4 + half
                        for fc in range(4):
                            s.op("pe", lambda e: e.matmul(X.ps[pd][:], lhsT=actb[ab][:, fc, tt * 128:(tt + 1) * 128],
                                                          rhs=wd[wb][:, fc, hs], start=(fc == 0), stop=(fc == 3)),
                                 reads=["actb%d" % ab, "wd%d" % wb], writes=["ps%d" % pd])
                        dst = acc[:, tt, hs]
                        if blk == 0:
                            s.op("dve", lambda e: e.tensor_copy(out=dst, in_=X.ps[pd][:]),
                                 reads=["ps%d" % pd], writes=["acc%d" % tt])
                        else:
                            s.op("dve", lambda e: e.tensor_tensor(out=dst, in0=X.ps[pd][:], in1=dst, op=ALU.add),
                                 reads=["ps%d" % pd], writes=["acc%d" % tt])

            def wload(J):
                blk, wb = J % 7, J % 3
                fs = slice(blk * 512, (blk + 1) * 512)
                s.dma("sp", wg[wb][:], G16[0][:, fs].rearrange("(k p) f -> p k f", p=128), reads=["W16"], writes=["wg%d" % wb])
                s.dma("sp", wu[wb][:], U16[0][:, fs].rearrange("(k p) f -> p k f", p=128), reads=["W16"], writes=["wu%d" % wb])
                s.dma("sp", wd[wb][:], D16[0][fs, :].rearrange("(c p) n -> p c n", p=128), reads=["W16"], writes=["wd%d" % wb])

            NJ = (NT // 4) * 7
            if tg == 0:
                wload(0)
                wload(1)
            for blk in range(7):
                J = tg * 7 + blk
                stage_a(J)
                if blk >= 1:
                    stage_b(J - 1)
                if J + 2 < NJ:
                    wload(J + 2)
            stage_b(tg * 7 + 6)
            for tt in range(4):
                s.op("pool", lambda e: e.tensor_tensor(out=h1[:, tt, :], in0=h1[:, tt, :], in1=acc[:, tt, :], op=ALU.add),
                     reads=["acc%d" % tt], writes=["h1_%d" % tt])
                ln_a(s, X, h1[:, tt, :], "h1_%d" % tt, tt)
            for tt in range(4):
                ln_b(s, X, tt, g3T[:], "g3T", hnT[:, :, tt * 128:(tt + 1) * 128], "f_hnT")
            psb = X.ps[7][:].bitcast(BF16)
            for tt in range(4):
                ti = 4 * tg + tt
                pt_ = ptile[ti % 2]
                s.dma("sp", pt_[:], X.p[L, ti * 128:(ti + 1) * 128, :], writes=["pt%d" % (ti % 2)])
                s.op("act", lambda e: e.activation(out=pb[:], in_=pt_[:], func=AF.Copy), reads=["pt%d" % (ti % 2)], writes=["pb"])
                for k in range(2):
                    s.op("pe", lambda e: e.transpose(out=psb[:, k * 128:(k + 1) * 128], in_=pb[:, k * 128:(k + 1) * 128],
                                                     identity=X.ident[:]), reads=["pb", "ident"], writes=["ps7"])
                s.op("act", lambda e: e.activation(out=pT[:, :, tt * 128:(tt + 1) * 128],
                                                   in_=psb[:, 0:256].rearrange("p (k t) -> p k t", k=2), func=AF.Copy),
                     reads=["ps7"], writes=["pT"])
            for tt in range(4):
                ti = 4 * tg + tt
                for half in range(2):
                    hs = slice(half * 512, (half + 1) * 512)
                    for k in range(2):
                        s.op("pe", lambda e: e.matmul(X.ps[6][:], lhsT=pT[:, k, tt * 128:(tt + 1) * 128], rhs=pproj[:, k, hs],
                                                      start=(k == 0), stop=(k == 1)), reads=["pT", "pproj"], writes=["ps6"])
                    pgb = 4 + half
                    for k in range(8):
                        s.op("pe", lambda e: e.matmul(X.ps[pgb][:], lhsT=hnT[:, k, tt * 128:(tt + 1) * 128], rhs=pgate[:, k, hs],
                                                      start=(k == 0), stop=(k == 7)), reads=["f_hnT", "pgate"], writes=["ps%d" % pgb])
                    s.op("act", lambda e: e.activation(out=sig[half][:], in_=X.ps[pgb][:], func=AF.Sigmoid),
                         reads=["ps%d" % pgb], writes=["sig%d" % half])
                    s.op("dve", lambda e: e.tensor_tensor(out=acc[:, tt, hs], in0=X.ps[6][:], in1=sig[half][:], op=ALU.mult),
                         reads=["ps6", "sig%d" % half], writes=["acc%d" % tt])
                s.op("pool", lambda e: e.tensor_tensor(out=h1[:, tt, :], in0=h1[:, tt, :], in1=acc[:, tt, :], op=ALU.add),
                     reads=["acc%d" % tt], writes=["h1_%d" % tt])
                if last:
                    ln_a(s, X, h1[:, tt, :], "h1_%d" % tt, tt)
                    s.op("act", lambda e: e.activation(out=acc[:, tt, :], in_=h1[:, tt, :], func=AF.Copy, scale=X.ss[tt][:, 2:3]),
                         reads=["h1_%d" % tt, "ss%d" % tt], writes=["acc%d" % tt])
                    s.op("pool", lambda e: e.tensor_tensor(out=acc[:, tt, :], in0=acc[:, tt, :], in1=gfin[:], op=ALU.mult),
                         reads=["gfin"], writes=["acc%d" % tt])
                    s.dma("pool", h_out[ti * 128:(ti + 1) * 128, :], acc[:, tt, :], reads=["acc%d" % tt], writes=["HOUT"])
                else:
                    s.dma("pool", h_out[ti * 128:(ti + 1) * 128, :], h1[:, tt, :], reads=["h1_%d" % tt], writes=["HB"])
        s.barrier()


PHASES.append(("ffn", phase_ffn))


def phase_moe(s, X, L, h_in):
    nc = X.nc
    last = (L == DEPTH - 1)
    h_out = X.out if last else X.HB
    with ExitStack() as es:
        sb = lambda name, shape, dt: es.enter_context(nc.sbuf_tensor("%s_L%d" % (name, L), shape, dt))
        OH = [sb("m_oh%d" % k, [128, NT, NE], F32) for k in range(2)]
        WK = [sb("m_wk%d" % k, [128, NT], F32) for k in range(2)]
        LG = sb("m_lg", [128, NT, NE], F32)
        POSI2 = sb("m_posi", [128, 2 * NT], I32)
        POSI = POSI2[:].rearrange("p (k i) -> p k i", k=2)
        WIDX2 = sb("m_widx", [128, NGRP * 7], I32)
        WIDX = WIDX2[:].rearrange("p (g b) -> p g b", b=7)
        g2T = sb("m_g2T", [128, 8], F32)
        g3T = sb("m_g3T", [128, 8], F32)
        hnT = sb("m_hnT", [128, 8, 512], BF16)
        s.dma("sp", g2T[:], X.g_ffn[L].rearrange("(k p) -> p k", p=128), writes=["g2T"], allow_slow_non_contiguous=True)
        s.dma("sp", g3T[:], X.g_ple[L].rearrange("(k p) -> p k", p=128), writes=["g3T"], allow_slow_non_contiguous=True)
        with ExitStack() as es2:
            sb2 = lambda name, shape, dt: es2.enter_context(nc.sbuf_tensor("%s_L%d" % (name, L), shape, dt))
            wout = sb2("ma_wout", [128, 8, D], BF16)
            rw = sb2("ma_rw", [128, 8, NE], BF16)
            cT = sb2("ma_cT", [128, 8, 512], BF16)
            hin = [sb2("ma_hin%d" % i, [128, D], F32) for i in range(2)]
            h1 = [sb2("ma_h1%d" % i, [128, D], F32) for i in range(4)]
            lg = sb2("ma_lg", [128, 16], F32)
            sm = sb2("ma_sm", [128, 32], F32)
            for kc in range(8):
                s.dma("pool", wout[:, kc, :], X.w_out[L, kc * 128:(kc + 1) * 128, :], writes=["wout"])
            s.dma("pool", rw[:], X.moe_router[0].rearrange("(k p) e -> p k e", p=128), writes=["rw"])
            for tg in range(NT // 4):
                toks = slice(tg * 512, (tg + 1) * 512)
                s.dma("sp", cT[:], X.AT[:, toks].rearrange("(k p) t -> p k t", p=128), reads=["AT"], writes=["cT"])
                for tt in range(4):
                    ti = 4 * tg + tt
                    hb_ = hin[ti % 2]
                    s.dma("sp", hb_[:], h_in[ti * 128:(ti + 1) * 128, :], reads=["HB"], writes=["hin%d" % (ti % 2)])
                    for half in range(2):
                        hs = slice(half * 512, (half + 1) * 512)
                        pbk = 4 + half
                        for k in range(8):
                            s.op("pe", lambda e: e.matmul(X.ps[pbk][:], lhsT=cT[:, k, tt * 128:(tt + 1) * 128], rhs=wout[:, k, hs],
                                                          start=(k == 0), stop=(k == 7)), reads=["cT", "wout"], writes=["ps%d" % pbk])
                        s.op("dve", lambda e: e.tensor_tensor(out=h1[tt][:, hs], in0=X.ps[pbk][:], in1=hb_[:, hs], op=ALU.add),
                             reads=["ps%d" % pbk, "hin%d" % (ti % 2)], writes=["h1_%d" % tt])
                    s.dma("pool", X.H1[ti * 128:(ti + 1) * 128, :], h1[tt][:], reads=["h1_%d" % tt], writes=["H1"])
                    ln_a(s, X, h1[tt][:], "h1_%d" % tt, tt)
                    s.dma("pool", X.XS2[ti * 128:(ti + 1) * 128, :], X.xs[tt][:], reads=["xs%d" % tt], writes=["XS2"])
                for tt in range(4):
                    ln_b(s, X, tt, g2T[:], "g2T", hnT[:, :, tt * 128:(tt + 1) * 128], "m_hnT")
                for tt in range(4):
                    ti = 4 * tg + tt
                    for k in range(8):
                        s.op("pe", lambda e: e.matmul(X.ps[6][:, 0:NE], lhsT=hnT[:, k, tt * 128:(tt + 1) * 128], rhs=rw[:, k, :],
                                                      start=(k == 0), stop=(k == 7)), reads=["m_hnT", "rw"], writes=["ps6"])
                    s.op("dve", lambda e: e.tensor_copy(out=LG[:, ti, :], in_=X.ps[6][:, 0:NE]), reads=["ps6"], writes=["LG"])
            s.barrier()
        with ExitStack() as es2:
            sb2 = lambda name, shape, dt: es2.enter_context(nc.sbuf_tensor("%s_L%d" % (name, L), shape, dt))
            Mf = sb2("mb_Mf", [128, NT, NE], F32)
            Mb = sb2("mb_Mb", [128, NT * NE], BF16)
            U = sb2("mb_U", [128, 128], BF16)
            ones = sb2("mb_ones", [128, 128], BF16)
            A_ = sb2("mb_A", [128, NT, NE], F32)
            B_ = sb2("mb_B", [128, NT, NE], F32)
            CN = sb2("mb_CN", [128, NT, NE], F32)
            POS = sb2("mb_POS", [128, NT, NE], F32)
            nE = sb2("mb_nE", [128, 64], F32)
            cmp16 = sb2("mb_cmp16", [128, NE, 16], F32)
            gi512 = sb2("mb_gi512", [128, NGRP], F32)
            tokid = sb2("mb_tokid", [128, NT], F32)
            blkp = sb2("mb_blkp", [128, 7], F32)
            cmpg = sb2("mb_cmpg", [128, NGRP, NE], F32)
            EG = sb2("mb_EG", [128, NGRP], F32)
            WF = sb2("mb_WF", [128, NGRP, 7], F32)
            PK = sb2("mb_PK", [128, 2, NT], F32)
            ROWS2 = sb2("mb_rows", [128, 2 * NT * 2], I32)
            ROWS = ROWS2[:].rearrange("p (k i c) -> p k i c", k=2, c=2)
            Z = sb2("mb_Z", [128, 2 * NSLOT // 128], I32)
            s.dma("pool", U[:], X.c["c_U"], writes=["U"])
            s.dma("sp", gi512[:], X.c["c_gi512"], writes=["gi512"])
            s.dma("sp", tokid[:], X.c["c_tokid"], writes=["tokid"])
            s.dma("sp", blkp[:], X.c["c_blkp"], writes=["blkp"])
            s.op("pool", lambda e: e.memset(ones[:], 1.0), writes=["ones"])
            s.op("pool", lambda e: e.memset(Z[:], 0), writes=["Z"])
            s.dma("sp", X.SLOT.rearrange("(p a) c -> p (a c)", p=128), Z[:], reads=["Z"], writes=["SLOT"])
            M12 = sb2("mb_M12", [128, 4, NT], F32)
            LG2 = sb2("mb_LG2", [128, NT, NE], F32)
            s.op("dve", lambda e: e.tensor_reduce(out=M12[:, 0, :], in_=LG[:], axis=AX.X, op=ALU.max), reads=["LG"], writes=["M12"])
            s.op("dve", lambda e: e.tensor_tensor(out=OH[0][:], in0=LG[:], in1=bcast_mid(M12[:, 0, :], NE), op=ALU.is_equal),
                 reads=["LG", "M12"], writes=["OH"])
            s.op("dve", lambda e: e.scalar_tensor_tensor(out=LG2[:], in0=OH[0][:], scalar=-1.0e30, in1=LG[:], op0=ALU.mult, op1=ALU.add),
                 reads=["OH", "LG"], writes=["LG2"])
            s.op("dve", lambda e: e.tensor_reduce(out=M12[:, 1, :], in_=LG2[:], axis=AX.X, op=ALU.max), reads=["LG2"], writes=["M12"])
            s.op("dve", lambda e: e.tensor_tensor(out=OH[1][:], in0=LG2[:], in1=bcast_mid(M12[:, 1, :], NE), op=ALU.is_equal),
                 reads=["LG2", "M12"], writes=["OH"])
            s.op("dve", lambda e: e.tensor_tensor(out=M12[:, 2, :], in0=M12[:, 1, :], in1=M12[:, 0, :], op=ALU.subtract),
                 reads=["M12"], writes=["M12"])
            s.op("act", lambda e: e.activation(out=M12[:, 3, :], in_=M12[:, 2, :], func=AF.Exp), reads=["M12"], writes=["M12"])
            s.op("dve", lambda e: e.tensor_scalar(out=M12[:, 2, :], in0=M12[:, 3, :], scalar1=1.0, scalar2=None, op0=ALU.add),
                 reads=["M12"], writes=["M12"])
            s.op("dve", lambda e: e.reciprocal(out=WK[0][:], in_=M12[:, 2, :]), reads=["M12"], writes=["WK"])
            s.op("dve", lambda e: e.tensor_tensor(out=WK[1][:], in0=M12[:, 3, :], in1=WK[0][:], op=ALU.mult), reads=["M12", "WK"], writes=["WK"])
            s.op("dve", lambda e: e.tensor_tensor(out=Mf[:], in0=OH[0][:], in1=OH[1][:], op=ALU.add), reads=["OH"], writes=["Mf"])
            s.op("dve", lambda e: e.tensor_copy(out=Mb[:], in_=Mf[:].rearrange("p i e -> p (i e)")), reads=["Mf"], writes=["Mb"])
            s.op("pe", lambda e: e.matmul(X.ps[0][:], lhsT=U[:], rhs=Mb[:], start=True, stop=True), reads=["U", "Mb"], writes=["ps0"])
            s.op("pe", lambda e: e.matmul(X.ps[1][:], lhsT=ones[:], rhs=Mb[:], start=True, stop=True), reads=["ones", "Mb"], writes=["ps1"])
            s.op("dve", lambda e: e.tensor_copy(out=CN[:].rearrange("p i e -> p (i e)"), in_=X.ps[1][:]), reads=["ps1"], writes=["CN"])
            s.op("dve", lambda e: e.tensor_copy(out=A_[:], in_=CN[:]), reads=["CN"], writes=["A"])
            src, dst, sres, dres = A_, B_, "A", "B"
            for sh in (1, 2, 4, 8, 16, 32):
                s.op("dve", lambda e: e.tensor_copy(out=dst[:, 0:sh, :], in_=src[:, 0:sh, :]), reads=[sres], writes=[dres])
                s.op("dve", lambda e: e.tensor_tensor(out=dst[:, sh:NT, :], in0=src[:, sh:NT, :], in1=src[:, 0:NT - sh, :],
                                                      op=ALU.add), reads=[sres], writes=[dres])
                src, dst, sres, dres = dst, src, dres, sres
            incl, ires = src, sres
            s.op("dve", lambda e: e.tensor_copy(out=nE[:, 0:8], in_=incl[:, NT - 1, :]), reads=[ires], writes=["nE"])
            s.op("dve", lambda e: e.tensor_tensor(out=cmp16[:], in0=bcast_mid(nE[:, 0:8], 16), in1=bcast_rep(gi512[:, 0:16], NE),
                                                  op=ALU.is_gt), reads=["nE", "gi512"], writes=["cmp16"])
            s.op("dve", lambda e: e.tensor_reduce(out=nE[:, 8:16], in_=cmp16[:], axis=AX.X, op=ALU.add), reads=["cmp16"], writes=["nE"])
            s.op("dve", lambda e: e.tensor_scalar(out=nE[:, 8:16], in0=nE[:, 8:16], scalar1=512.0, scalar2=None, op0=ALU.mult),
                 reads=["nE"], writes=["nE"])
            s.op("dve", lambda e: e.tensor_copy(out=nE[:, 16:17], in_=nE[:, 8:9]), reads=["nE"], writes=["nE"])
            for e_ in range(1, NE):
                s.op("dve", lambda e: e.tensor_tensor(out=nE[:, 16 + e_:17 + e_], in0=nE[:, 15 + e_:16 + e_], in1=nE[:, 8 + e_:9 + e_],
                                                      op=ALU.add), reads=["nE"], writes=["nE"])
            s.op("dve", lambda e: e.tensor_tensor(out=nE[:, 24:32], in0=nE[:, 16:24], in1=nE[:, 8:16], op=ALU.subtract),
                 reads=["nE"], writes=["nE"])
            s.op("dve", lambda e: e.tensor_tensor(out=POS[:], in0=incl[:], in1=CN[:], op=ALU.subtract), reads=[ires, "CN"], writes=["POS"])
            s.op("dve", lambda e: e.tensor_tensor(out=POS[:].rearrange("p i e -> p (i e)"), in0=POS[:].rearrange("p i e -> p (i e)"),
                                                  in1=X.ps[0][:], op=ALU.add), reads=["ps0"], writes=["POS"])
            s.op("dve", lambda e: e.tensor_tensor(out=POS[:], in0=POS[:], in1=bcast_rep(nE[:, 24:32], NT), op=ALU.add),
                 reads=["nE"], writes=["POS"])
            for k in range(2):
                s.op("dve", lambda e: e.tensor_tensor(out=Mf[:], in0=POS[:], in1=OH[k][:], op=ALU.mult), reads=["POS", "OH"], writes=["Mf"])
                s.op("dve", lambda e: e.tensor_reduce(out=PK[:, k, :], in_=Mf[:], axis=AX.X, op=ALU.add), reads=["Mf"], writes=["PK"])
                s.op("dve", lambda e: e.tensor_copy(out=ROWS[:, k, :, 0], in_=tokid[:]), reads=["tokid"], writes=["ROWS"])
                s.op("dve", lambda e: e.tensor_copy(out=ROWS[:, k, :, 1], in_=WK[k][:].bitcast(I32)), reads=["WK"], writes=["ROWS"])
            s.op("dve", lambda e: e.tensor_copy(out=POSI, in_=PK[:]), reads=["PK"], writes=["POSI"])
            s.op("dve", lambda e: e.tensor_tensor(out=cmpg[:], in0=bcast_rep(nE[:, 16:24], NGRP), in1=bcast_mid(gi512[:], NE),
                                                  op=ALU.is_le), reads=["nE", "gi512"], writes=["cmpg"])
            s.op("dve", lambda e: e.tensor_reduce(out=EG[:], in_=cmpg[:], axis=AX.X, op=ALU.add), reads=["cmpg"], writes=["EG"])
            s.op("dve", lambda e: e.tensor_scalar(out=EG[:], in0=EG[:], scalar1=float(NE - 1), scalar2=896.0, op0=ALU.min, op1=ALU.mult),
                 reads=["EG"], writes=["EG"])
            s.op("dve", lambda e: e.tensor_tensor(out=WF[:], in0=bcast_mid(EG[:], 7), in1=bcast_rep(blkp[:], NGRP), op=ALU.add),
                 reads=["EG", "blkp"], writes=["WF"])
            s.op("dve", lambda e: e.tensor_copy(out=WIDX, in_=WF[:]), reads=["WF"], writes=["WIDX"])
            for ti in range(NT):
                for k in range(2):
                    s.idma(X.SLOT[:, :], POSI2[:, k * NT + ti:k * NT + ti + 1],
                           ROWS2[:, (k * NT + ti) * 2:(k * NT + ti) * 2 + 2], None, NSLOT - 1,
                           reads=["ROWS", "POSI"], writes=["SLOT"])
            s.barrier()
        with ExitStack() as es2:
            sb2 = lambda name, shape, dt: es2.enter_context(nc.sbuf_tensor("%s_L%d" % (name, L), shape, dt))
            stt = [sb2("mc_st%d" % i, [128, 8], I32) for i in range(2)]
            xg = [sb2("mc_xg%d" % i, [128, D], BF16) for i in range(8)]
            acc = [sb2("mc_acc%d" % i, [128, 4, D], F32) for i in range(2)]
            wg = [sb2("mc_wg%d" % i, [128, 8, 512], BF16) for i in range(3)]
            wu = [sb2("mc_wu%d" % i, [128, 8, 512], BF16) for i in range(3)]
            wd = [sb2("mc_wd%d" % i, [128, 4, D], BF16) for i in range(3)]
            silu_t = [sb2("mc_silu%d" % i, [128, 512], BF16) for i in range(2)]
            actb = [sb2("mc_actb%d" % i, [128, 4, 512], BF16) for i in range(2)]
            hn = [sb2("mc_hn%d" % i, [128, 8, 512], BF16) for i in range(2)]
            NWB = len(wg)

            def prologue_loads(gi):
                gb = gi % 2
                s.dma("sp", stt[gb][:].rearrange("p (tt c) -> p tt c", c=2),
                      X.SLOT[gi * 512:(gi + 1) * 512, :].rearrange("(tt p) c -> p tt c", p=128),
                      reads=["SLOT"], writes=["st%d" % gb])
                for tt in range(4):
                    s.idma(xg[gb * 4 + tt][:, :], None, X.XS2[:, :], stt[gb][:, 2 * tt:2 * tt + 1], SEQ - 1,
                           reads=["XS2", "st%d" % gb], writes=["xg%d" % (gb * 4 + tt)])

            def prologue_tr(gi):
                gb = gi % 2
                for tt in range(4):
                    ln_b(s, X, tt, g2T[:], "g2T", hn[gb][:, :, tt * 128:(tt + 1) * 128], "hn%d" % gb,
                         src=xg[gb * 4 + tt], src_res="xg%d" % (gb * 4 + tt))

            def wload(j):
                gi, blk = divmod(j, 7)
                wb = j % NWB
                off = WIDX2[:, gi * 7 + blk:gi * 7 + blk + 1]
                s.idma(wg[wb][:].rearrange("p k f -> p (k f)"), None, X.WGB[:, :], off, 0, reads=["W16", "WIDX"], writes=["wg%d" % wb])
                s.idma(wu[wb][:].rearrange("p k f -> p (k f)"), None, X.WUB[:, :], off, 0, reads=["W16", "WIDX"], writes=["wu%d" % wb])
                s.idma(wd[wb][:].rearrange("p c n -> p (c n)"), None, X.WDB[:, :], off, 0, reads=["W16", "WIDX"], writes=["wd%d" % wb])

            def stage_a(j):
                gi, blk = divmod(j, 7)
                gb, wb, ab = gi % 2, j % NWB, j % 2
                for fc in range(4):
                    pg, pu = 2 * (fc % 2), 2 * (fc % 2) + 1
                    for k in range(8):
                        s.op("pe", lambda e: e.matmul(X.ps[pg][:], lhsT=wg[wb][:, k, fc * 128:(fc + 1) * 128], rhs=hn[gb][:, k, :],
                                                      start=(k == 0), stop=(k == 7)),
                             reads=["wg%d" % wb, "hn%d" % gb], writes=["ps%d" % pg])
                    for k in range(8):
                        s.op("pe", lambda e: e.matmul(X.ps[pu][:], lhsT=wu[wb][:, k, fc * 128:(fc + 1) * 128], rhs=hn[gb][:, k, :],
                                                      start=(k == 0), stop=(k == 7)),
                             reads=["wu%d" % wb, "hn%d" % gb], writes=["ps%d" % pu])
                    s.op("act", lambda e: e.activation(out=silu_t[fc % 2][:], in_=X.ps[pg][:], func=AF.Silu),
                         reads=["ps%d" % pg], writes=["silu%d" % (fc % 2)])
                    s.op("dve", lambda e: e.tensor_tensor(out=actb[ab][:, fc, :], in0=X.ps[pu][:], in1=silu_t[fc % 2][:],
                                                          op=ALU.mult),
                         reads=["ps%d" % pu, "silu%d" % (fc % 2)], writes=["actb%d" % ab])

            def stage_b(j):
                gi, blk = divmod(j, 7)
                gb, wb, ab = gi % 2, j % NWB, j % 2
                for tt in range(4):
                    gsc = stt[gb][:, 2 * tt + 1:2 * tt + 2].bitcast(F32)
                    for half in range(2):
                        hs = slice(half * 512, (half + 1) * 512)
                        pd = 4 + half
                        for fc in range(4):
                            s.op("pe", lambda e: e.matmul(X.ps[pd][:], lhsT=actb[ab][:, fc, tt * 128:(tt + 1) * 128],
                                                          rhs=wd[wb][:, fc, hs], start=(fc == 0), stop=(fc == 3)),
                                 reads=["actb%d" % ab, "wd%d" % wb], writes=["ps%d" % pd])
                        dst = acc[gb][:, tt, hs]
                        if blk == 0:
                            s.op("dve", lambda e: e.tensor_scalar(out=dst, in0=X.ps[pd][:], scalar1=gsc, scalar2=None, op0=ALU.mult),
                                 reads=["ps%d" % pd, "st%d" % gb], writes=["acc%d_%d" % (gb, tt)])
                        else:
                            s.op("dve", lambda e: e.scalar_tensor_tensor(out=dst, in0=X.ps[pd][:], scalar=gsc, in1=dst,
                                                                         op0=ALU.mult, op1=ALU.add),
                                 reads=["ps%d" % pd, "st%d" % gb], writes=["acc%d_%d" % (gb, tt)])
                if blk == 6:
                    s.dma("sp", X.YS[gi * 512:(gi + 1) * 512, :].rearrange("(tt p) n -> p tt n", p=128), acc[gb][:],
                          reads=["acc%d_%d" % (gb, tt) for tt in range(4)], writes=["YS"])

            NJ = NGRP * 7
            prologue_loads(0)
            prologue_tr(0)
            for j in range(min(NWB - 1, NJ)):
                wload(j)
            for j in range(NJ + 1):
                if j < NJ:
                    gi, blk = divmod(j, 7)
                    stage_a(j)
                    if blk == 6 and gi + 1 < NGRP:
                        prologue_tr(gi + 1)
                if j >= 1:
                    stage_b(j - 1)
                if j < NJ and j % 7 == 0 and j // 7 + 1 < NGRP:
                    prologue_loads(j // 7 + 1)
                if j + NWB - 1 < NJ:
                    wload(j + NWB - 1)
            s.barrier()
        with ExitStack() as es2:
            sb2 = lambda name, shape, dt: es2.enter_context(nc.sbuf_tensor("%s_L%d" % (name, L), shape, dt))
            pgate = sb2("md_pgate", [128, 8, D], BF16)
            pproj = sb2("md_pproj", [128, 2, D], BF16)
            h1 = [sb2("md_h1%d" % i, [128, 4, D], F32) for i in range(2)]
            acc = [sb2("md_acc%d" % i, [128, 4, D], F32) for i in range(2)]
            hnd = [sb2("md_hnd%d" % i, [128, 8, 512], BF16) for i in range(2)]
            yA = [sb2("md_yA%d" % i, [128, D], F32) for i in range(2)]
            yB = [sb2("md_yB%d" % i, [128, D], F32) for i in range(2)]
            sig = [sb2("md_sig%d" % i, [128, 512], F32) for i in range(2)]
            ptile = [sb2("md_pt%d" % i, [128, 256], F32) for i in range(2)]
            pb = sb2("md_pb", [128, 256], BF16)
            pT = [sb2("md_pT%d" % i, [128, 2, 512], BF16) for i in range(2)]
            for kc in range(8):
                s.dma("pool", pgate[:, kc, :], X.ple_gate[L, kc * 128:(kc + 1) * 128, :], writes=["pgate"])
            for kc in range(2):
                s.dma("pool", pproj[:, kc, :], X.ple_proj[L, kc * 128:(kc + 1) * 128, :], writes=["pproj"])
            if last:
                gfin = sb2("md_gfin", [128, D], F32)
                ones1 = sb2("md_ones1", [1, 128], F32)
                gf1 = sb2("md_gf1", [1, D], F32)
                s.dma("sp", gf1[:], X.g_final.rearrange("(o n) -> o n", o=1), writes=["gf1"])
                s.op("dve", lambda e: e.memset(ones1[:], 1.0), writes=["ones1"])
                for half in range(2):
                    s.op("pe", lambda e: e.matmul(X.ps[6][:], lhsT=ones1[:], rhs=gf1[:, half * 512:(half + 1) * 512],
                                                  start=True, stop=True), reads=["ones1", "gf1"], writes=["ps6"])
                    s.op("dve", lambda e: e.tensor_copy(out=gfin[:, half * 512:(half + 1) * 512], in_=X.ps[6][:]),
                         reads=["ps6"], writes=["gfin"])
            def stage1(tg):
                hb = tg % 2
                for tt in range(4):
                    ti = 4 * tg + tt
                    yb_ = ti % 2
                    hres = "h1_%d_%d" % (hb, tt)
                    s.dma("sp", h1[hb][:, tt, :], X.H1[ti * 128:(ti + 1) * 128, :], reads=["H1"], writes=[hres])
                    s.idma(yA[yb_][:, :], None, X.YS[:, :], POSI2[:, ti:ti + 1], 0, reads=["YS", "POSI"], writes=["yA%d" % yb_])
                    s.idma(yB[yb_][:, :], None, X.YS[:, :], POSI2[:, NT + ti:NT + ti + 1], 0, reads=["YS", "POSI"], writes=["yB%d" % yb_])
                    s.op("dve", lambda e: e.tensor_tensor(out=h1[hb][:, tt, :], in0=h1[hb][:, tt, :], in1=yA[yb_][:], op=ALU.add),
                         reads=["yA%d" % yb_], writes=[hres])
                    s.op("dve", lambda e: e.tensor_tensor(out=h1[hb][:, tt, :], in0=h1[hb][:, tt, :], in1=yB[yb_][:], op=ALU.add),
                         reads=["yB%d" % yb_], writes=[hres])
                    ln_a(s, X, h1[hb][:, tt, :], hres, tt)
                for tt in range(4):
                    ln_b(s, X, tt, g3T[:], "g3T", hnd[hb][:, :, tt * 128:(tt + 1) * 128], "hnd%d" % hb)
                psb = X.ps[7][:].bitcast(BF16)
                for tt in range(4):
                    ti = 4 * tg + tt
                    pt_ = ptile[ti % 2]
                    s.dma("sp", pt_[:], X.p[L, ti * 128:(ti + 1) * 128, :], writes=["pt%d" % (ti % 2)])
                    s.op("act", lambda e: e.activation(out=pb[:], in_=pt_[:], func=AF.Copy), reads=["pt%d" % (ti % 2)], writes=["pb"])
                    for k in range(2):
                        s.op("pe", lambda e: e.transpose(out=psb[:, k * 128:(k + 1) * 128], in_=pb[:, k * 128:(k + 1) * 128],
                                                         identity=X.ident[:]), reads=["pb", "ident"], writes=["ps7"])
                    s.op("act", lambda e: e.activation(out=pT[hb][:, :, tt * 128:(tt + 1) * 128],
                                                       in_=psb[:, 0:256].rearrange("p (k t) -> p k t", k=2), func=AF.Copy),
                         reads=["ps7"], writes=["pT%d" % hb])

            def stage2(tg):
                hb = tg % 2
                for tt in range(4):
                    ti = 4 * tg + tt
                    hres = "h1_%d_%d" % (hb, tt)
                    ares = "dacc%d_%d" % (hb, tt)
                    for half in range(2):
                        hs = slice(half * 512, (half + 1) * 512)
                        for k in range(2):
                            s.op("pe", lambda e: e.matmul(X.ps[6][:], lhsT=pT[hb][:, k, tt * 128:(tt + 1) * 128], rhs=pproj[:, k, hs],
                                                          start=(k == 0), stop=(k == 1)), reads=["pT%d" % hb, "pproj"], writes=["ps6"])
                        pgb = 4 + half
                        for k in range(8):
                            s.op("pe", lambda e: e.matmul(X.ps[pgb][:], lhsT=hnd[hb][:, k, tt * 128:(tt + 1) * 128], rhs=pgate[:, k, hs],
                                                          start=(k == 0), stop=(k == 7)), reads=["hnd%d" % hb, "pgate"], writes=["ps%d" % pgb])
                        s.op("act", lambda e: e.activation(out=sig[half][:], in_=X.ps[pgb][:], func=AF.Sigmoid),
                             reads=["ps%d" % pgb], writes=["sig%d" % half])
                        s.op("dve", lambda e: e.tensor_tensor(out=acc[hb][:, tt, hs], in0=X.ps[6][:], in1=sig[half][:], op=ALU.mult),
                             reads=["ps6", "sig%d" % half], writes=[ares])
                    s.op("pool", lambda e: e.tensor_tensor(out=h1[hb][:, tt, :], in0=h1[hb][:, tt, :], in1=acc[hb][:, tt, :], op=ALU.add),
                         reads=[ares], writes=[hres])
                    if last:
                        k4 = 4 + tt % 4 if len(X.ss) > 4 else tt
                        ln_a(s, X, h1[hb][:, tt, :], hres, k4)
                        s.op("act", lambda e: e.activation(out=acc[hb][:, tt, :], in_=h1[hb][:, tt, :], func=AF.Copy, scale=X.ss[k4][:, 2:3]),
                             reads=[hres, "ss%d" % k4], writes=[ares])
                        s.op("pool", lambda e: e.tensor_tensor(out=acc[hb][:, tt, :], in0=acc[hb][:, tt, :], in1=gfin[:], op=ALU.mult),
                             reads=["gfin"], writes=[ares])
                        s.dma("pool", h_out[ti * 128:(ti + 1) * 128, :], acc[hb][:, tt, :], reads=[ares], writes=["HOUT"])
                    else:
                        s.dma("pool", h_out[ti * 128:(ti + 1) * 128, :], h1[hb][:, tt, :], reads=[hres], writes=["HB"])

            stage1(0)
            for tg in range(NT // 4):
                if tg + 1 < NT // 4:
                    stage1(tg + 1)
                stage2(tg)
            s.barrier()
        s.barrier()
```

```python
import math
import os
from contextlib import ExitStack

import numpy as np
import concourse.bass as bass
import concourse.mybir as mybir
from concourse.bass_utils import run_bass_kernel_spmd

F32 = mybir.dt.float32
BF16 = mybir.dt.bfloat16
AF = mybir.ActivationFunctionType
ALU = mybir.AluOpType
AX = mybir.AxisListType

D = 1024
SEQ = 8192
NT = SEQ // 128
DEPTH = 2
HD = 64
IN_COLS = 3352
D_FF = 3584
NE = 8
EPS = 1e-6
NEG = -30000.0
NGRP = 40
NSLOT = NGRP * 512
I32 = mybir.dt.int32

C_QN, C_KC, C_VC, C_KS, C_VS, C_KW, C_VW, C_GL, C_QR, C_KR, C_VR, C_GR = (
    0, 512, 640, 768, 896, 1024, 1152, 1280, 1304, 1816, 2328, 2840)


class S:
    def __init__(self, nc, es, n_dma_sems=24):
        self.nc = nc
        self.eng = {"pe": nc.tensor, "dve": nc.vector, "act": nc.scalar,
                    "pool": nc.gpsimd, "sp": nc.sync}
        self.sem = {k: es.enter_context(nc.semaphore("c_" + k)) for k in self.eng}
        self.cnt = {k: 0 for k in self.eng}
        self.seen = {k: {} for k in self.eng}
        self.drained = {k: 0 for k in self.eng}
        self.dsem = [es.enter_context(nc.semaphore("d%d" % i)) for i in range(n_dma_sems)]
        self.dval = [0] * n_dma_sems
        self.dnext = 0
        self.lastw = {}
        self.readers = {}
        self.nops = 0

    def _wait(self, eng, tok):
        if tok is None:
            return
        if tok[0] == "c":
            _, e2, c = tok
            if e2 == eng:
                if eng != "pe" and c > self.drained[eng]:
                    self.eng[eng].drain()
                    self.drained[eng] = self.cnt[eng]
                return
            key = e2
            sem = self.sem[e2]
        else:
            _, si, c = tok
            key = ("d", si)
            sem = self.dsem[si]
        if self.seen[eng].get(key, 0) >= c:
            return
        self.eng[eng].wait_ge(sem, c)
        self.seen[eng][key] = c

    def _deps(self, eng, reads, writes):
        for r in reads:
            self._wait(eng, self.lastw.get(r))
        for w in writes:
            self._wait(eng, self.lastw.get(w))
            for t in self.readers.get(w, {}).values():
                self._wait(eng, t)

    def _commit(self, tok, reads, writes):
        for r in reads:
            self.readers.setdefault(r, {})[tok[1] if tok[0] == "c" else ("d", tok[1])] = tok
        for w in writes:
            self.lastw[w] = tok
            self.readers[w] = {}

    def op(self, eng, fn, reads=(), writes=()):
        self._deps(eng, reads, writes)
        ins = fn(self.eng[eng])
        self.cnt[eng] += 1
        ins.then_inc(self.sem[eng], 1)
        self._commit(("c", eng, self.cnt[eng]), reads, writes)
        self.nops += 1

    def dma(self, q, out, in_, reads=(), writes=(), **kw):
        self._deps(q, reads, writes)
        si = self.dnext
        self.dnext = (self.dnext + 1) % len(self.dsem)
        self._wait(q, ("d", si, self.dval[si]))
        ins = self.eng[q].dma_start(out=out, in_=in_, **kw)
        self.dval[si] += 16
        ins.then_inc(self.dsem[si], 16)
        self._commit(("d", si, self.dval[si]), reads, writes)
        self.nops += 1

    def idma(self, out, out_off, in_, in_off, bound, reads=(), writes=()):
        q = "pool"
        self._deps(q, reads, writes)
        si = self.dnext
        self.dnext = (self.dnext + 1) % len(self.dsem)
        self._wait(q, ("d", si, self.dval[si]))
        oo = None if out_off is None else bass.IndirectOffsetOnAxis(ap=out_off, axis=0)
        io = None if in_off is None else bass.IndirectOffsetOnAxis(ap=in_off, axis=0)
        ins = self.nc.gpsimd.indirect_dma_start(out=out, out_offset=oo, in_=in_, in_offset=io)
        self.dval[si] += 16
        ins.then_inc(self.dsem[si], 16)
        self._commit(("d", si, self.dval[si]), reads, writes)
        self.nops += 1

    def barrier(self):
        for e in self.eng:
            for e2 in self.eng:
                if e2 != e and self.cnt[e2] > 0:
                    self._wait(e, ("c", e2, self.cnt[e2]))
            for si in range(len(self.dsem)):
                if self.dval[si] > 0:
                    self._wait(e, ("d", si, self.dval[si]))
        self.lastw = {}
        self.readers = {}

    def drain(self, eng="sp"):
        for r, t in list(self.lastw.items()):
            self._wait(eng, t)
        for r, d in list(self.readers.items()):
            for t in d.values():
                self._wait(eng, t)


def bcast_mid(ap2, n):
    return ap2.unsqueeze(2).to_broadcast([ap2.shape[0], ap2.shape[1], n])


def bcast_rep(ap2, n):
    return ap2.unsqueeze(1).to_broadcast([ap2.shape[0], n, ap2.shape[1]])


def make_consts():
    c = {}
    c["c_ident"] = np.eye(128, dtype=np.float32)
    t = np.arange(SEQ)
    slopes = 2.0 ** (-(np.arange(8) + 1.0))
    qa = np.zeros((8, 3, SEQ), np.float32)
    for h in range(8):
        qa[h, 0] = 128.0 * slopes[h]
        qa[h, 1] = slopes[h]
        qa[h, 2] = -slopes[h] * 128.0 * (t // 128 + 1)
    c["c_qaug"] = qa
    ka = np.zeros((3, SEQ), np.float32)
    ka[0] = t // 128
    ka[1] = t % 128
    ka[2] = 1.0
    c["c_kaug"] = ka
    n = np.arange(512)
    pos = 16 * n + 31
    kc = np.zeros((3, 512), np.float32)
    kc[0] = pos // 128
    kc[1] = pos % 128
    kc[2] = 1.0
    c["c_kcaug"] = kc
    hh = np.arange(8)
    lg = np.log1p(-(2.0 ** (-5.0 - hh)))
    nn = np.arange(128)
    xiT = np.zeros((4, 128, 128), np.float32)
    kdT = np.zeros((4, 128, 128), np.float32)
    gC = np.zeros((128, 4), np.float32)
    for hp in range(4):
        for half in range(2):
            h = 2 * hp + half
            xiT[hp, half * 64:(half + 1) * 64, :] = np.exp((nn + 1.0) * lg[h])[None, :]
            kdT[hp, half * 64:(half + 1) * 64, :] = 0.125 * np.exp(-(nn + 1.0) * lg[h])[None, :]
            gC[half * 64:(half + 1) * 64, hp] = np.exp(128.0 * lg[h])
    c["c_xiT"] = xiT
    c["c_kdT"] = kdT
    c["c_gC"] = gC
    c["c_gC8"] = np.tile(np.exp(128.0 * lg)[None, :], (64, 1)).astype(np.float32)
    c["c_ktab"] = (0.125 * np.exp(-(nn[:, None] + 1.0) * lg[None, :])).astype(np.float32)
    dm = (nn[None, :] >= nn[:, None]).astype(np.float32)
    c["c_rmask"] = np.tile(dm, (1, 4))
    jj = nn[:, None]
    ii = nn[None, :]
    c["c_mcausal"] = np.tile(np.where(jj > ii, NEG, 0.0).astype(np.float32), (1, 4))
    c["c_mwin4"] = np.tile(np.where(jj > ii, 0.0, NEG).astype(np.float32), (1, 4))
    pats = []
    pidx = {}
    cmp_plan = []
    for qi in range(NT):
        row = []
        for cc in range(4):
            tq = 128 * qi + ii
            nk = 128 * cc + jj
            valid = (tq - (16 * nk + 31) >= 0) & (nk <= 510)
            if valid.all():
                row.append(-1)
            elif not valid.any():
                row.append(-2)
            else:
                key = valid.tobytes()
                if key not in pidx:
                    pidx[key] = len(pats)
                    pats.append(np.tile(np.where(valid, 0.0, NEG).astype(np.float32), (1, 4)))
                row.append(pidx[key])
        cmp_plan.append(row)
    c["c_mcmp"] = np.stack(pats, 0)
    E = np.zeros((128, NT, 128), np.float32)
    for kt in range(NT):
        E[2 * kt, kt, 0:64] = 1.0
        E[2 * kt + 1, kt, 64:128] = 1.0
    c["c_E"] = E
    ncmp = np.arange(512)[:, None]
    msel = np.arange(128)[None, :]
    ov = ((np.minimum(16 * ncmp + 32, 64 * msel + 64) > np.maximum(16 * ncmp, 64 * msel))
          & (ncmp <= 510)).astype(np.float32)
    c["c_ov"] = ov
    AB = np.zeros((NT, 128, 256), np.float32)
    for qi in range(NT):
        tq = 128 * qi + nn[:, None]
        back = tq // 64 - msel
        forced = (msel == 0) | ((back >= 0) & (back < 2))
        A = ((back >= 0) & (~forced)).astype(np.float32)
        B = np.where(forced, 1.0e9 + 1024.0 * msel, np.where(back >= 0, 0.0, -1.0)).astype(np.float32)
        AB[qi, :, 0:128] = A
        AB[qi, :, 128:256] = B
    c["c_AB"] = AB
    c["c_gi512"] = np.tile((512.0 * np.arange(NGRP))[None, :], (128, 1)).astype(np.float32)
    c["c_tokid"] = (np.arange(NT)[None, :] * 128 + np.arange(128)[:, None]).astype(np.float32)
    c["c_U"] = (nn[:, None] < nn[None, :]).astype(np.float32)
    c["c_blkp"] = (np.arange(7)[None, :] * 128 + np.arange(128)[:, None]).astype(np.float32)
    return c, cmp_plan


_CONSTS = None


def get_consts():
    global _CONSTS
    if _CONSTS is None:
        _CONSTS = make_consts()
    return _CONSTS


class Ctx:
    def __getattr__(self, k):
        if k in WEIGHT_SPECS:
            ap = self.nc.dram_tensor(k, WEIGHT_SPECS[k], F32, kind="ExternalInput").ap()
            self.__dict__[k] = ap
            self.used.append(k)
            return ap
        raise AttributeError(k)


def ln_a(s, X, src, src_res, k):
    junk, ss, xs = X.junk, X.ss[k], X.xs[k]
    s.op("act", lambda e: e.activation(out=junk[:], in_=src, func=AF.Square, accum_out=ss[:, 0:1]),
         reads=[src_res], writes=["junk", "ss%d" % k])
    s.op("dve", lambda e: e.tensor_scalar(out=ss[:, 1:2], in0=ss[:, 0:1], scalar1=1.0 / D, scalar2=EPS,
                                          op0=ALU.mult, op1=ALU.add), reads=["ss%d" % k], writes=["ss%d" % k])
    s.op("act", lambda e: e.activation(out=ss[:, 1:2], in_=ss[:, 1:2], func=AF.Sqrt),
         reads=["ss%d" % k], writes=["ss%d" % k])
    s.op("dve", lambda e: e.reciprocal(out=ss[:, 2:3], in_=ss[:, 1:2]), reads=["ss%d" % k], writes=["ss%d" % k])
    s.op("act", lambda e: e.activation(out=xs[:], in_=src, func=AF.Copy, scale=ss[:, 2:3]),
         reads=[src_res, "ss%d" % k], writes=["xs%d" % k])


def ln_b(s, X, k, gT, gT_res, dst, dst_res, src=None, src_res=None):
    xs = X.xs[k] if src is None else src
    xres = ("xs%d" % k) if src is None else src_res
    psb = X.ps[7][:].bitcast(BF16)
    for kc in range(8):
        s.op("pe", lambda e: e.transpose(out=psb[:, kc * 128:(kc + 1) * 128], in_=xs[:, kc * 128:(kc + 1) * 128],
                                         identity=X.ident[:]), reads=[xres, "ident"], writes=["ps7"])
    s.op("dve", lambda e: e.tensor_tensor(out=dst, in0=psb.rearrange("p (k t) -> p k t", k=8),
                                          in1=bcast_mid(gT, 128), op=ALU.mult),
         reads=["ps7", gT_res], writes=[dst_res])


def phase_proj(s, X, L, h_in):
    nc = X.nc
    with ExitStack() as es:
        sb = lambda name, shape, dt: es.enter_context(nc.sbuf_tensor("%s_L%d" % (name, L), shape, dt))
        wsb = sb("p1_w", [128, 8, IN_COLS], BF16)
        gT = sb("p1_gT", [128, 8], F32)
        ht = [sb("p1_ht%d" % i, [128, D], F32) for i in range(4)]
        hnT = [sb("p1_hnT%d" % i, [128, 8, 512], BF16) for i in range(2)]
        fm = [sb("p1_fm%d" % i, [128, 16, 512], BF16) for i in range(2)]
        vsx = [sb("p1_vsx%d" % i, [128, 4, 2, 65], BF16) for i in range(2)]
        vwx = [sb("p1_vwx%d" % i, [128, 4, 2, 65], BF16) for i in range(2)]
        gls = [sb("p1_gls%d" % i, [128, 4, 24], F32) for i in range(2)]
        krm = [sb("p1_krm%d" % i, [128, 4, 512], BF16) for i in range(2)]
        vrm = [sb("p1_vrm%d" % i, [128, 4, 512], BF16) for i in range(2)]
        grm = [sb("p1_grm%d" % i, [128, 4, 512], BF16) for i in range(2)]
        sg = sb("p1_sg", [128, 512], F32)
        xiT = sb("p1_xiT", [128, 4, 128], F32)
        kdT = sb("p1_kdT", [128, 4, 128], F32)
        ktab = sb("p1_ktab", [128, 8], F32)
        s.dma("sp", xiT[:], X.c["c_xiT"].rearrange("j p t -> p j t"), writes=["xiT"])
        s.dma("sp", kdT[:], X.c["c_kdT"].rearrange("j p t -> p j t"), writes=["kdT"])
        s.dma("sp", ktab[:], X.c["c_ktab"], writes=["ktab"])
        s.dma("sp", gT[:], X.g_mix[L].rearrange("(k p) -> p k", p=128), writes=["p1gT"],
              allow_slow_non_contiguous=True)
        for kc in range(8):
            s.dma("pool", wsb[:, kc, :], X.w_in[L, kc * 128:(kc + 1) * 128, :], writes=["p1w"])
        for i in range(2):
            s.op("pool", lambda e: e.memset(vsx[i][:, :, :, 64:65], 1.0), writes=["vsx%d" % i])
            s.op("pool", lambda e: e.memset(vwx[i][:, :, :, 64:65], 1.0), writes=["vwx%d" % i])
        fm_cols = [C_QN, C_QN + 128, C_QN + 256, C_QN + 384, C_KC, C_VC, C_KS, C_KW,
                   C_QR, C_QR + 128, C_QR + 256, C_QR + 384, C_KR, C_KR + 128, C_KR + 256, C_KR + 384]
        def emit_a(tg):
            for tt in range(4):
                ti = 4 * tg + tt
                s.dma("sp", ht[tt][:], h_in[ti * 128:(ti + 1) * 128, :], writes=["p1ht%d" % tt])
                ln_a(s, X, ht[tt][:], "p1ht%d" % tt, tt)

        def emit_b(tg):
            for tt in range(4):
                ln_b(s, X, tt, gT[:], "p1gT", hnT[tg % 2][:, :, tt * 128:(tt + 1) * 128], "hnT%d" % (tg % 2))

        emit_a(0)
        emit_b(0)
        for tg in range(NT // 4):
            b = tg % 2
            toks = slice(tg * 512, (tg + 1) * 512)
            if tg + 1 < NT // 4:
                emit_a(tg + 1)
            for ci, c0 in enumerate(fm_cols):
                pb = ci % 2
                ps = X.ps[pb]
                for kc in range(8):
                    s.op("pe", lambda e: e.matmul(ps[:], lhsT=wsb[:, kc, c0:c0 + 128], rhs=hnT[b][:, kc, :],
                                                  start=(kc == 0), stop=(kc == 7)),
                         reads=["p1w", "hnT%d" % b], writes=["ps%d" % pb])
                dst = fm[b][:, ci, :]
                if ci < 4:
                    s.op("act", lambda e: e.activation(out=dst, in_=ps[:], func=AF.Copy, scale=0.125),
                         reads=["ps%d" % pb], writes=["fm%d" % b])
                elif ci < 8:
                    eng = "act" if ci % 2 == 0 else "dve"
                    if eng == "act":
                        s.op("act", lambda e: e.activation(out=dst, in_=ps[:], func=AF.Copy),
                             reads=["ps%d" % pb], writes=["fm%d" % b])
                    else:
                        s.op("dve", lambda e: e.tensor_copy(out=dst, in_=ps[:]),
                             reads=["ps%d" % pb], writes=["fm%d" % b])
                else:
                    tab = xiT if ci < 12 else kdT
                    j = (ci - 8) % 4
                    s.op("dve", lambda e: e.tensor_tensor(
                        out=dst.rearrange("p (a t) -> p a t", a=4), in0=ps[:].rearrange("p (a t) -> p a t", a=4),
                        in1=bcast_rep(tab[:, j, :], 4), op=ALU.mult),
                        reads=["ps%d" % pb, "xiT", "kdT"], writes=["fm%d" % b])
            for tt in range(4):
                lhs = lambda kc: hnT[b][:, kc, tt * 128:(tt + 1) * 128]
                groups = [(2, C_VS, 408), (3, C_KR, 512), (4, C_VR, 512), (5, C_GR, 512)]
                for (pi, c0, n) in groups:
                    for kc in range(8):
                        s.op("pe", lambda e: e.matmul(X.ps[pi][:, 0:n], lhsT=lhs(kc), rhs=wsb[:, kc, c0:c0 + n],
                                                      start=(kc == 0), stop=(kc == 7)),
                             reads=["p1w", "hnT%d" % b], writes=["ps%d" % pi])
                pA = X.ps[2]
                s.op("dve", lambda e: e.tensor_copy(out=vsx[b][:, tt, :, 0:64],
                                                    in_=pA[:, 0:128].rearrange("p (g d) -> p g d", g=2)),
                     reads=["ps2"], writes=["vsx%d" % b])
                s.op("dve", lambda e: e.tensor_copy(out=vwx[b][:, tt, :, 0:64],
                                                    in_=pA[:, 256:384].rearrange("p (g d) -> p g d", g=2)),
                     reads=["ps2"], writes=["vwx%d" % b])
                s.op("act", lambda e: e.activation(out=gls[b][:, tt, :], in_=pA[:, 384:408], func=AF.Sigmoid),
                     reads=["ps2"], writes=["gls%d" % b])
                s.op("dve", lambda e: e.tensor_tensor(
                    out=krm[b][:, tt, :].rearrange("p (h d) -> p h d", h=8),
                    in0=X.ps[3][:].rearrange("p (h d) -> p h d", h=8),
                    in1=bcast_mid(ktab[:], 64), op=ALU.mult),
                    reads=["ps3", "ktab"], writes=["krm%d" % b])
                s.op("act", lambda e: e.activation(out=vrm[b][:, tt, :], in_=X.ps[4][:], func=AF.Copy),
                     reads=["ps4"], writes=["vrm%d" % b])
                s.op("act", lambda e: e.activation(out=sg[:], in_=X.ps[5][:], func=AF.Sigmoid),
                     reads=["ps5"], writes=["p1sg"])
                s.op("dve", lambda e: e.tensor_tensor(out=grm[b][:, tt, :], in0=X.ps[5][:], in1=sg[:], op=ALU.mult),
                     reads=["ps5", "p1sg"], writes=["grm%d" % b])
            if tg + 1 < NT // 4:
                emit_b(tg + 1)
            s.dma("pool", X.QTP[:, :, toks].rearrange("j p t -> p j t"), fm[b][:, 0:4, :], reads=["fm%d" % b], writes=["QTP"])
            s.dma("pool", X.KCP[:, toks], fm[b][:, 4, :], reads=["fm%d" % b], writes=["KCP"])
            s.dma("pool", X.VCP[:, toks], fm[b][:, 5, :], reads=["fm%d" % b], writes=["VCP"])
            s.dma("pool", X.KSP[:, toks], fm[b][:, 6, :], reads=["fm%d" % b], writes=["KSP"])
            s.dma("pool", X.KWP[:, toks], fm[b][:, 7, :], reads=["fm%d" % b], writes=["KWP"])
            s.dma("pool", X.QRT[:, :, toks].rearrange("j p t -> p j t"), fm[b][:, 8:12, :], reads=["fm%d" % b], writes=["QRT"])
            s.dma("pool", X.KRT[:, :, toks].rearrange("j p t -> p j t"), fm[b][:, 12:16, :], reads=["fm%d" % b], writes=["KRT"])
            s.dma("pool", X.VSX[toks].rearrange("(tt p) g c -> p tt g c", p=128), vsx[b][:], reads=["vsx%d" % b], writes=["VSX"])
            s.dma("pool", X.VWX[toks].rearrange("(tt p) g c -> p tt g c", p=128), vwx[b][:], reads=["vwx%d" % b], writes=["VWX"])
            s.dma("pool", X.GLS[toks].rearrange("(tt p) c -> p tt c", p=128), gls[b][:], reads=["gls%d" % b], writes=["GLS"])
            s.dma("pool", X.KRM[toks].rearrange("(tt p) c -> p tt c", p=128), krm[b][:], reads=["krm%d" % b], writes=["KRM"])
            s.dma("pool", X.VRM[toks].rearrange("(tt p) c -> p tt c", p=128), vrm[b][:], reads=["vrm%d" % b], writes=["VRM"])
            s.dma("pool", X.GRM[toks].rearrange("(tt p) c -> p tt c", p=128), grm[b][:], reads=["grm%d" % b], writes=["GRM"])
        s.barrier()


WEIGHT_SPECS = {
    "w_in": [DEPTH, D, IN_COLS], "w_out": [DEPTH, D, D], "g_mix": [DEPTH, D], "g_ffn": [DEPTH, D],
    "g_ple": [DEPTH, D], "g_final": [D], "cmp_pos": [DEPTH, 2, 32, 64], "cmp_w1": [DEPTH, 2, 32, 64, 256],
    "cmp_w2": [DEPTH, 2, 256, 64], "ret_gn": [DEPTH, 512], "ffn_gate": [1, D, D_FF], "ffn_up": [1, D, D_FF],
    "ffn_down": [1, D_FF, D], "moe_router": [1, D, NE], "moe_gate": [1, NE, D, D_FF], "moe_up": [1, NE, D, D_FF],
    "moe_down": [1, NE, D_FF, D], "ple_proj": [DEPTH, 256, D], "ple_gate": [DEPTH, D, D],
}

SCRATCH_SPECS = {
    "QTP": ([4, 128, SEQ], BF16), "QAUG": ([8, 3, SEQ], BF16), "KAUG": ([3, SEQ], BF16), "KCAUG": ([3, 512], BF16),
    "KSP": ([128, SEQ], BF16), "KWP": ([128, SEQ], BF16), "KCP": ([128, SEQ], BF16), "VCP": ([128, SEQ], BF16),
    "VSX": ([SEQ, 2, 65], BF16), "VWX": ([SEQ, 2, 65], BF16), "GLS": ([SEQ, 24], F32),
    "QRT": ([4, 128, SEQ], BF16), "KRT": ([4, 128, SEQ], BF16),
    "KRM": ([SEQ, 512], BF16), "VRM": ([SEQ, 512], BF16), "GRM": ([SEQ, 512], BF16),
    "AT": ([D, SEQ], BF16), "HB": ([SEQ, D], F32),
    "FG16": ([1, D, D_FF], BF16), "FU16": ([1, D, D_FF], BF16), "FD16": ([1, D_FF, D], BF16),
    "WGB": ([NE * 7 * 128, 4096], BF16), "WUB": ([NE * 7 * 128, 4096], BF16), "WDB": ([NE * 7 * 128, 4096], BF16),
    "H1": ([SEQ, D], F32), "XS2": ([SEQ, D], BF16), "SLOT": ([NSLOT, 2], I32), "YS": ([NSLOT, D], F32),
    "DBG_KCMP": ([67, 2, 512], BF16), "DBG_VCX": ([128, 2, 4, 193], BF16),
}


def build(stop_after=None, debug=(), nsa_tiles=NT):
    nc = bass.Bass("TRN2", target_bir_lowering=False)
    X = Ctx()
    X.nc = nc
    consts, cmp_plan = get_consts()
    X.cmp_plan = cmp_plan
    X.x = nc.dram_tensor("x", [SEQ, D], F32, kind="ExternalInput").ap()
    X.p = nc.dram_tensor("p", [DEPTH, SEQ, 256], F32, kind="ExternalInput").ap()
    X.used = []
    X.dbg_cmp = "DBG_KCMP" in debug
    X.nsa_tiles = nsa_tiles
    X.routed = True
    X.conv_layers = [0] if (stop_after is not None and stop_after[0] == 0) else [0, 1]
    X.ret_chunks = int(os.environ.get('RET_CHUNKS', NT))
    X.c = {k: nc.dram_tensor(k, list(v.shape), F32, kind="ExternalInput").ap() for k, v in consts.items()}
    X.out = nc.dram_tensor("out", [SEQ, D], F32, kind="ExternalOutput").ap()
    for k, (shp, dt) in SCRATCH_SPECS.items():
        kind = "ExternalOutput" if k in debug else "Internal"
        setattr(X, k, nc.dram_tensor(k, shp, dt, kind=kind).ap())
    with ExitStack() as es:
        s = S(nc, es)
        X.ps = [es.enter_context(nc.psum_tensor("ps%d" % i, [128, 512], F32)) for i in range(8)]
        X.ident = es.enter_context(nc.sbuf_tensor("ident", [128, 128], BF16))
        X.junk = es.enter_context(nc.sbuf_tensor("junk", [128, D], F32))
        X.ss = [es.enter_context(nc.sbuf_tensor("ss%d" % i, [128, 4], F32)) for i in range(8)]
        X.xs = [es.enter_context(nc.sbuf_tensor("xs%d" % i, [128, D], BF16)) for i in range(8)]
        s.dma("pool", X.ident[:], X.c["c_ident"], writes=["ident"])
        s.dma("pool", X.QAUG, X.c["c_qaug"], writes=["QAUG"])
        s.dma("pool", X.KAUG, X.c["c_kaug"], writes=["KAUG"])
        s.dma("pool", X.KCAUG, X.c["c_kcaug"], writes=["KCAUG"])
        done = False
        for L in range(DEPTH):
            h_in = X.x if L == 0 else X.HB
            for name, fn in PHASES:
                fn(s, X, L, h_in)

                if stop_after == (L, name):
                    done = True
                    break
            if done:
                break
        s.barrier()
        s.drain("sp")
    X.nops = s.nops
    return nc, X


PHASES = [("proj", phase_proj)]

_BUILT = {}


def run(inputs, stop_after=None, debug=(), cores=8, trace=False, nsa_tiles=NT):
    key = (stop_after, tuple(debug), nsa_tiles)
    if key not in _BUILT:
        _BUILT[key] = build(stop_after, debug, nsa_tiles)
    nc, X = _BUILT[key]
    consts, _ = get_consts()
    in_maps = []
    for b in range(cores):
        m = {"x": np.ascontiguousarray(inputs["x"][b]), "p": np.ascontiguousarray(inputs["p"][:, b])}
        for k in X.used:
            m[k] = np.ascontiguousarray(inputs[k])
        m.update(consts)
        in_maps.append(m)
    return run_bass_kernel_spmd(nc, in_maps, core_ids=list(range(cores)), trace=trace)


def kernel(**inputs):
    inputs = {k: np.asarray(v) for k, v in inputs.items()}
    res = run(inputs)
    return np.stack([r["out"] for r in res.results], 0).astype(np.float32)


def phase_nsa(s, X, L, h_in):
    nc = X.nc
    with ExitStack() as es:
        sb = lambda name, shape, dt: es.enter_context(nc.sbuf_tensor("%s_L%d" % (name, L), shape, dt))
        KCMP = sb("n_kcmp", [67, 2, 512], BF16)
        VCX = sb("n_vcx", [128, 2, 4, 193], BF16)
        s.op("pool", lambda e: e.memset(KCMP[:], 0.0), writes=["KCMP"])
        s.op("pool", lambda e: e.memset(VCX[:], 0.0), writes=["VCX"])
        s.op("pool", lambda e: e.memset(VCX[:, :, :, 64:65], 1.0), writes=["VCX"])
        for g in range(2):
            s.dma("pool", VCX[:, g, :, 65:193], X.c["c_ov"].rearrange("(ct p) m -> p ct m", p=128), writes=["VCX"])
            s.dma("sp", KCMP[64:67, g, :], X.KCAUG, reads=["KCAUG"], writes=["KCMP"])
        with ExitStack() as es2:
            sb2 = lambda name, shape, dt: es2.enter_context(nc.sbuf_tensor("%s_L%d" % (name, L), shape, dt))
            kcT = [sb2("c_kcT%d" % i, [64, SEQ], BF16) for i in range(2)]
            w1 = sb2("c_w1", [64, 32, 256], BF16)
            posT = sb2("c_posT", [64, 32], BF16)
            w2 = sb2("c_w2", [128, 2, 64], BF16)
            hb = sb2("c_hb", [128, 2], F32)
            u = sb2("c_u", [128, 2, 512], F32)
            t1 = sb2("c_t1", [128, 2, 512], F32)
            sg = sb2("c_sg", [128, 2, 512], F32)
            gel = sb2("c_gel", [128, 2, 512], BF16)
            it = 0
            for kv in range(2):
                s.dma("pool", w1[:], X.cmp_w1[L, kv].rearrange("l d h -> d l h"), writes=["c_w1"])
                s.dma("pool", posT[:], X.cmp_pos[L, kv].rearrange("l d -> d l"), writes=["c_posT"],
                      allow_slow_non_contiguous=True)
                s.dma("pool", w2[:], X.cmp_w2[L, kv].rearrange("(c p) d -> p c d", p=128), writes=["c_w2"])
                src = X.KCP if kv == 0 else X.VCP
                for g in range(2):
                    kt_ = kcT[it % 2]
                    kres = "c_kcT%d" % (it % 2)
                    it += 1
                    s.dma("sp", kt_[:], src[g * 64:(g + 1) * 64, :], reads=["KCP", "VCP"], writes=[kres])
                    kview = kt_[:].rearrange("d (n c) -> d n c", c=16)
                    for hc in range(2):
                        ps = X.ps[hc]
                        for l in range(32):
                            a, c_ = l // 16, l % 16
                            s.op("pe", lambda e: e.matmul(ps[:, 0:511], lhsT=w1[:, l, hc * 128:(hc + 1) * 128],
                                                          rhs=kview[:, a:a + 511, c_], start=(l == 0), stop=(l == 31)),
                                 reads=["c_w1", kres], writes=["ps%d" % hc])
                        for l in range(32):
                            s.op("pe", lambda e: e.matmul(X.ps[2][:, hc:hc + 1], lhsT=w1[:, l, hc * 128:(hc + 1) * 128],
                                                          rhs=posT[:, l:l + 1], start=(l == 0), stop=(l == 31)),
                                 reads=["c_w1", "c_posT"], writes=["ps2"])
                        s.op("dve", lambda e: e.tensor_copy(out=hb[:, hc:hc + 1], in_=X.ps[2][:, hc:hc + 1]),
                             reads=["ps2"], writes=["c_hb"])
                        s.op("act", lambda e: e.activation(out=u[:, hc, 0:511], in_=ps[:, 0:511], func=AF.Identity,
                                                           bias=hb[:, hc:hc + 1]),
                             reads=["ps%d" % hc, "c_hb"], writes=["c_u"])
                    uu = u[:, :, 0:511]
                    s.op("pool", lambda e: e.tensor_tensor(out=t1[:, :, 0:511], in0=uu, in1=uu, op=ALU.mult),
                         reads=["c_u"], writes=["c_t1"])
                    s.op("dve", lambda e: e.tensor_scalar(out=t1[:, :, 0:511], in0=t1[:, :, 0:511], scalar1=0.044715,
                                                          scalar2=1.0, op0=ALU.mult, op1=ALU.add),
                         reads=["c_t1"], writes=["c_t1"])
                    s.op("dve", lambda e: e.tensor_tensor(out=t1[:, :, 0:511], in0=t1[:, :, 0:511], in1=uu, op=ALU.mult),
                         reads=["c_t1", "c_u"], writes=["c_t1"])
                    s.op("act", lambda e: e.activation(out=sg[:, :, 0:511], in_=t1[:, :, 0:511], func=AF.Sigmoid,
                                                       scale=1.5957691216057308),
                         reads=["c_t1"], writes=["c_sg"])
                    s.op("dve", lambda e: e.tensor_tensor(out=gel[:, :, 0:511], in0=sg[:, :, 0:511], in1=uu, op=ALU.mult),
                         reads=["c_sg", "c_u"], writes=["c_gel"])
                    if kv == 0:
                        for hc in range(2):
                            s.op("pe", lambda e: e.matmul(X.ps[3][0:64, 0:511], lhsT=w2[:, hc, :], rhs=gel[:, hc, 0:511],
                                                          start=(hc == 0), stop=(hc == 1)),
                                 reads=["c_w2", "c_gel"], writes=["ps3"])
                        s.op("act", lambda e: e.activation(out=KCMP[0:64, g, 0:511], in_=X.ps[3][0:64, 0:511], func=AF.Copy),
                             reads=["ps3"], writes=["KCMP"])
                    else:
                        for ct in range(4):
                            nn = 128 if ct < 3 else 127
                            for hc in range(2):
                                s.op("pe", lambda e: e.matmul(X.ps[3][0:nn, ct * 64:(ct + 1) * 64],
                                                              lhsT=gel[:, hc, ct * 128:ct * 128 + nn], rhs=w2[:, hc, :],
                                                              start=(hc == 0), stop=(hc == 1)),
                                     reads=["c_w2", "c_gel"], writes=["ps3"])
                        for ct in range(4):
                            nn = 128 if ct < 3 else 127
                            s.op("act", lambda e: e.activation(out=VCX[0:nn, g, ct, 0:64],
                                                               in_=X.ps[3][0:nn, ct * 64:(ct + 1) * 64], func=AF.Copy),
                                 reads=["ps3"], writes=["VCX"])
            s.barrier()
        if X.dbg_cmp:
            s.dma("sp", X.DBG_KCMP, KCMP[:], reads=["KCMP"])
            s.dma("sp", X.DBG_VCX, VCX[:], reads=["VCX"])
        KS = sb("n_ks", [67, 2, SEQ], BF16)
        KW = sb("n_kw", [67, 2, SEQ], BF16)
        VS = sb("n_vs", [128, NT, 2, 65], BF16)
        VW = sb("n_vw", [128, NT, 2, 65], BF16)
        E = sb("n_E", [128, NT, 128], BF16)
        mca = sb("n_mca", [128, 512], BF16)
        mw4 = sb("n_mw4", [128, 512], BF16)
        npat = X.c["c_mcmp"].shape[0]
        mcmp = sb("n_mcmp", [128, npat, 512], BF16)
        qt = [sb("n_qt%d" % i, [67, 1024], BF16) for i in range(2)]
        AB = [sb("n_AB%d" % i, [128, 256], F32) for i in range(2)]
        glt = [sb("n_gl%d" % i, [128, 24], F32) for i in range(2)]
        PT = [sb("n_PT%d" % i, [128, 512], BF16) for i in range(4)]
        negT = [sb("n_negT%d" % i, [128, 512], BF16) for i in range(2)]
        acc = [sb("n_acc%d" % i, [128, 512], F32) for i in range(2)]
        accb = [sb("n_accb%d" % i, [128, 512], BF16) for i in range(2)]
        aT = [sb("n_aT%d" % i, [128, 4, 128], BF16) for i in range(2)]
        sm = [sb("n_sm%d" % i, [128, 64], F32) for i in range(2)]
        sc = [sb("n_sc%d" % i, [128, 128], F32) for i in range(2)]
        sc2 = [sb("n_sc2%d" % i, [128, 128], F32) for i in range(2)]
        seln = [sb("n_seln%d" % i, [128, 128], BF16) for i in range(2)]
        tmpacc = [sb("n_tmpacc%d" % i, [128, 256], F32) for i in range(2)]
        s.dma("sp", KS[0:64, :, :], X.KSP.rearrange("(g d) t -> d g t", g=2), reads=["KSP"], writes=["KS"])
        s.dma("sp", KW[0:64, :, :], X.KWP.rearrange("(g d) t -> d g t", g=2), reads=["KWP"], writes=["KW"])
        for g in range(2):
            s.dma("sp", KS[64:67, g, :], X.KAUG, reads=["KAUG"], writes=["KS"])
            s.dma("sp", KW[64:67, g, :], X.KAUG, reads=["KAUG"], writes=["KW"])
        for q4 in range(4):
            tsl = slice(q4 * 2048, (q4 + 1) * 2048)
            ksl = slice(q4 * 16, (q4 + 1) * 16)
            s.dma("sp", VS[:, ksl], X.VSX[tsl].rearrange("(kt p) g c -> p kt g c", p=128), reads=["VSX"], writes=["VS"])
            s.dma("sp", VW[:, ksl], X.VWX[tsl].rearrange("(kt p) g c -> p kt g c", p=128), reads=["VWX"], writes=["VW"])
            s.dma("pool", E[:, ksl, :], X.c["c_E"][:, ksl, :], writes=["E"])
        s.dma("pool", mca[:], X.c["c_mcausal"], writes=["mca"])
        s.dma("pool", mw4[:], X.c["c_mwin4"], writes=["mw4"])
        s.dma("pool", mcmp[:], X.c["c_mcmp"].rearrange("n p c -> p n c"), writes=["mcmp"])
        st = {"sb": 0, "pt": 0}

        def qk_tile(g, qb, lhsT, lres, extra, mask):
            pi = st["sb"] % 3
            st["sb"] += 1
            ps = X.ps[pi]
            pres = "ps%d" % pi
            nmm = 1 + (extra is not None) + (mask is not None)
            s.op("pe", lambda e: e.matmul(ps[:], lhsT=lhsT, rhs=qt[qb][:, g * 512:(g + 1) * 512], start=True,
                                          stop=(nmm == 1)), reads=[lres, "qt%d" % qb], writes=[pres])
            k = 1
            if extra is not None:
                el, er, eres = extra
                s.op("pe", lambda e: e.matmul(ps[:], lhsT=el, rhs=er, start=False, stop=(k + 1 == nmm)),
                     reads=eres, writes=[pres])
                k += 1
            if mask is not None:
                ml, mres = mask
                s.op("pe", lambda e: e.matmul(ps[:], lhsT=X.ident[:], rhs=ml, start=False, stop=True),
                     reads=["ident", mres], writes=[pres])
            pk = st["pt"] % 4
            st["pt"] += 1
            s.op("act", lambda e: e.activation(out=PT[pk][:], in_=ps[:], func=AF.Exp),
                 reads=[pres], writes=["PT%d" % pk])
            return PT[pk], "PT%d" % pk

        def coef_and_acc(g, qb, branch, heads_ap, heads_res, first):
            smt = sm[g]
            sres = "sm%d" % g
            if branch == 0:
                views = [(X.ps[3][:, 0:386].rearrange("p (r c) -> p r c", c=193), 0, 2),
                         (X.ps[4][:, 0:386].rearrange("p (r c) -> p r c", c=193), 2, 2)]
            else:
                views = [(X.ps[5 + g][:, 0:260].rearrange("p (r c) -> p r c", c=65), 0, 4)]
            for v, r0, n in views:
                s.op("dve", lambda e: e.tensor_scalar(out=smt[:, r0:r0 + n], in0=v[:, :, 64], scalar1=1e-36,
                                                      scalar2=None, op0=ALU.max),
                     reads=heads_res, writes=[sres])
            s.op("dve", lambda e: e.reciprocal(out=smt[:, 4:8], in_=smt[:, 0:4]), reads=[sres], writes=[sres])
            c0 = branch * 8 + g * 4
            s.op("dve", lambda e: e.tensor_tensor(out=smt[:, 8:12], in0=smt[:, 4:8], in1=glt[qb][:, c0:c0 + 4],
                                                  op=ALU.mult), reads=[sres, "gl%d" % qb], writes=[sres])
            for v, r0, n in views:
                dst = acc[qb][:, (g * 4 + r0) * 64:(g * 4 + r0 + n) * 64].rearrange("p (r d) -> p r d", d=64)
                cf = bcast_mid(smt[:, 8 + r0:8 + r0 + n], 64)
                if first:
                    s.op("dve", lambda e: e.tensor_tensor(out=dst, in0=v[:, :, 0:64], in1=cf, op=ALU.mult),
                         reads=heads_res + [sres], writes=["acc%d" % qb])
                else:
                    tmp = sc2[g][:, 0:n * 64].rearrange("p (r d) -> p r d", d=64) if False else None
                    s.op("dve", lambda e: e.tensor_tensor(out=tmpacc[g][:, 0:n * 64].rearrange("p (r d) -> p r d", d=64),
                                                          in0=v[:, :, 0:64], in1=cf, op=ALU.mult),
                         reads=heads_res + [sres], writes=["tmpacc%d" % g])
                    s.op("dve", lambda e: e.tensor_tensor(out=dst, in0=dst,
                                                          in1=tmpacc[g][:, 0:n * 64].rearrange("p (r d) -> p r d", d=64),
                                                          op=ALU.add),
                         reads=["tmpacc%d" % g], writes=["acc%d" % qb])

        def emit_loads(qi):
            qb = qi % 2
            t0 = qi * 128
            for j in range(4):
                s.dma("sp", qt[qb][0:64, :].rearrange("d (j two t) -> d j two t", j=4, two=2)[:, j],
                      X.QTP[j, :, t0:t0 + 128].rearrange("(two d) t -> d two t", two=2),
                      reads=["QTP"], writes=["qt%d" % qb])
            s.dma("sp", qt[qb][64:67, :].rearrange("r (h t) -> r h t", h=8),
                  X.QAUG[:, :, t0:t0 + 128].rearrange("h r t -> r h t"), reads=["QAUG"], writes=["qt%d" % qb])
            s.dma("sp", AB[qb][:], X.c["c_AB"][qi], writes=["AB%d" % qb])
            s.dma("sp", glt[qb][:], X.GLS[t0:t0 + 128, :], reads=["GLS"], writes=["gl%d" % qb])

        def cmp_post_a(qi, g):
            qb = qi % 2
            ocb = lambda r: X.ps[3 + r // 2][:, (r % 2) * 193:(r % 2) * 193 + 193]
            coef_and_acc(g, qb, 0, ocb, ["ps3", "ps4"], True)
            smt = sm[g]
            sres = "sm%d" % g
            for r in range(4):
                if r == 0:
                    s.op("dve", lambda e: e.tensor_scalar(out=sc[g][:], in0=ocb(r)[:, 65:193], scalar1=smt[:, 4:5],
                                                          scalar2=None, op0=ALU.mult),
                         reads=["ps3", "ps4", sres], writes=["sc%d" % g])
                else:
                    s.op("dve", lambda e: e.scalar_tensor_tensor(out=sc[g][:], in0=ocb(r)[:, 65:193],
                                                                 scalar=smt[:, 4 + r:5 + r], in1=sc[g][:],
                                                                 op0=ALU.mult, op1=ALU.add),
                         reads=["ps3", "ps4", sres], writes=["sc%d" % g])
            s.op("dve", lambda e: e.tensor_tensor(out=sc[g][:], in0=sc[g][:], in1=AB[qb][:, 0:128], op=ALU.mult),
                 reads=["AB%d" % qb], writes=["sc%d" % g])
            s.op("dve", lambda e: e.tensor_tensor(out=sc[g][:], in0=sc[g][:], in1=AB[qb][:, 128:256], op=ALU.add),
                 reads=["AB%d" % qb], writes=["sc%d" % g])
            s.op("dve", lambda e: e.max(out=smt[:, 16:24], in_=sc[g][:]), reads=["sc%d" % g], writes=[sres])
            s.op("dve", lambda e: e.match_replace(out=sc2[g][:], in_to_replace=smt[:, 16:24], in_values=sc[g][:],
                                                  imm_value=-2.0), reads=["sc%d" % g, sres], writes=["sc2%d" % g])
            s.op("dve", lambda e: e.max(out=smt[:, 24:32], in_=sc2[g][:]), reads=["sc2%d" % g], writes=[sres])
            s.op("dve", lambda e: e.tensor_scalar(out=seln[g][:], in0=sc[g][:], scalar1=smt[:, 31:32], scalar2=NEG,
                                                  op0=ALU.is_lt, op1=ALU.mult),
                 reads=["sc%d" % g, sres], writes=["seln%d" % g])

        def cmp_post_b(qi, g):
            psb = X.ps[7][:].bitcast(BF16)
            s.op("pe", lambda e: e.transpose(out=psb[:, g * 128:(g + 1) * 128], in_=seln[g][:], identity=X.ident[:]),
                 reads=["seln%d" % g, "ident"], writes=["ps7"])
            s.op("dve", lambda e: e.tensor_copy(out=negT[g][:].rearrange("p (a t) -> p a t", a=4),
                                                in_=bcast_rep(psb[:, g * 128:(g + 1) * 128], 4)),
                 reads=["ps7"], writes=["negT%d" % g])

        def write_out(qi):
            qb = qi % 2
            t0 = qi * 128
            s.op("act", lambda e: e.activation(out=accb[qb][:], in_=acc[qb][:], func=AF.Copy),
                 reads=["acc%d" % qb], writes=["accb%d" % qb])
            psb = X.ps[7][:].bitcast(BF16)
            for j in range(4):
                s.op("pe", lambda e: e.transpose(out=psb[:, 256 + j * 128:256 + (j + 1) * 128],
                                                 in_=accb[qb][:, j * 128:(j + 1) * 128], identity=X.ident[:]),
                     reads=["accb%d" % qb, "ident"], writes=["ps7"])
            s.op("act", lambda e: e.activation(out=aT[qb][:].rearrange("p j t -> p (j t)"), in_=psb[:, 256:768], func=AF.Copy),
                 reads=["ps7"], writes=["aT%d" % qb])
            s.dma("sp", X.AT[0:512, t0:t0 + 128].rearrange("(j p) t -> p j t", p=128), aT[qb][:],
                  reads=["aT%d" % qb], writes=["AT"])

        def pv_fn(out_ap, out_res, rhs, rhs_res, first, lastt, pair_start):
            def f(pt, ptres):
                for r in range(4):
                    st_ = first and ((r % 2 == 0) if pair_start else (r == 0))
                    s.op("pe", lambda e: e.matmul(out_ap(r), lhsT=pt[:, r * 128:(r + 1) * 128], rhs=rhs,
                                                  start=st_, stop=lastt), reads=[ptres, rhs_res], writes=out_res(r))
            return f

        jobs = []
        for qi in range(X.nsa_tiles):
            qb = qi % 2
            first_job = len(jobs)
            plan = X.cmp_plan[qi]
            cs = [c for c in range(4) if plan[c] != -2]
            ocb = lambda r: X.ps[3 + r // 2][:, (r % 2) * 193:(r % 2) * 193 + 193]
            ocres = lambda r: ["ps%d" % (3 + r // 2)]
            for g in range(2):
                for ci, c in enumerate(cs):
                    mask = None if plan[c] == -1 else (mcmp[:, plan[c], :], "mcmp")
                    jobs.append(dict(
                        qk=(lambda g=g, qb=qb, c=c, mask=mask: qk_tile(g, qb, KCMP[:, g, c * 128:(c + 1) * 128], "KCMP", None, mask)),
                        pv=pv_fn(ocb, ocres, VCX[:, g, c, :], "VCX", ci == 0, ci == len(cs) - 1, True),
                        post=((lambda qi=qi, g=g: (cmp_post_a(qi, g), (cmp_post_b(qi, g) if qi < 2 else None)))
                              if ci == len(cs) - 1 else None)))
            for g in range(2):
                obr = (lambda g: (lambda r: X.ps[5 + g][:, r * 65:(r + 1) * 65]))(g)
                obres = (lambda g: (lambda r: ["ps%d" % (5 + g)]))(g)
                dl = list(range(min(4, qi), -1, -1))
                for di, dd in enumerate(dl):
                    kt = qi - dd
                    mask = (mca[:], "mca") if dd == 0 else ((mw4[:], "mw4") if dd == 4 else None)
                    posts = []
                    if di == 0 and qi >= 2:
                        posts.append(lambda qi=qi, g=g: cmp_post_b(qi, g))
                    if di == len(dl) - 1:
                        posts.append(lambda g=g, qb=qb, obr=obr: coef_and_acc(g, qb, 2, obr, ["ps%d" % (5 + g)], False))
                    jobs.append(dict(
                        qk=(lambda g=g, qb=qb, kt=kt, mask=mask: qk_tile(g, qb, KW[:, g, kt * 128:(kt + 1) * 128], "KW", None, mask)),
                        pv=pv_fn(obr, obres, VW[:, kt, g, :], "VW", di == 0, di == len(dl) - 1, False),
                        post=(lambda posts=posts: [p_() for p_ in posts])))
            for g in range(2):
                obr = (lambda g: (lambda r: X.ps[5 + g][:, r * 65:(r + 1) * 65]))(g)
                obres = (lambda g: (lambda r: ["ps%d" % (5 + g)]))(g)
                for kt in range(qi + 1):
                    mask = (mca[:], "mca") if kt == qi else None
                    posts = []
                    if kt == qi:
                        posts.append(lambda g=g, qb=qb, obr=obr: coef_and_acc(g, qb, 1, obr, ["ps%d" % (5 + g)], False))
                        if g == 1:
                            posts.append(lambda qi=qi: write_out(qi))
                    jobs.append(dict(
                        qk=(lambda g=g, qb=qb, kt=kt, mask=mask: qk_tile(
                            g, qb, KS[:, g, kt * 128:(kt + 1) * 128], "KS", (E[:, kt, :], negT[g][:], ["E", "negT%d" % g]), mask)),
                        pv=pv_fn(obr, obres, VS[:, kt, g, :], "VS", kt == 0, kt == qi, False),
                        post=(lambda posts=posts: [p_() for p_ in posts])))
            if qi + 1 < X.nsa_tiles:
                old = jobs[first_job]["post"]
                jobs[first_job]["post"] = (lambda old=old, qi=qi: ((old() if old else None), emit_loads(qi + 1)))

        LA = 2
        emit_loads(0)
        pend = {}

        def start(j):
            pend[j] = jobs[j]["qk"]()

        for j in range(min(LA, len(jobs))):
            start(j)
        if L == 0:
            X.conv_todo = conv_thunks(s, X, X.conv_layers)
        n_here = len(X.conv_todo) if L == DEPTH - 1 else min(len(X.conv_todo), 11 + 80)
        every = max(1, (len(jobs) - 100) // max(1, n_here))
        for j in range(len(jobs)):
            if n_here > 0 and j >= 50 and (j - 50) % every == 0 and X.conv_todo:
                X.conv_todo.pop(0)()
                n_here -= 1
            if j + LA < len(jobs):
                start(j + LA)
            pt, ptres = pend.pop(j)
            jobs[j]["pv"](pt, ptres)
            if jobs[j]["post"]:
                jobs[j]["post"]()
        while L == DEPTH - 1 and X.conv_todo:
            X.conv_todo.pop(0)()
        s.barrier()


PHASES.append(("nsa", phase_nsa))


def phase_ret(s, X, L, h_in):
    nc = X.nc
    with ExitStack() as es:
        sb = lambda name, shape, dt: es.enter_context(nc.sbuf_tensor("%s_L%d" % (name, L), shape, dt))
        qT = [sb("r_qT%d" % i, [64, 8, 128], BF16) for i in range(2)]
        kT = [sb("r_kT%d" % i, [64, 8, 128], BF16) for i in range(2)]
        ones1 = sb("r_ones1", [1, 128], F32)
        gn1 = sb("r_gn1", [1, 512], F32)
        km = [sb("r_km%d" % i, [128, 512], BF16) for i in range(2)]
        vm = [sb("r_vm%d" % i, [128, 512], BF16) for i in range(2)]
        gm = [sb("r_gm%d" % i, [128, 512], BF16) for i in range(2)]
        state = sb("r_state", [64, 8, 64], F32)
        stateb = [sb("r_stateb%d" % i, [64, 8, 64], BF16) for i in range(2)]
        rmask = sb("r_mask", [128, 512], F32)
        gC = sb("r_gC", [64, 8], F32)
        gn = sb("r_gn", [128, 512], F32)
        Pm = [sb("r_Pm%d" % i, [128, 8, 128], BF16) for i in range(2)]
        sq = sb("r_sq", [128, 512], F32)
        y = sb("r_y", [128, 512], F32)
        yb = [sb("r_yb%d" % i, [128, 512], BF16) for i in range(2)]
        rT = [sb("r_rT%d" % i, [128, 4, 128], BF16) for i in range(2)]
        stt = sb("r_st", [128, 64], F32)
        s.dma("sp", rmask[:], X.c["c_rmask"], writes=["rmask"])
        s.dma("sp", gC[:], X.c["c_gC8"], writes=["gC"])
        s.dma("sp", gn1[:], X.ret_gn[L:L + 1, :], writes=["gn1"])
        s.op("dve", lambda e: e.memset(ones1[:], 1.0), writes=["ones1"])
        s.op("pe", lambda e: e.matmul(X.ps[6][:], lhsT=ones1[:], rhs=gn1[:], start=True, stop=True),
             reads=["ones1", "gn1"], writes=["ps6"])
        s.op("dve", lambda e: e.tensor_copy(out=gn[:], in_=X.ps[6][:]), reads=["ps6"], writes=["gn"])
        s.op("dve", lambda e: e.memset(state[:], 0.0), writes=["state"])
        def loads(c):
            b = c % 2
            tok = slice(c * 128, (c + 1) * 128)
            for j in range(4):
                s.dma("sp", qT[b][:, 2 * j:2 * j + 2, :], X.QRT[j, :, tok].rearrange("(two d) t -> d two t", two=2),
                      reads=["QRT"], writes=["qT%d" % b])
                s.dma("sp", kT[b][:, 2 * j:2 * j + 2, :], X.KRT[j, :, tok].rearrange("(two d) t -> d two t", two=2),
                      reads=["KRT"], writes=["kT%d" % b])
            s.dma("sp", km[b][:], X.KRM[tok, :], reads=["KRM"], writes=["km%d" % b])
            s.dma("sp", vm[b][:], X.VRM[tok, :], reads=["VRM"], writes=["vm%d" % b])
            s.dma("sp", gm[b][:], X.GRM[tok, :], reads=["GRM"], writes=["gm%d" % b])

        def t1m(c):
            b = c % 2
            for h in range(8):
                pb = h // 4
                s.op("pe", lambda e: e.matmul(X.ps[pb][:, (h % 4) * 128:(h % 4 + 1) * 128],
                                              lhsT=kT[b][:, h, :], rhs=qT[b][:, h, :],
                                              start=(h % 4 == 0), stop=True),
                     reads=["kT%d" % b, "qT%d" % b], writes=["ps%d" % pb])
            for pb in range(2):
                s.op("dve", lambda e: e.tensor_tensor(out=Pm[b][:, 4 * pb:4 * pb + 4, :].rearrange("p a t -> p (a t)"),
                                                      in0=X.ps[pb][:], in1=rmask[:], op=ALU.mult),
                     reads=["ps%d" % pb, "rmask"], writes=["Pm%d" % b])

        def upd(c):
            b = c % 2
            for h in range(8):
                s.op("pe", lambda e: e.matmul(X.ps[3][0:64, h * 64:(h + 1) * 64], lhsT=km[b][:, h * 64:(h + 1) * 64],
                                              rhs=vm[b][:, h * 64:(h + 1) * 64], start=(h == 0), stop=True),
                     reads=["km%d" % b, "vm%d" % b], writes=["ps3"])
            s.op("dve", lambda e: e.tensor_tensor(out=state[:], in0=state[:],
                                                  in1=X.ps[3][0:64, :].rearrange("p (h x) -> p h x", h=8), op=ALU.add),
                 reads=["ps3"], writes=["state"])
            s.op("dve", lambda e: e.tensor_tensor(out=state[:], in0=state[:], in1=bcast_mid(gC[:], 64), op=ALU.mult),
                 reads=["gC"], writes=["state"])
            s.op("dve", lambda e: e.tensor_copy(out=stateb[(c + 1) % 2][:], in_=state[:]), reads=["state"],
                 writes=["stateb%d" % ((c + 1) % 2)])

        def omm(c):
            b = c % 2
            po = 2 if b == 0 else 6
            for h in range(8):
                s.op("pe", lambda e: e.matmul(X.ps[po][:, h * 64:(h + 1) * 64], lhsT=Pm[b][:, h, :],
                                              rhs=vm[b][:, h * 64:(h + 1) * 64], start=(h == 0), stop=(c == 0)),
                     reads=["Pm%d" % b, "vm%d" % b], writes=["ps%d" % po])
                if c > 0:
                    s.op("pe", lambda e: e.matmul(X.ps[po][:, h * 64:(h + 1) * 64], lhsT=qT[b][:, h, :],
                                                  rhs=stateb[b][:, h, :], start=False, stop=True),
                         reads=["qT%d" % b, "stateb%d" % b], writes=["ps%d" % po])

        def gn_a(c):
            b = c % 2
            po = 2 if b == 0 else 6
            pres = "ps%d" % po
            o3 = X.ps[po][:].rearrange("p (h d) -> p h d", h=8)
            s.op("dve", lambda e: e.tensor_reduce(out=stt[:, 0:8], in_=o3, axis=AX.X, op=ALU.add),
                 reads=[pres], writes=["r_st"])
            s.op("act", lambda e: e.activation(out=sq[:], in_=X.ps[po][:], func=AF.Square), reads=[pres], writes=["r_sq"])
            s.op("dve", lambda e: e.tensor_reduce(out=stt[:, 8:16], in_=sq[:].rearrange("p (h d) -> p h d", h=8),
                                                  axis=AX.X, op=ALU.add), reads=["r_sq"], writes=["r_st"])
            s.op("dve", lambda e: e.tensor_scalar(out=stt[:, 16:24], in0=stt[:, 0:8], scalar1=1.0 / 64, scalar2=None,
                                                  op0=ALU.mult), reads=["r_st"], writes=["r_st"])
            s.op("dve", lambda e: e.tensor_tensor(out=stt[:, 24:32], in0=stt[:, 16:24], in1=stt[:, 16:24], op=ALU.mult),
                 reads=["r_st"], writes=["r_st"])
            s.op("dve", lambda e: e.scalar_tensor_tensor(out=stt[:, 32:40], in0=stt[:, 8:16], scalar=1.0 / 64,
                                                         in1=stt[:, 24:32], op0=ALU.mult, op1=ALU.subtract),
                 reads=["r_st"], writes=["r_st"])
            s.op("dve", lambda e: e.tensor_scalar(out=stt[:, 32:40], in0=stt[:, 32:40], scalar1=EPS, scalar2=None,
                                                  op0=ALU.add), reads=["r_st"], writes=["r_st"])
            s.op("act", lambda e: e.activation(out=stt[:, 40:48], in_=stt[:, 32:40], func=AF.Sqrt),
                 reads=["r_st"], writes=["r_st"])
            s.op("dve", lambda e: e.reciprocal(out=stt[:, 48:56], in_=stt[:, 40:48]), reads=["r_st"], writes=["r_st"])
            y3 = y[:].rearrange("p (h d) -> p h d", h=8)
            s.op("dve", lambda e: e.tensor_tensor(out=y3, in0=o3, in1=bcast_mid(stt[:, 16:24], 64), op=ALU.subtract),
                 reads=[pres, "r_st"], writes=["r_y"])
            s.op("dve", lambda e: e.tensor_tensor(out=y3, in0=y3, in1=bcast_mid(stt[:, 48:56], 64), op=ALU.mult),
                 reads=["r_st"], writes=["r_y"])
            s.op("pool", lambda e: e.tensor_tensor(out=y[:], in0=y[:], in1=gn[:], op=ALU.mult),
                 reads=["r_y", "gn"], writes=["r_y"])
            s.op("pool", lambda e: e.tensor_tensor(out=yb[b][:], in0=y[:], in1=gm[b][:], op=ALU.mult),
                 reads=["r_y", "gm%d" % b], writes=["r_yb%d" % b])

        def gn_b(c):
            b = c % 2
            tok = slice(c * 128, (c + 1) * 128)
            psb = X.ps[7][:].bitcast(BF16)
            for j in range(4):
                s.op("pe", lambda e: e.transpose(out=psb[:, j * 128:(j + 1) * 128], in_=yb[b][:, j * 128:(j + 1) * 128],
                                                 identity=X.ident[:]), reads=["r_yb%d" % b, "ident"], writes=["ps7"])
            s.op("act", lambda e: e.activation(out=rT[b][:].rearrange("p j t -> p (j t)"), in_=psb[:, 0:512], func=AF.Copy),
                 reads=["ps7"], writes=["r_rT%d" % b])
            s.dma("pool", X.AT[512:1024, tok].rearrange("(j p) t -> p j t", p=128), rT[b][:],
                  reads=["r_rT%d" % b], writes=["AT"])

        NCH = X.ret_chunks
        loads(0)
        for c in range(NCH):
            if c + 1 < NCH:
                loads(c + 1)
            t1m(c)
            upd(c)
            omm(c)
            if c >= 1:
                gn_b(c - 1)
            gn_a(c)
        gn_b(NCH - 1)
        s.barrier()


PHASES.append(("ret", phase_ret))


def conv_thunks(s, X, layers):
    th = []

    def conv(dst, src, rows):
        for r0 in range(0, rows, 512):
            th.append(lambda dst=dst, src=src, r0=r0: s.dma("pool", dst[r0:r0 + 512, :], src[r0:r0 + 512, :], writes=["W16"]))
    if 0 in layers:
        conv(X.FG16[0], X.ffn_gate[0], D)
        conv(X.FU16[0], X.ffn_up[0], D)
        conv(X.FD16[0], X.ffn_down[0], D_FF)
    if 1 in layers:
        for e in range(NE):
            for blk in range(7):
                r0 = (e * 7 + blk) * 128
                fs = slice(blk * 512, (blk + 1) * 512)
                th.append(lambda e=e, r0=r0, fs=fs: s.dma(
                    "pool", X.WGB[r0:r0 + 128, :].rearrange("p (k f) -> p k f", k=8),
                    X.moe_gate[0, e][:, fs].rearrange("(k p) f -> p k f", p=128), writes=["W16"]))
                th.append(lambda e=e, r0=r0, fs=fs: s.dma(
                    "pool", X.WUB[r0:r0 + 128, :].rearrange("p (k f) -> p k f", k=8),
                    X.moe_up[0, e][:, fs].rearrange("(k p) f -> p k f", p=128), writes=["W16"]))
                th.append(lambda e=e, r0=r0, fs=fs: s.dma(
                    "pool", X.WDB[r0:r0 + 128, :].rearrange("p (c n) -> p c n", c=4),
                    X.moe_down[0, e][fs, :].rearrange("(c p) n -> p c n", p=128), writes=["W16"]))
    return th


def phase_ffn(s, X, L, h_in):
    nc = X.nc
    last = (L == DEPTH - 1)
    moe = (L % 2 == 1)
    if moe and X.routed:
        return phase_moe(s, X, L, h_in)
    h_out = X.out if last else X.HB
    G16, U16, D16 = X.FG16, X.FU16, X.FD16
    with ExitStack() as es:
        sb = lambda name, shape, dt: es.enter_context(nc.sbuf_tensor("%s_L%d" % (name, L), shape, dt))
        wout = sb("f_wout", [128, 8, D], BF16)
        pgate = sb("f_pgate", [128, 8, D], BF16)
        pproj = sb("f_pproj", [128, 2, D], BF16)
        g2T = sb("f_g2T", [128, 8], F32)
        g3T = sb("f_g3T", [128, 8], F32)
        rw = sb("f_rw", [128, 8, NE], BF16)
        cT = sb("f_cT", [128, 8, 512], BF16)
        hin = [sb("f_hin%d" % i, [128, D], F32) for i in range(2)]
        h1 = sb("f_h1", [128, 4, D], F32)
        hnT = sb("f_hnT", [128, 8, 512], BF16)
        acc = sb("f_acc", [128, 4, D], F32)
        wg = [sb("f_wg%d" % i, [128, 8, 512], BF16) for i in range(3)]
        wu = [sb("f_wu%d" % i, [128, 8, 512], BF16) for i in range(3)]
        wd = [sb("f_wd%d" % i, [128, 4, D], BF16) for i in range(3)]
        silu_t = [sb("f_silu%d" % i, [128, 512], BF16) for i in range(2)]
        actb = [sb("f_actb%d" % i, [128, 4, 512], BF16) for i in range(2)]
        sig = [sb("f_sig%d" % i, [128, 512], F32) for i in range(2)]
        ptile = [sb("f_pt%d" % i, [128, 256], F32) for i in range(2)]
        pb = sb("f_pb", [128, 256], BF16)
        pT = sb("f_pT", [128, 2, 512], BF16)
        gates = sb("f_gates", [128, 4, NE], F32)
        lg = sb("f_lg", [128, 16], F32)
        sm = sb("f_sm", [128, 32], F32)
        for kc in range(8):
            s.dma("pool", wout[:, kc, :], X.w_out[L, kc * 128:(kc + 1) * 128, :], writes=["wout"])
            s.dma("pool", pgate[:, kc, :], X.ple_gate[L, kc * 128:(kc + 1) * 128, :], writes=["pgate"])
        for kc in range(2):
            s.dma("pool", pproj[:, kc, :], X.ple_proj[L, kc * 128:(kc + 1) * 128, :], writes=["pproj"])
        s.dma("sp", g2T[:], X.g_ffn[L].rearrange("(k p) -> p k", p=128), writes=["g2T"], allow_slow_non_contiguous=True)
        s.dma("sp", g3T[:], X.g_ple[L].rearrange("(k p) -> p k", p=128), writes=["g3T"], allow_slow_non_contiguous=True)
        if moe:
            s.dma("pool", rw[:], X.moe_router[0].rearrange("(k p) e -> p k e", p=128), writes=["rw"])
        if last:
            gfin = sb("f_gfin", [128, D], F32)
            ones1 = sb("f_ones1", [1, 128], F32)
            gf1 = sb("f_gf1", [1, D], F32)
            s.dma("sp", gf1[:], X.g_final.rearrange("(o n) -> o n", o=1), writes=["gf1"])
            s.op("dve", lambda e: e.memset(ones1[:], 1.0), writes=["ones1"])
            for half in range(2):
                s.op("pe", lambda e: e.matmul(X.ps[6][:], lhsT=ones1[:], rhs=gf1[:, half * 512:(half + 1) * 512],
                                              start=True, stop=True), reads=["ones1", "gf1"], writes=["ps6"])
                s.op("dve", lambda e: e.tensor_copy(out=gfin[:, half * 512:(half + 1) * 512], in_=X.ps[6][:]),
                     reads=["ps6"], writes=["gfin"])
        wcnt = 0
        for tg in range(NT // 4):
            toks = slice(tg * 512, (tg + 1) * 512)
            s.dma("sp", cT[:], X.AT[:, toks].rearrange("(k p) t -> p k t", p=128), reads=["AT"], writes=["cT"])
            for tt in range(4):
                ti = 4 * tg + tt
                hb_ = hin[ti % 2]
                s.dma("sp", hb_[:], h_in[ti * 128:(ti + 1) * 128, :], reads=["HB"], writes=["hin%d" % (ti % 2)])
                for half in range(2):
                    hs = slice(half * 512, (half + 1) * 512)
                    for k in range(8):
                        s.op("pe", lambda e: e.matmul(X.ps[6][:], lhsT=cT[:, k, tt * 128:(tt + 1) * 128], rhs=wout[:, k, hs],
                                                      start=(k == 0), stop=(k == 7)), reads=["cT", "wout"], writes=["ps6"])
                    s.op("dve", lambda e: e.tensor_tensor(out=h1[:, tt, hs], in0=X.ps[6][:], in1=hb_[:, hs], op=ALU.add),
                         reads=["ps6", "hin%d" % (ti % 2)], writes=["h1_%d" % tt])
                ln_a(s, X, h1[:, tt, :], "h1_%d" % tt, tt)
            for tt in range(4):
                ln_b(s, X, tt, g2T[:], "g2T", hnT[:, :, tt * 128:(tt + 1) * 128], "f_hnT")
            if moe:
                for tt in range(4):
                    for k in range(8):
                        s.op("pe", lambda e: e.matmul(X.ps[6][:, 0:NE], lhsT=hnT[:, k, tt * 128:(tt + 1) * 128], rhs=rw[:, k, :],
                                                      start=(k == 0), stop=(k == 7)), reads=["f_hnT", "rw"], writes=["ps6"])
                    s.op("dve", lambda e: e.tensor_copy(out=lg[:, 0:8], in_=X.ps[6][:, 0:NE]), reads=["ps6"], writes=["lg"])
                    s.op("dve", lambda e: e.max(out=sm[:, 0:8], in_=lg[:, 0:8]), reads=["lg"], writes=["fsm"])
                    s.op("dve", lambda e: e.tensor_tensor(out=sm[:, 8:9], in0=sm[:, 1:2], in1=sm[:, 0:1], op=ALU.subtract),
                         reads=["fsm"], writes=["fsm"])
                    s.op("act", lambda e: e.activation(out=sm[:, 9:10], in_=sm[:, 8:9], func=AF.Exp), reads=["fsm"], writes=["fsm"])
                    s.op("dve", lambda e: e.tensor_scalar(out=sm[:, 10:11], in0=sm[:, 9:10], scalar1=1.0, scalar2=None,
                                                          op0=ALU.add), reads=["fsm"], writes=["fsm"])
                    s.op("dve", lambda e: e.reciprocal(out=sm[:, 11:12], in_=sm[:, 10:11]), reads=["fsm"], writes=["fsm"])
                    s.op("dve", lambda e: e.tensor_tensor(out=sm[:, 12:13], in0=sm[:, 9:10], in1=sm[:, 11:12], op=ALU.mult),
                         reads=["fsm"], writes=["fsm"])
                    s.op("dve", lambda e: e.tensor_scalar(out=lg[:, 8:16], in0=lg[:, 0:8], scalar1=sm[:, 0:1],
                                                          scalar2=sm[:, 11:12], op0=ALU.is_equal, op1=ALU.mult),
                         reads=["lg", "fsm"], writes=["lg"])
                    s.op("dve", lambda e: e.tensor_scalar(out=gates[:, tt, :], in0=lg[:, 0:8], scalar1=sm[:, 1:2],
                                                          scalar2=sm[:, 12:13], op0=ALU.is_equal, op1=ALU.mult),
                         reads=["lg", "fsm"], writes=["gates"])
                    s.op("dve", lambda e: e.tensor_tensor(out=gates[:, tt, :], in0=gates[:, tt, :], in1=lg[:, 8:16], op=ALU.add),
                         reads=["lg"], writes=["gates"])
            def stage_a(J):
                wb, ab = J % 3, J % 2
                for fc in range(4):
                    pg, pu = 2 * (fc % 2), 2 * (fc % 2) + 1
                    for k in range(8):
                        s.op("pe", lambda e: e.matmul(X.ps[pg][:], lhsT=wg[wb][:, k, fc * 128:(fc + 1) * 128], rhs=hnT[:, k, :],
                                                      start=(k == 0), stop=(k == 7)),
                             reads=["wg%d" % wb, "f_hnT"], writes=["ps%d" % pg])
                    for k in range(8):
                        s.op("pe", lambda e: e.matmul(X.ps[pu][:], lhsT=wu[wb][:, k, fc * 128:(fc + 1) * 128], rhs=hnT[:, k, :],
                                                      start=(k == 0), stop=(k == 7)),
                             reads=["wu%d" % wb, "f_hnT"], writes=["ps%d" % pu])
                    s.op("act", lambda e: e.activation(out=silu_t[fc % 2][:], in_=X.ps[pg][:], func=AF.Silu),
                         reads=["ps%d" % pg], writes=["silu%d" % (fc % 2)])
                    s.op("dve", lambda e: e.tensor_tensor(out=actb[ab][:, fc, :], in0=X.ps[pu][:], in1=silu_t[fc % 2][:],
                                                          op=ALU.mult),
                         reads=["ps%d" % pu, "silu%d" % (fc % 2)], writes=["actb%d" % ab])

            def stage_b(J):
                wb, ab, blk = J % 3, J % 2, J % 7
                for tt in range(4):
                    for half in range(2):
                        hs = slice(half * 512, (half + 1) * 512)
                        pd = 4 + half
                        for fc in range(4):
                            s.op("pe", lambda e: e.matmul(X.ps[pd][:], lhsT=actb[ab][:, fc, tt * 128:(tt + 1) * 128],
                                                          rhs=wd[wb][:, fc, hs], start=(fc == 0), stop=(fc == 3)),
                                 reads=["actb%d" % ab, "wd%d" % wb], writes=["ps%d" % pd])
                        dst = acc[:, tt, hs]
                        if blk == 0:
                            s.op("dve", lambda e: e.tensor_copy(out=dst, in_=X.ps[pd][:]),
                                 reads=["ps%d" % pd], writes=["acc%d" % tt])
                        else:
                            s.op("dve", lambda e: e.tensor_tensor(out=dst, in0=X.ps[pd][:], in1=dst, op=ALU.add),
                                 reads=["ps%d" % pd], writes=["acc%d" % tt])

            def wload(J):
                blk, wb = J % 7, J % 3
                fs = slice(blk * 512, (blk + 1) * 512)
                s.dma("sp", wg[wb][:], G16[0][:, fs].rearrange("(k p) f -> p k f", p=128), reads=["W16"], writes=["wg%d" % wb])
                s.dma("sp", wu[wb][:], U16[0][:, fs].rearrange("(k p) f -> p k f", p=128), reads=["W16"], writes=["wu%d" % wb])
                s.dma("sp", wd[wb][:], D16[0][fs, :].rearrange("(c p) n -> p c n", p=128), reads=["W16"], writes=["wd%d" % wb])

            NJ = (NT // 4) * 7
            if tg == 0:
                wload(0)
                wload(1)
            for blk in range(7):
                J = tg * 7 + blk
                stage_a(J)
                if blk >= 1:
                    stage_b(J - 1)
                if J + 2 < NJ:
                    wload(J + 2)
            stage_b(tg * 7 + 6)
            for tt in range(4):
                s.op("pool", lambda e: e.tensor_tensor(out=h1[:, tt, :], in0=h1[:, tt, :], in1=acc[:, tt, :], op=ALU.add),
                     reads=["acc%d" % tt], writes=["h1_%d" % tt])
                ln_a(s, X, h1[:, tt, :], "h1_%d" % tt, tt)
            for tt in range(4):
                ln_b(s, X, tt, g3T[:], "g3T", hnT[:, :, tt * 128:(tt + 1) * 128], "f_hnT")
            psb = X.ps[7][:].bitcast(BF16)
            for tt in range(4):
                ti = 4 * tg + tt
                pt_ = ptile[ti % 2]
                s.dma("sp", pt_[:], X.p[L, ti * 128:(ti + 1) * 128, :], writes=["pt%d" % (ti % 2)])
                s.op("act", lambda e: e.activation(out=pb[:], in_=pt_[:], func=AF.Copy), reads=["pt%d" % (ti % 2)], writes=["pb"])
                for k in range(2):
                    s.op("pe", lambda e: e.transpose(out=psb[:, k * 128:(k + 1) * 128], in_=pb[:, k * 128:(k + 1) * 128],
                                                     identity=X.ident[:]), reads=["pb", "ident"], writes=["ps7"])
                s.op("act", lambda e: e.activation(out=pT[:, :, tt * 128:(tt + 1) * 128],
                                                   in_=psb[:, 0:256].rearrange("p (k t) -> p k t", k=2), func=AF.Copy),
                     reads=["ps7"], writes=["pT"])
            for tt in range(4):
                ti = 4 * tg + tt
                for half in range(2):
                    hs = slice(half * 512, (half + 1) * 512)
                    for k in range(2):
                        s.op("pe", lambda e: e.matmul(X.ps[6][:], lhsT=pT[:, k, tt * 128:(tt + 1) * 128], rhs=pproj[:, k, hs],
                                                      start=(k == 0), stop=(k == 1)), reads=["pT", "pproj"], writes=["ps6"])
                    pgb = 4 + half
                    for k in range(8):
                        s.op("pe", lambda e: e.matmul(X.ps[pgb][:], lhsT=hnT[:, k, tt * 128:(tt + 1) * 128], rhs=pgate[:, k, hs],
                                                      start=(k == 0), stop=(k == 7)), reads=["f_hnT", "pgate"], writes=["ps%d" % pgb])
                    s.op("act", lambda e: e.activation(out=sig[half][:], in_=X.ps[pgb][:], func=AF.Sigmoid),
                         reads=["ps%d" % pgb], writes=["sig%d" % half])
                    s.op("dve", lambda e: e.tensor_tensor(out=acc[:, tt, hs], in0=X.ps[6][:], in1=sig[half][:], op=ALU.mult),
                         reads=["ps6", "sig%d" % half], writes=["acc%d" % tt])
                s.op("pool", lambda e: e.tensor_tensor(out=h1[:, tt, :], in0=h1[:, tt, :], in1=acc[:, tt, :], op=ALU.add),
                     reads=["acc%d" % tt], writes=["h1_%d" % tt])
                if last:
                    ln_a(s, X, h1[:, tt, :], "h1_%d" % tt, tt)
                    s.op("act", lambda e: e.activation(out=acc[:, tt, :], in_=h1[:, tt, :], func=AF.Copy, scale=X.ss[tt][:, 2:3]),
                         reads=["h1_%d" % tt, "ss%d" % tt], writes=["acc%d" % tt])
                    s.op("pool", lambda e: e.tensor_tensor(out=acc[:, tt, :], in0=acc[:, tt, :], in1=gfin[:], op=ALU.mult),
                         reads=["gfin"], writes=["acc%d" % tt])
                    s.dma("pool", h_out[ti * 128:(ti + 1) * 128, :], acc[:, tt, :], reads=["acc%d" % tt], writes=["HOUT"])
                else:
                    s.dma("pool", h_out[ti * 128:(ti + 1) * 128, :], h1[:, tt, :], reads=["h1_%d" % tt], writes=["HB"])
        s.barrier()


PHASES.append(("ffn", phase_ffn))


def phase_moe(s, X, L, h_in):
    nc = X.nc
    last = (L == DEPTH - 1)
    h_out = X.out if last else X.HB
    with ExitStack() as es:
        sb = lambda name, shape, dt: es.enter_context(nc.sbuf_tensor("%s_L%d" % (name, L), shape, dt))
        OH = [sb("m_oh%d" % k, [128, NT, NE], F32) for k in range(2)]
        WK = [sb("m_wk%d" % k, [128, NT], F32) for k in range(2)]
        LG = sb("m_lg", [128, NT, NE], F32)
        POSI2 = sb("m_posi", [128, 2 * NT], I32)
        POSI = POSI2[:].rearrange("p (k i) -> p k i", k=2)
        WIDX2 = sb("m_widx", [128, NGRP * 7], I32)
        WIDX = WIDX2[:].rearrange("p (g b) -> p g b", b=7)
        g2T = sb("m_g2T", [128, 8], F32)
        g3T = sb("m_g3T", [128, 8], F32)
        hnT = sb("m_hnT", [128, 8, 512], BF16)
        s.dma("sp", g2T[:], X.g_ffn[L].rearrange("(k p) -> p k", p=128), writes=["g2T"], allow_slow_non_contiguous=True)
        s.dma("sp", g3T[:], X.g_ple[L].rearrange("(k p) -> p k", p=128), writes=["g3T"], allow_slow_non_contiguous=True)
        with ExitStack() as es2:
            sb2 = lambda name, shape, dt: es2.enter_context(nc.sbuf_tensor("%s_L%d" % (name, L), shape, dt))
            wout = sb2("ma_wout", [128, 8, D], BF16)
            rw = sb2("ma_rw", [128, 8, NE], BF16)
            cT = sb2("ma_cT", [128, 8, 512], BF16)
            hin = [sb2("ma_hin%d" % i, [128, D], F32) for i in range(2)]
            h1 = [sb2("ma_h1%d" % i, [128, D], F32) for i in range(4)]
            lg = sb2("ma_lg", [128, 16], F32)
            sm = sb2("ma_sm", [128, 32], F32)
            for kc in range(8):
                s.dma("pool", wout[:, kc, :], X.w_out[L, kc * 128:(kc + 1) * 128, :], writes=["wout"])
            s.dma("pool", rw[:], X.moe_router[0].rearrange("(k p) e -> p k e", p=128), writes=["rw"])
            for tg in range(NT // 4):
                toks = slice(tg * 512, (tg + 1) * 512)
                s.dma("sp", cT[:], X.AT[:, toks].rearrange("(k p) t -> p k t", p=128), reads=["AT"], writes=["cT"])
                for tt in range(4):
                    ti = 4 * tg + tt
                    hb_ = hin[ti % 2]
                    s.dma("sp", hb_[:], h_in[ti * 128:(ti + 1) * 128, :], reads=["HB"], writes=["hin%d" % (ti % 2)])
                    for half in range(2):
                        hs = slice(half * 512, (half + 1) * 512)
                        pbk = 4 + half
                        for k in range(8):
                            s.op("pe", lambda e: e.matmul(X.ps[pbk][:], lhsT=cT[:, k, tt * 128:(tt + 1) * 128], rhs=wout[:, k, hs],
                                                          start=(k == 0), stop=(k == 7)), reads=["cT", "wout"], writes=["ps%d" % pbk])
                        s.op("dve", lambda e: e.tensor_tensor(out=h1[tt][:, hs], in0=X.ps[pbk][:], in1=hb_[:, hs], op=ALU.add),
                             reads=["ps%d" % pbk, "hin%d" % (ti % 2)], writes=["h1_%d" % tt])
                    s.dma("pool", X.H1[ti * 128:(ti + 1) * 128, :], h1[tt][:], reads=["h1_%d" % tt], writes=["H1"])
                    ln_a(s, X, h1[tt][:], "h1_%d" % tt, tt)
                    s.dma("pool", X.XS2[ti * 128:(ti + 1) * 128, :], X.xs[tt][:], reads=["xs%d" % tt], writes=["XS2"])
                for tt in range(4):
                    ln_b(s, X, tt, g2T[:], "g2T", hnT[:, :, tt * 128:(tt + 1) * 128], "m_hnT")
                for tt in range(4):
                    ti = 4 * tg + tt
                    for k in range(8):
                        s.op("pe", lambda e: e.matmul(X.ps[6][:, 0:NE], lhsT=hnT[:, k, tt * 128:(tt + 1) * 128], rhs=rw[:, k, :],
                                                      start=(k == 0), stop=(k == 7)), reads=["m_hnT", "rw"], writes=["ps6"])
                    s.op("dve", lambda e: e.tensor_copy(out=LG[:, ti, :], in_=X.ps[6][:, 0:NE]), reads=["ps6"], writes=["LG"])
            s.barrier()
        with ExitStack() as es2:
            sb2 = lambda name, shape, dt: es2.enter_context(nc.sbuf_tensor("%s_L%d" % (name, L), shape, dt))
            Mf = sb2("mb_Mf", [128, NT, NE], F32)
            Mb = sb2("mb_Mb", [128, NT * NE], BF16)
            U = sb2("mb_U", [128, 128], BF16)
            ones = sb2("mb_ones", [128, 128], BF16)
            A_ = sb2("mb_A", [128, NT, NE], F32)
            B_ = sb2("mb_B", [128, NT, NE], F32)
            CN = sb2("mb_CN", [128, NT, NE], F32)
            POS = sb2("mb_POS", [128, NT, NE], F32)
            nE = sb2("mb_nE", [128, 64], F32)
            cmp16 = sb2("mb_cmp16", [128, NE, 16], F32)
            gi512 = sb2("mb_gi512", [128, NGRP], F32)
            tokid = sb2("mb_tokid", [128, NT], F32)
            blkp = sb2("mb_blkp", [128, 7], F32)
            cmpg = sb2("mb_cmpg", [128, NGRP, NE], F32)
            EG = sb2("mb_EG", [128, NGRP], F32)
            WF = sb2("mb_WF", [128, NGRP, 7], F32)
            PK = sb2("mb_PK", [128, 2, NT], F32)
            ROWS2 = sb2("mb_rows", [128, 2 * NT * 2], I32)
            ROWS = ROWS2[:].rearrange("p (k i c) -> p k i c", k=2, c=2)
            Z = sb2("mb_Z", [128, 2 * NSLOT // 128], I32)
            s.dma("pool", U[:], X.c["c_U"], writes=["U"])
            s.dma("sp", gi512[:], X.c["c_gi512"], writes=["gi512"])
            s.dma("sp", tokid[:], X.c["c_tokid"], writes=["tokid"])
            s.dma("sp", blkp[:], X.c["c_blkp"], writes=["blkp"])
            s.op("pool", lambda e: e.memset(ones[:], 1.0), writes=["ones"])
            s.op("pool", lambda e: e.memset(Z[:], 0), writes=["Z"])
            s.dma("sp", X.SLOT.rearrange("(p a) c -> p (a c)", p=128), Z[:], reads=["Z"], writes=["SLOT"])
            M12 = sb2("mb_M12", [128, 4, NT], F32)
            LG2 = sb2("mb_LG2", [128, NT, NE], F32)
            s.op("dve", lambda e: e.tensor_reduce(out=M12[:, 0, :], in_=LG[:], axis=AX.X, op=ALU.max), reads=["LG"], writes=["M12"])
            s.op("dve", lambda e: e.tensor_tensor(out=OH[0][:], in0=LG[:], in1=bcast_mid(M12[:, 0, :], NE), op=ALU.is_equal),
                 reads=["LG", "M12"], writes=["OH"])
            s.op("dve", lambda e: e.scalar_tensor_tensor(out=LG2[:], in0=OH[0][:], scalar=-1.0e30, in1=LG[:], op0=ALU.mult, op1=ALU.add),
                 reads=["OH", "LG"], writes=["LG2"])
            s.op("dve", lambda e: e.tensor_reduce(out=M12[:, 1, :], in_=LG2[:], axis=AX.X, op=ALU.max), reads=["LG2"], writes=["M12"])
            s.op("dve", lambda e: e.tensor_tensor(out=OH[1][:], in0=LG2[:], in1=bcast_mid(M12[:, 1, :], NE), op=ALU.is_equal),
                 reads=["LG2", "M12"], writes=["OH"])
            s.op("dve", lambda e: e.tensor_tensor(out=M12[:, 2, :], in0=M12[:, 1, :], in1=M12[:, 0, :], op=ALU.subtract),
                 reads=["M12"], writes=["M12"])
            s.op("act", lambda e: e.activation(out=M12[:, 3, :], in_=M12[:, 2, :], func=AF.Exp), reads=["M12"], writes=["M12"])
            s.op("dve", lambda e: e.tensor_scalar(out=M12[:, 2, :], in0=M12[:, 3, :], scalar1=1.0, scalar2=None, op0=ALU.add),
                 reads=["M12"], writes=["M12"])
            s.op("dve", lambda e: e.reciprocal(out=WK[0][:], in_=M12[:, 2, :]), reads=["M12"], writes=["WK"])
            s.op("dve", lambda e: e.tensor_tensor(out=WK[1][:], in0=M12[:, 3, :], in1=WK[0][:], op=ALU.mult), reads=["M12", "WK"], writes=["WK"])
            s.op("dve", lambda e: e.tensor_tensor(out=Mf[:], in0=OH[0][:], in1=OH[1][:], op=ALU.add), reads=["OH"], writes=["Mf"])
            s.op("dve", lambda e: e.tensor_copy(out=Mb[:], in_=Mf[:].rearrange("p i e -> p (i e)")), reads=["Mf"], writes=["Mb"])
            s.op("pe", lambda e: e.matmul(X.ps[0][:], lhsT=U[:], rhs=Mb[:], start=True, stop=True), reads=["U", "Mb"], writes=["ps0"])
            s.op("pe", lambda e: e.matmul(X.ps[1][:], lhsT=ones[:], rhs=Mb[:], start=True, stop=True), reads=["ones", "Mb"], writes=["ps1"])
            s.op("dve", lambda e: e.tensor_copy(out=CN[:].rearrange("p i e -> p (i e)"), in_=X.ps[1][:]), reads=["ps1"], writes=["CN"])
            s.op("dve", lambda e: e.tensor_copy(out=A_[:], in_=CN[:]), reads=["CN"], writes=["A"])
            src, dst, sres, dres = A_, B_, "A", "B"
            for sh in (1, 2, 4, 8, 16, 32):
                s.op("dve", lambda e: e.tensor_copy(out=dst[:, 0:sh, :], in_=src[:, 0:sh, :]), reads=[sres], writes=[dres])
                s.op("dve", lambda e: e.tensor_tensor(out=dst[:, sh:NT, :], in0=src[:, sh:NT, :], in1=src[:, 0:NT - sh, :],
                                                      op=ALU.add), reads=[sres], writes=[dres])
                src, dst, sres, dres = dst, src, dres, sres
            incl, ires = src, sres
            s.op("dve", lambda e: e.tensor_copy(out=nE[:, 0:8], in_=incl[:, NT - 1, :]), reads=[ires], writes=["nE"])
            s.op("dve", lambda e: e.tensor_tensor(out=cmp16[:], in0=bcast_mid(nE[:, 0:8], 16), in1=bcast_rep(gi512[:, 0:16], NE),
                                                  op=ALU.is_gt), reads=["nE", "gi512"], writes=["cmp16"])
            s.op("dve", lambda e: e.tensor_reduce(out=nE[:, 8:16], in_=cmp16[:], axis=AX.X, op=ALU.add), reads=["cmp16"], writes=["nE"])
            s.op("dve", lambda e: e.tensor_scalar(out=nE[:, 8:16], in0=nE[:, 8:16], scalar1=512.0, scalar2=None, op0=ALU.mult),
                 reads=["nE"], writes=["nE"])
            s.op("dve", lambda e: e.tensor_copy(out=nE[:, 16:17], in_=nE[:, 8:9]), reads=["nE"], writes=["nE"])
            for e_ in range(1, NE):
                s.op("dve", lambda e: e.tensor_tensor(out=nE[:, 16 + e_:17 + e_], in0=nE[:, 15 + e_:16 + e_], in1=nE[:, 8 + e_:9 + e_],
                                                      op=ALU.add), reads=["nE"], writes=["nE"])
            s.op("dve", lambda e: e.tensor_tensor(out=nE[:, 24:32], in0=nE[:, 16:24], in1=nE[:, 8:16], op=ALU.subtract),
                 reads=["nE"], writes=["nE"])
            s.op("dve", lambda e: e.tensor_tensor(out=POS[:], in0=incl[:], in1=CN[:], op=ALU.subtract), reads=[ires, "CN"], writes=["POS"])
            s.op("dve", lambda e: e.tensor_tensor(out=POS[:].rearrange("p i e -> p (i e)"), in0=POS[:].rearrange("p i e -> p (i e)"),
                                                  in1=X.ps[0][:], op=ALU.add), reads=["ps0"], writes=["POS"])
            s.op("dve", lambda e: e.tensor_tensor(out=POS[:], in0=POS[:], in1=bcast_rep(nE[:, 24:32], NT), op=ALU.add),
                 reads=["nE"], writes=["POS"])
            for k in range(2):
                s.op("dve", lambda e: e.tensor_tensor(out=Mf[:], in0=POS[:], in1=OH[k][:], op=ALU.mult), reads=["POS", "OH"], writes=["Mf"])
                s.op("dve", lambda e: e.tensor_reduce(out=PK[:, k, :], in_=Mf[:], axis=AX.X, op=ALU.add), reads=["Mf"], writes=["PK"])
                s.op("dve", lambda e: e.tensor_copy(out=ROWS[:, k, :, 0], in_=tokid[:]), reads=["tokid"], writes=["ROWS"])
                s.op("dve", lambda e: e.tensor_copy(out=ROWS[:, k, :, 1], in_=WK[k][:].bitcast(I32)), reads=["WK"], writes=["ROWS"])
            s.op("dve", lambda e: e.tensor_copy(out=POSI, in_=PK[:]), reads=["PK"], writes=["POSI"])
            s.op("dve", lambda e: e.tensor_tensor(out=cmpg[:], in0=bcast_rep(nE[:, 16:24], NGRP), in1=bcast_mid(gi512[:], NE),
                                                  op=ALU.is_le), reads=["nE", "gi512"], writes=["cmpg"])
            s.op("dve", lambda e: e.tensor_reduce(out=EG[:], in_=cmpg[:], axis=AX.X, op=ALU.add), reads=["cmpg"], writes=["EG"])
            s.op("dve", lambda e: e.tensor_scalar(out=EG[:], in0=EG[:], scalar1=float(NE - 1), scalar2=896.0, op0=ALU.min, op1=ALU.mult),
                 reads=["EG"], writes=["EG"])
            s.op("dve", lambda e: e.tensor_tensor(out=WF[:], in0=bcast_mid(EG[:], 7), in1=bcast_rep(blkp[:], NGRP), op=ALU.add),
                 reads=["EG", "blkp"], writes=["WF"])
            s.op("dve", lambda e: e.tensor_copy(out=WIDX, in_=WF[:]), reads=["WF"], writes=["WIDX"])
            for ti in range(NT):
                for k in range(2):
                    s.idma(X.SLOT[:, :], POSI2[:, k * NT + ti:k * NT + ti + 1],
                           ROWS2[:, (k * NT + ti) * 2:(k * NT + ti) * 2 + 2], None, NSLOT - 1,
                           reads=["ROWS", "POSI"], writes=["SLOT"])
            s.barrier()
        with ExitStack() as es2:
            sb2 = lambda name, shape, dt: es2.enter_context(nc.sbuf_tensor("%s_L%d" % (name, L), shape, dt))
            stt = [sb2("mc_st%d" % i, [128, 8], I32) for i in range(2)]
            xg = [sb2("mc_xg%d" % i, [128, D], BF16) for i in range(8)]
            acc = [sb2("mc_acc%d" % i, [128, 4, D], F32) for i in range(2)]
            wg = [sb2("mc_wg%d" % i, [128, 8, 512], BF16) for i in range(3)]
            wu = [sb2("mc_wu%d" % i, [128, 8, 512], BF16) for i in range(3)]
            wd = [sb2("mc_wd%d" % i, [128, 4, D], BF16) for i in range(3)]
            silu_t = [sb2("mc_silu%d" % i, [128, 512], BF16) for i in range(2)]
            actb = [sb2("mc_actb%d" % i, [128, 4, 512], BF16) for i in range(2)]
            hn = [sb2("mc_hn%d" % i, [128, 8, 512], BF16) for i in range(2)]
            NWB = len(wg)

            def prologue_loads(gi):
                gb = gi % 2
                s.dma("sp", stt[gb][:].rearrange("p (tt c) -> p tt c", c=2),
                      X.SLOT[gi * 512:(gi + 1) * 512, :].rearrange("(tt p) c -> p tt c", p=128),
                      reads=["SLOT"], writes=["st%d" % gb])
                for tt in range(4):
                    s.idma(xg[gb * 4 + tt][:, :], None, X.XS2[:, :], stt[gb][:, 2 * tt:2 * tt + 1], SEQ - 1,
                           reads=["XS2", "st%d" % gb], writes=["xg%d" % (gb * 4 + tt)])

            def prologue_tr(gi):
                gb = gi % 2
                for tt in range(4):
                    ln_b(s, X, tt, g2T[:], "g2T", hn[gb][:, :, tt * 128:(tt + 1) * 128], "hn%d" % gb,
                         src=xg[gb * 4 + tt], src_res="xg%d" % (gb * 4 + tt))

            def wload(j):
                gi, blk = divmod(j, 7)
                wb = j % NWB
                off = WIDX2[:, gi * 7 + blk:gi * 7 + blk + 1]
                s.idma(wg[wb][:].rearrange("p k f -> p (k f)"), None, X.WGB[:, :], off, 0, reads=["W16", "WIDX"], writes=["wg%d" % wb])
                s.idma(wu[wb][:].rearrange("p k f -> p (k f)"), None, X.WUB[:, :], off, 0, reads=["W16", "WIDX"], writes=["wu%d" % wb])
                s.idma(wd[wb][:].rearrange("p c n -> p (c n)"), None, X.WDB[:, :], off, 0, reads=["W16", "WIDX"], writes=["wd%d" % wb])

            def stage_a(j):
                gi, blk = divmod(j, 7)
                gb, wb, ab = gi % 2, j % NWB, j % 2
                for fc in range(4):
                    pg, pu = 2 * (fc % 2), 2 * (fc % 2) + 1
                    for k in range(8):
                        s.op("pe", lambda e: e.matmul(X.ps[pg][:], lhsT=wg[wb][:, k, fc * 128:(fc + 1) * 128], rhs=hn[gb][:, k, :],
                                                      start=(k == 0), stop=(k == 7)),
                             reads=["wg%d" % wb, "hn%d" % gb], writes=["ps%d" % pg])
                    for k in range(8):
                        s.op("pe", lambda e: e.matmul(X.ps[pu][:], lhsT=wu[wb][:, k, fc * 128:(fc + 1) * 128], rhs=hn[gb][:, k, :],
                                                      start=(k == 0), stop=(k == 7)),
                             reads=["wu%d" % wb, "hn%d" % gb], writes=["ps%d" % pu])
                    s.op("act", lambda e: e.activation(out=silu_t[fc % 2][:], in_=X.ps[pg][:], func=AF.Silu),
                         reads=["ps%d" % pg], writes=["silu%d" % (fc % 2)])
                    s.op("dve", lambda e: e.tensor_tensor(out=actb[ab][:, fc, :], in0=X.ps[pu][:], in1=silu_t[fc % 2][:],
                                                          op=ALU.mult),
                         reads=["ps%d" % pu, "silu%d" % (fc % 2)], writes=["actb%d" % ab])

            def stage_b(j):
                gi, blk = divmod(j, 7)
                gb, wb, ab = gi % 2, j % NWB, j % 2
                for tt in range(4):
                    gsc = stt[gb][:, 2 * tt + 1:2 * tt + 2].bitcast(F32)
                    for half in range(2):
                        hs = slice(half * 512, (half + 1) * 512)
                        pd = 4 + half
                        for fc in range(4):
                            s.op("pe", lambda e: e.matmul(X.ps[pd][:], lhsT=actb[ab][:, fc, tt * 128:(tt + 1) * 128],
                                                          rhs=wd[wb][:, fc, hs], start=(fc == 0), stop=(fc == 3)),
                                 reads=["actb%d" % ab, "wd%d" % wb], writes=["ps%d" % pd])
                        dst = acc[gb][:, tt, hs]
                        if blk == 0:
                            s.op("dve", lambda e: e.tensor_scalar(out=dst, in0=X.ps[pd][:], scalar1=gsc, scalar2=None, op0=ALU.mult),
                                 reads=["ps%d" % pd, "st%d" % gb], writes=["acc%d_%d" % (gb, tt)])
                        else:
                            s.op("dve", lambda e: e.scalar_tensor_tensor(out=dst, in0=X.ps[pd][:], scalar=gsc, in1=dst,
                                                                         op0=ALU.mult, op1=ALU.add),
                                 reads=["ps%d" % pd, "st%d" % gb], writes=["acc%d_%d" % (gb, tt)])
                if blk == 6:
                    s.dma("sp", X.YS[gi * 512:(gi + 1) * 512, :].rearrange("(tt p) n -> p tt n", p=128), acc[gb][:],
                          reads=["acc%d_%d" % (gb, tt) for tt in range(4)], writes=["YS"])

            NJ = NGRP * 7
            prologue_loads(0)
            prologue_tr(0)
            for j in range(min(NWB - 1, NJ)):
                wload(j)
            for j in range(NJ + 1):
                if j < NJ:
                    gi, blk = divmod(j, 7)
                    stage_a(j)
                    if blk == 6 and gi + 1 < NGRP:
                        prologue_tr(gi + 1)
                if j >= 1:
                    stage_b(j - 1)
                if j < NJ and j % 7 == 0 and j // 7 + 1 < NGRP:
                    prologue_loads(j // 7 + 1)
                if j + NWB - 1 < NJ:
                    wload(j + NWB - 1)
            s.barrier()
        with ExitStack() as es2:
            sb2 = lambda name, shape, dt: es2.enter_context(nc.sbuf_tensor("%s_L%d" % (name, L), shape, dt))
            pgate = sb2("md_pgate", [128, 8, D], BF16)
            pproj = sb2("md_pproj", [128, 2, D], BF16)
            h1 = [sb2("md_h1%d" % i, [128, 4, D], F32) for i in range(2)]
            acc = [sb2("md_acc%d" % i, [128, 4, D], F32) for i in range(2)]
            hnd = [sb2("md_hnd%d" % i, [128, 8, 512], BF16) for i in range(2)]
            yA = [sb2("md_yA%d" % i, [128, D], F32) for i in range(2)]
            yB = [sb2("md_yB%d" % i, [128, D], F32) for i in range(2)]
            sig = [sb2("md_sig%d" % i, [128, 512], F32) for i in range(2)]
            ptile = [sb2("md_pt%d" % i, [128, 256], F32) for i in range(2)]
            pb = sb2("md_pb", [128, 256], BF16)
            pT = [sb2("md_pT%d" % i, [128, 2, 512], BF16) for i in range(2)]
            for kc in range(8):
                s.dma("pool", pgate[:, kc, :], X.ple_gate[L, kc * 128:(kc + 1) * 128, :], writes=["pgate"])
            for kc in range(2):
                s.dma("pool", pproj[:, kc, :], X.ple_proj[L, kc * 128:(kc + 1) * 128, :], writes=["pproj"])
            if last:
                gfin = sb2("md_gfin", [128, D], F32)
                ones1 = sb2("md_ones1", [1, 128], F32)
                gf1 = sb2("md_gf1", [1, D], F32)
                s.dma("sp", gf1[:], X.g_final.rearrange("(o n) -> o n", o=1), writes=["gf1"])
                s.op("dve", lambda e: e.memset(ones1[:], 1.0), writes=["ones1"])
                for half in range(2):
                    s.op("pe", lambda e: e.matmul(X.ps[6][:], lhsT=ones1[:], rhs=gf1[:, half * 512:(half + 1) * 512],
                                                  start=True, stop=True), reads=["ones1", "gf1"], writes=["ps6"])
                    s.op("dve", lambda e: e.tensor_copy(out=gfin[:, half * 512:(half + 1) * 512], in_=X.ps[6][:]),
                         reads=["ps6"], writes=["gfin"])
            def stage1(tg):
                hb = tg % 2
                for tt in range(4):
                    ti = 4 * tg + tt
                    yb_ = ti % 2
                    hres = "h1_%d_%d" % (hb, tt)
                    s.dma("sp", h1[hb][:, tt, :], X.H1[ti * 128:(ti + 1) * 128, :], reads=["H1"], writes=[hres])
                    s.idma(yA[yb_][:, :], None, X.YS[:, :], POSI2[:, ti:ti + 1], 0, reads=["YS", "POSI"], writes=["yA%d" % yb_])
                    s.idma(yB[yb_][:, :], None, X.YS[:, :], POSI2[:, NT + ti:NT + ti + 1], 0, reads=["YS", "POSI"], writes=["yB%d" % yb_])
                    s.op("dve", lambda e: e.tensor_tensor(out=h1[hb][:, tt, :], in0=h1[hb][:, tt, :], in1=yA[yb_][:], op=ALU.add),
                         reads=["yA%d" % yb_], writes=[hres])
                    s.op("dve", lambda e: e.tensor_tensor(out=h1[hb][:, tt, :], in0=h1[hb][:, tt, :], in1=yB[yb_][:], op=ALU.add),
                         reads=["yB%d" % yb_], writes=[hres])
                    ln_a(s, X, h1[hb][:, tt, :], hres, tt)
                for tt in range(4):
                    ln_b(s, X, tt, g3T[:], "g3T", hnd[hb][:, :, tt * 128:(tt + 1) * 128], "hnd%d" % hb)
                psb = X.ps[7][:].bitcast(BF16)
                for tt in range(4):
                    ti = 4 * tg + tt
                    pt_ = ptile[ti % 2]
                    s.dma("sp", pt_[:], X.p[L, ti * 128:(ti + 1) * 128, :], writes=["pt%d" % (ti % 2)])
                    s.op("act", lambda e: e.activation(out=pb[:], in_=pt_[:], func=AF.Copy), reads=["pt%d" % (ti % 2)], writes=["pb"])
                    for k in range(2):
                        s.op("pe", lambda e: e.transpose(out=psb[:, k * 128:(k + 1) * 128], in_=pb[:, k * 128:(k + 1) * 128],
                                                         identity=X.ident[:]), reads=["pb", "ident"], writes=["ps7"])
                    s.op("act", lambda e: e.activation(out=pT[hb][:, :, tt * 128:(tt + 1) * 128],
                                                       in_=psb[:, 0:256].rearrange("p (k t) -> p k t", k=2), func=AF.Copy),
                         reads=["ps7"], writes=["pT%d" % hb])

            def stage2(tg):
                hb = tg % 2
                for tt in range(4):
                    ti = 4 * tg + tt
                    hres = "h1_%d_%d" % (hb, tt)
                    ares = "dacc%d_%d" % (hb, tt)
                    for half in range(2):
                        hs = slice(half * 512, (half + 1) * 512)
                        for k in range(2):
                            s.op("pe", lambda e: e.matmul(X.ps[6][:], lhsT=pT[hb][:, k, tt * 128:(tt + 1) * 128], rhs=pproj[:, k, hs],
                                                          start=(k == 0), stop=(k == 1)), reads=["pT%d" % hb, "pproj"], writes=["ps6"])
                        pgb = 4 + half
                        for k in range(8):
                            s.op("pe", lambda e: e.matmul(X.ps[pgb][:], lhsT=hnd[hb][:, k, tt * 128:(tt + 1) * 128], rhs=pgate[:, k, hs],
                                                          start=(k == 0), stop=(k == 7)), reads=["hnd%d" % hb, "pgate"], writes=["ps%d" % pgb])
                        s.op("act", lambda e: e.activation(out=sig[half][:], in_=X.ps[pgb][:], func=AF.Sigmoid),
                             reads=["ps%d" % pgb], writes=["sig%d" % half])
                        s.op("dve", lambda e: e.tensor_tensor(out=acc[hb][:, tt, hs], in0=X.ps[6][:], in1=sig[half][:], op=ALU.mult),
                             reads=["ps6", "sig%d" % half], writes=[ares])
                    s.op("pool", lambda e: e.tensor_tensor(out=h1[hb][:, tt, :], in0=h1[hb][:, tt, :], in1=acc[hb][:, tt, :], op=ALU.add),
                         reads=[ares], writes=[hres])
                    if last:
                        k4 = 4 + tt % 4 if len(X.ss) > 4 else tt
                        ln_a(s, X, h1[hb][:, tt, :], hres, k4)
                        s.op("act", lambda e: e.activation(out=acc[hb][:, tt, :], in_=h1[hb][:, tt, :], func=AF.Copy, scale=X.ss[k4][:, 2:3]),
                             reads=[hres, "ss%d" % k4], writes=[ares])
                        s.op("pool", lambda e: e.tensor_tensor(out=acc[hb][:, tt, :], in0=acc[hb][:, tt, :], in1=gfin[:], op=ALU.mult),
                             reads=["gfin"], writes=[ares])
                        s.dma("pool", h_out[ti * 128:(ti + 1) * 128, :], acc[hb][:, tt, :], reads=[ares], writes=["HOUT"])
                    else:
                        s.dma("pool", h_out[ti * 128:(ti + 1) * 128, :], h1[hb][:, tt, :], reads=[hres], writes=["HB"])

            stage1(0)
            for tg in range(NT // 4):
                if tg + 1 < NT // 4:
                    stage1(tg + 1)
                stage2(tg)
            s.barrier()
        s.barrier()
```
